# Optimizing a Trainium2 kernel written in Bass

```python
import jax, jax.numpy as jnp
from jax import lax
import numpy as np

D_MODEL = 1024
BATCH = 16
SEQ = 2048
DEPTH = 1

RWKV_HEAD_DIM = 64
RWKV_WIDTH = D_MODEL // 2
RWKV_HEADS = RWKV_WIDTH // RWKV_HEAD_DIM
W_LORA = 64
A_LORA = 64
G_LORA = 128
RWKV_GN_EPS = RWKV_HEAD_DIM * 1e-5
N_DIRECTIONS = 2
ATT_HEAD_DIM = 64
ATT_WIDTH = D_MODEL // 2
ATT_HEADS = ATT_WIDTH // ATT_HEAD_DIM
ATT_KV_HEADS = 2
ATT_GROUP = ATT_HEADS // ATT_KV_HEADS
KV_WIDTH = ATT_KV_HEADS * ATT_HEAD_DIM
WINDOW = 128
BLOCK = 128
ROPE_THETA = 500000.0
ROT_DIM = ATT_HEAD_DIM // 4
N_BRANCHES = 2
N_EXPERTS = 16
EXPERT_FF = D_MODEL
CAPACITY_FACTOR = 2
NORM_EPS = 1e-6

RWKV_COLS = 3 * RWKV_WIDTH + N_DIRECTIONS * W_LORA + N_DIRECTIONS * A_LORA + G_LORA
IN_COLS = RWKV_COLS + ATT_WIDTH + 2 * KV_WIDTH + N_BRANCHES * D_MODEL

kernel_name = 'hybrid_rwkv7_swa_ecmoe_block'


def rms_norm(x, g):
    xf = x.astype(jnp.float32)
    y = xf * lax.rsqrt(jnp.mean(xf * xf, axis=-1, keepdims=True) + NORM_EPS)
    return (y * g.astype(jnp.float32)).astype(x.dtype)


def centred_token_shift(z, mu_prev, mu_next):
    z_prev = jnp.pad(z[:, :-1], ((0, 0), (1, 0), (0, 0)))
    z_next = jnp.pad(z[:, 1:], ((0, 0), (0, 1), (0, 0)))
    return z + mu_prev * (z_prev - z) + mu_next * (z_next - z)


def rwkv7_scan(r, w, k, v, a, b, reverse):
    B, T, H, N = r.shape

    def step(S, inp):
        r_t, w_t, k_t, v_t, a_t, b_t = inp
        sa = jnp.einsum('bhij,bhj->bhi', S, a_t)
        S = (S * w_t[:, :, None, :] + sa[..., None] * b_t[:, :, None, :]
             + v_t[..., None] * k_t[:, :, None, :])
        y = jnp.einsum('bhij,bhj->bhi', S, r_t)
        return S, y

    xs = tuple(jnp.moveaxis(t, 1, 0) for t in (r, w, k, v, a, b))
    S0 = jnp.zeros((B, H, N, N), jnp.float32)
    _, y = lax.scan(step, S0, xs, reverse=reverse)
    return jnp.moveaxis(y, 0, 1)


def rwkv7_branch(z, w0, w2, a0, a2, g2, k_k, k_a, r_k, gn_w, gn_b):
    B, T, _ = z.shape
    H, N, RW = RWKV_HEADS, RWKV_HEAD_DIM, RWKV_WIDTH
    f32 = jnp.float32
    splits = [RW, 2 * RW, 3 * RW, 3 * RW + N_DIRECTIONS * W_LORA,
              3 * RW + N_DIRECTIONS * (W_LORA + A_LORA)]
    r, k, v, wd, ad, gd = jnp.split(z.astype(f32), splits, axis=-1)
    wd = wd.reshape(B, T, N_DIRECTIONS, W_LORA)
    ad = ad.reshape(B, T, N_DIRECTIONS, A_LORA)
    w = -jax.nn.softplus(-(w0.astype(f32) + jnp.einsum('btdl,dlc->btdc', jnp.tanh(wd), w2.astype(f32)))) - 0.5
    decay = jnp.exp(-jnp.exp(w))
    a = jax.nn.sigmoid(a0.astype(f32) + jnp.einsum('btdl,dlc->btdc', ad, a2.astype(f32)))
    g = jax.nn.sigmoid(gd) @ g2.astype(f32)

    def heads(t):
        return t.reshape(B, T, H, N)

    kk = heads(k * k_k.astype(f32))
    kk = kk / jnp.maximum(jnp.sqrt(jnp.sum(kk * kk, axis=-1, keepdims=True)), 1e-12)
    ys = []
    ks = []
    for d, rev in ((0, False), (1, True)):
        a_d = a[:, :, d]
        k_d = k * (1.0 + (a_d - 1.0) * k_a.astype(f32))
        ys.append(rwkv7_scan(heads(r), heads(decay[:, :, d]), heads(k_d), heads(v),
                             -kk, kk * heads(a_d), rev))
        ks.append(k_d)
    y = ys[0] + ys[1]
    mu = jnp.mean(y, axis=-1, keepdims=True)
    var = jnp.mean(jnp.square(y - mu), axis=-1, keepdims=True)
    y = ((y - mu) * lax.rsqrt(var + RWKV_GN_EPS)).reshape(B, T, RW) * gn_w.astype(f32) + gn_b.astype(f32)
    bonus = jnp.sum(heads(r) * heads(ks[0] + ks[1]) * r_k.astype(f32), axis=-1, keepdims=True) * heads(v)
    y = (y + bonus.reshape(B, T, RW)) * g
    return y.astype(z.dtype)


def partial_rope(x, cos, sin):
    half = ROT_DIM // 2
    x1 = x[..., :half]
    x2 = x[..., half:ROT_DIM]
    return jnp.concatenate([x1 * cos - x2 * sin, x2 * cos + x1 * sin, x[..., ROT_DIM:]], axis=-1)


def window_attention(q, k, v, sink):
    B, T, _, hd = q.shape
    nb = T // BLOCK
    qb = q.reshape(B, nb, BLOCK, ATT_KV_HEADS, ATT_GROUP, hd)

    def band(t):
        tp = jnp.pad(t, ((0, 0), (BLOCK, BLOCK), (0, 0), (0, 0))).reshape(B, nb + 2, BLOCK, ATT_KV_HEADS, hd)
        return jnp.concatenate([tp[:, :-2], tp[:, 1:-1], tp[:, 2:]], axis=2)

    kb = band(k)
    vb = band(v)
    s = jnp.einsum('bnqkgd,bnskd->bnkgqs', qb, kb).astype(jnp.float32) * (hd ** -0.5)
    blk = jnp.arange(nb)[:, None, None] * BLOCK
    qpos = blk + jnp.arange(BLOCK)[None, :, None]
    kpos = blk - BLOCK + jnp.arange(3 * BLOCK)[None, None, :]
    valid = (jnp.abs(kpos - qpos) <= WINDOW) & (kpos >= 0) & (kpos < T)
    s = jnp.where(valid[None, :, None, None], s, -jnp.inf)
    sk = sink.astype(jnp.float32).reshape(1, 1, ATT_KV_HEADS, ATT_GROUP, 1, 1)
    m = jnp.maximum(jnp.max(s, axis=-1, keepdims=True), sk)
    p = jnp.exp(s - m)
    denom = jnp.sum(p, axis=-1, keepdims=True) + jnp.exp(sk - m)
    o = jnp.einsum('bnkgqs,bnskd->bnqkgd', (p / denom).astype(v.dtype), vb)
    return o.reshape(B, T, ATT_HEADS * hd)


def expert_choice_moe(u, w_router, w_gate, w_up, w_down):
    B, T, D = u.shape
    cap = CAPACITY_FACTOR * T // N_EXPERTS
    aff = jax.nn.softmax(jnp.einsum('btd,de->bte', u, w_router).astype(jnp.float32), axis=-1)
    top_aff, top_idx = lax.top_k(jnp.swapaxes(aff, 1, 2), cap)
    bidx = jnp.arange(B)[:, None, None]
    xe = u[bidx, top_idx]
    h = jax.nn.silu(jnp.einsum('becd,edf->becf', xe, w_gate)) * jnp.einsum('becd,edf->becf', xe, w_up)
    ye = jnp.einsum('becf,efd->becd', h, w_down) * top_aff[..., None].astype(u.dtype)
    return jnp.zeros_like(u).at[bidx, top_idx].add(ye)


def setup_inputs(seed: int = 0) -> dict:
    key = jax.random.key(seed)
    ks = jax.random.split(key, 32)
    f32 = jnp.float32
    L, D, RW, E, F = DEPTH, D_MODEL, RWKV_WIDTH, N_EXPERTS, EXPERT_FF

    def nrm(k, shape, scale):
        return jax.random.normal(k, shape, f32) * scale

    return {
        'x': nrm(ks[0], (BATCH, SEQ, D), 1.0),
        'c': nrm(ks[1], (BATCH, D), 1.0),
        'positions': jnp.broadcast_to(jnp.arange(SEQ, dtype=jnp.int32)[None, :], (BATCH, SEQ)),
        'w_ada': nrm(ks[2], (L, D, 6 * D), 0.5 * D ** -0.5),
        'b_ada': nrm(ks[3], (L, 6 * D), 0.02),
        'norm1_g': 1.0 + nrm(ks[4], (L, D), 0.05),
        'w_in': nrm(ks[5], (L, D, IN_COLS), D ** -0.5),
        'mu_prev': jax.random.uniform(ks[6], (L, RWKV_COLS), f32, 0.0, 0.5),
        'mu_next': jax.random.uniform(ks[7], (L, RWKV_COLS), f32, 0.0, 0.5),
        'rwkv_w0': -2.0 + nrm(ks[8], (L, N_DIRECTIONS, RW), 1.0),
        'rwkv_w2': nrm(ks[9], (L, N_DIRECTIONS, W_LORA, RW), 0.5 * W_LORA ** -0.5),
        'rwkv_a0': nrm(ks[10], (L, N_DIRECTIONS, RW), 0.1),
        'rwkv_a2': nrm(ks[11], (L, N_DIRECTIONS, A_LORA, RW), 0.5 * A_LORA ** -0.5),
        'rwkv_g2': nrm(ks[12], (L, G_LORA, RW), G_LORA ** -0.5),
        'rwkv_k_k': 0.85 + nrm(ks[13], (L, RW), 0.05),
        'rwkv_k_a': 1.0 + nrm(ks[14], (L, RW), 0.05),
        'rwkv_r_k': nrm(ks[15], (L, RWKV_HEADS, RWKV_HEAD_DIM), 0.1),
        'rwkv_gn_w': 1.0 + nrm(ks[16], (L, RW), 0.05),
        'rwkv_gn_b': nrm(ks[17], (L, RW), 0.02),
        'q_norm_g': 1.0 + nrm(ks[18], (L, ATT_HEAD_DIM), 0.05),
        'k_norm_g': 1.0 + nrm(ks[19], (L, ATT_HEAD_DIM), 0.05),
        'attn_sink': nrm(ks[20], (L, ATT_HEADS), 1.0),
        'p_rwkv': nrm(ks[21], (L, RW, D), RW ** -0.5),
        'p_attn': nrm(ks[22], (L, ATT_WIDTH, D), ATT_WIDTH ** -0.5),
        'w_out': nrm(ks[23], (L, D, D), D ** -0.5),
        'norm2_g': 1.0 + nrm(ks[24], (L, D), 0.05),
        'w_router': nrm(ks[25], (L, D, E), D ** -0.5),
        'w_gate': nrm(ks[26], (L, E, D, F), D ** -0.5),
        'w_up': nrm(ks[27], (L, E, D, F), D ** -0.5),
        'w_down': nrm(ks[28], (L, E, F, D), F ** -0.5),
    }


def reference(x, c, positions, w_ada, b_ada, norm1_g, w_in, mu_prev, mu_next,
              rwkv_w0, rwkv_w2, rwkv_a0, rwkv_a2, rwkv_g2, rwkv_k_k, rwkv_k_a, rwkv_r_k,
              rwkv_gn_w, rwkv_gn_b, q_norm_g, k_norm_g, attn_sink, p_rwkv, p_attn, w_out,
              norm2_g, w_router, w_gate, w_up, w_down):
    B, T, D = x.shape
    hd = ATT_HEAD_DIM
    inv_freq = ROPE_THETA ** (-jnp.arange(0, ROT_DIM, 2, dtype=jnp.float32) / ROT_DIM)
    ang = positions.astype(jnp.float32)[..., None] * inv_freq
    cos = jnp.cos(ang)[:, :, None, :].astype(x.dtype)
    sin = jnp.sin(ang)[:, :, None, :].astype(x.dtype)
    col_splits = [RWKV_COLS, RWKV_COLS + ATT_WIDTH, RWKV_COLS + ATT_WIDTH + KV_WIDTH,
                  RWKV_COLS + ATT_WIDTH + 2 * KV_WIDTH]
    c_act = jax.nn.silu(c)

    for l in range(DEPTH):
        mod = jnp.einsum('bd,de->be', c_act, w_ada[l]) + b_ada[l]
        sh1, sc1, gt1, sh2, sc2, gt2 = jnp.split(mod[:, None, :], 6, axis=-1)

        u = rms_norm(x, norm1_g[l]) * (1.0 + sc1) + sh1
        z = u @ w_in[l]
        z_rwkv, q, k, v, gate_logits = jnp.split(z, col_splits, axis=-1)

        z_rwkv = centred_token_shift(z_rwkv, mu_prev[l], mu_next[l])
        y_a = rwkv7_branch(z_rwkv, rwkv_w0[l], rwkv_w2[l], rwkv_a0[l], rwkv_a2[l], rwkv_g2[l],
                           rwkv_k_k[l], rwkv_k_a[l], rwkv_r_k[l], rwkv_gn_w[l], rwkv_gn_b[l]) @ p_rwkv[l]

        q = partial_rope(rms_norm(q.reshape(B, T, ATT_HEADS, hd), q_norm_g[l]), cos, sin)
        k = partial_rope(rms_norm(k.reshape(B, T, ATT_KV_HEADS, hd), k_norm_g[l]), cos, sin)
        v = v.reshape(B, T, ATT_KV_HEADS, hd)
        y_b = window_attention(q, k, v, attn_sink[l]) @ p_attn[l]

        g_a, g_b = jnp.split(jax.nn.sigmoid(gate_logits), N_BRANCHES, axis=-1)
        x = x + gt1 * ((g_a * y_a + g_b * y_b) @ w_out[l])

        u2 = rms_norm(x, norm2_g[l]) * (1.0 + sc2) + sh2
        x = x + gt2 * expert_choice_moe(u2, w_router[l], w_gate[l], w_up[l], w_down[l])
    return x
```

```python
import numpy as np
import concourse.bass as bass
import concourse.mybir as mybir
from concourse.bass_utils import run_bass_kernel_spmd

F32 = mybir.dt.float32
BF16 = mybir.dt.bfloat16
I32 = mybir.dt.int32
ALU = mybir.AluOpType
AF = mybir.ActivationFunctionType
AX = mybir.AxisListType

T = 2048
D = 1024
NT = 16
NB = 4
NSEQ = 2
NCORES = 8
E = 16
CAP = 256
LAM = float(np.exp(-0.5))
NCH = 4
TBS = NCH * 64
NTB = T // TBS
TWO_PI = float(2 * np.pi)
C1 = 6.28125
C2 = TWO_PI - C1

PP = {}
_o = 0
for _n, _w in [("mp", 15), ("mn", 15), ("w0", 8), ("a0", 8), ("kk", 4), ("ka", 4), ("rk", 4), ("qg", 1), ("kg", 1),
               ("invf", 1), ("sink", 4)]:
    PP[_n] = (_o, _w)
    _o += _w
NPP = _o


class Ticket:
    __slots__ = ('ins', 'sem', 'val', 'parent')

    def __init__(self, ins):
        self.ins = ins
        self.sem = None
        self.val = None
        self.parent = None

    def root(self):
        t = self
        while t.parent is not None:
            t = t.parent
        return t


class Sync:
    SEM_MAX = 30000

    def __init__(self, nc):
        self.nc = nc
        self.E = {'pe': nc.tensor, 'act': nc.scalar, 'dve': nc.vector, 'pool': nc.gpsimd, 'sp': nc.sync}
        self.sem = {}
        self.cnt = {}
        self.nsem = 0
        for e in self.E:
            self._newsem(e)
        self.waited = {}
        self.lastw = {}
        self.reads = {}
        self.dma_sems = {}
        self.dma_rr = {}
        self.ninstr = 0
        self.pend = None
        self.pend_writes = None
        self.npe_inc = 0

    def _newsem(self, e):
        self.sem[e] = self.nc.alloc_semaphore(f"s_{e}_{self.nsem}")
        self.nsem += 1
        self.cnt[e] = 0

    def _flush_pe(self):
        t = self.pend
        if t is None:
            return
        if self.cnt['pe'] >= self.SEM_MAX:
            self._newsem('pe')
        self.cnt['pe'] += 1
        t.sem = self.sem['pe']
        t.val = self.cnt['pe']
        t.ins.then_inc(t.sem, 1)
        self.npe_inc += 1
        self.pend = None
        self.pend_writes = None

    def _wait(self, e, ev):
        if ev is None:
            return
        if isinstance(ev, Ticket):
            if e == 'pe':
                return
            t = ev.root()
            if t.val is None:
                assert t is self.pend
                self._flush_pe()
            sem, val = t.sem, t.val
        else:
            src, sem, val = ev
        k = (e, sem.name)
        if self.waited.get(k, 0) >= val:
            return
        self.waited[k] = val
        self.E[e].wait_ge(sem, val)

    def deps(self, e, reads, writes, pe_acc=False):
        for k in reads:
            self._wait(e, self.lastw.get(k))
        for k in writes:
            lw = self.lastw.get(k)
            if not (pe_acc and isinstance(lw, Ticket)):
                self._wait(e, lw)
            for ev in self.reads.get(k, {}).values():
                self._wait(e, ev)

    def commit(self, src, ev, reads, writes):
        for k in reads:
            self.reads.setdefault(k, {})[src] = ev
        for k in writes:
            self.lastw[k] = ev
            self.reads[k] = {}

    def op(self, e, fn, reads=(), writes=(), pe_acc=False):
        self.deps(e, reads, writes, pe_acc)
        if e == 'pe':
            ins = fn()
            t = Ticket(ins)
            if self.pend is not None:
                if self.pend_writes == tuple(writes):
                    self.pend.parent = t
                    self.pend = None
                else:
                    self._flush_pe()
            self.pend = t
            self.pend_writes = tuple(writes)
            self.commit('pe', t, reads, writes)
            self.ninstr += 1
            return t
        if self.cnt[e] >= self.SEM_MAX:
            self._newsem(e)
        ins = fn()
        self.cnt[e] += 1
        ev = (e, self.sem[e], self.cnt[e])
        ins.then_inc(self.sem[e], 1)
        self.commit(e, ev, reads, writes)
        self.ninstr += 1
        return ev

    def dma(self, e, out, in_, reads=(), writes=(), nslots=8, **kw):
        if e == 'pool':
            nslots = 2
        lst = self.dma_sems.setdefault(e, [])
        if len(lst) < nslots:
            lst.append([self.nc.alloc_semaphore(f"d_{e}_{len(lst)}"), 0])
        i = self.dma_rr.get(e, 0)
        self.dma_rr[e] = (i + 1) % nslots
        slot = lst[i % len(lst)]
        sem, uses = slot
        if uses > 0:
            self._wait(e, ('dma', sem, 16 * uses))
        self.deps(e, reads, writes)
        self.E[e].dma_start(out=out, in_=in_, **kw).then_inc(sem, 16)
        slot[1] = uses + 1
        ev = ('dma_%s_%d' % (e, i % len(lst)), sem, 16 * (uses + 1))
        self.commit(ev[0], ev, reads, writes)
        self.ninstr += 1
        return ev

    def barrier(self):
        self._flush_pe()
        evs = [(e, self.sem[e], self.cnt[e]) for e in self.E if self.cnt[e] > 0]
        for q, lst in self.dma_sems.items():
            for sem, uses in lst:
                if uses:
                    evs.append(('dma', sem, 16 * uses))
        for e in self.E:
            for ev in evs:
                if ev[0] != e:
                    self._wait(e, ev)
        self.lastw = {}
        self.reads = {}

    def finish(self, e='sp'):
        self._flush_pe()
        for q, lst in self.dma_sems.items():
            for sem, uses in lst:
                if uses:
                    self._wait(e, ('dma', sem, 16 * uses))


class Arena:
    def __init__(self, nc, name, nbytes):
        self.n4 = nbytes // 4
        self.t = nc.alloc_sbuf_tensor(name, [128, self.n4], F32).ap()
        self.ptr = 0
        self.hi = 0

    def seek(self, off):
        self.ptr = off

    def alloc(self, shape, dtype, parts=None):
        esz = 4 if dtype in (F32, I32) else 2
        n = int(np.prod(shape[1:]))
        nb = (n * esz + 31) // 32 * 32
        assert self.ptr % 4 == 0
        a = self.ptr // 4
        assert a + nb // 4 <= self.n4, f"arena overflow {self.ptr}+{nb} > {self.n4 * 4}"
        v = self.t[:, a:a + nb // 4]
        if dtype != F32:
            v = v.bitcast(dtype)
        v = v[0:shape[0], 0:n]
        if len(shape) > 2:
            names = " ".join(f"d{i}" for i in range(len(shape) - 1))
            kw = {f"d{i}": int(shape[i + 1]) for i in range(len(shape) - 1)}
            v = v.rearrange(f"p ({names}) -> p {names}", **kw)
        self.ptr += nb
        self.hi = max(self.hi, self.ptr)
        return v


def bc(ap, shape):
    return ap.to_broadcast(list(shape))


def build(nseq=NSEQ, dbg=None, stop_after=None):
    nc = bass.Bass("TRN2", target_bir_lowering=False)
    S = Sync(nc)
    V, ACT, POOL, PE = nc.vector, nc.scalar, nc.gpsimd, nc.tensor

    def din(name, shape, dt=F32):
        return nc.dram_tensor(name, list(shape), dt, kind="ExternalInput").ap()

    x_d = din("x", [nseq, T, D])
    cT_d = din("cT", [nseq, 128, 8])
    pos_d = din("pos", [nseq, 1, T], I32)
    wada_d = din("w_ada", [D, 6 * D])
    bada_d = din("b_ada", [1, 6 * D])
    n1g_d = din("norm1_g", [1, D])
    n2g_d = din("norm2_g", [1, D])
    win_d = din("w_in", [D, 4736])
    pp_d = din("pp", [128, NPP])
    w2c_d = din("w2cat", [128, 2 * 512])
    a2c_d = din("a2cat", [128, 2 * 512])
    g2_d = din("g2", [128, 512])
    gnw_d = din("gn_w", [1, 512])
    gnb_d = din("gn_b", [1, 512])
    prw_d = din("p_rwkv", [512, D])
    pat_d = din("p_attn", [512, D])
    wout_d = din("w_out", [D, D])
    wr_d = din("w_router", [D, E])
    wg_d = din("w_gate", [E, D, D])
    wu_d = din("w_up", [E, D, D])
    wd_d = din("w_down", [E, D, D])
    cm_d = din("cmats", [128, 13 * 128])
    out_d = nc.dram_tensor("out", [nseq, T, D], F32, kind="ExternalOutput").ap()
    mod_d = nc.dram_tensor("modscr", [nseq, 6, 128, D], F32, kind="Internal").ap()
    y_d = nc.dram_tensor("yscr", [2, T, 512], F32, kind="Internal").ap()
    u_d = nc.dram_tensor("uscr", [128, 8, T], BF16, kind="Internal").ap()
    dbg_d = None
    if dbg is not None:
        dbg_d = nc.dram_tensor("dbg", list(dbg[1]), F32, kind="ExternalOutput").ap()

    def sb(name, shape, dt=F32):
        return nc.alloc_sbuf_tensor('sb_' + name, list(shape), dt).ap()

    pp = sb("pp", [128, NPP])
    ident = sb("ident", [128, 128], BF16)
    identf = sb("identf", [128, 128])
    cmb = sb("cmb", [128, 13, 128], BF16)
    w2c = sb("w2c", [128, 2, 512], BF16)
    a2c = sb("a2c", [128, 2, 512], BF16)
    g2 = sb("g2", [128, 512], BF16)
    wr = sb("wr", [128, 8, E], BF16)
    epsc = sb("epsc", [128, 4])
    alpha = sb("alpha", [128, 15])
    oneminus_ka = sb("omka", [128, 4])
    two_omka = sb("omka2", [128, 4])
    negkkc = sb("negone", [128, 1])
    esk = sb("esk", [128, 4])
    rmask = sb("rmask", [128, TBS])
    iota_row = sb("iota_row", [128, CAP])
    ident4 = sb("ident4", [128, 4, 128], BF16)
    kar = sb("kar", [128, 4])
    c2r = sb("c2r", [128, 4])
    afftm = sb("afftm", [128, NT, E])
    slot_tm = sb("slot_tm", [128, NT, E])
    affhl = sb("affhl", [128, NT, E, 2], BF16)

    BLK1, ROT, MPREV, MNEXT = 0, 1, 2, 3
    MZT = (4, 5)
    MZ = (6, 7)
    HSEL = 8
    VP = (9, 10)

    ps = [nc.alloc_psum_tensor(f"ps{i}", [128, 512], F32).ap() for i in range(8)]
    psk = [f"ps{i}" for i in range(8)]

    AR = Arena(nc, "arena", 192 * 1024)

    def col(name, j=0, n=1):
        o, w = PP[name]
        return pp[:, o + j:o + j + n]

    S.dma('sp', pp, pp_d, writes=['pp'])
    S.dma('pool', cmb.rearrange("p a b -> p (a b)"), cm_d, writes=['cmb'])
    S.dma('pool', w2c.rearrange("p a b -> p (a b)"), w2c_d, writes=['w2c'])
    S.dma('pool', a2c.rearrange("p a b -> p (a b)"), a2c_d, writes=['a2c'])
    S.dma('pool', g2, g2_d, writes=['g2'])
    S.dma('pool', wr, wr_d.rearrange("(k p) e -> p k e", p=128), writes=['wr'])
    S.op('pool', lambda: POOL.memset(identf, 1.0), writes=['identf'])
    S.op('pool', lambda: POOL.affine_select(out=identf, in_=identf, pattern=[[1, 128]], compare_op=ALU.is_equal,
                                            fill=0.0, base=0, channel_multiplier=-1), reads=['identf'], writes=['identf'])
    S.op('dve', lambda: V.tensor_copy(out=ident, in_=identf), reads=['identf'], writes=['ident'])
    for j in range(4):
        S.op('dve', lambda: V.tensor_copy(out=ident4[:, j, :], in_=identf), reads=['identf'], writes=['ident4'])
    S.op('pool', lambda: POOL.memset(epsc[:, 0:1], 1e-6), writes=['epsc'])
    S.op('pool', lambda: POOL.memset(epsc[:, 1:2], 64e-5), reads=['epsc'], writes=['epsc'])
    S.op('pool', lambda: POOL.memset(epsc[:, 2:3], 1e-24), reads=['epsc'], writes=['epsc'])
    S.op('pool', lambda: POOL.memset(epsc[:, 3:4], 0.0), reads=['epsc'], writes=['epsc'])
    S.op('pool', lambda: POOL.memset(negkkc, -1.0), writes=['negone'])
    S.op('dve', lambda: V.tensor_tensor(out=alpha, in0=col("mp", 0, 15), in1=col("mn", 0, 15), op=ALU.add), reads=['pp'], writes=['alpha'])
    S.op('dve', lambda: V.tensor_scalar(out=alpha, in0=alpha, scalar1=-1.0, scalar2=1.0, op0=ALU.mult, op1=ALU.add), reads=['alpha'], writes=['alpha'])
    S.op('dve', lambda: V.tensor_scalar(out=oneminus_ka, in0=col("ka", 0, 4), scalar1=-1.0, scalar2=1.0, op0=ALU.mult, op1=ALU.add), reads=['pp'], writes=['omka'])
    S.op('dve', lambda: V.tensor_scalar(out=two_omka, in0=col("ka", 0, 4), scalar1=-2.0, scalar2=2.0, op0=ALU.mult, op1=ALU.add), reads=['pp'], writes=['omka2'])
    S.op('act', lambda: ACT.activation(out=esk, in_=col("sink", 0, 4), func=AF.Exp), reads=['pp'], writes=['esk'])
    S.op('dve', lambda: V.tensor_tensor(out=kar, in0=col("ka", 0, 4), in1=col("rk", 0, 4), op=ALU.mult), reads=['pp'], writes=['kar'])
    S.op('dve', lambda: V.tensor_tensor(out=c2r, in0=two_omka, in1=col("rk", 0, 4), op=ALU.mult), reads=['pp', 'omka2'], writes=['kar'])
    S.op('pool', lambda: POOL.memset(rmask, 1.0), writes=['rmask'])
    S.op('pool', lambda: POOL.memset(rmask.rearrange("p (c t) -> p c t", t=64)[:, :, 0:1], 0.0), reads=['rmask'], writes=['rmask'])
    S.op('pool', lambda: POOL.iota(iota_row, pattern=[[1, CAP]], base=0, channel_multiplier=0, allow_small_or_imprecise_dtypes=True), writes=['iota_row'])

    def debug_out(ap_sb, key, rows=None):
        S.dma('sp', dbg_d if rows is None else rows, ap_sb, reads=[key])

    def phase_adaln():
        AR.seek(0)
        csil = [AR.alloc([128, 8], F32) for _ in range(nseq)]
        crep = [AR.alloc([128, 9, 128], F32) for _ in range(nseq)]
        wblk = [AR.alloc([128, 9, 512], F32) for _ in range(3)]
        g1B = AR.alloc([128, D], F32)
        g2B = AR.alloc([128, D], F32)
        mt = [AR.alloc([128, 512], F32) for _ in range(4)]
        S.dma('sp', g1B, n1g_d.partition_broadcast(128), writes=['g1B'])
        S.dma('sp', g2B, n2g_d.partition_broadcast(128), writes=['g2B'])
        for b in range(3):
            S.op('pool', lambda: POOL.memset(wblk[b][:, 8, :], 0.0), writes=[('wblk', b)])
        for s in range(nseq):
            S.dma('sp', csil[s], cT_d[s], writes=[('csil', s)])
            S.op('act', lambda: ACT.activation(out=csil[s], in_=csil[s], func=AF.Silu), reads=[('csil', s)], writes=[('csil', s)])
            S.op('pool', lambda: POOL.memset(crep[s][:, 8, :], 0.0), writes=[('crep', s)])
            S.op('pool', lambda: POOL.memset(crep[s][0:1, 8, :], 1.0), reads=[('crep', s)], writes=[('crep', s)])
            S.op('dve', lambda: V.tensor_copy(out=crep[s][:, 0:8, :], in_=bc(csil[s].rearrange("p (k o) -> p k o", o=1), [128, 8, 128])),
                 reads=[('csil', s)], writes=[('crep', s)])
        ev = 0
        for jb in range(12):
            b = jb % 3
            piece = jb // 2
            c0 = jb * 512
            S.dma('sp', wblk[b][:, 0:4, :], wada_d[0:512, c0:c0 + 512].rearrange("(k p) n -> p k n", p=128), writes=[('wblk', b)])
            S.dma('act', wblk[b][:, 4:8, :], wada_d[512:1024, c0:c0 + 512].rearrange("(k p) n -> p k n", p=128), writes=[('wblk', b)])
            S.dma('sp', wblk[b][0:1, 8, :], bada_d[:, c0:c0 + 512], writes=[('wblk', b)])
            for s in range(nseq):
                pz, pkz = ps[ev % 4], psk[ev % 4]
                for k in range(9):
                    S.op('pe', lambda: PE.matmul(pz, lhsT=crep[s][:, k, :], rhs=wblk[b][:, k, :], start=(k == 0), stop=(k == 8)),
                         reads=[('crep', s), ('wblk', b)], writes=[pkz], pe_acc=True)
                m = mt[ev % 4]
                lc = (jb % 2) * 512
                if piece == 1:
                    S.op('dve', lambda: V.scalar_tensor_tensor(out=m, in0=pz, scalar=1.0, in1=g1B[:, lc:lc + 512], op0=ALU.add, op1=ALU.mult),
                         reads=[pkz, 'g1B'], writes=[('mt', ev % 4)])
                elif piece == 4:
                    S.op('dve', lambda: V.scalar_tensor_tensor(out=m, in0=pz, scalar=1.0, in1=g2B[:, lc:lc + 512], op0=ALU.add, op1=ALU.mult),
                         reads=[pkz, 'g2B'], writes=[('mt', ev % 4)])
                else:
                    S.op('act', lambda: ACT.copy(out=m, in_=pz), reads=[pkz], writes=[('mt', ev % 4)])
                S.dma('sp', mod_d[s, piece, :, lc:lc + 512], m, reads=[('mt', ev % 4)], writes=[('mod', s, piece)])
                ev += 1
        S.barrier()

    def phase_norm1(s, uT, base):
        AR.seek(base)
        scp = AR.alloc([128, D], F32)
        shp = AR.alloc([128, D], F32)
        xt = [AR.alloc([128, D], F32) for _ in range(2)]
        tmp2 = [AR.alloc([128, D], F32) for _ in range(2)]
        ub = [AR.alloc([128, D], BF16) for _ in range(2)]
        junk2 = [AR.alloc([128, D], BF16) for _ in range(2)]
        ss2 = [AR.alloc([128, 2], F32) for _ in range(2)]
        S.dma('sp', scp, mod_d[s, 1], reads=[('mod', s, 1)], writes=['scp'])
        S.dma('sp', shp, mod_d[s, 0], reads=[('mod', s, 0)], writes=['shp'])
        for i in range(NT):
            b = i % 2
            S.dma('sp', xt[b], x_d[s, i * 128:(i + 1) * 128, :], writes=[('xt', b)])
            tmp, junk, ss = tmp2[b], junk2[b], ss2[b]
            S.op('act', lambda: ACT.activation(out=junk, in_=xt[b], func=AF.Square, accum_out=ss[:, 0:1]), reads=[('xt', b)], writes=[('junk', b), ('ss', b)])
            S.op('act', lambda: ACT.activation(out=ss[:, 1:2], in_=ss[:, 0:1], func=AF.Sqrt, bias=epsc[:, 0:1], scale=1.0 / D), reads=[('ss', b), 'epsc'], writes=[('ss1', b)])
            S.op('dve', lambda: V.reciprocal(out=ss[:, 1:2], in_=ss[:, 1:2]), reads=[('ss1', b)], writes=[('ss1', b)])
            S.op('dve', lambda: V.scalar_tensor_tensor(out=tmp, in0=xt[b], scalar=ss[:, 1:2], in1=scp, op0=ALU.mult, op1=ALU.mult),
                 reads=[('xt', b), ('ss1', b), 'scp'], writes=[('tmp', b)])
            S.op('pool', lambda: POOL.tensor_tensor(out=ub[b], in0=tmp, in1=shp, op=ALU.add), reads=[('tmp', b), 'shp'], writes=[('ub', b)])
            pz = ps[i % 2].bitcast(BF16).rearrange("p (k t) -> p k t", k=8)
            for k in range(8):
                S.op('pe', lambda: PE.transpose(out=pz[:, k, :], in_=ub[b][:, k * 128:(k + 1) * 128], identity=ident),
                     reads=[('ub', b), 'ident'], writes=[psk[i % 2]], pe_acc=True)
            S.op('act', lambda: ACT.copy(out=uT[:, :, i * 128:(i + 1) * 128], in_=pz), reads=[psk[i % 2]], writes=[('uT', i // 4)])

    def phase_rwkv_cols(uT, zsT, base):
        AR.seek(base)
        wg = [AR.alloc([128, 8, 128], BF16) for _ in range(2)]
        ztmp = AR.alloc([128, T + 2], F32)
        sht = AR.alloc([128, T], F32)
        S.op('pool', lambda: POOL.memset(ztmp[:, 0:1], 0.0), writes=['ztmp'])
        S.op('pool', lambda: POOL.memset(ztmp[:, T + 1:T + 2], 0.0), reads=['ztmp'], writes=['ztmp'])
        for j in range(15):
            b = j % 2
            S.dma('pool', wg[b], win_d[:, j * 128:(j + 1) * 128].rearrange("(k p) n -> p k n", p=128), writes=[('wg', b)])
            for tb in range(NB):
                pz = ps[(j * NB + tb) % 4]
                pk = psk[(j * NB + tb) % 4]
                for k in range(8):
                    S.op('pe', lambda: PE.matmul(pz, lhsT=wg[b][:, k, :], rhs=uT[:, k, tb * 512:(tb + 1) * 512], start=(k == 0), stop=(k == 7)),
                         reads=[('wg', b), ('uT', tb)], writes=[pk], pe_acc=True)
                S.op('act', lambda: ACT.copy(out=ztmp[:, 1 + tb * 512:1 + (tb + 1) * 512], in_=pz), reads=[pk], writes=['ztmp'])
            S.op('dve', lambda: V.tensor_scalar(out=sht, in0=ztmp[:, 1:T + 1], scalar1=alpha[:, j:j + 1], scalar2=None, op0=ALU.mult),
                 reads=['ztmp', 'alpha'], writes=['sht'])
            S.op('dve', lambda: V.scalar_tensor_tensor(out=sht, in0=ztmp[:, 0:T], scalar=col("mp", j), in1=sht, op0=ALU.mult, op1=ALU.add),
                 reads=['ztmp', 'sht', 'pp'], writes=['sht'])
            S.op('dve', lambda: V.scalar_tensor_tensor(out=zsT[:, j, :], in0=ztmp[:, 2:T + 2], scalar=col("mn", j), in1=sht, op0=ALU.mult, op1=ALU.add),
                 reads=['ztmp', 'sht', 'pp'], writes=[('zs', j)])
            if j == 12:
                S.op('act', lambda: ACT.activation(out=zsT[:, j, :], in_=zsT[:, j, :], func=AF.Tanh), reads=[('zs', j)], writes=[('zs', j)])
            if j == 14:
                S.op('act', lambda: ACT.activation(out=zsT[:, j, :], in_=zsT[:, j, :], func=AF.Sigmoid), reads=[('zs', j)], writes=[('zs', j)])

    def phase_scan(zsT, kkT, base):
        rT = lambda c: zsT[:, c, :]
        kT = lambda c: zsT[:, 4 + c, :]
        vT = lambda c: zsT[:, 8 + c, :]
        wdT = zsT[:, 12, :]
        adT = zsT[:, 13, :]
        AR.seek(base)
        kraw = AR.alloc([128, 512], F32)
        ksq = AR.alloc([128, 512], BF16)
        krs = AR.alloc([128, 512], F32)
        for c in range(4):
            for tb in range(NB):
                sl = slice(tb * 512, (tb + 1) * 512)
                S.op('dve', lambda: V.tensor_scalar(out=kraw, in0=kT(c)[:, sl], scalar1=col("kk", c), scalar2=None, op0=ALU.mult), reads=[('zs', 4 + c), 'pp'], writes=['kraw'])
                S.op('act', lambda: ACT.activation(out=ksq, in_=kraw, func=AF.Square), reads=['kraw'], writes=['ksq'])
                pz, pk = ps[tb % 2], psk[tb % 2]
                S.op('pe', lambda: PE.matmul(pz, lhsT=cmb[:, BLK1, :], rhs=ksq, start=True, stop=True), reads=['ksq', 'cmb'], writes=[pk], pe_acc=True)
                S.op('act', lambda: ACT.activation(out=krs, in_=pz, func=AF.Sqrt, bias=epsc[:, 2:3], scale=1.0), reads=[pk, 'epsc'], writes=['krs'])
                S.op('dve', lambda: V.reciprocal(out=krs, in_=krs), reads=['krs'], writes=['krs'])
                S.op('dve', lambda: V.tensor_tensor(out=kkT[:, c, sl], in0=kraw, in1=krs, op=ALU.mult), reads=['kraw', 'krs'], writes=[('kk', c)])
        S.barrier()
        AR.seek(base)
        sg = AR.alloc([128, 4, TBS], F32)
        ad = AR.alloc([128, 4, TBS], F32)
        cc = AR.alloc([128, 4, TBS], F32)
        t1 = AR.alloc([128, 4, TBS], F32)
        ex = [[AR.alloc([128, TBS], F32) for _ in range(2)] for _ in range(4)]
        kd = AR.alloc([128, 4, TBS], F32)
        bb = AR.alloc([128, 4, TBS], F32)
        pdec = AR.alloc([128, 4, NCH], F32)
        ARz = AR.alloc([128, 4, NCH, 2, 2, 64], BF16)
        Bz = AR.alloc([128, 4, NCH, 2, 64], BF16)
        BKt = AR.alloc([128, 4, NCH, 2, 64], BF16)
        KBh = AR.alloc([128, 4, NCH, 2, 64], BF16)
        KBt = AR.alloc([128, 4, NCH, 128], BF16)
        VZ = AR.alloc([128, NCH, 8, 64], BF16)
        XV = AR.alloc([128, NCH, 8, 64], BF16)
        ZTs = [[AR.alloc([128, 4, 128], BF16) for _ in range(2)] for _ in range(NCH)]
        ATm = [[AR.alloc([128, 4, 128], BF16) for _ in range(2)] for _ in range(NCH)]
        PTm = [[AR.alloc([128, 4, 128], BF16) for _ in range(2)] for _ in range(2)]
        Pm = [[AR.alloc([128, 4, 128], BF16) for _ in range(2)] for _ in range(2)]
        Am = [[AR.alloc([128, 4, 128], BF16) for _ in range(2)] for _ in range(2)]
        W1s = AR.alloc([128, 4, 64], BF16)
        S32 = [AR.alloc([128, 4, 64], F32) for _ in range(2)]
        Sb = [AR.alloc([128, 4, 64], BF16) for _ in range(2)]
        ysb = [AR.alloc([64, 512], F32) for _ in range(2)]
        S.op('pool', lambda: POOL.memset(ARz.rearrange("p a b c d e -> p (a b c d e)"), 0.0), writes=['ARz'])
        S.op('pool', lambda: POOL.memset(Bz.rearrange("p a b c d -> p (a b c d)"), 0.0), writes=['Bz'])
        S.op('pool', lambda: POOL.memset(VZ.rearrange("p a b c -> p (a b c)"), 0.0), writes=['VZ'])

        def chain(gens):
            for g_ in gens:
                yield from g_

        def run_tasks(tasks):
            tasks = list(tasks)
            while tasks:
                for t_ in list(tasks):
                    try:
                        next(t_)
                    except StopIteration:
                        tasks.remove(t_)

        yev = 0
        for d in range(2):
            S.op('pool', lambda: POOL.memset(S32[d].rearrange("p a b -> p (a b)"), 0.0), writes=[('S32', d)])
            S.op('pool', lambda: POOL.memset(Sb[d].rearrange("p a b -> p (a b)"), 0.0), writes=[('Sb', d)])
            tbs = range(NTB) if d == 0 else range(NTB - 1, -1, -1)
            for tb in tbs:
                sl = slice(tb * TBS, (tb + 1) * TBS)
                def gen_prep(c):
                    pz, pk = ps[c % 2], psk[c % 2]
                    S.op('pe', lambda: PE.matmul(pz[:, 0:TBS], lhsT=w2c[:, d, c * 128:(c + 1) * 128], rhs=wdT[:, sl], start=True, stop=True),
                         reads=['w2c', ('zs', 12)], writes=[pk], pe_acc=True)
                    S.op('act', lambda: ACT.activation(out=sg[:, c, :], in_=pz[:, 0:TBS], func=AF.Sigmoid, bias=col("w0", d * 4 + c), scale=1.0),
                         reads=[pk, 'pp'], writes=[('sg', c)])
                    pz2, pk2 = ps[2 + c % 2], psk[2 + c % 2]
                    S.op('pe', lambda: PE.matmul(pz2[:, 0:TBS], lhsT=a2c[:, d, c * 128:(c + 1) * 128], rhs=adT[:, sl], start=True, stop=True),
                         reads=['a2c', ('zs', 13)], writes=[pk2], pe_acc=True)
                    S.op('act', lambda: ACT.activation(out=ad[:, c, :], in_=pz2[:, 0:TBS], func=AF.Sigmoid, bias=col("a0", d * 4 + c), scale=1.0),
                         reads=[pk2, 'pp'], writes=[('ad', c)])
                    yield
                    S.op('dve', lambda: V.tensor_tensor_scan(out=cc[:, c, :], data0=rmask, data1=sg[:, c, :], initial=0.0, op0=ALU.mult, op1=ALU.add),
                         reads=['rmask', ('sg', c)], writes=[('cc', c)])
                    cc3 = cc[:, c, :].rearrange("p (h t) -> p h t", t=64)
                    sg3 = sg[:, c, :].rearrange("p (h t) -> p h t", t=64)
                    t13 = t1[:, c, :].rearrange("p (h t) -> p h t", t=64)
                    if d == 1:
                        S.op('dve', lambda: V.tensor_tensor(out=t13, in0=bc(cc3[:, :, 63:64], [128, NCH, 64]), in1=cc3, op=ALU.subtract),
                             reads=[('cc', c)], writes=[('t1', c)])
                        S.op('dve', lambda: V.tensor_tensor(out=cc[:, c, :], in0=t1[:, c, :], in1=sg[:, c, :], op=ALU.add),
                             reads=[('t1', c), ('sg', c)], writes=[('cc', c)])
                    totp = 63 if d == 0 else 0
                    S.op('pool', lambda: POOL.tensor_scalar(out=kd[:, c, :], in0=ad[:, c, :], scalar1=col("ka", c), scalar2=oneminus_ka[:, c:c + 1], op0=ALU.mult, op1=ALU.add),
                         reads=[('ad', c), 'pp', 'omka'], writes=[('kd', c)])
                    S.op('pool', lambda: POOL.tensor_tensor(out=kd[:, c, :], in0=kd[:, c, :], in1=kT(c)[:, sl], op=ALU.mult),
                         reads=[('kd', c), ('zs', 4 + c)], writes=[('kd', c)])
                    S.op('pool', lambda: POOL.tensor_tensor(out=bb[:, c, :], in0=ad[:, c, :], in1=kkT[:, c, sl], op=ALU.mult),
                         reads=[('ad', c), ('kk', c)], writes=[('bb', c)])
                    yield
                    e = ex[c][0]
                    S.op('act', lambda: ACT.activation(out=e, in_=cc[:, c, :], func=AF.Exp, scale=-LAM), reads=[('cc', c)], writes=[('ex', c, 0)])
                    for hp in range(2):
                        pr = slice(hp * 64, (hp + 1) * 64)
                        S.op('dve', lambda: V.tensor_tensor(out=ARz[pr, c, :, 0, hp, :], in0=rT(c)[pr, sl].rearrange("p (h t) -> p h t", t=64),
                                                            in1=e[pr, :].rearrange("p (h t) -> p h t", t=64), op=ALU.mult),
                             reads=[('zs', c), ('ex', c, 0)], writes=['ARz'])
                    yield
                    e = ex[c][1]
                    S.op('act', lambda: ACT.activation(out=e, in_=cc[:, c, :], func=AF.Exp, scale=LAM), reads=[('cc', c)], writes=[('ex', c, 1)])
                    S.op('dve', lambda: V.tensor_tensor(out=BKt[:, c, :, 0, :], in0=kd[:, c, :].rearrange("p (h t) -> p h t", t=64),
                                                        in1=e.rearrange("p (h t) -> p h t", t=64), op=ALU.mult),
                         reads=[('kd', c), ('ex', c, 1)], writes=['BKt'])
                    S.op('dve', lambda: V.tensor_tensor(out=BKt[:, c, :, 1, :], in0=bb[:, c, :].rearrange("p (h t) -> p h t", t=64),
                                                        in1=e.rearrange("p (h t) -> p h t", t=64), op=ALU.mult),
                         reads=[('bb', c), ('ex', c, 1)], writes=['BKt'])
                    for hp in range(2):
                        pr = slice(hp * 64, (hp + 1) * 64)
                        S.op('act', lambda: ACT.copy(out=Bz[pr, c, :, hp, :], in_=BKt[pr, c, :, 1, :]), reads=['BKt'], writes=['Bz'])
                    yield
                    S.op('dve', lambda: V.tensor_tensor(out=t1[:, c, :], in0=cc[:, c, :], in1=sg[:, c, :], op=ALU.subtract),
                         reads=[('cc', c), ('sg', c)], writes=[('t1', c)])
                    e = ex[c][0]
                    S.op('act', lambda: ACT.activation(out=e, in_=t1[:, c, :], func=AF.Exp, scale=-LAM), reads=[('t1', c)], writes=[('ex', c, 0)])
                    for hp in range(2):
                        pr = slice(hp * 64, (hp + 1) * 64)
                        S.op('dve', lambda: V.scalar_tensor_tensor(out=ARz[pr, c, :, 1, hp, :], in0=kkT[pr, c, sl].rearrange("p (h t) -> p h t", t=64),
                                                                   scalar=-1.0, in1=e[pr, :].rearrange("p (h t) -> p h t", t=64), op0=ALU.mult, op1=ALU.mult),
                             reads=[('kk', c), ('ex', c, 0)], writes=['ARz'])
                    yield
                    S.op('dve', lambda: V.tensor_tensor(out=t13, in0=bc(cc3[:, :, totp:totp + 1], [128, NCH, 64]), in1=cc3, op=ALU.subtract),
                         reads=[('cc', c)], writes=[('t1', c)])
                    e = ex[c][1]
                    S.op('act', lambda: ACT.activation(out=e, in_=t1[:, c, :], func=AF.Exp, scale=-LAM), reads=[('t1', c)], writes=[('ex', c, 1)])
                    S.op('pool', lambda: POOL.tensor_tensor(out=KBh[:, c, :, 0, :], in0=kd[:, c, :].rearrange("p (h t) -> p h t", t=64),
                                                        in1=e.rearrange("p (h t) -> p h t", t=64), op=ALU.mult),
                         reads=[('kd', c), ('ex', c, 1)], writes=['KBh'])
                    S.op('pool', lambda: POOL.tensor_tensor(out=KBh[:, c, :, 1, :], in0=bb[:, c, :].rearrange("p (h t) -> p h t", t=64),
                                                        in1=e.rearrange("p (h t) -> p h t", t=64), op=ALU.mult),
                         reads=[('bb', c), ('ex', c, 1)], writes=['KBh'])
                    S.op('act', lambda: ACT.activation(out=pdec[:, c, :].rearrange("p (h o) -> p h o", o=1), in_=cc3[:, :, totp:totp + 1], func=AF.Exp, scale=-LAM), reads=[('cc', c)], writes=['pdec'])
                run_tasks([gen_prep(c_) for c_ in range(4)])
                for ch in range(NCH):
                    pz = ps[4 + ch % 2].bitcast(BF16)
                    pk = psk[4 + ch % 2]
                    pzv = pz[0:64, 0:512].rearrange("p (c n) -> p c n", c=4)
                    for c in range(4):
                        S.op('pe', lambda: PE.transpose(out=pzv[:, c, :], in_=vT(c)[:, tb * TBS + ch * 64: tb * TBS + (ch + 1) * 64], identity=ident),
                             reads=[('zs', 8 + c), 'ident'], writes=[pk], pe_acc=True)
                    S.op('act', lambda: ACT.copy(out=VZ[0:64, ch, :, :].rearrange("p h v -> p (h v)"), in_=pz[0:64, 0:512]), reads=[pk], writes=[('VZ', ch)])
                    S.op('act', lambda: ACT.copy(out=XV[0:64, ch, :, :].rearrange("p h v -> p (h v)"), in_=pz[0:64, 0:512]), reads=[pk], writes=[('XVv', ch)])
                    pzk = pz[:, 512:1024].rearrange("p (c n) -> p c n", c=4)
                    for c in range(4):
                        S.op('pe', lambda: PE.transpose(out=pzk[:, c, :], in_=KBh[:, c, ch, :, :].rearrange("p a t -> p (a t)"), identity=ident),
                             reads=['KBh', 'ident'], writes=[pk], pe_acc=True)
                    S.op('dve', lambda: V.tensor_copy(out=KBt[:, :, ch, :], in_=pzk), reads=[pk], writes=[('KBt', ch)])
                MNT = cmb[:, 11 + d, :]
                MN = cmb[:, 12 - d, :]

                def gen_D(ch, slot, par):
                    pA, pkA = ps[2 * par], psk[2 * par]
                    pB, pkB = ps[2 * par + 1], psk[2 * par + 1]
                    pA3 = pA.rearrange("p (j n) -> p j n", j=4)
                    pB3 = pB.rearrange("p (j n) -> p j n", j=4)
                    mzt = cmb[:, MZT[d], :]
                    for half in range(2):
                        pz3 = pA3 if half == 0 else pB3
                        pkz = pkA if half == 0 else pkB
                        for j in range(4):
                            h = half * 4 + j
                            c, hp = h // 2, h % 2
                            bk = BKt[:, c, ch, :, :].rearrange("p a t -> p (a t)")
                            S.op('pe', lambda: PE.matmul(pz3[:, j, :].rearrange("p (a t) -> p a t", a=2), lhsT=bk, rhs=ARz[:, c, ch, :, hp, :], start=True, stop=True),
                                 reads=['BKt', 'ARz'], writes=[pkz], pe_acc=True)
                        S.op('dve', lambda: V.tensor_tensor(out=ZTs[slot][half], in0=pz3, in1=bc(mzt.rearrange("p (o n) -> p o n", o=1), [128, 4, 128]), op=ALU.mult),
                             reads=[pkz, 'cmb'], writes=[('ZTs', slot, half)])
                    yield
                    for c in range(4):
                        bz = Bz[:, c, ch, :, :].rearrange("p a t -> p (a t)")
                        az = ARz[:, c, ch, 1, :, :].rearrange("p a t -> p (a t)")
                        S.op('pe', lambda: PE.matmul(pA3[:, c, :], lhsT=bz, rhs=az, start=True, stop=True), reads=['Bz', 'ARz'], writes=[pkA], pe_acc=True)
                        S.op('pe', lambda: PE.matmul(pB3[:, c, :], lhsT=az, rhs=bz, start=True, stop=True), reads=['Bz', 'ARz'], writes=[pkB], pe_acc=True)
                    S.op('dve', lambda: V.tensor_tensor(out=PTm[par][0], in0=pA3, in1=bc(MNT.rearrange("p (o n) -> p o n", o=1), [128, 4, 128]), op=ALU.mult),
                         reads=[pkA, 'cmb'], writes=[('PT', par, 0)])
                    S.op('dve', lambda: V.tensor_tensor(out=Pm[par][0], in0=pB3, in1=bc(MN.rearrange("p (o n) -> p o n", o=1), [128, 4, 128]), op=ALU.mult),
                         reads=[pkB, 'cmb'], writes=[('P', par, 0)])
                    S.op('pool', lambda: POOL.tensor_tensor(out=ATm[slot][0], in0=PTm[par][0], in1=ident4, op=ALU.add), reads=[('PT', par, 0), 'ident4'], writes=[('AT', slot, 0)])
                    S.op('pool', lambda: POOL.tensor_tensor(out=Am[par][0], in0=Pm[par][0], in1=ident4, op=ALU.add), reads=[('P', par, 0), 'ident4'], writes=[('A', par, 0)])
                    yield
                    cur = 0
                    for lev in range(1, 6):
                        nxt = 1 - cur
                        for j in range(4):
                            S.op('pe', lambda: PE.matmul(pA3[:, j, :], lhsT=Pm[par][cur][:, j, :], rhs=PTm[par][cur][:, j, :], start=True, stop=True),
                                 reads=[('P', par, cur), ('PT', par, cur)], writes=[pkA], pe_acc=True)
                            if lev < 5:
                                S.op('pe', lambda: PE.matmul(pB3[:, j, :], lhsT=PTm[par][cur][:, j, :], rhs=Pm[par][cur][:, j, :], start=True, stop=True),
                                     reads=[('P', par, cur), ('PT', par, cur)], writes=[pkB], pe_acc=True)
                        S.op('act', lambda: ACT.copy(out=PTm[par][nxt], in_=pA3), reads=[pkA], writes=[('PT', par, nxt)])
                        if lev < 5:
                            S.op('act', lambda: ACT.copy(out=Pm[par][nxt], in_=pB3), reads=[pkB], writes=[('P', par, nxt)])
                        yield
                        for j in range(4):
                            S.op('pe', lambda: PE.matmul(pA3[:, j, :], lhsT=Am[par][cur][:, j, :], rhs=PTm[par][nxt][:, j, :], start=True, stop=True),
                                 reads=[('A', par, cur), ('PT', par, nxt)], writes=[pkA], pe_acc=True)
                            if lev < 5:
                                S.op('pe', lambda: PE.matmul(pB3[:, j, :], lhsT=PTm[par][nxt][:, j, :], rhs=Am[par][cur][:, j, :], start=True, stop=True),
                                     reads=[('A', par, cur), ('PT', par, nxt)], writes=[pkB], pe_acc=True)
                        S.op('dve', lambda: V.tensor_tensor(out=ATm[slot][nxt], in0=pA3, in1=ATm[slot][cur], op=ALU.add), reads=[pkA, ('AT', slot, cur)], writes=[('AT', slot, nxt)])
                        if lev < 5:
                            S.op('dve', lambda: V.tensor_tensor(out=Am[par][nxt], in0=pB3, in1=Am[par][cur], op=ALU.add), reads=[pkB, ('A', par, cur)], writes=[('A', par, nxt)])
                        yield
                        cur = nxt
                    assert cur == 1

                def gen_Q(ch, slot):
                    nonlocal yev
                    fin = 1
                    gch = tb * NCH + ch
                    pW, pkW = ps[4], psk[4]
                    pW3 = pW[:, 0:256].rearrange("p (c v) -> p c v", c=4)
                    for h in range(8):
                        c, hp = h // 2, h % 2
                        S.op('pe', lambda: PE.matmul(pW3[hp * 64:(hp + 1) * 64, c, :], lhsT=ZTs[slot][h // 4][:, h % 4, 64:128], rhs=VZ[:, ch, h, :], start=True, stop=False),
                             reads=[('ZTs', slot, h // 4), ('VZ', ch)], writes=[pkW], pe_acc=True)
                        S.op('pe', lambda: PE.matmul(pW3[hp * 64:(hp + 1) * 64, c, :], lhsT=ARz[:, c, ch, 1, hp, :], rhs=Sb[d][:, c, :], start=False, stop=True),
                             reads=['ARz', ('Sb', d)], writes=[pkW], pe_acc=True)
                    S.op('act', lambda: ACT.copy(out=W1s, in_=pW3), reads=[pkW], writes=['W1s'])
                    yield
                    pX, pkX = ps[5], psk[5]
                    pX3 = pX.rearrange("p (h v) -> p h v", h=8)
                    for h in range(8):
                        c, hp = h // 2, h % 2
                        S.op('pe', lambda: PE.matmul(pX3[64:128, h, :], lhsT=ATm[slot][fin][:, c, hp * 64:(hp + 1) * 64], rhs=W1s[:, c, :], start=True, stop=True),
                             reads=[('AT', slot, fin), 'W1s'], writes=[pkX], pe_acc=True)
                    S.op('dve', lambda: V.tensor_copy(out=XV[64:128, ch, :, :], in_=pX3[64:128]), reads=[pkX], writes=[('XVu', ch)])
                    yield
                    pS, pkS = ps[7], psk[7]
                    pS3 = pS[:, 0:256].rearrange("p (c v) -> p c v", c=4)
                    for h in range(8):
                        c, hp = h // 2, h % 2
                        S.op('pe', lambda: PE.matmul(pS3[hp * 64:(hp + 1) * 64, c, :], lhsT=KBt[:, c, ch, hp * 64:(hp + 1) * 64], rhs=XV[:, ch, h, :], start=True, stop=True),
                             reads=[('KBt', ch), ('XVv', ch), ('XVu', ch)], writes=[pkS], pe_acc=True)
                    pY, pkY = ps[6], psk[6]
                    pY3 = pY.rearrange("p (h v) -> p h v", h=8)
                    for h in range(8):
                        c, hp = h // 2, h % 2
                        S.op('pe', lambda: PE.matmul(pY3[0:64, h, :], lhsT=ZTs[slot][h // 4][:, h % 4, 0:64], rhs=XV[:, ch, h, :], start=True, stop=False),
                             reads=[('ZTs', slot, h // 4), ('XVv', ch), ('XVu', ch)], writes=[pkY], pe_acc=True)
                        S.op('pe', lambda: PE.matmul(pY3[0:64, h, :], lhsT=ARz[:, c, ch, 0, hp, :], rhs=Sb[d][:, c, :], start=False, stop=True),
                             reads=['ARz', ('Sb', d)], writes=[pkY], pe_acc=True)
                    for c in range(4):
                        S.op('dve', lambda: V.scalar_tensor_tensor(out=S32[d][:, c, :], in0=S32[d][:, c, :], scalar=pdec[:, c, ch:ch + 1], in1=pS3[:, c, :], op0=ALU.mult, op1=ALU.add),
                             reads=[('S32', d), 'pdec', pkS], writes=[('S32', d)])
                    S.op('act', lambda: ACT.copy(out=Sb[d], in_=S32[d]), reads=[('S32', d)], writes=[('Sb', d)])
                    yb_ = ysb[yev % 2]
                    S.op('act', lambda: ACT.copy(out=yb_, in_=pY[0:64, :]), reads=[pkY], writes=[('ysb', yev % 2)])
                    S.dma('sp', y_d[d, gch * 64:(gch + 1) * 64, :], yb_, reads=[('ysb', yev % 2)], writes=[('yscr', d, gch // 2)])
                    yev += 1
                    yield

                chs = list(range(NCH)) if d == 0 else list(range(NCH - 1, -1, -1))
                pend = []
                for r in range(0, NCH, 2):
                    tasks = [gen_D(chs[r], r, 0), gen_D(chs[r + 1], r + 1, 1)]
                    if pend:
                        tasks.append(chain([gen_Q(c_, s_) for (c_, s_) in pend]))
                    run_tasks(tasks)
                    pend = [(chs[r], r), (chs[r + 1], r + 1)]
                run_tasks([chain([gen_Q(c_, s_) for (c_, s_) in pend])])
        S.barrier()


    def phase_post(zsT, yaT, base):
        rT4 = zsT[:, 0:4, :]
        kT4 = zsT[:, 4:8, :]
        adT = zsT[:, 13, :]
        gdT = zsT[:, 14, :]
        AR.seek(base)
        gnwB = AR.alloc([128, 512], F32)
        gnbB = AR.alloc([128, 512], F32)
        P2 = lambda shape, dt: [AR.alloc(shape, dt) for _ in range(2)]
        Yf, Yb = P2([128, 512], F32), P2([128, 512], F32)
        ta0, ta1 = P2([128, 4, 128], F32), P2([128, 4, 128], F32)
        kf2 = P2([128, 4, 128], F32)
        prod2 = P2([128, 4, 128], BF16)
        rows2 = P2([128, 8], F32)
        bon2 = P2([128, 512], F32)
        y2 = P2([128, 512], F32)
        sq2 = P2([128, 512], F32)
        st2 = P2([128, 4, 8], F32)
        yab2 = P2([128, 512], BF16)
        S.dma('sp', gnwB, gnw_d.partition_broadcast(128), writes=['gnwB'])
        S.dma('sp', gnbB, gnb_d.partition_broadcast(128), writes=['gnbB'])
        def gen_tile(i):
            b = i % 2
            sl = slice(i * 128, (i + 1) * 128)
            ta = (ta0[b], ta1[b])
            kf, prod, rows, bon, y, sq, st, yab = kf2[b], prod2[b], rows2[b], bon2[b], y2[b], sq2[b], st2[b], yab2[b]
            bA, bB, bC, bD = 4 * b, 4 * b + 1, 4 * b + 2, 4 * b + 3
            S.dma('sp', Yf[b], y_d[0, sl, :], writes=[('Yf', b)])
            S.dma('sp', Yb[b], y_d[1, sl, :], writes=[('Yb', b)])
            for d in range(2):
                bk_ = bA if d == 0 else bB
                pz3 = ps[bk_].rearrange("p (c n) -> p c n", c=4)
                for c in range(4):
                    S.op('pe', lambda: PE.matmul(pz3[:, c, :], lhsT=a2c[:, d, c * 128:(c + 1) * 128], rhs=adT[:, sl], start=True, stop=True),
                         reads=['a2c'], writes=[psk[bk_]], pe_acc=True)
                for c in range(4):
                    S.op('act', lambda: ACT.activation(out=ta[d][:, c, :], in_=pz3[:, c, :], func=AF.Sigmoid, bias=col("a0", d * 4 + c), scale=1.0),
                         reads=[psk[bk_], 'pp'], writes=[('ta', b, d)])
            yield
            S.op('pool', lambda: POOL.tensor_tensor(out=ta[0], in0=ta[0], in1=ta[1], op=ALU.add), reads=[('ta', b, 0), ('ta', b, 1)], writes=[('ta', b, 0)])
            for c in range(4):
                S.op('pool', lambda: POOL.tensor_scalar(out=kf[:, c, :], in0=ta[0][:, c, :], scalar1=kar[:, c:c + 1], scalar2=c2r[:, c:c + 1], op0=ALU.mult, op1=ALU.add),
                     reads=[('ta', b, 0), 'kar'], writes=[('kf', b)])
            S.op('pool', lambda: POOL.tensor_tensor(out=kf, in0=kf, in1=kT4[:, :, sl], op=ALU.mult), reads=[('kf', b)], writes=[('kf', b)])
            S.op('pool', lambda: POOL.tensor_tensor(out=prod, in0=kf, in1=rT4[:, :, sl], op=ALU.mult), reads=[('kf', b)], writes=[('prod', b)])
            yield
            pr = ps[bB]
            for c in range(4):
                S.op('pe', lambda: PE.matmul(pr[:, c * 2:(c + 1) * 2], lhsT=prod[:, c, :], rhs=cmb[:, HSEL, 0:2], start=True, stop=True),
                     reads=[('prod', b), 'cmb'], writes=[psk[bB]], pe_acc=True)
            S.op('act', lambda: ACT.copy(out=rows, in_=pr[:, 0:8]), reads=[psk[bB]], writes=[('rows', b)])
            yield
            pv = ps[bD].bitcast(BF16)[:, 0:512]
            for c in range(4):
                S.op('pe', lambda: PE.transpose(out=pv[:, c * 128:(c + 1) * 128], in_=zsT[:, 8 + c, sl], identity=ident),
                     reads=['ident'], writes=[psk[bD]], pe_acc=True)
            S.op('dve', lambda: V.tensor_tensor(out=bon.rearrange("p (h v) -> p h v", h=8), in0=pv.rearrange("p (h v) -> p h v", h=8),
                                                in1=bc(rows.rearrange("p (h o) -> p h o", o=1), [128, 8, 64]), op=ALU.mult),
                 reads=[psk[bD], ('rows', b)], writes=[('bon', b)])
            yield
            pg = ps[bC]
            S.op('pe', lambda: PE.matmul(pg, lhsT=gdT[:, sl], rhs=g2, start=True, stop=True), reads=['g2'], writes=[psk[bC]], pe_acc=True)
            y3 = y.rearrange("p (h v) -> p h v", h=8)
            sq3 = sq.rearrange("p (h v) -> p h v", h=8)
            S.op('dve', lambda: V.tensor_tensor(out=y, in0=Yf[b], in1=Yb[b], op=ALU.add), reads=[('Yf', b), ('Yb', b)], writes=[('y', b)])
            yield
            S.op('dve', lambda: V.tensor_reduce(out=st[:, 0, :], in_=y3, axis=AX.X, op=ALU.add), reads=[('y', b)], writes=[('st0', b)])
            S.op('dve', lambda: V.tensor_scalar(out=st[:, 1, :], in0=st[:, 0, :], scalar1=-1.0 / 64, scalar2=None, op0=ALU.mult), reads=[('st0', b)], writes=[('st1', b)])
            S.op('dve', lambda: V.tensor_tensor(out=y3, in0=y3, in1=bc(st[:, 1, :].rearrange("p (h o) -> p h o", o=1), [128, 8, 64]), op=ALU.add),
                 reads=[('y', b), ('st1', b)], writes=[('y', b)])
            yield
            S.op('act', lambda: ACT.activation(out=sq, in_=y, func=AF.Square), reads=[('y', b)], writes=[('sq', b)])
            S.op('dve', lambda: V.tensor_reduce(out=st[:, 2, :], in_=sq3, axis=AX.X, op=ALU.add), reads=[('sq', b)], writes=[('st2', b)])
            yield
            S.op('act', lambda: ACT.activation(out=st[:, 3, :], in_=st[:, 2, :], func=AF.Sqrt, bias=epsc[:, 1:2], scale=1.0 / 64), reads=[('st2', b), 'epsc'], writes=[('st3', b)])
            S.op('dve', lambda: V.reciprocal(out=st[:, 3, :], in_=st[:, 3, :]), reads=[('st3', b)], writes=[('st3', b)])
            S.op('dve', lambda: V.tensor_tensor(out=y3, in0=y3, in1=bc(st[:, 3, :].rearrange("p (h o) -> p h o", o=1), [128, 8, 64]), op=ALU.mult),
                 reads=[('y', b), ('st3', b)], writes=[('y', b)])
            yield
            S.op('dve', lambda: V.tensor_tensor(out=y, in0=y, in1=gnwB, op=ALU.mult), reads=[('y', b), 'gnwB'], writes=[('y', b)])
            S.op('pool', lambda: POOL.tensor_tensor(out=bon, in0=bon, in1=gnbB, op=ALU.add), reads=[('bon', b), 'gnbB'], writes=[('bon', b)])
            S.op('dve', lambda: V.tensor_tensor(out=y, in0=y, in1=bon, op=ALU.add), reads=[('y', b), ('bon', b)], writes=[('y', b)])
            S.op('dve', lambda: V.tensor_tensor(out=yab, in0=y, in1=pg, op=ALU.mult), reads=[('y', b), psk[bC]], writes=[('yab', b)])
            yield
            pt = ps[bA].bitcast(BF16)[:, 0:512]
            for c in range(4):
                S.op('pe', lambda: PE.transpose(out=pt[:, c * 128:(c + 1) * 128], in_=yab[:, c * 128:(c + 1) * 128], identity=ident),
                     reads=[('yab', b), 'ident'], writes=[psk[bA]], pe_acc=True)
            S.op('act', lambda: ACT.copy(out=yaT[:, :, sl], in_=pt.rearrange("p (c n) -> p c n", c=4)), reads=[psk[bA]], writes=['yaT'])
            yield

        def run_tasks(tasks):
            tasks = list(tasks)
            while tasks:
                for t_ in list(tasks):
                    try:
                        next(t_)
                    except StopIteration:
                        tasks.remove(t_)

        for i in range(0, NT, 2):
            run_tasks([gen_tile(i), gen_tile(i + 1)])

    def phase_attn(s, uT, ybT, baseA, baseB):
        AR.seek(baseA)
        cosT = AR.alloc([128, T], F32)
        sinT = AR.alloc([128, T], F32)
        qT = AR.alloc([128, 4, T], BF16)
        kTt = AR.alloc([128, T], BF16)
        vp = AR.alloc([128, 2, NT, 128], BF16)
        AR.seek(baseB)
        wq = [AR.alloc([128, 8, 128], BF16) for _ in range(2)]
        qfL = [AR.alloc([128, 512], F32) for _ in range(2)]
        sqbL = [AR.alloc([128, 512], BF16) for _ in range(2)]
        rsL = [AR.alloc([128, 512], F32) for _ in range(2)]
        qnL = [AR.alloc([128, 512], F32) for _ in range(2)]
        qnbL = [AR.alloc([128, 512], BF16) for _ in range(2)]
        t1L = [AR.alloc([128, 512], F32) for _ in range(2)]
        t2L = [AR.alloc([128, 512], F32) for _ in range(2)]
        pTs = [AR.alloc([128, 512], BF16) for _ in range(6)]
        dn = AR.alloc([128, 512], F32)
        posi = AR.alloc([128, T], I32)
        ang = AR.alloc([128, T], F32)
        ki = AR.alloc([128, T], I32)
        kf = AR.alloc([128, T], F32)
        m1 = AR.alloc([128, T], F32)
        S.dma('sp', posi, pos_d[s].partition_broadcast(128), writes=['posi'])

        def table(dst, shift):
            S.op('dve', lambda: V.tensor_copy(out=ang, in_=posi), reads=['posi'], writes=['ang'])
            S.op('dve', lambda: V.tensor_scalar(out=ang, in0=ang, scalar1=col("invf"), scalar2=shift, op0=ALU.mult, op1=ALU.add), reads=['ang', 'pp'], writes=['ang'])
            S.op('dve', lambda: V.tensor_scalar(out=ki, in0=ang, scalar1=1.0 / TWO_PI, scalar2=None, op0=ALU.mult), reads=['ang'], writes=['ki'])
            S.op('pool', lambda: POOL.tensor_copy(out=kf, in_=ki), reads=['ki'], writes=['kf'])
            S.op('dve', lambda: V.scalar_tensor_tensor(out=ang, in0=kf, scalar=-C1, in1=ang, op0=ALU.mult, op1=ALU.add), reads=['kf', 'ang'], writes=['ang'])
            S.op('dve', lambda: V.scalar_tensor_tensor(out=ang, in0=kf, scalar=-C2, in1=ang, op0=ALU.mult, op1=ALU.add), reads=['kf', 'ang'], writes=['ang'])
            S.op('dve', lambda: V.tensor_scalar(out=m1, in0=ang, scalar1=float(np.pi), scalar2=-TWO_PI, op0=ALU.is_gt, op1=ALU.mult), reads=['ang'], writes=['m1'])
            S.op('pool', lambda: POOL.tensor_tensor(out=ang, in0=ang, in1=m1, op=ALU.add), reads=['ang', 'm1'], writes=['ang'])
            S.op('dve', lambda: V.tensor_scalar(out=m1, in0=ang, scalar1=float(-np.pi), scalar2=TWO_PI, op0=ALU.is_lt, op1=ALU.mult), reads=['ang'], writes=['m1'])
            S.op('pool', lambda: POOL.tensor_tensor(out=ang, in0=ang, in1=m1, op=ALU.add), reads=['ang', 'm1'], writes=['ang'])
            S.op('act', lambda: ACT.activation(out=dst, in_=ang, func=AF.Sin), reads=['ang'], writes=['tab'])

        table(sinT, 0.0)
        table(cosT, float(np.pi / 2))
        def c0_of(c):
            return 1920 + c * 128 if c < 4 else 2432

        def gen_qk(c, tb, L):
            b = c % 2
            gcol = col("qg") if c < 4 else col("kg")
            sl = slice(tb * 512, (tb + 1) * 512)
            qf_, sqb_, rs_, qn_, qnb_, t1_, t2_ = qfL[L], sqbL[L], rsL[L], qnL[L], qnbL[L], t1L[L], t2L[L]
            pz, pk = ps[L], psk[L]
            for k in range(8):
                S.op('pe', lambda: PE.matmul(pz, lhsT=wq[b][:, k, :], rhs=uT[:, k, sl], start=(k == 0), stop=(k == 7)),
                     reads=[('wq', b), ('uT', tb)], writes=[pk], pe_acc=True)
            S.op('act', lambda: ACT.copy(out=qf_, in_=pz), reads=[pk], writes=[('qf', L)])
            S.op('act', lambda: ACT.activation(out=sqb_, in_=qf_, func=AF.Square), reads=[('qf', L)], writes=[('sqb', L)])
            yield
            pr, pkr = ps[2 + L], psk[2 + L]
            S.op('pe', lambda: PE.matmul(pr, lhsT=cmb[:, BLK1, :], rhs=sqb_, start=True, stop=True), reads=[('sqb', L), 'cmb'], writes=[pkr], pe_acc=True)
            S.op('act', lambda: ACT.activation(out=rs_, in_=pr, func=AF.Sqrt, bias=epsc[:, 0:1], scale=1.0 / 64), reads=[pkr, 'epsc'], writes=[('rs', L)])
            yield
            S.op('dve', lambda: V.reciprocal(out=rs_, in_=rs_), reads=[('rs', L)], writes=[('rs', L)])
            S.op('dve', lambda: V.scalar_tensor_tensor(out=qn_, in0=qf_, scalar=gcol, in1=rs_, op0=ALU.mult, op1=ALU.mult), reads=[('qf', L), ('rs', L), 'pp'], writes=[('qn', L)])
            S.op('act', lambda: ACT.copy(out=qnb_, in_=qn_), reads=[('qn', L)], writes=[('qnb', L)])
            yield
            pro, pkro = ps[4 + L], psk[4 + L]
            S.op('pe', lambda: PE.matmul(pro, lhsT=cmb[:, ROT, :], rhs=qnb_, start=True, stop=True), reads=[('qnb', L), 'cmb'], writes=[pkro], pe_acc=True)
            S.op('pool', lambda: POOL.tensor_tensor(out=t1_, in0=qn_, in1=cosT[:, sl], op=ALU.mult), reads=[('qn', L), 'tab'], writes=[('t1', L)])
            yield
            S.op('dve', lambda: V.tensor_tensor(out=t2_, in0=pro, in1=sinT[:, sl], op=ALU.mult), reads=[pkro, 'tab'], writes=[('t2', L)])
            dst = qT[:, c, sl] if c < 4 else kTt[:, sl]
            S.op('dve', lambda: V.tensor_tensor(out=dst, in0=t1_, in1=t2_, op=ALU.add), reads=[('t1', L), ('t2', L)], writes=['qk'])
            yield

        def run_tasks(tasks):
            tasks = list(tasks)
            while tasks:
                for t_ in list(tasks):
                    try:
                        next(t_)
                    except StopIteration:
                        tasks.remove(t_)

        S.dma('pool', wq[0], win_d[:, c0_of(0):c0_of(0) + 128].rearrange("(k p) n -> p k n", p=128), writes=[('wq', 0)])
        for c in range(5):
            if c + 1 < 5:
                S.dma('pool', wq[(c + 1) % 2], win_d[:, c0_of(c + 1):c0_of(c + 1) + 128].rearrange("(k p) n -> p k n", p=128), writes=[('wq', (c + 1) % 2)])
            for tb in range(0, NB, 2):
                run_tasks([gen_qk(c, tb, 0), gen_qk(c, tb + 1, 1)])
        S.op('pool', lambda: POOL.memset(vp.rearrange("p a b c -> p (a b c)"), 0.0), writes=['vp'])
        S.dma('pool', wq[0], win_d[:, 2560:2688].rearrange("(k p) n -> p k n", p=128), writes=[('wq', 0)])
        for i in range(NT):
            pz, pk = ps[i % 2], psk[i % 2]
            for k in range(8):
                S.op('pe', lambda: PE.matmul(pz[:, 0:128], lhsT=uT[:, k, i * 128:(i + 1) * 128], rhs=wq[0][:, k, :], start=(k == 0), stop=(k == 7)),
                     reads=[('wq', 0), ('uT', i // 4)], writes=[pk], pe_acc=True)
            S.op('act', lambda: ACT.copy(out=vp[:, 0, i, 0:64], in_=pz[:, 0:64]), reads=[pk], writes=['vp'])
            S.op('dve', lambda: V.tensor_copy(out=vp[:, 1, i, 64:128], in_=pz[:, 64:128]), reads=[pk], writes=['vp'])
        for n in range(NT):
            qs = slice(n * 128, (n + 1) * 128)
            kbs = [kb for kb in (n - 1, n, n + 1) if 0 <= kb < NT]
            items = [(g, kb) for g in range(2) for kb in kbs]
            for idx, (g, kb) in enumerate(items):
                gp = slice(g * 64, (g + 1) * 64)
                pz, pk = ps[idx % 4], psk[idx % 4]
                S.op('pe', lambda: PE.matmul(pz.rearrange("p (j q) -> p j q", j=4), lhsT=kTt[gp, kb * 128:(kb + 1) * 128], rhs=qT[gp, :, qs], start=True, stop=True),
                     reads=['qk'], writes=[pk], pe_acc=True)
                pt_ = pTs[idx]
                S.op('act', lambda: ACT.activation(out=pt_, in_=pz, func=AF.Exp, scale=0.125), reads=[pk], writes=[('pT', idx)])
                if kb != n:
                    mk = cmb[:, MPREV if kb < n else MNEXT, :]
                    S.op('pool', lambda: POOL.tensor_tensor(out=pt_.rearrange("p (j q) -> p j q", j=4), in0=pt_.rearrange("p (j q) -> p j q", j=4),
                                                            in1=bc(mk.rearrange("p (o q) -> p o q", o=1), [128, 4, 128]), op=ALU.mult),
                         reads=[('pT', idx), 'cmb'], writes=[('pT', idx)])
            po, pko = ps[4 + n % 2], psk[4 + n % 2]
            pd_, pkd = ps[6 + n % 2], psk[6 + n % 2]
            for idx, (g, kb) in enumerate(items):
                S.op('pe', lambda: PE.matmul(po, lhsT=vp[:, g, kb, :], rhs=pTs[idx], start=(idx == 0), stop=(idx == len(items) - 1)),
                     reads=['vp', ('pT', idx)], writes=[pko], pe_acc=True)
            for idx, (g, kb) in enumerate(items):
                S.op('pe', lambda: PE.matmul(pd_, lhsT=cmb[:, VP[g], :], rhs=pTs[idx], start=(idx == 0), stop=(idx == len(items) - 1)),
                     reads=['cmb', ('pT', idx)], writes=[pkd], pe_acc=True)
            S.op('dve', lambda: V.tensor_tensor(out=dn.rearrange("p (j q) -> p j q", j=4), in0=pd_.rearrange("p (j q) -> p j q", j=4),
                                                in1=bc(esk.rearrange("p (j o) -> p j o", o=1), [128, 4, 128]), op=ALU.add), reads=[pkd, 'esk'], writes=['dn'])
            S.op('dve', lambda: V.reciprocal(out=dn, in_=dn), reads=['dn'], writes=['dn'])
            S.op('dve', lambda: V.tensor_tensor(out=ybT[:, :, qs], in0=po.rearrange("p (j q) -> p j q", j=4), in1=dn.rearrange("p (j q) -> p j q", j=4), op=ALU.mult),
                 reads=[pko, 'dn'], writes=['ybT'])

    def phase_merge(uT, yaT, ybT, mergedT, offs):
        AR.seek(offs[0])
        prw = AR.alloc([128, 4, D], BF16)
        AR.seek(offs[1])
        pat = AR.alloc([128, 4, D], BF16)
        wga = [AR.alloc([128, 8, 128], BF16) for _ in range(2)]
        wgb = [AR.alloc([128, 8, 128], BF16) for _ in range(2)]
        sga = AR.alloc([128, 512], BF16)
        sgb = AR.alloc([128, 512], BF16)
        t1 = AR.alloc([128, 512], F32)
        t2 = AR.alloc([128, 512], F32)
        for hh in range(2):
            S.dma('pool', prw[:, hh * 2:(hh + 1) * 2, :], prw_d[hh * 256:(hh + 1) * 256, :].rearrange("(k p) n -> p k n", p=128), writes=['prw'])
            S.dma('pool', pat[:, hh * 2:(hh + 1) * 2, :], pat_d[hh * 256:(hh + 1) * 256, :].rearrange("(k p) n -> p k n", p=128), writes=['pat'])
        for oc in range(8):
            b = oc % 2
            S.dma('pool', wga[b], win_d[:, 2688 + oc * 128:2688 + (oc + 1) * 128].rearrange("(k p) n -> p k n", p=128), writes=[('wga', b)])
            S.dma('pool', wgb[b], win_d[:, 3712 + oc * 128:3712 + (oc + 1) * 128].rearrange("(k p) n -> p k n", p=128), writes=[('wgb', b)])
            for tb in range(NB):
                sl = slice(tb * 512, (tb + 1) * 512)
                for k in range(8):
                    S.op('pe', lambda: PE.matmul(ps[0], lhsT=wga[b][:, k, :], rhs=uT[:, k, sl], start=(k == 0), stop=(k == 7)),
                         reads=[('wga', b), ('uT', tb)], writes=[psk[0]], pe_acc=True)
                S.op('act', lambda: ACT.activation(out=sga, in_=ps[0], func=AF.Sigmoid), reads=[psk[0]], writes=['sga'])
                for k in range(8):
                    S.op('pe', lambda: PE.matmul(ps[1], lhsT=wgb[b][:, k, :], rhs=uT[:, k, sl], start=(k == 0), stop=(k == 7)),
                         reads=[('wgb', b), ('uT', tb)], writes=[psk[1]], pe_acc=True)
                S.op('act', lambda: ACT.activation(out=sgb, in_=ps[1], func=AF.Sigmoid), reads=[psk[1]], writes=['sgb'])
                for k in range(4):
                    S.op('pe', lambda: PE.matmul(ps[2], lhsT=prw[:, k, oc * 128:(oc + 1) * 128], rhs=yaT[:, k, sl], start=(k == 0), stop=(k == 3)),
                         reads=['prw', 'yaT'], writes=[psk[2]], pe_acc=True)
                for k in range(4):
                    S.op('pe', lambda: PE.matmul(ps[3], lhsT=pat[:, k, oc * 128:(oc + 1) * 128], rhs=ybT[:, k, sl], start=(k == 0), stop=(k == 3)),
                         reads=['pat', 'ybT'], writes=[psk[3]], pe_acc=True)
                S.op('dve', lambda: V.tensor_tensor(out=t1, in0=ps[2], in1=sga, op=ALU.mult), reads=[psk[2], 'sga'], writes=['t1'])
                S.op('dve', lambda: V.tensor_tensor(out=t2, in0=ps[3], in1=sgb, op=ALU.mult), reads=[psk[3], 'sgb'], writes=['t2'])
                S.op('pool', lambda: POOL.tensor_tensor(out=mergedT[:, oc, sl], in0=t1, in1=t2, op=ALU.add), reads=['t1', 't2'], writes=[('mg', tb)])

    def phase_x1(s, mergedT, u2tm, base):
        AR.seek(base)
        wo = AR.alloc([128, 8, D], BF16)
        gt1B = AR.alloc([128, D], F32)
        sc2 = AR.alloc([128, D], F32)
        sh2 = AR.alloc([128, D], F32)
        xt = [AR.alloc([128, D], F32) for _ in range(2)]
        x1t = [AR.alloc([128, D], F32) for _ in range(2)]
        tmpP = [AR.alloc([128, D], F32) for _ in range(2)]
        junkP = [AR.alloc([128, D], BF16) for _ in range(2)]
        u2TP = [AR.alloc([128, 8, 128], BF16) for _ in range(2)]
        ssP = [AR.alloc([128, 8], F32) for _ in range(2)]
        exP = [AR.alloc([128, E], F32) for _ in range(2)]
        for hh in range(4):
            S.dma('pool', wo[:, hh * 2:(hh + 1) * 2, :], wout_d[hh * 256:(hh + 1) * 256, :].rearrange("(k p) n -> p k n", p=128), writes=['wo'])
        S.dma('sp', gt1B, mod_d[s, 2], writes=['gt1B'])
        S.dma('sp', sc2, mod_d[s, 4], writes=['sc2'])
        S.dma('sp', sh2, mod_d[s, 3], writes=['sh2'])
        for i in range(NT):
            b = i % 2
            sl = slice(i * 128, (i + 1) * 128)
            S.dma('sp', xt[b], x_d[s, sl, :], writes=[('xt', b)])
            tmp, junk, u2T, ss, ex = tmpP[b], junkP[b], u2TP[b], ssP[b], exP[b]
            for cb in range(2):
                for k in range(8):
                    S.op('pe', lambda: PE.matmul(ps[cb + 6 * b], lhsT=mergedT[:, k, sl], rhs=wo[:, k, cb * 512:(cb + 1) * 512], start=(k == 0), stop=(k == 7)),
                         reads=[('mg', i // 4), 'wo'], writes=[psk[cb + 6 * b]], pe_acc=True)
                S.op('dve', lambda: V.tensor_tensor(out=tmp[:, cb * 512:(cb + 1) * 512], in0=ps[cb + 6 * b], in1=gt1B[:, cb * 512:(cb + 1) * 512], op=ALU.mult),
                     reads=[psk[cb + 6 * b], 'gt1B'], writes=[('tmp', b, cb)])
            S.op('pool', lambda: POOL.tensor_tensor(out=x1t[b], in0=tmp, in1=xt[b], op=ALU.add), reads=[('tmp', b, 0), ('tmp', b, 1), ('xt', b)], writes=[('x1t', b)])
            S.dma('sp', out_d[s, sl, :], x1t[b], reads=[('x1t', b)], writes=[('outd', i)])
            S.op('act', lambda: ACT.activation(out=junk, in_=x1t[b], func=AF.Square, accum_out=ss[:, 0:1]), reads=[('x1t', b)], writes=[('junk', b), ('ss0', b)])
            S.op('act', lambda: ACT.activation(out=ss[:, 1:2], in_=ss[:, 0:1], func=AF.Sqrt, bias=epsc[:, 0:1], scale=1.0 / D), reads=[('ss0', b), 'epsc'], writes=[('ss1', b)])
            S.op('dve', lambda: V.reciprocal(out=ss[:, 1:2], in_=ss[:, 1:2]), reads=[('ss1', b)], writes=[('ss1', b)])
            S.op('dve', lambda: V.scalar_tensor_tensor(out=tmp, in0=x1t[b], scalar=ss[:, 1:2], in1=sc2, op0=ALU.mult, op1=ALU.mult),
                 reads=[('x1t', b), ('ss1', b), 'sc2'], writes=[('tmp', b, 0), ('tmp', b, 1)])
            S.op('pool', lambda: POOL.tensor_tensor(out=u2tm[:, i, :], in0=tmp, in1=sh2, op=ALU.add), reads=[('tmp', b, 0), ('tmp', b, 1), 'sh2'], writes=[('u2', i)])
            pz = ps[2 + i % 2].bitcast(BF16).rearrange("p (k t) -> p k t", k=8)
            pk = psk[2 + i % 2]
            for k in range(8):
                S.op('pe', lambda: PE.transpose(out=pz[:, k, :], in_=u2tm[:, i, k * 128:(k + 1) * 128], identity=ident),
                     reads=[('u2', i), 'ident'], writes=[pk], pe_acc=True)
            S.op('act', lambda: ACT.copy(out=u2T, in_=pz), reads=[pk], writes=[('u2T', b)])
            pl, pkl = ps[4 + i % 2], psk[4 + i % 2]
            for k in range(8):
                S.op('pe', lambda: PE.matmul(pl[:, 0:E], lhsT=u2T[:, k, :], rhs=wr[:, k, :], start=(k == 0), stop=(k == 7)),
                     reads=[('u2T', b), 'wr'], writes=[pkl], pe_acc=True)
            S.op('dve', lambda: V.tensor_reduce(out=ss[:, 2:3], in_=pl[:, 0:E], axis=AX.X, op=ALU.max), reads=[pkl], writes=[('ss2', b)])
            S.op('dve', lambda: V.tensor_scalar(out=ss[:, 3:4], in0=ss[:, 2:3], scalar1=-1.0, scalar2=None, op0=ALU.mult), reads=[('ss2', b)], writes=[('ss3', b)])
            S.op('act', lambda: ACT.activation(out=ex, in_=pl[:, 0:E], func=AF.Exp, bias=ss[:, 3:4], scale=1.0, accum_out=ss[:, 4:5]), reads=[pkl, ('ss3', b)], writes=[('ex', b), ('ss4', b)])
            S.op('dve', lambda: V.reciprocal(out=ss[:, 5:6], in_=ss[:, 4:5]), reads=[('ss4', b)], writes=[('ss5', b)])
            S.op('dve', lambda: V.tensor_scalar(out=afftm[:, i, :], in0=ex, scalar1=ss[:, 5:6], scalar2=None, op0=ALU.mult), reads=[('ex', b), ('ss5', b)], writes=['afftm'])

    def phase_moe(s, u2tm, base):
        AR.seek(base)
        affT = AR.alloc([16, T], F32)
        work = AR.alloc([16, T], F32)
        maskT = AR.alloc([16, T], F32)
        slotT = AR.alloc([16, T], F32)
        mx8 = AR.alloc([16, 8], F32)
        for i in range(NT):
            pz = ps[i // 4]
            S.op('pe', lambda: PE.transpose(out=pz[0:16, (i % 4) * 128:(i % 4 + 1) * 128], in_=afftm[:, i, :], identity=identf),
                 reads=['afftm', 'identf'], writes=[psk[i // 4]], pe_acc=True)
        for q in range(4):
            S.op('act', lambda: ACT.copy(out=affT[:, q * 512:(q + 1) * 512], in_=ps[q][0:16, :]), reads=[psk[q]], writes=['affT'])
        S.op('dve', lambda: V.tensor_copy(out=work, in_=affT), reads=['affT'], writes=['work'])
        for it in range(CAP // 8):
            S.op('dve', lambda: V.max(out=mx8, in_=work), reads=['work'], writes=['mx8'])
            if it < CAP // 8 - 1:
                S.op('dve', lambda: V.match_replace(out=work, in_to_replace=mx8, in_values=work, imm_value=-1.0), reads=['work', 'mx8'], writes=['work'])
        S.op('dve', lambda: V.tensor_scalar(out=maskT, in0=affT, scalar1=mx8[:, 7:8], scalar2=None, op0=ALU.is_ge), reads=['affT', 'mx8'], writes=['maskT'])
        S.op('pool', lambda: POOL.memset(work, 1.0), reads=['work'], writes=['work'])
        S.op('dve', lambda: V.tensor_tensor_scan(out=slotT, data0=work, data1=maskT, initial=0.0, op0=ALU.mult, op1=ALU.add), reads=['work', 'maskT'], writes=['slotT'])
        S.op('dve', lambda: V.tensor_tensor(out=slotT, in0=slotT, in1=maskT, op=ALU.mult), reads=['slotT', 'maskT'], writes=['slotT'])
        S.op('dve', lambda: V.tensor_scalar(out=slotT, in0=slotT, scalar1=-1.0, scalar2=None, op0=ALU.add), reads=['slotT'], writes=['slotT'])
        pz = ps[4]
        for i in range(NT):
            S.op('pe', lambda: PE.transpose(out=pz[:, i * 16:(i + 1) * 16], in_=slotT[:, i * 128:(i + 1) * 128], identity=identf[0:16, 0:16]),
                 reads=['slotT', 'identf'], writes=[psk[4]], pe_acc=True)
        S.op('act', lambda: ACT.copy(out=slot_tm.rearrange("p i e -> p (i e)"), in_=pz[:, 0:256]), reads=[psk[4]], writes=['slot_tm'])
        S.op('dve', lambda: V.tensor_copy(out=affhl[:, :, :, 0], in_=afftm), reads=['afftm'], writes=['affhl'])
        S.op('dve', lambda: V.tensor_tensor(out=affhl[:, :, :, 1], in0=afftm, in1=affhl[:, :, :, 0], op=ALU.subtract), reads=['afftm', 'affhl'], writes=['affhl'])
        S.barrier()
        AR.seek(base)
        ye = AR.alloc([128, E, 2, D], BF16)
        Wg = AR.alloc([128, 8, D], BF16)
        Wu = AR.alloc([128, 8, D], BF16)
        Wd = AR.alloc([128, 8, D], BF16)
        wbase = AR.ptr
        Pe = AR.alloc([128, NT, CAP], BF16)
        xeT = AR.alloc([128, 8, CAP], BF16)
        hT = AR.alloc([128, 8, CAP], BF16)
        hs = AR.alloc([128, CAP], F32)
        affs = AR.alloc([128, 4], F32)
        gt2B = AR.alloc([128, D], F32)
        S.dma('sp', gt2B, mod_d[s, 5], writes=['gt2B'])
        for e in range(E):
            for (wt, wsrc, nm) in ((Wg, wg_d, 'Wg'), (Wu, wu_d, 'Wu'), (Wd, wd_d, 'Wd')):
                for hh in range(4):
                    S.dma('pool', wt[:, hh * 2:(hh + 1) * 2, :], wsrc[e, hh * 256:(hh + 1) * 256, :].rearrange("(k p) n -> p k n", p=128), writes=[(nm, hh)])
            for i in range(NT):
                S.op('dve', lambda: V.tensor_scalar(out=Pe[:, i, :], in0=iota_row, scalar1=slot_tm[:, i, e:e + 1], scalar2=None, op0=ALU.is_equal),
                     reads=['iota_row', 'slot_tm'], writes=[('Pe', i)])
            for fc in range(8):
                pz, pk = ps[fc // 2], psk[fc // 2]
                pzs = pz[:, (fc % 2) * 256:(fc % 2 + 1) * 256]
                for i in range(NT):
                    S.op('pe', lambda: PE.matmul(pzs, lhsT=u2tm[:, i, fc * 128:(fc + 1) * 128], rhs=Pe[:, i, :], start=(i == 0), stop=(i == NT - 1)),
                         reads=[('u2', i), ('Pe', i)], writes=[pk], pe_acc=True)
                S.op('act', lambda: ACT.copy(out=xeT[:, fc, :], in_=pzs), reads=[pk], writes=[('xeT', fc)])
            pa, pka = ps[4], psk[4]
            for half in range(2):
                for i in range(NT):
                    S.op('pe', lambda: PE.matmul(pa[:, half * 2:(half + 1) * 2], lhsT=Pe[:, i, half * 128:(half + 1) * 128], rhs=affhl[:, i, e, :], start=(i == 0), stop=(i == NT - 1)),
                         reads=[('Pe', i), 'affhl'], writes=[pka], pe_acc=True)
            S.op('dve', lambda: V.tensor_reduce(out=affs[:, 0:2], in_=pa[:, 0:4].rearrange("p (h t) -> p h t", t=2), axis=AX.X, op=ALU.add), reads=[pka], writes=['affs'])
            for fk in range(8):
                pg, pkg = ps[5], psk[5]
                pu, pku = ps[6], psk[6]
                for k in range(8):
                    S.op('pe', lambda: PE.matmul(pg[:, 0:CAP], lhsT=Wg[:, k, fk * 128:(fk + 1) * 128], rhs=xeT[:, k, :], start=(k == 0), stop=(k == 7)),
                         reads=[('Wg', k // 2), ('xeT', k)], writes=[pkg], pe_acc=True)
                for k in range(8):
                    S.op('pe', lambda: PE.matmul(pu[:, 0:CAP], lhsT=Wu[:, k, fk * 128:(fk + 1) * 128], rhs=xeT[:, k, :], start=(k == 0), stop=(k == 7)),
                         reads=[('Wu', k // 2), ('xeT', k)], writes=[pku], pe_acc=True)
                S.op('act', lambda: ACT.activation(out=hs, in_=pg[:, 0:CAP], func=AF.Silu), reads=[pkg], writes=['hs'])
                S.op('dve', lambda: V.tensor_tensor(out=hT[:, fk, :], in0=pu[:, 0:CAP], in1=hs, op=ALU.mult), reads=[pku, 'hs'], writes=[('hT', fk)])
            for half in range(2):
                for cb in range(2):
                    py, pky = ps[7] if (half * 2 + cb) % 2 else ps[4], psk[7] if (half * 2 + cb) % 2 else psk[4]
                    for fk in range(8):
                        S.op('pe', lambda: PE.matmul(py, lhsT=hT[:, fk, half * 128:(half + 1) * 128], rhs=Wd[:, fk, cb * 512:(cb + 1) * 512], start=(fk == 0), stop=(fk == 7)),
                             reads=[('hT', fk), ('Wd', fk // 2), 'affs'], writes=[pky], pe_acc=True)
                    S.op('dve', lambda: V.tensor_scalar(out=ye[:, e, half, cb * 512:(cb + 1) * 512], in0=py, scalar1=affs[:, half:half + 1], scalar2=None, op0=ALU.mult),
                         reads=[pky, 'affs'], writes=['ye'])
        S.barrier()
        AR.seek(wbase - 3 * 8 * D * 2)
        Pall = AR.alloc([128, E, CAP], BF16)
        PT = AR.alloc([128, 2 * E, 128], BF16)
        x1t = [AR.alloc([128, D], F32) for _ in range(2)]
        ot = [AR.alloc([128, D], F32) for _ in range(2)]
        for i in range(NT):
            b = i % 2
            sl = slice(i * 128, (i + 1) * 128)
            S.dma('sp', x1t[b], out_d[s, sl, :], reads=[('outd', i)], writes=[('x1t', b)])
            for e in range(E):
                S.op('dve', lambda: V.tensor_scalar(out=Pall[:, e, :], in0=iota_row, scalar1=slot_tm[:, i, e:e + 1], scalar2=None, op0=ALU.is_equal),
                     reads=['iota_row', 'slot_tm'], writes=[('Pall', e // 4)])
            for q in range(4):
                pz = ps[q].bitcast(BF16).rearrange("p (j t) -> p j t", j=8)
                for j in range(8):
                    idx = q * 8 + j
                    e, half = idx // 2, idx % 2
                    S.op('pe', lambda: PE.transpose(out=pz[:, j, :], in_=Pall[:, e, half * 128:(half + 1) * 128], identity=ident),
                         reads=[('Pall', e // 4), 'ident'], writes=[psk[q]], pe_acc=True)
                if q % 2 == 0:
                    S.op('act', lambda: ACT.copy(out=PT[:, q * 8:(q + 1) * 8, :], in_=pz), reads=[psk[q]], writes=[('PT', q)])
                else:
                    S.op('dve', lambda: V.tensor_copy(out=PT[:, q * 8:(q + 1) * 8, :], in_=pz), reads=[psk[q]], writes=[('PT', q)])
            for cb in range(2):
                po, pko = ps[4 + cb + 2 * (i % 2)], psk[4 + cb + 2 * (i % 2)]
                for idx in range(2 * E):
                    e, half = idx // 2, idx % 2
                    S.op('pe', lambda: PE.matmul(po, lhsT=PT[:, idx, :], rhs=ye[:, e, half, cb * 512:(cb + 1) * 512], start=(idx == 0), stop=(idx == 2 * E - 1)),
                         reads=[('PT', idx // 8), 'ye'], writes=[pko], pe_acc=True)
                S.op('dve', lambda: V.tensor_tensor(out=ot[b][:, cb * 512:(cb + 1) * 512], in0=po, in1=gt2B[:, cb * 512:(cb + 1) * 512], op=ALU.mult),
                     reads=[pko, 'gt2B'], writes=[('ot', b, cb)])
            S.op('dve', lambda: V.tensor_tensor(out=ot[b], in0=ot[b], in1=x1t[b], op=ALU.add), reads=[('ot', b, 0), ('ot', b, 1), ('x1t', b)], writes=[('ot', b, 0), ('ot', b, 1)])
            S.dma('sp', out_d[s, sl, :], ot[b], reads=[('ot', b, 0), ('ot', b, 1)], writes=[('outd', i)])

    def dbg_dump(src_ap, shape, key_reads=()):
        AR.seek(AR_TOP)
        t = AR.alloc(shape, F32)
        S.op('dve', lambda: V.tensor_copy(out=t, in_=src_ap), writes=['dbgt'])
        flat = t if len(shape) == 2 else t.rearrange("p a b -> p (a b)")
        S.dma('sp', dbg_d, flat, reads=['dbgt'])

    AR_TOP = 160 * 1024
    phase_adaln()
    for s in range(nseq):
        AR.seek(0)
        zsT = AR.alloc([128, 15, T], BF16)
        uT = AR.alloc([128, 8, T], BF16)
        base1 = AR.ptr
        phase_norm1(s, uT, base1)
        S.barrier()
        if dbg and dbg[0] == 'uT':
            dbg_dump(uT[:, :, 0:512], [128, 8, 512]); break
        for q in range(4):
            S.dma('sp', u_d[:, 2 * q:2 * q + 2, :], uT[:, 2 * q:2 * q + 2, :], reads=[('uT', 0), ('uT', 1), ('uT', 2), ('uT', 3)], writes=['uscr'])
        phase_rwkv_cols(uT, zsT, base1)
        S.barrier()
        if dbg and dbg[0] == 'zs':
            dbg_dump(zsT[:, :, 0:256], [128, 15, 256]); break
        AR.seek(61440)
        kkT = AR.alloc([128, 4, T], BF16)
        yaT = AR.alloc([128, 4, T], BF16)
        base3 = AR.ptr
        phase_scan(zsT, kkT, base3)
        if dbg and dbg[0] == 'yscan':
            AR.seek(AR_TOP)
            t = AR.alloc([128, 2, 512], F32)
            S.dma('sp', t[:, 0, :], y_d[0, 0:128, :], writes=['dbgt'])
            S.dma('sp', t[:, 1, :], y_d[1, 0:128, :], writes=['dbgt'])
            S.dma('sp', dbg_d, t.rearrange("p a b -> p (a b)"), reads=['dbgt']); break
        phase_post(zsT, yaT, base3)
        S.barrier()
        if dbg and dbg[0] == 'yaT':
            dbg_dump(yaT[:, :, 0:512], [128, 4, 512]); break
        AR.seek(0)
        uT = AR.alloc([128, 8, T], BF16)
        AR.seek(94208)
        ybT = AR.alloc([128, 4, T], BF16)
        baseB = AR.ptr
        for q in range(4):
            S.dma('sp' if q % 2 == 0 else 'act', uT[:, 2 * q:2 * q + 2, :], u_d[:, 2 * q:2 * q + 2, :], writes=[('uT', 0), ('uT', 1), ('uT', 2), ('uT', 3)])
        phase_attn(s, uT, ybT, 32768, baseB)
        S.barrier()
        if dbg and dbg[0] == 'ybT':
            dbg_dump(ybT[:, :, 0:512], [128, 4, 512]); break
        AR.seek(32768)
        mergedT = AR.alloc([128, 8, T], BF16)
        phase_merge(uT, yaT, ybT, mergedT, (65536, baseB))
        S.barrier()
        if dbg and dbg[0] == 'merged':
            dbg_dump(mergedT[:, :, 0:512], [128, 8, 512]); break
        AR.seek(0)
        u2tm = AR.alloc([128, NT, D], BF16)
        phase_x1(s, mergedT, u2tm, 65536)
        S.barrier()
        if dbg and dbg[0] == 'aff':
            dbg_dump(afftm.rearrange("p i e -> p (i e)"), [128, 256]); break
        phase_moe(s, u2tm, 32768)
        S.barrier()

    S.finish('sp')
    print("ninstr", S.ninstr, "pe_incs", S.npe_inc, "arena hi", AR.hi)
    return nc


def _consts():
    cm = np.zeros((13, 128, 128), np.float32)
    p = np.arange(128)
    cm[0] = (p[:, None] // 64 == p[None, :] // 64).astype(np.float32)
    R = np.zeros((128, 128), np.float32)
    for blk in range(2):
        o = blk * 64
        for d_ in range(8):
            R[o + d_ + 8, o + d_] = -1.0
            R[o + d_, o + d_ + 8] = 1.0
    cm[1] = R
    cm[2] = (p[:, None] >= p[None, :]).astype(np.float32)
    cm[3] = (p[:, None] <= p[None, :]).astype(np.float32)
    s_ = (p % 64)[:, None]
    t_ = (p % 64)[None, :]
    a_col = (p[None, :] >= 64)
    fwd = np.where(a_col, s_ < t_, s_ <= t_)
    bwd = np.where(a_col, s_ > t_, s_ >= t_)
    cm[4] = fwd.astype(np.float32)
    cm[5] = bwd.astype(np.float32)
    cm[6] = cm[4].T
    cm[7] = cm[5].T
    cm[8][:, 0] = (p < 64)
    cm[8][:, 1] = (p >= 64)
    cm[9][:, 0:64] = 1.0
    cm[10][:, 64:128] = 1.0
    cm[11] = ((p % 64)[:, None] < (p % 64)[None, :]).astype(np.float32)
    cm[12] = ((p % 64)[:, None] > (p % 64)[None, :]).astype(np.float32)
    return np.ascontiguousarray(cm.transpose(1, 0, 2).reshape(128, 13 * 128))


def _prep_shared(inp):
    f = lambda a: np.ascontiguousarray(np.asarray(a, dtype=np.float32))
    L = 0
    w_in = f(inp["w_in"][L]).copy()
    qoff = 1920
    perm = []
    for c in range(4):
        perm += list(range(c * 64, (c + 1) * 64)) + list(range((4 + c) * 64, (5 + c) * 64))
    perm = np.array(perm)
    w_in[:, qoff:qoff + 512] = w_in[:, qoff:qoff + 512][:, perm]
    p_attn = f(inp["p_attn"][L])[perm, :]
    pp = np.zeros((128, NPP), np.float32)

    def put(name, arr):
        o, w = PP[name]
        pp[:, o:o + w] = arr

    chunked = lambda v: np.asarray(v, np.float32).reshape(-1, 128).T
    put("mp", chunked(inp["mu_prev"][L]))
    put("mn", chunked(inp["mu_next"][L]))
    put("w0", np.concatenate([chunked(inp["rwkv_w0"][L][0]), chunked(inp["rwkv_w0"][L][1])], 1))
    put("a0", np.concatenate([chunked(inp["rwkv_a0"][L][0]), chunked(inp["rwkv_a0"][L][1])], 1))
    put("kk", chunked(inp["rwkv_k_k"][L]))
    put("ka", chunked(inp["rwkv_k_a"][L]))
    put("rk", chunked(np.asarray(inp["rwkv_r_k"][L]).reshape(-1)))
    put("qg", np.tile(np.asarray(inp["q_norm_g"][L], np.float32), 2)[:, None])
    put("kg", np.tile(np.asarray(inp["k_norm_g"][L], np.float32), 2)[:, None])
    inv_freq = (500000.0 ** (-np.arange(0, 16, 2, dtype=np.float32) / 16)).astype(np.float32)
    invf = np.zeros(64, np.float32)
    invf[0:8] = inv_freq
    invf[8:16] = inv_freq
    put("invf", np.tile(invf, 2)[:, None])
    sink = np.asarray(inp["attn_sink"][L], np.float32)
    sk = np.zeros((128, 4), np.float32)
    for j in range(4):
        sk[0:64, j] = sink[j]
        sk[64:128, j] = sink[4 + j]
    put("sink", sk)
    w2cat = np.zeros((128, 2, 512), np.float32)
    a2cat = np.zeros((128, 2, 512), np.float32)
    for d_ in range(2):
        w2cat[d_ * 64:(d_ + 1) * 64, d_, :] = inp["rwkv_w2"][L][d_]
        a2cat[d_ * 64:(d_ + 1) * 64, d_, :] = inp["rwkv_a2"][L][d_]
    return {
        "w_ada": f(inp["w_ada"][L]), "b_ada": f(inp["b_ada"][L])[None, :] if np.asarray(inp["b_ada"][L]).ndim == 1 else f(inp["b_ada"][L]),
        "norm1_g": f(inp["norm1_g"][L]).reshape(1, D), "norm2_g": f(inp["norm2_g"][L]).reshape(1, D),
        "w_in": w_in, "pp": pp, "w2cat": w2cat.reshape(128, 1024), "a2cat": a2cat.reshape(128, 1024),
        "g2": f(inp["rwkv_g2"][L]), "gn_w": f(inp["rwkv_gn_w"][L]).reshape(1, 512), "gn_b": f(inp["rwkv_gn_b"][L]).reshape(1, 512),
        "p_rwkv": f(inp["p_rwkv"][L]), "p_attn": np.ascontiguousarray(p_attn), "w_out": f(inp["w_out"][L]),
        "w_router": f(inp["w_router"][L]), "w_gate": f(inp["w_gate"][L]), "w_up": f(inp["w_up"][L]), "w_down": f(inp["w_down"][L]),
        "cmats": _consts(),
    }


def _core_inputs(inp, shared, seqs):
    x = np.ascontiguousarray(np.asarray(inp["x"], np.float32)[seqs])
    c = np.asarray(inp["c"], np.float32)[seqs]
    cT = np.ascontiguousarray(c.reshape(len(seqs), 8, 128).transpose(0, 2, 1))
    pos = np.ascontiguousarray(np.asarray(inp["positions"]).astype(np.int32)[seqs][:, None, :])
    m = dict(shared)
    m.update({"x": x, "cT": cT, "pos": pos})
    return m


def kernel(**inputs):
    shared = _prep_shared(inputs)
    nc = build(NSEQ)
    in_maps = [_core_inputs(inputs, shared, list(range(i * NSEQ, (i + 1) * NSEQ))) for i in range(NCORES)]
    res = run_bass_kernel_spmd(nc, in_maps, core_ids=list(range(NCORES)))
    out = np.concatenate([np.asarray(r["out"]) for r in res.results], axis=0)
    return out.astype(np.float32)
```

```python
import numpy as np
import concourse.bass as bass
import concourse.mybir as mybir
from concourse.bass_utils import run_bass_kernel_spmd

F32 = mybir.dt.float32
BF16 = mybir.dt.bfloat16
I32 = mybir.dt.int32
ALU = mybir.AluOpType
AF = mybir.ActivationFunctionType
AX = mybir.AxisListType

T = 2048
D = 1024
NT = 16
NB = 4
NSEQ = 2
NCORES = 8
E = 16
CAP = 256
LAM = float(np.exp(-0.5))
NCH = 4
TBS = NCH * 64
NTB = T // TBS
TWO_PI = float(2 * np.pi)
C1 = 6.28125
C2 = TWO_PI - C1

PP = {}
_o = 0
for _n, _w in [("mp", 15), ("mn", 15), ("w0", 8), ("a0", 8), ("kk", 4), ("ka", 4), ("rk", 4), ("qg", 1), ("kg", 1),
               ("invf", 1), ("sink", 4)]:
    PP[_n] = (_o, _w)
    _o += _w
NPP = _o


class Ticket:
    __slots__ = ('ins', 'sem', 'val', 'parent')

    def __init__(self, ins):
        self.ins = ins
        self.sem = None
        self.val = None
        self.parent = None

    def root(self):
        t = self
        while t.parent is not None:
            t = t.parent
        return t


class Sync:
    SEM_MAX = 30000

    def __init__(self, nc):
        self.nc = nc
        self.E = {'pe': nc.tensor, 'act': nc.scalar, 'dve': nc.vector, 'pool': nc.gpsimd, 'sp': nc.sync}
        self.sem = {}
        self.cnt = {}
        self.nsem = 0
        for e in self.E:
            self._newsem(e)
        self.waited = {}
        self.lastw = {}
        self.reads = {}
        self.dma_sems = {}
        self.dma_rr = {}
        self.ninstr = 0
        self.pend = None
        self.pend_writes = None
        self.npe_inc = 0

    def _newsem(self, e):
        self.sem[e] = self.nc.alloc_semaphore(f"s_{e}_{self.nsem}")
        self.nsem += 1
        self.cnt[e] = 0

    def _flush_pe(self):
        t = self.pend
        if t is None:
            return
        if self.cnt['pe'] >= self.SEM_MAX:
            self._newsem('pe')
        self.cnt['pe'] += 1
        t.sem = self.sem['pe']
        t.val = self.cnt['pe']
        t.ins.then_inc(t.sem, 1)
        self.npe_inc += 1
        self.pend = None
        self.pend_writes = None

    def _wait(self, e, ev):
        if ev is None:
            return
        if isinstance(ev, Ticket):
            if e == 'pe':
                return
            t = ev.root()
            if t.val is None:
                assert t is self.pend
                self._flush_pe()
            sem, val = t.sem, t.val
        else:
            src, sem, val = ev
        k = (e, sem.name)
        if self.waited.get(k, 0) >= val:
            return
        self.waited[k] = val
        self.E[e].wait_ge(sem, val)

    def deps(self, e, reads, writes, pe_acc=False):
        for k in reads:
            self._wait(e, self.lastw.get(k))
        for k in writes:
            lw = self.lastw.get(k)
            if not (pe_acc and isinstance(lw, Ticket)):
                self._wait(e, lw)
            for ev in self.reads.get(k, {}).values():
                self._wait(e, ev)

    def commit(self, src, ev, reads, writes):
        for k in reads:
            self.reads.setdefault(k, {})[src] = ev
        for k in writes:
            self.lastw[k] = ev
            self.reads[k] = {}

    def op(self, e, fn, reads=(), writes=(), pe_acc=False):
        self.deps(e, reads, writes, pe_acc)
        if e == 'pe':
            ins = fn()
            t = Ticket(ins)
            if self.pend is not None:
                if self.pend_writes == tuple(writes):
                    self.pend.parent = t
                    self.pend = None
                else:
                    self._flush_pe()
            self.pend = t
            self.pend_writes = tuple(writes)
            self.commit('pe', t, reads, writes)
            self.ninstr += 1
            return t
        if self.cnt[e] >= self.SEM_MAX:
            self._newsem(e)
        ins = fn()
        self.cnt[e] += 1
        ev = (e, self.sem[e], self.cnt[e])
        ins.then_inc(self.sem[e], 1)
        self.commit(e, ev, reads, writes)
        self.ninstr += 1
        return ev

    def dma(self, e, out, in_, reads=(), writes=(), nslots=8, **kw):
        if e == 'pool':
            nslots = 2
        lst = self.dma_sems.setdefault(e, [])
        if len(lst) < nslots:
            lst.append([self.nc.alloc_semaphore(f"d_{e}_{len(lst)}"), 0])
        i = self.dma_rr.get(e, 0)
        self.dma_rr[e] = (i + 1) % nslots
        slot = lst[i % len(lst)]
        sem, uses = slot
        if uses > 0:
            self._wait(e, ('dma', sem, 16 * uses))
        self.deps(e, reads, writes)
        self.E[e].dma_start(out=out, in_=in_, **kw).then_inc(sem, 16)
        slot[1] = uses + 1
        ev = ('dma_%s_%d' % (e, i % len(lst)), sem, 16 * (uses + 1))
        self.commit(ev[0], ev, reads, writes)
        self.ninstr += 1
        return ev

    def barrier(self):
        self._flush_pe()
        evs = [(e, self.sem[e], self.cnt[e]) for e in self.E if self.cnt[e] > 0]
        for q, lst in self.dma_sems.items():
            for sem, uses in lst:
                if uses:
                    evs.append(('dma', sem, 16 * uses))
        for e in self.E:
            for ev in evs:
                if ev[0] != e:
                    self._wait(e, ev)
        self.lastw = {}
        self.reads = {}

    def finish(self, e='sp'):
        self._flush_pe()
        for q, lst in self.dma_sems.items():
            for sem, uses in lst:
                if uses:
                    self._wait(e, ('dma', sem, 16 * uses))


class Arena:
    def __init__(self, nc, name, nbytes):
        self.n4 = nbytes // 4
        self.t = nc.alloc_sbuf_tensor(name, [128, self.n4], F32).ap()
        self.ptr = 0
        self.hi = 0

    def seek(self, off):
        self.ptr = off

    def alloc(self, shape, dtype, parts=None):
        esz = 4 if dtype in (F32, I32) else 2
        n = int(np.prod(shape[1:]))
        nb = (n * esz + 31) // 32 * 32
        assert self.ptr % 4 == 0
        a = self.ptr // 4
        assert a + nb // 4 <= self.n4, f"arena overflow {self.ptr}+{nb} > {self.n4 * 4}"
        v = self.t[:, a:a + nb // 4]
        if dtype != F32:
            v = v.bitcast(dtype)
        v = v[0:shape[0], 0:n]
        if len(shape) > 2:
            names = " ".join(f"d{i}" for i in range(len(shape) - 1))
            kw = {f"d{i}": int(shape[i + 1]) for i in range(len(shape) - 1)}
            v = v.rearrange(f"p ({names}) -> p {names}", **kw)
        self.ptr += nb
        self.hi = max(self.hi, self.ptr)
        return v


def bc(ap, shape):
    return ap.to_broadcast(list(shape))


def build(nseq=NSEQ, dbg=None, stop_after=None):
    nc = bass.Bass("TRN2", target_bir_lowering=False)
    S = Sync(nc)
    V, ACT, POOL, PE = nc.vector, nc.scalar, nc.gpsimd, nc.tensor

    def din(name, shape, dt=F32):
        return nc.dram_tensor(name, list(shape), dt, kind="ExternalInput").ap()

    x_d = din("x", [nseq, T, D])
    cT_d = din("cT", [nseq, 128, 8])
    pos_d = din("pos", [nseq, 1, T], I32)
    wada_d = din("w_ada", [D, 6 * D])
    bada_d = din("b_ada", [1, 6 * D])
    n1g_d = din("norm1_g", [1, D])
    n2g_d = din("norm2_g", [1, D])
    win_d = din("w_in", [D, 4736])
    pp_d = din("pp", [128, NPP])
    w2c_d = din("w2cat", [128, 2 * 512])
    a2c_d = din("a2cat", [128, 2 * 512])
    g2_d = din("g2", [128, 512])
    gnw_d = din("gn_w", [1, 512])
    gnb_d = din("gn_b", [1, 512])
    prw_d = din("p_rwkv", [512, D])
    pat_d = din("p_attn", [512, D])
    wout_d = din("w_out", [D, D])
    wr_d = din("w_router", [D, E])
    wg_d = din("w_gate", [E, D, D])
    wu_d = din("w_up", [E, D, D])
    wd_d = din("w_down", [E, D, D])
    cm_d = din("cmats", [128, 13 * 128])
    out_d = nc.dram_tensor("out", [nseq, T, D], F32, kind="ExternalOutput").ap()
    mod_d = nc.dram_tensor("modscr", [nseq, 6, 128, D], F32, kind="Internal").ap()
    y_d = nc.dram_tensor("yscr", [2, T, 512], F32, kind="Internal").ap()
    u_d = nc.dram_tensor("uscr", [128, 8, T], BF16, kind="Internal").ap()
    dbg_d = None
    if dbg is not None:
        dbg_d = nc.dram_tensor("dbg", list(dbg[1]), F32, kind="ExternalOutput").ap()

    def sb(name, shape, dt=F32):
        return nc.alloc_sbuf_tensor('sb_' + name, list(shape), dt).ap()

    pp = sb("pp", [128, NPP])
    ident = sb("ident", [128, 128], BF16)
    identf = sb("identf", [128, 128])
    cmb = sb("cmb", [128, 13, 128], BF16)
    w2c = sb("w2c", [128, 2, 512], BF16)
    a2c = sb("a2c", [128, 2, 512], BF16)
    g2 = sb("g2", [128, 512], BF16)
    wr = sb("wr", [128, 8, E], BF16)
    epsc = sb("epsc", [128, 4])
    alpha = sb("alpha", [128, 15])
    oneminus_ka = sb("omka", [128, 4])
    two_omka = sb("omka2", [128, 4])
    negkkc = sb("negone", [128, 1])
    esk = sb("esk", [128, 4])
    rmask = sb("rmask", [128, TBS])
    iota_row = sb("iota_row", [128, CAP])
    ident4 = sb("ident4", [128, 4, 128], BF16)
    kar = sb("kar", [128, 4])
    c2r = sb("c2r", [128, 4])
    afftm = sb("afftm", [128, NT, E])
    slot_tm = sb("slot_tm", [128, NT, E])
    affhl = sb("affhl", [128, NT, E, 2], BF16)

    BLK1, ROT, MPREV, MNEXT = 0, 1, 2, 3
    MZT = (4, 5)
    MZ = (6, 7)
    HSEL = 8
    VP = (9, 10)

    ps = [nc.alloc_psum_tensor(f"ps{i}", [128, 512], F32).ap() for i in range(8)]
    psk = [f"ps{i}" for i in range(8)]

    AR = Arena(nc, "arena", 192 * 1024)

    def col(name, j=0, n=1):
        o, w = PP[name]
        return pp[:, o + j:o + j + n]

    S.dma('sp', pp, pp_d, writes=['pp'])
    S.dma('pool', cmb.rearrange("p a b -> p (a b)"), cm_d, writes=['cmb'])
    S.dma('pool', w2c.rearrange("p a b -> p (a b)"), w2c_d, writes=['w2c'])
    S.dma('pool', a2c.rearrange("p a b -> p (a b)"), a2c_d, writes=['a2c'])
    S.dma('pool', g2, g2_d, writes=['g2'])
    S.dma('pool', wr, wr_d.rearrange("(k p) e -> p k e", p=128), writes=['wr'])
    S.op('pool', lambda: POOL.memset(identf, 1.0), writes=['identf'])
    S.op('pool', lambda: POOL.affine_select(out=identf, in_=identf, pattern=[[1, 128]], compare_op=ALU.is_equal,
                                            fill=0.0, base=0, channel_multiplier=-1), reads=['identf'], writes=['identf'])
    S.op('dve', lambda: V.tensor_copy(out=ident, in_=identf), reads=['identf'], writes=['ident'])
    for j in range(4):
        S.op('dve', lambda: V.tensor_copy(out=ident4[:, j, :], in_=identf), reads=['identf'], writes=['ident4'])
    S.op('pool', lambda: POOL.memset(epsc[:, 0:1], 1e-6), writes=['epsc'])
    S.op('pool', lambda: POOL.memset(epsc[:, 1:2], 64e-5), reads=['epsc'], writes=['epsc'])
    S.op('pool', lambda: POOL.memset(epsc[:, 2:3], 1e-24), reads=['epsc'], writes=['epsc'])
    S.op('pool', lambda: POOL.memset(epsc[:, 3:4], 0.0), reads=['epsc'], writes=['epsc'])
    S.op('pool', lambda: POOL.memset(negkkc, -1.0), writes=['negone'])
    S.op('dve', lambda: V.tensor_tensor(out=alpha, in0=col("mp", 0, 15), in1=col("mn", 0, 15), op=ALU.add), reads=['pp'], writes=['alpha'])
    S.op('dve', lambda: V.tensor_scalar(out=alpha, in0=alpha, scalar1=-1.0, scalar2=1.0, op0=ALU.mult, op1=ALU.add), reads=['alpha'], writes=['alpha'])
    S.op('dve', lambda: V.tensor_scalar(out=oneminus_ka, in0=col("ka", 0, 4), scalar1=-1.0, scalar2=1.0, op0=ALU.mult, op1=ALU.add), reads=['pp'], writes=['omka'])
    S.op('dve', lambda: V.tensor_scalar(out=two_omka, in0=col("ka", 0, 4), scalar1=-2.0, scalar2=2.0, op0=ALU.mult, op1=ALU.add), reads=['pp'], writes=['omka2'])
    S.op('act', lambda: ACT.activation(out=esk, in_=col("sink", 0, 4), func=AF.Exp), reads=['pp'], writes=['esk'])
    S.op('dve', lambda: V.tensor_tensor(out=kar, in0=col("ka", 0, 4), in1=col("rk", 0, 4), op=ALU.mult), reads=['pp'], writes=['kar'])
    S.op('dve', lambda: V.tensor_tensor(out=c2r, in0=two_omka, in1=col("rk", 0, 4), op=ALU.mult), reads=['pp', 'omka2'], writes=['kar'])
    S.op('pool', lambda: POOL.memset(rmask, 1.0), writes=['rmask'])
    S.op('pool', lambda: POOL.memset(rmask.rearrange("p (c t) -> p c t", t=64)[:, :, 0:1], 0.0), reads=['rmask'], writes=['rmask'])
    S.op('pool', lambda: POOL.iota(iota_row, pattern=[[1, CAP]], base=0, channel_multiplier=0, allow_small_or_imprecise_dtypes=True), writes=['iota_row'])

    def debug_out(ap_sb, key, rows=None):
        S.dma('sp', dbg_d if rows is None else rows, ap_sb, reads=[key])

    def phase_adaln():
        AR.seek(0)
        csil = [AR.alloc([128, 8], F32) for _ in range(nseq)]
        crep = [AR.alloc([128, 9, 128], F32) for _ in range(nseq)]
        wblk = [AR.alloc([128, 9, 512], F32) for _ in range(3)]
        g1B = AR.alloc([128, D], F32)
        g2B = AR.alloc([128, D], F32)
        mt = [AR.alloc([128, 512], F32) for _ in range(4)]
        S.dma('sp', g1B, n1g_d.partition_broadcast(128), writes=['g1B'])
        S.dma('sp', g2B, n2g_d.partition_broadcast(128), writes=['g2B'])
        for b in range(3):
            S.op('pool', lambda: POOL.memset(wblk[b][:, 8, :], 0.0), writes=[('wblk', b)])
        for s in range(nseq):
            S.dma('sp', csil[s], cT_d[s], writes=[('csil', s)])
            S.op('act', lambda: ACT.activation(out=csil[s], in_=csil[s], func=AF.Silu), reads=[('csil', s)], writes=[('csil', s)])
            S.op('pool', lambda: POOL.memset(crep[s][:, 8, :], 0.0), writes=[('crep', s)])
            S.op('pool', lambda: POOL.memset(crep[s][0:1, 8, :], 1.0), reads=[('crep', s)], writes=[('crep', s)])
            S.op('dve', lambda: V.tensor_copy(out=crep[s][:, 0:8, :], in_=bc(csil[s].rearrange("p (k o) -> p k o", o=1), [128, 8, 128])),
                 reads=[('csil', s)], writes=[('crep', s)])
        ev = 0
        for jb in range(12):
            b = jb % 3
            piece = jb // 2
            c0 = jb * 512
            S.dma('sp', wblk[b][:, 0:4, :], wada_d[0:512, c0:c0 + 512].rearrange("(k p) n -> p k n", p=128), writes=[('wblk', b)])
            S.dma('act', wblk[b][:, 4:8, :], wada_d[512:1024, c0:c0 + 512].rearrange("(k p) n -> p k n", p=128), writes=[('wblk', b)])
            S.dma('sp', wblk[b][0:1, 8, :], bada_d[:, c0:c0 + 512], writes=[('wblk', b)])
            for s in range(nseq):
                pz, pkz = ps[ev % 4], psk[ev % 4]
                for k in range(9):
                    S.op('pe', lambda: PE.matmul(pz, lhsT=crep[s][:, k, :], rhs=wblk[b][:, k, :], start=(k == 0), stop=(k == 8)),
                         reads=[('crep', s), ('wblk', b)], writes=[pkz], pe_acc=True)
                m = mt[ev % 4]
                lc = (jb % 2) * 512
                if piece == 1:
                    S.op('dve', lambda: V.scalar_tensor_tensor(out=m, in0=pz, scalar=1.0, in1=g1B[:, lc:lc + 512], op0=ALU.add, op1=ALU.mult),
                         reads=[pkz, 'g1B'], writes=[('mt', ev % 4)])
                elif piece == 4:
                    S.op('dve', lambda: V.scalar_tensor_tensor(out=m, in0=pz, scalar=1.0, in1=g2B[:, lc:lc + 512], op0=ALU.add, op1=ALU.mult),
                         reads=[pkz, 'g2B'], writes=[('mt', ev % 4)])
                else:
                    S.op('act', lambda: ACT.copy(out=m, in_=pz), reads=[pkz], writes=[('mt', ev % 4)])
                S.dma('sp', mod_d[s, piece, :, lc:lc + 512], m, reads=[('mt', ev % 4)], writes=[('mod', s, piece)])
                ev += 1
        S.barrier()

    def phase_norm1(s, uT, base):
        AR.seek(base)
        scp = AR.alloc([128, D], F32)
        shp = AR.alloc([128, D], F32)
        xt = [AR.alloc([128, D], F32) for _ in range(2)]
        tmp2 = [AR.alloc([128, D], F32) for _ in range(2)]
        ub = [AR.alloc([128, D], BF16) for _ in range(2)]
        junk2 = [AR.alloc([128, D], BF16) for _ in range(2)]
        ss2 = [AR.alloc([128, 2], F32) for _ in range(2)]
        S.dma('sp', scp, mod_d[s, 1], reads=[('mod', s, 1)], writes=['scp'])
        S.dma('sp', shp, mod_d[s, 0], reads=[('mod', s, 0)], writes=['shp'])
        for i in range(NT):
            b = i % 2
            S.dma('sp', xt[b], x_d[s, i * 128:(i + 1) * 128, :], writes=[('xt', b)])
            tmp, junk, ss = tmp2[b], junk2[b], ss2[b]
            S.op('act', lambda: ACT.activation(out=junk, in_=xt[b], func=AF.Square, accum_out=ss[:, 0:1]), reads=[('xt', b)], writes=[('junk', b), ('ss', b)])
            S.op('act', lambda: ACT.activation(out=ss[:, 1:2], in_=ss[:, 0:1], func=AF.Sqrt, bias=epsc[:, 0:1], scale=1.0 / D), reads=[('ss', b), 'epsc'], writes=[('ss1', b)])
            S.op('dve', lambda: V.reciprocal(out=ss[:, 1:2], in_=ss[:, 1:2]), reads=[('ss1', b)], writes=[('ss1', b)])
            S.op('dve', lambda: V.scalar_tensor_tensor(out=tmp, in0=xt[b], scalar=ss[:, 1:2], in1=scp, op0=ALU.mult, op1=ALU.mult),
                 reads=[('xt', b), ('ss1', b), 'scp'], writes=[('tmp', b)])
            S.op('pool', lambda: POOL.tensor_tensor(out=ub[b], in0=tmp, in1=shp, op=ALU.add), reads=[('tmp', b), 'shp'], writes=[('ub', b)])
            pz = ps[i % 2].bitcast(BF16).rearrange("p (k t) -> p k t", k=8)
            for k in range(8):
                S.op('pe', lambda: PE.transpose(out=pz[:, k, :], in_=ub[b][:, k * 128:(k + 1) * 128], identity=ident),
                     reads=[('ub', b), 'ident'], writes=[psk[i % 2]], pe_acc=True)
            S.op('act', lambda: ACT.copy(out=uT[:, :, i * 128:(i + 1) * 128], in_=pz), reads=[psk[i % 2]], writes=[('uT', i // 4)])

    def phase_rwkv_cols(uT, zsT, base):
        AR.seek(base)
        wg = [AR.alloc([128, 8, 128], BF16) for _ in range(2)]
        ztmp = AR.alloc([128, T + 2], F32)
        sht = AR.alloc([128, T], F32)
        S.op('pool', lambda: POOL.memset(ztmp[:, 0:1], 0.0), writes=['ztmp'])
        S.op('pool', lambda: POOL.memset(ztmp[:, T + 1:T + 2], 0.0), reads=['ztmp'], writes=['ztmp'])
        for j in range(15):
            b = j % 2
            S.dma('pool', wg[b], win_d[:, j * 128:(j + 1) * 128].rearrange("(k p) n -> p k n", p=128), writes=[('wg', b)])
            for tb in range(NB):
                pz = ps[(j * NB + tb) % 4]
                pk = psk[(j * NB + tb) % 4]
                for k in range(8):
                    S.op('pe', lambda: PE.matmul(pz, lhsT=wg[b][:, k, :], rhs=uT[:, k, tb * 512:(tb + 1) * 512], start=(k == 0), stop=(k == 7)),
                         reads=[('wg', b), ('uT', tb)], writes=[pk], pe_acc=True)
                S.op('act', lambda: ACT.copy(out=ztmp[:, 1 + tb * 512:1 + (tb + 1) * 512], in_=pz), reads=[pk], writes=['ztmp'])
            S.op('dve', lambda: V.tensor_scalar(out=sht, in0=ztmp[:, 1:T + 1], scalar1=alpha[:, j:j + 1], scalar2=None, op0=ALU.mult),
                 reads=['ztmp', 'alpha'], writes=['sht'])
            S.op('dve', lambda: V.scalar_tensor_tensor(out=sht, in0=ztmp[:, 0:T], scalar=col("mp", j), in1=sht, op0=ALU.mult, op1=ALU.add),
                 reads=['ztmp', 'sht', 'pp'], writes=['sht'])
            S.op('dve', lambda: V.scalar_tensor_tensor(out=zsT[:, j, :], in0=ztmp[:, 2:T + 2], scalar=col("mn", j), in1=sht, op0=ALU.mult, op1=ALU.add),
                 reads=['ztmp', 'sht', 'pp'], writes=[('zs', j)])
            if j == 12:
                S.op('act', lambda: ACT.activation(out=zsT[:, j, :], in_=zsT[:, j, :], func=AF.Tanh), reads=[('zs', j)], writes=[('zs', j)])
            if j == 14:
                S.op('act', lambda: ACT.activation(out=zsT[:, j, :], in_=zsT[:, j, :], func=AF.Sigmoid), reads=[('zs', j)], writes=[('zs', j)])

    def phase_scan(zsT, kkT, base):
        rT = lambda c: zsT[:, c, :]
        kT = lambda c: zsT[:, 4 + c, :]
        vT = lambda c: zsT[:, 8 + c, :]
        wdT = zsT[:, 12, :]
        adT = zsT[:, 13, :]
        AR.seek(base)
        kraw = AR.alloc([128, 512], F32)
        ksq = AR.alloc([128, 512], BF16)
        krs = AR.alloc([128, 512], F32)
        for c in range(4):
            for tb in range(NB):
                sl = slice(tb * 512, (tb + 1) * 512)
                S.op('dve', lambda: V.tensor_scalar(out=kraw, in0=kT(c)[:, sl], scalar1=col("kk", c), scalar2=None, op0=ALU.mult), reads=[('zs', 4 + c), 'pp'], writes=['kraw'])
                S.op('act', lambda: ACT.activation(out=ksq, in_=kraw, func=AF.Square), reads=['kraw'], writes=['ksq'])
                pz, pk = ps[tb % 2], psk[tb % 2]
                S.op('pe', lambda: PE.matmul(pz, lhsT=cmb[:, BLK1, :], rhs=ksq, start=True, stop=True), reads=['ksq', 'cmb'], writes=[pk], pe_acc=True)
                S.op('act', lambda: ACT.activation(out=krs, in_=pz, func=AF.Sqrt, bias=epsc[:, 2:3], scale=1.0), reads=[pk, 'epsc'], writes=['krs'])
                S.op('dve', lambda: V.reciprocal(out=krs, in_=krs), reads=['krs'], writes=['krs'])
                S.op('dve', lambda: V.tensor_tensor(out=kkT[:, c, sl], in0=kraw, in1=krs, op=ALU.mult), reads=['kraw', 'krs'], writes=[('kk', c)])
        S.barrier()
        AR.seek(base)
        sg = AR.alloc([128, 4, TBS], F32)
        ad = AR.alloc([128, 4, TBS], F32)
        cc = AR.alloc([128, 4, TBS], F32)
        t1 = AR.alloc([128, 4, TBS], F32)
        ex = [[AR.alloc([128, TBS], F32) for _ in range(2)] for _ in range(4)]
        kd = AR.alloc([128, 4, TBS], F32)
        bb = AR.alloc([128, 4, TBS], F32)
        pdec = AR.alloc([128, 4, NCH], F32)
        ARz = AR.alloc([128, 4, NCH, 2, 2, 64], BF16)
        Bz = AR.alloc([128, 4, NCH, 2, 64], BF16)
        BKt = AR.alloc([128, 4, NCH, 2, 64], BF16)
        KBh = AR.alloc([128, 4, NCH, 2, 64], BF16)
        KBt = AR.alloc([128, 4, NCH, 128], BF16)
        VZ = AR.alloc([128, NCH, 8, 64], BF16)
        XV = AR.alloc([128, NCH, 8, 64], BF16)
        ZTs = [[AR.alloc([128, 4, 128], BF16) for _ in range(2)] for _ in range(NCH)]
        ATm = [[AR.alloc([128, 4, 128], BF16) for _ in range(2)] for _ in range(NCH)]
        PTm = [[AR.alloc([128, 4, 128], BF16) for _ in range(2)] for _ in range(2)]
        Pm = [[AR.alloc([128, 4, 128], BF16) for _ in range(2)] for _ in range(2)]
        Am = [[AR.alloc([128, 4, 128], BF16) for _ in range(2)] for _ in range(2)]
        W1s = AR.alloc([128, 4, 64], BF16)
        S32 = [AR.alloc([128, 4, 64], F32) for _ in range(2)]
        Sb = [AR.alloc([128, 4, 64], BF16) for _ in range(2)]
        ysb = [AR.alloc([64, 512], F32) for _ in range(2)]
        S.op('pool', lambda: POOL.memset(ARz.rearrange("p a b c d e -> p (a b c d e)"), 0.0), writes=['ARz'])
        S.op('pool', lambda: POOL.memset(Bz.rearrange("p a b c d -> p (a b c d)"), 0.0), writes=['Bz'])
        S.op('pool', lambda: POOL.memset(VZ.rearrange("p a b c -> p (a b c)"), 0.0), writes=['VZ'])

        def chain(gens):
            for g_ in gens:
                yield from g_

        def run_tasks(tasks):
            tasks = list(tasks)
            while tasks:
                for t_ in list(tasks):
                    try:
                        next(t_)
                    except StopIteration:
                        tasks.remove(t_)

        yev = 0
        pendQ = None
        for d in range(2):
            S.op('pool', lambda: POOL.memset(S32[d].rearrange("p a b -> p (a b)"), 0.0), writes=[('S32', d)])
            S.op('pool', lambda: POOL.memset(Sb[d].rearrange("p a b -> p (a b)"), 0.0), writes=[('Sb', d)])
            tbs = range(NTB) if d == 0 else range(NTB - 1, -1, -1)
            for tb in tbs:
                sl = slice(tb * TBS, (tb + 1) * TBS)
                def gen_prep(c):
                    pz, pk = ps[c % 2], psk[c % 2]
                    S.op('pe', lambda: PE.matmul(pz[:, 0:TBS], lhsT=w2c[:, d, c * 128:(c + 1) * 128], rhs=wdT[:, sl], start=True, stop=True),
                         reads=['w2c', ('zs', 12)], writes=[pk], pe_acc=True)
                    S.op('act', lambda: ACT.activation(out=sg[:, c, :], in_=pz[:, 0:TBS], func=AF.Sigmoid, bias=col("w0", d * 4 + c), scale=1.0),
                         reads=[pk, 'pp'], writes=[('sg', c)])
                    pz2, pk2 = ps[2 + c % 2], psk[2 + c % 2]
                    S.op('pe', lambda: PE.matmul(pz2[:, 0:TBS], lhsT=a2c[:, d, c * 128:(c + 1) * 128], rhs=adT[:, sl], start=True, stop=True),
                         reads=['a2c', ('zs', 13)], writes=[pk2], pe_acc=True)
                    S.op('act', lambda: ACT.activation(out=ad[:, c, :], in_=pz2[:, 0:TBS], func=AF.Sigmoid, bias=col("a0", d * 4 + c), scale=1.0),
                         reads=[pk2, 'pp'], writes=[('ad', c)])
                    yield
                    S.op('dve', lambda: V.tensor_tensor_scan(out=cc[:, c, :], data0=rmask, data1=sg[:, c, :], initial=0.0, op0=ALU.mult, op1=ALU.add),
                         reads=['rmask', ('sg', c)], writes=[('cc', c)])
                    cc3 = cc[:, c, :].rearrange("p (h t) -> p h t", t=64)
                    sg3 = sg[:, c, :].rearrange("p (h t) -> p h t", t=64)
                    t13 = t1[:, c, :].rearrange("p (h t) -> p h t", t=64)
                    if d == 1:
                        S.op('dve', lambda: V.tensor_tensor(out=t13, in0=bc(cc3[:, :, 63:64], [128, NCH, 64]), in1=cc3, op=ALU.subtract),
                             reads=[('cc', c)], writes=[('t1', c)])
                        S.op('dve', lambda: V.tensor_tensor(out=cc[:, c, :], in0=t1[:, c, :], in1=sg[:, c, :], op=ALU.add),
                             reads=[('t1', c), ('sg', c)], writes=[('cc', c)])
                    totp = 63 if d == 0 else 0
                    S.op('pool', lambda: POOL.tensor_scalar(out=kd[:, c, :], in0=ad[:, c, :], scalar1=col("ka", c), scalar2=oneminus_ka[:, c:c + 1], op0=ALU.mult, op1=ALU.add),
                         reads=[('ad', c), 'pp', 'omka'], writes=[('kd', c)])
                    S.op('pool', lambda: POOL.tensor_tensor(out=kd[:, c, :], in0=kd[:, c, :], in1=kT(c)[:, sl], op=ALU.mult),
                         reads=[('kd', c), ('zs', 4 + c)], writes=[('kd', c)])
                    S.op('pool', lambda: POOL.tensor_tensor(out=bb[:, c, :], in0=ad[:, c, :], in1=kkT[:, c, sl], op=ALU.mult),
                         reads=[('ad', c), ('kk', c)], writes=[('bb', c)])
                    yield
                    e = ex[c][0]
                    S.op('act', lambda: ACT.activation(out=e, in_=cc[:, c, :], func=AF.Exp, scale=-LAM), reads=[('cc', c)], writes=[('ex', c, 0)])
                    for hp in range(2):
                        pr = slice(hp * 64, (hp + 1) * 64)
                        S.op('dve', lambda: V.tensor_tensor(out=ARz[pr, c, :, 0, hp, :], in0=rT(c)[pr, sl].rearrange("p (h t) -> p h t", t=64),
                                                            in1=e[pr, :].rearrange("p (h t) -> p h t", t=64), op=ALU.mult),
                             reads=[('zs', c), ('ex', c, 0)], writes=['ARz'])
                    yield
                    e = ex[c][1]
                    S.op('act', lambda: ACT.activation(out=e, in_=cc[:, c, :], func=AF.Exp, scale=LAM), reads=[('cc', c)], writes=[('ex', c, 1)])
                    S.op('dve', lambda: V.tensor_tensor(out=BKt[:, c, :, 0, :], in0=kd[:, c, :].rearrange("p (h t) -> p h t", t=64),
                                                        in1=e.rearrange("p (h t) -> p h t", t=64), op=ALU.mult),
                         reads=[('kd', c), ('ex', c, 1)], writes=['BKt'])
                    S.op('dve', lambda: V.tensor_tensor(out=BKt[:, c, :, 1, :], in0=bb[:, c, :].rearrange("p (h t) -> p h t", t=64),
                                                        in1=e.rearrange("p (h t) -> p h t", t=64), op=ALU.mult),
                         reads=[('bb', c), ('ex', c, 1)], writes=['BKt'])
                    for hp in range(2):
                        pr = slice(hp * 64, (hp + 1) * 64)
                        S.op('act', lambda: ACT.copy(out=Bz[pr, c, :, hp, :], in_=BKt[pr, c, :, 1, :]), reads=['BKt'], writes=['Bz'])
                    yield
                    S.op('dve', lambda: V.tensor_tensor(out=t1[:, c, :], in0=cc[:, c, :], in1=sg[:, c, :], op=ALU.subtract),
                         reads=[('cc', c), ('sg', c)], writes=[('t1', c)])
                    e = ex[c][0]
                    S.op('act', lambda: ACT.activation(out=e, in_=t1[:, c, :], func=AF.Exp, scale=-LAM), reads=[('t1', c)], writes=[('ex', c, 0)])
                    for hp in range(2):
                        pr = slice(hp * 64, (hp + 1) * 64)
                        S.op('dve', lambda: V.scalar_tensor_tensor(out=ARz[pr, c, :, 1, hp, :], in0=kkT[pr, c, sl].rearrange("p (h t) -> p h t", t=64),
                                                                   scalar=-1.0, in1=e[pr, :].rearrange("p (h t) -> p h t", t=64), op0=ALU.mult, op1=ALU.mult),
                             reads=[('kk', c), ('ex', c, 0)], writes=['ARz'])
                    yield
                    S.op('dve', lambda: V.tensor_tensor(out=t13, in0=bc(cc3[:, :, totp:totp + 1], [128, NCH, 64]), in1=cc3, op=ALU.subtract),
                         reads=[('cc', c)], writes=[('t1', c)])
                    e = ex[c][1]
                    S.op('act', lambda: ACT.activation(out=e, in_=t1[:, c, :], func=AF.Exp, scale=-LAM), reads=[('t1', c)], writes=[('ex', c, 1)])
                    S.op('pool', lambda: POOL.tensor_tensor(out=KBh[:, c, :, 0, :], in0=kd[:, c, :].rearrange("p (h t) -> p h t", t=64),
                                                        in1=e.rearrange("p (h t) -> p h t", t=64), op=ALU.mult),
                         reads=[('kd', c), ('ex', c, 1)], writes=['KBh'])
                    S.op('pool', lambda: POOL.tensor_tensor(out=KBh[:, c, :, 1, :], in0=bb[:, c, :].rearrange("p (h t) -> p h t", t=64),
                                                        in1=e.rearrange("p (h t) -> p h t", t=64), op=ALU.mult),
                         reads=[('bb', c), ('ex', c, 1)], writes=['KBh'])
                    S.op('act', lambda: ACT.activation(out=pdec[:, c, :].rearrange("p (h o) -> p h o", o=1), in_=cc3[:, :, totp:totp + 1], func=AF.Exp, scale=-LAM), reads=[('cc', c)], writes=['pdec'])
                ptasks = [gen_prep(c_) for c_ in range(4)]
                for t_ in ptasks:
                    next(t_)
                if pendQ is not None:
                    for _ in range(3):
                        next(pendQ, None)
                for t_ in ptasks:
                    next(t_)
                if pendQ is not None:
                    run_tasks([pendQ])
                    pendQ = None
                run_tasks(ptasks)
                for ch in range(NCH):
                    pz = ps[4 + ch % 2].bitcast(BF16)
                    pk = psk[4 + ch % 2]
                    pzv = pz[0:64, 0:512].rearrange("p (c n) -> p c n", c=4)
                    for c in range(4):
                        S.op('pe', lambda: PE.transpose(out=pzv[:, c, :], in_=vT(c)[:, tb * TBS + ch * 64: tb * TBS + (ch + 1) * 64], identity=ident),
                             reads=[('zs', 8 + c), 'ident'], writes=[pk], pe_acc=True)
                    S.op('act', lambda: ACT.copy(out=VZ[0:64, ch, :, :].rearrange("p h v -> p (h v)"), in_=pz[0:64, 0:512]), reads=[pk], writes=[('VZ', ch)])
                    S.op('act', lambda: ACT.copy(out=XV[0:64, ch, :, :].rearrange("p h v -> p (h v)"), in_=pz[0:64, 0:512]), reads=[pk], writes=[('XVv', ch)])
                    pzk = pz[:, 512:1024].rearrange("p (c n) -> p c n", c=4)
                    for c in range(4):
                        S.op('pe', lambda: PE.transpose(out=pzk[:, c, :], in_=KBh[:, c, ch, :, :].rearrange("p a t -> p (a t)"), identity=ident),
                             reads=['KBh', 'ident'], writes=[pk], pe_acc=True)
                    S.op('dve', lambda: V.tensor_copy(out=KBt[:, :, ch, :], in_=pzk), reads=[pk], writes=[('KBt', ch)])
                MNT = cmb[:, 11 + d, :]
                MN = cmb[:, 12 - d, :]

                def gen_D(ch, slot, par):
                    pA, pkA = ps[2 * par], psk[2 * par]
                    pB, pkB = ps[2 * par + 1], psk[2 * par + 1]
                    pA3 = pA.rearrange("p (j n) -> p j n", j=4)
                    pB3 = pB.rearrange("p (j n) -> p j n", j=4)
                    mzt = cmb[:, MZT[d], :]
                    for half in range(2):
                        pz3 = pA3 if half == 0 else pB3
                        pkz = pkA if half == 0 else pkB
                        for j in range(4):
                            h = half * 4 + j
                            c, hp = h // 2, h % 2
                            bk = BKt[:, c, ch, :, :].rearrange("p a t -> p (a t)")
                            S.op('pe', lambda: PE.matmul(pz3[:, j, :].rearrange("p (a t) -> p a t", a=2), lhsT=bk, rhs=ARz[:, c, ch, :, hp, :], start=True, stop=True),
                                 reads=['BKt', 'ARz'], writes=[pkz], pe_acc=True)
                        S.op('dve', lambda: V.tensor_tensor(out=ZTs[slot][half], in0=pz3, in1=bc(mzt.rearrange("p (o n) -> p o n", o=1), [128, 4, 128]), op=ALU.mult),
                             reads=[pkz, 'cmb'], writes=[('ZTs', slot, half)])
                    yield
                    for c in range(4):
                        bz = Bz[:, c, ch, :, :].rearrange("p a t -> p (a t)")
                        az = ARz[:, c, ch, 1, :, :].rearrange("p a t -> p (a t)")
                        S.op('pe', lambda: PE.matmul(pA3[:, c, :], lhsT=bz, rhs=az, start=True, stop=True), reads=['Bz', 'ARz'], writes=[pkA], pe_acc=True)
                        S.op('pe', lambda: PE.matmul(pB3[:, c, :], lhsT=az, rhs=bz, start=True, stop=True), reads=['Bz', 'ARz'], writes=[pkB], pe_acc=True)
                    S.op('dve', lambda: V.tensor_tensor(out=PTm[par][0], in0=pA3, in1=bc(MNT.rearrange("p (o n) -> p o n", o=1), [128, 4, 128]), op=ALU.mult),
                         reads=[pkA, 'cmb'], writes=[('PT', par, 0)])
                    S.op('dve', lambda: V.tensor_tensor(out=Pm[par][0], in0=pB3, in1=bc(MN.rearrange("p (o n) -> p o n", o=1), [128, 4, 128]), op=ALU.mult),
                         reads=[pkB, 'cmb'], writes=[('P', par, 0)])
                    S.op('pool', lambda: POOL.tensor_tensor(out=ATm[slot][0], in0=PTm[par][0], in1=ident4, op=ALU.add), reads=[('PT', par, 0), 'ident4'], writes=[('AT', slot, 0)])
                    S.op('pool', lambda: POOL.tensor_tensor(out=Am[par][0], in0=Pm[par][0], in1=ident4, op=ALU.add), reads=[('P', par, 0), 'ident4'], writes=[('A', par, 0)])
                    yield
                    cur = 0
                    for lev in range(1, 6):
                        nxt = 1 - cur
                        for j in range(4):
                            S.op('pe', lambda: PE.matmul(pA3[:, j, :], lhsT=Pm[par][cur][:, j, :], rhs=PTm[par][cur][:, j, :], start=True, stop=True),
                                 reads=[('P', par, cur), ('PT', par, cur)], writes=[pkA], pe_acc=True)
                            if lev < 5:
                                S.op('pe', lambda: PE.matmul(pB3[:, j, :], lhsT=PTm[par][cur][:, j, :], rhs=Pm[par][cur][:, j, :], start=True, stop=True),
                                     reads=[('P', par, cur), ('PT', par, cur)], writes=[pkB], pe_acc=True)
                        S.op('act', lambda: ACT.copy(out=PTm[par][nxt], in_=pA3), reads=[pkA], writes=[('PT', par, nxt)])
                        if lev < 5:
                            S.op('dve', lambda: V.tensor_copy(out=Pm[par][nxt], in_=pB3), reads=[pkB], writes=[('P', par, nxt)])
                        yield
                        for j in range(4):
                            S.op('pe', lambda: PE.matmul(pA3[:, j, :], lhsT=Am[par][cur][:, j, :], rhs=PTm[par][nxt][:, j, :], start=True, stop=True),
                                 reads=[('A', par, cur), ('PT', par, nxt)], writes=[pkA], pe_acc=True)
                            if lev < 5:
                                S.op('pe', lambda: PE.matmul(pB3[:, j, :], lhsT=PTm[par][nxt][:, j, :], rhs=Am[par][cur][:, j, :], start=True, stop=True),
                                     reads=[('A', par, cur), ('PT', par, nxt)], writes=[pkB], pe_acc=True)
                        S.op('dve', lambda: V.tensor_tensor(out=ATm[slot][nxt], in0=pA3, in1=ATm[slot][cur], op=ALU.add), reads=[pkA, ('AT', slot, cur)], writes=[('AT', slot, nxt)])
                        if lev < 5:
                            S.op('dve', lambda: V.tensor_tensor(out=Am[par][nxt], in0=pB3, in1=Am[par][cur], op=ALU.add), reads=[pkB, ('A', par, cur)], writes=[('A', par, nxt)])
                        yield
                        cur = nxt
                    assert cur == 1

                def gen_Q(ch, slot, tb=tb, d=d):
                    nonlocal yev
                    fin = 1
                    gch = tb * NCH + ch
                    pW, pkW = ps[4], psk[4]
                    pW3 = pW[:, 0:256].rearrange("p (c v) -> p c v", c=4)
                    for h in range(8):
                        c, hp = h // 2, h % 2
                        S.op('pe', lambda: PE.matmul(pW3[hp * 64:(hp + 1) * 64, c, :], lhsT=ZTs[slot][h // 4][:, h % 4, 64:128], rhs=VZ[:, ch, h, :], start=True, stop=False),
                             reads=[('ZTs', slot, h // 4), ('VZ', ch)], writes=[pkW], pe_acc=True)
                        S.op('pe', lambda: PE.matmul(pW3[hp * 64:(hp + 1) * 64, c, :], lhsT=ARz[:, c, ch, 1, hp, :], rhs=Sb[d][:, c, :], start=False, stop=True),
                             reads=['ARz', ('Sb', d)], writes=[pkW], pe_acc=True)
                    S.op('act', lambda: ACT.copy(out=W1s, in_=pW3), reads=[pkW], writes=['W1s'])
                    yield
                    pX, pkX = ps[5], psk[5]
                    pX3 = pX.rearrange("p (h v) -> p h v", h=8)
                    for h in range(8):
                        c, hp = h // 2, h % 2
                        S.op('pe', lambda: PE.matmul(pX3[64:128, h, :], lhsT=ATm[slot][fin][:, c, hp * 64:(hp + 1) * 64], rhs=W1s[:, c, :], start=True, stop=True),
                             reads=[('AT', slot, fin), 'W1s'], writes=[pkX], pe_acc=True)
                    S.op('dve', lambda: V.tensor_copy(out=XV[64:128, ch, :, :], in_=pX3[64:128]), reads=[pkX], writes=[('XVu', ch)])
                    yield
                    pS, pkS = ps[7], psk[7]
                    pS3 = pS[:, 0:256].rearrange("p (c v) -> p c v", c=4)
                    for h in range(8):
                        c, hp = h // 2, h % 2
                        S.op('pe', lambda: PE.matmul(pS3[hp * 64:(hp + 1) * 64, c, :], lhsT=KBt[:, c, ch, hp * 64:(hp + 1) * 64], rhs=XV[:, ch, h, :], start=True, stop=True),
                             reads=[('KBt', ch), ('XVv', ch), ('XVu', ch)], writes=[pkS], pe_acc=True)
                    pY, pkY = ps[6], psk[6]
                    pY3 = pY.rearrange("p (h v) -> p h v", h=8)
                    for h in range(8):
                        c, hp = h // 2, h % 2
                        S.op('pe', lambda: PE.matmul(pY3[0:64, h, :], lhsT=ZTs[slot][h // 4][:, h % 4, 0:64], rhs=XV[:, ch, h, :], start=True, stop=False),
                             reads=[('ZTs', slot, h // 4), ('XVv', ch), ('XVu', ch)], writes=[pkY], pe_acc=True)
                        S.op('pe', lambda: PE.matmul(pY3[0:64, h, :], lhsT=ARz[:, c, ch, 0, hp, :], rhs=Sb[d][:, c, :], start=False, stop=True),
                             reads=['ARz', ('Sb', d)], writes=[pkY], pe_acc=True)
                    for c in range(4):
                        S.op('dve', lambda: V.scalar_tensor_tensor(out=S32[d][:, c, :], in0=S32[d][:, c, :], scalar=pdec[:, c, ch:ch + 1], in1=pS3[:, c, :], op0=ALU.mult, op1=ALU.add),
                             reads=[('S32', d), 'pdec', pkS], writes=[('S32', d)])
                    S.op('act', lambda: ACT.copy(out=Sb[d], in_=S32[d]), reads=[('S32', d)], writes=[('Sb', d)])
                    yb_ = ysb[yev % 2]
                    S.op('act', lambda: ACT.copy(out=yb_, in_=pY[0:64, :]), reads=[pkY], writes=[('ysb', yev % 2)])
                    S.dma('sp', y_d[d, gch * 64:(gch + 1) * 64, :], yb_, reads=[('ysb', yev % 2)], writes=[('yscr', d, gch // 2)])
                    yev += 1
                    yield

                chs = list(range(NCH)) if d == 0 else list(range(NCH - 1, -1, -1))
                pend = []
                for r in range(0, NCH, 2):
                    tasks = [gen_D(chs[r], r, 0), gen_D(chs[r + 1], r + 1, 1)]
                    if pend:
                        tasks.append(chain([gen_Q(c_, s_) for (c_, s_) in pend]))
                    run_tasks(tasks)
                    pend = [(chs[r], r), (chs[r + 1], r + 1)]
                pendQ = chain([gen_Q(c_, s_) for (c_, s_) in pend])
        if pendQ is not None:
            run_tasks([pendQ])
            pendQ = None
        S.barrier()


    def phase_post(zsT, yaT, base):
        rT4 = zsT[:, 0:4, :]
        kT4 = zsT[:, 4:8, :]
        adT = zsT[:, 13, :]
        gdT = zsT[:, 14, :]
        AR.seek(base)
        gnwB = AR.alloc([128, 512], F32)
        gnbB = AR.alloc([128, 512], F32)
        P2 = lambda shape, dt: [AR.alloc(shape, dt) for _ in range(2)]
        Yf, Yb = P2([128, 512], F32), P2([128, 512], F32)
        ta0, ta1 = P2([128, 4, 128], F32), P2([128, 4, 128], F32)
        kf2 = P2([128, 4, 128], F32)
        prod2 = P2([128, 4, 128], BF16)
        rows2 = P2([128, 8], F32)
        bon2 = P2([128, 512], F32)
        y2 = P2([128, 512], F32)
        sq2 = P2([128, 512], F32)
        st2 = P2([128, 4, 8], F32)
        yab2 = P2([128, 512], BF16)
        S.dma('sp', gnwB, gnw_d.partition_broadcast(128), writes=['gnwB'])
        S.dma('sp', gnbB, gnb_d.partition_broadcast(128), writes=['gnbB'])
        def gen_tile(i):
            b = i % 2
            sl = slice(i * 128, (i + 1) * 128)
            ta = (ta0[b], ta1[b])
            kf, prod, rows, bon, y, sq, st, yab = kf2[b], prod2[b], rows2[b], bon2[b], y2[b], sq2[b], st2[b], yab2[b]
            bA, bB, bC, bD = 4 * b, 4 * b + 1, 4 * b + 2, 4 * b + 3
            S.dma('sp', Yf[b], y_d[0, sl, :], writes=[('Yf', b)])
            S.dma('sp', Yb[b], y_d[1, sl, :], writes=[('Yb', b)])
            for d in range(2):
                bk_ = bA if d == 0 else bB
                pz3 = ps[bk_].rearrange("p (c n) -> p c n", c=4)
                for c in range(4):
                    S.op('pe', lambda: PE.matmul(pz3[:, c, :], lhsT=a2c[:, d, c * 128:(c + 1) * 128], rhs=adT[:, sl], start=True, stop=True),
                         reads=['a2c'], writes=[psk[bk_]], pe_acc=True)
                for c in range(4):
                    S.op('act', lambda: ACT.activation(out=ta[d][:, c, :], in_=pz3[:, c, :], func=AF.Sigmoid, bias=col("a0", d * 4 + c), scale=1.0),
                         reads=[psk[bk_], 'pp'], writes=[('ta', b, d)])
            yield
            S.op('pool', lambda: POOL.tensor_tensor(out=ta[0], in0=ta[0], in1=ta[1], op=ALU.add), reads=[('ta', b, 0), ('ta', b, 1)], writes=[('ta', b, 0)])
            for c in range(4):
                S.op('pool', lambda: POOL.tensor_scalar(out=kf[:, c, :], in0=ta[0][:, c, :], scalar1=kar[:, c:c + 1], scalar2=c2r[:, c:c + 1], op0=ALU.mult, op1=ALU.add),
                     reads=[('ta', b, 0), 'kar'], writes=[('kf', b)])
            S.op('pool', lambda: POOL.tensor_tensor(out=kf, in0=kf, in1=kT4[:, :, sl], op=ALU.mult), reads=[('kf', b)], writes=[('kf', b)])
            S.op('pool', lambda: POOL.tensor_tensor(out=prod, in0=kf, in1=rT4[:, :, sl], op=ALU.mult), reads=[('kf', b)], writes=[('prod', b)])
            yield
            pr = ps[bB]
            for c in range(4):
                S.op('pe', lambda: PE.matmul(pr[:, c * 2:(c + 1) * 2], lhsT=prod[:, c, :], rhs=cmb[:, HSEL, 0:2], start=True, stop=True),
                     reads=[('prod', b), 'cmb'], writes=[psk[bB]], pe_acc=True)
            S.op('act', lambda: ACT.copy(out=rows, in_=pr[:, 0:8]), reads=[psk[bB]], writes=[('rows', b)])
            yield
            pv = ps[bD].bitcast(BF16)[:, 0:512]
            for c in range(4):
                S.op('pe', lambda: PE.transpose(out=pv[:, c * 128:(c + 1) * 128], in_=zsT[:, 8 + c, sl], identity=ident),
                     reads=['ident'], writes=[psk[bD]], pe_acc=True)
            S.op('dve', lambda: V.tensor_tensor(out=bon.rearrange("p (h v) -> p h v", h=8), in0=pv.rearrange("p (h v) -> p h v", h=8),
                                                in1=bc(rows.rearrange("p (h o) -> p h o", o=1), [128, 8, 64]), op=ALU.mult),
                 reads=[psk[bD], ('rows', b)], writes=[('bon', b)])
            yield
            pg = ps[bC]
            S.op('pe', lambda: PE.matmul(pg, lhsT=gdT[:, sl], rhs=g2, start=True, stop=True), reads=['g2'], writes=[psk[bC]], pe_acc=True)
            y3 = y.rearrange("p (h v) -> p h v", h=8)
            sq3 = sq.rearrange("p (h v) -> p h v", h=8)
            S.op('dve', lambda: V.tensor_tensor(out=y, in0=Yf[b], in1=Yb[b], op=ALU.add), reads=[('Yf', b), ('Yb', b)], writes=[('y', b)])
            yield
            S.op('dve', lambda: V.tensor_reduce(out=st[:, 0, :], in_=y3, axis=AX.X, op=ALU.add), reads=[('y', b)], writes=[('st0', b)])
            S.op('dve', lambda: V.tensor_scalar(out=st[:, 1, :], in0=st[:, 0, :], scalar1=-1.0 / 64, scalar2=None, op0=ALU.mult), reads=[('st0', b)], writes=[('st1', b)])
            S.op('dve', lambda: V.tensor_tensor(out=y3, in0=y3, in1=bc(st[:, 1, :].rearrange("p (h o) -> p h o", o=1), [128, 8, 64]), op=ALU.add),
                 reads=[('y', b), ('st1', b)], writes=[('y', b)])
            yield
            S.op('act', lambda: ACT.activation(out=sq, in_=y, func=AF.Square), reads=[('y', b)], writes=[('sq', b)])
            S.op('dve', lambda: V.tensor_reduce(out=st[:, 2, :], in_=sq3, axis=AX.X, op=ALU.add), reads=[('sq', b)], writes=[('st2', b)])
            yield
            S.op('act', lambda: ACT.activation(out=st[:, 3, :], in_=st[:, 2, :], func=AF.Sqrt, bias=epsc[:, 1:2], scale=1.0 / 64), reads=[('st2', b), 'epsc'], writes=[('st3', b)])
            S.op('dve', lambda: V.reciprocal(out=st[:, 3, :], in_=st[:, 3, :]), reads=[('st3', b)], writes=[('st3', b)])
            S.op('dve', lambda: V.tensor_tensor(out=y3, in0=y3, in1=bc(st[:, 3, :].rearrange("p (h o) -> p h o", o=1), [128, 8, 64]), op=ALU.mult),
                 reads=[('y', b), ('st3', b)], writes=[('y', b)])
            yield
            S.op('dve', lambda: V.tensor_tensor(out=y, in0=y, in1=gnwB, op=ALU.mult), reads=[('y', b), 'gnwB'], writes=[('y', b)])
            S.op('pool', lambda: POOL.tensor_tensor(out=bon, in0=bon, in1=gnbB, op=ALU.add), reads=[('bon', b), 'gnbB'], writes=[('bon', b)])
            S.op('dve', lambda: V.tensor_tensor(out=y, in0=y, in1=bon, op=ALU.add), reads=[('y', b), ('bon', b)], writes=[('y', b)])
            S.op('dve', lambda: V.tensor_tensor(out=yab, in0=y, in1=pg, op=ALU.mult), reads=[('y', b), psk[bC]], writes=[('yab', b)])
            yield
            pt = ps[bA].bitcast(BF16)[:, 0:512]
            for c in range(4):
                S.op('pe', lambda: PE.transpose(out=pt[:, c * 128:(c + 1) * 128], in_=yab[:, c * 128:(c + 1) * 128], identity=ident),
                     reads=[('yab', b), 'ident'], writes=[psk[bA]], pe_acc=True)
            S.op('act', lambda: ACT.copy(out=yaT[:, :, sl], in_=pt.rearrange("p (c n) -> p c n", c=4)), reads=[psk[bA]], writes=['yaT'])
            yield

        def run_tasks(tasks):
            tasks = list(tasks)
            while tasks:
                for t_ in list(tasks):
                    try:
                        next(t_)
                    except StopIteration:
                        tasks.remove(t_)

        for i in range(0, NT, 2):
            run_tasks([gen_tile(i), gen_tile(i + 1)])

    def phase_attn(s, uT, ybT, baseA, baseB):
        AR.seek(baseA)
        cosT = AR.alloc([128, T], F32)
        sinT = AR.alloc([128, T], F32)
        qT = AR.alloc([128, 4, T], BF16)
        kTt = AR.alloc([128, T], BF16)
        vp = AR.alloc([128, 2, NT, 128], BF16)
        AR.seek(baseB)
        wq = [AR.alloc([128, 8, 128], BF16) for _ in range(2)]
        qfL = [AR.alloc([128, 512], F32) for _ in range(2)]
        sqbL = [AR.alloc([128, 512], BF16) for _ in range(2)]
        rsL = [AR.alloc([128, 512], F32) for _ in range(2)]
        qnL = [AR.alloc([128, 512], F32) for _ in range(2)]
        qnbL = [AR.alloc([128, 512], BF16) for _ in range(2)]
        t1L = [AR.alloc([128, 512], F32) for _ in range(2)]
        t2L = [AR.alloc([128, 512], F32) for _ in range(2)]
        pTs = [AR.alloc([128, 512], BF16) for _ in range(6)]
        dn = AR.alloc([128, 512], F32)
        posi = AR.alloc([128, T], I32)
        ang = AR.alloc([128, T], F32)
        ki = AR.alloc([128, T], I32)
        kf = AR.alloc([128, T], F32)
        m1 = AR.alloc([128, T], F32)
        S.dma('sp', posi, pos_d[s].partition_broadcast(128), writes=['posi'])

        def table(dst, shift):
            S.op('dve', lambda: V.tensor_copy(out=ang, in_=posi), reads=['posi'], writes=['ang'])
            S.op('dve', lambda: V.tensor_scalar(out=ang, in0=ang, scalar1=col("invf"), scalar2=shift, op0=ALU.mult, op1=ALU.add), reads=['ang', 'pp'], writes=['ang'])
            S.op('dve', lambda: V.tensor_scalar(out=ki, in0=ang, scalar1=1.0 / TWO_PI, scalar2=None, op0=ALU.mult), reads=['ang'], writes=['ki'])
            S.op('pool', lambda: POOL.tensor_copy(out=kf, in_=ki), reads=['ki'], writes=['kf'])
            S.op('dve', lambda: V.scalar_tensor_tensor(out=ang, in0=kf, scalar=-C1, in1=ang, op0=ALU.mult, op1=ALU.add), reads=['kf', 'ang'], writes=['ang'])
            S.op('dve', lambda: V.scalar_tensor_tensor(out=ang, in0=kf, scalar=-C2, in1=ang, op0=ALU.mult, op1=ALU.add), reads=['kf', 'ang'], writes=['ang'])
            S.op('dve', lambda: V.tensor_scalar(out=m1, in0=ang, scalar1=float(np.pi), scalar2=-TWO_PI, op0=ALU.is_gt, op1=ALU.mult), reads=['ang'], writes=['m1'])
            S.op('pool', lambda: POOL.tensor_tensor(out=ang, in0=ang, in1=m1, op=ALU.add), reads=['ang', 'm1'], writes=['ang'])
            S.op('dve', lambda: V.tensor_scalar(out=m1, in0=ang, scalar1=float(-np.pi), scalar2=TWO_PI, op0=ALU.is_lt, op1=ALU.mult), reads=['ang'], writes=['m1'])
            S.op('pool', lambda: POOL.tensor_tensor(out=ang, in0=ang, in1=m1, op=ALU.add), reads=['ang', 'm1'], writes=['ang'])
            S.op('act', lambda: ACT.activation(out=dst, in_=ang, func=AF.Sin), reads=['ang'], writes=['tab'])

        table(sinT, 0.0)
        table(cosT, float(np.pi / 2))
        def c0_of(c):
            return 1920 + c * 128 if c < 4 else 2432

        def gen_qk(c, tb, L):
            b = c % 2
            gcol = col("qg") if c < 4 else col("kg")
            sl = slice(tb * 512, (tb + 1) * 512)
            qf_, sqb_, rs_, qn_, qnb_, t1_, t2_ = qfL[L], sqbL[L], rsL[L], qnL[L], qnbL[L], t1L[L], t2L[L]
            pz, pk = ps[L], psk[L]
            for k in range(8):
                S.op('pe', lambda: PE.matmul(pz, lhsT=wq[b][:, k, :], rhs=uT[:, k, sl], start=(k == 0), stop=(k == 7)),
                     reads=[('wq', b), ('uT', tb)], writes=[pk], pe_acc=True)
            S.op('act', lambda: ACT.copy(out=qf_, in_=pz), reads=[pk], writes=[('qf', L)])
            S.op('act', lambda: ACT.activation(out=sqb_, in_=qf_, func=AF.Square), reads=[('qf', L)], writes=[('sqb', L)])
            yield
            pr, pkr = ps[2 + L], psk[2 + L]
            S.op('pe', lambda: PE.matmul(pr, lhsT=cmb[:, BLK1, :], rhs=sqb_, start=True, stop=True), reads=[('sqb', L), 'cmb'], writes=[pkr], pe_acc=True)
            S.op('act', lambda: ACT.activation(out=rs_, in_=pr, func=AF.Sqrt, bias=epsc[:, 0:1], scale=1.0 / 64), reads=[pkr, 'epsc'], writes=[('rs', L)])
            yield
            S.op('dve', lambda: V.reciprocal(out=rs_, in_=rs_), reads=[('rs', L)], writes=[('rs', L)])
            S.op('dve', lambda: V.scalar_tensor_tensor(out=qn_, in0=qf_, scalar=gcol, in1=rs_, op0=ALU.mult, op1=ALU.mult), reads=[('qf', L), ('rs', L), 'pp'], writes=[('qn', L)])
            S.op('act', lambda: ACT.copy(out=qnb_, in_=qn_), reads=[('qn', L)], writes=[('qnb', L)])
            yield
            pro, pkro = ps[4 + L], psk[4 + L]
            S.op('pe', lambda: PE.matmul(pro, lhsT=cmb[:, ROT, :], rhs=qnb_, start=True, stop=True), reads=[('qnb', L), 'cmb'], writes=[pkro], pe_acc=True)
            S.op('pool', lambda: POOL.tensor_tensor(out=t1_, in0=qn_, in1=cosT[:, sl], op=ALU.mult), reads=[('qn', L), 'tab'], writes=[('t1', L)])
            yield
            S.op('dve', lambda: V.tensor_tensor(out=t2_, in0=pro, in1=sinT[:, sl], op=ALU.mult), reads=[pkro, 'tab'], writes=[('t2', L)])
            dst = qT[:, c, sl] if c < 4 else kTt[:, sl]
            S.op('dve', lambda: V.tensor_tensor(out=dst, in0=t1_, in1=t2_, op=ALU.add), reads=[('t1', L), ('t2', L)], writes=['qk'])
            yield

        def run_tasks(tasks):
            tasks = list(tasks)
            while tasks:
                for t_ in list(tasks):
                    try:
                        next(t_)
                    except StopIteration:
                        tasks.remove(t_)

        S.dma('pool', wq[0], win_d[:, c0_of(0):c0_of(0) + 128].rearrange("(k p) n -> p k n", p=128), writes=[('wq', 0)])
        for c in range(5):
            if c + 1 < 5:
                S.dma('pool', wq[(c + 1) % 2], win_d[:, c0_of(c + 1):c0_of(c + 1) + 128].rearrange("(k p) n -> p k n", p=128), writes=[('wq', (c + 1) % 2)])
            for tb in range(0, NB, 2):
                run_tasks([gen_qk(c, tb, 0), gen_qk(c, tb + 1, 1)])
        S.op('pool', lambda: POOL.memset(vp.rearrange("p a b c -> p (a b c)"), 0.0), writes=['vp'])
        S.dma('pool', wq[0], win_d[:, 2560:2688].rearrange("(k p) n -> p k n", p=128), writes=[('wq', 0)])
        for i in range(NT):
            pz, pk = ps[i % 2], psk[i % 2]
            for k in range(8):
                S.op('pe', lambda: PE.matmul(pz[:, 0:128], lhsT=uT[:, k, i * 128:(i + 1) * 128], rhs=wq[0][:, k, :], start=(k == 0), stop=(k == 7)),
                     reads=[('wq', 0), ('uT', i // 4)], writes=[pk], pe_acc=True)
            S.op('act', lambda: ACT.copy(out=vp[:, 0, i, 0:64], in_=pz[:, 0:64]), reads=[pk], writes=['vp'])
            S.op('dve', lambda: V.tensor_copy(out=vp[:, 1, i, 64:128], in_=pz[:, 64:128]), reads=[pk], writes=['vp'])
        for n in range(NT):
            qs = slice(n * 128, (n + 1) * 128)
            kbs = [kb for kb in (n - 1, n, n + 1) if 0 <= kb < NT]
            items = [(g, kb) for g in range(2) for kb in kbs]
            for idx, (g, kb) in enumerate(items):
                gp = slice(g * 64, (g + 1) * 64)
                pz, pk = ps[idx % 4], psk[idx % 4]
                S.op('pe', lambda: PE.matmul(pz.rearrange("p (j q) -> p j q", j=4), lhsT=kTt[gp, kb * 128:(kb + 1) * 128], rhs=qT[gp, :, qs], start=True, stop=True),
                     reads=['qk'], writes=[pk], pe_acc=True)
                pt_ = pTs[idx]
                S.op('act', lambda: ACT.activation(out=pt_, in_=pz, func=AF.Exp, scale=0.125), reads=[pk], writes=[('pT', idx)])
                if kb != n:
                    mk = cmb[:, MPREV if kb < n else MNEXT, :]
                    S.op('pool', lambda: POOL.tensor_tensor(out=pt_.rearrange("p (j q) -> p j q", j=4), in0=pt_.rearrange("p (j q) -> p j q", j=4),
                                                            in1=bc(mk.rearrange("p (o q) -> p o q", o=1), [128, 4, 128]), op=ALU.mult),
                         reads=[('pT', idx), 'cmb'], writes=[('pT', idx)])
            po, pko = ps[4 + n % 2], psk[4 + n % 2]
            pd_, pkd = ps[6 + n % 2], psk[6 + n % 2]
            for idx, (g, kb) in enumerate(items):
                S.op('pe', lambda: PE.matmul(po, lhsT=vp[:, g, kb, :], rhs=pTs[idx], start=(idx == 0), stop=(idx == len(items) - 1)),
                     reads=['vp', ('pT', idx)], writes=[pko], pe_acc=True)
            for idx, (g, kb) in enumerate(items):
                S.op('pe', lambda: PE.matmul(pd_, lhsT=cmb[:, VP[g], :], rhs=pTs[idx], start=(idx == 0), stop=(idx == len(items) - 1)),
                     reads=['cmb', ('pT', idx)], writes=[pkd], pe_acc=True)
            S.op('dve', lambda: V.tensor_tensor(out=dn.rearrange("p (j q) -> p j q", j=4), in0=pd_.rearrange("p (j q) -> p j q", j=4),
                                                in1=bc(esk.rearrange("p (j o) -> p j o", o=1), [128, 4, 128]), op=ALU.add), reads=[pkd, 'esk'], writes=['dn'])
            S.op('dve', lambda: V.reciprocal(out=dn, in_=dn), reads=['dn'], writes=['dn'])
            S.op('dve', lambda: V.tensor_tensor(out=ybT[:, :, qs], in0=po.rearrange("p (j q) -> p j q", j=4), in1=dn.rearrange("p (j q) -> p j q", j=4), op=ALU.mult),
                 reads=[pko, 'dn'], writes=['ybT'])

    def phase_merge(uT, yaT, ybT, mergedT, offs):
        AR.seek(offs[0])
        prw = AR.alloc([128, 4, D], BF16)
        AR.seek(offs[1])
        pat = AR.alloc([128, 4, D], BF16)
        wga = [AR.alloc([128, 8, 128], BF16) for _ in range(2)]
        wgb = [AR.alloc([128, 8, 128], BF16) for _ in range(2)]
        sga = AR.alloc([128, 512], BF16)
        sgb = AR.alloc([128, 512], BF16)
        t1 = AR.alloc([128, 512], F32)
        t2 = AR.alloc([128, 512], F32)
        for hh in range(2):
            S.dma('pool', prw[:, hh * 2:(hh + 1) * 2, :], prw_d[hh * 256:(hh + 1) * 256, :].rearrange("(k p) n -> p k n", p=128), writes=['prw'])
            S.dma('pool', pat[:, hh * 2:(hh + 1) * 2, :], pat_d[hh * 256:(hh + 1) * 256, :].rearrange("(k p) n -> p k n", p=128), writes=['pat'])
        for oc in range(8):
            b = oc % 2
            S.dma('pool', wga[b], win_d[:, 2688 + oc * 128:2688 + (oc + 1) * 128].rearrange("(k p) n -> p k n", p=128), writes=[('wga', b)])
            S.dma('pool', wgb[b], win_d[:, 3712 + oc * 128:3712 + (oc + 1) * 128].rearrange("(k p) n -> p k n", p=128), writes=[('wgb', b)])
            for tb in range(NB):
                sl = slice(tb * 512, (tb + 1) * 512)
                for k in range(8):
                    S.op('pe', lambda: PE.matmul(ps[0], lhsT=wga[b][:, k, :], rhs=uT[:, k, sl], start=(k == 0), stop=(k == 7)),
                         reads=[('wga', b), ('uT', tb)], writes=[psk[0]], pe_acc=True)
                S.op('act', lambda: ACT.activation(out=sga, in_=ps[0], func=AF.Sigmoid), reads=[psk[0]], writes=['sga'])
                for k in range(8):
                    S.op('pe', lambda: PE.matmul(ps[1], lhsT=wgb[b][:, k, :], rhs=uT[:, k, sl], start=(k == 0), stop=(k == 7)),
                         reads=[('wgb', b), ('uT', tb)], writes=[psk[1]], pe_acc=True)
                S.op('act', lambda: ACT.activation(out=sgb, in_=ps[1], func=AF.Sigmoid), reads=[psk[1]], writes=['sgb'])
                for k in range(4):
                    S.op('pe', lambda: PE.matmul(ps[2], lhsT=prw[:, k, oc * 128:(oc + 1) * 128], rhs=yaT[:, k, sl], start=(k == 0), stop=(k == 3)),
                         reads=['prw', 'yaT'], writes=[psk[2]], pe_acc=True)
                for k in range(4):
                    S.op('pe', lambda: PE.matmul(ps[3], lhsT=pat[:, k, oc * 128:(oc + 1) * 128], rhs=ybT[:, k, sl], start=(k == 0), stop=(k == 3)),
                         reads=['pat', 'ybT'], writes=[psk[3]], pe_acc=True)
                S.op('dve', lambda: V.tensor_tensor(out=t1, in0=ps[2], in1=sga, op=ALU.mult), reads=[psk[2], 'sga'], writes=['t1'])
                S.op('dve', lambda: V.tensor_tensor(out=t2, in0=ps[3], in1=sgb, op=ALU.mult), reads=[psk[3], 'sgb'], writes=['t2'])
                S.op('pool', lambda: POOL.tensor_tensor(out=mergedT[:, oc, sl], in0=t1, in1=t2, op=ALU.add), reads=['t1', 't2'], writes=[('mg', tb)])

    def phase_x1(s, mergedT, u2tm, base):
        AR.seek(base)
        wo = AR.alloc([128, 8, D], BF16)
        gt1B = AR.alloc([128, D], F32)
        sc2 = AR.alloc([128, D], F32)
        sh2 = AR.alloc([128, D], F32)
        xt = [AR.alloc([128, D], F32) for _ in range(2)]
        x1t = [AR.alloc([128, D], F32) for _ in range(2)]
        tmpP = [AR.alloc([128, D], F32) for _ in range(2)]
        junkP = [AR.alloc([128, D], BF16) for _ in range(2)]
        u2TP = [AR.alloc([128, 8, 128], BF16) for _ in range(2)]
        ssP = [AR.alloc([128, 8], F32) for _ in range(2)]
        exP = [AR.alloc([128, E], F32) for _ in range(2)]
        for hh in range(4):
            S.dma('pool', wo[:, hh * 2:(hh + 1) * 2, :], wout_d[hh * 256:(hh + 1) * 256, :].rearrange("(k p) n -> p k n", p=128), writes=['wo'])
        S.dma('sp', gt1B, mod_d[s, 2], writes=['gt1B'])
        S.dma('sp', sc2, mod_d[s, 4], writes=['sc2'])
        S.dma('sp', sh2, mod_d[s, 3], writes=['sh2'])
        for i in range(NT):
            b = i % 2
            sl = slice(i * 128, (i + 1) * 128)
            S.dma('sp', xt[b], x_d[s, sl, :], writes=[('xt', b)])
            tmp, junk, u2T, ss, ex = tmpP[b], junkP[b], u2TP[b], ssP[b], exP[b]
            for cb in range(2):
                for k in range(8):
                    S.op('pe', lambda: PE.matmul(ps[cb + 6 * b], lhsT=mergedT[:, k, sl], rhs=wo[:, k, cb * 512:(cb + 1) * 512], start=(k == 0), stop=(k == 7)),
                         reads=[('mg', i // 4), 'wo'], writes=[psk[cb + 6 * b]], pe_acc=True)
                S.op('dve', lambda: V.tensor_tensor(out=tmp[:, cb * 512:(cb + 1) * 512], in0=ps[cb + 6 * b], in1=gt1B[:, cb * 512:(cb + 1) * 512], op=ALU.mult),
                     reads=[psk[cb + 6 * b], 'gt1B'], writes=[('tmp', b, cb)])
            S.op('pool', lambda: POOL.tensor_tensor(out=x1t[b], in0=tmp, in1=xt[b], op=ALU.add), reads=[('tmp', b, 0), ('tmp', b, 1), ('xt', b)], writes=[('x1t', b)])
            S.dma('sp', out_d[s, sl, :], x1t[b], reads=[('x1t', b)], writes=[('outd', i)])
            S.op('act', lambda: ACT.activation(out=junk, in_=x1t[b], func=AF.Square, accum_out=ss[:, 0:1]), reads=[('x1t', b)], writes=[('junk', b), ('ss0', b)])
            S.op('act', lambda: ACT.activation(out=ss[:, 1:2], in_=ss[:, 0:1], func=AF.Sqrt, bias=epsc[:, 0:1], scale=1.0 / D), reads=[('ss0', b), 'epsc'], writes=[('ss1', b)])
            S.op('dve', lambda: V.reciprocal(out=ss[:, 1:2], in_=ss[:, 1:2]), reads=[('ss1', b)], writes=[('ss1', b)])
            S.op('dve', lambda: V.scalar_tensor_tensor(out=tmp, in0=x1t[b], scalar=ss[:, 1:2], in1=sc2, op0=ALU.mult, op1=ALU.mult),
                 reads=[('x1t', b), ('ss1', b), 'sc2'], writes=[('tmp', b, 0), ('tmp', b, 1)])
            S.op('pool', lambda: POOL.tensor_tensor(out=u2tm[:, i, :], in0=tmp, in1=sh2, op=ALU.add), reads=[('tmp', b, 0), ('tmp', b, 1), 'sh2'], writes=[('u2', i)])
            pz = ps[2 + i % 2].bitcast(BF16).rearrange("p (k t) -> p k t", k=8)
            pk = psk[2 + i % 2]
            for k in range(8):
                S.op('pe', lambda: PE.transpose(out=pz[:, k, :], in_=u2tm[:, i, k * 128:(k + 1) * 128], identity=ident),
                     reads=[('u2', i), 'ident'], writes=[pk], pe_acc=True)
            S.op('act', lambda: ACT.copy(out=u2T, in_=pz), reads=[pk], writes=[('u2T', b)])
            pl, pkl = ps[4 + i % 2], psk[4 + i % 2]
            for k in range(8):
                S.op('pe', lambda: PE.matmul(pl[:, 0:E], lhsT=u2T[:, k, :], rhs=wr[:, k, :], start=(k == 0), stop=(k == 7)),
                     reads=[('u2T', b), 'wr'], writes=[pkl], pe_acc=True)
            S.op('dve', lambda: V.tensor_reduce(out=ss[:, 2:3], in_=pl[:, 0:E], axis=AX.X, op=ALU.max), reads=[pkl], writes=[('ss2', b)])
            S.op('dve', lambda: V.tensor_scalar(out=ss[:, 3:4], in0=ss[:, 2:3], scalar1=-1.0, scalar2=None, op0=ALU.mult), reads=[('ss2', b)], writes=[('ss3', b)])
            S.op('act', lambda: ACT.activation(out=ex, in_=pl[:, 0:E], func=AF.Exp, bias=ss[:, 3:4], scale=1.0, accum_out=ss[:, 4:5]), reads=[pkl, ('ss3', b)], writes=[('ex', b), ('ss4', b)])
            S.op('dve', lambda: V.reciprocal(out=ss[:, 5:6], in_=ss[:, 4:5]), reads=[('ss4', b)], writes=[('ss5', b)])
            S.op('dve', lambda: V.tensor_scalar(out=afftm[:, i, :], in0=ex, scalar1=ss[:, 5:6], scalar2=None, op0=ALU.mult), reads=[('ex', b), ('ss5', b)], writes=['afftm'])

    def phase_moe(s, u2tm, base):
        AR.seek(base)
        affT = AR.alloc([16, T], F32)
        work = AR.alloc([16, T], F32)
        maskT = AR.alloc([16, T], F32)
        slotT = AR.alloc([16, T], F32)
        mx8 = AR.alloc([16, 8], F32)
        for i in range(NT):
            pz = ps[i // 4]
            S.op('pe', lambda: PE.transpose(out=pz[0:16, (i % 4) * 128:(i % 4 + 1) * 128], in_=afftm[:, i, :], identity=identf),
                 reads=['afftm', 'identf'], writes=[psk[i // 4]], pe_acc=True)
        for q in range(4):
            S.op('act', lambda: ACT.copy(out=affT[:, q * 512:(q + 1) * 512], in_=ps[q][0:16, :]), reads=[psk[q]], writes=['affT'])
        S.op('dve', lambda: V.tensor_copy(out=work, in_=affT), reads=['affT'], writes=['work'])
        for it in range(CAP // 8):
            S.op('dve', lambda: V.max(out=mx8, in_=work), reads=['work'], writes=['mx8'])
            if it < CAP // 8 - 1:
                S.op('dve', lambda: V.match_replace(out=work, in_to_replace=mx8, in_values=work, imm_value=-1.0), reads=['work', 'mx8'], writes=['work'])
        S.op('dve', lambda: V.tensor_scalar(out=maskT, in0=affT, scalar1=mx8[:, 7:8], scalar2=None, op0=ALU.is_ge), reads=['affT', 'mx8'], writes=['maskT'])
        S.op('pool', lambda: POOL.memset(work, 1.0), reads=['work'], writes=['work'])
        S.op('dve', lambda: V.tensor_tensor_scan(out=slotT, data0=work, data1=maskT, initial=0.0, op0=ALU.mult, op1=ALU.add), reads=['work', 'maskT'], writes=['slotT'])
        S.op('dve', lambda: V.tensor_tensor(out=slotT, in0=slotT, in1=maskT, op=ALU.mult), reads=['slotT', 'maskT'], writes=['slotT'])
        S.op('dve', lambda: V.tensor_scalar(out=slotT, in0=slotT, scalar1=-1.0, scalar2=None, op0=ALU.add), reads=['slotT'], writes=['slotT'])
        pz = ps[4]
        for i in range(NT):
            S.op('pe', lambda: PE.transpose(out=pz[:, i * 16:(i + 1) * 16], in_=slotT[:, i * 128:(i + 1) * 128], identity=identf[0:16, 0:16]),
                 reads=['slotT', 'identf'], writes=[psk[4]], pe_acc=True)
        S.op('act', lambda: ACT.copy(out=slot_tm.rearrange("p i e -> p (i e)"), in_=pz[:, 0:256]), reads=[psk[4]], writes=['slot_tm'])
        S.op('dve', lambda: V.tensor_copy(out=affhl[:, :, :, 0], in_=afftm), reads=['afftm'], writes=['affhl'])
        S.op('dve', lambda: V.tensor_tensor(out=affhl[:, :, :, 1], in0=afftm, in1=affhl[:, :, :, 0], op=ALU.subtract), reads=['afftm', 'affhl'], writes=['affhl'])
        S.barrier()
        AR.seek(base)
        ye = AR.alloc([128, E, 2, D], BF16)
        Wg = AR.alloc([128, 8, D], BF16)
        Wu = AR.alloc([128, 8, D], BF16)
        Wd = AR.alloc([128, 8, D], BF16)
        wbase = AR.ptr
        Pe = AR.alloc([128, NT, CAP], BF16)
        xeT = AR.alloc([128, 8, CAP], BF16)
        hT = AR.alloc([128, 8, CAP], BF16)
        hs = AR.alloc([128, CAP], F32)
        affs = AR.alloc([128, 4], F32)
        gt2B = AR.alloc([128, D], F32)
        S.dma('sp', gt2B, mod_d[s, 5], writes=['gt2B'])
        for e in range(E):
            for (wt, wsrc, nm) in ((Wg, wg_d, 'Wg'), (Wu, wu_d, 'Wu'), (Wd, wd_d, 'Wd')):
                for hh in range(4):
                    S.dma('pool', wt[:, hh * 2:(hh + 1) * 2, :], wsrc[e, hh * 256:(hh + 1) * 256, :].rearrange("(k p) n -> p k n", p=128), writes=[(nm, hh)])
            for i in range(NT):
                S.op('dve', lambda: V.tensor_scalar(out=Pe[:, i, :], in0=iota_row, scalar1=slot_tm[:, i, e:e + 1], scalar2=None, op0=ALU.is_equal),
                     reads=['iota_row', 'slot_tm'], writes=[('Pe', i)])
            for fc in range(8):
                pz, pk = ps[fc // 2], psk[fc // 2]
                pzs = pz[:, (fc % 2) * 256:(fc % 2 + 1) * 256]
                for i in range(NT):
                    S.op('pe', lambda: PE.matmul(pzs, lhsT=u2tm[:, i, fc * 128:(fc + 1) * 128], rhs=Pe[:, i, :], start=(i == 0), stop=(i == NT - 1)),
                         reads=[('u2', i), ('Pe', i)], writes=[pk], pe_acc=True)
                S.op('act', lambda: ACT.copy(out=xeT[:, fc, :], in_=pzs), reads=[pk], writes=[('xeT', fc)])
            pa, pka = ps[4], psk[4]
            for half in range(2):
                for i in range(NT):
                    S.op('pe', lambda: PE.matmul(pa[:, half * 2:(half + 1) * 2], lhsT=Pe[:, i, half * 128:(half + 1) * 128], rhs=affhl[:, i, e, :], start=(i == 0), stop=(i == NT - 1)),
                         reads=[('Pe', i), 'affhl'], writes=[pka], pe_acc=True)
            S.op('dve', lambda: V.tensor_reduce(out=affs[:, 0:2], in_=pa[:, 0:4].rearrange("p (h t) -> p h t", t=2), axis=AX.X, op=ALU.add), reads=[pka], writes=['affs'])
            for fk in range(8):
                pg, pkg = ps[5], psk[5]
                pu, pku = ps[6], psk[6]
                for k in range(8):
                    S.op('pe', lambda: PE.matmul(pg[:, 0:CAP], lhsT=Wg[:, k, fk * 128:(fk + 1) * 128], rhs=xeT[:, k, :], start=(k == 0), stop=(k == 7)),
                         reads=[('Wg', k // 2), ('xeT', k)], writes=[pkg], pe_acc=True)
                for k in range(8):
                    S.op('pe', lambda: PE.matmul(pu[:, 0:CAP], lhsT=Wu[:, k, fk * 128:(fk + 1) * 128], rhs=xeT[:, k, :], start=(k == 0), stop=(k == 7)),
                         reads=[('Wu', k // 2), ('xeT', k)], writes=[pku], pe_acc=True)
                S.op('act', lambda: ACT.activation(out=hs, in_=pg[:, 0:CAP], func=AF.Silu), reads=[pkg], writes=['hs'])
                S.op('dve', lambda: V.tensor_tensor(out=hT[:, fk, :], in0=pu[:, 0:CAP], in1=hs, op=ALU.mult), reads=[pku, 'hs'], writes=[('hT', fk)])
            for half in range(2):
                for cb in range(2):
                    py, pky = ps[7] if (half * 2 + cb) % 2 else ps[4], psk[7] if (half * 2 + cb) % 2 else psk[4]
                    for fk in range(8):
                        S.op('pe', lambda: PE.matmul(py, lhsT=hT[:, fk, half * 128:(half + 1) * 128], rhs=Wd[:, fk, cb * 512:(cb + 1) * 512], start=(fk == 0), stop=(fk == 7)),
                             reads=[('hT', fk), ('Wd', fk // 2), 'affs'], writes=[pky], pe_acc=True)
                    S.op('dve', lambda: V.tensor_scalar(out=ye[:, e, half, cb * 512:(cb + 1) * 512], in0=py, scalar1=affs[:, half:half + 1], scalar2=None, op0=ALU.mult),
                         reads=[pky, 'affs'], writes=['ye'])
        S.barrier()
        AR.seek(wbase - 3 * 8 * D * 2)
        Pall = AR.alloc([128, E, CAP], BF16)
        PT = AR.alloc([128, 2 * E, 128], BF16)
        x1t = [AR.alloc([128, D], F32) for _ in range(2)]
        ot = [AR.alloc([128, D], F32) for _ in range(2)]
        for i in range(NT):
            b = i % 2
            sl = slice(i * 128, (i + 1) * 128)
            S.dma('sp', x1t[b], out_d[s, sl, :], reads=[('outd', i)], writes=[('x1t', b)])
            for e in range(E):
                S.op('dve', lambda: V.tensor_scalar(out=Pall[:, e, :], in0=iota_row, scalar1=slot_tm[:, i, e:e + 1], scalar2=None, op0=ALU.is_equal),
                     reads=['iota_row', 'slot_tm'], writes=[('Pall', e // 4)])
            for q in range(4):
                pz = ps[q].bitcast(BF16).rearrange("p (j t) -> p j t", j=8)
                for j in range(8):
                    idx = q * 8 + j
                    e, half = idx // 2, idx % 2
                    S.op('pe', lambda: PE.transpose(out=pz[:, j, :], in_=Pall[:, e, half * 128:(half + 1) * 128], identity=ident),
                         reads=[('Pall', e // 4), 'ident'], writes=[psk[q]], pe_acc=True)
                if q % 2 == 0:
                    S.op('act', lambda: ACT.copy(out=PT[:, q * 8:(q + 1) * 8, :], in_=pz), reads=[psk[q]], writes=[('PT', q)])
                else:
                    S.op('dve', lambda: V.tensor_copy(out=PT[:, q * 8:(q + 1) * 8, :], in_=pz), reads=[psk[q]], writes=[('PT', q)])
            for cb in range(2):
                po, pko = ps[4 + cb + 2 * (i % 2)], psk[4 + cb + 2 * (i % 2)]
                for idx in range(2 * E):
                    e, half = idx // 2, idx % 2
                    S.op('pe', lambda: PE.matmul(po, lhsT=PT[:, idx, :], rhs=ye[:, e, half, cb * 512:(cb + 1) * 512], start=(idx == 0), stop=(idx == 2 * E - 1)),
                         reads=[('PT', idx // 8), 'ye'], writes=[pko], pe_acc=True)
                S.op('dve', lambda: V.tensor_tensor(out=ot[b][:, cb * 512:(cb + 1) * 512], in0=po, in1=gt2B[:, cb * 512:(cb + 1) * 512], op=ALU.mult),
                     reads=[pko, 'gt2B'], writes=[('ot', b, cb)])
            S.op('dve', lambda: V.tensor_tensor(out=ot[b], in0=ot[b], in1=x1t[b], op=ALU.add), reads=[('ot', b, 0), ('ot', b, 1), ('x1t', b)], writes=[('ot', b, 0), ('ot', b, 1)])
            S.dma('sp', out_d[s, sl, :], ot[b], reads=[('ot', b, 0), ('ot', b, 1)], writes=[('outd', i)])

    def dbg_dump(src_ap, shape, key_reads=()):
        AR.seek(AR_TOP)
        t = AR.alloc(shape, F32)
        S.op('dve', lambda: V.tensor_copy(out=t, in_=src_ap), writes=['dbgt'])
        flat = t if len(shape) == 2 else t.rearrange("p a b -> p (a b)")
        S.dma('sp', dbg_d, flat, reads=['dbgt'])

    AR_TOP = 160 * 1024
    phase_adaln()
    for s in range(nseq):
        AR.seek(0)
        zsT = AR.alloc([128, 15, T], BF16)
        uT = AR.alloc([128, 8, T], BF16)
        base1 = AR.ptr
        phase_norm1(s, uT, base1)
        S.barrier()
        if dbg and dbg[0] == 'uT':
            dbg_dump(uT[:, :, 0:512], [128, 8, 512]); break
        for q in range(4):
            S.dma('sp', u_d[:, 2 * q:2 * q + 2, :], uT[:, 2 * q:2 * q + 2, :], reads=[('uT', 0), ('uT', 1), ('uT', 2), ('uT', 3)], writes=['uscr'])
        phase_rwkv_cols(uT, zsT, base1)
        S.barrier()
        if dbg and dbg[0] == 'zs':
            dbg_dump(zsT[:, :, 0:256], [128, 15, 256]); break
        AR.seek(61440)
        kkT = AR.alloc([128, 4, T], BF16)
        yaT = AR.alloc([128, 4, T], BF16)
        base3 = AR.ptr
        phase_scan(zsT, kkT, base3)
        if dbg and dbg[0] == 'yscan':
            AR.seek(AR_TOP)
            t = AR.alloc([128, 2, 512], F32)
            S.dma('sp', t[:, 0, :], y_d[0, 0:128, :], writes=['dbgt'])
            S.dma('sp', t[:, 1, :], y_d[1, 0:128, :], writes=['dbgt'])
            S.dma('sp', dbg_d, t.rearrange("p a b -> p (a b)"), reads=['dbgt']); break
        phase_post(zsT, yaT, base3)
        S.barrier()
        if dbg and dbg[0] == 'yaT':
            dbg_dump(yaT[:, :, 0:512], [128, 4, 512]); break
        AR.seek(0)
        uT = AR.alloc([128, 8, T], BF16)
        AR.seek(94208)
        ybT = AR.alloc([128, 4, T], BF16)
        baseB = AR.ptr
        for q in range(4):
            S.dma('sp' if q % 2 == 0 else 'act', uT[:, 2 * q:2 * q + 2, :], u_d[:, 2 * q:2 * q + 2, :], writes=[('uT', 0), ('uT', 1), ('uT', 2), ('uT', 3)])
        phase_attn(s, uT, ybT, 32768, baseB)
        S.barrier()
        if dbg and dbg[0] == 'ybT':
            dbg_dump(ybT[:, :, 0:512], [128, 4, 512]); break
        AR.seek(32768)
        mergedT = AR.alloc([128, 8, T], BF16)
        phase_merge(uT, yaT, ybT, mergedT, (65536, baseB))
        S.barrier()
        if dbg and dbg[0] == 'merged':
            dbg_dump(mergedT[:, :, 0:512], [128, 8, 512]); break
        AR.seek(0)
        u2tm = AR.alloc([128, NT, D], BF16)
        phase_x1(s, mergedT, u2tm, 65536)
        S.barrier()
        if dbg and dbg[0] == 'aff':
            dbg_dump(afftm.rearrange("p i e -> p (i e)"), [128, 256]); break
        phase_moe(s, u2tm, 32768)
        S.barrier()

    S.finish('sp')
    print("ninstr", S.ninstr, "pe_incs", S.npe_inc, "arena hi", AR.hi)
    return nc


def _consts():
    cm = np.zeros((13, 128, 128), np.float32)
    p = np.arange(128)
    cm[0] = (p[:, None] // 64 == p[None, :] // 64).astype(np.float32)
    R = np.zeros((128, 128), np.float32)
    for blk in range(2):
        o = blk * 64
        for d_ in range(8):
            R[o + d_ + 8, o + d_] = -1.0
            R[o + d_, o + d_ + 8] = 1.0
    cm[1] = R
    cm[2] = (p[:, None] >= p[None, :]).astype(np.float32)
    cm[3] = (p[:, None] <= p[None, :]).astype(np.float32)
    s_ = (p % 64)[:, None]
    t_ = (p % 64)[None, :]
    a_col = (p[None, :] >= 64)
    fwd = np.where(a_col, s_ < t_, s_ <= t_)
    bwd = np.where(a_col, s_ > t_, s_ >= t_)
    cm[4] = fwd.astype(np.float32)
    cm[5] = bwd.astype(np.float32)
    cm[6] = cm[4].T
    cm[7] = cm[5].T
    cm[8][:, 0] = (p < 64)
    cm[8][:, 1] = (p >= 64)
    cm[9][:, 0:64] = 1.0
    cm[10][:, 64:128] = 1.0
    cm[11] = ((p % 64)[:, None] < (p % 64)[None, :]).astype(np.float32)
    cm[12] = ((p % 64)[:, None] > (p % 64)[None, :]).astype(np.float32)
    return np.ascontiguousarray(cm.transpose(1, 0, 2).reshape(128, 13 * 128))


def _prep_shared(inp):
    f = lambda a: np.ascontiguousarray(np.asarray(a, dtype=np.float32))
    L = 0
    w_in = f(inp["w_in"][L]).copy()
    qoff = 1920
    perm = []
    for c in range(4):
        perm += list(range(c * 64, (c + 1) * 64)) + list(range((4 + c) * 64, (5 + c) * 64))
    perm = np.array(perm)
    w_in[:, qoff:qoff + 512] = w_in[:, qoff:qoff + 512][:, perm]
    p_attn = f(inp["p_attn"][L])[perm, :]
    pp = np.zeros((128, NPP), np.float32)

    def put(name, arr):
        o, w = PP[name]
        pp[:, o:o + w] = arr

    chunked = lambda v: np.asarray(v, np.float32).reshape(-1, 128).T
    put("mp", chunked(inp["mu_prev"][L]))
    put("mn", chunked(inp["mu_next"][L]))
    put("w0", np.concatenate([chunked(inp["rwkv_w0"][L][0]), chunked(inp["rwkv_w0"][L][1])], 1))
    put("a0", np.concatenate([chunked(inp["rwkv_a0"][L][0]), chunked(inp["rwkv_a0"][L][1])], 1))
    put("kk", chunked(inp["rwkv_k_k"][L]))
    put("ka", chunked(inp["rwkv_k_a"][L]))
    put("rk", chunked(np.asarray(inp["rwkv_r_k"][L]).reshape(-1)))
    put("qg", np.tile(np.asarray(inp["q_norm_g"][L], np.float32), 2)[:, None])
    put("kg", np.tile(np.asarray(inp["k_norm_g"][L], np.float32), 2)[:, None])
    inv_freq = (500000.0 ** (-np.arange(0, 16, 2, dtype=np.float32) / 16)).astype(np.float32)
    invf = np.zeros(64, np.float32)
    invf[0:8] = inv_freq
    invf[8:16] = inv_freq
    put("invf", np.tile(invf, 2)[:, None])
    sink = np.asarray(inp["attn_sink"][L], np.float32)
    sk = np.zeros((128, 4), np.float32)
    for j in range(4):
        sk[0:64, j] = sink[j]
        sk[64:128, j] = sink[4 + j]
    put("sink", sk)
    w2cat = np.zeros((128, 2, 512), np.float32)
    a2cat = np.zeros((128, 2, 512), np.float32)
    for d_ in range(2):
        w2cat[d_ * 64:(d_ + 1) * 64, d_, :] = inp["rwkv_w2"][L][d_]
        a2cat[d_ * 64:(d_ + 1) * 64, d_, :] = inp["rwkv_a2"][L][d_]
    return {
        "w_ada": f(inp["w_ada"][L]), "b_ada": f(inp["b_ada"][L])[None, :] if np.asarray(inp["b_ada"][L]).ndim == 1 else f(inp["b_ada"][L]),
        "norm1_g": f(inp["norm1_g"][L]).reshape(1, D), "norm2_g": f(inp["norm2_g"][L]).reshape(1, D),
        "w_in": w_in, "pp": pp, "w2cat": w2cat.reshape(128, 1024), "a2cat": a2cat.reshape(128, 1024),
        "g2": f(inp["rwkv_g2"][L]), "gn_w": f(inp["rwkv_gn_w"][L]).reshape(1, 512), "gn_b": f(inp["rwkv_gn_b"][L]).reshape(1, 512),
        "p_rwkv": f(inp["p_rwkv"][L]), "p_attn": np.ascontiguousarray(p_attn), "w_out": f(inp["w_out"][L]),
        "w_router": f(inp["w_router"][L]), "w_gate": f(inp["w_gate"][L]), "w_up": f(inp["w_up"][L]), "w_down": f(inp["w_down"][L]),
        "cmats": _consts(),
    }


def _core_inputs(inp, shared, seqs):
    x = np.ascontiguousarray(np.asarray(inp["x"], np.float32)[seqs])
    c = np.asarray(inp["c"], np.float32)[seqs]
    cT = np.ascontiguousarray(c.reshape(len(seqs), 8, 128).transpose(0, 2, 1))
    pos = np.ascontiguousarray(np.asarray(inp["positions"]).astype(np.int32)[seqs][:, None, :])
    m = dict(shared)
    m.update({"x": x, "cT": cT, "pos": pos})
    return m


def kernel(**inputs):
    shared = _prep_shared(inputs)
    nc = build(NSEQ)
    in_maps = [_core_inputs(inputs, shared, list(range(i * NSEQ, (i + 1) * NSEQ))) for i in range(NCORES)]
    res = run_bass_kernel_spmd(nc, in_maps, core_ids=list(range(NCORES)))
    out = np.concatenate([np.asarray(r["out"]) for r in res.results], axis=0)
    return out.astype(np.float32)
```

```python
import numpy as np
import concourse.bass as bass
import concourse.mybir as mybir
from concourse.bass_utils import run_bass_kernel_spmd

F32 = mybir.dt.float32
BF16 = mybir.dt.bfloat16
I32 = mybir.dt.int32
ALU = mybir.AluOpType
AF = mybir.ActivationFunctionType
AX = mybir.AxisListType

T = 2048
D = 1024
NT = 16
NB = 4
NSEQ = 2
NCORES = 8
E = 16
CAP = 256
LAM = float(np.exp(-0.5))
NCH = 4
TBS = NCH * 64
NTB = T // TBS
TWO_PI = float(2 * np.pi)
C1 = 6.28125
C2 = TWO_PI - C1

PP = {}
_o = 0
for _n, _w in [("mp", 15), ("mn", 15), ("w0", 8), ("a0", 8), ("kk", 4), ("ka", 4), ("rk", 4), ("qg", 1), ("kg", 1),
               ("invf", 1), ("sink", 4)]:
    PP[_n] = (_o, _w)
    _o += _w
NPP = _o


class Ticket:
    __slots__ = ('ins', 'sem', 'val', 'parent')

    def __init__(self, ins):
        self.ins = ins
        self.sem = None
        self.val = None
        self.parent = None

    def root(self):
        t = self
        while t.parent is not None:
            t = t.parent
        return t


class Sync:
    SEM_MAX = 30000

    def __init__(self, nc):
        self.nc = nc
        self.E = {'pe': nc.tensor, 'act': nc.scalar, 'dve': nc.vector, 'pool': nc.gpsimd, 'sp': nc.sync}
        self.sem = {}
        self.cnt = {}
        self.nsem = 0
        for e in self.E:
            self._newsem(e)
        self.waited = {}
        self.lastw = {}
        self.reads = {}
        self.dma_sems = {}
        self.dma_rr = {}
        self.ninstr = 0
        self.pend = None
        self.pend_writes = None
        self.npe_inc = 0

    def _newsem(self, e):
        self.sem[e] = self.nc.alloc_semaphore(f"s_{e}_{self.nsem}")
        self.nsem += 1
        self.cnt[e] = 0

    def _flush_pe(self):
        t = self.pend
        if t is None:
            return
        if self.cnt['pe'] >= self.SEM_MAX:
            self._newsem('pe')
        self.cnt['pe'] += 1
        t.sem = self.sem['pe']
        t.val = self.cnt['pe']
        t.ins.then_inc(t.sem, 1)
        self.npe_inc += 1
        self.pend = None
        self.pend_writes = None

    def _wait(self, e, ev):
        if ev is None:
            return
        if isinstance(ev, Ticket):
            if e == 'pe':
                return
            t = ev.root()
            if t.val is None:
                assert t is self.pend
                self._flush_pe()
            sem, val = t.sem, t.val
        else:
            src, sem, val = ev
        k = (e, sem.name)
        if self.waited.get(k, 0) >= val:
            return
        self.waited[k] = val
        self.E[e].wait_ge(sem, val)

    def deps(self, e, reads, writes, pe_acc=False):
        for k in reads:
            self._wait(e, self.lastw.get(k))
        for k in writes:
            lw = self.lastw.get(k)
            if not (pe_acc and isinstance(lw, Ticket)):
                self._wait(e, lw)
            for ev in self.reads.get(k, {}).values():
                self._wait(e, ev)

    def commit(self, src, ev, reads, writes):
        for k in reads:
            self.reads.setdefault(k, {})[src] = ev
        for k in writes:
            self.lastw[k] = ev
            self.reads[k] = {}

    def op(self, e, fn, reads=(), writes=(), pe_acc=False):
        self.deps(e, reads, writes, pe_acc)
        if e == 'pe':
            ins = fn()
            t = Ticket(ins)
            if self.pend is not None:
                if self.pend_writes == tuple(writes):
                    self.pend.parent = t
                    self.pend = None
                else:
                    self._flush_pe()
            self.pend = t
            self.pend_writes = tuple(writes)
            self.commit('pe', t, reads, writes)
            self.ninstr += 1
            return t
        if self.cnt[e] >= self.SEM_MAX:
            self._newsem(e)
        ins = fn()
        self.cnt[e] += 1
        ev = (e, self.sem[e], self.cnt[e])
        ins.then_inc(self.sem[e], 1)
        self.commit(e, ev, reads, writes)
        self.ninstr += 1
        return ev

    def dma(self, e, out, in_, reads=(), writes=(), nslots=8, **kw):
        if e == 'pool':
            nslots = 2
        lst = self.dma_sems.setdefault(e, [])
        if len(lst) < nslots:
            lst.append([self.nc.alloc_semaphore(f"d_{e}_{len(lst)}"), 0])
        i = self.dma_rr.get(e, 0)
        self.dma_rr[e] = (i + 1) % nslots
        slot = lst[i % len(lst)]
        sem, uses = slot
        if uses > 0:
            self._wait(e, ('dma', sem, 16 * uses))
        self.deps(e, reads, writes)
        self.E[e].dma_start(out=out, in_=in_, **kw).then_inc(sem, 16)
        slot[1] = uses + 1
        ev = ('dma_%s_%d' % (e, i % len(lst)), sem, 16 * (uses + 1))
        self.commit(ev[0], ev, reads, writes)
        self.ninstr += 1
        return ev

    def barrier(self):
        self._flush_pe()
        evs = [(e, self.sem[e], self.cnt[e]) for e in self.E if self.cnt[e] > 0]
        for q, lst in self.dma_sems.items():
            for sem, uses in lst:
                if uses:
                    evs.append(('dma', sem, 16 * uses))
        for e in self.E:
            for ev in evs:
                if ev[0] != e:
                    self._wait(e, ev)
        self.lastw = {}
        self.reads = {}

    def finish(self, e='sp'):
        self._flush_pe()
        for q, lst in self.dma_sems.items():
            for sem, uses in lst:
                if uses:
                    self._wait(e, ('dma', sem, 16 * uses))


class Arena:
    def __init__(self, nc, name, nbytes):
        self.n4 = nbytes // 4
        self.t = nc.alloc_sbuf_tensor(name, [128, self.n4], F32).ap()
        self.ptr = 0
        self.hi = 0

    def seek(self, off):
        self.ptr = off

    def alloc(self, shape, dtype, parts=None):
        esz = 4 if dtype in (F32, I32) else 2
        n = int(np.prod(shape[1:]))
        nb = (n * esz + 31) // 32 * 32
        assert self.ptr % 4 == 0
        a = self.ptr // 4
        assert a + nb // 4 <= self.n4, f"arena overflow {self.ptr}+{nb} > {self.n4 * 4}"
        v = self.t[:, a:a + nb // 4]
        if dtype != F32:
            v = v.bitcast(dtype)
        v = v[0:shape[0], 0:n]
        if len(shape) > 2:
            names = " ".join(f"d{i}" for i in range(len(shape) - 1))
            kw = {f"d{i}": int(shape[i + 1]) for i in range(len(shape) - 1)}
            v = v.rearrange(f"p ({names}) -> p {names}", **kw)
        self.ptr += nb
        self.hi = max(self.hi, self.ptr)
        return v


def bc(ap, shape):
    return ap.to_broadcast(list(shape))


def build(nseq=NSEQ, dbg=None, stop_after=None):
    nc = bass.Bass("TRN2", target_bir_lowering=False)
    S = Sync(nc)
    V, ACT, POOL, PE = nc.vector, nc.scalar, nc.gpsimd, nc.tensor

    def din(name, shape, dt=F32):
        return nc.dram_tensor(name, list(shape), dt, kind="ExternalInput").ap()

    x_d = din("x", [nseq, T, D])
    cT_d = din("cT", [nseq, 128, 8])
    pos_d = din("pos", [nseq, 1, T], I32)
    wada_d = din("w_ada", [D, 6 * D])
    bada_d = din("b_ada", [1, 6 * D])
    n1g_d = din("norm1_g", [1, D])
    n2g_d = din("norm2_g", [1, D])
    win_d = din("w_in", [D, 4736])
    pp_d = din("pp", [128, NPP])
    w2c_d = din("w2cat", [128, 2 * 512])
    a2c_d = din("a2cat", [128, 2 * 512])
    g2_d = din("g2", [128, 512])
    gnw_d = din("gn_w", [1, 512])
    gnb_d = din("gn_b", [1, 512])
    prw_d = din("p_rwkv", [512, D])
    pat_d = din("p_attn", [512, D])
    wout_d = din("w_out", [D, D])
    wr_d = din("w_router", [D, E])
    wg_d = din("w_gate", [E, D, D])
    wu_d = din("w_up", [E, D, D])
    wd_d = din("w_down", [E, D, D])
    cm_d = din("cmats", [128, 13 * 128])
    out_d = nc.dram_tensor("out", [nseq, T, D], F32, kind="ExternalOutput").ap()
    mod_d = nc.dram_tensor("modscr", [nseq, 6, 128, D], F32, kind="Internal").ap()
    y_d = nc.dram_tensor("yscr", [2, T, 512], F32, kind="Internal").ap()
    u_d = nc.dram_tensor("uscr", [128, 8, T], BF16, kind="Internal").ap()
    dbg_d = None
    if dbg is not None:
        dbg_d = nc.dram_tensor("dbg", list(dbg[1]), F32, kind="ExternalOutput").ap()

    def sb(name, shape, dt=F32):
        return nc.alloc_sbuf_tensor('sb_' + name, list(shape), dt).ap()

    pp = sb("pp", [128, NPP])
    ident = sb("ident", [128, 128], BF16)
    identf = sb("identf", [128, 128])
    cmb = sb("cmb", [128, 13, 128], BF16)
    w2c = sb("w2c", [128, 2, 512], BF16)
    a2c = sb("a2c", [128, 2, 512], BF16)
    g2 = sb("g2", [128, 512], BF16)
    wr = sb("wr", [128, 8, E], BF16)
    epsc = sb("epsc", [128, 4])
    alpha = sb("alpha", [128, 15])
    oneminus_ka = sb("omka", [128, 4])
    two_omka = sb("omka2", [128, 4])
    negkkc = sb("negone", [128, 1])
    esk = sb("esk", [128, 4])
    rmask = sb("rmask", [128, TBS])
    iota_row = sb("iota_row", [128, CAP])
    ident4 = sb("ident4", [128, 4, 128], BF16)
    kar = sb("kar", [128, 4])
    c2r = sb("c2r", [128, 4])
    afftm = sb("afftm", [128, NT, E])
    slot_tm = sb("slot_tm", [128, NT, E])
    affhl = sb("affhl", [128, NT, E, 2], BF16)

    BLK1, ROT, MPREV, MNEXT = 0, 1, 2, 3
    MZT = (4, 5)
    MZ = (6, 7)
    HSEL = 8
    VP = (9, 10)

    ps = [nc.alloc_psum_tensor(f"ps{i}", [128, 512], F32).ap() for i in range(8)]
    psk = [f"ps{i}" for i in range(8)]

    AR = Arena(nc, "arena", 192 * 1024)

    def col(name, j=0, n=1):
        o, w = PP[name]
        return pp[:, o + j:o + j + n]

    S.dma('sp', pp, pp_d, writes=['pp'])
    S.dma('pool', cmb.rearrange("p a b -> p (a b)"), cm_d, writes=['cmb'])
    S.dma('pool', w2c.rearrange("p a b -> p (a b)"), w2c_d, writes=['w2c'])
    S.dma('pool', a2c.rearrange("p a b -> p (a b)"), a2c_d, writes=['a2c'])
    S.dma('pool', g2, g2_d, writes=['g2'])
    S.dma('pool', wr, wr_d.rearrange("(k p) e -> p k e", p=128), writes=['wr'])
    S.op('pool', lambda: POOL.memset(identf, 1.0), writes=['identf'])
    S.op('pool', lambda: POOL.affine_select(out=identf, in_=identf, pattern=[[1, 128]], compare_op=ALU.is_equal,
                                            fill=0.0, base=0, channel_multiplier=-1), reads=['identf'], writes=['identf'])
    S.op('dve', lambda: V.tensor_copy(out=ident, in_=identf), reads=['identf'], writes=['ident'])
    for j in range(4):
        S.op('dve', lambda: V.tensor_copy(out=ident4[:, j, :], in_=identf), reads=['identf'], writes=['ident4'])
    S.op('pool', lambda: POOL.memset(epsc[:, 0:1], 1e-6), writes=['epsc'])
    S.op('pool', lambda: POOL.memset(epsc[:, 1:2], 64e-5), reads=['epsc'], writes=['epsc'])
    S.op('pool', lambda: POOL.memset(epsc[:, 2:3], 1e-24), reads=['epsc'], writes=['epsc'])
    S.op('pool', lambda: POOL.memset(epsc[:, 3:4], 0.0), reads=['epsc'], writes=['epsc'])
    S.op('pool', lambda: POOL.memset(negkkc, -1.0), writes=['negone'])
    S.op('dve', lambda: V.tensor_tensor(out=alpha, in0=col("mp", 0, 15), in1=col("mn", 0, 15), op=ALU.add), reads=['pp'], writes=['alpha'])
    S.op('dve', lambda: V.tensor_scalar(out=alpha, in0=alpha, scalar1=-1.0, scalar2=1.0, op0=ALU.mult, op1=ALU.add), reads=['alpha'], writes=['alpha'])
    S.op('dve', lambda: V.tensor_scalar(out=oneminus_ka, in0=col("ka", 0, 4), scalar1=-1.0, scalar2=1.0, op0=ALU.mult, op1=ALU.add), reads=['pp'], writes=['omka'])
    S.op('dve', lambda: V.tensor_scalar(out=two_omka, in0=col("ka", 0, 4), scalar1=-2.0, scalar2=2.0, op0=ALU.mult, op1=ALU.add), reads=['pp'], writes=['omka2'])
    S.op('act', lambda: ACT.activation(out=esk, in_=col("sink", 0, 4), func=AF.Exp), reads=['pp'], writes=['esk'])
    S.op('dve', lambda: V.tensor_tensor(out=kar, in0=col("ka", 0, 4), in1=col("rk", 0, 4), op=ALU.mult), reads=['pp'], writes=['kar'])
    S.op('dve', lambda: V.tensor_tensor(out=c2r, in0=two_omka, in1=col("rk", 0, 4), op=ALU.mult), reads=['pp', 'omka2'], writes=['kar'])
    S.op('pool', lambda: POOL.memset(rmask, 1.0), writes=['rmask'])
    S.op('pool', lambda: POOL.memset(rmask.rearrange("p (c t) -> p c t", t=64)[:, :, 0:1], 0.0), reads=['rmask'], writes=['rmask'])
    S.op('pool', lambda: POOL.iota(iota_row, pattern=[[1, CAP]], base=0, channel_multiplier=0, allow_small_or_imprecise_dtypes=True), writes=['iota_row'])

    def debug_out(ap_sb, key, rows=None):
        S.dma('sp', dbg_d if rows is None else rows, ap_sb, reads=[key])

    def phase_adaln():
        AR.seek(0)
        csil = [AR.alloc([128, 8], F32) for _ in range(nseq)]
        crep = [AR.alloc([128, 9, 128], F32) for _ in range(nseq)]
        wblk = [AR.alloc([128, 9, 512], F32) for _ in range(3)]
        g1B = AR.alloc([128, D], F32)
        g2B = AR.alloc([128, D], F32)
        mt = [AR.alloc([128, 512], F32) for _ in range(4)]
        S.dma('sp', g1B, n1g_d.partition_broadcast(128), writes=['g1B'])
        S.dma('sp', g2B, n2g_d.partition_broadcast(128), writes=['g2B'])
        for b in range(3):
            S.op('pool', lambda: POOL.memset(wblk[b][:, 8, :], 0.0), writes=[('wblk', b)])
        for s in range(nseq):
            S.dma('sp', csil[s], cT_d[s], writes=[('csil', s)])
            S.op('act', lambda: ACT.activation(out=csil[s], in_=csil[s], func=AF.Silu), reads=[('csil', s)], writes=[('csil', s)])
            S.op('pool', lambda: POOL.memset(crep[s][:, 8, :], 0.0), writes=[('crep', s)])
            S.op('pool', lambda: POOL.memset(crep[s][0:1, 8, :], 1.0), reads=[('crep', s)], writes=[('crep', s)])
            S.op('dve', lambda: V.tensor_copy(out=crep[s][:, 0:8, :], in_=bc(csil[s].rearrange("p (k o) -> p k o", o=1), [128, 8, 128])),
                 reads=[('csil', s)], writes=[('crep', s)])
        ev = 0
        for jb in range(12):
            b = jb % 3
            piece = jb // 2
            c0 = jb * 512
            S.dma('sp', wblk[b][:, 0:4, :], wada_d[0:512, c0:c0 + 512].rearrange("(k p) n -> p k n", p=128), writes=[('wblk', b)])
            S.dma('act', wblk[b][:, 4:8, :], wada_d[512:1024, c0:c0 + 512].rearrange("(k p) n -> p k n", p=128), writes=[('wblk', b)])
            S.dma('sp', wblk[b][0:1, 8, :], bada_d[:, c0:c0 + 512], writes=[('wblk', b)])
            for s in range(nseq):
                pz, pkz = ps[ev % 4], psk[ev % 4]
                for k in range(9):
                    S.op('pe', lambda: PE.matmul(pz, lhsT=crep[s][:, k, :], rhs=wblk[b][:, k, :], start=(k == 0), stop=(k == 8)),
                         reads=[('crep', s), ('wblk', b)], writes=[pkz], pe_acc=True)
                m = mt[ev % 4]
                lc = (jb % 2) * 512
                if piece == 1:
                    S.op('dve', lambda: V.scalar_tensor_tensor(out=m, in0=pz, scalar=1.0, in1=g1B[:, lc:lc + 512], op0=ALU.add, op1=ALU.mult),
                         reads=[pkz, 'g1B'], writes=[('mt', ev % 4)])
                elif piece == 4:
                    S.op('dve', lambda: V.scalar_tensor_tensor(out=m, in0=pz, scalar=1.0, in1=g2B[:, lc:lc + 512], op0=ALU.add, op1=ALU.mult),
                         reads=[pkz, 'g2B'], writes=[('mt', ev % 4)])
                else:
                    S.op('act', lambda: ACT.copy(out=m, in_=pz), reads=[pkz], writes=[('mt', ev % 4)])
                S.dma('sp', mod_d[s, piece, :, lc:lc + 512], m, reads=[('mt', ev % 4)], writes=[('mod', s, piece)])
                ev += 1
        S.barrier()

    def phase_norm1(s, uT, base):
        AR.seek(base)
        scp = AR.alloc([128, D], F32)
        shp = AR.alloc([128, D], F32)
        xt = [AR.alloc([128, D], F32) for _ in range(2)]
        tmp2 = [AR.alloc([128, D], F32) for _ in range(2)]
        ub = [AR.alloc([128, D], BF16) for _ in range(2)]
        junk2 = [AR.alloc([128, D], BF16) for _ in range(2)]
        ss2 = [AR.alloc([128, 2], F32) for _ in range(2)]
        S.dma('sp', scp, mod_d[s, 1], reads=[('mod', s, 1)], writes=['scp'])
        S.dma('sp', shp, mod_d[s, 0], reads=[('mod', s, 0)], writes=['shp'])
        for i in range(NT):
            b = i % 2
            S.dma('sp', xt[b], x_d[s, i * 128:(i + 1) * 128, :], writes=[('xt', b)])
            tmp, junk, ss = tmp2[b], junk2[b], ss2[b]
            S.op('act', lambda: ACT.activation(out=junk, in_=xt[b], func=AF.Square, accum_out=ss[:, 0:1]), reads=[('xt', b)], writes=[('junk', b), ('ss', b)])
            S.op('act', lambda: ACT.activation(out=ss[:, 1:2], in_=ss[:, 0:1], func=AF.Sqrt, bias=epsc[:, 0:1], scale=1.0 / D), reads=[('ss', b), 'epsc'], writes=[('ss1', b)])
            S.op('dve', lambda: V.reciprocal(out=ss[:, 1:2], in_=ss[:, 1:2]), reads=[('ss1', b)], writes=[('ss1', b)])
            S.op('dve', lambda: V.scalar_tensor_tensor(out=tmp, in0=xt[b], scalar=ss[:, 1:2], in1=scp, op0=ALU.mult, op1=ALU.mult),
                 reads=[('xt', b), ('ss1', b), 'scp'], writes=[('tmp', b)])
            S.op('pool', lambda: POOL.tensor_tensor(out=ub[b], in0=tmp, in1=shp, op=ALU.add), reads=[('tmp', b), 'shp'], writes=[('ub', b)])
            pz = ps[i % 2].bitcast(BF16).rearrange("p (k t) -> p k t", k=8)
            for k in range(8):
                S.op('pe', lambda: PE.transpose(out=pz[:, k, :], in_=ub[b][:, k * 128:(k + 1) * 128], identity=ident),
                     reads=[('ub', b), 'ident'], writes=[psk[i % 2]], pe_acc=True)
            S.op('act', lambda: ACT.copy(out=uT[:, :, i * 128:(i + 1) * 128], in_=pz), reads=[psk[i % 2]], writes=[('uT', i // 4)])

    def phase_rwkv_cols(uT, zsT, base):
        AR.seek(base)
        wg = [AR.alloc([128, 8, 128], BF16) for _ in range(2)]
        ztmpP = [AR.alloc([128, T + 2], F32) for _ in range(2)]
        shtP = [AR.alloc([128, T], F32) for _ in range(2)]
        for q in range(2):
            S.op('pool', lambda: POOL.memset(ztmpP[q][:, 0:1], 0.0), writes=[('ztmp', q)])
            S.op('pool', lambda: POOL.memset(ztmpP[q][:, T + 1:T + 2], 0.0), reads=[('ztmp', q)], writes=[('ztmp', q)])
        for j in range(15):
            b = j % 2
            ztmp, sht = ztmpP[b], shtP[b]
            S.dma('pool', wg[b], win_d[:, j * 128:(j + 1) * 128].rearrange("(k p) n -> p k n", p=128), writes=[('wg', b)])
            for tb in range(NB):
                pz = ps[(j * NB + tb) % 4]
                pk = psk[(j * NB + tb) % 4]
                for k in range(8):
                    S.op('pe', lambda: PE.matmul(pz, lhsT=wg[b][:, k, :], rhs=uT[:, k, tb * 512:(tb + 1) * 512], start=(k == 0), stop=(k == 7)),
                         reads=[('wg', b), ('uT', tb)], writes=[pk], pe_acc=True)
                S.op('act', lambda: ACT.copy(out=ztmp[:, 1 + tb * 512:1 + (tb + 1) * 512], in_=pz), reads=[pk], writes=[('ztmp', b)])
            S.op('dve', lambda: V.tensor_scalar(out=sht, in0=ztmp[:, 1:T + 1], scalar1=alpha[:, j:j + 1], scalar2=None, op0=ALU.mult),
                 reads=[('ztmp', b), 'alpha'], writes=[('sht', b)])
            S.op('dve', lambda: V.scalar_tensor_tensor(out=sht, in0=ztmp[:, 0:T], scalar=col("mp", j), in1=sht, op0=ALU.mult, op1=ALU.add),
                 reads=[('ztmp', b), ('sht', b), 'pp'], writes=[('sht', b)])
            S.op('dve', lambda: V.scalar_tensor_tensor(out=zsT[:, j, :], in0=ztmp[:, 2:T + 2], scalar=col("mn", j), in1=sht, op0=ALU.mult, op1=ALU.add),
                 reads=[('ztmp', b), ('sht', b), 'pp'], writes=[('zs', j)])
            if j == 12:
                S.op('act', lambda: ACT.activation(out=zsT[:, j, :], in_=zsT[:, j, :], func=AF.Tanh), reads=[('zs', j)], writes=[('zs', j)])
            if j == 14:
                S.op('act', lambda: ACT.activation(out=zsT[:, j, :], in_=zsT[:, j, :], func=AF.Sigmoid), reads=[('zs', j)], writes=[('zs', j)])

    def phase_scan(zsT, kkT, base):
        rT = lambda c: zsT[:, c, :]
        kT = lambda c: zsT[:, 4 + c, :]
        vT = lambda c: zsT[:, 8 + c, :]
        wdT = zsT[:, 12, :]
        adT = zsT[:, 13, :]
        AR.seek(base)
        kraw = AR.alloc([128, 512], F32)
        ksq = AR.alloc([128, 512], BF16)
        krs = AR.alloc([128, 512], F32)
        for c in range(4):
            for tb in range(NB):
                sl = slice(tb * 512, (tb + 1) * 512)
                S.op('dve', lambda: V.tensor_scalar(out=kraw, in0=kT(c)[:, sl], scalar1=col("kk", c), scalar2=None, op0=ALU.mult), reads=[('zs', 4 + c), 'pp'], writes=['kraw'])
                S.op('act', lambda: ACT.activation(out=ksq, in_=kraw, func=AF.Square), reads=['kraw'], writes=['ksq'])
                pz, pk = ps[tb % 2], psk[tb % 2]
                S.op('pe', lambda: PE.matmul(pz, lhsT=cmb[:, BLK1, :], rhs=ksq, start=True, stop=True), reads=['ksq', 'cmb'], writes=[pk], pe_acc=True)
                S.op('act', lambda: ACT.activation(out=krs, in_=pz, func=AF.Sqrt, bias=epsc[:, 2:3], scale=1.0), reads=[pk, 'epsc'], writes=['krs'])
                S.op('dve', lambda: V.reciprocal(out=krs, in_=krs), reads=['krs'], writes=['krs'])
                S.op('dve', lambda: V.tensor_tensor(out=kkT[:, c, sl], in0=kraw, in1=krs, op=ALU.mult), reads=['kraw', 'krs'], writes=[('kk', c)])
        S.barrier()
        AR.seek(base)
        sg = AR.alloc([128, 4, TBS], F32)
        ad = AR.alloc([128, 4, TBS], F32)
        cc = AR.alloc([128, 4, TBS], F32)
        t1 = AR.alloc([128, 4, TBS], F32)
        ex = [[AR.alloc([128, TBS], F32) for _ in range(2)] for _ in range(4)]
        kd = AR.alloc([128, 4, TBS], F32)
        bb = AR.alloc([128, 4, TBS], F32)
        pdec = AR.alloc([128, 4, NCH], F32)
        ARz = AR.alloc([128, 4, NCH, 2, 2, 64], BF16)
        Bz = AR.alloc([128, 4, NCH, 2, 64], BF16)
        BKt = AR.alloc([128, 4, NCH, 2, 64], BF16)
        KBh = AR.alloc([128, 4, NCH, 2, 64], BF16)
        KBt = AR.alloc([128, 4, NCH, 128], BF16)
        VZ = AR.alloc([128, NCH, 8, 64], BF16)
        XV = AR.alloc([128, NCH, 8, 64], BF16)
        ZTs = [[AR.alloc([128, 4, 128], BF16) for _ in range(2)] for _ in range(NCH)]
        ATm = [[AR.alloc([128, 4, 128], BF16) for _ in range(2)] for _ in range(NCH)]
        PTm = [[AR.alloc([128, 4, 128], BF16) for _ in range(2)] for _ in range(2)]
        Pm = [[AR.alloc([128, 4, 128], BF16) for _ in range(2)] for _ in range(2)]
        Am = [[AR.alloc([128, 4, 128], BF16) for _ in range(2)] for _ in range(2)]
        W1s = AR.alloc([128, 4, 64], BF16)
        S32 = [AR.alloc([128, 4, 64], F32) for _ in range(2)]
        Sb = [AR.alloc([128, 4, 64], BF16) for _ in range(2)]
        ysb = [AR.alloc([64, 512], F32) for _ in range(2)]
        S.op('pool', lambda: POOL.memset(ARz.rearrange("p a b c d e -> p (a b c d e)"), 0.0), writes=['ARz'])
        S.op('pool', lambda: POOL.memset(Bz.rearrange("p a b c d -> p (a b c d)"), 0.0), writes=['Bz'])
        S.op('pool', lambda: POOL.memset(VZ.rearrange("p a b c -> p (a b c)"), 0.0), writes=['VZ'])

        def chain(gens):
            for g_ in gens:
                yield from g_

        def run_tasks(tasks):
            tasks = list(tasks)
            while tasks:
                for t_ in list(tasks):
                    try:
                        next(t_)
                    except StopIteration:
                        tasks.remove(t_)

        yev = 0
        pendQ = None
        for d in range(2):
            S.op('pool', lambda: POOL.memset(S32[d].rearrange("p a b -> p (a b)"), 0.0), writes=[('S32', d)])
            S.op('pool', lambda: POOL.memset(Sb[d].rearrange("p a b -> p (a b)"), 0.0), writes=[('Sb', d)])
            tbs = range(NTB) if d == 0 else range(NTB - 1, -1, -1)
            for tb in tbs:
                sl = slice(tb * TBS, (tb + 1) * TBS)
                def gen_prep(c):
                    pz, pk = ps[c % 2], psk[c % 2]
                    S.op('pe', lambda: PE.matmul(pz[:, 0:TBS], lhsT=w2c[:, d, c * 128:(c + 1) * 128], rhs=wdT[:, sl], start=True, stop=True),
                         reads=['w2c', ('zs', 12)], writes=[pk], pe_acc=True)
                    S.op('act', lambda: ACT.activation(out=sg[:, c, :], in_=pz[:, 0:TBS], func=AF.Sigmoid, bias=col("w0", d * 4 + c), scale=1.0),
                         reads=[pk, 'pp'], writes=[('sg', c)])
                    pz2, pk2 = ps[2 + c % 2], psk[2 + c % 2]
                    S.op('pe', lambda: PE.matmul(pz2[:, 0:TBS], lhsT=a2c[:, d, c * 128:(c + 1) * 128], rhs=adT[:, sl], start=True, stop=True),
                         reads=['a2c', ('zs', 13)], writes=[pk2], pe_acc=True)
                    S.op('act', lambda: ACT.activation(out=ad[:, c, :], in_=pz2[:, 0:TBS], func=AF.Sigmoid, bias=col("a0", d * 4 + c), scale=1.0),
                         reads=[pk2, 'pp'], writes=[('ad', c)])
                    yield
                    S.op('dve', lambda: V.tensor_tensor_scan(out=cc[:, c, :], data0=rmask, data1=sg[:, c, :], initial=0.0, op0=ALU.mult, op1=ALU.add),
                         reads=['rmask', ('sg', c)], writes=[('cc', c)])
                    cc3 = cc[:, c, :].rearrange("p (h t) -> p h t", t=64)
                    sg3 = sg[:, c, :].rearrange("p (h t) -> p h t", t=64)
                    t13 = t1[:, c, :].rearrange("p (h t) -> p h t", t=64)
                    if d == 1:
                        S.op('dve', lambda: V.tensor_tensor(out=t13, in0=bc(cc3[:, :, 63:64], [128, NCH, 64]), in1=cc3, op=ALU.subtract),
                             reads=[('cc', c)], writes=[('t1', c)])
                        S.op('dve', lambda: V.tensor_tensor(out=cc[:, c, :], in0=t1[:, c, :], in1=sg[:, c, :], op=ALU.add),
                             reads=[('t1', c), ('sg', c)], writes=[('cc', c)])
                    totp = 63 if d == 0 else 0
                    S.op('pool', lambda: POOL.tensor_scalar(out=kd[:, c, :], in0=ad[:, c, :], scalar1=col("ka", c), scalar2=oneminus_ka[:, c:c + 1], op0=ALU.mult, op1=ALU.add),
                         reads=[('ad', c), 'pp', 'omka'], writes=[('kd', c)])
                    S.op('pool', lambda: POOL.tensor_tensor(out=kd[:, c, :], in0=kd[:, c, :], in1=kT(c)[:, sl], op=ALU.mult),
                         reads=[('kd', c), ('zs', 4 + c)], writes=[('kd', c)])
                    S.op('pool', lambda: POOL.tensor_tensor(out=bb[:, c, :], in0=ad[:, c, :], in1=kkT[:, c, sl], op=ALU.mult),
                         reads=[('ad', c), ('kk', c)], writes=[('bb', c)])
                    yield
                    e = ex[c][0]
                    S.op('act', lambda: ACT.activation(out=e, in_=cc[:, c, :], func=AF.Exp, scale=-LAM), reads=[('cc', c)], writes=[('ex', c, 0)])
                    for hp in range(2):
                        pr = slice(hp * 64, (hp + 1) * 64)
                        S.op('dve', lambda: V.tensor_tensor(out=ARz[pr, c, :, 0, hp, :], in0=rT(c)[pr, sl].rearrange("p (h t) -> p h t", t=64),
                                                            in1=e[pr, :].rearrange("p (h t) -> p h t", t=64), op=ALU.mult),
                             reads=[('zs', c), ('ex', c, 0)], writes=['ARz'])
                    yield
                    e = ex[c][1]
                    S.op('act', lambda: ACT.activation(out=e, in_=cc[:, c, :], func=AF.Exp, scale=LAM), reads=[('cc', c)], writes=[('ex', c, 1)])
                    S.op('dve', lambda: V.tensor_tensor(out=BKt[:, c, :, 0, :], in0=kd[:, c, :].rearrange("p (h t) -> p h t", t=64),
                                                        in1=e.rearrange("p (h t) -> p h t", t=64), op=ALU.mult),
                         reads=[('kd', c), ('ex', c, 1)], writes=['BKt'])
                    S.op('dve', lambda: V.tensor_tensor(out=BKt[:, c, :, 1, :], in0=bb[:, c, :].rearrange("p (h t) -> p h t", t=64),
                                                        in1=e.rearrange("p (h t) -> p h t", t=64), op=ALU.mult),
                         reads=[('bb', c), ('ex', c, 1)], writes=['BKt'])
                    for hp in range(2):
                        pr = slice(hp * 64, (hp + 1) * 64)
                        S.op('act', lambda: ACT.copy(out=Bz[pr, c, :, hp, :], in_=BKt[pr, c, :, 1, :]), reads=['BKt'], writes=['Bz'])
                    yield
                    S.op('dve', lambda: V.tensor_tensor(out=t1[:, c, :], in0=cc[:, c, :], in1=sg[:, c, :], op=ALU.subtract),
                         reads=[('cc', c), ('sg', c)], writes=[('t1', c)])
                    e = ex[c][0]
                    S.op('act', lambda: ACT.activation(out=e, in_=t1[:, c, :], func=AF.Exp, scale=-LAM), reads=[('t1', c)], writes=[('ex', c, 0)])
                    for hp in range(2):
                        pr = slice(hp * 64, (hp + 1) * 64)
                        S.op('dve', lambda: V.scalar_tensor_tensor(out=ARz[pr, c, :, 1, hp, :], in0=kkT[pr, c, sl].rearrange("p (h t) -> p h t", t=64),
                                                                   scalar=-1.0, in1=e[pr, :].rearrange("p (h t) -> p h t", t=64), op0=ALU.mult, op1=ALU.mult),
                             reads=[('kk', c), ('ex', c, 0)], writes=['ARz'])
                    yield
                    S.op('dve', lambda: V.tensor_tensor(out=t13, in0=bc(cc3[:, :, totp:totp + 1], [128, NCH, 64]), in1=cc3, op=ALU.subtract),
                         reads=[('cc', c)], writes=[('t1', c)])
                    e = ex[c][1]
                    S.op('act', lambda: ACT.activation(out=e, in_=t1[:, c, :], func=AF.Exp, scale=-LAM), reads=[('t1', c)], writes=[('ex', c, 1)])
                    S.op('pool', lambda: POOL.tensor_tensor(out=KBh[:, c, :, 0, :], in0=kd[:, c, :].rearrange("p (h t) -> p h t", t=64),
                                                        in1=e.rearrange("p (h t) -> p h t", t=64), op=ALU.mult),
                         reads=[('kd', c), ('ex', c, 1)], writes=['KBh'])
                    S.op('pool', lambda: POOL.tensor_tensor(out=KBh[:, c, :, 1, :], in0=bb[:, c, :].rearrange("p (h t) -> p h t", t=64),
                                                        in1=e.rearrange("p (h t) -> p h t", t=64), op=ALU.mult),
                         reads=[('bb', c), ('ex', c, 1)], writes=['KBh'])
                    S.op('act', lambda: ACT.activation(out=pdec[:, c, :].rearrange("p (h o) -> p h o", o=1), in_=cc3[:, :, totp:totp + 1], func=AF.Exp, scale=-LAM), reads=[('cc', c)], writes=['pdec'])
                ptasks = [gen_prep(c_) for c_ in range(4)]
                for t_ in ptasks:
                    next(t_)
                if pendQ is not None:
                    for _ in range(3):
                        next(pendQ, None)
                for t_ in ptasks:
                    next(t_)
                if pendQ is not None:
                    run_tasks([pendQ])
                    pendQ = None
                run_tasks(ptasks)
                for ch in range(NCH):
                    pz = ps[4 + ch % 2].bitcast(BF16)
                    pk = psk[4 + ch % 2]
                    pzv = pz[0:64, 0:512].rearrange("p (c n) -> p c n", c=4)
                    for c in range(4):
                        S.op('pe', lambda: PE.transpose(out=pzv[:, c, :], in_=vT(c)[:, tb * TBS + ch * 64: tb * TBS + (ch + 1) * 64], identity=ident),
                             reads=[('zs', 8 + c), 'ident'], writes=[pk], pe_acc=True)
                    S.op('act', lambda: ACT.copy(out=VZ[0:64, ch, :, :].rearrange("p h v -> p (h v)"), in_=pz[0:64, 0:512]), reads=[pk], writes=[('VZ', ch)])
                    S.op('act', lambda: ACT.copy(out=XV[0:64, ch, :, :].rearrange("p h v -> p (h v)"), in_=pz[0:64, 0:512]), reads=[pk], writes=[('XVv', ch)])
                    pzk = pz[:, 512:1024].rearrange("p (c n) -> p c n", c=4)
                    for c in range(4):
                        S.op('pe', lambda: PE.transpose(out=pzk[:, c, :], in_=KBh[:, c, ch, :, :].rearrange("p a t -> p (a t)"), identity=ident),
                             reads=['KBh', 'ident'], writes=[pk], pe_acc=True)
                    S.op('dve', lambda: V.tensor_copy(out=KBt[:, :, ch, :], in_=pzk), reads=[pk], writes=[('KBt', ch)])
                MNT = cmb[:, 11 + d, :]
                MN = cmb[:, 12 - d, :]

                def gen_D(ch, slot, par):
                    pA, pkA = ps[2 * par], psk[2 * par]
                    pB, pkB = ps[2 * par + 1], psk[2 * par + 1]
                    pA3 = pA.rearrange("p (j n) -> p j n", j=4)
                    pB3 = pB.rearrange("p (j n) -> p j n", j=4)
                    mzt = cmb[:, MZT[d], :]
                    for half in range(2):
                        pz3 = pA3 if half == 0 else pB3
                        pkz = pkA if half == 0 else pkB
                        for j in range(4):
                            h = half * 4 + j
                            c, hp = h // 2, h % 2
                            bk = BKt[:, c, ch, :, :].rearrange("p a t -> p (a t)")
                            S.op('pe', lambda: PE.matmul(pz3[:, j, :].rearrange("p (a t) -> p a t", a=2), lhsT=bk, rhs=ARz[:, c, ch, :, hp, :], start=True, stop=True),
                                 reads=['BKt', 'ARz'], writes=[pkz], pe_acc=True)
                        S.op('dve', lambda: V.tensor_tensor(out=ZTs[slot][half], in0=pz3, in1=bc(mzt.rearrange("p (o n) -> p o n", o=1), [128, 4, 128]), op=ALU.mult),
                             reads=[pkz, 'cmb'], writes=[('ZTs', slot, half)])
                    yield
                    for c in range(4):
                        bz = Bz[:, c, ch, :, :].rearrange("p a t -> p (a t)")
                        az = ARz[:, c, ch, 1, :, :].rearrange("p a t -> p (a t)")
                        S.op('pe', lambda: PE.matmul(pA3[:, c, :], lhsT=bz, rhs=az, start=True, stop=True), reads=['Bz', 'ARz'], writes=[pkA], pe_acc=True)
                        S.op('pe', lambda: PE.matmul(pB3[:, c, :], lhsT=az, rhs=bz, start=True, stop=True), reads=['Bz', 'ARz'], writes=[pkB], pe_acc=True)
                    S.op('dve', lambda: V.tensor_tensor(out=PTm[par][0], in0=pA3, in1=bc(MNT.rearrange("p (o n) -> p o n", o=1), [128, 4, 128]), op=ALU.mult),
                         reads=[pkA, 'cmb'], writes=[('PT', par, 0)])
                    S.op('dve', lambda: V.tensor_tensor(out=Pm[par][0], in0=pB3, in1=bc(MN.rearrange("p (o n) -> p o n", o=1), [128, 4, 128]), op=ALU.mult),
                         reads=[pkB, 'cmb'], writes=[('P', par, 0)])
                    S.op('pool', lambda: POOL.tensor_tensor(out=ATm[slot][0], in0=PTm[par][0], in1=ident4, op=ALU.add), reads=[('PT', par, 0), 'ident4'], writes=[('AT', slot, 0)])
                    S.op('pool', lambda: POOL.tensor_tensor(out=Am[par][0], in0=Pm[par][0], in1=ident4, op=ALU.add), reads=[('P', par, 0), 'ident4'], writes=[('A', par, 0)])
                    yield
                    cur = 0
                    for lev in range(1, 6):
                        nxt = 1 - cur
                        for j in range(4):
                            S.op('pe', lambda: PE.matmul(pA3[:, j, :], lhsT=Pm[par][cur][:, j, :], rhs=PTm[par][cur][:, j, :], start=True, stop=True),
                                 reads=[('P', par, cur), ('PT', par, cur)], writes=[pkA], pe_acc=True)
                            if lev < 5:
                                S.op('pe', lambda: PE.matmul(pB3[:, j, :], lhsT=PTm[par][cur][:, j, :], rhs=Pm[par][cur][:, j, :], start=True, stop=True),
                                     reads=[('P', par, cur), ('PT', par, cur)], writes=[pkB], pe_acc=True)
                        S.op('act', lambda: ACT.copy(out=PTm[par][nxt], in_=pA3), reads=[pkA], writes=[('PT', par, nxt)])
                        if lev < 5:
                            S.op('dve', lambda: V.tensor_copy(out=Pm[par][nxt], in_=pB3), reads=[pkB], writes=[('P', par, nxt)])
                        yield
                        for j in range(4):
                            S.op('pe', lambda: PE.matmul(pA3[:, j, :], lhsT=Am[par][cur][:, j, :], rhs=PTm[par][nxt][:, j, :], start=True, stop=True),
                                 reads=[('A', par, cur), ('PT', par, nxt)], writes=[pkA], pe_acc=True)
                            if lev < 5:
                                S.op('pe', lambda: PE.matmul(pB3[:, j, :], lhsT=PTm[par][nxt][:, j, :], rhs=Am[par][cur][:, j, :], start=True, stop=True),
                                     reads=[('A', par, cur), ('PT', par, nxt)], writes=[pkB], pe_acc=True)
                        S.op('dve', lambda: V.tensor_tensor(out=ATm[slot][nxt], in0=pA3, in1=ATm[slot][cur], op=ALU.add), reads=[pkA, ('AT', slot, cur)], writes=[('AT', slot, nxt)])
                        if lev < 5:
                            S.op('dve', lambda: V.tensor_tensor(out=Am[par][nxt], in0=pB3, in1=Am[par][cur], op=ALU.add), reads=[pkB, ('A', par, cur)], writes=[('A', par, nxt)])
                        yield
                        cur = nxt
                    assert cur == 1

                def gen_Q(ch, slot, tb=tb, d=d):
                    nonlocal yev
                    fin = 1
                    gch = tb * NCH + ch
                    pW, pkW = ps[4], psk[4]
                    pW3 = pW[:, 0:256].rearrange("p (c v) -> p c v", c=4)
                    for h in range(8):
                        c, hp = h // 2, h % 2
                        S.op('pe', lambda: PE.matmul(pW3[hp * 64:(hp + 1) * 64, c, :], lhsT=ZTs[slot][h // 4][:, h % 4, 64:128], rhs=VZ[:, ch, h, :], start=True, stop=False),
                             reads=[('ZTs', slot, h // 4), ('VZ', ch)], writes=[pkW], pe_acc=True)
                        S.op('pe', lambda: PE.matmul(pW3[hp * 64:(hp + 1) * 64, c, :], lhsT=ARz[:, c, ch, 1, hp, :], rhs=Sb[d][:, c, :], start=False, stop=True),
                             reads=['ARz', ('Sb', d)], writes=[pkW], pe_acc=True)
                    S.op('act', lambda: ACT.copy(out=W1s, in_=pW3), reads=[pkW], writes=['W1s'])
                    yield
                    pX, pkX = ps[5], psk[5]
                    pX3 = pX.rearrange("p (h v) -> p h v", h=8)
                    for h in range(8):
                        c, hp = h // 2, h % 2
                        S.op('pe', lambda: PE.matmul(pX3[64:128, h, :], lhsT=ATm[slot][fin][:, c, hp * 64:(hp + 1) * 64], rhs=W1s[:, c, :], start=True, stop=True),
                             reads=[('AT', slot, fin), 'W1s'], writes=[pkX], pe_acc=True)
                    S.op('dve', lambda: V.tensor_copy(out=XV[64:128, ch, :, :], in_=pX3[64:128]), reads=[pkX], writes=[('XVu', ch)])
                    yield
                    pS, pkS = ps[7], psk[7]
                    pS3 = pS[:, 0:256].rearrange("p (c v) -> p c v", c=4)
                    for h in range(8):
                        c, hp = h // 2, h % 2
                        S.op('pe', lambda: PE.matmul(pS3[hp * 64:(hp + 1) * 64, c, :], lhsT=KBt[:, c, ch, hp * 64:(hp + 1) * 64], rhs=XV[:, ch, h, :], start=True, stop=True),
                             reads=[('KBt', ch), ('XVv', ch), ('XVu', ch)], writes=[pkS], pe_acc=True)
                    pY, pkY = ps[6], psk[6]
                    pY3 = pY.rearrange("p (h v) -> p h v", h=8)
                    for h in range(8):
                        c, hp = h // 2, h % 2
                        S.op('pe', lambda: PE.matmul(pY3[0:64, h, :], lhsT=ZTs[slot][h // 4][:, h % 4, 0:64], rhs=XV[:, ch, h, :], start=True, stop=False),
                             reads=[('ZTs', slot, h // 4), ('XVv', ch), ('XVu', ch)], writes=[pkY], pe_acc=True)
                        S.op('pe', lambda: PE.matmul(pY3[0:64, h, :], lhsT=ARz[:, c, ch, 0, hp, :], rhs=Sb[d][:, c, :], start=False, stop=True),
                             reads=['ARz', ('Sb', d)], writes=[pkY], pe_acc=True)
                    for c in range(4):
                        S.op('dve', lambda: V.scalar_tensor_tensor(out=S32[d][:, c, :], in0=S32[d][:, c, :], scalar=pdec[:, c, ch:ch + 1], in1=pS3[:, c, :], op0=ALU.mult, op1=ALU.add),
                             reads=[('S32', d), 'pdec', pkS], writes=[('S32', d)])
                    S.op('act', lambda: ACT.copy(out=Sb[d], in_=S32[d]), reads=[('S32', d)], writes=[('Sb', d)])
                    yb_ = ysb[yev % 2]
                    S.op('act', lambda: ACT.copy(out=yb_, in_=pY[0:64, :]), reads=[pkY], writes=[('ysb', yev % 2)])
                    S.dma('sp', y_d[d, gch * 64:(gch + 1) * 64, :], yb_, reads=[('ysb', yev % 2)], writes=[('yscr', d, gch // 2)])
                    yev += 1
                    yield

                chs = list(range(NCH)) if d == 0 else list(range(NCH - 1, -1, -1))
                pend = []
                for r in range(0, NCH, 2):
                    tasks = [gen_D(chs[r], r, 0), gen_D(chs[r + 1], r + 1, 1)]
                    if pend:
                        tasks.append(chain([gen_Q(c_, s_) for (c_, s_) in pend]))
                    run_tasks(tasks)
                    pend = [(chs[r], r), (chs[r + 1], r + 1)]
                pendQ = chain([gen_Q(c_, s_) for (c_, s_) in pend])
        if pendQ is not None:
            run_tasks([pendQ])
            pendQ = None
        S.barrier()


    def phase_post(zsT, yaT, base):
        rT4 = zsT[:, 0:4, :]
        kT4 = zsT[:, 4:8, :]
        adT = zsT[:, 13, :]
        gdT = zsT[:, 14, :]
        AR.seek(base)
        gnwB = AR.alloc([128, 512], F32)
        gnbB = AR.alloc([128, 512], F32)
        P2 = lambda shape, dt: [AR.alloc(shape, dt) for _ in range(2)]
        Yf, Yb = P2([128, 512], F32), P2([128, 512], F32)
        ta0, ta1 = P2([128, 4, 128], F32), P2([128, 4, 128], F32)
        kf2 = P2([128, 4, 128], F32)
        prod2 = P2([128, 4, 128], BF16)
        rows2 = P2([128, 8], F32)
        bon2 = P2([128, 512], F32)
        y2 = P2([128, 512], F32)
        sq2 = P2([128, 512], F32)
        st2 = P2([128, 4, 8], F32)
        yab2 = P2([128, 512], BF16)
        S.dma('sp', gnwB, gnw_d.partition_broadcast(128), writes=['gnwB'])
        S.dma('sp', gnbB, gnb_d.partition_broadcast(128), writes=['gnbB'])
        def gen_tile(i):
            b = i % 2
            sl = slice(i * 128, (i + 1) * 128)
            ta = (ta0[b], ta1[b])
            kf, prod, rows, bon, y, sq, st, yab = kf2[b], prod2[b], rows2[b], bon2[b], y2[b], sq2[b], st2[b], yab2[b]
            bA, bB, bC, bD = 4 * b, 4 * b + 1, 4 * b + 2, 4 * b + 3
            S.dma('sp', Yf[b], y_d[0, sl, :], writes=[('Yf', b)])
            S.dma('sp', Yb[b], y_d[1, sl, :], writes=[('Yb', b)])
            for d in range(2):
                bk_ = bA if d == 0 else bB
                pz3 = ps[bk_].rearrange("p (c n) -> p c n", c=4)
                for c in range(4):
                    S.op('pe', lambda: PE.matmul(pz3[:, c, :], lhsT=a2c[:, d, c * 128:(c + 1) * 128], rhs=adT[:, sl], start=True, stop=True),
                         reads=['a2c'], writes=[psk[bk_]], pe_acc=True)
                for c in range(4):
                    S.op('act', lambda: ACT.activation(out=ta[d][:, c, :], in_=pz3[:, c, :], func=AF.Sigmoid, bias=col("a0", d * 4 + c), scale=1.0),
                         reads=[psk[bk_], 'pp'], writes=[('ta', b, d)])
            yield
            S.op('pool', lambda: POOL.tensor_tensor(out=ta[0], in0=ta[0], in1=ta[1], op=ALU.add), reads=[('ta', b, 0), ('ta', b, 1)], writes=[('ta', b, 0)])
            for c in range(4):
                S.op('pool', lambda: POOL.tensor_scalar(out=kf[:, c, :], in0=ta[0][:, c, :], scalar1=kar[:, c:c + 1], scalar2=c2r[:, c:c + 1], op0=ALU.mult, op1=ALU.add),
                     reads=[('ta', b, 0), 'kar'], writes=[('kf', b)])
            S.op('pool', lambda: POOL.tensor_tensor(out=kf, in0=kf, in1=kT4[:, :, sl], op=ALU.mult), reads=[('kf', b)], writes=[('kf', b)])
            S.op('pool', lambda: POOL.tensor_tensor(out=prod, in0=kf, in1=rT4[:, :, sl], op=ALU.mult), reads=[('kf', b)], writes=[('prod', b)])
            yield
            pr = ps[bB]
            for c in range(4):
                S.op('pe', lambda: PE.matmul(pr[:, c * 2:(c + 1) * 2], lhsT=prod[:, c, :], rhs=cmb[:, HSEL, 0:2], start=True, stop=True),
                     reads=[('prod', b), 'cmb'], writes=[psk[bB]], pe_acc=True)
            S.op('act', lambda: ACT.copy(out=rows, in_=pr[:, 0:8]), reads=[psk[bB]], writes=[('rows', b)])
            yield
            pv = ps[bD].bitcast(BF16)[:, 0:512]
            for c in range(4):
                S.op('pe', lambda: PE.transpose(out=pv[:, c * 128:(c + 1) * 128], in_=zsT[:, 8 + c, sl], identity=ident),
                     reads=['ident'], writes=[psk[bD]], pe_acc=True)
            S.op('dve', lambda: V.tensor_tensor(out=bon.rearrange("p (h v) -> p h v", h=8), in0=pv.rearrange("p (h v) -> p h v", h=8),
                                                in1=bc(rows.rearrange("p (h o) -> p h o", o=1), [128, 8, 64]), op=ALU.mult),
                 reads=[psk[bD], ('rows', b)], writes=[('bon', b)])
            yield
            pg = ps[bC]
            S.op('pe', lambda: PE.matmul(pg, lhsT=gdT[:, sl], rhs=g2, start=True, stop=True), reads=['g2'], writes=[psk[bC]], pe_acc=True)
            y3 = y.rearrange("p (h v) -> p h v", h=8)
            sq3 = sq.rearrange("p (h v) -> p h v", h=8)
            S.op('dve', lambda: V.tensor_tensor(out=y, in0=Yf[b], in1=Yb[b], op=ALU.add), reads=[('Yf', b), ('Yb', b)], writes=[('y', b)])
            yield
            S.op('dve', lambda: V.tensor_reduce(out=st[:, 0, :], in_=y3, axis=AX.X, op=ALU.add), reads=[('y', b)], writes=[('st0', b)])
            S.op('dve', lambda: V.tensor_scalar(out=st[:, 1, :], in0=st[:, 0, :], scalar1=-1.0 / 64, scalar2=None, op0=ALU.mult), reads=[('st0', b)], writes=[('st1', b)])
            S.op('dve', lambda: V.tensor_tensor(out=y3, in0=y3, in1=bc(st[:, 1, :].rearrange("p (h o) -> p h o", o=1), [128, 8, 64]), op=ALU.add),
                 reads=[('y', b), ('st1', b)], writes=[('y', b)])
            yield
            S.op('act', lambda: ACT.activation(out=sq, in_=y, func=AF.Square), reads=[('y', b)], writes=[('sq', b)])
            S.op('dve', lambda: V.tensor_reduce(out=st[:, 2, :], in_=sq3, axis=AX.X, op=ALU.add), reads=[('sq', b)], writes=[('st2', b)])
            yield
            S.op('act', lambda: ACT.activation(out=st[:, 3, :], in_=st[:, 2, :], func=AF.Sqrt, bias=epsc[:, 1:2], scale=1.0 / 64), reads=[('st2', b), 'epsc'], writes=[('st3', b)])
            S.op('dve', lambda: V.reciprocal(out=st[:, 3, :], in_=st[:, 3, :]), reads=[('st3', b)], writes=[('st3', b)])
            S.op('dve', lambda: V.tensor_tensor(out=y3, in0=y3, in1=bc(st[:, 3, :].rearrange("p (h o) -> p h o", o=1), [128, 8, 64]), op=ALU.mult),
                 reads=[('y', b), ('st3', b)], writes=[('y', b)])
            yield
            S.op('dve', lambda: V.tensor_tensor(out=y, in0=y, in1=gnwB, op=ALU.mult), reads=[('y', b), 'gnwB'], writes=[('y', b)])
            S.op('pool', lambda: POOL.tensor_tensor(out=bon, in0=bon, in1=gnbB, op=ALU.add), reads=[('bon', b), 'gnbB'], writes=[('bon', b)])
            S.op('dve', lambda: V.tensor_tensor(out=y, in0=y, in1=bon, op=ALU.add), reads=[('y', b), ('bon', b)], writes=[('y', b)])
            S.op('dve', lambda: V.tensor_tensor(out=yab, in0=y, in1=pg, op=ALU.mult), reads=[('y', b), psk[bC]], writes=[('yab', b)])
            yield
            pt = ps[bA].bitcast(BF16)[:, 0:512]
            for c in range(4):
                S.op('pe', lambda: PE.transpose(out=pt[:, c * 128:(c + 1) * 128], in_=yab[:, c * 128:(c + 1) * 128], identity=ident),
                     reads=[('yab', b), 'ident'], writes=[psk[bA]], pe_acc=True)
            S.op('act', lambda: ACT.copy(out=yaT[:, :, sl], in_=pt.rearrange("p (c n) -> p c n", c=4)), reads=[psk[bA]], writes=['yaT'])
            yield

        def run_tasks(tasks):
            tasks = list(tasks)
            while tasks:
                for t_ in list(tasks):
                    try:
                        next(t_)
                    except StopIteration:
                        tasks.remove(t_)

        for i in range(0, NT, 2):
            run_tasks([gen_tile(i), gen_tile(i + 1)])

    def phase_attn(s, uT, ybT, baseA, baseB):
        AR.seek(baseA)
        cosT = AR.alloc([128, T], F32)
        sinT = AR.alloc([128, T], F32)
        qT = AR.alloc([128, 4, T], BF16)
        kTt = AR.alloc([128, T], BF16)
        vp = AR.alloc([128, 2, NT, 128], BF16)
        AR.seek(baseB)
        wq = [AR.alloc([128, 8, 128], BF16) for _ in range(2)]
        qfL = [AR.alloc([128, 512], F32) for _ in range(2)]
        sqbL = [AR.alloc([128, 512], BF16) for _ in range(2)]
        rsL = [AR.alloc([128, 512], F32) for _ in range(2)]
        qnL = [AR.alloc([128, 512], F32) for _ in range(2)]
        qnbL = [AR.alloc([128, 512], BF16) for _ in range(2)]
        t1L = [AR.alloc([128, 512], F32) for _ in range(2)]
        t2L = [AR.alloc([128, 512], F32) for _ in range(2)]
        pTs = [AR.alloc([128, 512], BF16) for _ in range(6)]
        dn = AR.alloc([128, 512], F32)
        posi = AR.alloc([128, T], I32)
        ang = AR.alloc([128, T], F32)
        ki = AR.alloc([128, T], I32)
        kf = AR.alloc([128, T], F32)
        m1 = AR.alloc([128, T], F32)
        S.dma('sp', posi, pos_d[s].partition_broadcast(128), writes=['posi'])

        def table(dst, shift):
            S.op('dve', lambda: V.tensor_copy(out=ang, in_=posi), reads=['posi'], writes=['ang'])
            S.op('dve', lambda: V.tensor_scalar(out=ang, in0=ang, scalar1=col("invf"), scalar2=shift, op0=ALU.mult, op1=ALU.add), reads=['ang', 'pp'], writes=['ang'])
            S.op('dve', lambda: V.tensor_scalar(out=ki, in0=ang, scalar1=1.0 / TWO_PI, scalar2=None, op0=ALU.mult), reads=['ang'], writes=['ki'])
            S.op('pool', lambda: POOL.tensor_copy(out=kf, in_=ki), reads=['ki'], writes=['kf'])
            S.op('dve', lambda: V.scalar_tensor_tensor(out=ang, in0=kf, scalar=-C1, in1=ang, op0=ALU.mult, op1=ALU.add), reads=['kf', 'ang'], writes=['ang'])
            S.op('dve', lambda: V.scalar_tensor_tensor(out=ang, in0=kf, scalar=-C2, in1=ang, op0=ALU.mult, op1=ALU.add), reads=['kf', 'ang'], writes=['ang'])
            S.op('dve', lambda: V.tensor_scalar(out=m1, in0=ang, scalar1=float(np.pi), scalar2=-TWO_PI, op0=ALU.is_gt, op1=ALU.mult), reads=['ang'], writes=['m1'])
            S.op('pool', lambda: POOL.tensor_tensor(out=ang, in0=ang, in1=m1, op=ALU.add), reads=['ang', 'm1'], writes=['ang'])
            S.op('dve', lambda: V.tensor_scalar(out=m1, in0=ang, scalar1=float(-np.pi), scalar2=TWO_PI, op0=ALU.is_lt, op1=ALU.mult), reads=['ang'], writes=['m1'])
            S.op('pool', lambda: POOL.tensor_tensor(out=ang, in0=ang, in1=m1, op=ALU.add), reads=['ang', 'm1'], writes=['ang'])
            S.op('act', lambda: ACT.activation(out=dst, in_=ang, func=AF.Sin), reads=['ang'], writes=['tab'])

        table(sinT, 0.0)
        table(cosT, float(np.pi / 2))
        def c0_of(c):
            return 1920 + c * 128 if c < 4 else 2432

        def gen_qk(c, tb, L):
            b = c % 2
            gcol = col("qg") if c < 4 else col("kg")
            sl = slice(tb * 512, (tb + 1) * 512)
            qf_, sqb_, rs_, qn_, qnb_, t1_, t2_ = qfL[L], sqbL[L], rsL[L], qnL[L], qnbL[L], t1L[L], t2L[L]
            pz, pk = ps[L], psk[L]
            for k in range(8):
                S.op('pe', lambda: PE.matmul(pz, lhsT=wq[b][:, k, :], rhs=uT[:, k, sl], start=(k == 0), stop=(k == 7)),
                     reads=[('wq', b), ('uT', tb)], writes=[pk], pe_acc=True)
            S.op('act', lambda: ACT.copy(out=qf_, in_=pz), reads=[pk], writes=[('qf', L)])
            S.op('act', lambda: ACT.activation(out=sqb_, in_=qf_, func=AF.Square), reads=[('qf', L)], writes=[('sqb', L)])
            yield
            pr, pkr = ps[2 + L], psk[2 + L]
            S.op('pe', lambda: PE.matmul(pr, lhsT=cmb[:, BLK1, :], rhs=sqb_, start=True, stop=True), reads=[('sqb', L), 'cmb'], writes=[pkr], pe_acc=True)
            S.op('act', lambda: ACT.activation(out=rs_, in_=pr, func=AF.Sqrt, bias=epsc[:, 0:1], scale=1.0 / 64), reads=[pkr, 'epsc'], writes=[('rs', L)])
            yield
            S.op('dve', lambda: V.reciprocal(out=rs_, in_=rs_), reads=[('rs', L)], writes=[('rs', L)])
            S.op('dve', lambda: V.scalar_tensor_tensor(out=qn_, in0=qf_, scalar=gcol, in1=rs_, op0=ALU.mult, op1=ALU.mult), reads=[('qf', L), ('rs', L), 'pp'], writes=[('qn', L)])
            S.op('act', lambda: ACT.copy(out=qnb_, in_=qn_), reads=[('qn', L)], writes=[('qnb', L)])
            yield
            pro, pkro = ps[4 + L], psk[4 + L]
            S.op('pe', lambda: PE.matmul(pro, lhsT=cmb[:, ROT, :], rhs=qnb_, start=True, stop=True), reads=[('qnb', L), 'cmb'], writes=[pkro], pe_acc=True)
            S.op('pool', lambda: POOL.tensor_tensor(out=t1_, in0=qn_, in1=cosT[:, sl], op=ALU.mult), reads=[('qn', L), 'tab'], writes=[('t1', L)])
            yield
            S.op('dve', lambda: V.tensor_tensor(out=t2_, in0=pro, in1=sinT[:, sl], op=ALU.mult), reads=[pkro, 'tab'], writes=[('t2', L)])
            dst = qT[:, c, sl] if c < 4 else kTt[:, sl]
            S.op('dve', lambda: V.tensor_tensor(out=dst, in0=t1_, in1=t2_, op=ALU.add), reads=[('t1', L), ('t2', L)], writes=['qk'])
            yield

        def run_tasks(tasks):
            tasks = list(tasks)
            while tasks:
                for t_ in list(tasks):
                    try:
                        next(t_)
                    except StopIteration:
                        tasks.remove(t_)

        S.dma('pool', wq[0], win_d[:, c0_of(0):c0_of(0) + 128].rearrange("(k p) n -> p k n", p=128), writes=[('wq', 0)])
        for c in range(5):
            if c + 1 < 5:
                S.dma('pool', wq[(c + 1) % 2], win_d[:, c0_of(c + 1):c0_of(c + 1) + 128].rearrange("(k p) n -> p k n", p=128), writes=[('wq', (c + 1) % 2)])
            for tb in range(0, NB, 2):
                run_tasks([gen_qk(c, tb, 0), gen_qk(c, tb + 1, 1)])
        S.op('pool', lambda: POOL.memset(vp.rearrange("p a b c -> p (a b c)"), 0.0), writes=['vp'])
        S.dma('pool', wq[0], win_d[:, 2560:2688].rearrange("(k p) n -> p k n", p=128), writes=[('wq', 0)])
        for i in range(NT):
            pz, pk = ps[i % 2], psk[i % 2]
            for k in range(8):
                S.op('pe', lambda: PE.matmul(pz[:, 0:128], lhsT=uT[:, k, i * 128:(i + 1) * 128], rhs=wq[0][:, k, :], start=(k == 0), stop=(k == 7)),
                     reads=[('wq', 0), ('uT', i // 4)], writes=[pk], pe_acc=True)
            S.op('act', lambda: ACT.copy(out=vp[:, 0, i, 0:64], in_=pz[:, 0:64]), reads=[pk], writes=['vp'])
            S.op('dve', lambda: V.tensor_copy(out=vp[:, 1, i, 64:128], in_=pz[:, 64:128]), reads=[pk], writes=['vp'])
        for n in range(NT):
            qs = slice(n * 128, (n + 1) * 128)
            kbs = [kb for kb in (n - 1, n, n + 1) if 0 <= kb < NT]
            items = [(g, kb) for g in range(2) for kb in kbs]
            for idx, (g, kb) in enumerate(items):
                gp = slice(g * 64, (g + 1) * 64)
                pz, pk = ps[idx % 4], psk[idx % 4]
                S.op('pe', lambda: PE.matmul(pz.rearrange("p (j q) -> p j q", j=4), lhsT=kTt[gp, kb * 128:(kb + 1) * 128], rhs=qT[gp, :, qs], start=True, stop=True),
                     reads=['qk'], writes=[pk], pe_acc=True)
                pt_ = pTs[idx]
                S.op('act', lambda: ACT.activation(out=pt_, in_=pz, func=AF.Exp, scale=0.125), reads=[pk], writes=[('pT', idx)])
                if kb != n:
                    mk = cmb[:, MPREV if kb < n else MNEXT, :]
                    S.op('pool', lambda: POOL.tensor_tensor(out=pt_.rearrange("p (j q) -> p j q", j=4), in0=pt_.rearrange("p (j q) -> p j q", j=4),
                                                            in1=bc(mk.rearrange("p (o q) -> p o q", o=1), [128, 4, 128]), op=ALU.mult),
                         reads=[('pT', idx), 'cmb'], writes=[('pT', idx)])
            po, pko = ps[4 + n % 2], psk[4 + n % 2]
            pd_, pkd = ps[6 + n % 2], psk[6 + n % 2]
            for idx, (g, kb) in enumerate(items):
                S.op('pe', lambda: PE.matmul(po, lhsT=vp[:, g, kb, :], rhs=pTs[idx], start=(idx == 0), stop=(idx == len(items) - 1)),
                     reads=['vp', ('pT', idx)], writes=[pko], pe_acc=True)
            for idx, (g, kb) in enumerate(items):
                S.op('pe', lambda: PE.matmul(pd_, lhsT=cmb[:, VP[g], :], rhs=pTs[idx], start=(idx == 0), stop=(idx == len(items) - 1)),
                     reads=['cmb', ('pT', idx)], writes=[pkd], pe_acc=True)
            S.op('dve', lambda: V.tensor_tensor(out=dn.rearrange("p (j q) -> p j q", j=4), in0=pd_.rearrange("p (j q) -> p j q", j=4),
                                                in1=bc(esk.rearrange("p (j o) -> p j o", o=1), [128, 4, 128]), op=ALU.add), reads=[pkd, 'esk'], writes=['dn'])
            S.op('dve', lambda: V.reciprocal(out=dn, in_=dn), reads=['dn'], writes=['dn'])
            S.op('dve', lambda: V.tensor_tensor(out=ybT[:, :, qs], in0=po.rearrange("p (j q) -> p j q", j=4), in1=dn.rearrange("p (j q) -> p j q", j=4), op=ALU.mult),
                 reads=[pko, 'dn'], writes=['ybT'])

    def phase_merge(uT, yaT, ybT, mergedT, offs):
        AR.seek(offs[0])
        prw = AR.alloc([128, 4, D], BF16)
        AR.seek(offs[1])
        pat = AR.alloc([128, 4, D], BF16)
        wga = [AR.alloc([128, 8, 128], BF16) for _ in range(2)]
        wgb = [AR.alloc([128, 8, 128], BF16) for _ in range(2)]
        sgaP = [AR.alloc([128, 512], BF16) for _ in range(2)]
        sgbP = [AR.alloc([128, 512], BF16) for _ in range(2)]
        t1P = [AR.alloc([128, 512], F32) for _ in range(2)]
        t2P = [AR.alloc([128, 512], F32) for _ in range(2)]
        for hh in range(2):
            S.dma('pool', prw[:, hh * 2:(hh + 1) * 2, :], prw_d[hh * 256:(hh + 1) * 256, :].rearrange("(k p) n -> p k n", p=128), writes=['prw'])
            S.dma('pool', pat[:, hh * 2:(hh + 1) * 2, :], pat_d[hh * 256:(hh + 1) * 256, :].rearrange("(k p) n -> p k n", p=128), writes=['pat'])
        for oc in range(8):
            b = oc % 2
            S.dma('pool', wga[b], win_d[:, 2688 + oc * 128:2688 + (oc + 1) * 128].rearrange("(k p) n -> p k n", p=128), writes=[('wga', b)])
            S.dma('pool', wgb[b], win_d[:, 3712 + oc * 128:3712 + (oc + 1) * 128].rearrange("(k p) n -> p k n", p=128), writes=[('wgb', b)])
            for tb in range(NB):
                sl = slice(tb * 512, (tb + 1) * 512)
                L = tb % 2
                sga, sgb, t1, t2 = sgaP[L], sgbP[L], t1P[L], t2P[L]
                for k in range(8):
                    S.op('pe', lambda: PE.matmul(ps[0 + 4 * L], lhsT=wga[b][:, k, :], rhs=uT[:, k, sl], start=(k == 0), stop=(k == 7)),
                         reads=[('wga', b), ('uT', tb)], writes=[psk[0 + 4 * L]], pe_acc=True)
                S.op('act', lambda: ACT.activation(out=sga, in_=ps[0 + 4 * L], func=AF.Sigmoid), reads=[psk[0 + 4 * L]], writes=[('sga', L)])
                for k in range(8):
                    S.op('pe', lambda: PE.matmul(ps[1 + 4 * L], lhsT=wgb[b][:, k, :], rhs=uT[:, k, sl], start=(k == 0), stop=(k == 7)),
                         reads=[('wgb', b), ('uT', tb)], writes=[psk[1 + 4 * L]], pe_acc=True)
                S.op('act', lambda: ACT.activation(out=sgb, in_=ps[1 + 4 * L], func=AF.Sigmoid), reads=[psk[1 + 4 * L]], writes=[('sgb', L)])
                for k in range(4):
                    S.op('pe', lambda: PE.matmul(ps[2 + 4 * L], lhsT=prw[:, k, oc * 128:(oc + 1) * 128], rhs=yaT[:, k, sl], start=(k == 0), stop=(k == 3)),
                         reads=['prw', 'yaT'], writes=[psk[2 + 4 * L]], pe_acc=True)
                for k in range(4):
                    S.op('pe', lambda: PE.matmul(ps[3 + 4 * L], lhsT=pat[:, k, oc * 128:(oc + 1) * 128], rhs=ybT[:, k, sl], start=(k == 0), stop=(k == 3)),
                         reads=['pat', 'ybT'], writes=[psk[3 + 4 * L]], pe_acc=True)
                S.op('dve', lambda: V.tensor_tensor(out=t1, in0=ps[2 + 4 * L], in1=sga, op=ALU.mult), reads=[psk[2 + 4 * L], ('sga', L)], writes=[('t1', L)])
                S.op('dve', lambda: V.tensor_tensor(out=t2, in0=ps[3 + 4 * L], in1=sgb, op=ALU.mult), reads=[psk[3 + 4 * L], ('sgb', L)], writes=[('t2', L)])
                S.op('pool', lambda: POOL.tensor_tensor(out=mergedT[:, oc, sl], in0=t1, in1=t2, op=ALU.add), reads=[('t1', L), ('t2', L)], writes=[('mg', tb)])

    def phase_x1(s, mergedT, u2tm, base):
        AR.seek(base)
        wo = AR.alloc([128, 8, D], BF16)
        gt1B = AR.alloc([128, D], F32)
        sc2 = AR.alloc([128, D], F32)
        sh2 = AR.alloc([128, D], F32)
        xt = [AR.alloc([128, D], F32) for _ in range(2)]
        x1t = [AR.alloc([128, D], F32) for _ in range(2)]
        tmpP = [AR.alloc([128, D], F32) for _ in range(2)]
        junkP = [AR.alloc([128, D], BF16) for _ in range(2)]
        u2TP = [AR.alloc([128, 8, 128], BF16) for _ in range(2)]
        ssP = [AR.alloc([128, 8], F32) for _ in range(2)]
        exP = [AR.alloc([128, E], F32) for _ in range(2)]
        for hh in range(4):
            S.dma('pool', wo[:, hh * 2:(hh + 1) * 2, :], wout_d[hh * 256:(hh + 1) * 256, :].rearrange("(k p) n -> p k n", p=128), writes=['wo'])
        S.dma('sp', gt1B, mod_d[s, 2], writes=['gt1B'])
        S.dma('sp', sc2, mod_d[s, 4], writes=['sc2'])
        S.dma('sp', sh2, mod_d[s, 3], writes=['sh2'])
        for i in range(NT):
            b = i % 2
            sl = slice(i * 128, (i + 1) * 128)
            S.dma('sp', xt[b], x_d[s, sl, :], writes=[('xt', b)])
            tmp, junk, u2T, ss, ex = tmpP[b], junkP[b], u2TP[b], ssP[b], exP[b]
            for cb in range(2):
                for k in range(8):
                    S.op('pe', lambda: PE.matmul(ps[cb + 6 * b], lhsT=mergedT[:, k, sl], rhs=wo[:, k, cb * 512:(cb + 1) * 512], start=(k == 0), stop=(k == 7)),
                         reads=[('mg', i // 4), 'wo'], writes=[psk[cb + 6 * b]], pe_acc=True)
                S.op('dve', lambda: V.tensor_tensor(out=tmp[:, cb * 512:(cb + 1) * 512], in0=ps[cb + 6 * b], in1=gt1B[:, cb * 512:(cb + 1) * 512], op=ALU.mult),
                     reads=[psk[cb + 6 * b], 'gt1B'], writes=[('tmp', b, cb)])
            S.op('pool', lambda: POOL.tensor_tensor(out=x1t[b], in0=tmp, in1=xt[b], op=ALU.add), reads=[('tmp', b, 0), ('tmp', b, 1), ('xt', b)], writes=[('x1t', b)])
            S.dma('sp', out_d[s, sl, :], x1t[b], reads=[('x1t', b)], writes=[('outd', i)])
            S.op('act', lambda: ACT.activation(out=junk, in_=x1t[b], func=AF.Square, accum_out=ss[:, 0:1]), reads=[('x1t', b)], writes=[('junk', b), ('ss0', b)])
            S.op('act', lambda: ACT.activation(out=ss[:, 1:2], in_=ss[:, 0:1], func=AF.Sqrt, bias=epsc[:, 0:1], scale=1.0 / D), reads=[('ss0', b), 'epsc'], writes=[('ss1', b)])
            S.op('dve', lambda: V.reciprocal(out=ss[:, 1:2], in_=ss[:, 1:2]), reads=[('ss1', b)], writes=[('ss1', b)])
            S.op('dve', lambda: V.scalar_tensor_tensor(out=tmp, in0=x1t[b], scalar=ss[:, 1:2], in1=sc2, op0=ALU.mult, op1=ALU.mult),
                 reads=[('x1t', b), ('ss1', b), 'sc2'], writes=[('tmp', b, 0), ('tmp', b, 1)])
            S.op('pool', lambda: POOL.tensor_tensor(out=u2tm[:, i, :], in0=tmp, in1=sh2, op=ALU.add), reads=[('tmp', b, 0), ('tmp', b, 1), 'sh2'], writes=[('u2', i)])
            pz = ps[2 + i % 2].bitcast(BF16).rearrange("p (k t) -> p k t", k=8)
            pk = psk[2 + i % 2]
            for k in range(8):
                S.op('pe', lambda: PE.transpose(out=pz[:, k, :], in_=u2tm[:, i, k * 128:(k + 1) * 128], identity=ident),
                     reads=[('u2', i), 'ident'], writes=[pk], pe_acc=True)
            S.op('act', lambda: ACT.copy(out=u2T, in_=pz), reads=[pk], writes=[('u2T', b)])
            pl, pkl = ps[4 + i % 2], psk[4 + i % 2]
            for k in range(8):
                S.op('pe', lambda: PE.matmul(pl[:, 0:E], lhsT=u2T[:, k, :], rhs=wr[:, k, :], start=(k == 0), stop=(k == 7)),
                     reads=[('u2T', b), 'wr'], writes=[pkl], pe_acc=True)
            S.op('dve', lambda: V.tensor_reduce(out=ss[:, 2:3], in_=pl[:, 0:E], axis=AX.X, op=ALU.max), reads=[pkl], writes=[('ss2', b)])
            S.op('dve', lambda: V.tensor_scalar(out=ss[:, 3:4], in0=ss[:, 2:3], scalar1=-1.0, scalar2=None, op0=ALU.mult), reads=[('ss2', b)], writes=[('ss3', b)])
            S.op('act', lambda: ACT.activation(out=ex, in_=pl[:, 0:E], func=AF.Exp, bias=ss[:, 3:4], scale=1.0, accum_out=ss[:, 4:5]), reads=[pkl, ('ss3', b)], writes=[('ex', b), ('ss4', b)])
            S.op('dve', lambda: V.reciprocal(out=ss[:, 5:6], in_=ss[:, 4:5]), reads=[('ss4', b)], writes=[('ss5', b)])
            S.op('dve', lambda: V.tensor_scalar(out=afftm[:, i, :], in0=ex, scalar1=ss[:, 5:6], scalar2=None, op0=ALU.mult), reads=[('ex', b), ('ss5', b)], writes=['afftm'])

    def phase_moe(s, u2tm, base):
        AR.seek(base)
        affT = AR.alloc([16, T], F32)
        work = AR.alloc([16, T], F32)
        maskT = AR.alloc([16, T], F32)
        slotT = AR.alloc([16, T], F32)
        mx8 = AR.alloc([16, 8], F32)
        for i in range(NT):
            pz = ps[i // 4]
            S.op('pe', lambda: PE.transpose(out=pz[0:16, (i % 4) * 128:(i % 4 + 1) * 128], in_=afftm[:, i, :], identity=identf),
                 reads=['afftm', 'identf'], writes=[psk[i // 4]], pe_acc=True)
        for q in range(4):
            S.op('act', lambda: ACT.copy(out=affT[:, q * 512:(q + 1) * 512], in_=ps[q][0:16, :]), reads=[psk[q]], writes=['affT'])
        S.op('dve', lambda: V.tensor_copy(out=work, in_=affT), reads=['affT'], writes=['work'])
        for it in range(CAP // 8):
            S.op('dve', lambda: V.max(out=mx8, in_=work), reads=['work'], writes=['mx8'])
            if it < CAP // 8 - 1:
                S.op('dve', lambda: V.match_replace(out=work, in_to_replace=mx8, in_values=work, imm_value=-1.0), reads=['work', 'mx8'], writes=['work'])
        S.op('dve', lambda: V.tensor_scalar(out=maskT, in0=affT, scalar1=mx8[:, 7:8], scalar2=None, op0=ALU.is_ge), reads=['affT', 'mx8'], writes=['maskT'])
        S.op('pool', lambda: POOL.memset(work, 1.0), reads=['work'], writes=['work'])
        S.op('dve', lambda: V.tensor_tensor_scan(out=slotT, data0=work, data1=maskT, initial=0.0, op0=ALU.mult, op1=ALU.add), reads=['work', 'maskT'], writes=['slotT'])
        S.op('dve', lambda: V.tensor_tensor(out=slotT, in0=slotT, in1=maskT, op=ALU.mult), reads=['slotT', 'maskT'], writes=['slotT'])
        S.op('dve', lambda: V.tensor_scalar(out=slotT, in0=slotT, scalar1=-1.0, scalar2=None, op0=ALU.add), reads=['slotT'], writes=['slotT'])
        pz = ps[4]
        for i in range(NT):
            S.op('pe', lambda: PE.transpose(out=pz[:, i * 16:(i + 1) * 16], in_=slotT[:, i * 128:(i + 1) * 128], identity=identf[0:16, 0:16]),
                 reads=['slotT', 'identf'], writes=[psk[4]], pe_acc=True)
        S.op('act', lambda: ACT.copy(out=slot_tm.rearrange("p i e -> p (i e)"), in_=pz[:, 0:256]), reads=[psk[4]], writes=['slot_tm'])
        S.op('dve', lambda: V.tensor_copy(out=affhl[:, :, :, 0], in_=afftm), reads=['afftm'], writes=['affhl'])
        S.op('dve', lambda: V.tensor_tensor(out=affhl[:, :, :, 1], in0=afftm, in1=affhl[:, :, :, 0], op=ALU.subtract), reads=['afftm', 'affhl'], writes=['affhl'])
        S.barrier()
        AR.seek(base)
        ye = AR.alloc([128, E, 2, D], BF16)
        Wg = AR.alloc([128, 8, D], BF16)
        Wu = AR.alloc([128, 8, D], BF16)
        Wd = AR.alloc([128, 8, D], BF16)
        wbase = AR.ptr
        Pe = AR.alloc([128, NT, CAP], BF16)
        xeT = AR.alloc([128, 8, CAP], BF16)
        hT = AR.alloc([128, 8, CAP], BF16)
        hs = AR.alloc([128, CAP], F32)
        affs = AR.alloc([128, 4], F32)
        gt2B = AR.alloc([128, D], F32)
        S.dma('sp', gt2B, mod_d[s, 5], writes=['gt2B'])
        for e in range(E):
            for (wt, wsrc, nm) in ((Wg, wg_d, 'Wg'), (Wu, wu_d, 'Wu'), (Wd, wd_d, 'Wd')):
                for hh in range(4):
                    S.dma('pool', wt[:, hh * 2:(hh + 1) * 2, :], wsrc[e, hh * 256:(hh + 1) * 256, :].rearrange("(k p) n -> p k n", p=128), writes=[(nm, hh)])
            for i in range(NT):
                S.op('dve', lambda: V.tensor_scalar(out=Pe[:, i, :], in0=iota_row, scalar1=slot_tm[:, i, e:e + 1], scalar2=None, op0=ALU.is_equal),
                     reads=['iota_row', 'slot_tm'], writes=[('Pe', i)])
            for fc in range(8):
                pz, pk = ps[fc // 2], psk[fc // 2]
                pzs = pz[:, (fc % 2) * 256:(fc % 2 + 1) * 256]
                for i in range(NT):
                    S.op('pe', lambda: PE.matmul(pzs, lhsT=u2tm[:, i, fc * 128:(fc + 1) * 128], rhs=Pe[:, i, :], start=(i == 0), stop=(i == NT - 1)),
                         reads=[('u2', i), ('Pe', i)], writes=[pk], pe_acc=True)
                S.op('act', lambda: ACT.copy(out=xeT[:, fc, :], in_=pzs), reads=[pk], writes=[('xeT', fc)])
            pa, pka = ps[4], psk[4]
            for half in range(2):
                for i in range(NT):
                    S.op('pe', lambda: PE.matmul(pa[:, half * 2:(half + 1) * 2], lhsT=Pe[:, i, half * 128:(half + 1) * 128], rhs=affhl[:, i, e, :], start=(i == 0), stop=(i == NT - 1)),
                         reads=[('Pe', i), 'affhl'], writes=[pka], pe_acc=True)
            S.op('dve', lambda: V.tensor_reduce(out=affs[:, 0:2], in_=pa[:, 0:4].rearrange("p (h t) -> p h t", t=2), axis=AX.X, op=ALU.add), reads=[pka], writes=['affs'])
            for fk in range(8):
                pg, pkg = ps[5], psk[5]
                pu, pku = ps[6], psk[6]
                for k in range(8):
                    S.op('pe', lambda: PE.matmul(pg[:, 0:CAP], lhsT=Wg[:, k, fk * 128:(fk + 1) * 128], rhs=xeT[:, k, :], start=(k == 0), stop=(k == 7)),
                         reads=[('Wg', k // 2), ('xeT', k)], writes=[pkg], pe_acc=True)
                for k in range(8):
                    S.op('pe', lambda: PE.matmul(pu[:, 0:CAP], lhsT=Wu[:, k, fk * 128:(fk + 1) * 128], rhs=xeT[:, k, :], start=(k == 0), stop=(k == 7)),
                         reads=[('Wu', k // 2), ('xeT', k)], writes=[pku], pe_acc=True)
                S.op('act', lambda: ACT.activation(out=hs, in_=pg[:, 0:CAP], func=AF.Silu), reads=[pkg], writes=['hs'])
                S.op('dve', lambda: V.tensor_tensor(out=hT[:, fk, :], in0=pu[:, 0:CAP], in1=hs, op=ALU.mult), reads=[pku, 'hs'], writes=[('hT', fk)])
            for half in range(2):
                for cb in range(2):
                    py, pky = ps[7] if (half * 2 + cb) % 2 else ps[4], psk[7] if (half * 2 + cb) % 2 else psk[4]
                    for fk in range(8):
                        S.op('pe', lambda: PE.matmul(py, lhsT=hT[:, fk, half * 128:(half + 1) * 128], rhs=Wd[:, fk, cb * 512:(cb + 1) * 512], start=(fk == 0), stop=(fk == 7)),
                             reads=[('hT', fk), ('Wd', fk // 2), 'affs'], writes=[pky], pe_acc=True)
                    S.op('dve', lambda: V.tensor_scalar(out=ye[:, e, half, cb * 512:(cb + 1) * 512], in0=py, scalar1=affs[:, half:half + 1], scalar2=None, op0=ALU.mult),
                         reads=[pky, 'affs'], writes=['ye'])
        S.barrier()
        AR.seek(wbase - 3 * 8 * D * 2)
        Pall = AR.alloc([128, E, CAP], BF16)
        PT = AR.alloc([128, 2 * E, 128], BF16)
        x1t = [AR.alloc([128, D], F32) for _ in range(2)]
        ot = [AR.alloc([128, D], F32) for _ in range(2)]
        for i in range(NT):
            b = i % 2
            sl = slice(i * 128, (i + 1) * 128)
            S.dma('sp', x1t[b], out_d[s, sl, :], reads=[('outd', i)], writes=[('x1t', b)])
            for e in range(E):
                S.op('dve', lambda: V.tensor_scalar(out=Pall[:, e, :], in0=iota_row, scalar1=slot_tm[:, i, e:e + 1], scalar2=None, op0=ALU.is_equal),
                     reads=['iota_row', 'slot_tm'], writes=[('Pall', e // 4)])
            for q in range(4):
                pz = ps[q].bitcast(BF16).rearrange("p (j t) -> p j t", j=8)
                for j in range(8):
                    idx = q * 8 + j
                    e, half = idx // 2, idx % 2
                    S.op('pe', lambda: PE.transpose(out=pz[:, j, :], in_=Pall[:, e, half * 128:(half + 1) * 128], identity=ident),
                         reads=[('Pall', e // 4), 'ident'], writes=[psk[q]], pe_acc=True)
                if q % 2 == 0:
                    S.op('act', lambda: ACT.copy(out=PT[:, q * 8:(q + 1) * 8, :], in_=pz), reads=[psk[q]], writes=[('PT', q)])
                else:
                    S.op('dve', lambda: V.tensor_copy(out=PT[:, q * 8:(q + 1) * 8, :], in_=pz), reads=[psk[q]], writes=[('PT', q)])
            for cb in range(2):
                po, pko = ps[4 + cb + 2 * (i % 2)], psk[4 + cb + 2 * (i % 2)]
                for idx in range(2 * E):
                    e, half = idx // 2, idx % 2
                    S.op('pe', lambda: PE.matmul(po, lhsT=PT[:, idx, :], rhs=ye[:, e, half, cb * 512:(cb + 1) * 512], start=(idx == 0), stop=(idx == 2 * E - 1)),
                         reads=[('PT', idx // 8), 'ye'], writes=[pko], pe_acc=True)
                S.op('dve', lambda: V.tensor_tensor(out=ot[b][:, cb * 512:(cb + 1) * 512], in0=po, in1=gt2B[:, cb * 512:(cb + 1) * 512], op=ALU.mult),
                     reads=[pko, 'gt2B'], writes=[('ot', b, cb)])
            S.op('dve', lambda: V.tensor_tensor(out=ot[b], in0=ot[b], in1=x1t[b], op=ALU.add), reads=[('ot', b, 0), ('ot', b, 1), ('x1t', b)], writes=[('ot', b, 0), ('ot', b, 1)])
            S.dma('sp', out_d[s, sl, :], ot[b], reads=[('ot', b, 0), ('ot', b, 1)], writes=[('outd', i)])

    def dbg_dump(src_ap, shape, key_reads=()):
        AR.seek(AR_TOP)
        t = AR.alloc(shape, F32)
        S.op('dve', lambda: V.tensor_copy(out=t, in_=src_ap), writes=['dbgt'])
        flat = t if len(shape) == 2 else t.rearrange("p a b -> p (a b)")
        S.dma('sp', dbg_d, flat, reads=['dbgt'])

    AR_TOP = 160 * 1024
    phase_adaln()
    for s in range(nseq):
        AR.seek(0)
        zsT = AR.alloc([128, 15, T], BF16)
        uT = AR.alloc([128, 8, T], BF16)
        base1 = AR.ptr
        phase_norm1(s, uT, base1)
        S.barrier()
        if dbg and dbg[0] == 'uT':
            dbg_dump(uT[:, :, 0:512], [128, 8, 512]); break
        for q in range(4):
            S.dma('sp', u_d[:, 2 * q:2 * q + 2, :], uT[:, 2 * q:2 * q + 2, :], reads=[('uT', 0), ('uT', 1), ('uT', 2), ('uT', 3)], writes=['uscr'])
        phase_rwkv_cols(uT, zsT, base1)
        S.barrier()
        if dbg and dbg[0] == 'zs':
            dbg_dump(zsT[:, :, 0:256], [128, 15, 256]); break
        AR.seek(61440)
        kkT = AR.alloc([128, 4, T], BF16)
        yaT = AR.alloc([128, 4, T], BF16)
        base3 = AR.ptr
        phase_scan(zsT, kkT, base3)
        if dbg and dbg[0] == 'yscan':
            AR.seek(AR_TOP)
            t = AR.alloc([128, 2, 512], F32)
            S.dma('sp', t[:, 0, :], y_d[0, 0:128, :], writes=['dbgt'])
            S.dma('sp', t[:, 1, :], y_d[1, 0:128, :], writes=['dbgt'])
            S.dma('sp', dbg_d, t.rearrange("p a b -> p (a b)"), reads=['dbgt']); break
        phase_post(zsT, yaT, base3)
        S.barrier()
        if dbg and dbg[0] == 'yaT':
            dbg_dump(yaT[:, :, 0:512], [128, 4, 512]); break
        AR.seek(0)
        uT = AR.alloc([128, 8, T], BF16)
        AR.seek(94208)
        ybT = AR.alloc([128, 4, T], BF16)
        baseB = AR.ptr
        for q in range(4):
            S.dma('sp' if q % 2 == 0 else 'act', uT[:, 2 * q:2 * q + 2, :], u_d[:, 2 * q:2 * q + 2, :], writes=[('uT', 0), ('uT', 1), ('uT', 2), ('uT', 3)])
        phase_attn(s, uT, ybT, 32768, baseB)
        S.barrier()
        if dbg and dbg[0] == 'ybT':
            dbg_dump(ybT[:, :, 0:512], [128, 4, 512]); break
        AR.seek(32768)
        mergedT = AR.alloc([128, 8, T], BF16)
        phase_merge(uT, yaT, ybT, mergedT, (65536, baseB))
        S.barrier()
        if dbg and dbg[0] == 'merged':
            dbg_dump(mergedT[:, :, 0:512], [128, 8, 512]); break
        AR.seek(0)
        u2tm = AR.alloc([128, NT, D], BF16)
        phase_x1(s, mergedT, u2tm, 65536)
        S.barrier()
        if dbg and dbg[0] == 'aff':
            dbg_dump(afftm.rearrange("p i e -> p (i e)"), [128, 256]); break
        phase_moe(s, u2tm, 32768)
        S.barrier()

    S.finish('sp')
    print("ninstr", S.ninstr, "pe_incs", S.npe_inc, "arena hi", AR.hi)
    return nc


def _consts():
    cm = np.zeros((13, 128, 128), np.float32)
    p = np.arange(128)
    cm[0] = (p[:, None] // 64 == p[None, :] // 64).astype(np.float32)
    R = np.zeros((128, 128), np.float32)
    for blk in range(2):
        o = blk * 64
        for d_ in range(8):
            R[o + d_ + 8, o + d_] = -1.0
            R[o + d_, o + d_ + 8] = 1.0
    cm[1] = R
    cm[2] = (p[:, None] >= p[None, :]).astype(np.float32)
    cm[3] = (p[:, None] <= p[None, :]).astype(np.float32)
    s_ = (p % 64)[:, None]
    t_ = (p % 64)[None, :]
    a_col = (p[None, :] >= 64)
    fwd = np.where(a_col, s_ < t_, s_ <= t_)
    bwd = np.where(a_col, s_ > t_, s_ >= t_)
    cm[4] = fwd.astype(np.float32)
    cm[5] = bwd.astype(np.float32)
    cm[6] = cm[4].T
    cm[7] = cm[5].T
    cm[8][:, 0] = (p < 64)
    cm[8][:, 1] = (p >= 64)
    cm[9][:, 0:64] = 1.0
    cm[10][:, 64:128] = 1.0
    cm[11] = ((p % 64)[:, None] < (p % 64)[None, :]).astype(np.float32)
    cm[12] = ((p % 64)[:, None] > (p % 64)[None, :]).astype(np.float32)
    return np.ascontiguousarray(cm.transpose(1, 0, 2).reshape(128, 13 * 128))


def _prep_shared(inp):
    f = lambda a: np.ascontiguousarray(np.asarray(a, dtype=np.float32))
    L = 0
    w_in = f(inp["w_in"][L]).copy()
    qoff = 1920
    perm = []
    for c in range(4):
        perm += list(range(c * 64, (c + 1) * 64)) + list(range((4 + c) * 64, (5 + c) * 64))
    perm = np.array(perm)
    w_in[:, qoff:qoff + 512] = w_in[:, qoff:qoff + 512][:, perm]
    p_attn = f(inp["p_attn"][L])[perm, :]
    pp = np.zeros((128, NPP), np.float32)

    def put(name, arr):
        o, w = PP[name]
        pp[:, o:o + w] = arr

    chunked = lambda v: np.asarray(v, np.float32).reshape(-1, 128).T
    put("mp", chunked(inp["mu_prev"][L]))
    put("mn", chunked(inp["mu_next"][L]))
    put("w0", np.concatenate([chunked(inp["rwkv_w0"][L][0]), chunked(inp["rwkv_w0"][L][1])], 1))
    put("a0", np.concatenate([chunked(inp["rwkv_a0"][L][0]), chunked(inp["rwkv_a0"][L][1])], 1))
    put("kk", chunked(inp["rwkv_k_k"][L]))
    put("ka", chunked(inp["rwkv_k_a"][L]))
    put("rk", chunked(np.asarray(inp["rwkv_r_k"][L]).reshape(-1)))
    put("qg", np.tile(np.asarray(inp["q_norm_g"][L], np.float32), 2)[:, None])
    put("kg", np.tile(np.asarray(inp["k_norm_g"][L], np.float32), 2)[:, None])
    inv_freq = (500000.0 ** (-np.arange(0, 16, 2, dtype=np.float32) / 16)).astype(np.float32)
    invf = np.zeros(64, np.float32)
    invf[0:8] = inv_freq
    invf[8:16] = inv_freq
    put("invf", np.tile(invf, 2)[:, None])
    sink = np.asarray(inp["attn_sink"][L], np.float32)
    sk = np.zeros((128, 4), np.float32)
    for j in range(4):
        sk[0:64, j] = sink[j]
        sk[64:128, j] = sink[4 + j]
    put("sink", sk)
    w2cat = np.zeros((128, 2, 512), np.float32)
    a2cat = np.zeros((128, 2, 512), np.float32)
    for d_ in range(2):
        w2cat[d_ * 64:(d_ + 1) * 64, d_, :] = inp["rwkv_w2"][L][d_]
        a2cat[d_ * 64:(d_ + 1) * 64, d_, :] = inp["rwkv_a2"][L][d_]
    return {
        "w_ada": f(inp["w_ada"][L]), "b_ada": f(inp["b_ada"][L])[None, :] if np.asarray(inp["b_ada"][L]).ndim == 1 else f(inp["b_ada"][L]),
        "norm1_g": f(inp["norm1_g"][L]).reshape(1, D), "norm2_g": f(inp["norm2_g"][L]).reshape(1, D),
        "w_in": w_in, "pp": pp, "w2cat": w2cat.reshape(128, 1024), "a2cat": a2cat.reshape(128, 1024),
        "g2": f(inp["rwkv_g2"][L]), "gn_w": f(inp["rwkv_gn_w"][L]).reshape(1, 512), "gn_b": f(inp["rwkv_gn_b"][L]).reshape(1, 512),
        "p_rwkv": f(inp["p_rwkv"][L]), "p_attn": np.ascontiguousarray(p_attn), "w_out": f(inp["w_out"][L]),
        "w_router": f(inp["w_router"][L]), "w_gate": f(inp["w_gate"][L]), "w_up": f(inp["w_up"][L]), "w_down": f(inp["w_down"][L]),
        "cmats": _consts(),
    }


def _core_inputs(inp, shared, seqs):
    x = np.ascontiguousarray(np.asarray(inp["x"], np.float32)[seqs])
    c = np.asarray(inp["c"], np.float32)[seqs]
    cT = np.ascontiguousarray(c.reshape(len(seqs), 8, 128).transpose(0, 2, 1))
    pos = np.ascontiguousarray(np.asarray(inp["positions"]).astype(np.int32)[seqs][:, None, :])
    m = dict(shared)
    m.update({"x": x, "cT": cT, "pos": pos})
    return m


def kernel(**inputs):
    shared = _prep_shared(inputs)
    nc = build(NSEQ)
    in_maps = [_core_inputs(inputs, shared, list(range(i * NSEQ, (i + 1) * NSEQ))) for i in range(NCORES)]
    res = run_bass_kernel_spmd(nc, in_maps, core_ids=list(range(NCORES)))
    out = np.concatenate([np.asarray(r["out"]) for r in res.results], axis=0)
    return out.astype(np.float32)
```

```python
import numpy as np
import concourse.bass as bass
import concourse.mybir as mybir
from concourse.bass_utils import run_bass_kernel_spmd

F32 = mybir.dt.float32
BF16 = mybir.dt.bfloat16
I32 = mybir.dt.int32
ALU = mybir.AluOpType
AF = mybir.ActivationFunctionType
AX = mybir.AxisListType

T = 2048
D = 1024
NT = 16
NB = 4
NSEQ = 2
NCORES = 8
E = 16
CAP = 256
LAM = float(np.exp(-0.5))
NCH = 4
TBS = NCH * 64
NTB = T // TBS
TWO_PI = float(2 * np.pi)
C1 = 6.28125
C2 = TWO_PI - C1

PP = {}
_o = 0
for _n, _w in [("mp", 15), ("mn", 15), ("w0", 8), ("a0", 8), ("kk", 4), ("ka", 4), ("rk", 4), ("qg", 1), ("kg", 1),
               ("invf", 1), ("sink", 4)]:
    PP[_n] = (_o, _w)
    _o += _w
NPP = _o


class Ticket:
    __slots__ = ('ins', 'sem', 'val', 'parent')

    def __init__(self, ins):
        self.ins = ins
        self.sem = None
        self.val = None
        self.parent = None

    def root(self):
        t = self
        while t.parent is not None:
            t = t.parent
        return t


class Sync:
    SEM_MAX = 30000

    def __init__(self, nc):
        self.nc = nc
        self.E = {'pe': nc.tensor, 'act': nc.scalar, 'dve': nc.vector, 'pool': nc.gpsimd, 'sp': nc.sync}
        self.sem = {}
        self.cnt = {}
        self.nsem = 0
        for e in self.E:
            self._newsem(e)
        self.waited = {}
        self.lastw = {}
        self.reads = {}
        self.dma_sems = {}
        self.dma_rr = {}
        self.ninstr = 0
        self.pend = None
        self.pend_writes = None
        self.npe_inc = 0

    def _newsem(self, e):
        self.sem[e] = self.nc.alloc_semaphore(f"s_{e}_{self.nsem}")
        self.nsem += 1
        self.cnt[e] = 0

    def _flush_pe(self):
        t = self.pend
        if t is None:
            return
        if self.cnt['pe'] >= self.SEM_MAX:
            self._newsem('pe')
        self.cnt['pe'] += 1
        t.sem = self.sem['pe']
        t.val = self.cnt['pe']
        t.ins.then_inc(t.sem, 1)
        self.npe_inc += 1
        self.pend = None
        self.pend_writes = None

    def _wait(self, e, ev):
        if ev is None:
            return
        if isinstance(ev, Ticket):
            if e == 'pe':
                return
            t = ev.root()
            if t.val is None:
                assert t is self.pend
                self._flush_pe()
            sem, val = t.sem, t.val
        else:
            src, sem, val = ev
        k = (e, sem.name)
        if self.waited.get(k, 0) >= val:
            return
        self.waited[k] = val
        self.E[e].wait_ge(sem, val)

    def deps(self, e, reads, writes, pe_acc=False):
        for k in reads:
            self._wait(e, self.lastw.get(k))
        for k in writes:
            lw = self.lastw.get(k)
            if not (pe_acc and isinstance(lw, Ticket)):
                self._wait(e, lw)
            for ev in self.reads.get(k, {}).values():
                self._wait(e, ev)

    def commit(self, src, ev, reads, writes):
        for k in reads:
            self.reads.setdefault(k, {})[src] = ev
        for k in writes:
            self.lastw[k] = ev
            self.reads[k] = {}

    def op(self, e, fn, reads=(), writes=(), pe_acc=False):
        self.deps(e, reads, writes, pe_acc)
        if e == 'pe':
            ins = fn()
            t = Ticket(ins)
            if self.pend is not None:
                if self.pend_writes == tuple(writes):
                    self.pend.parent = t
                    self.pend = None
                else:
                    self._flush_pe()
            self.pend = t
            self.pend_writes = tuple(writes)
            self.commit('pe', t, reads, writes)
            self.ninstr += 1
            return t
        if self.cnt[e] >= self.SEM_MAX:
            self._newsem(e)
        ins = fn()
        self.cnt[e] += 1
        ev = (e, self.sem[e], self.cnt[e])
        ins.then_inc(self.sem[e], 1)
        self.commit(e, ev, reads, writes)
        self.ninstr += 1
        return ev

    def dma(self, e, out, in_, reads=(), writes=(), nslots=8, **kw):
        if e == 'pool':
            nslots = 2
        lst = self.dma_sems.setdefault(e, [])
        if len(lst) < nslots:
            lst.append([self.nc.alloc_semaphore(f"d_{e}_{len(lst)}"), 0])
        i = self.dma_rr.get(e, 0)
        self.dma_rr[e] = (i + 1) % nslots
        slot = lst[i % len(lst)]
        sem, uses = slot
        if uses > 0:
            self._wait(e, ('dma', sem, 16 * uses))
        self.deps(e, reads, writes)
        self.E[e].dma_start(out=out, in_=in_, **kw).then_inc(sem, 16)
        slot[1] = uses + 1
        ev = ('dma_%s_%d' % (e, i % len(lst)), sem, 16 * (uses + 1))
        self.commit(ev[0], ev, reads, writes)
        self.ninstr += 1
        return ev

    def barrier(self):
        self._flush_pe()
        evs = [(e, self.sem[e], self.cnt[e]) for e in self.E if self.cnt[e] > 0]
        for q, lst in self.dma_sems.items():
            for sem, uses in lst:
                if uses:
                    evs.append(('dma', sem, 16 * uses))
        for e in self.E:
            for ev in evs:
                if ev[0] != e:
                    self._wait(e, ev)
        self.lastw = {}
        self.reads = {}

    def finish(self, e='sp'):
        self._flush_pe()
        for q, lst in self.dma_sems.items():
            for sem, uses in lst:
                if uses:
                    self._wait(e, ('dma', sem, 16 * uses))


class Arena:
    def __init__(self, nc, name, nbytes):
        self.n4 = nbytes // 4
        self.t = nc.alloc_sbuf_tensor(name, [128, self.n4], F32).ap()
        self.ptr = 0
        self.hi = 0

    def seek(self, off):
        self.ptr = off

    def alloc(self, shape, dtype, parts=None):
        esz = 4 if dtype in (F32, I32) else 2
        n = int(np.prod(shape[1:]))
        nb = (n * esz + 31) // 32 * 32
        assert self.ptr % 4 == 0
        a = self.ptr // 4
        assert a + nb // 4 <= self.n4, f"arena overflow {self.ptr}+{nb} > {self.n4 * 4}"
        v = self.t[:, a:a + nb // 4]
        if dtype != F32:
            v = v.bitcast(dtype)
        v = v[0:shape[0], 0:n]
        if len(shape) > 2:
            names = " ".join(f"d{i}" for i in range(len(shape) - 1))
            kw = {f"d{i}": int(shape[i + 1]) for i in range(len(shape) - 1)}
            v = v.rearrange(f"p ({names}) -> p {names}", **kw)
        self.ptr += nb
        self.hi = max(self.hi, self.ptr)
        return v


def bc(ap, shape):
    return ap.to_broadcast(list(shape))


def build(nseq=NSEQ, dbg=None, stop_after=None):
    nc = bass.Bass("TRN2", target_bir_lowering=False)
    S = Sync(nc)
    V, ACT, POOL, PE = nc.vector, nc.scalar, nc.gpsimd, nc.tensor

    def din(name, shape, dt=F32):
        return nc.dram_tensor(name, list(shape), dt, kind="ExternalInput").ap()

    x_d = din("x", [nseq, T, D])
    cT_d = din("cT", [nseq, 128, 8])
    pos_d = din("pos", [nseq, 1, T], I32)
    wada_d = din("w_ada", [D, 6 * D])
    bada_d = din("b_ada", [1, 6 * D])
    n1g_d = din("norm1_g", [1, D])
    n2g_d = din("norm2_g", [1, D])
    win_d = din("w_in", [D, 4736])
    pp_d = din("pp", [128, NPP])
    w2c_d = din("w2cat", [128, 2 * 512])
    a2c_d = din("a2cat", [128, 2 * 512])
    g2_d = din("g2", [128, 512])
    gnw_d = din("gn_w", [1, 512])
    gnb_d = din("gn_b", [1, 512])
    prw_d = din("p_rwkv", [512, D])
    pat_d = din("p_attn", [512, D])
    wout_d = din("w_out", [D, D])
    wr_d = din("w_router", [D, E])
    wg_d = din("w_gate", [E, D, D])
    wu_d = din("w_up", [E, D, D])
    wd_d = din("w_down", [E, D, D])
    cm_d = din("cmats", [128, 13 * 128])
    out_d = nc.dram_tensor("out", [nseq, T, D], F32, kind="ExternalOutput").ap()
    mod_d = nc.dram_tensor("modscr", [nseq, 6, 128, D], F32, kind="Internal").ap()
    y_d = nc.dram_tensor("yscr", [2, T, 512], F32, kind="Internal").ap()
    u_d = nc.dram_tensor("uscr", [128, 8, T], BF16, kind="Internal").ap()
    dbg_d = None
    if dbg is not None:
        dbg_d = nc.dram_tensor("dbg", list(dbg[1]), F32, kind="ExternalOutput").ap()

    def sb(name, shape, dt=F32):
        return nc.alloc_sbuf_tensor('sb_' + name, list(shape), dt).ap()

    pp = sb("pp", [128, NPP])
    ident = sb("ident", [128, 128], BF16)
    identf = sb("identf", [128, 128])
    cmb = sb("cmb", [128, 13, 128], BF16)
    w2c = sb("w2c", [128, 2, 512], BF16)
    a2c = sb("a2c", [128, 2, 512], BF16)
    g2 = sb("g2", [128, 512], BF16)
    wr = sb("wr", [128, 8, E], BF16)
    epsc = sb("epsc", [128, 4])
    alpha = sb("alpha", [128, 15])
    oneminus_ka = sb("omka", [128, 4])
    two_omka = sb("omka2", [128, 4])
    negkkc = sb("negone", [128, 1])
    esk = sb("esk", [128, 4])
    rmask = sb("rmask", [128, TBS])
    iota_row = sb("iota_row", [128, CAP])
    ident4 = sb("ident4", [128, 4, 128], BF16)
    kar = sb("kar", [128, 4])
    c2r = sb("c2r", [128, 4])
    afftm = sb("afftm", [128, NT, E])
    slot_tm = sb("slot_tm", [128, NT, E])
    affhl = sb("affhl", [128, NT, E, 2], BF16)

    BLK1, ROT, MPREV, MNEXT = 0, 1, 2, 3
    MZT = (4, 5)
    MZ = (6, 7)
    HSEL = 8
    VP = (9, 10)

    ps = [nc.alloc_psum_tensor(f"ps{i}", [128, 512], F32).ap() for i in range(8)]
    psk = [f"ps{i}" for i in range(8)]

    AR = Arena(nc, "arena", 192 * 1024)

    def col(name, j=0, n=1):
        o, w = PP[name]
        return pp[:, o + j:o + j + n]

    S.dma('sp', pp, pp_d, writes=['pp'])
    S.dma('pool', cmb.rearrange("p a b -> p (a b)"), cm_d, writes=['cmb'])
    S.dma('pool', w2c.rearrange("p a b -> p (a b)"), w2c_d, writes=['w2c'])
    S.dma('pool', a2c.rearrange("p a b -> p (a b)"), a2c_d, writes=['a2c'])
    S.dma('pool', g2, g2_d, writes=['g2'])
    S.dma('pool', wr, wr_d.rearrange("(k p) e -> p k e", p=128), writes=['wr'])
    S.op('pool', lambda: POOL.memset(identf, 1.0), writes=['identf'])
    S.op('pool', lambda: POOL.affine_select(out=identf, in_=identf, pattern=[[1, 128]], compare_op=ALU.is_equal,
                                            fill=0.0, base=0, channel_multiplier=-1), reads=['identf'], writes=['identf'])
    S.op('dve', lambda: V.tensor_copy(out=ident, in_=identf), reads=['identf'], writes=['ident'])
    for j in range(4):
        S.op('dve', lambda: V.tensor_copy(out=ident4[:, j, :], in_=identf), reads=['identf'], writes=['ident4'])
    S.op('pool', lambda: POOL.memset(epsc[:, 0:1], 1e-6), writes=['epsc'])
    S.op('pool', lambda: POOL.memset(epsc[:, 1:2], 64e-5), reads=['epsc'], writes=['epsc'])
    S.op('pool', lambda: POOL.memset(epsc[:, 2:3], 1e-24), reads=['epsc'], writes=['epsc'])
    S.op('pool', lambda: POOL.memset(epsc[:, 3:4], 0.0), reads=['epsc'], writes=['epsc'])
    S.op('pool', lambda: POOL.memset(negkkc, -1.0), writes=['negone'])
    S.op('dve', lambda: V.tensor_tensor(out=alpha, in0=col("mp", 0, 15), in1=col("mn", 0, 15), op=ALU.add), reads=['pp'], writes=['alpha'])
    S.op('dve', lambda: V.tensor_scalar(out=alpha, in0=alpha, scalar1=-1.0, scalar2=1.0, op0=ALU.mult, op1=ALU.add), reads=['alpha'], writes=['alpha'])
    S.op('dve', lambda: V.tensor_scalar(out=oneminus_ka, in0=col("ka", 0, 4), scalar1=-1.0, scalar2=1.0, op0=ALU.mult, op1=ALU.add), reads=['pp'], writes=['omka'])
    S.op('dve', lambda: V.tensor_scalar(out=two_omka, in0=col("ka", 0, 4), scalar1=-2.0, scalar2=2.0, op0=ALU.mult, op1=ALU.add), reads=['pp'], writes=['omka2'])
    S.op('act', lambda: ACT.activation(out=esk, in_=col("sink", 0, 4), func=AF.Exp), reads=['pp'], writes=['esk'])
    S.op('dve', lambda: V.tensor_tensor(out=kar, in0=col("ka", 0, 4), in1=col("rk", 0, 4), op=ALU.mult), reads=['pp'], writes=['kar'])
    S.op('dve', lambda: V.tensor_tensor(out=c2r, in0=two_omka, in1=col("rk", 0, 4), op=ALU.mult), reads=['pp', 'omka2'], writes=['kar'])
    S.op('pool', lambda: POOL.memset(rmask, 1.0), writes=['rmask'])
    S.op('pool', lambda: POOL.memset(rmask.rearrange("p (c t) -> p c t", t=64)[:, :, 0:1], 0.0), reads=['rmask'], writes=['rmask'])
    S.op('pool', lambda: POOL.iota(iota_row, pattern=[[1, CAP]], base=0, channel_multiplier=0, allow_small_or_imprecise_dtypes=True), writes=['iota_row'])

    def debug_out(ap_sb, key, rows=None):
        S.dma('sp', dbg_d if rows is None else rows, ap_sb, reads=[key])

    def phase_adaln():
        AR.seek(0)
        csil = [AR.alloc([128, 8], F32) for _ in range(nseq)]
        crep = [AR.alloc([128, 9, 128], F32) for _ in range(nseq)]
        wblk = [AR.alloc([128, 9, 512], F32) for _ in range(3)]
        g1B = AR.alloc([128, D], F32)
        g2B = AR.alloc([128, D], F32)
        mt = [AR.alloc([128, 512], F32) for _ in range(4)]
        S.dma('sp', g1B, n1g_d.partition_broadcast(128), writes=['g1B'])
        S.dma('sp', g2B, n2g_d.partition_broadcast(128), writes=['g2B'])
        for b in range(3):
            S.op('pool', lambda: POOL.memset(wblk[b][:, 8, :], 0.0), writes=[('wblk', b)])
        for s in range(nseq):
            S.dma('sp', csil[s], cT_d[s], writes=[('csil', s)])
            S.op('act', lambda: ACT.activation(out=csil[s], in_=csil[s], func=AF.Silu), reads=[('csil', s)], writes=[('csil', s)])
            S.op('pool', lambda: POOL.memset(crep[s][:, 8, :], 0.0), writes=[('crep', s)])
            S.op('pool', lambda: POOL.memset(crep[s][0:1, 8, :], 1.0), reads=[('crep', s)], writes=[('crep', s)])
            S.op('dve', lambda: V.tensor_copy(out=crep[s][:, 0:8, :], in_=bc(csil[s].rearrange("p (k o) -> p k o", o=1), [128, 8, 128])),
                 reads=[('csil', s)], writes=[('crep', s)])
        ev = 0
        for jb in range(12):
            b = jb % 3
            piece = jb // 2
            c0 = jb * 512
            S.dma('sp', wblk[b][:, 0:4, :], wada_d[0:512, c0:c0 + 512].rearrange("(k p) n -> p k n", p=128), writes=[('wblk', b)])
            S.dma('act', wblk[b][:, 4:8, :], wada_d[512:1024, c0:c0 + 512].rearrange("(k p) n -> p k n", p=128), writes=[('wblk', b)])
            S.dma('sp', wblk[b][0:1, 8, :], bada_d[:, c0:c0 + 512], writes=[('wblk', b)])
            for s in range(nseq):
                pz, pkz = ps[ev % 4], psk[ev % 4]
                for k in range(9):
                    S.op('pe', lambda: PE.matmul(pz, lhsT=crep[s][:, k, :], rhs=wblk[b][:, k, :], start=(k == 0), stop=(k == 8)),
                         reads=[('crep', s), ('wblk', b)], writes=[pkz], pe_acc=True)
                m = mt[ev % 4]
                lc = (jb % 2) * 512
                if piece == 1:
                    S.op('dve', lambda: V.scalar_tensor_tensor(out=m, in0=pz, scalar=1.0, in1=g1B[:, lc:lc + 512], op0=ALU.add, op1=ALU.mult),
                         reads=[pkz, 'g1B'], writes=[('mt', ev % 4)])
                elif piece == 4:
                    S.op('dve', lambda: V.scalar_tensor_tensor(out=m, in0=pz, scalar=1.0, in1=g2B[:, lc:lc + 512], op0=ALU.add, op1=ALU.mult),
                         reads=[pkz, 'g2B'], writes=[('mt', ev % 4)])
                else:
                    S.op('act', lambda: ACT.copy(out=m, in_=pz), reads=[pkz], writes=[('mt', ev % 4)])
                S.dma('sp', mod_d[s, piece, :, lc:lc + 512], m, reads=[('mt', ev % 4)], writes=[('mod', s, piece)])
                ev += 1
        S.barrier()

    def phase_norm1(s, uT, base):
        AR.seek(base)
        scp = AR.alloc([128, D], F32)
        shp = AR.alloc([128, D], F32)
        xt = [AR.alloc([128, D], F32) for _ in range(2)]
        tmp2 = [AR.alloc([128, D], F32) for _ in range(2)]
        ub = [AR.alloc([128, D], BF16) for _ in range(2)]
        junk2 = [AR.alloc([128, D], BF16) for _ in range(2)]
        ss2 = [AR.alloc([128, 2], F32) for _ in range(2)]
        S.dma('sp', scp, mod_d[s, 1], reads=[('mod', s, 1)], writes=['scp'])
        S.dma('sp', shp, mod_d[s, 0], reads=[('mod', s, 0)], writes=['shp'])
        for i in range(NT):
            b = i % 2
            S.dma('sp', xt[b], x_d[s, i * 128:(i + 1) * 128, :], writes=[('xt', b)])
            tmp, junk, ss = tmp2[b], junk2[b], ss2[b]
            S.op('act', lambda: ACT.activation(out=junk, in_=xt[b], func=AF.Square, accum_out=ss[:, 0:1]), reads=[('xt', b)], writes=[('junk', b), ('ss', b)])
            S.op('act', lambda: ACT.activation(out=ss[:, 1:2], in_=ss[:, 0:1], func=AF.Sqrt, bias=epsc[:, 0:1], scale=1.0 / D), reads=[('ss', b), 'epsc'], writes=[('ss1', b)])
            S.op('dve', lambda: V.reciprocal(out=ss[:, 1:2], in_=ss[:, 1:2]), reads=[('ss1', b)], writes=[('ss1', b)])
            S.op('dve', lambda: V.scalar_tensor_tensor(out=tmp, in0=xt[b], scalar=ss[:, 1:2], in1=scp, op0=ALU.mult, op1=ALU.mult),
                 reads=[('xt', b), ('ss1', b), 'scp'], writes=[('tmp', b)])
            S.op('pool', lambda: POOL.tensor_tensor(out=ub[b], in0=tmp, in1=shp, op=ALU.add), reads=[('tmp', b), 'shp'], writes=[('ub', b)])
            pz = ps[i % 2].bitcast(BF16).rearrange("p (k t) -> p k t", k=8)
            for k in range(8):
                S.op('pe', lambda: PE.transpose(out=pz[:, k, :], in_=ub[b][:, k * 128:(k + 1) * 128], identity=ident),
                     reads=[('ub', b), 'ident'], writes=[psk[i % 2]], pe_acc=True)
            S.op('act', lambda: ACT.copy(out=uT[:, :, i * 128:(i + 1) * 128], in_=pz), reads=[psk[i % 2]], writes=[('uT', i // 4)])

    def phase_rwkv_cols(uT, zsT, base):
        AR.seek(base)
        wg = [AR.alloc([128, 8, 128], BF16) for _ in range(2)]
        ztmpP = [AR.alloc([128, T + 2], F32) for _ in range(2)]
        shtP = [AR.alloc([128, T], F32) for _ in range(2)]
        for q in range(2):
            S.op('pool', lambda: POOL.memset(ztmpP[q][:, 0:1], 0.0), writes=[('ztmp', q)])
            S.op('pool', lambda: POOL.memset(ztmpP[q][:, T + 1:T + 2], 0.0), reads=[('ztmp', q)], writes=[('ztmp', q)])
        for j in range(15):
            b = j % 2
            ztmp, sht = ztmpP[b], shtP[b]
            S.dma('pool', wg[b], win_d[:, j * 128:(j + 1) * 128].rearrange("(k p) n -> p k n", p=128), writes=[('wg', b)])
            for tb in range(NB):
                pz = ps[(j * NB + tb) % 4]
                pk = psk[(j * NB + tb) % 4]
                for k in range(8):
                    S.op('pe', lambda: PE.matmul(pz, lhsT=wg[b][:, k, :], rhs=uT[:, k, tb * 512:(tb + 1) * 512], start=(k == 0), stop=(k == 7)),
                         reads=[('wg', b), ('uT', tb)], writes=[pk], pe_acc=True)
                S.op('act', lambda: ACT.copy(out=ztmp[:, 1 + tb * 512:1 + (tb + 1) * 512], in_=pz), reads=[pk], writes=[('ztmp', b)])
            S.op('dve', lambda: V.tensor_scalar(out=sht, in0=ztmp[:, 1:T + 1], scalar1=alpha[:, j:j + 1], scalar2=None, op0=ALU.mult),
                 reads=[('ztmp', b), 'alpha'], writes=[('sht', b)])
            S.op('dve', lambda: V.scalar_tensor_tensor(out=sht, in0=ztmp[:, 0:T], scalar=col("mp", j), in1=sht, op0=ALU.mult, op1=ALU.add),
                 reads=[('ztmp', b), ('sht', b), 'pp'], writes=[('sht', b)])
            S.op('dve', lambda: V.scalar_tensor_tensor(out=zsT[:, j, :], in0=ztmp[:, 2:T + 2], scalar=col("mn", j), in1=sht, op0=ALU.mult, op1=ALU.add),
                 reads=[('ztmp', b), ('sht', b), 'pp'], writes=[('zs', j)])
            if j == 12:
                S.op('act', lambda: ACT.activation(out=zsT[:, j, :], in_=zsT[:, j, :], func=AF.Tanh), reads=[('zs', j)], writes=[('zs', j)])
            if j == 14:
                S.op('act', lambda: ACT.activation(out=zsT[:, j, :], in_=zsT[:, j, :], func=AF.Sigmoid), reads=[('zs', j)], writes=[('zs', j)])

    def phase_scan(zsT, kkT, base):
        rT = lambda c: zsT[:, c, :]
        kT = lambda c: zsT[:, 4 + c, :]
        vT = lambda c: zsT[:, 8 + c, :]
        wdT = zsT[:, 12, :]
        adT = zsT[:, 13, :]
        AR.seek(base)
        kraw = AR.alloc([128, 512], F32)
        ksq = AR.alloc([128, 512], BF16)
        krs = AR.alloc([128, 512], F32)
        for c in range(4):
            for tb in range(NB):
                sl = slice(tb * 512, (tb + 1) * 512)
                S.op('dve', lambda: V.tensor_scalar(out=kraw, in0=kT(c)[:, sl], scalar1=col("kk", c), scalar2=None, op0=ALU.mult), reads=[('zs', 4 + c), 'pp'], writes=['kraw'])
                S.op('act', lambda: ACT.activation(out=ksq, in_=kraw, func=AF.Square), reads=['kraw'], writes=['ksq'])
                pz, pk = ps[tb % 2], psk[tb % 2]
                S.op('pe', lambda: PE.matmul(pz, lhsT=cmb[:, BLK1, :], rhs=ksq, start=True, stop=True), reads=['ksq', 'cmb'], writes=[pk], pe_acc=True)
                S.op('act', lambda: ACT.activation(out=krs, in_=pz, func=AF.Sqrt, bias=epsc[:, 2:3], scale=1.0), reads=[pk, 'epsc'], writes=['krs'])
                S.op('dve', lambda: V.reciprocal(out=krs, in_=krs), reads=['krs'], writes=['krs'])
                S.op('dve', lambda: V.tensor_tensor(out=kkT[:, c, sl], in0=kraw, in1=krs, op=ALU.mult), reads=['kraw', 'krs'], writes=[('kk', c)])
        S.barrier()
        AR.seek(base)
        sg = AR.alloc([128, 4, TBS], F32)
        ad = AR.alloc([128, 4, TBS], F32)
        cc = AR.alloc([128, 4, TBS], F32)
        t1 = AR.alloc([128, 4, TBS], F32)
        ex = [[AR.alloc([128, TBS], F32) for _ in range(2)] for _ in range(4)]
        kd = AR.alloc([128, 4, TBS], F32)
        bb = AR.alloc([128, 4, TBS], F32)
        pdec = AR.alloc([128, 4, NCH], F32)
        ARz = AR.alloc([128, 4, NCH, 2, 2, 64], BF16)
        Bz = AR.alloc([128, 4, NCH, 2, 64], BF16)
        BKt = AR.alloc([128, 4, NCH, 2, 64], BF16)
        KBh = AR.alloc([128, 4, NCH, 2, 64], BF16)
        KBt = AR.alloc([128, 4, NCH, 128], BF16)
        VZ = AR.alloc([128, NCH, 8, 64], BF16)
        XV = AR.alloc([128, NCH, 8, 64], BF16)
        ZTs = [[AR.alloc([128, 4, 128], BF16) for _ in range(2)] for _ in range(NCH)]
        ATm = [[AR.alloc([128, 4, 128], BF16) for _ in range(2)] for _ in range(NCH)]
        PTm = [[AR.alloc([128, 4, 128], BF16) for _ in range(2)] for _ in range(2)]
        Pm = [[AR.alloc([128, 4, 128], BF16) for _ in range(2)] for _ in range(2)]
        Am = [[AR.alloc([128, 4, 128], BF16) for _ in range(2)] for _ in range(2)]
        W1s = AR.alloc([128, 4, 64], BF16)
        S32 = [AR.alloc([128, 4, 64], F32) for _ in range(2)]
        Sb = [AR.alloc([128, 4, 64], BF16) for _ in range(2)]
        ysb = [AR.alloc([64, 512], F32) for _ in range(2)]
        S.op('pool', lambda: POOL.memset(ARz.rearrange("p a b c d e -> p (a b c d e)"), 0.0), writes=['ARz'])
        S.op('pool', lambda: POOL.memset(Bz.rearrange("p a b c d -> p (a b c d)"), 0.0), writes=['Bz'])
        S.op('pool', lambda: POOL.memset(VZ.rearrange("p a b c -> p (a b c)"), 0.0), writes=['VZ'])

        def chain(gens):
            for g_ in gens:
                yield from g_

        def run_tasks(tasks):
            tasks = list(tasks)
            while tasks:
                for t_ in list(tasks):
                    try:
                        next(t_)
                    except StopIteration:
                        tasks.remove(t_)

        yev = 0
        pendQ = None
        for d in range(2):
            S.op('pool', lambda: POOL.memset(S32[d].rearrange("p a b -> p (a b)"), 0.0), writes=[('S32', d)])
            S.op('pool', lambda: POOL.memset(Sb[d].rearrange("p a b -> p (a b)"), 0.0), writes=[('Sb', d)])
            tbs = range(NTB) if d == 0 else range(NTB - 1, -1, -1)
            for tb in tbs:
                sl = slice(tb * TBS, (tb + 1) * TBS)
                def gen_prep(c):
                    pz, pk = ps[c % 2], psk[c % 2]
                    S.op('pe', lambda: PE.matmul(pz[:, 0:TBS], lhsT=w2c[:, d, c * 128:(c + 1) * 128], rhs=wdT[:, sl], start=True, stop=True),
                         reads=['w2c', ('zs', 12)], writes=[pk], pe_acc=True)
                    S.op('act', lambda: ACT.activation(out=sg[:, c, :], in_=pz[:, 0:TBS], func=AF.Sigmoid, bias=col("w0", d * 4 + c), scale=1.0),
                         reads=[pk, 'pp'], writes=[('sg', c)])
                    pz2, pk2 = ps[2 + c % 2], psk[2 + c % 2]
                    S.op('pe', lambda: PE.matmul(pz2[:, 0:TBS], lhsT=a2c[:, d, c * 128:(c + 1) * 128], rhs=adT[:, sl], start=True, stop=True),
                         reads=['a2c', ('zs', 13)], writes=[pk2], pe_acc=True)
                    S.op('act', lambda: ACT.activation(out=ad[:, c, :], in_=pz2[:, 0:TBS], func=AF.Sigmoid, bias=col("a0", d * 4 + c), scale=1.0),
                         reads=[pk2, 'pp'], writes=[('ad', c)])
                    yield
                    S.op('dve', lambda: V.tensor_tensor_scan(out=cc[:, c, :], data0=rmask, data1=sg[:, c, :], initial=0.0, op0=ALU.mult, op1=ALU.add),
                         reads=['rmask', ('sg', c)], writes=[('cc', c)])
                    cc3 = cc[:, c, :].rearrange("p (h t) -> p h t", t=64)
                    sg3 = sg[:, c, :].rearrange("p (h t) -> p h t", t=64)
                    t13 = t1[:, c, :].rearrange("p (h t) -> p h t", t=64)
                    if d == 1:
                        S.op('dve', lambda: V.tensor_tensor(out=t13, in0=bc(cc3[:, :, 63:64], [128, NCH, 64]), in1=cc3, op=ALU.subtract),
                             reads=[('cc', c)], writes=[('t1', c)])
                        S.op('dve', lambda: V.tensor_tensor(out=cc[:, c, :], in0=t1[:, c, :], in1=sg[:, c, :], op=ALU.add),
                             reads=[('t1', c), ('sg', c)], writes=[('cc', c)])
                    totp = 63 if d == 0 else 0
                    S.op('pool', lambda: POOL.tensor_scalar(out=kd[:, c, :], in0=ad[:, c, :], scalar1=col("ka", c), scalar2=oneminus_ka[:, c:c + 1], op0=ALU.mult, op1=ALU.add),
                         reads=[('ad', c), 'pp', 'omka'], writes=[('kd', c)])
                    S.op('pool', lambda: POOL.tensor_tensor(out=kd[:, c, :], in0=kd[:, c, :], in1=kT(c)[:, sl], op=ALU.mult),
                         reads=[('kd', c), ('zs', 4 + c)], writes=[('kd', c)])
                    S.op('pool', lambda: POOL.tensor_tensor(out=bb[:, c, :], in0=ad[:, c, :], in1=kkT[:, c, sl], op=ALU.mult),
                         reads=[('ad', c), ('kk', c)], writes=[('bb', c)])
                    yield
                    e = ex[c][0]
                    S.op('act', lambda: ACT.activation(out=e, in_=cc[:, c, :], func=AF.Exp, scale=-LAM), reads=[('cc', c)], writes=[('ex', c, 0)])
                    for hp in range(2):
                        pr = slice(hp * 64, (hp + 1) * 64)
                        S.op('dve', lambda: V.tensor_tensor(out=ARz[pr, c, :, 0, hp, :], in0=rT(c)[pr, sl].rearrange("p (h t) -> p h t", t=64),
                                                            in1=e[pr, :].rearrange("p (h t) -> p h t", t=64), op=ALU.mult),
                             reads=[('zs', c), ('ex', c, 0)], writes=['ARz'])
                    yield
                    e = ex[c][1]
                    S.op('act', lambda: ACT.activation(out=e, in_=cc[:, c, :], func=AF.Exp, scale=LAM), reads=[('cc', c)], writes=[('ex', c, 1)])
                    S.op('dve', lambda: V.tensor_tensor(out=BKt[:, c, :, 0, :], in0=kd[:, c, :].rearrange("p (h t) -> p h t", t=64),
                                                        in1=e.rearrange("p (h t) -> p h t", t=64), op=ALU.mult),
                         reads=[('kd', c), ('ex', c, 1)], writes=['BKt'])
                    S.op('dve', lambda: V.tensor_tensor(out=BKt[:, c, :, 1, :], in0=bb[:, c, :].rearrange("p (h t) -> p h t", t=64),
                                                        in1=e.rearrange("p (h t) -> p h t", t=64), op=ALU.mult),
                         reads=[('bb', c), ('ex', c, 1)], writes=['BKt'])
                    for hp in range(2):
                        pr = slice(hp * 64, (hp + 1) * 64)
                        S.op('act', lambda: ACT.copy(out=Bz[pr, c, :, hp, :], in_=BKt[pr, c, :, 1, :]), reads=['BKt'], writes=['Bz'])
                    yield
                    S.op('dve', lambda: V.tensor_tensor(out=t1[:, c, :], in0=cc[:, c, :], in1=sg[:, c, :], op=ALU.subtract),
                         reads=[('cc', c), ('sg', c)], writes=[('t1', c)])
                    e = ex[c][0]
                    S.op('act', lambda: ACT.activation(out=e, in_=t1[:, c, :], func=AF.Exp, scale=-LAM), reads=[('t1', c)], writes=[('ex', c, 0)])
                    for hp in range(2):
                        pr = slice(hp * 64, (hp + 1) * 64)
                        S.op('dve', lambda: V.scalar_tensor_tensor(out=ARz[pr, c, :, 1, hp, :], in0=kkT[pr, c, sl].rearrange("p (h t) -> p h t", t=64),
                                                                   scalar=-1.0, in1=e[pr, :].rearrange("p (h t) -> p h t", t=64), op0=ALU.mult, op1=ALU.mult),
                             reads=[('kk', c), ('ex', c, 0)], writes=['ARz'])
                    yield
                    S.op('dve', lambda: V.tensor_tensor(out=t13, in0=bc(cc3[:, :, totp:totp + 1], [128, NCH, 64]), in1=cc3, op=ALU.subtract),
                         reads=[('cc', c)], writes=[('t1', c)])
                    e = ex[c][1]
                    S.op('act', lambda: ACT.activation(out=e, in_=t1[:, c, :], func=AF.Exp, scale=-LAM), reads=[('t1', c)], writes=[('ex', c, 1)])
                    S.op('pool', lambda: POOL.tensor_tensor(out=KBh[:, c, :, 0, :], in0=kd[:, c, :].rearrange("p (h t) -> p h t", t=64),
                                                        in1=e.rearrange("p (h t) -> p h t", t=64), op=ALU.mult),
                         reads=[('kd', c), ('ex', c, 1)], writes=['KBh'])
                    S.op('pool', lambda: POOL.tensor_tensor(out=KBh[:, c, :, 1, :], in0=bb[:, c, :].rearrange("p (h t) -> p h t", t=64),
                                                        in1=e.rearrange("p (h t) -> p h t", t=64), op=ALU.mult),
                         reads=[('bb', c), ('ex', c, 1)], writes=['KBh'])
                    S.op('act', lambda: ACT.activation(out=pdec[:, c, :].rearrange("p (h o) -> p h o", o=1), in_=cc3[:, :, totp:totp + 1], func=AF.Exp, scale=-LAM), reads=[('cc', c)], writes=['pdec'])
                ptasks = [gen_prep(c_) for c_ in range(4)]
                for t_ in ptasks:
                    next(t_)
                if pendQ is not None:
                    for _ in range(3):
                        next(pendQ, None)
                for t_ in ptasks:
                    next(t_)
                if pendQ is not None:
                    run_tasks([pendQ])
                    pendQ = None
                run_tasks(ptasks)
                for ch in range(NCH):
                    pz = ps[4 + ch % 2].bitcast(BF16)
                    pk = psk[4 + ch % 2]
                    pzv = pz[0:64, 0:512].rearrange("p (c n) -> p c n", c=4)
                    for c in range(4):
                        S.op('pe', lambda: PE.transpose(out=pzv[:, c, :], in_=vT(c)[:, tb * TBS + ch * 64: tb * TBS + (ch + 1) * 64], identity=ident),
                             reads=[('zs', 8 + c), 'ident'], writes=[pk], pe_acc=True)
                    S.op('act', lambda: ACT.copy(out=VZ[0:64, ch, :, :].rearrange("p h v -> p (h v)"), in_=pz[0:64, 0:512]), reads=[pk], writes=[('VZ', ch)])
                    S.op('act', lambda: ACT.copy(out=XV[0:64, ch, :, :].rearrange("p h v -> p (h v)"), in_=pz[0:64, 0:512]), reads=[pk], writes=[('XVv', ch)])
                    pzk = pz[:, 512:1024].rearrange("p (c n) -> p c n", c=4)
                    for c in range(4):
                        S.op('pe', lambda: PE.transpose(out=pzk[:, c, :], in_=KBh[:, c, ch, :, :].rearrange("p a t -> p (a t)"), identity=ident),
                             reads=['KBh', 'ident'], writes=[pk], pe_acc=True)
                    S.op('dve', lambda: V.tensor_copy(out=KBt[:, :, ch, :], in_=pzk), reads=[pk], writes=[('KBt', ch)])
                MNT = cmb[:, 11 + d, :]
                MN = cmb[:, 12 - d, :]

                def gen_D(ch, slot, par):
                    pA, pkA = ps[2 * par], psk[2 * par]
                    pB, pkB = ps[2 * par + 1], psk[2 * par + 1]
                    pA3 = pA.rearrange("p (j n) -> p j n", j=4)
                    pB3 = pB.rearrange("p (j n) -> p j n", j=4)
                    mzt = cmb[:, MZT[d], :]
                    for half in range(2):
                        pz3 = pA3 if half == 0 else pB3
                        pkz = pkA if half == 0 else pkB
                        for j in range(4):
                            h = half * 4 + j
                            c, hp = h // 2, h % 2
                            bk = BKt[:, c, ch, :, :].rearrange("p a t -> p (a t)")
                            S.op('pe', lambda: PE.matmul(pz3[:, j, :].rearrange("p (a t) -> p a t", a=2), lhsT=bk, rhs=ARz[:, c, ch, :, hp, :], start=True, stop=True),
                                 reads=['BKt', 'ARz'], writes=[pkz], pe_acc=True)
                        S.op('dve', lambda: V.tensor_tensor(out=ZTs[slot][half], in0=pz3, in1=bc(mzt.rearrange("p (o n) -> p o n", o=1), [128, 4, 128]), op=ALU.mult),
                             reads=[pkz, 'cmb'], writes=[('ZTs', slot, half)])
                    yield
                    for c in range(4):
                        bz = Bz[:, c, ch, :, :].rearrange("p a t -> p (a t)")
                        az = ARz[:, c, ch, 1, :, :].rearrange("p a t -> p (a t)")
                        S.op('pe', lambda: PE.matmul(pA3[:, c, :], lhsT=bz, rhs=az, start=True, stop=True), reads=['Bz', 'ARz'], writes=[pkA], pe_acc=True)
                        S.op('pe', lambda: PE.matmul(pB3[:, c, :], lhsT=az, rhs=bz, start=True, stop=True), reads=['Bz', 'ARz'], writes=[pkB], pe_acc=True)
                    S.op('dve', lambda: V.tensor_tensor(out=PTm[par][0], in0=pA3, in1=bc(MNT.rearrange("p (o n) -> p o n", o=1), [128, 4, 128]), op=ALU.mult),
                         reads=[pkA, 'cmb'], writes=[('PT', par, 0)])
                    S.op('dve', lambda: V.tensor_tensor(out=Pm[par][0], in0=pB3, in1=bc(MN.rearrange("p (o n) -> p o n", o=1), [128, 4, 128]), op=ALU.mult),
                         reads=[pkB, 'cmb'], writes=[('P', par, 0)])
                    S.op('pool', lambda: POOL.tensor_tensor(out=ATm[slot][0], in0=PTm[par][0], in1=ident4, op=ALU.add), reads=[('PT', par, 0), 'ident4'], writes=[('AT', slot, 0)])
                    S.op('pool', lambda: POOL.tensor_tensor(out=Am[par][0], in0=Pm[par][0], in1=ident4, op=ALU.add), reads=[('P', par, 0), 'ident4'], writes=[('A', par, 0)])
                    yield
                    cur = 0
                    for lev in range(1, 6):
                        nxt = 1 - cur
                        for j in range(4):
                            S.op('pe', lambda: PE.matmul(pA3[:, j, :], lhsT=Pm[par][cur][:, j, :], rhs=PTm[par][cur][:, j, :], start=True, stop=True),
                                 reads=[('P', par, cur), ('PT', par, cur)], writes=[pkA], pe_acc=True)
                            if lev < 5:
                                S.op('pe', lambda: PE.matmul(pB3[:, j, :], lhsT=PTm[par][cur][:, j, :], rhs=Pm[par][cur][:, j, :], start=True, stop=True),
                                     reads=[('P', par, cur), ('PT', par, cur)], writes=[pkB], pe_acc=True)
                        S.op('act', lambda: ACT.copy(out=PTm[par][nxt], in_=pA3), reads=[pkA], writes=[('PT', par, nxt)])
                        if lev < 5:
                            S.op('dve', lambda: V.tensor_copy(out=Pm[par][nxt], in_=pB3), reads=[pkB], writes=[('P', par, nxt)])
                        yield
                        for j in range(4):
                            S.op('pe', lambda: PE.matmul(pA3[:, j, :], lhsT=Am[par][cur][:, j, :], rhs=PTm[par][nxt][:, j, :], start=True, stop=True),
                                 reads=[('A', par, cur), ('PT', par, nxt)], writes=[pkA], pe_acc=True)
                            if lev < 5:
                                S.op('pe', lambda: PE.matmul(pB3[:, j, :], lhsT=PTm[par][nxt][:, j, :], rhs=Am[par][cur][:, j, :], start=True, stop=True),
                                     reads=[('A', par, cur), ('PT', par, nxt)], writes=[pkB], pe_acc=True)
                        S.op('dve', lambda: V.tensor_tensor(out=ATm[slot][nxt], in0=pA3, in1=ATm[slot][cur], op=ALU.add), reads=[pkA, ('AT', slot, cur)], writes=[('AT', slot, nxt)])
                        if lev < 5:
                            S.op('dve', lambda: V.tensor_tensor(out=Am[par][nxt], in0=pB3, in1=Am[par][cur], op=ALU.add), reads=[pkB, ('A', par, cur)], writes=[('A', par, nxt)])
                        yield
                        cur = nxt
                    assert cur == 1

                def gen_Q(ch, slot, tb=tb, d=d):
                    nonlocal yev
                    fin = 1
                    gch = tb * NCH + ch
                    pW, pkW = ps[4], psk[4]
                    pW3 = pW[:, 0:256].rearrange("p (c v) -> p c v", c=4)
                    for h in range(8):
                        c, hp = h // 2, h % 2
                        S.op('pe', lambda: PE.matmul(pW3[hp * 64:(hp + 1) * 64, c, :], lhsT=ZTs[slot][h // 4][:, h % 4, 64:128], rhs=VZ[:, ch, h, :], start=True, stop=False),
                             reads=[('ZTs', slot, h // 4), ('VZ', ch)], writes=[pkW], pe_acc=True)
                        S.op('pe', lambda: PE.matmul(pW3[hp * 64:(hp + 1) * 64, c, :], lhsT=ARz[:, c, ch, 1, hp, :], rhs=Sb[d][:, c, :], start=False, stop=True),
                             reads=['ARz', ('Sb', d)], writes=[pkW], pe_acc=True)
                    S.op('act', lambda: ACT.copy(out=W1s, in_=pW3), reads=[pkW], writes=['W1s'])
                    yield
                    pX, pkX = ps[5], psk[5]
                    pX3 = pX.rearrange("p (h v) -> p h v", h=8)
                    for h in range(8):
                        c, hp = h // 2, h % 2
                        S.op('pe', lambda: PE.matmul(pX3[64:128, h, :], lhsT=ATm[slot][fin][:, c, hp * 64:(hp + 1) * 64], rhs=W1s[:, c, :], start=True, stop=True),
                             reads=[('AT', slot, fin), 'W1s'], writes=[pkX], pe_acc=True)
                    S.op('dve', lambda: V.tensor_copy(out=XV[64:128, ch, :, :], in_=pX3[64:128]), reads=[pkX], writes=[('XVu', ch)])
                    yield
                    pS, pkS = ps[7], psk[7]
                    pS3 = pS[:, 0:256].rearrange("p (c v) -> p c v", c=4)
                    for h in range(8):
                        c, hp = h // 2, h % 2
                        S.op('pe', lambda: PE.matmul(pS3[hp * 64:(hp + 1) * 64, c, :], lhsT=KBt[:, c, ch, hp * 64:(hp + 1) * 64], rhs=XV[:, ch, h, :], start=True, stop=True),
                             reads=[('KBt', ch), ('XVv', ch), ('XVu', ch)], writes=[pkS], pe_acc=True)
                    pY, pkY = ps[6], psk[6]
                    pY3 = pY.rearrange("p (h v) -> p h v", h=8)
                    for h in range(8):
                        c, hp = h // 2, h % 2
                        S.op('pe', lambda: PE.matmul(pY3[0:64, h, :], lhsT=ZTs[slot][h // 4][:, h % 4, 0:64], rhs=XV[:, ch, h, :], start=True, stop=False),
                             reads=[('ZTs', slot, h // 4), ('XVv', ch), ('XVu', ch)], writes=[pkY], pe_acc=True)
                        S.op('pe', lambda: PE.matmul(pY3[0:64, h, :], lhsT=ARz[:, c, ch, 0, hp, :], rhs=Sb[d][:, c, :], start=False, stop=True),
                             reads=['ARz', ('Sb', d)], writes=[pkY], pe_acc=True)
                    for c in range(4):
                        S.op('dve', lambda: V.scalar_tensor_tensor(out=S32[d][:, c, :], in0=S32[d][:, c, :], scalar=pdec[:, c, ch:ch + 1], in1=pS3[:, c, :], op0=ALU.mult, op1=ALU.add),
                             reads=[('S32', d), 'pdec', pkS], writes=[('S32', d)])
                    S.op('act', lambda: ACT.copy(out=Sb[d], in_=S32[d]), reads=[('S32', d)], writes=[('Sb', d)])
                    yb_ = ysb[yev % 2]
                    S.op('act', lambda: ACT.copy(out=yb_, in_=pY[0:64, :]), reads=[pkY], writes=[('ysb', yev % 2)])
                    S.dma('sp', y_d[d, gch * 64:(gch + 1) * 64, :], yb_, reads=[('ysb', yev % 2)], writes=[('yscr', d, gch // 2)])
                    yev += 1
                    yield

                chs = list(range(NCH)) if d == 0 else list(range(NCH - 1, -1, -1))
                pend = []
                for r in range(0, NCH, 2):
                    tasks = [gen_D(chs[r], r, 0), gen_D(chs[r + 1], r + 1, 1)]
                    if pend:
                        tasks.append(chain([gen_Q(c_, s_) for (c_, s_) in pend]))
                    run_tasks(tasks)
                    pend = [(chs[r], r), (chs[r + 1], r + 1)]
                pendQ = chain([gen_Q(c_, s_) for (c_, s_) in pend])
        if pendQ is not None:
            run_tasks([pendQ])
            pendQ = None
        S.barrier()


    def phase_post(zsT, yaT, base):
        rT4 = zsT[:, 0:4, :]
        kT4 = zsT[:, 4:8, :]
        adT = zsT[:, 13, :]
        gdT = zsT[:, 14, :]
        AR.seek(base)
        gnwB = AR.alloc([128, 512], F32)
        gnbB = AR.alloc([128, 512], F32)
        P2 = lambda shape, dt: [AR.alloc(shape, dt) for _ in range(2)]
        Yf, Yb = P2([128, 512], F32), P2([128, 512], F32)
        ta0, ta1 = P2([128, 4, 128], F32), P2([128, 4, 128], F32)
        kf2 = P2([128, 4, 128], F32)
        prod2 = P2([128, 4, 128], BF16)
        rows2 = P2([128, 8], F32)
        bon2 = P2([128, 512], F32)
        y2 = P2([128, 512], F32)
        sq2 = P2([128, 512], F32)
        st2 = P2([128, 4, 8], F32)
        yab2 = P2([128, 512], BF16)
        S.dma('sp', gnwB, gnw_d.partition_broadcast(128), writes=['gnwB'])
        S.dma('sp', gnbB, gnb_d.partition_broadcast(128), writes=['gnbB'])
        def gen_tile(i):
            b = i % 2
            sl = slice(i * 128, (i + 1) * 128)
            ta = (ta0[b], ta1[b])
            kf, prod, rows, bon, y, sq, st, yab = kf2[b], prod2[b], rows2[b], bon2[b], y2[b], sq2[b], st2[b], yab2[b]
            bA, bB, bC, bD = 4 * b, 4 * b + 1, 4 * b + 2, 4 * b + 3
            S.dma('sp', Yf[b], y_d[0, sl, :], writes=[('Yf', b)])
            S.dma('sp', Yb[b], y_d[1, sl, :], writes=[('Yb', b)])
            for d in range(2):
                bk_ = bA if d == 0 else bB
                pz3 = ps[bk_].rearrange("p (c n) -> p c n", c=4)
                for c in range(4):
                    S.op('pe', lambda: PE.matmul(pz3[:, c, :], lhsT=a2c[:, d, c * 128:(c + 1) * 128], rhs=adT[:, sl], start=True, stop=True),
                         reads=['a2c'], writes=[psk[bk_]], pe_acc=True)
                for c in range(4):
                    S.op('act', lambda: ACT.activation(out=ta[d][:, c, :], in_=pz3[:, c, :], func=AF.Sigmoid, bias=col("a0", d * 4 + c), scale=1.0),
                         reads=[psk[bk_], 'pp'], writes=[('ta', b, d)])
            yield
            S.op('pool', lambda: POOL.tensor_tensor(out=ta[0], in0=ta[0], in1=ta[1], op=ALU.add), reads=[('ta', b, 0), ('ta', b, 1)], writes=[('ta', b, 0)])
            for c in range(4):
                S.op('pool', lambda: POOL.tensor_scalar(out=kf[:, c, :], in0=ta[0][:, c, :], scalar1=kar[:, c:c + 1], scalar2=c2r[:, c:c + 1], op0=ALU.mult, op1=ALU.add),
                     reads=[('ta', b, 0), 'kar'], writes=[('kf', b)])
            S.op('pool', lambda: POOL.tensor_tensor(out=kf, in0=kf, in1=kT4[:, :, sl], op=ALU.mult), reads=[('kf', b)], writes=[('kf', b)])
            S.op('pool', lambda: POOL.tensor_tensor(out=prod, in0=kf, in1=rT4[:, :, sl], op=ALU.mult), reads=[('kf', b)], writes=[('prod', b)])
            yield
            pr = ps[bB]
            for c in range(4):
                S.op('pe', lambda: PE.matmul(pr[:, c * 2:(c + 1) * 2], lhsT=prod[:, c, :], rhs=cmb[:, HSEL, 0:2], start=True, stop=True),
                     reads=[('prod', b), 'cmb'], writes=[psk[bB]], pe_acc=True)
            S.op('act', lambda: ACT.copy(out=rows, in_=pr[:, 0:8]), reads=[psk[bB]], writes=[('rows', b)])
            yield
            pv = ps[bD].bitcast(BF16)[:, 0:512]
            for c in range(4):
                S.op('pe', lambda: PE.transpose(out=pv[:, c * 128:(c + 1) * 128], in_=zsT[:, 8 + c, sl], identity=ident),
                     reads=['ident'], writes=[psk[bD]], pe_acc=True)
            S.op('dve', lambda: V.tensor_tensor(out=bon.rearrange("p (h v) -> p h v", h=8), in0=pv.rearrange("p (h v) -> p h v", h=8),
                                                in1=bc(rows.rearrange("p (h o) -> p h o", o=1), [128, 8, 64]), op=ALU.mult),
                 reads=[psk[bD], ('rows', b)], writes=[('bon', b)])
            yield
            pg = ps[bC]
            S.op('pe', lambda: PE.matmul(pg, lhsT=gdT[:, sl], rhs=g2, start=True, stop=True), reads=['g2'], writes=[psk[bC]], pe_acc=True)
            y3 = y.rearrange("p (h v) -> p h v", h=8)
            sq3 = sq.rearrange("p (h v) -> p h v", h=8)
            S.op('dve', lambda: V.tensor_tensor(out=y, in0=Yf[b], in1=Yb[b], op=ALU.add), reads=[('Yf', b), ('Yb', b)], writes=[('y', b)])
            yield
            S.op('dve', lambda: V.tensor_reduce(out=st[:, 0, :], in_=y3, axis=AX.X, op=ALU.add), reads=[('y', b)], writes=[('st0', b)])
            S.op('dve', lambda: V.tensor_scalar(out=st[:, 1, :], in0=st[:, 0, :], scalar1=-1.0 / 64, scalar2=None, op0=ALU.mult), reads=[('st0', b)], writes=[('st1', b)])
            S.op('dve', lambda: V.tensor_tensor(out=y3, in0=y3, in1=bc(st[:, 1, :].rearrange("p (h o) -> p h o", o=1), [128, 8, 64]), op=ALU.add),
                 reads=[('y', b), ('st1', b)], writes=[('y', b)])
            yield
            S.op('act', lambda: ACT.activation(out=sq, in_=y, func=AF.Square), reads=[('y', b)], writes=[('sq', b)])
            S.op('dve', lambda: V.tensor_reduce(out=st[:, 2, :], in_=sq3, axis=AX.X, op=ALU.add), reads=[('sq', b)], writes=[('st2', b)])
            yield
            S.op('act', lambda: ACT.activation(out=st[:, 3, :], in_=st[:, 2, :], func=AF.Sqrt, bias=epsc[:, 1:2], scale=1.0 / 64), reads=[('st2', b), 'epsc'], writes=[('st3', b)])
            S.op('dve', lambda: V.reciprocal(out=st[:, 3, :], in_=st[:, 3, :]), reads=[('st3', b)], writes=[('st3', b)])
            S.op('dve', lambda: V.tensor_tensor(out=y3, in0=y3, in1=bc(st[:, 3, :].rearrange("p (h o) -> p h o", o=1), [128, 8, 64]), op=ALU.mult),
                 reads=[('y', b), ('st3', b)], writes=[('y', b)])
            yield
            S.op('dve', lambda: V.tensor_tensor(out=y, in0=y, in1=gnwB, op=ALU.mult), reads=[('y', b), 'gnwB'], writes=[('y', b)])
            S.op('pool', lambda: POOL.tensor_tensor(out=bon, in0=bon, in1=gnbB, op=ALU.add), reads=[('bon', b), 'gnbB'], writes=[('bon', b)])
            S.op('dve', lambda: V.tensor_tensor(out=y, in0=y, in1=bon, op=ALU.add), reads=[('y', b), ('bon', b)], writes=[('y', b)])
            S.op('dve', lambda: V.tensor_tensor(out=yab, in0=y, in1=pg, op=ALU.mult), reads=[('y', b), psk[bC]], writes=[('yab', b)])
            yield
            pt = ps[bA].bitcast(BF16)[:, 0:512]
            for c in range(4):
                S.op('pe', lambda: PE.transpose(out=pt[:, c * 128:(c + 1) * 128], in_=yab[:, c * 128:(c + 1) * 128], identity=ident),
                     reads=[('yab', b), 'ident'], writes=[psk[bA]], pe_acc=True)
            S.op('act', lambda: ACT.copy(out=yaT[:, :, sl], in_=pt.rearrange("p (c n) -> p c n", c=4)), reads=[psk[bA]], writes=['yaT'])
            yield

        def run_tasks(tasks):
            tasks = list(tasks)
            while tasks:
                for t_ in list(tasks):
                    try:
                        next(t_)
                    except StopIteration:
                        tasks.remove(t_)

        for i in range(0, NT, 2):
            run_tasks([gen_tile(i), gen_tile(i + 1)])

    def phase_attn(s, uT, ybT, baseA, baseB):
        AR.seek(baseA)
        cosT = AR.alloc([128, T], F32)
        sinT = AR.alloc([128, T], F32)
        qT = AR.alloc([128, 4, T], BF16)
        kTt = AR.alloc([128, T], BF16)
        vp = AR.alloc([128, 2, NT, 128], BF16)
        AR.seek(baseB)
        wq = [AR.alloc([128, 8, 128], BF16) for _ in range(2)]
        qfL = [AR.alloc([128, 512], F32) for _ in range(2)]
        sqbL = [AR.alloc([128, 512], BF16) for _ in range(2)]
        rsL = [AR.alloc([128, 512], F32) for _ in range(2)]
        qnL = [AR.alloc([128, 512], F32) for _ in range(2)]
        qnbL = [AR.alloc([128, 512], BF16) for _ in range(2)]
        t1L = [AR.alloc([128, 512], F32) for _ in range(2)]
        t2L = [AR.alloc([128, 512], F32) for _ in range(2)]
        pTs = [AR.alloc([128, 512], BF16) for _ in range(6)]
        dn = AR.alloc([128, 512], F32)
        posi = AR.alloc([128, T], I32)
        ang = AR.alloc([128, T], F32)
        ki = AR.alloc([128, T], I32)
        kf = AR.alloc([128, T], F32)
        m1 = AR.alloc([128, T], F32)
        S.dma('sp', posi, pos_d[s].partition_broadcast(128), writes=['posi'])

        def table(dst, shift):
            S.op('dve', lambda: V.tensor_copy(out=ang, in_=posi), reads=['posi'], writes=['ang'])
            S.op('dve', lambda: V.tensor_scalar(out=ang, in0=ang, scalar1=col("invf"), scalar2=shift, op0=ALU.mult, op1=ALU.add), reads=['ang', 'pp'], writes=['ang'])
            S.op('dve', lambda: V.tensor_scalar(out=ki, in0=ang, scalar1=1.0 / TWO_PI, scalar2=None, op0=ALU.mult), reads=['ang'], writes=['ki'])
            S.op('pool', lambda: POOL.tensor_copy(out=kf, in_=ki), reads=['ki'], writes=['kf'])
            S.op('dve', lambda: V.scalar_tensor_tensor(out=ang, in0=kf, scalar=-C1, in1=ang, op0=ALU.mult, op1=ALU.add), reads=['kf', 'ang'], writes=['ang'])
            S.op('dve', lambda: V.scalar_tensor_tensor(out=ang, in0=kf, scalar=-C2, in1=ang, op0=ALU.mult, op1=ALU.add), reads=['kf', 'ang'], writes=['ang'])
            S.op('dve', lambda: V.tensor_scalar(out=m1, in0=ang, scalar1=float(np.pi), scalar2=-TWO_PI, op0=ALU.is_gt, op1=ALU.mult), reads=['ang'], writes=['m1'])
            S.op('pool', lambda: POOL.tensor_tensor(out=ang, in0=ang, in1=m1, op=ALU.add), reads=['ang', 'm1'], writes=['ang'])
            S.op('dve', lambda: V.tensor_scalar(out=m1, in0=ang, scalar1=float(-np.pi), scalar2=TWO_PI, op0=ALU.is_lt, op1=ALU.mult), reads=['ang'], writes=['m1'])
            S.op('pool', lambda: POOL.tensor_tensor(out=ang, in0=ang, in1=m1, op=ALU.add), reads=['ang', 'm1'], writes=['ang'])
            S.op('act', lambda: ACT.activation(out=dst, in_=ang, func=AF.Sin), reads=['ang'], writes=['tab'])

        table(sinT, 0.0)
        table(cosT, float(np.pi / 2))
        def c0_of(c):
            return 1920 + c * 128 if c < 4 else 2432

        def gen_qk(c, tb, L):
            b = c % 2
            gcol = col("qg") if c < 4 else col("kg")
            sl = slice(tb * 512, (tb + 1) * 512)
            qf_, sqb_, rs_, qn_, qnb_, t1_, t2_ = qfL[L], sqbL[L], rsL[L], qnL[L], qnbL[L], t1L[L], t2L[L]
            pz, pk = ps[L], psk[L]
            for k in range(8):
                S.op('pe', lambda: PE.matmul(pz, lhsT=wq[b][:, k, :], rhs=uT[:, k, sl], start=(k == 0), stop=(k == 7)),
                     reads=[('wq', b), ('uT', tb)], writes=[pk], pe_acc=True)
            S.op('act', lambda: ACT.copy(out=qf_, in_=pz), reads=[pk], writes=[('qf', L)])
            S.op('act', lambda: ACT.activation(out=sqb_, in_=qf_, func=AF.Square), reads=[('qf', L)], writes=[('sqb', L)])
            yield
            pr, pkr = ps[2 + L], psk[2 + L]
            S.op('pe', lambda: PE.matmul(pr, lhsT=cmb[:, BLK1, :], rhs=sqb_, start=True, stop=True), reads=[('sqb', L), 'cmb'], writes=[pkr], pe_acc=True)
            S.op('act', lambda: ACT.activation(out=rs_, in_=pr, func=AF.Sqrt, bias=epsc[:, 0:1], scale=1.0 / 64), reads=[pkr, 'epsc'], writes=[('rs', L)])
            yield
            S.op('dve', lambda: V.reciprocal(out=rs_, in_=rs_), reads=[('rs', L)], writes=[('rs', L)])
            S.op('dve', lambda: V.scalar_tensor_tensor(out=qn_, in0=qf_, scalar=gcol, in1=rs_, op0=ALU.mult, op1=ALU.mult), reads=[('qf', L), ('rs', L), 'pp'], writes=[('qn', L)])
            S.op('act', lambda: ACT.copy(out=qnb_, in_=qn_), reads=[('qn', L)], writes=[('qnb', L)])
            yield
            pro, pkro = ps[4 + L], psk[4 + L]
            S.op('pe', lambda: PE.matmul(pro, lhsT=cmb[:, ROT, :], rhs=qnb_, start=True, stop=True), reads=[('qnb', L), 'cmb'], writes=[pkro], pe_acc=True)
            S.op('pool', lambda: POOL.tensor_tensor(out=t1_, in0=qn_, in1=cosT[:, sl], op=ALU.mult), reads=[('qn', L), 'tab'], writes=[('t1', L)])
            yield
            S.op('dve', lambda: V.tensor_tensor(out=t2_, in0=pro, in1=sinT[:, sl], op=ALU.mult), reads=[pkro, 'tab'], writes=[('t2', L)])
            dst = qT[:, c, sl] if c < 4 else kTt[:, sl]
            S.op('dve', lambda: V.tensor_tensor(out=dst, in0=t1_, in1=t2_, op=ALU.add), reads=[('t1', L), ('t2', L)], writes=['qk'])
            yield

        def run_tasks(tasks):
            tasks = list(tasks)
            while tasks:
                for t_ in list(tasks):
                    try:
                        next(t_)
                    except StopIteration:
                        tasks.remove(t_)

        S.dma('pool', wq[0], win_d[:, c0_of(0):c0_of(0) + 128].rearrange("(k p) n -> p k n", p=128), writes=[('wq', 0)])
        for c in range(5):
            if c + 1 < 5:
                S.dma('pool', wq[(c + 1) % 2], win_d[:, c0_of(c + 1):c0_of(c + 1) + 128].rearrange("(k p) n -> p k n", p=128), writes=[('wq', (c + 1) % 2)])
            for tb in range(0, NB, 2):
                run_tasks([gen_qk(c, tb, 0), gen_qk(c, tb + 1, 1)])
        S.op('pool', lambda: POOL.memset(vp.rearrange("p a b c -> p (a b c)"), 0.0), writes=['vp'])
        S.dma('pool', wq[0], win_d[:, 2560:2688].rearrange("(k p) n -> p k n", p=128), writes=[('wq', 0)])
        for i in range(NT):
            pz, pk = ps[i % 2], psk[i % 2]
            for k in range(8):
                S.op('pe', lambda: PE.matmul(pz[:, 0:128], lhsT=uT[:, k, i * 128:(i + 1) * 128], rhs=wq[0][:, k, :], start=(k == 0), stop=(k == 7)),
                     reads=[('wq', 0), ('uT', i // 4)], writes=[pk], pe_acc=True)
            S.op('act', lambda: ACT.copy(out=vp[:, 0, i, 0:64], in_=pz[:, 0:64]), reads=[pk], writes=['vp'])
            S.op('dve', lambda: V.tensor_copy(out=vp[:, 1, i, 64:128], in_=pz[:, 64:128]), reads=[pk], writes=['vp'])
        for n in range(NT):
            qs = slice(n * 128, (n + 1) * 128)
            kbs = [kb for kb in (n - 1, n, n + 1) if 0 <= kb < NT]
            items = [(g, kb) for g in range(2) for kb in kbs]
            for idx, (g, kb) in enumerate(items):
                gp = slice(g * 64, (g + 1) * 64)
                pz, pk = ps[idx % 4], psk[idx % 4]
                S.op('pe', lambda: PE.matmul(pz.rearrange("p (j q) -> p j q", j=4), lhsT=kTt[gp, kb * 128:(kb + 1) * 128], rhs=qT[gp, :, qs], start=True, stop=True),
                     reads=['qk'], writes=[pk], pe_acc=True)
                pt_ = pTs[idx]
                S.op('act', lambda: ACT.activation(out=pt_, in_=pz, func=AF.Exp, scale=0.125), reads=[pk], writes=[('pT', idx)])
                if kb != n:
                    mk = cmb[:, MPREV if kb < n else MNEXT, :]
                    S.op('pool', lambda: POOL.tensor_tensor(out=pt_.rearrange("p (j q) -> p j q", j=4), in0=pt_.rearrange("p (j q) -> p j q", j=4),
                                                            in1=bc(mk.rearrange("p (o q) -> p o q", o=1), [128, 4, 128]), op=ALU.mult),
                         reads=[('pT', idx), 'cmb'], writes=[('pT', idx)])
            po, pko = ps[4 + n % 2], psk[4 + n % 2]
            pd_, pkd = ps[6 + n % 2], psk[6 + n % 2]
            for idx, (g, kb) in enumerate(items):
                S.op('pe', lambda: PE.matmul(po, lhsT=vp[:, g, kb, :], rhs=pTs[idx], start=(idx == 0), stop=(idx == len(items) - 1)),
                     reads=['vp', ('pT', idx)], writes=[pko], pe_acc=True)
            for idx, (g, kb) in enumerate(items):
                S.op('pe', lambda: PE.matmul(pd_, lhsT=cmb[:, VP[g], :], rhs=pTs[idx], start=(idx == 0), stop=(idx == len(items) - 1)),
                     reads=['cmb', ('pT', idx)], writes=[pkd], pe_acc=True)
            S.op('dve', lambda: V.tensor_tensor(out=dn.rearrange("p (j q) -> p j q", j=4), in0=pd_.rearrange("p (j q) -> p j q", j=4),
                                                in1=bc(esk.rearrange("p (j o) -> p j o", o=1), [128, 4, 128]), op=ALU.add), reads=[pkd, 'esk'], writes=['dn'])
            S.op('dve', lambda: V.reciprocal(out=dn, in_=dn), reads=['dn'], writes=['dn'])
            S.op('dve', lambda: V.tensor_tensor(out=ybT[:, :, qs], in0=po.rearrange("p (j q) -> p j q", j=4), in1=dn.rearrange("p (j q) -> p j q", j=4), op=ALU.mult),
                 reads=[pko, 'dn'], writes=['ybT'])

    def phase_merge(uT, yaT, ybT, mergedT, offs):
        AR.seek(offs[0])
        prw = AR.alloc([128, 4, D], BF16)
        AR.seek(offs[1])
        pat = AR.alloc([128, 4, D], BF16)
        wga = [AR.alloc([128, 8, 128], BF16) for _ in range(2)]
        wgb = [AR.alloc([128, 8, 128], BF16) for _ in range(2)]
        sgaP = [AR.alloc([128, 512], BF16) for _ in range(2)]
        sgbP = [AR.alloc([128, 512], BF16) for _ in range(2)]
        t1P = [AR.alloc([128, 512], F32) for _ in range(2)]
        t2P = [AR.alloc([128, 512], F32) for _ in range(2)]
        for hh in range(2):
            S.dma('pool', prw[:, hh * 2:(hh + 1) * 2, :], prw_d[hh * 256:(hh + 1) * 256, :].rearrange("(k p) n -> p k n", p=128), writes=['prw'])
            S.dma('pool', pat[:, hh * 2:(hh + 1) * 2, :], pat_d[hh * 256:(hh + 1) * 256, :].rearrange("(k p) n -> p k n", p=128), writes=['pat'])
        for oc in range(8):
            b = oc % 2
            S.dma('pool', wga[b], win_d[:, 2688 + oc * 128:2688 + (oc + 1) * 128].rearrange("(k p) n -> p k n", p=128), writes=[('wga', b)])
            S.dma('pool', wgb[b], win_d[:, 3712 + oc * 128:3712 + (oc + 1) * 128].rearrange("(k p) n -> p k n", p=128), writes=[('wgb', b)])
            for tb in range(NB):
                sl = slice(tb * 512, (tb + 1) * 512)
                L = tb % 2
                sga, sgb, t1, t2 = sgaP[L], sgbP[L], t1P[L], t2P[L]
                for k in range(8):
                    S.op('pe', lambda: PE.matmul(ps[0 + 4 * L], lhsT=wga[b][:, k, :], rhs=uT[:, k, sl], start=(k == 0), stop=(k == 7)),
                         reads=[('wga', b), ('uT', tb)], writes=[psk[0 + 4 * L]], pe_acc=True)
                S.op('act', lambda: ACT.activation(out=sga, in_=ps[0 + 4 * L], func=AF.Sigmoid), reads=[psk[0 + 4 * L]], writes=[('sga', L)])
                for k in range(8):
                    S.op('pe', lambda: PE.matmul(ps[1 + 4 * L], lhsT=wgb[b][:, k, :], rhs=uT[:, k, sl], start=(k == 0), stop=(k == 7)),
                         reads=[('wgb', b), ('uT', tb)], writes=[psk[1 + 4 * L]], pe_acc=True)
                S.op('act', lambda: ACT.activation(out=sgb, in_=ps[1 + 4 * L], func=AF.Sigmoid), reads=[psk[1 + 4 * L]], writes=[('sgb', L)])
                for k in range(4):
                    S.op('pe', lambda: PE.matmul(ps[2 + 4 * L], lhsT=prw[:, k, oc * 128:(oc + 1) * 128], rhs=yaT[:, k, sl], start=(k == 0), stop=(k == 3)),
                         reads=['prw', 'yaT'], writes=[psk[2 + 4 * L]], pe_acc=True)
                for k in range(4):
                    S.op('pe', lambda: PE.matmul(ps[3 + 4 * L], lhsT=pat[:, k, oc * 128:(oc + 1) * 128], rhs=ybT[:, k, sl], start=(k == 0), stop=(k == 3)),
                         reads=['pat', 'ybT'], writes=[psk[3 + 4 * L]], pe_acc=True)
                S.op('dve', lambda: V.tensor_tensor(out=t1, in0=ps[2 + 4 * L], in1=sga, op=ALU.mult), reads=[psk[2 + 4 * L], ('sga', L)], writes=[('t1', L)])
                S.op('dve', lambda: V.tensor_tensor(out=t2, in0=ps[3 + 4 * L], in1=sgb, op=ALU.mult), reads=[psk[3 + 4 * L], ('sgb', L)], writes=[('t2', L)])
                S.op('pool', lambda: POOL.tensor_tensor(out=mergedT[:, oc, sl], in0=t1, in1=t2, op=ALU.add), reads=[('t1', L), ('t2', L)], writes=[('mg', tb)])

    def phase_x1(s, mergedT, u2tm, base):
        AR.seek(base)
        wo = AR.alloc([128, 8, D], BF16)
        gt1B = AR.alloc([128, D], F32)
        sc2 = AR.alloc([128, D], F32)
        sh2 = AR.alloc([128, D], F32)
        xt = [AR.alloc([128, D], F32) for _ in range(2)]
        x1t = [AR.alloc([128, D], F32) for _ in range(2)]
        tmpP = [AR.alloc([128, D], F32) for _ in range(2)]
        junkP = [AR.alloc([128, D], BF16) for _ in range(2)]
        u2TP = [AR.alloc([128, 8, 128], BF16) for _ in range(2)]
        ssP = [AR.alloc([128, 8], F32) for _ in range(2)]
        exP = [AR.alloc([128, E], F32) for _ in range(2)]
        for hh in range(4):
            S.dma('pool', wo[:, hh * 2:(hh + 1) * 2, :], wout_d[hh * 256:(hh + 1) * 256, :].rearrange("(k p) n -> p k n", p=128), writes=['wo'])
        S.dma('sp', gt1B, mod_d[s, 2], writes=['gt1B'])
        S.dma('sp', sc2, mod_d[s, 4], writes=['sc2'])
        S.dma('sp', sh2, mod_d[s, 3], writes=['sh2'])
        lg = AR.alloc([128, NT, E], F32)
        mxs = AR.alloc([128, 3, NT], F32)

        def gen_x1(i):
            b = i % 2
            sl = slice(i * 128, (i + 1) * 128)
            S.dma('sp', xt[b], x_d[s, sl, :], writes=[('xt', b)])
            tmp, junk, u2T, ss = tmpP[b], junkP[b], u2TP[b], ssP[b]
            for cb in range(2):
                for k in range(8):
                    S.op('pe', lambda: PE.matmul(ps[cb + 6 * b], lhsT=mergedT[:, k, sl], rhs=wo[:, k, cb * 512:(cb + 1) * 512], start=(k == 0), stop=(k == 7)),
                         reads=[('mg', i // 4), 'wo'], writes=[psk[cb + 6 * b]], pe_acc=True)
                S.op('dve', lambda: V.tensor_tensor(out=tmp[:, cb * 512:(cb + 1) * 512], in0=ps[cb + 6 * b], in1=gt1B[:, cb * 512:(cb + 1) * 512], op=ALU.mult),
                     reads=[psk[cb + 6 * b], 'gt1B'], writes=[('tmp', b, cb)])
            yield
            S.op('pool', lambda: POOL.tensor_tensor(out=x1t[b], in0=tmp, in1=xt[b], op=ALU.add), reads=[('tmp', b, 0), ('tmp', b, 1), ('xt', b)], writes=[('x1t', b)])
            S.dma('sp', out_d[s, sl, :], x1t[b], reads=[('x1t', b)], writes=[('outd', i)])
            S.op('act', lambda: ACT.activation(out=junk, in_=x1t[b], func=AF.Square, accum_out=ss[:, 0:1]), reads=[('x1t', b)], writes=[('junk', b), ('ss0', b)])
            yield
            S.op('act', lambda: ACT.activation(out=ss[:, 1:2], in_=ss[:, 0:1], func=AF.Sqrt, bias=epsc[:, 0:1], scale=1.0 / D), reads=[('ss0', b), 'epsc'], writes=[('ss1', b)])
            S.op('dve', lambda: V.reciprocal(out=ss[:, 1:2], in_=ss[:, 1:2]), reads=[('ss1', b)], writes=[('ss1', b)])
            S.op('dve', lambda: V.scalar_tensor_tensor(out=tmp, in0=x1t[b], scalar=ss[:, 1:2], in1=sc2, op0=ALU.mult, op1=ALU.mult),
                 reads=[('x1t', b), ('ss1', b), 'sc2'], writes=[('tmp', b, 0), ('tmp', b, 1)])
            yield
            S.op('pool', lambda: POOL.tensor_tensor(out=u2tm[:, i, :], in0=tmp, in1=sh2, op=ALU.add), reads=[('tmp', b, 0), ('tmp', b, 1), 'sh2'], writes=[('u2', i)])
            pz = ps[2 + b].bitcast(BF16).rearrange("p (k t) -> p k t", k=8)
            pk = psk[2 + b]
            for k in range(8):
                S.op('pe', lambda: PE.transpose(out=pz[:, k, :], in_=u2tm[:, i, k * 128:(k + 1) * 128], identity=ident),
                     reads=[('u2', i), 'ident'], writes=[pk], pe_acc=True)
            yield
            S.op('act', lambda: ACT.copy(out=u2T, in_=pz), reads=[pk], writes=[('u2T', b)])
            pl, pkl = ps[4 + b], psk[4 + b]
            for k in range(8):
                S.op('pe', lambda: PE.matmul(pl[:, 0:E], lhsT=u2T[:, k, :], rhs=wr[:, k, :], start=(k == 0), stop=(k == 7)),
                     reads=[('u2T', b), 'wr'], writes=[pkl], pe_acc=True)
            yield
            S.op('act', lambda: ACT.copy(out=lg[:, i, :], in_=pl[:, 0:E]), reads=[pkl], writes=['lg'])
            yield

        def run_tasks(tasks):
            tasks = list(tasks)
            while tasks:
                for t_ in list(tasks):
                    try:
                        next(t_)
                    except StopIteration:
                        tasks.remove(t_)

        for i in range(0, NT, 2):
            run_tasks([gen_x1(i), gen_x1(i + 1)])
        S.op('dve', lambda: V.tensor_reduce(out=mxs[:, 0, :], in_=lg, axis=AX.X, op=ALU.max), reads=['lg'], writes=['mx0'])
        S.op('dve', lambda: V.tensor_tensor(out=lg, in0=lg, in1=bc(mxs[:, 0, :].rearrange("p (i o) -> p i o", o=1), [128, NT, E]), op=ALU.subtract),
             reads=['lg', 'mx0'], writes=['lg'])
        S.op('act', lambda: ACT.activation(out=lg, in_=lg, func=AF.Exp), reads=['lg'], writes=['lg'])
        S.op('dve', lambda: V.tensor_reduce(out=mxs[:, 1, :], in_=lg, axis=AX.X, op=ALU.add), reads=['lg'], writes=['mx1'])
        S.op('dve', lambda: V.reciprocal(out=mxs[:, 2, :], in_=mxs[:, 1, :]), reads=['mx1'], writes=['mx2'])
        S.op('dve', lambda: V.tensor_tensor(out=afftm, in0=lg, in1=bc(mxs[:, 2, :].rearrange("p (i o) -> p i o", o=1), [128, NT, E]), op=ALU.mult),
             reads=['lg', 'mx2'], writes=['afftm'])

    def phase_moe(s, u2tm, base):
        AR.seek(base)
        affT = AR.alloc([16, T], F32)
        work = AR.alloc([16, T], F32)
        maskT = AR.alloc([16, T], F32)
        slotT = AR.alloc([16, T], F32)
        mx8 = AR.alloc([16, 8], F32)
        for i in range(NT):
            pz = ps[i // 4]
            S.op('pe', lambda: PE.transpose(out=pz[0:16, (i % 4) * 128:(i % 4 + 1) * 128], in_=afftm[:, i, :], identity=identf),
                 reads=['afftm', 'identf'], writes=[psk[i // 4]], pe_acc=True)
        for q in range(4):
            S.op('act', lambda: ACT.copy(out=affT[:, q * 512:(q + 1) * 512], in_=ps[q][0:16, :]), reads=[psk[q]], writes=['affT'])
        S.op('dve', lambda: V.tensor_copy(out=work, in_=affT), reads=['affT'], writes=['work'])
        for it in range(CAP // 8):
            S.op('dve', lambda: V.max(out=mx8, in_=work), reads=['work'], writes=['mx8'])
            if it < CAP // 8 - 1:
                S.op('dve', lambda: V.match_replace(out=work, in_to_replace=mx8, in_values=work, imm_value=-1.0), reads=['work', 'mx8'], writes=['work'])
        S.op('dve', lambda: V.tensor_scalar(out=maskT, in0=affT, scalar1=mx8[:, 7:8], scalar2=None, op0=ALU.is_ge), reads=['affT', 'mx8'], writes=['maskT'])
        S.op('pool', lambda: POOL.memset(work, 1.0), reads=['work'], writes=['work'])
        S.op('dve', lambda: V.tensor_tensor_scan(out=slotT, data0=work, data1=maskT, initial=0.0, op0=ALU.mult, op1=ALU.add), reads=['work', 'maskT'], writes=['slotT'])
        S.op('dve', lambda: V.tensor_tensor(out=slotT, in0=slotT, in1=maskT, op=ALU.mult), reads=['slotT', 'maskT'], writes=['slotT'])
        S.op('dve', lambda: V.tensor_scalar(out=slotT, in0=slotT, scalar1=-1.0, scalar2=None, op0=ALU.add), reads=['slotT'], writes=['slotT'])
        pz = ps[4]
        for i in range(NT):
            S.op('pe', lambda: PE.transpose(out=pz[:, i * 16:(i + 1) * 16], in_=slotT[:, i * 128:(i + 1) * 128], identity=identf[0:16, 0:16]),
                 reads=['slotT', 'identf'], writes=[psk[4]], pe_acc=True)
        S.op('act', lambda: ACT.copy(out=slot_tm.rearrange("p i e -> p (i e)"), in_=pz[:, 0:256]), reads=[psk[4]], writes=['slot_tm'])
        S.op('dve', lambda: V.tensor_copy(out=affhl[:, :, :, 0], in_=afftm), reads=['afftm'], writes=['affhl'])
        S.op('dve', lambda: V.tensor_tensor(out=affhl[:, :, :, 1], in0=afftm, in1=affhl[:, :, :, 0], op=ALU.subtract), reads=['afftm', 'affhl'], writes=['affhl'])
        S.barrier()
        AR.seek(base)
        ye = AR.alloc([128, E, 2, D], BF16)
        Wg = AR.alloc([128, 8, D], BF16)
        Wu = AR.alloc([128, 8, D], BF16)
        Wd = AR.alloc([128, 8, D], BF16)
        wbase = AR.ptr
        Pe = AR.alloc([128, NT, CAP], BF16)
        xeT = AR.alloc([128, 8, CAP], BF16)
        hT = AR.alloc([128, 8, CAP], BF16)
        hs = AR.alloc([128, CAP], F32)
        affs = AR.alloc([128, 4], F32)
        gt2B = AR.alloc([128, D], F32)
        S.dma('sp', gt2B, mod_d[s, 5], writes=['gt2B'])
        for e in range(E):
            for (wt, wsrc, nm) in ((Wg, wg_d, 'Wg'), (Wu, wu_d, 'Wu'), (Wd, wd_d, 'Wd')):
                for hh in range(4):
                    S.dma('pool', wt[:, hh * 2:(hh + 1) * 2, :], wsrc[e, hh * 256:(hh + 1) * 256, :].rearrange("(k p) n -> p k n", p=128), writes=[(nm, hh)])
            for i in range(NT):
                S.op('dve', lambda: V.tensor_scalar(out=Pe[:, i, :], in0=iota_row, scalar1=slot_tm[:, i, e:e + 1], scalar2=None, op0=ALU.is_equal),
                     reads=['iota_row', 'slot_tm'], writes=[('Pe', i)])
            for fc in range(8):
                pz, pk = ps[fc // 2], psk[fc // 2]
                pzs = pz[:, (fc % 2) * 256:(fc % 2 + 1) * 256]
                for i in range(NT):
                    S.op('pe', lambda: PE.matmul(pzs, lhsT=u2tm[:, i, fc * 128:(fc + 1) * 128], rhs=Pe[:, i, :], start=(i == 0), stop=(i == NT - 1)),
                         reads=[('u2', i), ('Pe', i)], writes=[pk], pe_acc=True)
                S.op('act', lambda: ACT.copy(out=xeT[:, fc, :], in_=pzs), reads=[pk], writes=[('xeT', fc)])
            pa, pka = ps[4], psk[4]
            for half in range(2):
                for i in range(NT):
                    S.op('pe', lambda: PE.matmul(pa[:, half * 2:(half + 1) * 2], lhsT=Pe[:, i, half * 128:(half + 1) * 128], rhs=affhl[:, i, e, :], start=(i == 0), stop=(i == NT - 1)),
                         reads=[('Pe', i), 'affhl'], writes=[pka], pe_acc=True)
            S.op('dve', lambda: V.tensor_reduce(out=affs[:, 0:2], in_=pa[:, 0:4].rearrange("p (h t) -> p h t", t=2), axis=AX.X, op=ALU.add), reads=[pka], writes=['affs'])
            for fk in range(8):
                pg, pkg = ps[5], psk[5]
                pu, pku = ps[6], psk[6]
                for k in range(8):
                    S.op('pe', lambda: PE.matmul(pg[:, 0:CAP], lhsT=Wg[:, k, fk * 128:(fk + 1) * 128], rhs=xeT[:, k, :], start=(k == 0), stop=(k == 7)),
                         reads=[('Wg', k // 2), ('xeT', k)], writes=[pkg], pe_acc=True)
                for k in range(8):
                    S.op('pe', lambda: PE.matmul(pu[:, 0:CAP], lhsT=Wu[:, k, fk * 128:(fk + 1) * 128], rhs=xeT[:, k, :], start=(k == 0), stop=(k == 7)),
                         reads=[('Wu', k // 2), ('xeT', k)], writes=[pku], pe_acc=True)
                S.op('act', lambda: ACT.activation(out=hs, in_=pg[:, 0:CAP], func=AF.Silu), reads=[pkg], writes=['hs'])
                S.op('dve', lambda: V.tensor_tensor(out=hT[:, fk, :], in0=pu[:, 0:CAP], in1=hs, op=ALU.mult), reads=[pku, 'hs'], writes=[('hT', fk)])
            for half in range(2):
                for cb in range(2):
                    py, pky = ps[7] if (half * 2 + cb) % 2 else ps[4], psk[7] if (half * 2 + cb) % 2 else psk[4]
                    for fk in range(8):
                        S.op('pe', lambda: PE.matmul(py, lhsT=hT[:, fk, half * 128:(half + 1) * 128], rhs=Wd[:, fk, cb * 512:(cb + 1) * 512], start=(fk == 0), stop=(fk == 7)),
                             reads=[('hT', fk), ('Wd', fk // 2), 'affs'], writes=[pky], pe_acc=True)
                    S.op('dve', lambda: V.tensor_scalar(out=ye[:, e, half, cb * 512:(cb + 1) * 512], in0=py, scalar1=affs[:, half:half + 1], scalar2=None, op0=ALU.mult),
                         reads=[pky, 'affs'], writes=['ye'])
        S.barrier()
        AR.seek(wbase - 3 * 8 * D * 2)
        Pall = AR.alloc([128, E, CAP], BF16)
        PT = AR.alloc([128, 2 * E, 128], BF16)
        x1t = [AR.alloc([128, D], F32) for _ in range(2)]
        ot = [AR.alloc([128, D], F32) for _ in range(2)]
        for i in range(NT):
            b = i % 2
            sl = slice(i * 128, (i + 1) * 128)
            S.dma('sp', x1t[b], out_d[s, sl, :], reads=[('outd', i)], writes=[('x1t', b)])
            for e in range(E):
                S.op('dve', lambda: V.tensor_scalar(out=Pall[:, e, :], in0=iota_row, scalar1=slot_tm[:, i, e:e + 1], scalar2=None, op0=ALU.is_equal),
                     reads=['iota_row', 'slot_tm'], writes=[('Pall', e // 4)])
            for q in range(4):
                pz = ps[q].bitcast(BF16).rearrange("p (j t) -> p j t", j=8)
                for j in range(8):
                    idx = q * 8 + j
                    e, half = idx // 2, idx % 2
                    S.op('pe', lambda: PE.transpose(out=pz[:, j, :], in_=Pall[:, e, half * 128:(half + 1) * 128], identity=ident),
                         reads=[('Pall', e // 4), 'ident'], writes=[psk[q]], pe_acc=True)
                if q % 2 == 0:
                    S.op('act', lambda: ACT.copy(out=PT[:, q * 8:(q + 1) * 8, :], in_=pz), reads=[psk[q]], writes=[('PT', q)])
                else:
                    S.op('dve', lambda: V.tensor_copy(out=PT[:, q * 8:(q + 1) * 8, :], in_=pz), reads=[psk[q]], writes=[('PT', q)])
            for cb in range(2):
                po, pko = ps[4 + cb + 2 * (i % 2)], psk[4 + cb + 2 * (i % 2)]
                for idx in range(2 * E):
                    e, half = idx // 2, idx % 2
                    S.op('pe', lambda: PE.matmul(po, lhsT=PT[:, idx, :], rhs=ye[:, e, half, cb * 512:(cb + 1) * 512], start=(idx == 0), stop=(idx == 2 * E - 1)),
                         reads=[('PT', idx // 8), 'ye'], writes=[pko], pe_acc=True)
                S.op('dve', lambda: V.tensor_tensor(out=ot[b][:, cb * 512:(cb + 1) * 512], in0=po, in1=gt2B[:, cb * 512:(cb + 1) * 512], op=ALU.mult),
                     reads=[pko, 'gt2B'], writes=[('ot', b, cb)])
            S.op('dve', lambda: V.tensor_tensor(out=ot[b], in0=ot[b], in1=x1t[b], op=ALU.add), reads=[('ot', b, 0), ('ot', b, 1), ('x1t', b)], writes=[('ot', b, 0), ('ot', b, 1)])
            S.dma('sp', out_d[s, sl, :], ot[b], reads=[('ot', b, 0), ('ot', b, 1)], writes=[('outd', i)])

    def dbg_dump(src_ap, shape, key_reads=()):
        AR.seek(AR_TOP)
        t = AR.alloc(shape, F32)
        S.op('dve', lambda: V.tensor_copy(out=t, in_=src_ap), writes=['dbgt'])
        flat = t if len(shape) == 2 else t.rearrange("p a b -> p (a b)")
        S.dma('sp', dbg_d, flat, reads=['dbgt'])

    AR_TOP = 160 * 1024
    phase_adaln()
    for s in range(nseq):
        AR.seek(0)
        zsT = AR.alloc([128, 15, T], BF16)
        uT = AR.alloc([128, 8, T], BF16)
        base1 = AR.ptr
        phase_norm1(s, uT, base1)
        S.barrier()
        if dbg and dbg[0] == 'uT':
            dbg_dump(uT[:, :, 0:512], [128, 8, 512]); break
        for q in range(4):
            S.dma('sp', u_d[:, 2 * q:2 * q + 2, :], uT[:, 2 * q:2 * q + 2, :], reads=[('uT', 0), ('uT', 1), ('uT', 2), ('uT', 3)], writes=['uscr'])
        phase_rwkv_cols(uT, zsT, base1)
        S.barrier()
        if dbg and dbg[0] == 'zs':
            dbg_dump(zsT[:, :, 0:256], [128, 15, 256]); break
        AR.seek(61440)
        kkT = AR.alloc([128, 4, T], BF16)
        yaT = AR.alloc([128, 4, T], BF16)
        base3 = AR.ptr
        phase_scan(zsT, kkT, base3)
        if dbg and dbg[0] == 'yscan':
            AR.seek(AR_TOP)
            t = AR.alloc([128, 2, 512], F32)
            S.dma('sp', t[:, 0, :], y_d[0, 0:128, :], writes=['dbgt'])
            S.dma('sp', t[:, 1, :], y_d[1, 0:128, :], writes=['dbgt'])
            S.dma('sp', dbg_d, t.rearrange("p a b -> p (a b)"), reads=['dbgt']); break
        phase_post(zsT, yaT, base3)
        S.barrier()
        if dbg and dbg[0] == 'yaT':
            dbg_dump(yaT[:, :, 0:512], [128, 4, 512]); break
        AR.seek(0)
        uT = AR.alloc([128, 8, T], BF16)
        AR.seek(94208)
        ybT = AR.alloc([128, 4, T], BF16)
        baseB = AR.ptr
        for q in range(4):
            S.dma('sp' if q % 2 == 0 else 'act', uT[:, 2 * q:2 * q + 2, :], u_d[:, 2 * q:2 * q + 2, :], writes=[('uT', 0), ('uT', 1), ('uT', 2), ('uT', 3)])
        phase_attn(s, uT, ybT, 32768, baseB)
        S.barrier()
        if dbg and dbg[0] == 'ybT':
            dbg_dump(ybT[:, :, 0:512], [128, 4, 512]); break
        AR.seek(32768)
        mergedT = AR.alloc([128, 8, T], BF16)
        phase_merge(uT, yaT, ybT, mergedT, (65536, baseB))
        S.barrier()
        if dbg and dbg[0] == 'merged':
            dbg_dump(mergedT[:, :, 0:512], [128, 8, 512]); break
        AR.seek(0)
        u2tm = AR.alloc([128, NT, D], BF16)
        phase_x1(s, mergedT, u2tm, 65536)
        S.barrier()
        if dbg and dbg[0] == 'aff':
            dbg_dump(afftm.rearrange("p i e -> p (i e)"), [128, 256]); break
        phase_moe(s, u2tm, 32768)
        S.barrier()

    S.finish('sp')
    print("ninstr", S.ninstr, "pe_incs", S.npe_inc, "arena hi", AR.hi)
    return nc


def _consts():
    cm = np.zeros((13, 128, 128), np.float32)
    p = np.arange(128)
    cm[0] = (p[:, None] // 64 == p[None, :] // 64).astype(np.float32)
    R = np.zeros((128, 128), np.float32)
    for blk in range(2):
        o = blk * 64
        for d_ in range(8):
            R[o + d_ + 8, o + d_] = -1.0
            R[o + d_, o + d_ + 8] = 1.0
    cm[1] = R
    cm[2] = (p[:, None] >= p[None, :]).astype(np.float32)
    cm[3] = (p[:, None] <= p[None, :]).astype(np.float32)
    s_ = (p % 64)[:, None]
    t_ = (p % 64)[None, :]
    a_col = (p[None, :] >= 64)
    fwd = np.where(a_col, s_ < t_, s_ <= t_)
    bwd = np.where(a_col, s_ > t_, s_ >= t_)
    cm[4] = fwd.astype(np.float32)
    cm[5] = bwd.astype(np.float32)
    cm[6] = cm[4].T
    cm[7] = cm[5].T
    cm[8][:, 0] = (p < 64)
    cm[8][:, 1] = (p >= 64)
    cm[9][:, 0:64] = 1.0
    cm[10][:, 64:128] = 1.0
    cm[11] = ((p % 64)[:, None] < (p % 64)[None, :]).astype(np.float32)
    cm[12] = ((p % 64)[:, None] > (p % 64)[None, :]).astype(np.float32)
    return np.ascontiguousarray(cm.transpose(1, 0, 2).reshape(128, 13 * 128))


def _prep_shared(inp):
    f = lambda a: np.ascontiguousarray(np.asarray(a, dtype=np.float32))
    L = 0
    w_in = f(inp["w_in"][L]).copy()
    qoff = 1920
    perm = []
    for c in range(4):
        perm += list(range(c * 64, (c + 1) * 64)) + list(range((4 + c) * 64, (5 + c) * 64))
    perm = np.array(perm)
    w_in[:, qoff:qoff + 512] = w_in[:, qoff:qoff + 512][:, perm]
    p_attn = f(inp["p_attn"][L])[perm, :]
    pp = np.zeros((128, NPP), np.float32)

    def put(name, arr):
        o, w = PP[name]
        pp[:, o:o + w] = arr

    chunked = lambda v: np.asarray(v, np.float32).reshape(-1, 128).T
    put("mp", chunked(inp["mu_prev"][L]))
    put("mn", chunked(inp["mu_next"][L]))
    put("w0", np.concatenate([chunked(inp["rwkv_w0"][L][0]), chunked(inp["rwkv_w0"][L][1])], 1))
    put("a0", np.concatenate([chunked(inp["rwkv_a0"][L][0]), chunked(inp["rwkv_a0"][L][1])], 1))
    put("kk", chunked(inp["rwkv_k_k"][L]))
    put("ka", chunked(inp["rwkv_k_a"][L]))
    put("rk", chunked(np.asarray(inp["rwkv_r_k"][L]).reshape(-1)))
    put("qg", np.tile(np.asarray(inp["q_norm_g"][L], np.float32), 2)[:, None])
    put("kg", np.tile(np.asarray(inp["k_norm_g"][L], np.float32), 2)[:, None])
    inv_freq = (500000.0 ** (-np.arange(0, 16, 2, dtype=np.float32) / 16)).astype(np.float32)
    invf = np.zeros(64, np.float32)
    invf[0:8] = inv_freq
    invf[8:16] = inv_freq
    put("invf", np.tile(invf, 2)[:, None])
    sink = np.asarray(inp["attn_sink"][L], np.float32)
    sk = np.zeros((128, 4), np.float32)
    for j in range(4):
        sk[0:64, j] = sink[j]
        sk[64:128, j] = sink[4 + j]
    put("sink", sk)
    w2cat = np.zeros((128, 2, 512), np.float32)
    a2cat = np.zeros((128, 2, 512), np.float32)
    for d_ in range(2):
        w2cat[d_ * 64:(d_ + 1) * 64, d_, :] = inp["rwkv_w2"][L][d_]
        a2cat[d_ * 64:(d_ + 1) * 64, d_, :] = inp["rwkv_a2"][L][d_]
    return {
        "w_ada": f(inp["w_ada"][L]), "b_ada": f(inp["b_ada"][L])[None, :] if np.asarray(inp["b_ada"][L]).ndim == 1 else f(inp["b_ada"][L]),
        "norm1_g": f(inp["norm1_g"][L]).reshape(1, D), "norm2_g": f(inp["norm2_g"][L]).reshape(1, D),
        "w_in": w_in, "pp": pp, "w2cat": w2cat.reshape(128, 1024), "a2cat": a2cat.reshape(128, 1024),
        "g2": f(inp["rwkv_g2"][L]), "gn_w": f(inp["rwkv_gn_w"][L]).reshape(1, 512), "gn_b": f(inp["rwkv_gn_b"][L]).reshape(1, 512),
        "p_rwkv": f(inp["p_rwkv"][L]), "p_attn": np.ascontiguousarray(p_attn), "w_out": f(inp["w_out"][L]),
        "w_router": f(inp["w_router"][L]), "w_gate": f(inp["w_gate"][L]), "w_up": f(inp["w_up"][L]), "w_down": f(inp["w_down"][L]),
        "cmats": _consts(),
    }


def _core_inputs(inp, shared, seqs):
    x = np.ascontiguousarray(np.asarray(inp["x"], np.float32)[seqs])
    c = np.asarray(inp["c"], np.float32)[seqs]
    cT = np.ascontiguousarray(c.reshape(len(seqs), 8, 128).transpose(0, 2, 1))
    pos = np.ascontiguousarray(np.asarray(inp["positions"]).astype(np.int32)[seqs][:, None, :])
    m = dict(shared)
    m.update({"x": x, "cT": cT, "pos": pos})
    return m


def kernel(**inputs):
    shared = _prep_shared(inputs)
    nc = build(NSEQ)
    in_maps = [_core_inputs(inputs, shared, list(range(i * NSEQ, (i + 1) * NSEQ))) for i in range(NCORES)]
    res = run_bass_kernel_spmd(nc, in_maps, core_ids=list(range(NCORES)))
    out = np.concatenate([np.asarray(r["out"]) for r in res.results], axis=0)
    return out.astype(np.float32)
```

```python
import numpy as np
import concourse.bass as bass
import concourse.mybir as mybir
from concourse.bass_utils import run_bass_kernel_spmd

F32 = mybir.dt.float32
BF16 = mybir.dt.bfloat16
I32 = mybir.dt.int32
ALU = mybir.AluOpType
AF = mybir.ActivationFunctionType
AX = mybir.AxisListType

T = 2048
D = 1024
NT = 16
NB = 4
NSEQ = 2
NCORES = 8
E = 16
CAP = 256
LAM = float(np.exp(-0.5))
NCH = 4
TBS = NCH * 64
NTB = T // TBS
TWO_PI = float(2 * np.pi)
C1 = 6.28125
C2 = TWO_PI - C1

PP = {}
_o = 0
for _n, _w in [("mp", 15), ("mn", 15), ("w0", 8), ("a0", 8), ("kk", 4), ("ka", 4), ("rk", 4), ("qg", 1), ("kg", 1),
               ("invf", 1), ("sink", 4)]:
    PP[_n] = (_o, _w)
    _o += _w
NPP = _o


class Ticket:
    __slots__ = ('ins', 'sem', 'val', 'parent')

    def __init__(self, ins):
        self.ins = ins
        self.sem = None
        self.val = None
        self.parent = None

    def root(self):
        t = self
        while t.parent is not None:
            t = t.parent
        return t


class Sync:
    SEM_MAX = 30000

    def __init__(self, nc):
        self.nc = nc
        self.E = {'pe': nc.tensor, 'act': nc.scalar, 'dve': nc.vector, 'pool': nc.gpsimd, 'sp': nc.sync}
        self.sem = {}
        self.cnt = {}
        self.nsem = 0
        for e in self.E:
            self._newsem(e)
        self.waited = {}
        self.lastw = {}
        self.reads = {}
        self.dma_sems = {}
        self.dma_rr = {}
        self.ninstr = 0
        self.pend = None
        self.pend_writes = None
        self.npe_inc = 0

    def _newsem(self, e):
        self.sem[e] = self.nc.alloc_semaphore(f"s_{e}_{self.nsem}")
        self.nsem += 1
        self.cnt[e] = 0

    def _flush_pe(self):
        t = self.pend
        if t is None:
            return
        if self.cnt['pe'] >= self.SEM_MAX:
            self._newsem('pe')
        self.cnt['pe'] += 1
        t.sem = self.sem['pe']
        t.val = self.cnt['pe']
        t.ins.then_inc(t.sem, 1)
        self.npe_inc += 1
        self.pend = None
        self.pend_writes = None

    def _wait(self, e, ev):
        if ev is None:
            return
        if isinstance(ev, Ticket):
            if e == 'pe':
                return
            t = ev.root()
            if t.val is None:
                assert t is self.pend
                self._flush_pe()
            sem, val = t.sem, t.val
        else:
            src, sem, val = ev
        k = (e, sem.name)
        if self.waited.get(k, 0) >= val:
            return
        self.waited[k] = val
        self.E[e].wait_ge(sem, val)

    def deps(self, e, reads, writes, pe_acc=False):
        for k in reads:
            self._wait(e, self.lastw.get(k))
        for k in writes:
            lw = self.lastw.get(k)
            if not (pe_acc and isinstance(lw, Ticket)):
                self._wait(e, lw)
            for ev in self.reads.get(k, {}).values():
                self._wait(e, ev)

    def commit(self, src, ev, reads, writes):
        for k in reads:
            self.reads.setdefault(k, {})[src] = ev
        for k in writes:
            self.lastw[k] = ev
            self.reads[k] = {}

    def op(self, e, fn, reads=(), writes=(), pe_acc=False):
        self.deps(e, reads, writes, pe_acc)
        if e == 'pe':
            ins = fn()
            t = Ticket(ins)
            if self.pend is not None:
                if self.pend_writes == tuple(writes):
                    self.pend.parent = t
                    self.pend = None
                else:
                    self._flush_pe()
            self.pend = t
            self.pend_writes = tuple(writes)
            self.commit('pe', t, reads, writes)
            self.ninstr += 1
            return t
        if self.cnt[e] >= self.SEM_MAX:
            self._newsem(e)
        ins = fn()
        self.cnt[e] += 1
        ev = (e, self.sem[e], self.cnt[e])
        ins.then_inc(self.sem[e], 1)
        self.commit(e, ev, reads, writes)
        self.ninstr += 1
        return ev

    def dma(self, e, out, in_, reads=(), writes=(), nslots=8, **kw):
        if e == 'pool':
            nslots = 2
        lst = self.dma_sems.setdefault(e, [])
        if len(lst) < nslots:
            lst.append([self.nc.alloc_semaphore(f"d_{e}_{len(lst)}"), 0])
        i = self.dma_rr.get(e, 0)
        self.dma_rr[e] = (i + 1) % nslots
        slot = lst[i % len(lst)]
        sem, uses = slot
        if uses > 0:
            self._wait(e, ('dma', sem, 16 * uses))
        self.deps(e, reads, writes)
        self.E[e].dma_start(out=out, in_=in_, **kw).then_inc(sem, 16)
        slot[1] = uses + 1
        ev = ('dma_%s_%d' % (e, i % len(lst)), sem, 16 * (uses + 1))
        self.commit(ev[0], ev, reads, writes)
        self.ninstr += 1
        return ev

    def barrier(self):
        self._flush_pe()
        evs = [(e, self.sem[e], self.cnt[e]) for e in self.E if self.cnt[e] > 0]
        for q, lst in self.dma_sems.items():
            for sem, uses in lst:
                if uses:
                    evs.append(('dma', sem, 16 * uses))
        for e in self.E:
            for ev in evs:
                if ev[0] != e:
                    self._wait(e, ev)
        self.lastw = {}
        self.reads = {}

    def finish(self, e='sp'):
        self._flush_pe()
        for q, lst in self.dma_sems.items():
            for sem, uses in lst:
                if uses:
                    self._wait(e, ('dma', sem, 16 * uses))


class Arena:
    def __init__(self, nc, name, nbytes):
        self.n4 = nbytes // 4
        self.t = nc.alloc_sbuf_tensor(name, [128, self.n4], F32).ap()
        self.ptr = 0
        self.hi = 0

    def seek(self, off):
        self.ptr = off

    def alloc(self, shape, dtype, parts=None):
        esz = 4 if dtype in (F32, I32) else 2
        n = int(np.prod(shape[1:]))
        nb = (n * esz + 31) // 32 * 32
        assert self.ptr % 4 == 0
        a = self.ptr // 4
        assert a + nb // 4 <= self.n4, f"arena overflow {self.ptr}+{nb} > {self.n4 * 4}"
        v = self.t[:, a:a + nb // 4]
        if dtype != F32:
            v = v.bitcast(dtype)
        v = v[0:shape[0], 0:n]
        if len(shape) > 2:
            names = " ".join(f"d{i}" for i in range(len(shape) - 1))
            kw = {f"d{i}": int(shape[i + 1]) for i in range(len(shape) - 1)}
            v = v.rearrange(f"p ({names}) -> p {names}", **kw)
        self.ptr += nb
        self.hi = max(self.hi, self.ptr)
        return v


def bc(ap, shape):
    return ap.to_broadcast(list(shape))


def build(nseq=NSEQ, dbg=None, stop_after=None):
    nc = bass.Bass("TRN2", target_bir_lowering=False)
    S = Sync(nc)
    V, ACT, POOL, PE = nc.vector, nc.scalar, nc.gpsimd, nc.tensor

    def din(name, shape, dt=F32):
        return nc.dram_tensor(name, list(shape), dt, kind="ExternalInput").ap()

    x_d = din("x", [nseq, T, D])
    cT_d = din("cT", [nseq, 128, 8])
    pos_d = din("pos", [nseq, 1, T], I32)
    wada_d = din("w_ada", [D, 6 * D])
    bada_d = din("b_ada", [1, 6 * D])
    n1g_d = din("norm1_g", [1, D])
    n2g_d = din("norm2_g", [1, D])
    win_d = din("w_in", [D, 4736])
    pp_d = din("pp", [128, NPP])
    w2c_d = din("w2cat", [128, 2 * 512])
    a2c_d = din("a2cat", [128, 2 * 512])
    g2_d = din("g2", [128, 512])
    gnw_d = din("gn_w", [1, 512])
    gnb_d = din("gn_b", [1, 512])
    prw_d = din("p_rwkv", [512, D])
    pat_d = din("p_attn", [512, D])
    wout_d = din("w_out", [D, D])
    wr_d = din("w_router", [D, E])
    wg_d = din("w_gate", [E, D, D])
    wu_d = din("w_up", [E, D, D])
    wd_d = din("w_down", [E, D, D])
    cm_d = din("cmats", [128, 13 * 128])
    out_d = nc.dram_tensor("out", [nseq, T, D], F32, kind="ExternalOutput").ap()
    mod_d = nc.dram_tensor("modscr", [nseq, 6, 128, D], F32, kind="Internal").ap()
    y_d = nc.dram_tensor("yscr", [2, T, 512], F32, kind="Internal").ap()
    u_d = nc.dram_tensor("uscr", [128, 8, T], BF16, kind="Internal").ap()
    dbg_d = None
    if dbg is not None:
        dbg_d = nc.dram_tensor("dbg", list(dbg[1]), F32, kind="ExternalOutput").ap()

    def sb(name, shape, dt=F32):
        return nc.alloc_sbuf_tensor('sb_' + name, list(shape), dt).ap()

    pp = sb("pp", [128, NPP])
    ident = sb("ident", [128, 128], BF16)
    identf = sb("identf", [128, 128])
    cmb = sb("cmb", [128, 13, 128], BF16)
    w2c = sb("w2c", [128, 2, 512], BF16)
    a2c = sb("a2c", [128, 2, 512], BF16)
    g2 = sb("g2", [128, 512], BF16)
    wr = sb("wr", [128, 8, E], BF16)
    epsc = sb("epsc", [128, 4])
    alpha = sb("alpha", [128, 15])
    oneminus_ka = sb("omka", [128, 4])
    two_omka = sb("omka2", [128, 4])
    negkkc = sb("negone", [128, 1])
    esk = sb("esk", [128, 4])
    rmask = sb("rmask", [128, TBS])
    iota_row = sb("iota_row", [128, CAP])
    ident4 = sb("ident4", [128, 4, 128], BF16)
    kar = sb("kar", [128, 4])
    c2r = sb("c2r", [128, 4])
    afftm = sb("afftm", [128, NT, E])
    slot_tm = sb("slot_tm", [128, NT, E])
    affhl = sb("affhl", [128, NT, E, 2], BF16)

    BLK1, ROT, MPREV, MNEXT = 0, 1, 2, 3
    MZT = (4, 5)
    MZ = (6, 7)
    HSEL = 8
    VP = (9, 10)

    ps = [nc.alloc_psum_tensor(f"ps{i}", [128, 512], F32).ap() for i in range(8)]
    psk = [f"ps{i}" for i in range(8)]

    AR = Arena(nc, "arena", 192 * 1024)

    def col(name, j=0, n=1):
        o, w = PP[name]
        return pp[:, o + j:o + j + n]

    S.dma('sp', pp, pp_d, writes=['pp'])
    S.dma('pool', cmb.rearrange("p a b -> p (a b)"), cm_d, writes=['cmb'])
    S.dma('pool', w2c.rearrange("p a b -> p (a b)"), w2c_d, writes=['w2c'])
    S.dma('pool', a2c.rearrange("p a b -> p (a b)"), a2c_d, writes=['a2c'])
    S.dma('pool', g2, g2_d, writes=['g2'])
    S.dma('pool', wr, wr_d.rearrange("(k p) e -> p k e", p=128), writes=['wr'])
    S.op('pool', lambda: POOL.memset(identf, 1.0), writes=['identf'])
    S.op('pool', lambda: POOL.affine_select(out=identf, in_=identf, pattern=[[1, 128]], compare_op=ALU.is_equal,
                                            fill=0.0, base=0, channel_multiplier=-1), reads=['identf'], writes=['identf'])
    S.op('dve', lambda: V.tensor_copy(out=ident, in_=identf), reads=['identf'], writes=['ident'])
    for j in range(4):
        S.op('dve', lambda: V.tensor_copy(out=ident4[:, j, :], in_=identf), reads=['identf'], writes=['ident4'])
    S.op('pool', lambda: POOL.memset(epsc[:, 0:1], 1e-6), writes=['epsc'])
    S.op('pool', lambda: POOL.memset(epsc[:, 1:2], 64e-5), reads=['epsc'], writes=['epsc'])
    S.op('pool', lambda: POOL.memset(epsc[:, 2:3], 1e-24), reads=['epsc'], writes=['epsc'])
    S.op('pool', lambda: POOL.memset(epsc[:, 3:4], 0.0), reads=['epsc'], writes=['epsc'])
    S.op('pool', lambda: POOL.memset(negkkc, -1.0), writes=['negone'])
    S.op('dve', lambda: V.tensor_tensor(out=alpha, in0=col("mp", 0, 15), in1=col("mn", 0, 15), op=ALU.add), reads=['pp'], writes=['alpha'])
    S.op('dve', lambda: V.tensor_scalar(out=alpha, in0=alpha, scalar1=-1.0, scalar2=1.0, op0=ALU.mult, op1=ALU.add), reads=['alpha'], writes=['alpha'])
    S.op('dve', lambda: V.tensor_scalar(out=oneminus_ka, in0=col("ka", 0, 4), scalar1=-1.0, scalar2=1.0, op0=ALU.mult, op1=ALU.add), reads=['pp'], writes=['omka'])
    S.op('dve', lambda: V.tensor_scalar(out=two_omka, in0=col("ka", 0, 4), scalar1=-2.0, scalar2=2.0, op0=ALU.mult, op1=ALU.add), reads=['pp'], writes=['omka2'])
    S.op('act', lambda: ACT.activation(out=esk, in_=col("sink", 0, 4), func=AF.Exp), reads=['pp'], writes=['esk'])
    S.op('dve', lambda: V.tensor_tensor(out=kar, in0=col("ka", 0, 4), in1=col("rk", 0, 4), op=ALU.mult), reads=['pp'], writes=['kar'])
    S.op('dve', lambda: V.tensor_tensor(out=c2r, in0=two_omka, in1=col("rk", 0, 4), op=ALU.mult), reads=['pp', 'omka2'], writes=['kar'])
    S.op('pool', lambda: POOL.memset(rmask, 1.0), writes=['rmask'])
    S.op('pool', lambda: POOL.memset(rmask.rearrange("p (c t) -> p c t", t=64)[:, :, 0:1], 0.0), reads=['rmask'], writes=['rmask'])
    S.op('pool', lambda: POOL.iota(iota_row, pattern=[[1, CAP]], base=0, channel_multiplier=0, allow_small_or_imprecise_dtypes=True), writes=['iota_row'])

    def debug_out(ap_sb, key, rows=None):
        S.dma('sp', dbg_d if rows is None else rows, ap_sb, reads=[key])

    def phase_adaln():
        AR.seek(0)
        csil = [AR.alloc([128, 8], F32) for _ in range(nseq)]
        crep = [AR.alloc([128, 9, 128], F32) for _ in range(nseq)]
        wblk = [AR.alloc([128, 9, 512], F32) for _ in range(3)]
        g1B = AR.alloc([128, D], F32)
        g2B = AR.alloc([128, D], F32)
        mt = [AR.alloc([128, 512], F32) for _ in range(4)]
        S.dma('sp', g1B, n1g_d.partition_broadcast(128), writes=['g1B'])
        S.dma('sp', g2B, n2g_d.partition_broadcast(128), writes=['g2B'])
        for b in range(3):
            S.op('pool', lambda: POOL.memset(wblk[b][:, 8, :], 0.0), writes=[('wblk', b)])
        for s in range(nseq):
            S.dma('sp', csil[s], cT_d[s], writes=[('csil', s)])
            S.op('act', lambda: ACT.activation(out=csil[s], in_=csil[s], func=AF.Silu), reads=[('csil', s)], writes=[('csil', s)])
            S.op('pool', lambda: POOL.memset(crep[s][:, 8, :], 0.0), writes=[('crep', s)])
            S.op('pool', lambda: POOL.memset(crep[s][0:1, 8, :], 1.0), reads=[('crep', s)], writes=[('crep', s)])
            S.op('dve', lambda: V.tensor_copy(out=crep[s][:, 0:8, :], in_=bc(csil[s].rearrange("p (k o) -> p k o", o=1), [128, 8, 128])),
                 reads=[('csil', s)], writes=[('crep', s)])
        ev = 0
        for jb in range(12):
            b = jb % 3
            piece = jb // 2
            c0 = jb * 512
            S.dma('sp', wblk[b][:, 0:4, :], wada_d[0:512, c0:c0 + 512].rearrange("(k p) n -> p k n", p=128), writes=[('wblk', b)])
            S.dma('act', wblk[b][:, 4:8, :], wada_d[512:1024, c0:c0 + 512].rearrange("(k p) n -> p k n", p=128), writes=[('wblk', b)])
            S.dma('sp', wblk[b][0:1, 8, :], bada_d[:, c0:c0 + 512], writes=[('wblk', b)])
            for s in range(nseq):
                pz, pkz = ps[ev % 4], psk[ev % 4]
                for k in range(9):
                    S.op('pe', lambda: PE.matmul(pz, lhsT=crep[s][:, k, :], rhs=wblk[b][:, k, :], start=(k == 0), stop=(k == 8)),
                         reads=[('crep', s), ('wblk', b)], writes=[pkz], pe_acc=True)
                m = mt[ev % 4]
                lc = (jb % 2) * 512
                if piece == 1:
                    S.op('dve', lambda: V.scalar_tensor_tensor(out=m, in0=pz, scalar=1.0, in1=g1B[:, lc:lc + 512], op0=ALU.add, op1=ALU.mult),
                         reads=[pkz, 'g1B'], writes=[('mt', ev % 4)])
                elif piece == 4:
                    S.op('dve', lambda: V.scalar_tensor_tensor(out=m, in0=pz, scalar=1.0, in1=g2B[:, lc:lc + 512], op0=ALU.add, op1=ALU.mult),
                         reads=[pkz, 'g2B'], writes=[('mt', ev % 4)])
                else:
                    S.op('act', lambda: ACT.copy(out=m, in_=pz), reads=[pkz], writes=[('mt', ev % 4)])
                S.dma('sp', mod_d[s, piece, :, lc:lc + 512], m, reads=[('mt', ev % 4)], writes=[('mod', s, piece)])
                ev += 1
        S.barrier()

    def phase_norm1(s, uT, base):
        AR.seek(base)
        scp = AR.alloc([128, D], F32)
        shp = AR.alloc([128, D], F32)
        xt = [AR.alloc([128, D], F32) for _ in range(2)]
        tmp2 = [AR.alloc([128, D], F32) for _ in range(2)]
        ub = [AR.alloc([128, D], BF16) for _ in range(2)]
        junk2 = [AR.alloc([128, D], BF16) for _ in range(2)]
        ss2 = [AR.alloc([128, 2], F32) for _ in range(2)]
        S.dma('sp', scp, mod_d[s, 1], reads=[('mod', s, 1)], writes=['scp'])
        S.dma('sp', shp, mod_d[s, 0], reads=[('mod', s, 0)], writes=['shp'])
        for i in range(NT):
            b = i % 2
            S.dma('sp', xt[b], x_d[s, i * 128:(i + 1) * 128, :], writes=[('xt', b)])
            tmp, junk, ss = tmp2[b], junk2[b], ss2[b]
            S.op('act', lambda: ACT.activation(out=junk, in_=xt[b], func=AF.Square, accum_out=ss[:, 0:1]), reads=[('xt', b)], writes=[('junk', b), ('ss', b)])
            S.op('act', lambda: ACT.activation(out=ss[:, 1:2], in_=ss[:, 0:1], func=AF.Sqrt, bias=epsc[:, 0:1], scale=1.0 / D), reads=[('ss', b), 'epsc'], writes=[('ss1', b)])
            S.op('dve', lambda: V.reciprocal(out=ss[:, 1:2], in_=ss[:, 1:2]), reads=[('ss1', b)], writes=[('ss1', b)])
            S.op('dve', lambda: V.scalar_tensor_tensor(out=tmp, in0=xt[b], scalar=ss[:, 1:2], in1=scp, op0=ALU.mult, op1=ALU.mult),
                 reads=[('xt', b), ('ss1', b), 'scp'], writes=[('tmp', b)])
            S.op('pool', lambda: POOL.tensor_tensor(out=ub[b], in0=tmp, in1=shp, op=ALU.add), reads=[('tmp', b), 'shp'], writes=[('ub', b)])
            pz = ps[i % 2].bitcast(BF16).rearrange("p (k t) -> p k t", k=8)
            for k in range(8):
                S.op('pe', lambda: PE.transpose(out=pz[:, k, :], in_=ub[b][:, k * 128:(k + 1) * 128], identity=ident),
                     reads=[('ub', b), 'ident'], writes=[psk[i % 2]], pe_acc=True)
            S.op('act', lambda: ACT.copy(out=uT[:, :, i * 128:(i + 1) * 128], in_=pz), reads=[psk[i % 2]], writes=[('uT', i // 4)])

    def phase_rwkv_cols(uT, zsT, base):
        AR.seek(base)
        wg = [AR.alloc([128, 8, 128], BF16) for _ in range(2)]
        ztmpP = [AR.alloc([128, T + 2], F32) for _ in range(2)]
        shtP = [AR.alloc([128, T], F32) for _ in range(2)]
        for q in range(2):
            S.op('pool', lambda: POOL.memset(ztmpP[q][:, 0:1], 0.0), writes=[('ztmp', q)])
            S.op('pool', lambda: POOL.memset(ztmpP[q][:, T + 1:T + 2], 0.0), reads=[('ztmp', q)], writes=[('ztmp', q)])
        for j in range(15):
            b = j % 2
            ztmp, sht = ztmpP[b], shtP[b]
            S.dma('pool', wg[b], win_d[:, j * 128:(j + 1) * 128].rearrange("(k p) n -> p k n", p=128), writes=[('wg', b)])
            for tb in range(NB):
                pz = ps[(j * NB + tb) % 4]
                pk = psk[(j * NB + tb) % 4]
                for k in range(8):
                    S.op('pe', lambda: PE.matmul(pz, lhsT=wg[b][:, k, :], rhs=uT[:, k, tb * 512:(tb + 1) * 512], start=(k == 0), stop=(k == 7)),
                         reads=[('wg', b), ('uT', tb)], writes=[pk], pe_acc=True)
                S.op('act', lambda: ACT.copy(out=ztmp[:, 1 + tb * 512:1 + (tb + 1) * 512], in_=pz), reads=[pk], writes=[('ztmp', b)])
            S.op('dve', lambda: V.tensor_scalar(out=sht, in0=ztmp[:, 1:T + 1], scalar1=alpha[:, j:j + 1], scalar2=None, op0=ALU.mult),
                 reads=[('ztmp', b), 'alpha'], writes=[('sht', b)])
            S.op('dve', lambda: V.scalar_tensor_tensor(out=sht, in0=ztmp[:, 0:T], scalar=col("mp", j), in1=sht, op0=ALU.mult, op1=ALU.add),
                 reads=[('ztmp', b), ('sht', b), 'pp'], writes=[('sht', b)])
            S.op('dve', lambda: V.scalar_tensor_tensor(out=zsT[:, j, :], in0=ztmp[:, 2:T + 2], scalar=col("mn", j), in1=sht, op0=ALU.mult, op1=ALU.add),
                 reads=[('ztmp', b), ('sht', b), 'pp'], writes=[('zs', j)])
            if j == 12:
                S.op('act', lambda: ACT.activation(out=zsT[:, j, :], in_=zsT[:, j, :], func=AF.Tanh), reads=[('zs', j)], writes=[('zs', j)])
            if j == 14:
                S.op('act', lambda: ACT.activation(out=zsT[:, j, :], in_=zsT[:, j, :], func=AF.Sigmoid), reads=[('zs', j)], writes=[('zs', j)])

    def phase_scan(zsT, kkT, base):
        rT = lambda c: zsT[:, c, :]
        kT = lambda c: zsT[:, 4 + c, :]
        vT = lambda c: zsT[:, 8 + c, :]
        wdT = zsT[:, 12, :]
        adT = zsT[:, 13, :]
        AR.seek(base)
        kraw = AR.alloc([128, 512], F32)
        ksq = AR.alloc([128, 512], BF16)
        krs = AR.alloc([128, 512], F32)
        for c in range(4):
            for tb in range(NB):
                sl = slice(tb * 512, (tb + 1) * 512)
                S.op('dve', lambda: V.tensor_scalar(out=kraw, in0=kT(c)[:, sl], scalar1=col("kk", c), scalar2=None, op0=ALU.mult), reads=[('zs', 4 + c), 'pp'], writes=['kraw'])
                S.op('act', lambda: ACT.activation(out=ksq, in_=kraw, func=AF.Square), reads=['kraw'], writes=['ksq'])
                pz, pk = ps[tb % 2], psk[tb % 2]
                S.op('pe', lambda: PE.matmul(pz, lhsT=cmb[:, BLK1, :], rhs=ksq, start=True, stop=True), reads=['ksq', 'cmb'], writes=[pk], pe_acc=True)
                S.op('act', lambda: ACT.activation(out=krs, in_=pz, func=AF.Sqrt, bias=epsc[:, 2:3], scale=1.0), reads=[pk, 'epsc'], writes=['krs'])
                S.op('dve', lambda: V.reciprocal(out=krs, in_=krs), reads=['krs'], writes=['krs'])
                S.op('dve', lambda: V.tensor_tensor(out=kkT[:, c, sl], in0=kraw, in1=krs, op=ALU.mult), reads=['kraw', 'krs'], writes=[('kk', c)])
        S.barrier()
        AR.seek(base)
        sg = AR.alloc([128, 4, TBS], F32)
        ad = AR.alloc([128, 4, TBS], F32)
        cc = AR.alloc([128, 4, TBS], F32)
        t1 = AR.alloc([128, 4, TBS], F32)
        ex = [[AR.alloc([128, TBS], F32) for _ in range(2)] for _ in range(4)]
        kd = AR.alloc([128, 4, TBS], F32)
        bb = AR.alloc([128, 4, TBS], F32)
        pdec = AR.alloc([128, 4, NCH], F32)
        ARz = AR.alloc([128, 4, NCH, 2, 2, 64], BF16)
        Bz = AR.alloc([128, 4, NCH, 2, 64], BF16)
        BKt = AR.alloc([128, 4, NCH, 2, 64], BF16)
        KBh = AR.alloc([128, 4, NCH, 2, 64], BF16)
        KBt = AR.alloc([128, 4, NCH, 128], BF16)
        VZ = AR.alloc([128, NCH, 8, 64], BF16)
        XV = AR.alloc([128, NCH, 8, 64], BF16)
        ZTs = [[AR.alloc([128, 4, 128], BF16) for _ in range(2)] for _ in range(NCH)]
        ATm = [[AR.alloc([128, 4, 128], BF16) for _ in range(2)] for _ in range(NCH)]
        PTm = [[AR.alloc([128, 4, 128], BF16) for _ in range(2)] for _ in range(2)]
        Pm = [[AR.alloc([128, 4, 128], BF16) for _ in range(2)] for _ in range(2)]
        Am = [[AR.alloc([128, 4, 128], BF16) for _ in range(2)] for _ in range(2)]
        W1s = AR.alloc([128, 4, 64], BF16)
        S32 = [AR.alloc([128, 4, 64], F32) for _ in range(2)]
        Sb = [AR.alloc([128, 4, 64], BF16) for _ in range(2)]
        ysb = [AR.alloc([64, 512], F32) for _ in range(2)]
        S.op('pool', lambda: POOL.memset(ARz.rearrange("p a b c d e -> p (a b c d e)"), 0.0), writes=['ARz'])
        S.op('pool', lambda: POOL.memset(Bz.rearrange("p a b c d -> p (a b c d)"), 0.0), writes=['Bz'])
        S.op('pool', lambda: POOL.memset(VZ.rearrange("p a b c -> p (a b c)"), 0.0), writes=['VZ'])

        def chain(gens):
            for g_ in gens:
                yield from g_

        def run_tasks(tasks):
            tasks = list(tasks)
            while tasks:
                for t_ in list(tasks):
                    try:
                        next(t_)
                    except StopIteration:
                        tasks.remove(t_)

        yev = 0
        pendQ = None
        for d in range(2):
            S.op('pool', lambda: POOL.memset(S32[d].rearrange("p a b -> p (a b)"), 0.0), writes=[('S32', d)])
            S.op('pool', lambda: POOL.memset(Sb[d].rearrange("p a b -> p (a b)"), 0.0), writes=[('Sb', d)])
            tbs = range(NTB) if d == 0 else range(NTB - 1, -1, -1)
            for tb in tbs:
                sl = slice(tb * TBS, (tb + 1) * TBS)
                def gen_prep(c):
                    pz, pk = ps[c % 2], psk[c % 2]
                    S.op('pe', lambda: PE.matmul(pz[:, 0:TBS], lhsT=w2c[:, d, c * 128:(c + 1) * 128], rhs=wdT[:, sl], start=True, stop=True),
                         reads=['w2c', ('zs', 12)], writes=[pk], pe_acc=True)
                    S.op('act', lambda: ACT.activation(out=sg[:, c, :], in_=pz[:, 0:TBS], func=AF.Sigmoid, bias=col("w0", d * 4 + c), scale=1.0),
                         reads=[pk, 'pp'], writes=[('sg', c)])
                    pz2, pk2 = ps[2 + c % 2], psk[2 + c % 2]
                    S.op('pe', lambda: PE.matmul(pz2[:, 0:TBS], lhsT=a2c[:, d, c * 128:(c + 1) * 128], rhs=adT[:, sl], start=True, stop=True),
                         reads=['a2c', ('zs', 13)], writes=[pk2], pe_acc=True)
                    S.op('act', lambda: ACT.activation(out=ad[:, c, :], in_=pz2[:, 0:TBS], func=AF.Sigmoid, bias=col("a0", d * 4 + c), scale=1.0),
                         reads=[pk2, 'pp'], writes=[('ad', c)])
                    yield
                    S.op('dve', lambda: V.tensor_tensor_scan(out=cc[:, c, :], data0=rmask, data1=sg[:, c, :], initial=0.0, op0=ALU.mult, op1=ALU.add),
                         reads=['rmask', ('sg', c)], writes=[('cc', c)])
                    cc3 = cc[:, c, :].rearrange("p (h t) -> p h t", t=64)
                    sg3 = sg[:, c, :].rearrange("p (h t) -> p h t", t=64)
                    t13 = t1[:, c, :].rearrange("p (h t) -> p h t", t=64)
                    if d == 1:
                        S.op('dve', lambda: V.tensor_tensor(out=t13, in0=bc(cc3[:, :, 63:64], [128, NCH, 64]), in1=cc3, op=ALU.subtract),
                             reads=[('cc', c)], writes=[('t1', c)])
                        S.op('dve', lambda: V.tensor_tensor(out=cc[:, c, :], in0=t1[:, c, :], in1=sg[:, c, :], op=ALU.add),
                             reads=[('t1', c), ('sg', c)], writes=[('cc', c)])
                    totp = 63 if d == 0 else 0
                    S.op('pool', lambda: POOL.tensor_scalar(out=kd[:, c, :], in0=ad[:, c, :], scalar1=col("ka", c), scalar2=oneminus_ka[:, c:c + 1], op0=ALU.mult, op1=ALU.add),
                         reads=[('ad', c), 'pp', 'omka'], writes=[('kd', c)])
                    S.op('pool', lambda: POOL.tensor_tensor(out=kd[:, c, :], in0=kd[:, c, :], in1=kT(c)[:, sl], op=ALU.mult),
                         reads=[('kd', c), ('zs', 4 + c)], writes=[('kd', c)])
                    S.op('pool', lambda: POOL.tensor_tensor(out=bb[:, c, :], in0=ad[:, c, :], in1=kkT[:, c, sl], op=ALU.mult),
                         reads=[('ad', c), ('kk', c)], writes=[('bb', c)])
                    yield
                    e = ex[c][0]
                    S.op('act', lambda: ACT.activation(out=e, in_=cc[:, c, :], func=AF.Exp, scale=-LAM), reads=[('cc', c)], writes=[('ex', c, 0)])
                    for hp in range(2):
                        pr = slice(hp * 64, (hp + 1) * 64)
                        S.op('dve', lambda: V.tensor_tensor(out=ARz[pr, c, :, 0, hp, :], in0=rT(c)[pr, sl].rearrange("p (h t) -> p h t", t=64),
                                                            in1=e[pr, :].rearrange("p (h t) -> p h t", t=64), op=ALU.mult),
                             reads=[('zs', c), ('ex', c, 0)], writes=['ARz'])
                    yield
                    e = ex[c][1]
                    S.op('act', lambda: ACT.activation(out=e, in_=cc[:, c, :], func=AF.Exp, scale=LAM), reads=[('cc', c)], writes=[('ex', c, 1)])
                    S.op('dve', lambda: V.tensor_tensor(out=BKt[:, c, :, 0, :], in0=kd[:, c, :].rearrange("p (h t) -> p h t", t=64),
                                                        in1=e.rearrange("p (h t) -> p h t", t=64), op=ALU.mult),
                         reads=[('kd', c), ('ex', c, 1)], writes=['BKt'])
                    S.op('dve', lambda: V.tensor_tensor(out=BKt[:, c, :, 1, :], in0=bb[:, c, :].rearrange("p (h t) -> p h t", t=64),
                                                        in1=e.rearrange("p (h t) -> p h t", t=64), op=ALU.mult),
                         reads=[('bb', c), ('ex', c, 1)], writes=['BKt'])
                    for hp in range(2):
                        pr = slice(hp * 64, (hp + 1) * 64)
                        S.op('act', lambda: ACT.copy(out=Bz[pr, c, :, hp, :], in_=BKt[pr, c, :, 1, :]), reads=['BKt'], writes=['Bz'])
                    yield
                    S.op('dve', lambda: V.tensor_tensor(out=t1[:, c, :], in0=cc[:, c, :], in1=sg[:, c, :], op=ALU.subtract),
                         reads=[('cc', c), ('sg', c)], writes=[('t1', c)])
                    e = ex[c][0]
                    S.op('act', lambda: ACT.activation(out=e, in_=t1[:, c, :], func=AF.Exp, scale=-LAM), reads=[('t1', c)], writes=[('ex', c, 0)])
                    for hp in range(2):
                        pr = slice(hp * 64, (hp + 1) * 64)
                        S.op('dve', lambda: V.scalar_tensor_tensor(out=ARz[pr, c, :, 1, hp, :], in0=kkT[pr, c, sl].rearrange("p (h t) -> p h t", t=64),
                                                                   scalar=-1.0, in1=e[pr, :].rearrange("p (h t) -> p h t", t=64), op0=ALU.mult, op1=ALU.mult),
                             reads=[('kk', c), ('ex', c, 0)], writes=['ARz'])
                    yield
                    S.op('dve', lambda: V.tensor_tensor(out=t13, in0=bc(cc3[:, :, totp:totp + 1], [128, NCH, 64]), in1=cc3, op=ALU.subtract),
                         reads=[('cc', c)], writes=[('t1', c)])
                    e = ex[c][1]
                    S.op('act', lambda: ACT.activation(out=e, in_=t1[:, c, :], func=AF.Exp, scale=-LAM), reads=[('t1', c)], writes=[('ex', c, 1)])
                    S.op('pool', lambda: POOL.tensor_tensor(out=KBh[:, c, :, 0, :], in0=kd[:, c, :].rearrange("p (h t) -> p h t", t=64),
                                                        in1=e.rearrange("p (h t) -> p h t", t=64), op=ALU.mult),
                         reads=[('kd', c), ('ex', c, 1)], writes=['KBh'])
                    S.op('pool', lambda: POOL.tensor_tensor(out=KBh[:, c, :, 1, :], in0=bb[:, c, :].rearrange("p (h t) -> p h t", t=64),
                                                        in1=e.rearrange("p (h t) -> p h t", t=64), op=ALU.mult),
                         reads=[('bb', c), ('ex', c, 1)], writes=['KBh'])
                    S.op('act', lambda: ACT.activation(out=pdec[:, c, :].rearrange("p (h o) -> p h o", o=1), in_=cc3[:, :, totp:totp + 1], func=AF.Exp, scale=-LAM), reads=[('cc', c)], writes=['pdec'])
                ptasks = [gen_prep(c_) for c_ in range(4)]
                for t_ in ptasks:
                    next(t_)
                if pendQ is not None:
                    for _ in range(3):
                        next(pendQ, None)
                for t_ in ptasks:
                    next(t_)
                if pendQ is not None:
                    run_tasks([pendQ])
                    pendQ = None
                run_tasks(ptasks)
                for ch in range(NCH):
                    pz = ps[4 + ch % 2].bitcast(BF16)
                    pk = psk[4 + ch % 2]
                    pzv = pz[0:64, 0:512].rearrange("p (c n) -> p c n", c=4)
                    for c in range(4):
                        S.op('pe', lambda: PE.transpose(out=pzv[:, c, :], in_=vT(c)[:, tb * TBS + ch * 64: tb * TBS + (ch + 1) * 64], identity=ident),
                             reads=[('zs', 8 + c), 'ident'], writes=[pk], pe_acc=True)
                    S.op('act', lambda: ACT.copy(out=VZ[0:64, ch, :, :].rearrange("p h v -> p (h v)"), in_=pz[0:64, 0:512]), reads=[pk], writes=[('VZ', ch)])
                    S.op('act', lambda: ACT.copy(out=XV[0:64, ch, :, :].rearrange("p h v -> p (h v)"), in_=pz[0:64, 0:512]), reads=[pk], writes=[('XVv', ch)])
                    pzk = pz[:, 512:1024].rearrange("p (c n) -> p c n", c=4)
                    for c in range(4):
                        S.op('pe', lambda: PE.transpose(out=pzk[:, c, :], in_=KBh[:, c, ch, :, :].rearrange("p a t -> p (a t)"), identity=ident),
                             reads=['KBh', 'ident'], writes=[pk], pe_acc=True)
                    S.op('dve', lambda: V.tensor_copy(out=KBt[:, :, ch, :], in_=pzk), reads=[pk], writes=[('KBt', ch)])
                MNT = cmb[:, 11 + d, :]
                MN = cmb[:, 12 - d, :]

                def gen_D(ch, slot, par):
                    pA, pkA = ps[2 * par], psk[2 * par]
                    pB, pkB = ps[2 * par + 1], psk[2 * par + 1]
                    pA3 = pA.rearrange("p (j n) -> p j n", j=4)
                    pB3 = pB.rearrange("p (j n) -> p j n", j=4)
                    mzt = cmb[:, MZT[d], :]
                    for half in range(2):
                        pz3 = pA3 if half == 0 else pB3
                        pkz = pkA if half == 0 else pkB
                        for j in range(4):
                            h = half * 4 + j
                            c, hp = h // 2, h % 2
                            bk = BKt[:, c, ch, :, :].rearrange("p a t -> p (a t)")
                            S.op('pe', lambda: PE.matmul(pz3[:, j, :].rearrange("p (a t) -> p a t", a=2), lhsT=bk, rhs=ARz[:, c, ch, :, hp, :], start=True, stop=True),
                                 reads=['BKt', 'ARz'], writes=[pkz], pe_acc=True)
                        S.op('dve', lambda: V.tensor_tensor(out=ZTs[slot][half], in0=pz3, in1=bc(mzt.rearrange("p (o n) -> p o n", o=1), [128, 4, 128]), op=ALU.mult),
                             reads=[pkz, 'cmb'], writes=[('ZTs', slot, half)])
                    yield
                    for c in range(4):
                        bz = Bz[:, c, ch, :, :].rearrange("p a t -> p (a t)")
                        az = ARz[:, c, ch, 1, :, :].rearrange("p a t -> p (a t)")
                        S.op('pe', lambda: PE.matmul(pA3[:, c, :], lhsT=bz, rhs=az, start=True, stop=True), reads=['Bz', 'ARz'], writes=[pkA], pe_acc=True)
                        S.op('pe', lambda: PE.matmul(pB3[:, c, :], lhsT=az, rhs=bz, start=True, stop=True), reads=['Bz', 'ARz'], writes=[pkB], pe_acc=True)
                    S.op('dve', lambda: V.tensor_tensor(out=PTm[par][0], in0=pA3, in1=bc(MNT.rearrange("p (o n) -> p o n", o=1), [128, 4, 128]), op=ALU.mult),
                         reads=[pkA, 'cmb'], writes=[('PT', par, 0)])
                    S.op('dve', lambda: V.tensor_tensor(out=Pm[par][0], in0=pB3, in1=bc(MN.rearrange("p (o n) -> p o n", o=1), [128, 4, 128]), op=ALU.mult),
                         reads=[pkB, 'cmb'], writes=[('P', par, 0)])
                    S.op('pool', lambda: POOL.tensor_tensor(out=ATm[slot][0], in0=PTm[par][0], in1=ident4, op=ALU.add), reads=[('PT', par, 0), 'ident4'], writes=[('AT', slot, 0)])
                    S.op('pool', lambda: POOL.tensor_tensor(out=Am[par][0], in0=Pm[par][0], in1=ident4, op=ALU.add), reads=[('P', par, 0), 'ident4'], writes=[('A', par, 0)])
                    yield
                    cur = 0
                    for lev in range(1, 6):
                        nxt = 1 - cur
                        for j in range(4):
                            S.op('pe', lambda: PE.matmul(pA3[:, j, :], lhsT=Pm[par][cur][:, j, :], rhs=PTm[par][cur][:, j, :], start=True, stop=True),
                                 reads=[('P', par, cur), ('PT', par, cur)], writes=[pkA], pe_acc=True)
                            if lev < 5:
                                S.op('pe', lambda: PE.matmul(pB3[:, j, :], lhsT=PTm[par][cur][:, j, :], rhs=Pm[par][cur][:, j, :], start=True, stop=True),
                                     reads=[('P', par, cur), ('PT', par, cur)], writes=[pkB], pe_acc=True)
                        S.op('act', lambda: ACT.copy(out=PTm[par][nxt], in_=pA3), reads=[pkA], writes=[('PT', par, nxt)])
                        if lev < 5:
                            S.op('dve', lambda: V.tensor_copy(out=Pm[par][nxt], in_=pB3), reads=[pkB], writes=[('P', par, nxt)])
                        yield
                        for j in range(4):
                            S.op('pe', lambda: PE.matmul(pA3[:, j, :], lhsT=Am[par][cur][:, j, :], rhs=PTm[par][nxt][:, j, :], start=True, stop=True),
                                 reads=[('A', par, cur), ('PT', par, nxt)], writes=[pkA], pe_acc=True)
                            if lev < 5:
                                S.op('pe', lambda: PE.matmul(pB3[:, j, :], lhsT=PTm[par][nxt][:, j, :], rhs=Am[par][cur][:, j, :], start=True, stop=True),
                                     reads=[('A', par, cur), ('PT', par, nxt)], writes=[pkB], pe_acc=True)
                        S.op('dve', lambda: V.tensor_tensor(out=ATm[slot][nxt], in0=pA3, in1=ATm[slot][cur], op=ALU.add), reads=[pkA, ('AT', slot, cur)], writes=[('AT', slot, nxt)])
                        if lev < 5:
                            S.op('act', lambda: ACT.copy(out=Am[par][nxt], in_=pB3), reads=[pkB], writes=[('A', par, nxt)])
                            S.op('pool', lambda: POOL.tensor_tensor(out=Am[par][nxt], in0=Am[par][nxt], in1=Am[par][cur], op=ALU.add),
                                 reads=[('A', par, nxt), ('A', par, cur)], writes=[('A', par, nxt)])
                        yield
                        cur = nxt
                    assert cur == 1

                def gen_Q(ch, slot, tb=tb, d=d):
                    nonlocal yev
                    fin = 1
                    gch = tb * NCH + ch
                    pW, pkW = ps[4], psk[4]
                    pW3 = pW[:, 0:256].rearrange("p (c v) -> p c v", c=4)
                    for h in range(8):
                        c, hp = h // 2, h % 2
                        S.op('pe', lambda: PE.matmul(pW3[hp * 64:(hp + 1) * 64, c, :], lhsT=ZTs[slot][h // 4][:, h % 4, 64:128], rhs=VZ[:, ch, h, :], start=True, stop=False),
                             reads=[('ZTs', slot, h // 4), ('VZ', ch)], writes=[pkW], pe_acc=True)
                        S.op('pe', lambda: PE.matmul(pW3[hp * 64:(hp + 1) * 64, c, :], lhsT=ARz[:, c, ch, 1, hp, :], rhs=Sb[d][:, c, :], start=False, stop=True),
                             reads=['ARz', ('Sb', d)], writes=[pkW], pe_acc=True)
                    S.op('act', lambda: ACT.copy(out=W1s, in_=pW3), reads=[pkW], writes=['W1s'])
                    yield
                    pX, pkX = ps[5], psk[5]
                    pX3 = pX.rearrange("p (h v) -> p h v", h=8)
                    for h in range(8):
                        c, hp = h // 2, h % 2
                        S.op('pe', lambda: PE.matmul(pX3[64:128, h, :], lhsT=ATm[slot][fin][:, c, hp * 64:(hp + 1) * 64], rhs=W1s[:, c, :], start=True, stop=True),
                             reads=[('AT', slot, fin), 'W1s'], writes=[pkX], pe_acc=True)
                    S.op('dve', lambda: V.tensor_copy(out=XV[64:128, ch, :, :], in_=pX3[64:128]), reads=[pkX], writes=[('XVu', ch)])
                    yield
                    pS, pkS = ps[7], psk[7]
                    pS3 = pS[:, 0:256].rearrange("p (c v) -> p c v", c=4)
                    for h in range(8):
                        c, hp = h // 2, h % 2
                        S.op('pe', lambda: PE.matmul(pS3[hp * 64:(hp + 1) * 64, c, :], lhsT=KBt[:, c, ch, hp * 64:(hp + 1) * 64], rhs=XV[:, ch, h, :], start=True, stop=True),
                             reads=[('KBt', ch), ('XVv', ch), ('XVu', ch)], writes=[pkS], pe_acc=True)
                    pY, pkY = ps[6], psk[6]
                    pY3 = pY.rearrange("p (h v) -> p h v", h=8)
                    for h in range(8):
                        c, hp = h // 2, h % 2
                        S.op('pe', lambda: PE.matmul(pY3[0:64, h, :], lhsT=ZTs[slot][h // 4][:, h % 4, 0:64], rhs=XV[:, ch, h, :], start=True, stop=False),
                             reads=[('ZTs', slot, h // 4), ('XVv', ch), ('XVu', ch)], writes=[pkY], pe_acc=True)
                        S.op('pe', lambda: PE.matmul(pY3[0:64, h, :], lhsT=ARz[:, c, ch, 0, hp, :], rhs=Sb[d][:, c, :], start=False, stop=True),
                             reads=['ARz', ('Sb', d)], writes=[pkY], pe_acc=True)
                    for c in range(4):
                        S.op('dve', lambda: V.scalar_tensor_tensor(out=S32[d][:, c, :], in0=S32[d][:, c, :], scalar=pdec[:, c, ch:ch + 1], in1=pS3[:, c, :], op0=ALU.mult, op1=ALU.add),
                             reads=[('S32', d), 'pdec', pkS], writes=[('S32', d)])
                    S.op('act', lambda: ACT.copy(out=Sb[d], in_=S32[d]), reads=[('S32', d)], writes=[('Sb', d)])
                    yb_ = ysb[yev % 2]
                    S.op('act', lambda: ACT.copy(out=yb_, in_=pY[0:64, :]), reads=[pkY], writes=[('ysb', yev % 2)])
                    S.dma('sp', y_d[d, gch * 64:(gch + 1) * 64, :], yb_, reads=[('ysb', yev % 2)], writes=[('yscr', d, gch // 2)])
                    yev += 1
                    yield

                chs = list(range(NCH)) if d == 0 else list(range(NCH - 1, -1, -1))
                pend = []
                for r in range(0, NCH, 2):
                    tasks = [gen_D(chs[r], r, 0), gen_D(chs[r + 1], r + 1, 1)]
                    if pend:
                        tasks.append(chain([gen_Q(c_, s_) for (c_, s_) in pend]))
                    run_tasks(tasks)
                    pend = [(chs[r], r), (chs[r + 1], r + 1)]
                pendQ = chain([gen_Q(c_, s_) for (c_, s_) in pend])
        if pendQ is not None:
            run_tasks([pendQ])
            pendQ = None
        S.barrier()


    def phase_post(zsT, yaT, base):
        rT4 = zsT[:, 0:4, :]
        kT4 = zsT[:, 4:8, :]
        adT = zsT[:, 13, :]
        gdT = zsT[:, 14, :]
        AR.seek(base)
        gnwB = AR.alloc([128, 512], F32)
        gnbB = AR.alloc([128, 512], F32)
        P2 = lambda shape, dt: [AR.alloc(shape, dt) for _ in range(2)]
        Yf, Yb = P2([128, 512], F32), P2([128, 512], F32)
        ta0, ta1 = P2([128, 4, 128], F32), P2([128, 4, 128], F32)
        kf2 = P2([128, 4, 128], F32)
        prod2 = P2([128, 4, 128], BF16)
        rows2 = P2([128, 8], F32)
        bon2 = P2([128, 512], F32)
        y2 = P2([128, 512], F32)
        sq2 = P2([128, 512], F32)
        st2 = P2([128, 4, 8], F32)
        yab2 = P2([128, 512], BF16)
        S.dma('sp', gnwB, gnw_d.partition_broadcast(128), writes=['gnwB'])
        S.dma('sp', gnbB, gnb_d.partition_broadcast(128), writes=['gnbB'])
        def gen_tile(i):
            b = i % 2
            sl = slice(i * 128, (i + 1) * 128)
            ta = (ta0[b], ta1[b])
            kf, prod, rows, bon, y, sq, st, yab = kf2[b], prod2[b], rows2[b], bon2[b], y2[b], sq2[b], st2[b], yab2[b]
            bA, bB, bC, bD = 4 * b, 4 * b + 1, 4 * b + 2, 4 * b + 3
            S.dma('sp', Yf[b], y_d[0, sl, :], writes=[('Yf', b)])
            S.dma('sp', Yb[b], y_d[1, sl, :], writes=[('Yb', b)])
            for d in range(2):
                bk_ = bA if d == 0 else bB
                pz3 = ps[bk_].rearrange("p (c n) -> p c n", c=4)
                for c in range(4):
                    S.op('pe', lambda: PE.matmul(pz3[:, c, :], lhsT=a2c[:, d, c * 128:(c + 1) * 128], rhs=adT[:, sl], start=True, stop=True),
                         reads=['a2c'], writes=[psk[bk_]], pe_acc=True)
                for c in range(4):
                    S.op('act', lambda: ACT.activation(out=ta[d][:, c, :], in_=pz3[:, c, :], func=AF.Sigmoid, bias=col("a0", d * 4 + c), scale=1.0),
                         reads=[psk[bk_], 'pp'], writes=[('ta', b, d)])
            yield
            S.op('pool', lambda: POOL.tensor_tensor(out=ta[0], in0=ta[0], in1=ta[1], op=ALU.add), reads=[('ta', b, 0), ('ta', b, 1)], writes=[('ta', b, 0)])
            for c in range(4):
                S.op('pool', lambda: POOL.tensor_scalar(out=kf[:, c, :], in0=ta[0][:, c, :], scalar1=kar[:, c:c + 1], scalar2=c2r[:, c:c + 1], op0=ALU.mult, op1=ALU.add),
                     reads=[('ta', b, 0), 'kar'], writes=[('kf', b)])
            S.op('pool', lambda: POOL.tensor_tensor(out=kf, in0=kf, in1=kT4[:, :, sl], op=ALU.mult), reads=[('kf', b)], writes=[('kf', b)])
            S.op('pool', lambda: POOL.tensor_tensor(out=prod, in0=kf, in1=rT4[:, :, sl], op=ALU.mult), reads=[('kf', b)], writes=[('prod', b)])
            yield
            pr = ps[bB]
            for c in range(4):
                S.op('pe', lambda: PE.matmul(pr[:, c * 2:(c + 1) * 2], lhsT=prod[:, c, :], rhs=cmb[:, HSEL, 0:2], start=True, stop=True),
                     reads=[('prod', b), 'cmb'], writes=[psk[bB]], pe_acc=True)
            S.op('act', lambda: ACT.copy(out=rows, in_=pr[:, 0:8]), reads=[psk[bB]], writes=[('rows', b)])
            yield
            pv = ps[bD].bitcast(BF16)[:, 0:512]
            for c in range(4):
                S.op('pe', lambda: PE.transpose(out=pv[:, c * 128:(c + 1) * 128], in_=zsT[:, 8 + c, sl], identity=ident),
                     reads=['ident'], writes=[psk[bD]], pe_acc=True)
            S.op('dve', lambda: V.tensor_tensor(out=bon.rearrange("p (h v) -> p h v", h=8), in0=pv.rearrange("p (h v) -> p h v", h=8),
                                                in1=bc(rows.rearrange("p (h o) -> p h o", o=1), [128, 8, 64]), op=ALU.mult),
                 reads=[psk[bD], ('rows', b)], writes=[('bon', b)])
            yield
            pg = ps[bC]
            S.op('pe', lambda: PE.matmul(pg, lhsT=gdT[:, sl], rhs=g2, start=True, stop=True), reads=['g2'], writes=[psk[bC]], pe_acc=True)
            y3 = y.rearrange("p (h v) -> p h v", h=8)
            sq3 = sq.rearrange("p (h v) -> p h v", h=8)
            S.op('dve', lambda: V.tensor_tensor(out=y, in0=Yf[b], in1=Yb[b], op=ALU.add), reads=[('Yf', b), ('Yb', b)], writes=[('y', b)])
            yield
            S.op('dve', lambda: V.tensor_reduce(out=st[:, 0, :], in_=y3, axis=AX.X, op=ALU.add), reads=[('y', b)], writes=[('st0', b)])
            S.op('dve', lambda: V.tensor_scalar(out=st[:, 1, :], in0=st[:, 0, :], scalar1=-1.0 / 64, scalar2=None, op0=ALU.mult), reads=[('st0', b)], writes=[('st1', b)])
            S.op('dve', lambda: V.tensor_tensor(out=y3, in0=y3, in1=bc(st[:, 1, :].rearrange("p (h o) -> p h o", o=1), [128, 8, 64]), op=ALU.add),
                 reads=[('y', b), ('st1', b)], writes=[('y', b)])
            yield
            S.op('act', lambda: ACT.activation(out=sq, in_=y, func=AF.Square), reads=[('y', b)], writes=[('sq', b)])
            S.op('dve', lambda: V.tensor_reduce(out=st[:, 2, :], in_=sq3, axis=AX.X, op=ALU.add), reads=[('sq', b)], writes=[('st2', b)])
            yield
            S.op('act', lambda: ACT.activation(out=st[:, 3, :], in_=st[:, 2, :], func=AF.Sqrt, bias=epsc[:, 1:2], scale=1.0 / 64), reads=[('st2', b), 'epsc'], writes=[('st3', b)])
            S.op('dve', lambda: V.reciprocal(out=st[:, 3, :], in_=st[:, 3, :]), reads=[('st3', b)], writes=[('st3', b)])
            S.op('dve', lambda: V.tensor_tensor(out=y3, in0=y3, in1=bc(st[:, 3, :].rearrange("p (h o) -> p h o", o=1), [128, 8, 64]), op=ALU.mult),
                 reads=[('y', b), ('st3', b)], writes=[('y', b)])
            yield
            S.op('dve', lambda: V.tensor_tensor(out=y, in0=y, in1=gnwB, op=ALU.mult), reads=[('y', b), 'gnwB'], writes=[('y', b)])
            S.op('pool', lambda: POOL.tensor_tensor(out=bon, in0=bon, in1=gnbB, op=ALU.add), reads=[('bon', b), 'gnbB'], writes=[('bon', b)])
            S.op('dve', lambda: V.tensor_tensor(out=y, in0=y, in1=bon, op=ALU.add), reads=[('y', b), ('bon', b)], writes=[('y', b)])
            S.op('dve', lambda: V.tensor_tensor(out=yab, in0=y, in1=pg, op=ALU.mult), reads=[('y', b), psk[bC]], writes=[('yab', b)])
            yield
            pt = ps[bA].bitcast(BF16)[:, 0:512]
            for c in range(4):
                S.op('pe', lambda: PE.transpose(out=pt[:, c * 128:(c + 1) * 128], in_=yab[:, c * 128:(c + 1) * 128], identity=ident),
                     reads=[('yab', b), 'ident'], writes=[psk[bA]], pe_acc=True)
            S.op('act', lambda: ACT.copy(out=yaT[:, :, sl], in_=pt.rearrange("p (c n) -> p c n", c=4)), reads=[psk[bA]], writes=['yaT'])
            yield

        def run_tasks(tasks):
            tasks = list(tasks)
            while tasks:
                for t_ in list(tasks):
                    try:
                        next(t_)
                    except StopIteration:
                        tasks.remove(t_)

        for i in range(0, NT, 2):
            run_tasks([gen_tile(i), gen_tile(i + 1)])

    def phase_attn(s, uT, ybT, baseA, baseB):
        AR.seek(baseA)
        cosT = AR.alloc([128, T], F32)
        sinT = AR.alloc([128, T], F32)
        qT = AR.alloc([128, 4, T], BF16)
        kTt = AR.alloc([128, T], BF16)
        vp = AR.alloc([128, 2, NT, 128], BF16)
        AR.seek(baseB)
        wq = [AR.alloc([128, 8, 128], BF16) for _ in range(2)]
        qfL = [AR.alloc([128, 512], F32) for _ in range(2)]
        sqbL = [AR.alloc([128, 512], BF16) for _ in range(2)]
        rsL = [AR.alloc([128, 512], F32) for _ in range(2)]
        qnL = [AR.alloc([128, 512], F32) for _ in range(2)]
        qnbL = [AR.alloc([128, 512], BF16) for _ in range(2)]
        t1L = [AR.alloc([128, 512], F32) for _ in range(2)]
        t2L = [AR.alloc([128, 512], F32) for _ in range(2)]
        pTs = [AR.alloc([128, 512], BF16) for _ in range(6)]
        dn = AR.alloc([128, 512], F32)
        posi = AR.alloc([128, T], I32)
        ang = AR.alloc([128, T], F32)
        ki = AR.alloc([128, T], I32)
        kf = AR.alloc([128, T], F32)
        m1 = AR.alloc([128, T], F32)
        S.dma('sp', posi, pos_d[s].partition_broadcast(128), writes=['posi'])

        def table(dst, shift):
            S.op('dve', lambda: V.tensor_copy(out=ang, in_=posi), reads=['posi'], writes=['ang'])
            S.op('dve', lambda: V.tensor_scalar(out=ang, in0=ang, scalar1=col("invf"), scalar2=shift, op0=ALU.mult, op1=ALU.add), reads=['ang', 'pp'], writes=['ang'])
            S.op('dve', lambda: V.tensor_scalar(out=ki, in0=ang, scalar1=1.0 / TWO_PI, scalar2=None, op0=ALU.mult), reads=['ang'], writes=['ki'])
            S.op('pool', lambda: POOL.tensor_copy(out=kf, in_=ki), reads=['ki'], writes=['kf'])
            S.op('dve', lambda: V.scalar_tensor_tensor(out=ang, in0=kf, scalar=-C1, in1=ang, op0=ALU.mult, op1=ALU.add), reads=['kf', 'ang'], writes=['ang'])
            S.op('dve', lambda: V.scalar_tensor_tensor(out=ang, in0=kf, scalar=-C2, in1=ang, op0=ALU.mult, op1=ALU.add), reads=['kf', 'ang'], writes=['ang'])
            S.op('dve', lambda: V.tensor_scalar(out=m1, in0=ang, scalar1=float(np.pi), scalar2=-TWO_PI, op0=ALU.is_gt, op1=ALU.mult), reads=['ang'], writes=['m1'])
            S.op('pool', lambda: POOL.tensor_tensor(out=ang, in0=ang, in1=m1, op=ALU.add), reads=['ang', 'm1'], writes=['ang'])
            S.op('dve', lambda: V.tensor_scalar(out=m1, in0=ang, scalar1=float(-np.pi), scalar2=TWO_PI, op0=ALU.is_lt, op1=ALU.mult), reads=['ang'], writes=['m1'])
            S.op('pool', lambda: POOL.tensor_tensor(out=ang, in0=ang, in1=m1, op=ALU.add), reads=['ang', 'm1'], writes=['ang'])
            S.op('act', lambda: ACT.activation(out=dst, in_=ang, func=AF.Sin), reads=['ang'], writes=['tab'])

        table(sinT, 0.0)
        table(cosT, float(np.pi / 2))
        def c0_of(c):
            return 1920 + c * 128 if c < 4 else 2432

        def gen_qk(c, tb, L):
            b = c % 2
            gcol = col("qg") if c < 4 else col("kg")
            sl = slice(tb * 512, (tb + 1) * 512)
            qf_, sqb_, rs_, qn_, qnb_, t1_, t2_ = qfL[L], sqbL[L], rsL[L], qnL[L], qnbL[L], t1L[L], t2L[L]
            pz, pk = ps[L], psk[L]
            for k in range(8):
                S.op('pe', lambda: PE.matmul(pz, lhsT=wq[b][:, k, :], rhs=uT[:, k, sl], start=(k == 0), stop=(k == 7)),
                     reads=[('wq', b), ('uT', tb)], writes=[pk], pe_acc=True)
            S.op('act', lambda: ACT.copy(out=qf_, in_=pz), reads=[pk], writes=[('qf', L)])
            S.op('act', lambda: ACT.activation(out=sqb_, in_=qf_, func=AF.Square), reads=[('qf', L)], writes=[('sqb', L)])
            yield
            pr, pkr = ps[2 + L], psk[2 + L]
            S.op('pe', lambda: PE.matmul(pr, lhsT=cmb[:, BLK1, :], rhs=sqb_, start=True, stop=True), reads=[('sqb', L), 'cmb'], writes=[pkr], pe_acc=True)
            S.op('act', lambda: ACT.activation(out=rs_, in_=pr, func=AF.Sqrt, bias=epsc[:, 0:1], scale=1.0 / 64), reads=[pkr, 'epsc'], writes=[('rs', L)])
            yield
            S.op('dve', lambda: V.reciprocal(out=rs_, in_=rs_), reads=[('rs', L)], writes=[('rs', L)])
            S.op('dve', lambda: V.scalar_tensor_tensor(out=qn_, in0=qf_, scalar=gcol, in1=rs_, op0=ALU.mult, op1=ALU.mult), reads=[('qf', L), ('rs', L), 'pp'], writes=[('qn', L)])
            S.op('act', lambda: ACT.copy(out=qnb_, in_=qn_), reads=[('qn', L)], writes=[('qnb', L)])
            yield
            pro, pkro = ps[4 + L], psk[4 + L]
            S.op('pe', lambda: PE.matmul(pro, lhsT=cmb[:, ROT, :], rhs=qnb_, start=True, stop=True), reads=[('qnb', L), 'cmb'], writes=[pkro], pe_acc=True)
            S.op('pool', lambda: POOL.tensor_tensor(out=t1_, in0=qn_, in1=cosT[:, sl], op=ALU.mult), reads=[('qn', L), 'tab'], writes=[('t1', L)])
            yield
            S.op('dve', lambda: V.tensor_tensor(out=t2_, in0=pro, in1=sinT[:, sl], op=ALU.mult), reads=[pkro, 'tab'], writes=[('t2', L)])
            dst = qT[:, c, sl] if c < 4 else kTt[:, sl]
            S.op('dve', lambda: V.tensor_tensor(out=dst, in0=t1_, in1=t2_, op=ALU.add), reads=[('t1', L), ('t2', L)], writes=['qk'])
            yield

        def run_tasks(tasks):
            tasks = list(tasks)
            while tasks:
                for t_ in list(tasks):
                    try:
                        next(t_)
                    except StopIteration:
                        tasks.remove(t_)

        S.dma('pool', wq[0], win_d[:, c0_of(0):c0_of(0) + 128].rearrange("(k p) n -> p k n", p=128), writes=[('wq', 0)])
        for c in range(5):
            if c + 1 < 5:
                S.dma('pool', wq[(c + 1) % 2], win_d[:, c0_of(c + 1):c0_of(c + 1) + 128].rearrange("(k p) n -> p k n", p=128), writes=[('wq', (c + 1) % 2)])
            for tb in range(0, NB, 2):
                run_tasks([gen_qk(c, tb, 0), gen_qk(c, tb + 1, 1)])
        S.op('pool', lambda: POOL.memset(vp.rearrange("p a b c -> p (a b c)"), 0.0), writes=['vp'])
        S.dma('pool', wq[0], win_d[:, 2560:2688].rearrange("(k p) n -> p k n", p=128), writes=[('wq', 0)])
        for i in range(NT):
            pz, pk = ps[i % 2], psk[i % 2]
            for k in range(8):
                S.op('pe', lambda: PE.matmul(pz[:, 0:128], lhsT=uT[:, k, i * 128:(i + 1) * 128], rhs=wq[0][:, k, :], start=(k == 0), stop=(k == 7)),
                     reads=[('wq', 0), ('uT', i // 4)], writes=[pk], pe_acc=True)
            S.op('act', lambda: ACT.copy(out=vp[:, 0, i, 0:64], in_=pz[:, 0:64]), reads=[pk], writes=['vp'])
            S.op('dve', lambda: V.tensor_copy(out=vp[:, 1, i, 64:128], in_=pz[:, 64:128]), reads=[pk], writes=['vp'])
        for n in range(NT):
            qs = slice(n * 128, (n + 1) * 128)
            kbs = [kb for kb in (n - 1, n, n + 1) if 0 <= kb < NT]
            items = [(g, kb) for g in range(2) for kb in kbs]
            for idx, (g, kb) in enumerate(items):
                gp = slice(g * 64, (g + 1) * 64)
                pz, pk = ps[idx % 4], psk[idx % 4]
                S.op('pe', lambda: PE.matmul(pz.rearrange("p (j q) -> p j q", j=4), lhsT=kTt[gp, kb * 128:(kb + 1) * 128], rhs=qT[gp, :, qs], start=True, stop=True),
                     reads=['qk'], writes=[pk], pe_acc=True)
                pt_ = pTs[idx]
                S.op('act', lambda: ACT.activation(out=pt_, in_=pz, func=AF.Exp, scale=0.125), reads=[pk], writes=[('pT', idx)])
                if kb != n:
                    mk = cmb[:, MPREV if kb < n else MNEXT, :]
                    S.op('pool', lambda: POOL.tensor_tensor(out=pt_.rearrange("p (j q) -> p j q", j=4), in0=pt_.rearrange("p (j q) -> p j q", j=4),
                                                            in1=bc(mk.rearrange("p (o q) -> p o q", o=1), [128, 4, 128]), op=ALU.mult),
                         reads=[('pT', idx), 'cmb'], writes=[('pT', idx)])
            po, pko = ps[4 + n % 2], psk[4 + n % 2]
            pd_, pkd = ps[6 + n % 2], psk[6 + n % 2]
            for idx, (g, kb) in enumerate(items):
                S.op('pe', lambda: PE.matmul(po, lhsT=vp[:, g, kb, :], rhs=pTs[idx], start=(idx == 0), stop=(idx == len(items) - 1)),
                     reads=['vp', ('pT', idx)], writes=[pko], pe_acc=True)
            for idx, (g, kb) in enumerate(items):
                S.op('pe', lambda: PE.matmul(pd_, lhsT=cmb[:, VP[g], :], rhs=pTs[idx], start=(idx == 0), stop=(idx == len(items) - 1)),
                     reads=['cmb', ('pT', idx)], writes=[pkd], pe_acc=True)
            S.op('dve', lambda: V.tensor_tensor(out=dn.rearrange("p (j q) -> p j q", j=4), in0=pd_.rearrange("p (j q) -> p j q", j=4),
                                                in1=bc(esk.rearrange("p (j o) -> p j o", o=1), [128, 4, 128]), op=ALU.add), reads=[pkd, 'esk'], writes=['dn'])
            S.op('dve', lambda: V.reciprocal(out=dn, in_=dn), reads=['dn'], writes=['dn'])
            S.op('dve', lambda: V.tensor_tensor(out=ybT[:, :, qs], in0=po.rearrange("p (j q) -> p j q", j=4), in1=dn.rearrange("p (j q) -> p j q", j=4), op=ALU.mult),
                 reads=[pko, 'dn'], writes=['ybT'])

    def phase_merge(uT, yaT, ybT, mergedT, offs):
        AR.seek(offs[0])
        prw = AR.alloc([128, 4, D], BF16)
        AR.seek(offs[1])
        pat = AR.alloc([128, 4, D], BF16)
        wga = [AR.alloc([128, 8, 128], BF16) for _ in range(2)]
        wgb = [AR.alloc([128, 8, 128], BF16) for _ in range(2)]
        sgaP = [AR.alloc([128, 512], BF16) for _ in range(2)]
        sgbP = [AR.alloc([128, 512], BF16) for _ in range(2)]
        t1P = [AR.alloc([128, 512], F32) for _ in range(2)]
        t2P = [AR.alloc([128, 512], F32) for _ in range(2)]
        for hh in range(2):
            S.dma('pool', prw[:, hh * 2:(hh + 1) * 2, :], prw_d[hh * 256:(hh + 1) * 256, :].rearrange("(k p) n -> p k n", p=128), writes=['prw'])
            S.dma('pool', pat[:, hh * 2:(hh + 1) * 2, :], pat_d[hh * 256:(hh + 1) * 256, :].rearrange("(k p) n -> p k n", p=128), writes=['pat'])
        for oc in range(8):
            b = oc % 2
            S.dma('pool', wga[b], win_d[:, 2688 + oc * 128:2688 + (oc + 1) * 128].rearrange("(k p) n -> p k n", p=128), writes=[('wga', b)])
            S.dma('pool', wgb[b], win_d[:, 3712 + oc * 128:3712 + (oc + 1) * 128].rearrange("(k p) n -> p k n", p=128), writes=[('wgb', b)])
            for tb in range(NB):
                sl = slice(tb * 512, (tb + 1) * 512)
                L = tb % 2
                sga, sgb, t1, t2 = sgaP[L], sgbP[L], t1P[L], t2P[L]
                for k in range(8):
                    S.op('pe', lambda: PE.matmul(ps[0 + 4 * L], lhsT=wga[b][:, k, :], rhs=uT[:, k, sl], start=(k == 0), stop=(k == 7)),
                         reads=[('wga', b), ('uT', tb)], writes=[psk[0 + 4 * L]], pe_acc=True)
                S.op('act', lambda: ACT.activation(out=sga, in_=ps[0 + 4 * L], func=AF.Sigmoid), reads=[psk[0 + 4 * L]], writes=[('sga', L)])
                for k in range(8):
                    S.op('pe', lambda: PE.matmul(ps[1 + 4 * L], lhsT=wgb[b][:, k, :], rhs=uT[:, k, sl], start=(k == 0), stop=(k == 7)),
                         reads=[('wgb', b), ('uT', tb)], writes=[psk[1 + 4 * L]], pe_acc=True)
                S.op('act', lambda: ACT.activation(out=sgb, in_=ps[1 + 4 * L], func=AF.Sigmoid), reads=[psk[1 + 4 * L]], writes=[('sgb', L)])
                for k in range(4):
                    S.op('pe', lambda: PE.matmul(ps[2 + 4 * L], lhsT=prw[:, k, oc * 128:(oc + 1) * 128], rhs=yaT[:, k, sl], start=(k == 0), stop=(k == 3)),
                         reads=['prw', 'yaT'], writes=[psk[2 + 4 * L]], pe_acc=True)
                for k in range(4):
                    S.op('pe', lambda: PE.matmul(ps[3 + 4 * L], lhsT=pat[:, k, oc * 128:(oc + 1) * 128], rhs=ybT[:, k, sl], start=(k == 0), stop=(k == 3)),
                         reads=['pat', 'ybT'], writes=[psk[3 + 4 * L]], pe_acc=True)
                S.op('dve', lambda: V.tensor_tensor(out=t1, in0=ps[2 + 4 * L], in1=sga, op=ALU.mult), reads=[psk[2 + 4 * L], ('sga', L)], writes=[('t1', L)])
                S.op('dve', lambda: V.tensor_tensor(out=t2, in0=ps[3 + 4 * L], in1=sgb, op=ALU.mult), reads=[psk[3 + 4 * L], ('sgb', L)], writes=[('t2', L)])
                S.op('pool', lambda: POOL.tensor_tensor(out=mergedT[:, oc, sl], in0=t1, in1=t2, op=ALU.add), reads=[('t1', L), ('t2', L)], writes=[('mg', tb)])

    def phase_x1(s, mergedT, u2tm, base):
        AR.seek(base)
        wo = AR.alloc([128, 8, D], BF16)
        gt1B = AR.alloc([128, D], F32)
        sc2 = AR.alloc([128, D], F32)
        sh2 = AR.alloc([128, D], F32)
        xt = [AR.alloc([128, D], F32) for _ in range(2)]
        x1t = [AR.alloc([128, D], F32) for _ in range(2)]
        tmpP = [AR.alloc([128, D], F32) for _ in range(2)]
        junkP = [AR.alloc([128, D], BF16) for _ in range(2)]
        u2TP = [AR.alloc([128, 8, 128], BF16) for _ in range(2)]
        ssP = [AR.alloc([128, 8], F32) for _ in range(2)]
        exP = [AR.alloc([128, E], F32) for _ in range(2)]
        for hh in range(4):
            S.dma('pool', wo[:, hh * 2:(hh + 1) * 2, :], wout_d[hh * 256:(hh + 1) * 256, :].rearrange("(k p) n -> p k n", p=128), writes=['wo'])
        S.dma('sp', gt1B, mod_d[s, 2], writes=['gt1B'])
        S.dma('sp', sc2, mod_d[s, 4], writes=['sc2'])
        S.dma('sp', sh2, mod_d[s, 3], writes=['sh2'])
        lg = AR.alloc([128, NT, E], F32)
        mxs = AR.alloc([128, 3, NT], F32)

        def gen_x1(i):
            b = i % 2
            sl = slice(i * 128, (i + 1) * 128)
            S.dma('sp', xt[b], x_d[s, sl, :], writes=[('xt', b)])
            tmp, junk, u2T, ss = tmpP[b], junkP[b], u2TP[b], ssP[b]
            for cb in range(2):
                for k in range(8):
                    S.op('pe', lambda: PE.matmul(ps[cb + 6 * b], lhsT=mergedT[:, k, sl], rhs=wo[:, k, cb * 512:(cb + 1) * 512], start=(k == 0), stop=(k == 7)),
                         reads=[('mg', i // 4), 'wo'], writes=[psk[cb + 6 * b]], pe_acc=True)
                S.op('dve', lambda: V.tensor_tensor(out=tmp[:, cb * 512:(cb + 1) * 512], in0=ps[cb + 6 * b], in1=gt1B[:, cb * 512:(cb + 1) * 512], op=ALU.mult),
                     reads=[psk[cb + 6 * b], 'gt1B'], writes=[('tmp', b, cb)])
            yield
            S.op('pool', lambda: POOL.tensor_tensor(out=x1t[b], in0=tmp, in1=xt[b], op=ALU.add), reads=[('tmp', b, 0), ('tmp', b, 1), ('xt', b)], writes=[('x1t', b)])
            S.dma('sp', out_d[s, sl, :], x1t[b], reads=[('x1t', b)], writes=[('outd', i)])
            S.op('act', lambda: ACT.activation(out=junk, in_=x1t[b], func=AF.Square, accum_out=ss[:, 0:1]), reads=[('x1t', b)], writes=[('junk', b), ('ss0', b)])
            yield
            S.op('act', lambda: ACT.activation(out=ss[:, 1:2], in_=ss[:, 0:1], func=AF.Sqrt, bias=epsc[:, 0:1], scale=1.0 / D), reads=[('ss0', b), 'epsc'], writes=[('ss1', b)])
            S.op('dve', lambda: V.reciprocal(out=ss[:, 1:2], in_=ss[:, 1:2]), reads=[('ss1', b)], writes=[('ss1', b)])
            S.op('dve', lambda: V.scalar_tensor_tensor(out=tmp, in0=x1t[b], scalar=ss[:, 1:2], in1=sc2, op0=ALU.mult, op1=ALU.mult),
                 reads=[('x1t', b), ('ss1', b), 'sc2'], writes=[('tmp', b, 0), ('tmp', b, 1)])
            yield
            S.op('pool', lambda: POOL.tensor_tensor(out=u2tm[:, i, :], in0=tmp, in1=sh2, op=ALU.add), reads=[('tmp', b, 0), ('tmp', b, 1), 'sh2'], writes=[('u2', i)])
            pz = ps[2 + b].bitcast(BF16).rearrange("p (k t) -> p k t", k=8)
            pk = psk[2 + b]
            for k in range(8):
                S.op('pe', lambda: PE.transpose(out=pz[:, k, :], in_=u2tm[:, i, k * 128:(k + 1) * 128], identity=ident),
                     reads=[('u2', i), 'ident'], writes=[pk], pe_acc=True)
            yield
            S.op('act', lambda: ACT.copy(out=u2T, in_=pz), reads=[pk], writes=[('u2T', b)])
            pl, pkl = ps[4 + b], psk[4 + b]
            for k in range(8):
                S.op('pe', lambda: PE.matmul(pl[:, 0:E], lhsT=u2T[:, k, :], rhs=wr[:, k, :], start=(k == 0), stop=(k == 7)),
                     reads=[('u2T', b), 'wr'], writes=[pkl], pe_acc=True)
            yield
            S.op('act', lambda: ACT.copy(out=lg[:, i, :], in_=pl[:, 0:E]), reads=[pkl], writes=['lg'])
            yield

        def run_tasks(tasks):
            tasks = list(tasks)
            while tasks:
                for t_ in list(tasks):
                    try:
                        next(t_)
                    except StopIteration:
                        tasks.remove(t_)

        for i in range(0, NT, 2):
            run_tasks([gen_x1(i), gen_x1(i + 1)])
        S.op('dve', lambda: V.tensor_reduce(out=mxs[:, 0, :], in_=lg, axis=AX.X, op=ALU.max), reads=['lg'], writes=['mx0'])
        S.op('dve', lambda: V.tensor_tensor(out=lg, in0=lg, in1=bc(mxs[:, 0, :].rearrange("p (i o) -> p i o", o=1), [128, NT, E]), op=ALU.subtract),
             reads=['lg', 'mx0'], writes=['lg'])
        S.op('act', lambda: ACT.activation(out=lg, in_=lg, func=AF.Exp), reads=['lg'], writes=['lg'])
        S.op('dve', lambda: V.tensor_reduce(out=mxs[:, 1, :], in_=lg, axis=AX.X, op=ALU.add), reads=['lg'], writes=['mx1'])
        S.op('dve', lambda: V.reciprocal(out=mxs[:, 2, :], in_=mxs[:, 1, :]), reads=['mx1'], writes=['mx2'])
        S.op('dve', lambda: V.tensor_tensor(out=afftm, in0=lg, in1=bc(mxs[:, 2, :].rearrange("p (i o) -> p i o", o=1), [128, NT, E]), op=ALU.mult),
             reads=['lg', 'mx2'], writes=['afftm'])

    def phase_moe(s, u2tm, base):
        AR.seek(base)
        affT = AR.alloc([16, T], F32)
        work = AR.alloc([16, T], F32)
        maskT = AR.alloc([16, T], F32)
        slotT = AR.alloc([16, T], F32)
        mx8 = AR.alloc([16, 8], F32)
        for i in range(NT):
            pz = ps[i // 4]
            S.op('pe', lambda: PE.transpose(out=pz[0:16, (i % 4) * 128:(i % 4 + 1) * 128], in_=afftm[:, i, :], identity=identf),
                 reads=['afftm', 'identf'], writes=[psk[i // 4]], pe_acc=True)
        for q in range(4):
            S.op('act', lambda: ACT.copy(out=affT[:, q * 512:(q + 1) * 512], in_=ps[q][0:16, :]), reads=[psk[q]], writes=['affT'])
        S.op('dve', lambda: V.tensor_copy(out=work, in_=affT), reads=['affT'], writes=['work'])
        for it in range(CAP // 8):
            S.op('dve', lambda: V.max(out=mx8, in_=work), reads=['work'], writes=['mx8'])
            if it < CAP // 8 - 1:
                S.op('dve', lambda: V.match_replace(out=work, in_to_replace=mx8, in_values=work, imm_value=-1.0), reads=['work', 'mx8'], writes=['work'])
        S.op('dve', lambda: V.tensor_scalar(out=maskT, in0=affT, scalar1=mx8[:, 7:8], scalar2=None, op0=ALU.is_ge), reads=['affT', 'mx8'], writes=['maskT'])
        S.op('pool', lambda: POOL.memset(work, 1.0), reads=['work'], writes=['work'])
        S.op('dve', lambda: V.tensor_tensor_scan(out=slotT, data0=work, data1=maskT, initial=0.0, op0=ALU.mult, op1=ALU.add), reads=['work', 'maskT'], writes=['slotT'])
        S.op('dve', lambda: V.tensor_tensor(out=slotT, in0=slotT, in1=maskT, op=ALU.mult), reads=['slotT', 'maskT'], writes=['slotT'])
        S.op('dve', lambda: V.tensor_scalar(out=slotT, in0=slotT, scalar1=-1.0, scalar2=None, op0=ALU.add), reads=['slotT'], writes=['slotT'])
        pz = ps[4]
        for i in range(NT):
            S.op('pe', lambda: PE.transpose(out=pz[:, i * 16:(i + 1) * 16], in_=slotT[:, i * 128:(i + 1) * 128], identity=identf[0:16, 0:16]),
                 reads=['slotT', 'identf'], writes=[psk[4]], pe_acc=True)
        S.op('act', lambda: ACT.copy(out=slot_tm.rearrange("p i e -> p (i e)"), in_=pz[:, 0:256]), reads=[psk[4]], writes=['slot_tm'])
        S.op('dve', lambda: V.tensor_copy(out=affhl[:, :, :, 0], in_=afftm), reads=['afftm'], writes=['affhl'])
        S.op('dve', lambda: V.tensor_tensor(out=affhl[:, :, :, 1], in0=afftm, in1=affhl[:, :, :, 0], op=ALU.subtract), reads=['afftm', 'affhl'], writes=['affhl'])
        S.barrier()
        AR.seek(base)
        ye = AR.alloc([128, E, 2, D], BF16)
        Wg = AR.alloc([128, 8, D], BF16)
        Wu = AR.alloc([128, 8, D], BF16)
        Wd = AR.alloc([128, 8, D], BF16)
        wbase = AR.ptr
        Pe = AR.alloc([128, NT, CAP], BF16)
        xeT = AR.alloc([128, 8, CAP], BF16)
        hT = AR.alloc([128, 8, CAP], BF16)
        hs = AR.alloc([128, CAP], F32)
        affs = AR.alloc([128, 4], F32)
        gt2B = AR.alloc([128, D], F32)
        S.dma('sp', gt2B, mod_d[s, 5], writes=['gt2B'])
        for e in range(E):
            for (wt, wsrc, nm) in ((Wg, wg_d, 'Wg'), (Wu, wu_d, 'Wu'), (Wd, wd_d, 'Wd')):
                for hh in range(4):
                    S.dma('pool', wt[:, hh * 2:(hh + 1) * 2, :], wsrc[e, hh * 256:(hh + 1) * 256, :].rearrange("(k p) n -> p k n", p=128), writes=[(nm, hh)])
            for i in range(NT):
                S.op('dve', lambda: V.tensor_scalar(out=Pe[:, i, :], in0=iota_row, scalar1=slot_tm[:, i, e:e + 1], scalar2=None, op0=ALU.is_equal),
                     reads=['iota_row', 'slot_tm'], writes=[('Pe', i)])
            for fc in range(8):
                pz, pk = ps[fc // 2], psk[fc // 2]
                pzs = pz[:, (fc % 2) * 256:(fc % 2 + 1) * 256]
                for i in range(NT):
                    S.op('pe', lambda: PE.matmul(pzs, lhsT=u2tm[:, i, fc * 128:(fc + 1) * 128], rhs=Pe[:, i, :], start=(i == 0), stop=(i == NT - 1)),
                         reads=[('u2', i), ('Pe', i)], writes=[pk], pe_acc=True)
                S.op('act', lambda: ACT.copy(out=xeT[:, fc, :], in_=pzs), reads=[pk], writes=[('xeT', fc)])
            pa, pka = ps[4], psk[4]
            for half in range(2):
                for i in range(NT):
                    S.op('pe', lambda: PE.matmul(pa[:, half * 2:(half + 1) * 2], lhsT=Pe[:, i, half * 128:(half + 1) * 128], rhs=affhl[:, i, e, :], start=(i == 0), stop=(i == NT - 1)),
                         reads=[('Pe', i), 'affhl'], writes=[pka], pe_acc=True)
            S.op('dve', lambda: V.tensor_reduce(out=affs[:, 0:2], in_=pa[:, 0:4].rearrange("p (h t) -> p h t", t=2), axis=AX.X, op=ALU.add), reads=[pka], writes=['affs'])
            for fk in range(8):
                pg, pkg = ps[5], psk[5]
                pu, pku = ps[6], psk[6]
                for k in range(8):
                    S.op('pe', lambda: PE.matmul(pg[:, 0:CAP], lhsT=Wg[:, k, fk * 128:(fk + 1) * 128], rhs=xeT[:, k, :], start=(k == 0), stop=(k == 7)),
                         reads=[('Wg', k // 2), ('xeT', k)], writes=[pkg], pe_acc=True)
                for k in range(8):
                    S.op('pe', lambda: PE.matmul(pu[:, 0:CAP], lhsT=Wu[:, k, fk * 128:(fk + 1) * 128], rhs=xeT[:, k, :], start=(k == 0), stop=(k == 7)),
                         reads=[('Wu', k // 2), ('xeT', k)], writes=[pku], pe_acc=True)
                S.op('act', lambda: ACT.activation(out=hs, in_=pg[:, 0:CAP], func=AF.Silu), reads=[pkg], writes=['hs'])
                S.op('dve', lambda: V.tensor_tensor(out=hT[:, fk, :], in0=pu[:, 0:CAP], in1=hs, op=ALU.mult), reads=[pku, 'hs'], writes=[('hT', fk)])
            for half in range(2):
                for cb in range(2):
                    py, pky = ps[7] if (half * 2 + cb) % 2 else ps[4], psk[7] if (half * 2 + cb) % 2 else psk[4]
                    for fk in range(8):
                        S.op('pe', lambda: PE.matmul(py, lhsT=hT[:, fk, half * 128:(half + 1) * 128], rhs=Wd[:, fk, cb * 512:(cb + 1) * 512], start=(fk == 0), stop=(fk == 7)),
                             reads=[('hT', fk), ('Wd', fk // 2), 'affs'], writes=[pky], pe_acc=True)
                    S.op('dve', lambda: V.tensor_scalar(out=ye[:, e, half, cb * 512:(cb + 1) * 512], in0=py, scalar1=affs[:, half:half + 1], scalar2=None, op0=ALU.mult),
                         reads=[pky, 'affs'], writes=['ye'])
        S.barrier()
        AR.seek(wbase - 3 * 8 * D * 2)
        Pall = AR.alloc([128, E, CAP], BF16)
        PT = AR.alloc([128, 2 * E, 128], BF16)
        x1t = [AR.alloc([128, D], F32) for _ in range(2)]
        ot = [AR.alloc([128, D], F32) for _ in range(2)]
        for i in range(NT):
            b = i % 2
            sl = slice(i * 128, (i + 1) * 128)
            S.dma('sp', x1t[b], out_d[s, sl, :], reads=[('outd', i)], writes=[('x1t', b)])
            for e in range(E):
                S.op('dve', lambda: V.tensor_scalar(out=Pall[:, e, :], in0=iota_row, scalar1=slot_tm[:, i, e:e + 1], scalar2=None, op0=ALU.is_equal),
                     reads=['iota_row', 'slot_tm'], writes=[('Pall', e // 4)])
            for q in range(4):
                pz = ps[q].bitcast(BF16).rearrange("p (j t) -> p j t", j=8)
                for j in range(8):
                    idx = q * 8 + j
                    e, half = idx // 2, idx % 2
                    S.op('pe', lambda: PE.transpose(out=pz[:, j, :], in_=Pall[:, e, half * 128:(half + 1) * 128], identity=ident),
                         reads=[('Pall', e // 4), 'ident'], writes=[psk[q]], pe_acc=True)
                if q % 2 == 0:
                    S.op('act', lambda: ACT.copy(out=PT[:, q * 8:(q + 1) * 8, :], in_=pz), reads=[psk[q]], writes=[('PT', q)])
                else:
                    S.op('dve', lambda: V.tensor_copy(out=PT[:, q * 8:(q + 1) * 8, :], in_=pz), reads=[psk[q]], writes=[('PT', q)])
            for cb in range(2):
                po, pko = ps[4 + cb + 2 * (i % 2)], psk[4 + cb + 2 * (i % 2)]
                for idx in range(2 * E):
                    e, half = idx // 2, idx % 2
                    S.op('pe', lambda: PE.matmul(po, lhsT=PT[:, idx, :], rhs=ye[:, e, half, cb * 512:(cb + 1) * 512], start=(idx == 0), stop=(idx == 2 * E - 1)),
                         reads=[('PT', idx // 8), 'ye'], writes=[pko], pe_acc=True)
                S.op('dve', lambda: V.tensor_tensor(out=ot[b][:, cb * 512:(cb + 1) * 512], in0=po, in1=gt2B[:, cb * 512:(cb + 1) * 512], op=ALU.mult),
                     reads=[pko, 'gt2B'], writes=[('ot', b, cb)])
            S.op('dve', lambda: V.tensor_tensor(out=ot[b], in0=ot[b], in1=x1t[b], op=ALU.add), reads=[('ot', b, 0), ('ot', b, 1), ('x1t', b)], writes=[('ot', b, 0), ('ot', b, 1)])
            S.dma('sp', out_d[s, sl, :], ot[b], reads=[('ot', b, 0), ('ot', b, 1)], writes=[('outd', i)])

    def dbg_dump(src_ap, shape, key_reads=()):
        AR.seek(AR_TOP)
        t = AR.alloc(shape, F32)
        S.op('dve', lambda: V.tensor_copy(out=t, in_=src_ap), writes=['dbgt'])
        flat = t if len(shape) == 2 else t.rearrange("p a b -> p (a b)")
        S.dma('sp', dbg_d, flat, reads=['dbgt'])

    AR_TOP = 160 * 1024
    phase_adaln()
    for s in range(nseq):
        AR.seek(0)
        zsT = AR.alloc([128, 15, T], BF16)
        uT = AR.alloc([128, 8, T], BF16)
        base1 = AR.ptr
        phase_norm1(s, uT, base1)
        S.barrier()
        if dbg and dbg[0] == 'uT':
            dbg_dump(uT[:, :, 0:512], [128, 8, 512]); break
        for q in range(4):
            S.dma('sp', u_d[:, 2 * q:2 * q + 2, :], uT[:, 2 * q:2 * q + 2, :], reads=[('uT', 0), ('uT', 1), ('uT', 2), ('uT', 3)], writes=['uscr'])
        phase_rwkv_cols(uT, zsT, base1)
        S.barrier()
        if dbg and dbg[0] == 'zs':
            dbg_dump(zsT[:, :, 0:256], [128, 15, 256]); break
        AR.seek(61440)
        kkT = AR.alloc([128, 4, T], BF16)
        yaT = AR.alloc([128, 4, T], BF16)
        base3 = AR.ptr
        phase_scan(zsT, kkT, base3)
        if dbg and dbg[0] == 'yscan':
            AR.seek(AR_TOP)
            t = AR.alloc([128, 2, 512], F32)
            S.dma('sp', t[:, 0, :], y_d[0, 0:128, :], writes=['dbgt'])
            S.dma('sp', t[:, 1, :], y_d[1, 0:128, :], writes=['dbgt'])
            S.dma('sp', dbg_d, t.rearrange("p a b -> p (a b)"), reads=['dbgt']); break
        phase_post(zsT, yaT, base3)
        S.barrier()
        if dbg and dbg[0] == 'yaT':
            dbg_dump(yaT[:, :, 0:512], [128, 4, 512]); break
        AR.seek(0)
        uT = AR.alloc([128, 8, T], BF16)
        AR.seek(94208)
        ybT = AR.alloc([128, 4, T], BF16)
        baseB = AR.ptr
        for q in range(4):
            S.dma('sp' if q % 2 == 0 else 'act', uT[:, 2 * q:2 * q + 2, :], u_d[:, 2 * q:2 * q + 2, :], writes=[('uT', 0), ('uT', 1), ('uT', 2), ('uT', 3)])
        phase_attn(s, uT, ybT, 32768, baseB)
        S.barrier()
        if dbg and dbg[0] == 'ybT':
            dbg_dump(ybT[:, :, 0:512], [128, 4, 512]); break
        AR.seek(32768)
        mergedT = AR.alloc([128, 8, T], BF16)
        phase_merge(uT, yaT, ybT, mergedT, (65536, baseB))
        S.barrier()
        if dbg and dbg[0] == 'merged':
            dbg_dump(mergedT[:, :, 0:512], [128, 8, 512]); break
        AR.seek(0)
        u2tm = AR.alloc([128, NT, D], BF16)
        phase_x1(s, mergedT, u2tm, 65536)
        S.barrier()
        if dbg and dbg[0] == 'aff':
            dbg_dump(afftm.rearrange("p i e -> p (i e)"), [128, 256]); break
        phase_moe(s, u2tm, 32768)
        S.barrier()

    S.finish('sp')
    print("ninstr", S.ninstr, "pe_incs", S.npe_inc, "arena hi", AR.hi)
    return nc


def _consts():
    cm = np.zeros((13, 128, 128), np.float32)
    p = np.arange(128)
    cm[0] = (p[:, None] // 64 == p[None, :] // 64).astype(np.float32)
    R = np.zeros((128, 128), np.float32)
    for blk in range(2):
        o = blk * 64
        for d_ in range(8):
            R[o + d_ + 8, o + d_] = -1.0
            R[o + d_, o + d_ + 8] = 1.0
    cm[1] = R
    cm[2] = (p[:, None] >= p[None, :]).astype(np.float32)
    cm[3] = (p[:, None] <= p[None, :]).astype(np.float32)
    s_ = (p % 64)[:, None]
    t_ = (p % 64)[None, :]
    a_col = (p[None, :] >= 64)
    fwd = np.where(a_col, s_ < t_, s_ <= t_)
    bwd = np.where(a_col, s_ > t_, s_ >= t_)
    cm[4] = fwd.astype(np.float32)
    cm[5] = bwd.astype(np.float32)
    cm[6] = cm[4].T
    cm[7] = cm[5].T
    cm[8][:, 0] = (p < 64)
    cm[8][:, 1] = (p >= 64)
    cm[9][:, 0:64] = 1.0
    cm[10][:, 64:128] = 1.0
    cm[11] = ((p % 64)[:, None] < (p % 64)[None, :]).astype(np.float32)
    cm[12] = ((p % 64)[:, None] > (p % 64)[None, :]).astype(np.float32)
    return np.ascontiguousarray(cm.transpose(1, 0, 2).reshape(128, 13 * 128))


def _prep_shared(inp):
    f = lambda a: np.ascontiguousarray(np.asarray(a, dtype=np.float32))
    L = 0
    w_in = f(inp["w_in"][L]).copy()
    qoff = 1920
    perm = []
    for c in range(4):
        perm += list(range(c * 64, (c + 1) * 64)) + list(range((4 + c) * 64, (5 + c) * 64))
    perm = np.array(perm)
    w_in[:, qoff:qoff + 512] = w_in[:, qoff:qoff + 512][:, perm]
    p_attn = f(inp["p_attn"][L])[perm, :]
    pp = np.zeros((128, NPP), np.float32)

    def put(name, arr):
        o, w = PP[name]
        pp[:, o:o + w] = arr

    chunked = lambda v: np.asarray(v, np.float32).reshape(-1, 128).T
    put("mp", chunked(inp["mu_prev"][L]))
    put("mn", chunked(inp["mu_next"][L]))
    put("w0", np.concatenate([chunked(inp["rwkv_w0"][L][0]), chunked(inp["rwkv_w0"][L][1])], 1))
    put("a0", np.concatenate([chunked(inp["rwkv_a0"][L][0]), chunked(inp["rwkv_a0"][L][1])], 1))
    put("kk", chunked(inp["rwkv_k_k"][L]))
    put("ka", chunked(inp["rwkv_k_a"][L]))
    put("rk", chunked(np.asarray(inp["rwkv_r_k"][L]).reshape(-1)))
    put("qg", np.tile(np.asarray(inp["q_norm_g"][L], np.float32), 2)[:, None])
    put("kg", np.tile(np.asarray(inp["k_norm_g"][L], np.float32), 2)[:, None])
    inv_freq = (500000.0 ** (-np.arange(0, 16, 2, dtype=np.float32) / 16)).astype(np.float32)
    invf = np.zeros(64, np.float32)
    invf[0:8] = inv_freq
    invf[8:16] = inv_freq
    put("invf", np.tile(invf, 2)[:, None])
    sink = np.asarray(inp["attn_sink"][L], np.float32)
    sk = np.zeros((128, 4), np.float32)
    for j in range(4):
        sk[0:64, j] = sink[j]
        sk[64:128, j] = sink[4 + j]
    put("sink", sk)
    w2cat = np.zeros((128, 2, 512), np.float32)
    a2cat = np.zeros((128, 2, 512), np.float32)
    for d_ in range(2):
        w2cat[d_ * 64:(d_ + 1) * 64, d_, :] = inp["rwkv_w2"][L][d_]
        a2cat[d_ * 64:(d_ + 1) * 64, d_, :] = inp["rwkv_a2"][L][d_]
    return {
        "w_ada": f(inp["w_ada"][L]), "b_ada": f(inp["b_ada"][L])[None, :] if np.asarray(inp["b_ada"][L]).ndim == 1 else f(inp["b_ada"][L]),
        "norm1_g": f(inp["norm1_g"][L]).reshape(1, D), "norm2_g": f(inp["norm2_g"][L]).reshape(1, D),
        "w_in": w_in, "pp": pp, "w2cat": w2cat.reshape(128, 1024), "a2cat": a2cat.reshape(128, 1024),
        "g2": f(inp["rwkv_g2"][L]), "gn_w": f(inp["rwkv_gn_w"][L]).reshape(1, 512), "gn_b": f(inp["rwkv_gn_b"][L]).reshape(1, 512),
        "p_rwkv": f(inp["p_rwkv"][L]), "p_attn": np.ascontiguousarray(p_attn), "w_out": f(inp["w_out"][L]),
        "w_router": f(inp["w_router"][L]), "w_gate": f(inp["w_gate"][L]), "w_up": f(inp["w_up"][L]), "w_down": f(inp["w_down"][L]),
        "cmats": _consts(),
    }


def _core_inputs(inp, shared, seqs):
    x = np.ascontiguousarray(np.asarray(inp["x"], np.float32)[seqs])
    c = np.asarray(inp["c"], np.float32)[seqs]
    cT = np.ascontiguousarray(c.reshape(len(seqs), 8, 128).transpose(0, 2, 1))
    pos = np.ascontiguousarray(np.asarray(inp["positions"]).astype(np.int32)[seqs][:, None, :])
    m = dict(shared)
    m.update({"x": x, "cT": cT, "pos": pos})
    return m


def kernel(**inputs):
    shared = _prep_shared(inputs)
    nc = build(NSEQ)
    in_maps = [_core_inputs(inputs, shared, list(range(i * NSEQ, (i + 1) * NSEQ))) for i in range(NCORES)]
    res = run_bass_kernel_spmd(nc, in_maps, core_ids=list(range(NCORES)))
    out = np.concatenate([np.asarray(r["out"]) for r in res.results], axis=0)
    return out.astype(np.float32)
```

```python
import numpy as np
import concourse.bass as bass
import concourse.mybir as mybir
from concourse.bass_utils import run_bass_kernel_spmd

F32 = mybir.dt.float32
BF16 = mybir.dt.bfloat16
I32 = mybir.dt.int32
ALU = mybir.AluOpType
AF = mybir.ActivationFunctionType
AX = mybir.AxisListType

T = 2048
D = 1024
NT = 16
NB = 4
NSEQ = 2
NCORES = 8
E = 16
CAP = 256
LAM = float(np.exp(-0.5))
NCH = 4
TBS = NCH * 64
NTB = T // TBS
TWO_PI = float(2 * np.pi)
C1 = 6.28125
C2 = TWO_PI - C1

PP = {}
_o = 0
for _n, _w in [("mp", 15), ("mn", 15), ("w0", 8), ("a0", 8), ("kk", 4), ("ka", 4), ("rk", 4), ("qg", 1), ("kg", 1),
               ("invf", 1), ("sink", 4)]:
    PP[_n] = (_o, _w)
    _o += _w
NPP = _o


class Ticket:
    __slots__ = ('ins', 'sem', 'val', 'parent')

    def __init__(self, ins):
        self.ins = ins
        self.sem = None
        self.val = None
        self.parent = None

    def root(self):
        t = self
        while t.parent is not None:
            t = t.parent
        return t


class Sync:
    SEM_MAX = 30000

    def __init__(self, nc):
        self.nc = nc
        self.E = {'pe': nc.tensor, 'act': nc.scalar, 'dve': nc.vector, 'pool': nc.gpsimd, 'sp': nc.sync}
        self.sem = {}
        self.cnt = {}
        self.nsem = 0
        for e in self.E:
            self._newsem(e)
        self.waited = {}
        self.lastw = {}
        self.reads = {}
        self.dma_sems = {}
        self.dma_rr = {}
        self.ninstr = 0
        self.pend = None
        self.pend_writes = None
        self.npe_inc = 0

    def _newsem(self, e):
        self.sem[e] = self.nc.alloc_semaphore(f"s_{e}_{self.nsem}")
        self.nsem += 1
        self.cnt[e] = 0

    def _flush_pe(self):
        t = self.pend
        if t is None:
            return
        if self.cnt['pe'] >= self.SEM_MAX:
            self._newsem('pe')
        self.cnt['pe'] += 1
        t.sem = self.sem['pe']
        t.val = self.cnt['pe']
        t.ins.then_inc(t.sem, 1)
        self.npe_inc += 1
        self.pend = None
        self.pend_writes = None

    def _wait(self, e, ev):
        if ev is None:
            return
        if isinstance(ev, Ticket):
            if e == 'pe':
                return
            t = ev.root()
            if t.val is None:
                assert t is self.pend
                self._flush_pe()
            sem, val = t.sem, t.val
        else:
            src, sem, val = ev
        k = (e, sem.name)
        if self.waited.get(k, 0) >= val:
            return
        self.waited[k] = val
        self.E[e].wait_ge(sem, val)

    def deps(self, e, reads, writes, pe_acc=False):
        for k in reads:
            self._wait(e, self.lastw.get(k))
        for k in writes:
            lw = self.lastw.get(k)
            if not (pe_acc and isinstance(lw, Ticket)):
                self._wait(e, lw)
            for ev in self.reads.get(k, {}).values():
                self._wait(e, ev)

    def commit(self, src, ev, reads, writes):
        for k in reads:
            self.reads.setdefault(k, {})[src] = ev
        for k in writes:
            self.lastw[k] = ev
            self.reads[k] = {}

    def op(self, e, fn, reads=(), writes=(), pe_acc=False):
        self.deps(e, reads, writes, pe_acc)
        if e == 'pe':
            ins = fn()
            t = Ticket(ins)
            if self.pend is not None:
                if self.pend_writes == tuple(writes):
                    self.pend.parent = t
                    self.pend = None
                else:
                    self._flush_pe()
            self.pend = t
            self.pend_writes = tuple(writes)
            self.commit('pe', t, reads, writes)
            self.ninstr += 1
            return t
        if self.cnt[e] >= self.SEM_MAX:
            self._newsem(e)
        ins = fn()
        self.cnt[e] += 1
        ev = (e, self.sem[e], self.cnt[e])
        ins.then_inc(self.sem[e], 1)
        self.commit(e, ev, reads, writes)
        self.ninstr += 1
        return ev

    def dma(self, e, out, in_, reads=(), writes=(), nslots=8, **kw):
        if e == 'pool':
            nslots = 2
        lst = self.dma_sems.setdefault(e, [])
        if len(lst) < nslots:
            lst.append([self.nc.alloc_semaphore(f"d_{e}_{len(lst)}"), 0])
        i = self.dma_rr.get(e, 0)
        self.dma_rr[e] = (i + 1) % nslots
        slot = lst[i % len(lst)]
        sem, uses = slot
        if uses > 0:
            self._wait(e, ('dma', sem, 16 * uses))
        self.deps(e, reads, writes)
        self.E[e].dma_start(out=out, in_=in_, **kw).then_inc(sem, 16)
        slot[1] = uses + 1
        ev = ('dma_%s_%d' % (e, i % len(lst)), sem, 16 * (uses + 1))
        self.commit(ev[0], ev, reads, writes)
        self.ninstr += 1
        return ev

    def barrier(self):
        self._flush_pe()
        evs = [(e, self.sem[e], self.cnt[e]) for e in self.E if self.cnt[e] > 0]
        for q, lst in self.dma_sems.items():
            for sem, uses in lst:
                if uses:
                    evs.append(('dma', sem, 16 * uses))
        for e in self.E:
            for ev in evs:
                if ev[0] != e:
                    self._wait(e, ev)
        self.lastw = {}
        self.reads = {}

    def finish(self, e='sp'):
        self._flush_pe()
        for q, lst in self.dma_sems.items():
            for sem, uses in lst:
                if uses:
                    self._wait(e, ('dma', sem, 16 * uses))


class Arena:
    def __init__(self, nc, name, nbytes):
        self.n4 = nbytes // 4
        self.t = nc.alloc_sbuf_tensor(name, [128, self.n4], F32).ap()
        self.ptr = 0
        self.hi = 0

    def seek(self, off):
        self.ptr = off

    def alloc(self, shape, dtype, parts=None):
        esz = 4 if dtype in (F32, I32) else 2
        n = int(np.prod(shape[1:]))
        nb = (n * esz + 31) // 32 * 32
        assert self.ptr % 4 == 0
        a = self.ptr // 4
        assert a + nb // 4 <= self.n4, f"arena overflow {self.ptr}+{nb} > {self.n4 * 4}"
        v = self.t[:, a:a + nb // 4]
        if dtype != F32:
            v = v.bitcast(dtype)
        v = v[0:shape[0], 0:n]
        if len(shape) > 2:
            names = " ".join(f"d{i}" for i in range(len(shape) - 1))
            kw = {f"d{i}": int(shape[i + 1]) for i in range(len(shape) - 1)}
            v = v.rearrange(f"p ({names}) -> p {names}", **kw)
        self.ptr += nb
        self.hi = max(self.hi, self.ptr)
        return v


def bc(ap, shape):
    return ap.to_broadcast(list(shape))


def build(nseq=NSEQ, dbg=None, stop_after=None):
    nc = bass.Bass("TRN2", target_bir_lowering=False)
    S = Sync(nc)
    V, ACT, POOL, PE = nc.vector, nc.scalar, nc.gpsimd, nc.tensor

    def din(name, shape, dt=F32):
        return nc.dram_tensor(name, list(shape), dt, kind="ExternalInput").ap()

    x_d = din("x", [nseq, T, D])
    cT_d = din("cT", [nseq, 128, 8])
    pos_d = din("pos", [nseq, 1, T], I32)
    wada_d = din("w_ada", [D, 6 * D])
    bada_d = din("b_ada", [1, 6 * D])
    n1g_d = din("norm1_g", [1, D])
    n2g_d = din("norm2_g", [1, D])
    win_d = din("w_in", [D, 4736])
    pp_d = din("pp", [128, NPP])
    w2c_d = din("w2cat", [128, 2 * 512])
    a2c_d = din("a2cat", [128, 2 * 512])
    g2_d = din("g2", [128, 512])
    gnw_d = din("gn_w", [1, 512])
    gnb_d = din("gn_b", [1, 512])
    prw_d = din("p_rwkv", [512, D])
    pat_d = din("p_attn", [512, D])
    wout_d = din("w_out", [D, D])
    wr_d = din("w_router", [D, E])
    wg_d = din("w_gate", [E, D, D])
    wu_d = din("w_up", [E, D, D])
    wd_d = din("w_down", [E, D, D])
    cm_d = din("cmats", [128, 13 * 128])
    out_d = nc.dram_tensor("out", [nseq, T, D], F32, kind="ExternalOutput").ap()
    mod_d = nc.dram_tensor("modscr", [nseq, 6, 128, D], F32, kind="Internal").ap()
    y_d = nc.dram_tensor("yscr", [2, T, 512], F32, kind="Internal").ap()
    u_d = nc.dram_tensor("uscr", [128, 8, T], BF16, kind="Internal").ap()
    dbg_d = None
    if dbg is not None:
        dbg_d = nc.dram_tensor("dbg", list(dbg[1]), F32, kind="ExternalOutput").ap()

    def sb(name, shape, dt=F32):
        return nc.alloc_sbuf_tensor('sb_' + name, list(shape), dt).ap()

    pp = sb("pp", [128, NPP])
    ident = sb("ident", [128, 128], BF16)
    identf = sb("identf", [128, 128])
    cmb = sb("cmb", [128, 13, 128], BF16)
    w2c = sb("w2c", [128, 2, 512], BF16)
    a2c = sb("a2c", [128, 2, 512], BF16)
    g2 = sb("g2", [128, 512], BF16)
    wr = sb("wr", [128, 8, E], BF16)
    epsc = sb("epsc", [128, 4])
    alpha = sb("alpha", [128, 15])
    oneminus_ka = sb("omka", [128, 4])
    two_omka = sb("omka2", [128, 4])
    negkkc = sb("negone", [128, 1])
    esk = sb("esk", [128, 4])
    rmask = sb("rmask", [128, TBS])
    iota_row = sb("iota_row", [128, CAP])
    ident4 = sb("ident4", [128, 4, 128], BF16)
    kar = sb("kar", [128, 4])
    c2r = sb("c2r", [128, 4])
    afftm = sb("afftm", [128, NT, E])
    slot_tm = sb("slot_tm", [128, NT, E])
    affhl = sb("affhl", [128, NT, E, 2], BF16)

    BLK1, ROT, MPREV, MNEXT = 0, 1, 2, 3
    MZT = (4, 5)
    MZ = (6, 7)
    HSEL = 8
    VP = (9, 10)

    ps = [nc.alloc_psum_tensor(f"ps{i}", [128, 512], F32).ap() for i in range(8)]
    psk = [f"ps{i}" for i in range(8)]

    AR = Arena(nc, "arena", 192 * 1024)

    def col(name, j=0, n=1):
        o, w = PP[name]
        return pp[:, o + j:o + j + n]

    S.dma('sp', pp, pp_d, writes=['pp'])
    S.dma('pool', cmb.rearrange("p a b -> p (a b)"), cm_d, writes=['cmb'])
    S.dma('pool', w2c.rearrange("p a b -> p (a b)"), w2c_d, writes=['w2c'])
    S.dma('pool', a2c.rearrange("p a b -> p (a b)"), a2c_d, writes=['a2c'])
    S.dma('pool', g2, g2_d, writes=['g2'])
    S.dma('pool', wr, wr_d.rearrange("(k p) e -> p k e", p=128), writes=['wr'])
    S.op('pool', lambda: POOL.memset(identf, 1.0), writes=['identf'])
    S.op('pool', lambda: POOL.affine_select(out=identf, in_=identf, pattern=[[1, 128]], compare_op=ALU.is_equal,
                                            fill=0.0, base=0, channel_multiplier=-1), reads=['identf'], writes=['identf'])
    S.op('dve', lambda: V.tensor_copy(out=ident, in_=identf), reads=['identf'], writes=['ident'])
    for j in range(4):
        S.op('dve', lambda: V.tensor_copy(out=ident4[:, j, :], in_=identf), reads=['identf'], writes=['ident4'])
    S.op('pool', lambda: POOL.memset(epsc[:, 0:1], 1e-6), writes=['epsc'])
    S.op('pool', lambda: POOL.memset(epsc[:, 1:2], 64e-5), reads=['epsc'], writes=['epsc'])
    S.op('pool', lambda: POOL.memset(epsc[:, 2:3], 1e-24), reads=['epsc'], writes=['epsc'])
    S.op('pool', lambda: POOL.memset(epsc[:, 3:4], 0.0), reads=['epsc'], writes=['epsc'])
    S.op('pool', lambda: POOL.memset(negkkc, -1.0), writes=['negone'])
    S.op('dve', lambda: V.tensor_tensor(out=alpha, in0=col("mp", 0, 15), in1=col("mn", 0, 15), op=ALU.add), reads=['pp'], writes=['alpha'])
    S.op('dve', lambda: V.tensor_scalar(out=alpha, in0=alpha, scalar1=-1.0, scalar2=1.0, op0=ALU.mult, op1=ALU.add), reads=['alpha'], writes=['alpha'])
    S.op('dve', lambda: V.tensor_scalar(out=oneminus_ka, in0=col("ka", 0, 4), scalar1=-1.0, scalar2=1.0, op0=ALU.mult, op1=ALU.add), reads=['pp'], writes=['omka'])
    S.op('dve', lambda: V.tensor_scalar(out=two_omka, in0=col("ka", 0, 4), scalar1=-2.0, scalar2=2.0, op0=ALU.mult, op1=ALU.add), reads=['pp'], writes=['omka2'])
    S.op('act', lambda: ACT.activation(out=esk, in_=col("sink", 0, 4), func=AF.Exp), reads=['pp'], writes=['esk'])
    S.op('dve', lambda: V.tensor_tensor(out=kar, in0=col("ka", 0, 4), in1=col("rk", 0, 4), op=ALU.mult), reads=['pp'], writes=['kar'])
    S.op('dve', lambda: V.tensor_tensor(out=c2r, in0=two_omka, in1=col("rk", 0, 4), op=ALU.mult), reads=['pp', 'omka2'], writes=['kar'])
    S.op('pool', lambda: POOL.memset(rmask, 1.0), writes=['rmask'])
    S.op('pool', lambda: POOL.memset(rmask.rearrange("p (c t) -> p c t", t=64)[:, :, 0:1], 0.0), reads=['rmask'], writes=['rmask'])
    S.op('pool', lambda: POOL.iota(iota_row, pattern=[[1, CAP]], base=0, channel_multiplier=0, allow_small_or_imprecise_dtypes=True), writes=['iota_row'])

    def debug_out(ap_sb, key, rows=None):
        S.dma('sp', dbg_d if rows is None else rows, ap_sb, reads=[key])

    def phase_adaln():
        AR.seek(0)
        csil = [AR.alloc([128, 8], F32) for _ in range(nseq)]
        crep = [AR.alloc([128, 9, 128], F32) for _ in range(nseq)]
        wblk = [AR.alloc([128, 9, 512], F32) for _ in range(3)]
        g1B = AR.alloc([128, D], F32)
        g2B = AR.alloc([128, D], F32)
        mt = [AR.alloc([128, 512], F32) for _ in range(4)]
        S.dma('sp', g1B, n1g_d.partition_broadcast(128), writes=['g1B'])
        S.dma('sp', g2B, n2g_d.partition_broadcast(128), writes=['g2B'])
        for b in range(3):
            S.op('pool', lambda: POOL.memset(wblk[b][:, 8, :], 0.0), writes=[('wblk', b)])
        for s in range(nseq):
            S.dma('sp', csil[s], cT_d[s], writes=[('csil', s)])
            S.op('act', lambda: ACT.activation(out=csil[s], in_=csil[s], func=AF.Silu), reads=[('csil', s)], writes=[('csil', s)])
            S.op('pool', lambda: POOL.memset(crep[s][:, 8, :], 0.0), writes=[('crep', s)])
            S.op('pool', lambda: POOL.memset(crep[s][0:1, 8, :], 1.0), reads=[('crep', s)], writes=[('crep', s)])
            S.op('dve', lambda: V.tensor_copy(out=crep[s][:, 0:8, :], in_=bc(csil[s].rearrange("p (k o) -> p k o", o=1), [128, 8, 128])),
                 reads=[('csil', s)], writes=[('crep', s)])
        ev = 0
        for jb in range(12):
            b = jb % 3
            piece = jb // 2
            c0 = jb * 512
            S.dma('sp', wblk[b][:, 0:4, :], wada_d[0:512, c0:c0 + 512].rearrange("(k p) n -> p k n", p=128), writes=[('wblk', b)])
            S.dma('act', wblk[b][:, 4:8, :], wada_d[512:1024, c0:c0 + 512].rearrange("(k p) n -> p k n", p=128), writes=[('wblk', b)])
            S.dma('sp', wblk[b][0:1, 8, :], bada_d[:, c0:c0 + 512], writes=[('wblk', b)])
            for s in range(nseq):
                pz, pkz = ps[ev % 4], psk[ev % 4]
                for k in range(9):
                    S.op('pe', lambda: PE.matmul(pz, lhsT=crep[s][:, k, :], rhs=wblk[b][:, k, :], start=(k == 0), stop=(k == 8)),
                         reads=[('crep', s), ('wblk', b)], writes=[pkz], pe_acc=True)
                m = mt[ev % 4]
                lc = (jb % 2) * 512
                if piece == 1:
                    S.op('dve', lambda: V.scalar_tensor_tensor(out=m, in0=pz, scalar=1.0, in1=g1B[:, lc:lc + 512], op0=ALU.add, op1=ALU.mult),
                         reads=[pkz, 'g1B'], writes=[('mt', ev % 4)])
                elif piece == 4:
                    S.op('dve', lambda: V.scalar_tensor_tensor(out=m, in0=pz, scalar=1.0, in1=g2B[:, lc:lc + 512], op0=ALU.add, op1=ALU.mult),
                         reads=[pkz, 'g2B'], writes=[('mt', ev % 4)])
                else:
                    S.op('act', lambda: ACT.copy(out=m, in_=pz), reads=[pkz], writes=[('mt', ev % 4)])
                S.dma('sp', mod_d[s, piece, :, lc:lc + 512], m, reads=[('mt', ev % 4)], writes=[('mod', s, piece)])
                ev += 1
        S.barrier()

    def phase_norm1(s, uT, base):
        AR.seek(base)
        scp = AR.alloc([128, D], F32)
        shp = AR.alloc([128, D], F32)
        xt = [AR.alloc([128, D], F32) for _ in range(2)]
        tmp2 = [AR.alloc([128, D], F32) for _ in range(2)]
        ub = [AR.alloc([128, D], BF16) for _ in range(2)]
        junk2 = [AR.alloc([128, D], BF16) for _ in range(2)]
        ss2 = [AR.alloc([128, 2], F32) for _ in range(2)]
        S.dma('sp', scp, mod_d[s, 1], reads=[('mod', s, 1)], writes=['scp'])
        S.dma('sp', shp, mod_d[s, 0], reads=[('mod', s, 0)], writes=['shp'])
        for i in range(NT):
            b = i % 2
            S.dma('sp', xt[b], x_d[s, i * 128:(i + 1) * 128, :], writes=[('xt', b)])
            tmp, junk, ss = tmp2[b], junk2[b], ss2[b]
            S.op('act', lambda: ACT.activation(out=junk, in_=xt[b], func=AF.Square, accum_out=ss[:, 0:1]), reads=[('xt', b)], writes=[('junk', b), ('ss', b)])
            S.op('act', lambda: ACT.activation(out=ss[:, 1:2], in_=ss[:, 0:1], func=AF.Sqrt, bias=epsc[:, 0:1], scale=1.0 / D), reads=[('ss', b), 'epsc'], writes=[('ss1', b)])
            S.op('dve', lambda: V.reciprocal(out=ss[:, 1:2], in_=ss[:, 1:2]), reads=[('ss1', b)], writes=[('ss1', b)])
            S.op('dve', lambda: V.scalar_tensor_tensor(out=tmp, in0=xt[b], scalar=ss[:, 1:2], in1=scp, op0=ALU.mult, op1=ALU.mult),
                 reads=[('xt', b), ('ss1', b), 'scp'], writes=[('tmp', b)])
            S.op('pool', lambda: POOL.tensor_tensor(out=ub[b], in0=tmp, in1=shp, op=ALU.add), reads=[('tmp', b), 'shp'], writes=[('ub', b)])
            pz = ps[i % 2].bitcast(BF16).rearrange("p (k t) -> p k t", k=8)
            for k in range(8):
                S.op('pe', lambda: PE.transpose(out=pz[:, k, :], in_=ub[b][:, k * 128:(k + 1) * 128], identity=ident),
                     reads=[('ub', b), 'ident'], writes=[psk[i % 2]], pe_acc=True)
            S.op('act', lambda: ACT.copy(out=uT[:, :, i * 128:(i + 1) * 128], in_=pz), reads=[psk[i % 2]], writes=[('uT', i // 4)])

    def phase_rwkv_cols(uT, zsT, base):
        AR.seek(base)
        wg = [AR.alloc([128, 8, 128], BF16) for _ in range(2)]
        ztmpP = [AR.alloc([128, T + 2], F32) for _ in range(2)]
        shtP = [AR.alloc([128, T], F32) for _ in range(2)]
        for q in range(2):
            S.op('pool', lambda: POOL.memset(ztmpP[q][:, 0:1], 0.0), writes=[('ztmp', q)])
            S.op('pool', lambda: POOL.memset(ztmpP[q][:, T + 1:T + 2], 0.0), reads=[('ztmp', q)], writes=[('ztmp', q)])
        for j in range(15):
            b = j % 2
            ztmp, sht = ztmpP[b], shtP[b]
            S.dma('pool', wg[b], win_d[:, j * 128:(j + 1) * 128].rearrange("(k p) n -> p k n", p=128), writes=[('wg', b)])
            for tb in range(NB):
                pz = ps[(j * NB + tb) % 4]
                pk = psk[(j * NB + tb) % 4]
                for k in range(8):
                    S.op('pe', lambda: PE.matmul(pz, lhsT=wg[b][:, k, :], rhs=uT[:, k, tb * 512:(tb + 1) * 512], start=(k == 0), stop=(k == 7)),
                         reads=[('wg', b), ('uT', tb)], writes=[pk], pe_acc=True)
                S.op('act', lambda: ACT.copy(out=ztmp[:, 1 + tb * 512:1 + (tb + 1) * 512], in_=pz), reads=[pk], writes=[('ztmp', b)])
            S.op('dve', lambda: V.tensor_scalar(out=sht, in0=ztmp[:, 1:T + 1], scalar1=alpha[:, j:j + 1], scalar2=None, op0=ALU.mult),
                 reads=[('ztmp', b), 'alpha'], writes=[('sht', b)])
            S.op('dve', lambda: V.scalar_tensor_tensor(out=sht, in0=ztmp[:, 0:T], scalar=col("mp", j), in1=sht, op0=ALU.mult, op1=ALU.add),
                 reads=[('ztmp', b), ('sht', b), 'pp'], writes=[('sht', b)])
            S.op('dve', lambda: V.scalar_tensor_tensor(out=zsT[:, j, :], in0=ztmp[:, 2:T + 2], scalar=col("mn", j), in1=sht, op0=ALU.mult, op1=ALU.add),
                 reads=[('ztmp', b), ('sht', b), 'pp'], writes=[('zs', j)])
            if j == 12:
                S.op('act', lambda: ACT.activation(out=zsT[:, j, :], in_=zsT[:, j, :], func=AF.Tanh), reads=[('zs', j)], writes=[('zs', j)])
            if j == 14:
                S.op('act', lambda: ACT.activation(out=zsT[:, j, :], in_=zsT[:, j, :], func=AF.Sigmoid), reads=[('zs', j)], writes=[('zs', j)])

    def phase_scan(zsT, kkT, base):
        rT = lambda c: zsT[:, c, :]
        kT = lambda c: zsT[:, 4 + c, :]
        vT = lambda c: zsT[:, 8 + c, :]
        wdT = zsT[:, 12, :]
        adT = zsT[:, 13, :]
        AR.seek(base)
        kraw = AR.alloc([128, 512], F32)
        ksq = AR.alloc([128, 512], BF16)
        krs = AR.alloc([128, 512], F32)
        for c in range(4):
            for tb in range(NB):
                sl = slice(tb * 512, (tb + 1) * 512)
                S.op('dve', lambda: V.tensor_scalar(out=kraw, in0=kT(c)[:, sl], scalar1=col("kk", c), scalar2=None, op0=ALU.mult), reads=[('zs', 4 + c), 'pp'], writes=['kraw'])
                S.op('act', lambda: ACT.activation(out=ksq, in_=kraw, func=AF.Square), reads=['kraw'], writes=['ksq'])
                pz, pk = ps[tb % 2], psk[tb % 2]
                S.op('pe', lambda: PE.matmul(pz, lhsT=cmb[:, BLK1, :], rhs=ksq, start=True, stop=True), reads=['ksq', 'cmb'], writes=[pk], pe_acc=True)
                S.op('act', lambda: ACT.activation(out=krs, in_=pz, func=AF.Sqrt, bias=epsc[:, 2:3], scale=1.0), reads=[pk, 'epsc'], writes=['krs'])
                S.op('dve', lambda: V.reciprocal(out=krs, in_=krs), reads=['krs'], writes=['krs'])
                S.op('dve', lambda: V.tensor_tensor(out=kkT[:, c, sl], in0=kraw, in1=krs, op=ALU.mult), reads=['kraw', 'krs'], writes=[('kk', c)])
        S.barrier()
        AR.seek(base)
        sg = AR.alloc([128, 4, TBS], F32)
        ad = AR.alloc([128, 4, TBS], F32)
        cc = AR.alloc([128, 4, TBS], F32)
        t1 = AR.alloc([128, 4, TBS], F32)
        ex = [[AR.alloc([128, TBS], F32) for _ in range(2)] for _ in range(4)]
        kd = AR.alloc([128, 4, TBS], F32)
        bb = AR.alloc([128, 4, TBS], F32)
        pdec = AR.alloc([128, 4, NCH], F32)
        ARz = AR.alloc([128, 4, NCH, 2, 2, 64], BF16)
        Bz = AR.alloc([128, 4, NCH, 2, 64], BF16)
        BKt = AR.alloc([128, 4, NCH, 2, 64], BF16)
        KBh = AR.alloc([128, 4, NCH, 2, 64], BF16)
        KBt = AR.alloc([128, 4, NCH, 128], BF16)
        VZ = AR.alloc([128, NCH, 8, 64], BF16)
        XV = AR.alloc([128, NCH, 8, 64], BF16)
        ZTs = [[AR.alloc([128, 4, 128], BF16) for _ in range(2)] for _ in range(NCH)]
        ATm = [[AR.alloc([128, 4, 128], BF16) for _ in range(2)] for _ in range(NCH)]
        PTm = [[AR.alloc([128, 4, 128], BF16) for _ in range(2)] for _ in range(NCH)]
        Pm = [[AR.alloc([128, 4, 128], BF16) for _ in range(2)] for _ in range(NCH)]
        Am = [[AR.alloc([128, 4, 128], BF16) for _ in range(2)] for _ in range(NCH)]
        W1s = AR.alloc([128, 4, 64], BF16)
        S32 = [AR.alloc([128, 4, 64], F32) for _ in range(2)]
        Sb = [AR.alloc([128, 4, 64], BF16) for _ in range(2)]
        ysb = [AR.alloc([64, 512], F32) for _ in range(2)]
        S.op('pool', lambda: POOL.memset(ARz.rearrange("p a b c d e -> p (a b c d e)"), 0.0), writes=['ARz'])
        S.op('pool', lambda: POOL.memset(Bz.rearrange("p a b c d -> p (a b c d)"), 0.0), writes=['Bz'])
        S.op('pool', lambda: POOL.memset(VZ.rearrange("p a b c -> p (a b c)"), 0.0), writes=['VZ'])

        def chain(gens):
            for g_ in gens:
                yield from g_

        def run_tasks(tasks):
            tasks = list(tasks)
            while tasks:
                for t_ in list(tasks):
                    try:
                        next(t_)
                    except StopIteration:
                        tasks.remove(t_)

        yev = 0
        pendQ = None
        for d in range(2):
            S.op('pool', lambda: POOL.memset(S32[d].rearrange("p a b -> p (a b)"), 0.0), writes=[('S32', d)])
            S.op('pool', lambda: POOL.memset(Sb[d].rearrange("p a b -> p (a b)"), 0.0), writes=[('Sb', d)])
            tbs = range(NTB) if d == 0 else range(NTB - 1, -1, -1)
            for tb in tbs:
                sl = slice(tb * TBS, (tb + 1) * TBS)
                def gen_prep(c):
                    pz, pk = ps[c % 2], psk[c % 2]
                    S.op('pe', lambda: PE.matmul(pz[:, 0:TBS], lhsT=w2c[:, d, c * 128:(c + 1) * 128], rhs=wdT[:, sl], start=True, stop=True),
                         reads=['w2c', ('zs', 12)], writes=[pk], pe_acc=True)
                    S.op('act', lambda: ACT.activation(out=sg[:, c, :], in_=pz[:, 0:TBS], func=AF.Sigmoid, bias=col("w0", d * 4 + c), scale=1.0),
                         reads=[pk, 'pp'], writes=[('sg', c)])
                    pz2, pk2 = ps[2 + c % 2], psk[2 + c % 2]
                    S.op('pe', lambda: PE.matmul(pz2[:, 0:TBS], lhsT=a2c[:, d, c * 128:(c + 1) * 128], rhs=adT[:, sl], start=True, stop=True),
                         reads=['a2c', ('zs', 13)], writes=[pk2], pe_acc=True)
                    S.op('act', lambda: ACT.activation(out=ad[:, c, :], in_=pz2[:, 0:TBS], func=AF.Sigmoid, bias=col("a0", d * 4 + c), scale=1.0),
                         reads=[pk2, 'pp'], writes=[('ad', c)])
                    yield
                    S.op('dve', lambda: V.tensor_tensor_scan(out=cc[:, c, :], data0=rmask, data1=sg[:, c, :], initial=0.0, op0=ALU.mult, op1=ALU.add),
                         reads=['rmask', ('sg', c)], writes=[('cc', c)])
                    cc3 = cc[:, c, :].rearrange("p (h t) -> p h t", t=64)
                    sg3 = sg[:, c, :].rearrange("p (h t) -> p h t", t=64)
                    t13 = t1[:, c, :].rearrange("p (h t) -> p h t", t=64)
                    if d == 1:
                        S.op('dve', lambda: V.tensor_tensor(out=t13, in0=bc(cc3[:, :, 63:64], [128, NCH, 64]), in1=cc3, op=ALU.subtract),
                             reads=[('cc', c)], writes=[('t1', c)])
                        S.op('dve', lambda: V.tensor_tensor(out=cc[:, c, :], in0=t1[:, c, :], in1=sg[:, c, :], op=ALU.add),
                             reads=[('t1', c), ('sg', c)], writes=[('cc', c)])
                    totp = 63 if d == 0 else 0
                    S.op('pool', lambda: POOL.tensor_scalar(out=kd[:, c, :], in0=ad[:, c, :], scalar1=col("ka", c), scalar2=oneminus_ka[:, c:c + 1], op0=ALU.mult, op1=ALU.add),
                         reads=[('ad', c), 'pp', 'omka'], writes=[('kd', c)])
                    S.op('pool', lambda: POOL.tensor_tensor(out=kd[:, c, :], in0=kd[:, c, :], in1=kT(c)[:, sl], op=ALU.mult),
                         reads=[('kd', c), ('zs', 4 + c)], writes=[('kd', c)])
                    S.op('pool', lambda: POOL.tensor_tensor(out=bb[:, c, :], in0=ad[:, c, :], in1=kkT[:, c, sl], op=ALU.mult),
                         reads=[('ad', c), ('kk', c)], writes=[('bb', c)])
                    yield
                    e = ex[c][0]
                    S.op('act', lambda: ACT.activation(out=e, in_=cc[:, c, :], func=AF.Exp, scale=-LAM), reads=[('cc', c)], writes=[('ex', c, 0)])
                    for hp in range(2):
                        pr = slice(hp * 64, (hp + 1) * 64)
                        S.op('dve', lambda: V.tensor_tensor(out=ARz[pr, c, :, 0, hp, :], in0=rT(c)[pr, sl].rearrange("p (h t) -> p h t", t=64),
                                                            in1=e[pr, :].rearrange("p (h t) -> p h t", t=64), op=ALU.mult),
                             reads=[('zs', c), ('ex', c, 0)], writes=['ARz'])
                    yield
                    e = ex[c][1]
                    S.op('act', lambda: ACT.activation(out=e, in_=cc[:, c, :], func=AF.Exp, scale=LAM), reads=[('cc', c)], writes=[('ex', c, 1)])
                    S.op('dve', lambda: V.tensor_tensor(out=BKt[:, c, :, 0, :], in0=kd[:, c, :].rearrange("p (h t) -> p h t", t=64),
                                                        in1=e.rearrange("p (h t) -> p h t", t=64), op=ALU.mult),
                         reads=[('kd', c), ('ex', c, 1)], writes=['BKt'])
                    S.op('dve', lambda: V.tensor_tensor(out=BKt[:, c, :, 1, :], in0=bb[:, c, :].rearrange("p (h t) -> p h t", t=64),
                                                        in1=e.rearrange("p (h t) -> p h t", t=64), op=ALU.mult),
                         reads=[('bb', c), ('ex', c, 1)], writes=['BKt'])
                    for hp in range(2):
                        pr = slice(hp * 64, (hp + 1) * 64)
                        S.op('act', lambda: ACT.copy(out=Bz[pr, c, :, hp, :], in_=BKt[pr, c, :, 1, :]), reads=['BKt'], writes=['Bz'])
                    yield
                    S.op('dve', lambda: V.tensor_tensor(out=t1[:, c, :], in0=cc[:, c, :], in1=sg[:, c, :], op=ALU.subtract),
                         reads=[('cc', c), ('sg', c)], writes=[('t1', c)])
                    e = ex[c][0]
                    S.op('act', lambda: ACT.activation(out=e, in_=t1[:, c, :], func=AF.Exp, scale=-LAM), reads=[('t1', c)], writes=[('ex', c, 0)])
                    for hp in range(2):
                        pr = slice(hp * 64, (hp + 1) * 64)
                        S.op('dve', lambda: V.scalar_tensor_tensor(out=ARz[pr, c, :, 1, hp, :], in0=kkT[pr, c, sl].rearrange("p (h t) -> p h t", t=64),
                                                                   scalar=-1.0, in1=e[pr, :].rearrange("p (h t) -> p h t", t=64), op0=ALU.mult, op1=ALU.mult),
                             reads=[('kk', c), ('ex', c, 0)], writes=['ARz'])
                    yield
                    S.op('dve', lambda: V.tensor_tensor(out=t13, in0=bc(cc3[:, :, totp:totp + 1], [128, NCH, 64]), in1=cc3, op=ALU.subtract),
                         reads=[('cc', c)], writes=[('t1', c)])
                    e = ex[c][1]
                    S.op('act', lambda: ACT.activation(out=e, in_=t1[:, c, :], func=AF.Exp, scale=-LAM), reads=[('t1', c)], writes=[('ex', c, 1)])
                    S.op('pool', lambda: POOL.tensor_tensor(out=KBh[:, c, :, 0, :], in0=kd[:, c, :].rearrange("p (h t) -> p h t", t=64),
                                                        in1=e.rearrange("p (h t) -> p h t", t=64), op=ALU.mult),
                         reads=[('kd', c), ('ex', c, 1)], writes=['KBh'])
                    S.op('pool', lambda: POOL.tensor_tensor(out=KBh[:, c, :, 1, :], in0=bb[:, c, :].rearrange("p (h t) -> p h t", t=64),
                                                        in1=e.rearrange("p (h t) -> p h t", t=64), op=ALU.mult),
                         reads=[('bb', c), ('ex', c, 1)], writes=['KBh'])
                    S.op('act', lambda: ACT.activation(out=pdec[:, c, :].rearrange("p (h o) -> p h o", o=1), in_=cc3[:, :, totp:totp + 1], func=AF.Exp, scale=-LAM), reads=[('cc', c)], writes=['pdec'])
                ptasks = [gen_prep(c_) for c_ in range(4)]
                for t_ in ptasks:
                    next(t_)
                if pendQ is not None:
                    for _ in range(3):
                        next(pendQ, None)
                for t_ in ptasks:
                    next(t_)
                if pendQ is not None:
                    run_tasks([pendQ])
                    pendQ = None
                run_tasks(ptasks)
                for ch in range(NCH):
                    pz = ps[4 + ch % 2].bitcast(BF16)
                    pk = psk[4 + ch % 2]
                    pzv = pz[0:64, 0:512].rearrange("p (c n) -> p c n", c=4)
                    for c in range(4):
                        S.op('pe', lambda: PE.transpose(out=pzv[:, c, :], in_=vT(c)[:, tb * TBS + ch * 64: tb * TBS + (ch + 1) * 64], identity=ident),
                             reads=[('zs', 8 + c), 'ident'], writes=[pk], pe_acc=True)
                    S.op('act', lambda: ACT.copy(out=VZ[0:64, ch, :, :].rearrange("p h v -> p (h v)"), in_=pz[0:64, 0:512]), reads=[pk], writes=[('VZ', ch)])
                    S.op('act', lambda: ACT.copy(out=XV[0:64, ch, :, :].rearrange("p h v -> p (h v)"), in_=pz[0:64, 0:512]), reads=[pk], writes=[('XVv', ch)])
                    pzk = pz[:, 512:1024].rearrange("p (c n) -> p c n", c=4)
                    for c in range(4):
                        S.op('pe', lambda: PE.transpose(out=pzk[:, c, :], in_=KBh[:, c, ch, :, :].rearrange("p a t -> p (a t)"), identity=ident),
                             reads=['KBh', 'ident'], writes=[pk], pe_acc=True)
                    S.op('dve', lambda: V.tensor_copy(out=KBt[:, :, ch, :], in_=pzk), reads=[pk], writes=[('KBt', ch)])
                MNT = cmb[:, 11 + d, :]
                MN = cmb[:, 12 - d, :]

                def gen_D(ch, slot, par):
                    pA, pkA = ps[2 * par], psk[2 * par]
                    pB, pkB = ps[2 * par + 1], psk[2 * par + 1]
                    pA3 = pA.rearrange("p (j n) -> p j n", j=4)
                    pB3 = pB.rearrange("p (j n) -> p j n", j=4)
                    mzt = cmb[:, MZT[d], :]
                    for half in range(2):
                        pz3 = pA3 if half == 0 else pB3
                        pkz = pkA if half == 0 else pkB
                        for j in range(4):
                            h = half * 4 + j
                            c, hp = h // 2, h % 2
                            bk = BKt[:, c, ch, :, :].rearrange("p a t -> p (a t)")
                            S.op('pe', lambda: PE.matmul(pz3[:, j, :].rearrange("p (a t) -> p a t", a=2), lhsT=bk, rhs=ARz[:, c, ch, :, hp, :], start=True, stop=True),
                                 reads=['BKt', 'ARz'], writes=[pkz], pe_acc=True)
                        S.op('dve', lambda: V.tensor_tensor(out=ZTs[slot][half], in0=pz3, in1=bc(mzt.rearrange("p (o n) -> p o n", o=1), [128, 4, 128]), op=ALU.mult),
                             reads=[pkz, 'cmb'], writes=[('ZTs', slot, half)])
                    yield
                    for c in range(4):
                        bz = Bz[:, c, ch, :, :].rearrange("p a t -> p (a t)")
                        az = ARz[:, c, ch, 1, :, :].rearrange("p a t -> p (a t)")
                        S.op('pe', lambda: PE.matmul(pA3[:, c, :], lhsT=bz, rhs=az, start=True, stop=True), reads=['Bz', 'ARz'], writes=[pkA], pe_acc=True)
                        S.op('pe', lambda: PE.matmul(pB3[:, c, :], lhsT=az, rhs=bz, start=True, stop=True), reads=['Bz', 'ARz'], writes=[pkB], pe_acc=True)
                    S.op('dve', lambda: V.tensor_tensor(out=PTm[par][0], in0=pA3, in1=bc(MNT.rearrange("p (o n) -> p o n", o=1), [128, 4, 128]), op=ALU.mult),
                         reads=[pkA, 'cmb'], writes=[('PT', par, 0)])
                    S.op('dve', lambda: V.tensor_tensor(out=Pm[par][0], in0=pB3, in1=bc(MN.rearrange("p (o n) -> p o n", o=1), [128, 4, 128]), op=ALU.mult),
                         reads=[pkB, 'cmb'], writes=[('P', par, 0)])
                    S.op('pool', lambda: POOL.tensor_tensor(out=ATm[slot][0], in0=PTm[par][0], in1=ident4, op=ALU.add), reads=[('PT', par, 0), 'ident4'], writes=[('AT', slot, 0)])
                    S.op('pool', lambda: POOL.tensor_tensor(out=Am[par][0], in0=Pm[par][0], in1=ident4, op=ALU.add), reads=[('P', par, 0), 'ident4'], writes=[('A', par, 0)])
                    yield
                    cur = 0
                    for lev in range(1, 6):
                        nxt = 1 - cur
                        for j in range(4):
                            S.op('pe', lambda: PE.matmul(pA3[:, j, :], lhsT=Pm[par][cur][:, j, :], rhs=PTm[par][cur][:, j, :], start=True, stop=True),
                                 reads=[('P', par, cur), ('PT', par, cur)], writes=[pkA], pe_acc=True)
                            if lev < 5:
                                S.op('pe', lambda: PE.matmul(pB3[:, j, :], lhsT=PTm[par][cur][:, j, :], rhs=Pm[par][cur][:, j, :], start=True, stop=True),
                                     reads=[('P', par, cur), ('PT', par, cur)], writes=[pkB], pe_acc=True)
                        S.op('act', lambda: ACT.copy(out=PTm[par][nxt], in_=pA3), reads=[pkA], writes=[('PT', par, nxt)])
                        if lev < 5:
                            S.op('dve', lambda: V.tensor_copy(out=Pm[par][nxt], in_=pB3), reads=[pkB], writes=[('P', par, nxt)])
                        yield
                        for j in range(4):
                            S.op('pe', lambda: PE.matmul(pA3[:, j, :], lhsT=Am[par][cur][:, j, :], rhs=PTm[par][nxt][:, j, :], start=True, stop=True),
                                 reads=[('A', par, cur), ('PT', par, nxt)], writes=[pkA], pe_acc=True)
                            if lev < 5:
                                S.op('pe', lambda: PE.matmul(pB3[:, j, :], lhsT=PTm[par][nxt][:, j, :], rhs=Am[par][cur][:, j, :], start=True, stop=True),
                                     reads=[('A', par, cur), ('PT', par, nxt)], writes=[pkB], pe_acc=True)
                        S.op('dve', lambda: V.tensor_tensor(out=ATm[slot][nxt], in0=pA3, in1=ATm[slot][cur], op=ALU.add), reads=[pkA, ('AT', slot, cur)], writes=[('AT', slot, nxt)])
                        if lev < 5:
                            S.op('act', lambda: ACT.copy(out=Am[par][nxt], in_=pB3), reads=[pkB], writes=[('A', par, nxt)])
                            S.op('pool', lambda: POOL.tensor_tensor(out=Am[par][nxt], in0=Am[par][nxt], in1=Am[par][cur], op=ALU.add),
                                 reads=[('A', par, nxt), ('A', par, cur)], writes=[('A', par, nxt)])
                        yield
                        cur = nxt
                    assert cur == 1

                def gen_Q(ch, slot, tb=tb, d=d):
                    nonlocal yev
                    fin = 1
                    gch = tb * NCH + ch
                    pW, pkW = ps[4], psk[4]
                    pW3 = pW[:, 0:256].rearrange("p (c v) -> p c v", c=4)
                    for h in range(8):
                        c, hp = h // 2, h % 2
                        S.op('pe', lambda: PE.matmul(pW3[hp * 64:(hp + 1) * 64, c, :], lhsT=ZTs[slot][h // 4][:, h % 4, 64:128], rhs=VZ[:, ch, h, :], start=True, stop=False),
                             reads=[('ZTs', slot, h // 4), ('VZ', ch)], writes=[pkW], pe_acc=True)
                        S.op('pe', lambda: PE.matmul(pW3[hp * 64:(hp + 1) * 64, c, :], lhsT=ARz[:, c, ch, 1, hp, :], rhs=Sb[d][:, c, :], start=False, stop=True),
                             reads=['ARz', ('Sb', d)], writes=[pkW], pe_acc=True)
                    S.op('act', lambda: ACT.copy(out=W1s, in_=pW3), reads=[pkW], writes=['W1s'])
                    yield
                    pX, pkX = ps[5], psk[5]
                    pX3 = pX.rearrange("p (h v) -> p h v", h=8)
                    for h in range(8):
                        c, hp = h // 2, h % 2
                        S.op('pe', lambda: PE.matmul(pX3[64:128, h, :], lhsT=ATm[slot][fin][:, c, hp * 64:(hp + 1) * 64], rhs=W1s[:, c, :], start=True, stop=True),
                             reads=[('AT', slot, fin), 'W1s'], writes=[pkX], pe_acc=True)
                    S.op('dve', lambda: V.tensor_copy(out=XV[64:128, ch, :, :], in_=pX3[64:128]), reads=[pkX], writes=[('XVu', ch)])
                    yield
                    pS, pkS = ps[7], psk[7]
                    pS3 = pS[:, 0:256].rearrange("p (c v) -> p c v", c=4)
                    for h in range(8):
                        c, hp = h // 2, h % 2
                        S.op('pe', lambda: PE.matmul(pS3[hp * 64:(hp + 1) * 64, c, :], lhsT=KBt[:, c, ch, hp * 64:(hp + 1) * 64], rhs=XV[:, ch, h, :], start=True, stop=True),
                             reads=[('KBt', ch), ('XVv', ch), ('XVu', ch)], writes=[pkS], pe_acc=True)
                    pY, pkY = ps[6], psk[6]
                    pY3 = pY.rearrange("p (h v) -> p h v", h=8)
                    for h in range(8):
                        c, hp = h // 2, h % 2
                        S.op('pe', lambda: PE.matmul(pY3[0:64, h, :], lhsT=ZTs[slot][h // 4][:, h % 4, 0:64], rhs=XV[:, ch, h, :], start=True, stop=False),
                             reads=[('ZTs', slot, h // 4), ('XVv', ch), ('XVu', ch)], writes=[pkY], pe_acc=True)
                        S.op('pe', lambda: PE.matmul(pY3[0:64, h, :], lhsT=ARz[:, c, ch, 0, hp, :], rhs=Sb[d][:, c, :], start=False, stop=True),
                             reads=['ARz', ('Sb', d)], writes=[pkY], pe_acc=True)
                    for c in range(4):
                        S.op('dve', lambda: V.scalar_tensor_tensor(out=S32[d][:, c, :], in0=S32[d][:, c, :], scalar=pdec[:, c, ch:ch + 1], in1=pS3[:, c, :], op0=ALU.mult, op1=ALU.add),
                             reads=[('S32', d), 'pdec', pkS], writes=[('S32', d)])
                    S.op('act', lambda: ACT.copy(out=Sb[d], in_=S32[d]), reads=[('S32', d)], writes=[('Sb', d)])
                    yb_ = ysb[yev % 2]
                    S.op('act', lambda: ACT.copy(out=yb_, in_=pY[0:64, :]), reads=[pkY], writes=[('ysb', yev % 2)])
                    S.dma('sp', y_d[d, gch * 64:(gch + 1) * 64, :], yb_, reads=[('ysb', yev % 2)], writes=[('yscr', d, gch // 2)])
                    yev += 1
                    yield

                chs = list(range(NCH)) if d == 0 else list(range(NCH - 1, -1, -1))
                run_tasks([gen_D(chs[i_], i_, i_) for i_ in range(NCH)])
                pendQ = chain([gen_Q(chs[i_], i_) for i_ in range(NCH)])
        if pendQ is not None:
            run_tasks([pendQ])
            pendQ = None
        S.barrier()


    def phase_post(zsT, yaT, base):
        rT4 = zsT[:, 0:4, :]
        kT4 = zsT[:, 4:8, :]
        adT = zsT[:, 13, :]
        gdT = zsT[:, 14, :]
        AR.seek(base)
        gnwB = AR.alloc([128, 512], F32)
        gnbB = AR.alloc([128, 512], F32)
        P2 = lambda shape, dt: [AR.alloc(shape, dt) for _ in range(2)]
        Yf, Yb = P2([128, 512], F32), P2([128, 512], F32)
        ta0, ta1 = P2([128, 4, 128], F32), P2([128, 4, 128], F32)
        kf2 = P2([128, 4, 128], F32)
        prod2 = P2([128, 4, 128], BF16)
        rows2 = P2([128, 8], F32)
        bon2 = P2([128, 512], F32)
        y2 = P2([128, 512], F32)
        sq2 = P2([128, 512], F32)
        st2 = P2([128, 4, 8], F32)
        yab2 = P2([128, 512], BF16)
        S.dma('sp', gnwB, gnw_d.partition_broadcast(128), writes=['gnwB'])
        S.dma('sp', gnbB, gnb_d.partition_broadcast(128), writes=['gnbB'])
        def gen_tile(i):
            b = i % 2
            sl = slice(i * 128, (i + 1) * 128)
            ta = (ta0[b], ta1[b])
            kf, prod, rows, bon, y, sq, st, yab = kf2[b], prod2[b], rows2[b], bon2[b], y2[b], sq2[b], st2[b], yab2[b]
            bA, bB, bC, bD = 4 * b, 4 * b + 1, 4 * b + 2, 4 * b + 3
            S.dma('sp', Yf[b], y_d[0, sl, :], writes=[('Yf', b)])
            S.dma('sp', Yb[b], y_d[1, sl, :], writes=[('Yb', b)])
            for d in range(2):
                bk_ = bA if d == 0 else bB
                pz3 = ps[bk_].rearrange("p (c n) -> p c n", c=4)
                for c in range(4):
                    S.op('pe', lambda: PE.matmul(pz3[:, c, :], lhsT=a2c[:, d, c * 128:(c + 1) * 128], rhs=adT[:, sl], start=True, stop=True),
                         reads=['a2c'], writes=[psk[bk_]], pe_acc=True)
                for c in range(4):
                    S.op('act', lambda: ACT.activation(out=ta[d][:, c, :], in_=pz3[:, c, :], func=AF.Sigmoid, bias=col("a0", d * 4 + c), scale=1.0),
                         reads=[psk[bk_], 'pp'], writes=[('ta', b, d)])
            yield
            S.op('pool', lambda: POOL.tensor_tensor(out=ta[0], in0=ta[0], in1=ta[1], op=ALU.add), reads=[('ta', b, 0), ('ta', b, 1)], writes=[('ta', b, 0)])
            for c in range(4):
                S.op('pool', lambda: POOL.tensor_scalar(out=kf[:, c, :], in0=ta[0][:, c, :], scalar1=kar[:, c:c + 1], scalar2=c2r[:, c:c + 1], op0=ALU.mult, op1=ALU.add),
                     reads=[('ta', b, 0), 'kar'], writes=[('kf', b)])
            S.op('pool', lambda: POOL.tensor_tensor(out=kf, in0=kf, in1=kT4[:, :, sl], op=ALU.mult), reads=[('kf', b)], writes=[('kf', b)])
            S.op('pool', lambda: POOL.tensor_tensor(out=prod, in0=kf, in1=rT4[:, :, sl], op=ALU.mult), reads=[('kf', b)], writes=[('prod', b)])
            yield
            pr = ps[bB]
            for c in range(4):
                S.op('pe', lambda: PE.matmul(pr[:, c * 2:(c + 1) * 2], lhsT=prod[:, c, :], rhs=cmb[:, HSEL, 0:2], start=True, stop=True),
                     reads=[('prod', b), 'cmb'], writes=[psk[bB]], pe_acc=True)
            S.op('act', lambda: ACT.copy(out=rows, in_=pr[:, 0:8]), reads=[psk[bB]], writes=[('rows', b)])
            yield
            pv = ps[bD].bitcast(BF16)[:, 0:512]
            for c in range(4):
                S.op('pe', lambda: PE.transpose(out=pv[:, c * 128:(c + 1) * 128], in_=zsT[:, 8 + c, sl], identity=ident),
                     reads=['ident'], writes=[psk[bD]], pe_acc=True)
            S.op('dve', lambda: V.tensor_tensor(out=bon.rearrange("p (h v) -> p h v", h=8), in0=pv.rearrange("p (h v) -> p h v", h=8),
                                                in1=bc(rows.rearrange("p (h o) -> p h o", o=1), [128, 8, 64]), op=ALU.mult),
                 reads=[psk[bD], ('rows', b)], writes=[('bon', b)])
            yield
            pg = ps[bC]
            S.op('pe', lambda: PE.matmul(pg, lhsT=gdT[:, sl], rhs=g2, start=True, stop=True), reads=['g2'], writes=[psk[bC]], pe_acc=True)
            y3 = y.rearrange("p (h v) -> p h v", h=8)
            sq3 = sq.rearrange("p (h v) -> p h v", h=8)
            S.op('dve', lambda: V.tensor_tensor(out=y, in0=Yf[b], in1=Yb[b], op=ALU.add), reads=[('Yf', b), ('Yb', b)], writes=[('y', b)])
            yield
            S.op('dve', lambda: V.tensor_reduce(out=st[:, 0, :], in_=y3, axis=AX.X, op=ALU.add), reads=[('y', b)], writes=[('st0', b)])
            S.op('dve', lambda: V.tensor_scalar(out=st[:, 1, :], in0=st[:, 0, :], scalar1=-1.0 / 64, scalar2=None, op0=ALU.mult), reads=[('st0', b)], writes=[('st1', b)])
            S.op('dve', lambda: V.tensor_tensor(out=y3, in0=y3, in1=bc(st[:, 1, :].rearrange("p (h o) -> p h o", o=1), [128, 8, 64]), op=ALU.add),
                 reads=[('y', b), ('st1', b)], writes=[('y', b)])
            yield
            S.op('act', lambda: ACT.activation(out=sq, in_=y, func=AF.Square), reads=[('y', b)], writes=[('sq', b)])
            S.op('dve', lambda: V.tensor_reduce(out=st[:, 2, :], in_=sq3, axis=AX.X, op=ALU.add), reads=[('sq', b)], writes=[('st2', b)])
            yield
            S.op('act', lambda: ACT.activation(out=st[:, 3, :], in_=st[:, 2, :], func=AF.Sqrt, bias=epsc[:, 1:2], scale=1.0 / 64), reads=[('st2', b), 'epsc'], writes=[('st3', b)])
            S.op('dve', lambda: V.reciprocal(out=st[:, 3, :], in_=st[:, 3, :]), reads=[('st3', b)], writes=[('st3', b)])
            S.op('dve', lambda: V.tensor_tensor(out=y3, in0=y3, in1=bc(st[:, 3, :].rearrange("p (h o) -> p h o", o=1), [128, 8, 64]), op=ALU.mult),
                 reads=[('y', b), ('st3', b)], writes=[('y', b)])
            yield
            S.op('dve', lambda: V.tensor_tensor(out=y, in0=y, in1=gnwB, op=ALU.mult), reads=[('y', b), 'gnwB'], writes=[('y', b)])
            S.op('pool', lambda: POOL.tensor_tensor(out=bon, in0=bon, in1=gnbB, op=ALU.add), reads=[('bon', b), 'gnbB'], writes=[('bon', b)])
            S.op('dve', lambda: V.tensor_tensor(out=y, in0=y, in1=bon, op=ALU.add), reads=[('y', b), ('bon', b)], writes=[('y', b)])
            S.op('dve', lambda: V.tensor_tensor(out=yab, in0=y, in1=pg, op=ALU.mult), reads=[('y', b), psk[bC]], writes=[('yab', b)])
            yield
            pt = ps[bA].bitcast(BF16)[:, 0:512]
            for c in range(4):
                S.op('pe', lambda: PE.transpose(out=pt[:, c * 128:(c + 1) * 128], in_=yab[:, c * 128:(c + 1) * 128], identity=ident),
                     reads=[('yab', b), 'ident'], writes=[psk[bA]], pe_acc=True)
            S.op('act', lambda: ACT.copy(out=yaT[:, :, sl], in_=pt.rearrange("p (c n) -> p c n", c=4)), reads=[psk[bA]], writes=['yaT'])
            yield

        def run_tasks(tasks):
            tasks = list(tasks)
            while tasks:
                for t_ in list(tasks):
                    try:
                        next(t_)
                    except StopIteration:
                        tasks.remove(t_)

        for i in range(0, NT, 2):
            run_tasks([gen_tile(i), gen_tile(i + 1)])

    def phase_attn(s, uT, ybT, baseA, baseB):
        AR.seek(baseA)
        cosT = AR.alloc([128, T], F32)
        sinT = AR.alloc([128, T], F32)
        qT = AR.alloc([128, 4, T], BF16)
        kTt = AR.alloc([128, T], BF16)
        vp = AR.alloc([128, 2, NT, 128], BF16)
        AR.seek(baseB)
        wq = [AR.alloc([128, 8, 128], BF16) for _ in range(2)]
        qfL = [AR.alloc([128, 512], F32) for _ in range(2)]
        sqbL = [AR.alloc([128, 512], BF16) for _ in range(2)]
        rsL = [AR.alloc([128, 512], F32) for _ in range(2)]
        qnL = [AR.alloc([128, 512], F32) for _ in range(2)]
        qnbL = [AR.alloc([128, 512], BF16) for _ in range(2)]
        t1L = [AR.alloc([128, 512], F32) for _ in range(2)]
        t2L = [AR.alloc([128, 512], F32) for _ in range(2)]
        pTs = [AR.alloc([128, 512], BF16) for _ in range(6)]
        dn = AR.alloc([128, 512], F32)
        posi = AR.alloc([128, T], I32)
        ang = AR.alloc([128, T], F32)
        ki = AR.alloc([128, T], I32)
        kf = AR.alloc([128, T], F32)
        m1 = AR.alloc([128, T], F32)
        S.dma('sp', posi, pos_d[s].partition_broadcast(128), writes=['posi'])

        def table(dst, shift):
            S.op('dve', lambda: V.tensor_copy(out=ang, in_=posi), reads=['posi'], writes=['ang'])
            S.op('dve', lambda: V.tensor_scalar(out=ang, in0=ang, scalar1=col("invf"), scalar2=shift, op0=ALU.mult, op1=ALU.add), reads=['ang', 'pp'], writes=['ang'])
            S.op('dve', lambda: V.tensor_scalar(out=ki, in0=ang, scalar1=1.0 / TWO_PI, scalar2=None, op0=ALU.mult), reads=['ang'], writes=['ki'])
            S.op('pool', lambda: POOL.tensor_copy(out=kf, in_=ki), reads=['ki'], writes=['kf'])
            S.op('dve', lambda: V.scalar_tensor_tensor(out=ang, in0=kf, scalar=-C1, in1=ang, op0=ALU.mult, op1=ALU.add), reads=['kf', 'ang'], writes=['ang'])
            S.op('dve', lambda: V.scalar_tensor_tensor(out=ang, in0=kf, scalar=-C2, in1=ang, op0=ALU.mult, op1=ALU.add), reads=['kf', 'ang'], writes=['ang'])
            S.op('dve', lambda: V.tensor_scalar(out=m1, in0=ang, scalar1=float(np.pi), scalar2=-TWO_PI, op0=ALU.is_gt, op1=ALU.mult), reads=['ang'], writes=['m1'])
            S.op('pool', lambda: POOL.tensor_tensor(out=ang, in0=ang, in1=m1, op=ALU.add), reads=['ang', 'm1'], writes=['ang'])
            S.op('dve', lambda: V.tensor_scalar(out=m1, in0=ang, scalar1=float(-np.pi), scalar2=TWO_PI, op0=ALU.is_lt, op1=ALU.mult), reads=['ang'], writes=['m1'])
            S.op('pool', lambda: POOL.tensor_tensor(out=ang, in0=ang, in1=m1, op=ALU.add), reads=['ang', 'm1'], writes=['ang'])
            S.op('act', lambda: ACT.activation(out=dst, in_=ang, func=AF.Sin), reads=['ang'], writes=['tab'])

        table(sinT, 0.0)
        table(cosT, float(np.pi / 2))
        def c0_of(c):
            return 1920 + c * 128 if c < 4 else 2432

        def gen_qk(c, tb, L):
            b = c % 2
            gcol = col("qg") if c < 4 else col("kg")
            sl = slice(tb * 512, (tb + 1) * 512)
            qf_, sqb_, rs_, qn_, qnb_, t1_, t2_ = qfL[L], sqbL[L], rsL[L], qnL[L], qnbL[L], t1L[L], t2L[L]
            pz, pk = ps[L], psk[L]
            for k in range(8):
                S.op('pe', lambda: PE.matmul(pz, lhsT=wq[b][:, k, :], rhs=uT[:, k, sl], start=(k == 0), stop=(k == 7)),
                     reads=[('wq', b), ('uT', tb)], writes=[pk], pe_acc=True)
            S.op('act', lambda: ACT.copy(out=qf_, in_=pz), reads=[pk], writes=[('qf', L)])
            S.op('act', lambda: ACT.activation(out=sqb_, in_=qf_, func=AF.Square), reads=[('qf', L)], writes=[('sqb', L)])
            yield
            pr, pkr = ps[2 + L], psk[2 + L]
            S.op('pe', lambda: PE.matmul(pr, lhsT=cmb[:, BLK1, :], rhs=sqb_, start=True, stop=True), reads=[('sqb', L), 'cmb'], writes=[pkr], pe_acc=True)
            S.op('act', lambda: ACT.activation(out=rs_, in_=pr, func=AF.Sqrt, bias=epsc[:, 0:1], scale=1.0 / 64), reads=[pkr, 'epsc'], writes=[('rs', L)])
            yield
            S.op('dve', lambda: V.reciprocal(out=rs_, in_=rs_), reads=[('rs', L)], writes=[('rs', L)])
            S.op('dve', lambda: V.scalar_tensor_tensor(out=qn_, in0=qf_, scalar=gcol, in1=rs_, op0=ALU.mult, op1=ALU.mult), reads=[('qf', L), ('rs', L), 'pp'], writes=[('qn', L)])
            S.op('act', lambda: ACT.copy(out=qnb_, in_=qn_), reads=[('qn', L)], writes=[('qnb', L)])
            yield
            pro, pkro = ps[4 + L], psk[4 + L]
            S.op('pe', lambda: PE.matmul(pro, lhsT=cmb[:, ROT, :], rhs=qnb_, start=True, stop=True), reads=[('qnb', L), 'cmb'], writes=[pkro], pe_acc=True)
            S.op('pool', lambda: POOL.tensor_tensor(out=t1_, in0=qn_, in1=cosT[:, sl], op=ALU.mult), reads=[('qn', L), 'tab'], writes=[('t1', L)])
            yield
            S.op('dve', lambda: V.tensor_tensor(out=t2_, in0=pro, in1=sinT[:, sl], op=ALU.mult), reads=[pkro, 'tab'], writes=[('t2', L)])
            dst = qT[:, c, sl] if c < 4 else kTt[:, sl]
            S.op('dve', lambda: V.tensor_tensor(out=dst, in0=t1_, in1=t2_, op=ALU.add), reads=[('t1', L), ('t2', L)], writes=['qk'])
            yield

        def run_tasks(tasks):
            tasks = list(tasks)
            while tasks:
                for t_ in list(tasks):
                    try:
                        next(t_)
                    except StopIteration:
                        tasks.remove(t_)

        S.dma('pool', wq[0], win_d[:, c0_of(0):c0_of(0) + 128].rearrange("(k p) n -> p k n", p=128), writes=[('wq', 0)])
        for c in range(5):
            if c + 1 < 5:
                S.dma('pool', wq[(c + 1) % 2], win_d[:, c0_of(c + 1):c0_of(c + 1) + 128].rearrange("(k p) n -> p k n", p=128), writes=[('wq', (c + 1) % 2)])
            for tb in range(0, NB, 2):
                run_tasks([gen_qk(c, tb, 0), gen_qk(c, tb + 1, 1)])
        S.op('pool', lambda: POOL.memset(vp.rearrange("p a b c -> p (a b c)"), 0.0), writes=['vp'])
        S.dma('pool', wq[0], win_d[:, 2560:2688].rearrange("(k p) n -> p k n", p=128), writes=[('wq', 0)])
        for i in range(NT):
            pz, pk = ps[i % 2], psk[i % 2]
            for k in range(8):
                S.op('pe', lambda: PE.matmul(pz[:, 0:128], lhsT=uT[:, k, i * 128:(i + 1) * 128], rhs=wq[0][:, k, :], start=(k == 0), stop=(k == 7)),
                     reads=[('wq', 0), ('uT', i // 4)], writes=[pk], pe_acc=True)
            S.op('act', lambda: ACT.copy(out=vp[:, 0, i, 0:64], in_=pz[:, 0:64]), reads=[pk], writes=['vp'])
            S.op('dve', lambda: V.tensor_copy(out=vp[:, 1, i, 64:128], in_=pz[:, 64:128]), reads=[pk], writes=['vp'])
        for n in range(NT):
            qs = slice(n * 128, (n + 1) * 128)
            kbs = [kb for kb in (n - 1, n, n + 1) if 0 <= kb < NT]
            items = [(g, kb) for g in range(2) for kb in kbs]
            for idx, (g, kb) in enumerate(items):
                gp = slice(g * 64, (g + 1) * 64)
                pz, pk = ps[idx % 4], psk[idx % 4]
                S.op('pe', lambda: PE.matmul(pz.rearrange("p (j q) -> p j q", j=4), lhsT=kTt[gp, kb * 128:(kb + 1) * 128], rhs=qT[gp, :, qs], start=True, stop=True),
                     reads=['qk'], writes=[pk], pe_acc=True)
                pt_ = pTs[idx]
                S.op('act', lambda: ACT.activation(out=pt_, in_=pz, func=AF.Exp, scale=0.125), reads=[pk], writes=[('pT', idx)])
                if kb != n:
                    mk = cmb[:, MPREV if kb < n else MNEXT, :]
                    S.op('pool', lambda: POOL.tensor_tensor(out=pt_.rearrange("p (j q) -> p j q", j=4), in0=pt_.rearrange("p (j q) -> p j q", j=4),
                                                            in1=bc(mk.rearrange("p (o q) -> p o q", o=1), [128, 4, 128]), op=ALU.mult),
                         reads=[('pT', idx), 'cmb'], writes=[('pT', idx)])
            po, pko = ps[4 + n % 2], psk[4 + n % 2]
            pd_, pkd = ps[6 + n % 2], psk[6 + n % 2]
            for idx, (g, kb) in enumerate(items):
                S.op('pe', lambda: PE.matmul(po, lhsT=vp[:, g, kb, :], rhs=pTs[idx], start=(idx == 0), stop=(idx == len(items) - 1)),
                     reads=['vp', ('pT', idx)], writes=[pko], pe_acc=True)
            for idx, (g, kb) in enumerate(items):
                S.op('pe', lambda: PE.matmul(pd_, lhsT=cmb[:, VP[g], :], rhs=pTs[idx], start=(idx == 0), stop=(idx == len(items) - 1)),
                     reads=['cmb', ('pT', idx)], writes=[pkd], pe_acc=True)
            S.op('dve', lambda: V.tensor_tensor(out=dn.rearrange("p (j q) -> p j q", j=4), in0=pd_.rearrange("p (j q) -> p j q", j=4),
                                                in1=bc(esk.rearrange("p (j o) -> p j o", o=1), [128, 4, 128]), op=ALU.add), reads=[pkd, 'esk'], writes=['dn'])
            S.op('dve', lambda: V.reciprocal(out=dn, in_=dn), reads=['dn'], writes=['dn'])
            S.op('dve', lambda: V.tensor_tensor(out=ybT[:, :, qs], in0=po.rearrange("p (j q) -> p j q", j=4), in1=dn.rearrange("p (j q) -> p j q", j=4), op=ALU.mult),
                 reads=[pko, 'dn'], writes=['ybT'])

    def phase_merge(uT, yaT, ybT, mergedT, offs):
        AR.seek(offs[0])
        prw = AR.alloc([128, 4, D], BF16)
        AR.seek(offs[1])
        pat = AR.alloc([128, 4, D], BF16)
        wga = [AR.alloc([128, 8, 128], BF16) for _ in range(2)]
        wgb = [AR.alloc([128, 8, 128], BF16) for _ in range(2)]
        sgaP = [AR.alloc([128, 512], BF16) for _ in range(2)]
        sgbP = [AR.alloc([128, 512], BF16) for _ in range(2)]
        t1P = [AR.alloc([128, 512], F32) for _ in range(2)]
        t2P = [AR.alloc([128, 512], F32) for _ in range(2)]
        for hh in range(2):
            S.dma('pool', prw[:, hh * 2:(hh + 1) * 2, :], prw_d[hh * 256:(hh + 1) * 256, :].rearrange("(k p) n -> p k n", p=128), writes=['prw'])
            S.dma('pool', pat[:, hh * 2:(hh + 1) * 2, :], pat_d[hh * 256:(hh + 1) * 256, :].rearrange("(k p) n -> p k n", p=128), writes=['pat'])
        for oc in range(8):
            b = oc % 2
            S.dma('pool', wga[b], win_d[:, 2688 + oc * 128:2688 + (oc + 1) * 128].rearrange("(k p) n -> p k n", p=128), writes=[('wga', b)])
            S.dma('pool', wgb[b], win_d[:, 3712 + oc * 128:3712 + (oc + 1) * 128].rearrange("(k p) n -> p k n", p=128), writes=[('wgb', b)])
            for tb in range(NB):
                sl = slice(tb * 512, (tb + 1) * 512)
                L = tb % 2
                sga, sgb, t1, t2 = sgaP[L], sgbP[L], t1P[L], t2P[L]
                for k in range(8):
                    S.op('pe', lambda: PE.matmul(ps[0 + 4 * L], lhsT=wga[b][:, k, :], rhs=uT[:, k, sl], start=(k == 0), stop=(k == 7)),
                         reads=[('wga', b), ('uT', tb)], writes=[psk[0 + 4 * L]], pe_acc=True)
                S.op('act', lambda: ACT.activation(out=sga, in_=ps[0 + 4 * L], func=AF.Sigmoid), reads=[psk[0 + 4 * L]], writes=[('sga', L)])
                for k in range(8):
                    S.op('pe', lambda: PE.matmul(ps[1 + 4 * L], lhsT=wgb[b][:, k, :], rhs=uT[:, k, sl], start=(k == 0), stop=(k == 7)),
                         reads=[('wgb', b), ('uT', tb)], writes=[psk[1 + 4 * L]], pe_acc=True)
                S.op('act', lambda: ACT.activation(out=sgb, in_=ps[1 + 4 * L], func=AF.Sigmoid), reads=[psk[1 + 4 * L]], writes=[('sgb', L)])
                for k in range(4):
                    S.op('pe', lambda: PE.matmul(ps[2 + 4 * L], lhsT=prw[:, k, oc * 128:(oc + 1) * 128], rhs=yaT[:, k, sl], start=(k == 0), stop=(k == 3)),
                         reads=['prw', 'yaT'], writes=[psk[2 + 4 * L]], pe_acc=True)
                for k in range(4):
                    S.op('pe', lambda: PE.matmul(ps[3 + 4 * L], lhsT=pat[:, k, oc * 128:(oc + 1) * 128], rhs=ybT[:, k, sl], start=(k == 0), stop=(k == 3)),
                         reads=['pat', 'ybT'], writes=[psk[3 + 4 * L]], pe_acc=True)
                S.op('dve', lambda: V.tensor_tensor(out=t1, in0=ps[2 + 4 * L], in1=sga, op=ALU.mult), reads=[psk[2 + 4 * L], ('sga', L)], writes=[('t1', L)])
                S.op('dve', lambda: V.tensor_tensor(out=t2, in0=ps[3 + 4 * L], in1=sgb, op=ALU.mult), reads=[psk[3 + 4 * L], ('sgb', L)], writes=[('t2', L)])
                S.op('pool', lambda: POOL.tensor_tensor(out=mergedT[:, oc, sl], in0=t1, in1=t2, op=ALU.add), reads=[('t1', L), ('t2', L)], writes=[('mg', tb)])

    def phase_x1(s, mergedT, u2tm, base):
        AR.seek(base)
        wo = AR.alloc([128, 8, D], BF16)
        gt1B = AR.alloc([128, D], F32)
        sc2 = AR.alloc([128, D], F32)
        sh2 = AR.alloc([128, D], F32)
        xt = [AR.alloc([128, D], F32) for _ in range(2)]
        x1t = [AR.alloc([128, D], F32) for _ in range(2)]
        tmpP = [AR.alloc([128, D], F32) for _ in range(2)]
        junkP = [AR.alloc([128, D], BF16) for _ in range(2)]
        u2TP = [AR.alloc([128, 8, 128], BF16) for _ in range(2)]
        ssP = [AR.alloc([128, 8], F32) for _ in range(2)]
        exP = [AR.alloc([128, E], F32) for _ in range(2)]
        for hh in range(4):
            S.dma('pool', wo[:, hh * 2:(hh + 1) * 2, :], wout_d[hh * 256:(hh + 1) * 256, :].rearrange("(k p) n -> p k n", p=128), writes=['wo'])
        S.dma('sp', gt1B, mod_d[s, 2], writes=['gt1B'])
        S.dma('sp', sc2, mod_d[s, 4], writes=['sc2'])
        S.dma('sp', sh2, mod_d[s, 3], writes=['sh2'])
        lg = AR.alloc([128, NT, E], F32)
        mxs = AR.alloc([128, 3, NT], F32)

        def gen_x1(i):
            b = i % 2
            sl = slice(i * 128, (i + 1) * 128)
            S.dma('sp', xt[b], x_d[s, sl, :], writes=[('xt', b)])
            tmp, junk, u2T, ss = tmpP[b], junkP[b], u2TP[b], ssP[b]
            for cb in range(2):
                for k in range(8):
                    S.op('pe', lambda: PE.matmul(ps[cb + 6 * b], lhsT=mergedT[:, k, sl], rhs=wo[:, k, cb * 512:(cb + 1) * 512], start=(k == 0), stop=(k == 7)),
                         reads=[('mg', i // 4), 'wo'], writes=[psk[cb + 6 * b]], pe_acc=True)
                S.op('dve', lambda: V.tensor_tensor(out=tmp[:, cb * 512:(cb + 1) * 512], in0=ps[cb + 6 * b], in1=gt1B[:, cb * 512:(cb + 1) * 512], op=ALU.mult),
                     reads=[psk[cb + 6 * b], 'gt1B'], writes=[('tmp', b, cb)])
            yield
            S.op('pool', lambda: POOL.tensor_tensor(out=x1t[b], in0=tmp, in1=xt[b], op=ALU.add), reads=[('tmp', b, 0), ('tmp', b, 1), ('xt', b)], writes=[('x1t', b)])
            S.dma('sp', out_d[s, sl, :], x1t[b], reads=[('x1t', b)], writes=[('outd', i)])
            S.op('act', lambda: ACT.activation(out=junk, in_=x1t[b], func=AF.Square, accum_out=ss[:, 0:1]), reads=[('x1t', b)], writes=[('junk', b), ('ss0', b)])
            yield
            S.op('act', lambda: ACT.activation(out=ss[:, 1:2], in_=ss[:, 0:1], func=AF.Sqrt, bias=epsc[:, 0:1], scale=1.0 / D), reads=[('ss0', b), 'epsc'], writes=[('ss1', b)])
            S.op('dve', lambda: V.reciprocal(out=ss[:, 1:2], in_=ss[:, 1:2]), reads=[('ss1', b)], writes=[('ss1', b)])
            S.op('dve', lambda: V.scalar_tensor_tensor(out=tmp, in0=x1t[b], scalar=ss[:, 1:2], in1=sc2, op0=ALU.mult, op1=ALU.mult),
                 reads=[('x1t', b), ('ss1', b), 'sc2'], writes=[('tmp', b, 0), ('tmp', b, 1)])
            yield
            S.op('pool', lambda: POOL.tensor_tensor(out=u2tm[:, i, :], in0=tmp, in1=sh2, op=ALU.add), reads=[('tmp', b, 0), ('tmp', b, 1), 'sh2'], writes=[('u2', i)])
            pz = ps[2 + b].bitcast(BF16).rearrange("p (k t) -> p k t", k=8)
            pk = psk[2 + b]
            for k in range(8):
                S.op('pe', lambda: PE.transpose(out=pz[:, k, :], in_=u2tm[:, i, k * 128:(k + 1) * 128], identity=ident),
                     reads=[('u2', i), 'ident'], writes=[pk], pe_acc=True)
            yield
            S.op('act', lambda: ACT.copy(out=u2T, in_=pz), reads=[pk], writes=[('u2T', b)])
            pl, pkl = ps[4 + b], psk[4 + b]
            for k in range(8):
                S.op('pe', lambda: PE.matmul(pl[:, 0:E], lhsT=u2T[:, k, :], rhs=wr[:, k, :], start=(k == 0), stop=(k == 7)),
                     reads=[('u2T', b), 'wr'], writes=[pkl], pe_acc=True)
            yield
            S.op('act', lambda: ACT.copy(out=lg[:, i, :], in_=pl[:, 0:E]), reads=[pkl], writes=['lg'])
            yield

        def run_tasks(tasks):
            tasks = list(tasks)
            while tasks:
                for t_ in list(tasks):
                    try:
                        next(t_)
                    except StopIteration:
                        tasks.remove(t_)

        for i in range(0, NT, 2):
            run_tasks([gen_x1(i), gen_x1(i + 1)])
        S.op('dve', lambda: V.tensor_reduce(out=mxs[:, 0, :], in_=lg, axis=AX.X, op=ALU.max), reads=['lg'], writes=['mx0'])
        S.op('dve', lambda: V.tensor_tensor(out=lg, in0=lg, in1=bc(mxs[:, 0, :].rearrange("p (i o) -> p i o", o=1), [128, NT, E]), op=ALU.subtract),
             reads=['lg', 'mx0'], writes=['lg'])
        S.op('act', lambda: ACT.activation(out=lg, in_=lg, func=AF.Exp), reads=['lg'], writes=['lg'])
        S.op('dve', lambda: V.tensor_reduce(out=mxs[:, 1, :], in_=lg, axis=AX.X, op=ALU.add), reads=['lg'], writes=['mx1'])
        S.op('dve', lambda: V.reciprocal(out=mxs[:, 2, :], in_=mxs[:, 1, :]), reads=['mx1'], writes=['mx2'])
        S.op('dve', lambda: V.tensor_tensor(out=afftm, in0=lg, in1=bc(mxs[:, 2, :].rearrange("p (i o) -> p i o", o=1), [128, NT, E]), op=ALU.mult),
             reads=['lg', 'mx2'], writes=['afftm'])

    def phase_moe(s, u2tm, base):
        AR.seek(base)
        affT = AR.alloc([16, T], F32)
        work = AR.alloc([16, T], F32)
        maskT = AR.alloc([16, T], F32)
        slotT = AR.alloc([16, T], F32)
        mx8 = AR.alloc([16, 8], F32)
        for i in range(NT):
            pz = ps[i // 4]
            S.op('pe', lambda: PE.transpose(out=pz[0:16, (i % 4) * 128:(i % 4 + 1) * 128], in_=afftm[:, i, :], identity=identf),
                 reads=['afftm', 'identf'], writes=[psk[i // 4]], pe_acc=True)
        for q in range(4):
            S.op('act', lambda: ACT.copy(out=affT[:, q * 512:(q + 1) * 512], in_=ps[q][0:16, :]), reads=[psk[q]], writes=['affT'])
        S.op('dve', lambda: V.tensor_copy(out=work, in_=affT), reads=['affT'], writes=['work'])
        for it in range(CAP // 8):
            S.op('dve', lambda: V.max(out=mx8, in_=work), reads=['work'], writes=['mx8'])
            if it < CAP // 8 - 1:
                S.op('dve', lambda: V.match_replace(out=work, in_to_replace=mx8, in_values=work, imm_value=-1.0), reads=['work', 'mx8'], writes=['work'])
        S.op('dve', lambda: V.tensor_scalar(out=maskT, in0=affT, scalar1=mx8[:, 7:8], scalar2=None, op0=ALU.is_ge), reads=['affT', 'mx8'], writes=['maskT'])
        S.op('pool', lambda: POOL.memset(work, 1.0), reads=['work'], writes=['work'])
        S.op('dve', lambda: V.tensor_tensor_scan(out=slotT, data0=work, data1=maskT, initial=0.0, op0=ALU.mult, op1=ALU.add), reads=['work', 'maskT'], writes=['slotT'])
        S.op('dve', lambda: V.tensor_tensor(out=slotT, in0=slotT, in1=maskT, op=ALU.mult), reads=['slotT', 'maskT'], writes=['slotT'])
        S.op('dve', lambda: V.tensor_scalar(out=slotT, in0=slotT, scalar1=-1.0, scalar2=None, op0=ALU.add), reads=['slotT'], writes=['slotT'])
        pz = ps[4]
        for i in range(NT):
            S.op('pe', lambda: PE.transpose(out=pz[:, i * 16:(i + 1) * 16], in_=slotT[:, i * 128:(i + 1) * 128], identity=identf[0:16, 0:16]),
                 reads=['slotT', 'identf'], writes=[psk[4]], pe_acc=True)
        S.op('act', lambda: ACT.copy(out=slot_tm.rearrange("p i e -> p (i e)"), in_=pz[:, 0:256]), reads=[psk[4]], writes=['slot_tm'])
        S.op('dve', lambda: V.tensor_copy(out=affhl[:, :, :, 0], in_=afftm), reads=['afftm'], writes=['affhl'])
        S.op('dve', lambda: V.tensor_tensor(out=affhl[:, :, :, 1], in0=afftm, in1=affhl[:, :, :, 0], op=ALU.subtract), reads=['afftm', 'affhl'], writes=['affhl'])
        S.barrier()
        AR.seek(base)
        ye = AR.alloc([128, E, 2, D], BF16)
        Wg = AR.alloc([128, 8, D], BF16)
        Wu = AR.alloc([128, 8, D], BF16)
        Wd = AR.alloc([128, 8, D], BF16)
        wbase = AR.ptr
        Pe = AR.alloc([128, NT, CAP], BF16)
        xeT = AR.alloc([128, 8, CAP], BF16)
        hT = AR.alloc([128, 8, CAP], BF16)
        hs = AR.alloc([128, CAP], F32)
        affs = AR.alloc([128, 4], F32)
        gt2B = AR.alloc([128, D], F32)
        S.dma('sp', gt2B, mod_d[s, 5], writes=['gt2B'])
        for e in range(E):
            for (wt, wsrc, nm) in ((Wg, wg_d, 'Wg'), (Wu, wu_d, 'Wu'), (Wd, wd_d, 'Wd')):
                for hh in range(4):
                    S.dma('pool', wt[:, hh * 2:(hh + 1) * 2, :], wsrc[e, hh * 256:(hh + 1) * 256, :].rearrange("(k p) n -> p k n", p=128), writes=[(nm, hh)])
            for i in range(NT):
                S.op('dve', lambda: V.tensor_scalar(out=Pe[:, i, :], in0=iota_row, scalar1=slot_tm[:, i, e:e + 1], scalar2=None, op0=ALU.is_equal),
                     reads=['iota_row', 'slot_tm'], writes=[('Pe', i)])
            for fc in range(8):
                pz, pk = ps[fc // 2], psk[fc // 2]
                pzs = pz[:, (fc % 2) * 256:(fc % 2 + 1) * 256]
                for i in range(NT):
                    S.op('pe', lambda: PE.matmul(pzs, lhsT=u2tm[:, i, fc * 128:(fc + 1) * 128], rhs=Pe[:, i, :], start=(i == 0), stop=(i == NT - 1)),
                         reads=[('u2', i), ('Pe', i)], writes=[pk], pe_acc=True)
                S.op('act', lambda: ACT.copy(out=xeT[:, fc, :], in_=pzs), reads=[pk], writes=[('xeT', fc)])
            pa, pka = ps[4], psk[4]
            for half in range(2):
                for i in range(NT):
                    S.op('pe', lambda: PE.matmul(pa[:, half * 2:(half + 1) * 2], lhsT=Pe[:, i, half * 128:(half + 1) * 128], rhs=affhl[:, i, e, :], start=(i == 0), stop=(i == NT - 1)),
                         reads=[('Pe', i), 'affhl'], writes=[pka], pe_acc=True)
            S.op('dve', lambda: V.tensor_reduce(out=affs[:, 0:2], in_=pa[:, 0:4].rearrange("p (h t) -> p h t", t=2), axis=AX.X, op=ALU.add), reads=[pka], writes=['affs'])
            for fk in range(8):
                pg, pkg = ps[5], psk[5]
                pu, pku = ps[6], psk[6]
                for k in range(8):
                    S.op('pe', lambda: PE.matmul(pg[:, 0:CAP], lhsT=Wg[:, k, fk * 128:(fk + 1) * 128], rhs=xeT[:, k, :], start=(k == 0), stop=(k == 7)),
                         reads=[('Wg', k // 2), ('xeT', k)], writes=[pkg], pe_acc=True)
                for k in range(8):
                    S.op('pe', lambda: PE.matmul(pu[:, 0:CAP], lhsT=Wu[:, k, fk * 128:(fk + 1) * 128], rhs=xeT[:, k, :], start=(k == 0), stop=(k == 7)),
                         reads=[('Wu', k // 2), ('xeT', k)], writes=[pku], pe_acc=True)
                S.op('act', lambda: ACT.activation(out=hs, in_=pg[:, 0:CAP], func=AF.Silu), reads=[pkg], writes=['hs'])
                S.op('dve', lambda: V.tensor_tensor(out=hT[:, fk, :], in0=pu[:, 0:CAP], in1=hs, op=ALU.mult), reads=[pku, 'hs'], writes=[('hT', fk)])
            for half in range(2):
                for cb in range(2):
                    py, pky = ps[7] if (half * 2 + cb) % 2 else ps[4], psk[7] if (half * 2 + cb) % 2 else psk[4]
                    for fk in range(8):
                        S.op('pe', lambda: PE.matmul(py, lhsT=hT[:, fk, half * 128:(half + 1) * 128], rhs=Wd[:, fk, cb * 512:(cb + 1) * 512], start=(fk == 0), stop=(fk == 7)),
                             reads=[('hT', fk), ('Wd', fk // 2), 'affs'], writes=[pky], pe_acc=True)
                    S.op('dve', lambda: V.tensor_scalar(out=ye[:, e, half, cb * 512:(cb + 1) * 512], in0=py, scalar1=affs[:, half:half + 1], scalar2=None, op0=ALU.mult),
                         reads=[pky, 'affs'], writes=['ye'])
        S.barrier()
        AR.seek(wbase - 3 * 8 * D * 2)
        Pall = AR.alloc([128, E, CAP], BF16)
        PT = AR.alloc([128, 2 * E, 128], BF16)
        x1t = [AR.alloc([128, D], F32) for _ in range(2)]
        ot = [AR.alloc([128, D], F32) for _ in range(2)]
        for i in range(NT):
            b = i % 2
            sl = slice(i * 128, (i + 1) * 128)
            S.dma('sp', x1t[b], out_d[s, sl, :], reads=[('outd', i)], writes=[('x1t', b)])
            for e in range(E):
                S.op('dve', lambda: V.tensor_scalar(out=Pall[:, e, :], in0=iota_row, scalar1=slot_tm[:, i, e:e + 1], scalar2=None, op0=ALU.is_equal),
                     reads=['iota_row', 'slot_tm'], writes=[('Pall', e // 4)])
            for q in range(4):
                pz = ps[q].bitcast(BF16).rearrange("p (j t) -> p j t", j=8)
                for j in range(8):
                    idx = q * 8 + j
                    e, half = idx // 2, idx % 2
                    S.op('pe', lambda: PE.transpose(out=pz[:, j, :], in_=Pall[:, e, half * 128:(half + 1) * 128], identity=ident),
                         reads=[('Pall', e // 4), 'ident'], writes=[psk[q]], pe_acc=True)
                if q % 2 == 0:
                    S.op('act', lambda: ACT.copy(out=PT[:, q * 8:(q + 1) * 8, :], in_=pz), reads=[psk[q]], writes=[('PT', q)])
                else:
                    S.op('dve', lambda: V.tensor_copy(out=PT[:, q * 8:(q + 1) * 8, :], in_=pz), reads=[psk[q]], writes=[('PT', q)])
            for cb in range(2):
                po, pko = ps[4 + cb + 2 * (i % 2)], psk[4 + cb + 2 * (i % 2)]
                for idx in range(2 * E):
                    e, half = idx // 2, idx % 2
                    S.op('pe', lambda: PE.matmul(po, lhsT=PT[:, idx, :], rhs=ye[:, e, half, cb * 512:(cb + 1) * 512], start=(idx == 0), stop=(idx == 2 * E - 1)),
                         reads=[('PT', idx // 8), 'ye'], writes=[pko], pe_acc=True)
                S.op('dve', lambda: V.tensor_tensor(out=ot[b][:, cb * 512:(cb + 1) * 512], in0=po, in1=gt2B[:, cb * 512:(cb + 1) * 512], op=ALU.mult),
                     reads=[pko, 'gt2B'], writes=[('ot', b, cb)])
            S.op('dve', lambda: V.tensor_tensor(out=ot[b], in0=ot[b], in1=x1t[b], op=ALU.add), reads=[('ot', b, 0), ('ot', b, 1), ('x1t', b)], writes=[('ot', b, 0), ('ot', b, 1)])
            S.dma('sp', out_d[s, sl, :], ot[b], reads=[('ot', b, 0), ('ot', b, 1)], writes=[('outd', i)])

    def dbg_dump(src_ap, shape, key_reads=()):
        AR.seek(AR_TOP)
        t = AR.alloc(shape, F32)
        S.op('dve', lambda: V.tensor_copy(out=t, in_=src_ap), writes=['dbgt'])
        flat = t if len(shape) == 2 else t.rearrange("p a b -> p (a b)")
        S.dma('sp', dbg_d, flat, reads=['dbgt'])

    AR_TOP = 160 * 1024
    phase_adaln()
    for s in range(nseq):
        AR.seek(0)
        zsT = AR.alloc([128, 15, T], BF16)
        uT = AR.alloc([128, 8, T], BF16)
        base1 = AR.ptr
        phase_norm1(s, uT, base1)
        S.barrier()
        if dbg and dbg[0] == 'uT':
            dbg_dump(uT[:, :, 0:512], [128, 8, 512]); break
        for q in range(4):
            S.dma('sp', u_d[:, 2 * q:2 * q + 2, :], uT[:, 2 * q:2 * q + 2, :], reads=[('uT', 0), ('uT', 1), ('uT', 2), ('uT', 3)], writes=['uscr'])
        phase_rwkv_cols(uT, zsT, base1)
        S.barrier()
        if dbg and dbg[0] == 'zs':
            dbg_dump(zsT[:, :, 0:256], [128, 15, 256]); break
        AR.seek(61440)
        kkT = AR.alloc([128, 4, T], BF16)
        yaT = AR.alloc([128, 4, T], BF16)
        base3 = AR.ptr
        phase_scan(zsT, kkT, 77824)
        if dbg and dbg[0] == 'yscan':
            AR.seek(AR_TOP)
            t = AR.alloc([128, 2, 512], F32)
            S.dma('sp', t[:, 0, :], y_d[0, 0:128, :], writes=['dbgt'])
            S.dma('sp', t[:, 1, :], y_d[1, 0:128, :], writes=['dbgt'])
            S.dma('sp', dbg_d, t.rearrange("p a b -> p (a b)"), reads=['dbgt']); break
        phase_post(zsT, yaT, base3)
        S.barrier()
        if dbg and dbg[0] == 'yaT':
            dbg_dump(yaT[:, :, 0:512], [128, 4, 512]); break
        AR.seek(0)
        uT = AR.alloc([128, 8, T], BF16)
        AR.seek(94208)
        ybT = AR.alloc([128, 4, T], BF16)
        baseB = AR.ptr
        for q in range(4):
            S.dma('sp' if q % 2 == 0 else 'act', uT[:, 2 * q:2 * q + 2, :], u_d[:, 2 * q:2 * q + 2, :], writes=[('uT', 0), ('uT', 1), ('uT', 2), ('uT', 3)])
        phase_attn(s, uT, ybT, 32768, baseB)
        S.barrier()
        if dbg and dbg[0] == 'ybT':
            dbg_dump(ybT[:, :, 0:512], [128, 4, 512]); break
        AR.seek(32768)
        mergedT = AR.alloc([128, 8, T], BF16)
        phase_merge(uT, yaT, ybT, mergedT, (65536, baseB))
        S.barrier()
        if dbg and dbg[0] == 'merged':
            dbg_dump(mergedT[:, :, 0:512], [128, 8, 512]); break
        AR.seek(0)
        u2tm = AR.alloc([128, NT, D], BF16)
        phase_x1(s, mergedT, u2tm, 65536)
        S.barrier()
        if dbg and dbg[0] == 'aff':
            dbg_dump(afftm.rearrange("p i e -> p (i e)"), [128, 256]); break
        phase_moe(s, u2tm, 32768)
        S.barrier()

    S.finish('sp')
    print("ninstr", S.ninstr, "pe_incs", S.npe_inc, "arena hi", AR.hi)
    return nc


def _consts():
    cm = np.zeros((13, 128, 128), np.float32)
    p = np.arange(128)
    cm[0] = (p[:, None] // 64 == p[None, :] // 64).astype(np.float32)
    R = np.zeros((128, 128), np.float32)
    for blk in range(2):
        o = blk * 64
        for d_ in range(8):
            R[o + d_ + 8, o + d_] = -1.0
            R[o + d_, o + d_ + 8] = 1.0
    cm[1] = R
    cm[2] = (p[:, None] >= p[None, :]).astype(np.float32)
    cm[3] = (p[:, None] <= p[None, :]).astype(np.float32)
    s_ = (p % 64)[:, None]
    t_ = (p % 64)[None, :]
    a_col = (p[None, :] >= 64)
    fwd = np.where(a_col, s_ < t_, s_ <= t_)
    bwd = np.where(a_col, s_ > t_, s_ >= t_)
    cm[4] = fwd.astype(np.float32)
    cm[5] = bwd.astype(np.float32)
    cm[6] = cm[4].T
    cm[7] = cm[5].T
    cm[8][:, 0] = (p < 64)
    cm[8][:, 1] = (p >= 64)
    cm[9][:, 0:64] = 1.0
    cm[10][:, 64:128] = 1.0
    cm[11] = ((p % 64)[:, None] < (p % 64)[None, :]).astype(np.float32)
    cm[12] = ((p % 64)[:, None] > (p % 64)[None, :]).astype(np.float32)
    return np.ascontiguousarray(cm.transpose(1, 0, 2).reshape(128, 13 * 128))


def _prep_shared(inp):
    f = lambda a: np.ascontiguousarray(np.asarray(a, dtype=np.float32))
    L = 0
    w_in = f(inp["w_in"][L]).copy()
    qoff = 1920
    perm = []
    for c in range(4):
        perm += list(range(c * 64, (c + 1) * 64)) + list(range((4 + c) * 64, (5 + c) * 64))
    perm = np.array(perm)
    w_in[:, qoff:qoff + 512] = w_in[:, qoff:qoff + 512][:, perm]
    p_attn = f(inp["p_attn"][L])[perm, :]
    pp = np.zeros((128, NPP), np.float32)

    def put(name, arr):
        o, w = PP[name]
        pp[:, o:o + w] = arr

    chunked = lambda v: np.asarray(v, np.float32).reshape(-1, 128).T
    put("mp", chunked(inp["mu_prev"][L]))
    put("mn", chunked(inp["mu_next"][L]))
    put("w0", np.concatenate([chunked(inp["rwkv_w0"][L][0]), chunked(inp["rwkv_w0"][L][1])], 1))
    put("a0", np.concatenate([chunked(inp["rwkv_a0"][L][0]), chunked(inp["rwkv_a0"][L][1])], 1))
    put("kk", chunked(inp["rwkv_k_k"][L]))
    put("ka", chunked(inp["rwkv_k_a"][L]))
    put("rk", chunked(np.asarray(inp["rwkv_r_k"][L]).reshape(-1)))
    put("qg", np.tile(np.asarray(inp["q_norm_g"][L], np.float32), 2)[:, None])
    put("kg", np.tile(np.asarray(inp["k_norm_g"][L], np.float32), 2)[:, None])
    inv_freq = (500000.0 ** (-np.arange(0, 16, 2, dtype=np.float32) / 16)).astype(np.float32)
    invf = np.zeros(64, np.float32)
    invf[0:8] = inv_freq
    invf[8:16] = inv_freq
    put("invf", np.tile(invf, 2)[:, None])
    sink = np.asarray(inp["attn_sink"][L], np.float32)
    sk = np.zeros((128, 4), np.float32)
    for j in range(4):
        sk[0:64, j] = sink[j]
        sk[64:128, j] = sink[4 + j]
    put("sink", sk)
    w2cat = np.zeros((128, 2, 512), np.float32)
    a2cat = np.zeros((128, 2, 512), np.float32)
    for d_ in range(2):
        w2cat[d_ * 64:(d_ + 1) * 64, d_, :] = inp["rwkv_w2"][L][d_]
        a2cat[d_ * 64:(d_ + 1) * 64, d_, :] = inp["rwkv_a2"][L][d_]
    return {
        "w_ada": f(inp["w_ada"][L]), "b_ada": f(inp["b_ada"][L])[None, :] if np.asarray(inp["b_ada"][L]).ndim == 1 else f(inp["b_ada"][L]),
        "norm1_g": f(inp["norm1_g"][L]).reshape(1, D), "norm2_g": f(inp["norm2_g"][L]).reshape(1, D),
        "w_in": w_in, "pp": pp, "w2cat": w2cat.reshape(128, 1024), "a2cat": a2cat.reshape(128, 1024),
        "g2": f(inp["rwkv_g2"][L]), "gn_w": f(inp["rwkv_gn_w"][L]).reshape(1, 512), "gn_b": f(inp["rwkv_gn_b"][L]).reshape(1, 512),
        "p_rwkv": f(inp["p_rwkv"][L]), "p_attn": np.ascontiguousarray(p_attn), "w_out": f(inp["w_out"][L]),
        "w_router": f(inp["w_router"][L]), "w_gate": f(inp["w_gate"][L]), "w_up": f(inp["w_up"][L]), "w_down": f(inp["w_down"][L]),
        "cmats": _consts(),
    }


def _core_inputs(inp, shared, seqs):
    x = np.ascontiguousarray(np.asarray(inp["x"], np.float32)[seqs])
    c = np.asarray(inp["c"], np.float32)[seqs]
    cT = np.ascontiguousarray(c.reshape(len(seqs), 8, 128).transpose(0, 2, 1))
    pos = np.ascontiguousarray(np.asarray(inp["positions"]).astype(np.int32)[seqs][:, None, :])
    m = dict(shared)
    m.update({"x": x, "cT": cT, "pos": pos})
    return m


def kernel(**inputs):
    shared = _prep_shared(inputs)
    nc = build(NSEQ)
    in_maps = [_core_inputs(inputs, shared, list(range(i * NSEQ, (i + 1) * NSEQ))) for i in range(NCORES)]
    res = run_bass_kernel_spmd(nc, in_maps, core_ids=list(range(NCORES)))
    out = np.concatenate([np.asarray(r["out"]) for r in res.results], axis=0)
    return out.astype(np.float32)
```

```python
import numpy as np
import concourse.bass as bass
import concourse.mybir as mybir
from concourse.bass_utils import run_bass_kernel_spmd

F32 = mybir.dt.float32
BF16 = mybir.dt.bfloat16
I32 = mybir.dt.int32
ALU = mybir.AluOpType
AF = mybir.ActivationFunctionType
AX = mybir.AxisListType

T = 2048
D = 1024
NT = 16
NB = 4
NSEQ = 2
NCORES = 8
E = 16
CAP = 256
LAM = float(np.exp(-0.5))
NCH = 4
TBS = NCH * 64
NTB = T // TBS
TWO_PI = float(2 * np.pi)
C1 = 6.28125
C2 = TWO_PI - C1

PP = {}
_o = 0
for _n, _w in [("mp", 15), ("mn", 15), ("w0", 8), ("a0", 8), ("kk", 4), ("ka", 4), ("rk", 4), ("qg", 1), ("kg", 1),
               ("invf", 1), ("sink", 4)]:
    PP[_n] = (_o, _w)
    _o += _w
NPP = _o


class Ticket:
    __slots__ = ('ins', 'sem', 'val', 'parent')

    def __init__(self, ins):
        self.ins = ins
        self.sem = None
        self.val = None
        self.parent = None

    def root(self):
        t = self
        while t.parent is not None:
            t = t.parent
        return t


class Sync:
    SEM_MAX = 30000

    def __init__(self, nc):
        self.nc = nc
        self.E = {'pe': nc.tensor, 'act': nc.scalar, 'dve': nc.vector, 'pool': nc.gpsimd, 'sp': nc.sync}
        self.sem = {}
        self.cnt = {}
        self.nsem = 0
        for e in self.E:
            self._newsem(e)
        self.waited = {}
        self.lastw = {}
        self.reads = {}
        self.dma_sems = {}
        self.dma_rr = {}
        self.ninstr = 0
        self.pend = None
        self.pend_writes = None
        self.npe_inc = 0

    def _newsem(self, e):
        self.sem[e] = self.nc.alloc_semaphore(f"s_{e}_{self.nsem}")
        self.nsem += 1
        self.cnt[e] = 0

    def _flush_pe(self):
        t = self.pend
        if t is None:
            return
        if self.cnt['pe'] >= self.SEM_MAX:
            self._newsem('pe')
        self.cnt['pe'] += 1
        t.sem = self.sem['pe']
        t.val = self.cnt['pe']
        t.ins.then_inc(t.sem, 1)
        self.npe_inc += 1
        self.pend = None
        self.pend_writes = None

    def _wait(self, e, ev):
        if ev is None:
            return
        if isinstance(ev, Ticket):
            if e == 'pe':
                return
            t = ev.root()
            if t.val is None:
                assert t is self.pend
                self._flush_pe()
            sem, val = t.sem, t.val
        else:
            src, sem, val = ev
        k = (e, sem.name)
        if self.waited.get(k, 0) >= val:
            return
        self.waited[k] = val
        self.E[e].wait_ge(sem, val)

    def deps(self, e, reads, writes, pe_acc=False):
        for k in reads:
            self._wait(e, self.lastw.get(k))
        for k in writes:
            lw = self.lastw.get(k)
            if not (pe_acc and isinstance(lw, Ticket)):
                self._wait(e, lw)
            for ev in self.reads.get(k, {}).values():
                self._wait(e, ev)

    def commit(self, src, ev, reads, writes):
        for k in reads:
            self.reads.setdefault(k, {})[src] = ev
        for k in writes:
            self.lastw[k] = ev
            self.reads[k] = {}

    def op(self, e, fn, reads=(), writes=(), pe_acc=False):
        self.deps(e, reads, writes, pe_acc)
        if e == 'pe':
            ins = fn()
            t = Ticket(ins)
            if self.pend is not None:
                if self.pend_writes == tuple(writes):
                    self.pend.parent = t
                    self.pend = None
                else:
                    self._flush_pe()
            self.pend = t
            self.pend_writes = tuple(writes)
            self.commit('pe', t, reads, writes)
            self.ninstr += 1
            return t
        if self.cnt[e] >= self.SEM_MAX:
            self._newsem(e)
        ins = fn()
        self.cnt[e] += 1
        ev = (e, self.sem[e], self.cnt[e])
        ins.then_inc(self.sem[e], 1)
        self.commit(e, ev, reads, writes)
        self.ninstr += 1
        return ev

    def dma(self, e, out, in_, reads=(), writes=(), nslots=8, **kw):
        if e == 'pool':
            nslots = 2
        lst = self.dma_sems.setdefault(e, [])
        if len(lst) < nslots:
            lst.append([self.nc.alloc_semaphore(f"d_{e}_{len(lst)}"), 0])
        i = self.dma_rr.get(e, 0)
        self.dma_rr[e] = (i + 1) % nslots
        slot = lst[i % len(lst)]
        sem, uses = slot
        if uses > 0:
            self._wait(e, ('dma', sem, 16 * uses))
        self.deps(e, reads, writes)
        self.E[e].dma_start(out=out, in_=in_, **kw).then_inc(sem, 16)
        slot[1] = uses + 1
        ev = ('dma_%s_%d' % (e, i % len(lst)), sem, 16 * (uses + 1))
        self.commit(ev[0], ev, reads, writes)
        self.ninstr += 1
        return ev

    def barrier(self):
        self._flush_pe()
        evs = [(e, self.sem[e], self.cnt[e]) for e in self.E if self.cnt[e] > 0]
        for q, lst in self.dma_sems.items():
            for sem, uses in lst:
                if uses:
                    evs.append(('dma', sem, 16 * uses))
        for e in self.E:
            for ev in evs:
                if ev[0] != e:
                    self._wait(e, ev)
        self.lastw = {}
        self.reads = {}

    def finish(self, e='sp'):
        self._flush_pe()
        for q, lst in self.dma_sems.items():
            for sem, uses in lst:
                if uses:
                    self._wait(e, ('dma', sem, 16 * uses))


class Arena:
    def __init__(self, nc, name, nbytes):
        self.n4 = nbytes // 4
        self.t = nc.alloc_sbuf_tensor(name, [128, self.n4], F32).ap()
        self.ptr = 0
        self.hi = 0

    def seek(self, off):
        self.ptr = off

    def alloc(self, shape, dtype, parts=None):
        esz = 4 if dtype in (F32, I32) else 2
        n = int(np.prod(shape[1:]))
        nb = (n * esz + 31) // 32 * 32
        assert self.ptr % 4 == 0
        a = self.ptr // 4
        assert a + nb // 4 <= self.n4, f"arena overflow {self.ptr}+{nb} > {self.n4 * 4}"
        v = self.t[:, a:a + nb // 4]
        if dtype != F32:
            v = v.bitcast(dtype)
        v = v[0:shape[0], 0:n]
        if len(shape) > 2:
            names = " ".join(f"d{i}" for i in range(len(shape) - 1))
            kw = {f"d{i}": int(shape[i + 1]) for i in range(len(shape) - 1)}
            v = v.rearrange(f"p ({names}) -> p {names}", **kw)
        self.ptr += nb
        self.hi = max(self.hi, self.ptr)
        return v


def bc(ap, shape):
    return ap.to_broadcast(list(shape))


def build(nseq=NSEQ, dbg=None, stop_after=None):
    nc = bass.Bass("TRN2", target_bir_lowering=False)
    S = Sync(nc)
    V, ACT, POOL, PE = nc.vector, nc.scalar, nc.gpsimd, nc.tensor

    def din(name, shape, dt=F32):
        return nc.dram_tensor(name, list(shape), dt, kind="ExternalInput").ap()

    x_d = din("x", [nseq, T, D])
    cT_d = din("cT", [nseq, 128, 8])
    pos_d = din("pos", [nseq, 1, T], I32)
    wada_d = din("w_ada", [D, 6 * D])
    bada_d = din("b_ada", [1, 6 * D])
    n1g_d = din("norm1_g", [1, D])
    n2g_d = din("norm2_g", [1, D])
    win_d = din("w_in", [D, 4736])
    pp_d = din("pp", [128, NPP])
    w2c_d = din("w2cat", [128, 2 * 512])
    a2c_d = din("a2cat", [128, 2 * 512])
    g2_d = din("g2", [128, 512])
    gnw_d = din("gn_w", [1, 512])
    gnb_d = din("gn_b", [1, 512])
    prw_d = din("p_rwkv", [512, D])
    pat_d = din("p_attn", [512, D])
    wout_d = din("w_out", [D, D])
    wr_d = din("w_router", [D, E])
    wg_d = din("w_gate", [E, D, D])
    wu_d = din("w_up", [E, D, D])
    wd_d = din("w_down", [E, D, D])
    cm_d = din("cmats", [128, 13 * 128])
    out_d = nc.dram_tensor("out", [nseq, T, D], F32, kind="ExternalOutput").ap()
    mod_d = nc.dram_tensor("modscr", [nseq, 6, 128, D], F32, kind="Internal").ap()
    y_d = nc.dram_tensor("yscr", [2, T, 512], F32, kind="Internal").ap()
    u_d = nc.dram_tensor("uscr", [128, 8, T], BF16, kind="Internal").ap()
    dbg_d = None
    if dbg is not None:
        dbg_d = nc.dram_tensor("dbg", list(dbg[1]), F32, kind="ExternalOutput").ap()

    def sb(name, shape, dt=F32):
        return nc.alloc_sbuf_tensor('sb_' + name, list(shape), dt).ap()

    pp = sb("pp", [128, NPP])
    ident = sb("ident", [128, 128], BF16)
    identf = sb("identf", [128, 128])
    cmb = sb("cmb", [128, 13, 128], BF16)
    w2c = sb("w2c", [128, 2, 512], BF16)
    a2c = sb("a2c", [128, 2, 512], BF16)
    g2 = sb("g2", [128, 512], BF16)
    wr = sb("wr", [128, 8, E], BF16)
    epsc = sb("epsc", [128, 4])
    alpha = sb("alpha", [128, 15])
    oneminus_ka = sb("omka", [128, 4])
    two_omka = sb("omka2", [128, 4])
    negkkc = sb("negone", [128, 1])
    esk = sb("esk", [128, 4])
    rmask = sb("rmask", [128, TBS])
    iota_row = sb("iota_row", [128, CAP])
    ident4 = sb("ident4", [128, 4, 128], BF16)
    kar = sb("kar", [128, 4])
    c2r = sb("c2r", [128, 4])
    afftm = sb("afftm", [128, NT, E])
    slot_tm = sb("slot_tm", [128, NT, E])
    affhl = sb("affhl", [128, NT, E, 2], BF16)

    BLK1, ROT, MPREV, MNEXT = 0, 1, 2, 3
    MZT = (4, 5)
    MZ = (6, 7)
    HSEL = 8
    VP = (9, 10)

    ps = [nc.alloc_psum_tensor(f"ps{i}", [128, 512], F32).ap() for i in range(8)]
    psk = [f"ps{i}" for i in range(8)]

    AR = Arena(nc, "arena", 192 * 1024)

    def col(name, j=0, n=1):
        o, w = PP[name]
        return pp[:, o + j:o + j + n]

    S.dma('sp', pp, pp_d, writes=['pp'])
    S.dma('pool', cmb.rearrange("p a b -> p (a b)"), cm_d, writes=['cmb'])
    S.dma('pool', w2c.rearrange("p a b -> p (a b)"), w2c_d, writes=['w2c'])
    S.dma('pool', a2c.rearrange("p a b -> p (a b)"), a2c_d, writes=['a2c'])
    S.dma('pool', g2, g2_d, writes=['g2'])
    S.dma('pool', wr, wr_d.rearrange("(k p) e -> p k e", p=128), writes=['wr'])
    S.op('pool', lambda: POOL.memset(identf, 1.0), writes=['identf'])
    S.op('pool', lambda: POOL.affine_select(out=identf, in_=identf, pattern=[[1, 128]], compare_op=ALU.is_equal,
                                            fill=0.0, base=0, channel_multiplier=-1), reads=['identf'], writes=['identf'])
    S.op('dve', lambda: V.tensor_copy(out=ident, in_=identf), reads=['identf'], writes=['ident'])
    for j in range(4):
        S.op('dve', lambda: V.tensor_copy(out=ident4[:, j, :], in_=identf), reads=['identf'], writes=['ident4'])
    S.op('pool', lambda: POOL.memset(epsc[:, 0:1], 1e-6), writes=['epsc'])
    S.op('pool', lambda: POOL.memset(epsc[:, 1:2], 64e-5), reads=['epsc'], writes=['epsc'])
    S.op('pool', lambda: POOL.memset(epsc[:, 2:3], 1e-24), reads=['epsc'], writes=['epsc'])
    S.op('pool', lambda: POOL.memset(epsc[:, 3:4], 0.0), reads=['epsc'], writes=['epsc'])
    S.op('pool', lambda: POOL.memset(negkkc, -1.0), writes=['negone'])
    S.op('dve', lambda: V.tensor_tensor(out=alpha, in0=col("mp", 0, 15), in1=col("mn", 0, 15), op=ALU.add), reads=['pp'], writes=['alpha'])
    S.op('dve', lambda: V.tensor_scalar(out=alpha, in0=alpha, scalar1=-1.0, scalar2=1.0, op0=ALU.mult, op1=ALU.add), reads=['alpha'], writes=['alpha'])
    S.op('dve', lambda: V.tensor_scalar(out=oneminus_ka, in0=col("ka", 0, 4), scalar1=-1.0, scalar2=1.0, op0=ALU.mult, op1=ALU.add), reads=['pp'], writes=['omka'])
    S.op('dve', lambda: V.tensor_scalar(out=two_omka, in0=col("ka", 0, 4), scalar1=-2.0, scalar2=2.0, op0=ALU.mult, op1=ALU.add), reads=['pp'], writes=['omka2'])
    S.op('act', lambda: ACT.activation(out=esk, in_=col("sink", 0, 4), func=AF.Exp), reads=['pp'], writes=['esk'])
    S.op('dve', lambda: V.tensor_tensor(out=kar, in0=col("ka", 0, 4), in1=col("rk", 0, 4), op=ALU.mult), reads=['pp'], writes=['kar'])
    S.op('dve', lambda: V.tensor_tensor(out=c2r, in0=two_omka, in1=col("rk", 0, 4), op=ALU.mult), reads=['pp', 'omka2'], writes=['kar'])
    S.op('pool', lambda: POOL.memset(rmask, 1.0), writes=['rmask'])
    S.op('pool', lambda: POOL.memset(rmask.rearrange("p (c t) -> p c t", t=64)[:, :, 0:1], 0.0), reads=['rmask'], writes=['rmask'])
    S.op('pool', lambda: POOL.iota(iota_row, pattern=[[1, CAP]], base=0, channel_multiplier=0, allow_small_or_imprecise_dtypes=True), writes=['iota_row'])

    def debug_out(ap_sb, key, rows=None):
        S.dma('sp', dbg_d if rows is None else rows, ap_sb, reads=[key])

    def phase_adaln():
        AR.seek(0)
        csil = [AR.alloc([128, 8], F32) for _ in range(nseq)]
        crep = [AR.alloc([128, 9, 128], F32) for _ in range(nseq)]
        wblk = [AR.alloc([128, 9, 512], F32) for _ in range(3)]
        g1B = AR.alloc([128, D], F32)
        g2B = AR.alloc([128, D], F32)
        mt = [AR.alloc([128, 512], F32) for _ in range(4)]
        S.dma('sp', g1B, n1g_d.partition_broadcast(128), writes=['g1B'])
        S.dma('sp', g2B, n2g_d.partition_broadcast(128), writes=['g2B'])
        for b in range(3):
            S.op('pool', lambda: POOL.memset(wblk[b][:, 8, :], 0.0), writes=[('wblk', b)])
        for s in range(nseq):
            S.dma('sp', csil[s], cT_d[s], writes=[('csil', s)])
            S.op('act', lambda: ACT.activation(out=csil[s], in_=csil[s], func=AF.Silu), reads=[('csil', s)], writes=[('csil', s)])
            S.op('pool', lambda: POOL.memset(crep[s][:, 8, :], 0.0), writes=[('crep', s)])
            S.op('pool', lambda: POOL.memset(crep[s][0:1, 8, :], 1.0), reads=[('crep', s)], writes=[('crep', s)])
            S.op('dve', lambda: V.tensor_copy(out=crep[s][:, 0:8, :], in_=bc(csil[s].rearrange("p (k o) -> p k o", o=1), [128, 8, 128])),
                 reads=[('csil', s)], writes=[('crep', s)])
        ev = 0
        for jb in range(12):
            b = jb % 3
            piece = jb // 2
            c0 = jb * 512
            S.dma('sp', wblk[b][:, 0:4, :], wada_d[0:512, c0:c0 + 512].rearrange("(k p) n -> p k n", p=128), writes=[('wblk', b)])
            S.dma('act', wblk[b][:, 4:8, :], wada_d[512:1024, c0:c0 + 512].rearrange("(k p) n -> p k n", p=128), writes=[('wblk', b)])
            S.dma('sp', wblk[b][0:1, 8, :], bada_d[:, c0:c0 + 512], writes=[('wblk', b)])
            for s in range(nseq):
                pz, pkz = ps[ev % 4], psk[ev % 4]
                for k in range(9):
                    S.op('pe', lambda: PE.matmul(pz, lhsT=crep[s][:, k, :], rhs=wblk[b][:, k, :], start=(k == 0), stop=(k == 8)),
                         reads=[('crep', s), ('wblk', b)], writes=[pkz], pe_acc=True)
                m = mt[ev % 4]
                lc = (jb % 2) * 512
                if piece == 1:
                    S.op('dve', lambda: V.scalar_tensor_tensor(out=m, in0=pz, scalar=1.0, in1=g1B[:, lc:lc + 512], op0=ALU.add, op1=ALU.mult),
                         reads=[pkz, 'g1B'], writes=[('mt', ev % 4)])
                elif piece == 4:
                    S.op('dve', lambda: V.scalar_tensor_tensor(out=m, in0=pz, scalar=1.0, in1=g2B[:, lc:lc + 512], op0=ALU.add, op1=ALU.mult),
                         reads=[pkz, 'g2B'], writes=[('mt', ev % 4)])
                else:
                    S.op('act', lambda: ACT.copy(out=m, in_=pz), reads=[pkz], writes=[('mt', ev % 4)])
                S.dma('sp', mod_d[s, piece, :, lc:lc + 512], m, reads=[('mt', ev % 4)], writes=[('mod', s, piece)])
                ev += 1
        S.barrier()

    def phase_norm1(s, uT, base):
        AR.seek(base)
        scp = AR.alloc([128, D], F32)
        shp = AR.alloc([128, D], F32)
        xt = [AR.alloc([128, D], F32) for _ in range(2)]
        tmp2 = [AR.alloc([128, D], F32) for _ in range(2)]
        ub = [AR.alloc([128, D], BF16) for _ in range(2)]
        junk2 = [AR.alloc([128, D], BF16) for _ in range(2)]
        ss2 = [AR.alloc([128, 2], F32) for _ in range(2)]
        S.dma('sp', scp, mod_d[s, 1], reads=[('mod', s, 1)], writes=['scp'])
        S.dma('sp', shp, mod_d[s, 0], reads=[('mod', s, 0)], writes=['shp'])
        for i in range(NT):
            b = i % 2
            S.dma('sp', xt[b], x_d[s, i * 128:(i + 1) * 128, :], writes=[('xt', b)])
            tmp, junk, ss = tmp2[b], junk2[b], ss2[b]
            S.op('act', lambda: ACT.activation(out=junk, in_=xt[b], func=AF.Square, accum_out=ss[:, 0:1]), reads=[('xt', b)], writes=[('junk', b), ('ss', b)])
            S.op('act', lambda: ACT.activation(out=ss[:, 1:2], in_=ss[:, 0:1], func=AF.Sqrt, bias=epsc[:, 0:1], scale=1.0 / D), reads=[('ss', b), 'epsc'], writes=[('ss1', b)])
            S.op('dve', lambda: V.reciprocal(out=ss[:, 1:2], in_=ss[:, 1:2]), reads=[('ss1', b)], writes=[('ss1', b)])
            S.op('dve', lambda: V.scalar_tensor_tensor(out=tmp, in0=xt[b], scalar=ss[:, 1:2], in1=scp, op0=ALU.mult, op1=ALU.mult),
                 reads=[('xt', b), ('ss1', b), 'scp'], writes=[('tmp', b)])
            S.op('pool', lambda: POOL.tensor_tensor(out=ub[b], in0=tmp, in1=shp, op=ALU.add), reads=[('tmp', b), 'shp'], writes=[('ub', b)])
            pz = ps[i % 2].bitcast(BF16).rearrange("p (k t) -> p k t", k=8)
            for k in range(8):
                S.op('pe', lambda: PE.transpose(out=pz[:, k, :], in_=ub[b][:, k * 128:(k + 1) * 128], identity=ident),
                     reads=[('ub', b), 'ident'], writes=[psk[i % 2]], pe_acc=True)
            S.op('act', lambda: ACT.copy(out=uT[:, :, i * 128:(i + 1) * 128], in_=pz), reads=[psk[i % 2]], writes=[('uT', i // 4)])

    def phase_rwkv_cols(uT, zsT, base):
        AR.seek(base)
        wg = [AR.alloc([128, 8, 128], BF16) for _ in range(2)]
        ztmpP = [AR.alloc([128, T + 2], F32) for _ in range(2)]
        shtP = [AR.alloc([128, T], F32) for _ in range(2)]
        for q in range(2):
            S.op('pool', lambda: POOL.memset(ztmpP[q][:, 0:1], 0.0), writes=[('ztmp', q)])
            S.op('pool', lambda: POOL.memset(ztmpP[q][:, T + 1:T + 2], 0.0), reads=[('ztmp', q)], writes=[('ztmp', q)])
        for j in range(15):
            b = j % 2
            ztmp, sht = ztmpP[b], shtP[b]
            S.dma('pool', wg[b], win_d[:, j * 128:(j + 1) * 128].rearrange("(k p) n -> p k n", p=128), writes=[('wg', b)])
            for tb in range(NB):
                pz = ps[(j * NB + tb) % 4]
                pk = psk[(j * NB + tb) % 4]
                for k in range(8):
                    S.op('pe', lambda: PE.matmul(pz, lhsT=wg[b][:, k, :], rhs=uT[:, k, tb * 512:(tb + 1) * 512], start=(k == 0), stop=(k == 7)),
                         reads=[('wg', b), ('uT', tb)], writes=[pk], pe_acc=True)
                S.op('act', lambda: ACT.copy(out=ztmp[:, 1 + tb * 512:1 + (tb + 1) * 512], in_=pz), reads=[pk], writes=[('ztmp', b)])
            S.op('dve', lambda: V.tensor_scalar(out=sht, in0=ztmp[:, 1:T + 1], scalar1=alpha[:, j:j + 1], scalar2=None, op0=ALU.mult),
                 reads=[('ztmp', b), 'alpha'], writes=[('sht', b)])
            S.op('dve', lambda: V.scalar_tensor_tensor(out=sht, in0=ztmp[:, 0:T], scalar=col("mp", j), in1=sht, op0=ALU.mult, op1=ALU.add),
                 reads=[('ztmp', b), ('sht', b), 'pp'], writes=[('sht', b)])
            S.op('dve', lambda: V.scalar_tensor_tensor(out=zsT[:, j, :], in0=ztmp[:, 2:T + 2], scalar=col("mn", j), in1=sht, op0=ALU.mult, op1=ALU.add),
                 reads=[('ztmp', b), ('sht', b), 'pp'], writes=[('zs', j)])
            if j == 12:
                S.op('act', lambda: ACT.activation(out=zsT[:, j, :], in_=zsT[:, j, :], func=AF.Tanh), reads=[('zs', j)], writes=[('zs', j)])
            if j == 14:
                S.op('act', lambda: ACT.activation(out=zsT[:, j, :], in_=zsT[:, j, :], func=AF.Sigmoid), reads=[('zs', j)], writes=[('zs', j)])

    def phase_scan(zsT, kkT, base):
        rT = lambda c: zsT[:, c, :]
        kT = lambda c: zsT[:, 4 + c, :]
        vT = lambda c: zsT[:, 8 + c, :]
        wdT = zsT[:, 12, :]
        adT = zsT[:, 13, :]
        AR.seek(base)
        kraw = AR.alloc([128, 512], F32)
        ksq = AR.alloc([128, 512], BF16)
        krs = AR.alloc([128, 512], F32)
        for c in range(4):
            for tb in range(NB):
                sl = slice(tb * 512, (tb + 1) * 512)
                S.op('dve', lambda: V.tensor_scalar(out=kraw, in0=kT(c)[:, sl], scalar1=col("kk", c), scalar2=None, op0=ALU.mult), reads=[('zs', 4 + c), 'pp'], writes=['kraw'])
                S.op('act', lambda: ACT.activation(out=ksq, in_=kraw, func=AF.Square), reads=['kraw'], writes=['ksq'])
                pz, pk = ps[tb % 2], psk[tb % 2]
                S.op('pe', lambda: PE.matmul(pz, lhsT=cmb[:, BLK1, :], rhs=ksq, start=True, stop=True), reads=['ksq', 'cmb'], writes=[pk], pe_acc=True)
                S.op('act', lambda: ACT.activation(out=krs, in_=pz, func=AF.Sqrt, bias=epsc[:, 2:3], scale=1.0), reads=[pk, 'epsc'], writes=['krs'])
                S.op('dve', lambda: V.reciprocal(out=krs, in_=krs), reads=['krs'], writes=['krs'])
                S.op('dve', lambda: V.tensor_tensor(out=kkT[:, c, sl], in0=kraw, in1=krs, op=ALU.mult), reads=['kraw', 'krs'], writes=[('kk', c)])
        S.barrier()
        AR.seek(base)
        sg = AR.alloc([128, 4, TBS], F32)
        ad = AR.alloc([128, 4, TBS], F32)
        cc = AR.alloc([128, 4, TBS], F32)
        t1 = AR.alloc([128, 4, TBS], F32)
        ex = [[AR.alloc([128, TBS], F32) for _ in range(2)] for _ in range(4)]
        kd = AR.alloc([128, 4, TBS], F32)
        bb = AR.alloc([128, 4, TBS], F32)
        pdec = AR.alloc([128, 4, NCH], F32)
        ARz = AR.alloc([128, 4, NCH, 2, 2, 64], BF16)
        Bz = AR.alloc([128, 4, NCH, 2, 64], BF16)
        BKt = AR.alloc([128, 4, NCH, 2, 64], BF16)
        KBh = AR.alloc([128, 4, NCH, 2, 64], BF16)
        KBt = AR.alloc([128, 4, NCH, 128], BF16)
        VZ = AR.alloc([128, NCH, 8, 64], BF16)
        XV = AR.alloc([128, NCH, 8, 64], BF16)
        ZTs = [[AR.alloc([128, 4, 128], BF16) for _ in range(2)] for _ in range(NCH)]
        ATm = [[AR.alloc([128, 4, 128], BF16) for _ in range(2)] for _ in range(NCH)]
        PTm = [[AR.alloc([128, 4, 128], BF16) for _ in range(2)] for _ in range(NCH)]
        Pm = [[AR.alloc([128, 4, 128], BF16) for _ in range(2)] for _ in range(NCH)]
        Am = [[AR.alloc([128, 4, 128], BF16) for _ in range(2)] for _ in range(NCH)]
        W1s = AR.alloc([128, 4, 64], BF16)
        S32 = [AR.alloc([128, 4, 64], F32) for _ in range(2)]
        Sb = [AR.alloc([128, 4, 64], BF16) for _ in range(2)]
        ysb = [AR.alloc([64, 512], F32) for _ in range(2)]
        S.op('pool', lambda: POOL.memset(ARz.rearrange("p a b c d e -> p (a b c d e)"), 0.0), writes=['ARz'])
        S.op('pool', lambda: POOL.memset(Bz.rearrange("p a b c d -> p (a b c d)"), 0.0), writes=['Bz'])
        S.op('pool', lambda: POOL.memset(VZ.rearrange("p a b c -> p (a b c)"), 0.0), writes=['VZ'])

        def chain(gens):
            for g_ in gens:
                yield from g_

        def run_tasks(tasks):
            tasks = list(tasks)
            while tasks:
                for t_ in list(tasks):
                    try:
                        next(t_)
                    except StopIteration:
                        tasks.remove(t_)

        yev = 0
        pendQ = None
        for d in range(2):
            S.op('pool', lambda: POOL.memset(S32[d].rearrange("p a b -> p (a b)"), 0.0), writes=[('S32', d)])
            S.op('pool', lambda: POOL.memset(Sb[d].rearrange("p a b -> p (a b)"), 0.0), writes=[('Sb', d)])
            tbs = range(NTB) if d == 0 else range(NTB - 1, -1, -1)
            for tb in tbs:
                sl = slice(tb * TBS, (tb + 1) * TBS)
                def gen_prep(c):
                    pz, pk = ps[c % 2], psk[c % 2]
                    S.op('pe', lambda: PE.matmul(pz[:, 0:TBS], lhsT=w2c[:, d, c * 128:(c + 1) * 128], rhs=wdT[:, sl], start=True, stop=True),
                         reads=['w2c', ('zs', 12)], writes=[pk], pe_acc=True)
                    S.op('act', lambda: ACT.activation(out=sg[:, c, :], in_=pz[:, 0:TBS], func=AF.Sigmoid, bias=col("w0", d * 4 + c), scale=1.0),
                         reads=[pk, 'pp'], writes=[('sg', c)])
                    pz2, pk2 = ps[2 + c % 2], psk[2 + c % 2]
                    S.op('pe', lambda: PE.matmul(pz2[:, 0:TBS], lhsT=a2c[:, d, c * 128:(c + 1) * 128], rhs=adT[:, sl], start=True, stop=True),
                         reads=['a2c', ('zs', 13)], writes=[pk2], pe_acc=True)
                    S.op('act', lambda: ACT.activation(out=ad[:, c, :], in_=pz2[:, 0:TBS], func=AF.Sigmoid, bias=col("a0", d * 4 + c), scale=1.0),
                         reads=[pk2, 'pp'], writes=[('ad', c)])
                    yield
                    S.op('dve', lambda: V.tensor_tensor_scan(out=cc[:, c, :], data0=rmask, data1=sg[:, c, :], initial=0.0, op0=ALU.mult, op1=ALU.add),
                         reads=['rmask', ('sg', c)], writes=[('cc', c)])
                    cc3 = cc[:, c, :].rearrange("p (h t) -> p h t", t=64)
                    sg3 = sg[:, c, :].rearrange("p (h t) -> p h t", t=64)
                    t13 = t1[:, c, :].rearrange("p (h t) -> p h t", t=64)
                    if d == 1:
                        S.op('dve', lambda: V.tensor_tensor(out=t13, in0=bc(cc3[:, :, 63:64], [128, NCH, 64]), in1=cc3, op=ALU.subtract),
                             reads=[('cc', c)], writes=[('t1', c)])
                        S.op('dve', lambda: V.tensor_tensor(out=cc[:, c, :], in0=t1[:, c, :], in1=sg[:, c, :], op=ALU.add),
                             reads=[('t1', c), ('sg', c)], writes=[('cc', c)])
                    totp = 63 if d == 0 else 0
                    S.op('pool', lambda: POOL.tensor_scalar(out=kd[:, c, :], in0=ad[:, c, :], scalar1=col("ka", c), scalar2=oneminus_ka[:, c:c + 1], op0=ALU.mult, op1=ALU.add),
                         reads=[('ad', c), 'pp', 'omka'], writes=[('kd', c)])
                    S.op('pool', lambda: POOL.tensor_tensor(out=kd[:, c, :], in0=kd[:, c, :], in1=kT(c)[:, sl], op=ALU.mult),
                         reads=[('kd', c), ('zs', 4 + c)], writes=[('kd', c)])
                    S.op('pool', lambda: POOL.tensor_tensor(out=bb[:, c, :], in0=ad[:, c, :], in1=kkT[:, c, sl], op=ALU.mult),
                         reads=[('ad', c), ('kk', c)], writes=[('bb', c)])
                    yield
                    e = ex[c][0]
                    S.op('act', lambda: ACT.activation(out=e, in_=cc[:, c, :], func=AF.Exp, scale=-LAM), reads=[('cc', c)], writes=[('ex', c, 0)])
                    for hp in range(2):
                        pr = slice(hp * 64, (hp + 1) * 64)
                        S.op('dve', lambda: V.tensor_tensor(out=ARz[pr, c, :, 0, hp, :], in0=rT(c)[pr, sl].rearrange("p (h t) -> p h t", t=64),
                                                            in1=e[pr, :].rearrange("p (h t) -> p h t", t=64), op=ALU.mult),
                             reads=[('zs', c), ('ex', c, 0)], writes=['ARz'])
                    yield
                    e = ex[c][1]
                    S.op('act', lambda: ACT.activation(out=e, in_=cc[:, c, :], func=AF.Exp, scale=LAM), reads=[('cc', c)], writes=[('ex', c, 1)])
                    S.op('dve', lambda: V.tensor_tensor(out=BKt[:, c, :, 0, :], in0=kd[:, c, :].rearrange("p (h t) -> p h t", t=64),
                                                        in1=e.rearrange("p (h t) -> p h t", t=64), op=ALU.mult),
                         reads=[('kd', c), ('ex', c, 1)], writes=['BKt'])
                    S.op('dve', lambda: V.tensor_tensor(out=BKt[:, c, :, 1, :], in0=bb[:, c, :].rearrange("p (h t) -> p h t", t=64),
                                                        in1=e.rearrange("p (h t) -> p h t", t=64), op=ALU.mult),
                         reads=[('bb', c), ('ex', c, 1)], writes=['BKt'])
                    for hp in range(2):
                        pr = slice(hp * 64, (hp + 1) * 64)
                        S.op('act', lambda: ACT.copy(out=Bz[pr, c, :, hp, :], in_=BKt[pr, c, :, 1, :]), reads=['BKt'], writes=['Bz'])
                    yield
                    S.op('dve', lambda: V.tensor_tensor(out=t1[:, c, :], in0=cc[:, c, :], in1=sg[:, c, :], op=ALU.subtract),
                         reads=[('cc', c), ('sg', c)], writes=[('t1', c)])
                    e = ex[c][0]
                    S.op('act', lambda: ACT.activation(out=e, in_=t1[:, c, :], func=AF.Exp, scale=-LAM), reads=[('t1', c)], writes=[('ex', c, 0)])
                    for hp in range(2):
                        pr = slice(hp * 64, (hp + 1) * 64)
                        S.op('dve', lambda: V.scalar_tensor_tensor(out=ARz[pr, c, :, 1, hp, :], in0=kkT[pr, c, sl].rearrange("p (h t) -> p h t", t=64),
                                                                   scalar=-1.0, in1=e[pr, :].rearrange("p (h t) -> p h t", t=64), op0=ALU.mult, op1=ALU.mult),
                             reads=[('kk', c), ('ex', c, 0)], writes=['ARz'])
                    yield
                    S.op('dve', lambda: V.tensor_tensor(out=t13, in0=bc(cc3[:, :, totp:totp + 1], [128, NCH, 64]), in1=cc3, op=ALU.subtract),
                         reads=[('cc', c)], writes=[('t1', c)])
                    e = ex[c][1]
                    S.op('act', lambda: ACT.activation(out=e, in_=t1[:, c, :], func=AF.Exp, scale=-LAM), reads=[('t1', c)], writes=[('ex', c, 1)])
                    S.op('pool', lambda: POOL.tensor_tensor(out=KBh[:, c, :, 0, :], in0=kd[:, c, :].rearrange("p (h t) -> p h t", t=64),
                                                        in1=e.rearrange("p (h t) -> p h t", t=64), op=ALU.mult),
                         reads=[('kd', c), ('ex', c, 1)], writes=['KBh'])
                    S.op('pool', lambda: POOL.tensor_tensor(out=KBh[:, c, :, 1, :], in0=bb[:, c, :].rearrange("p (h t) -> p h t", t=64),
                                                        in1=e.rearrange("p (h t) -> p h t", t=64), op=ALU.mult),
                         reads=[('bb', c), ('ex', c, 1)], writes=['KBh'])
                    S.op('act', lambda: ACT.activation(out=pdec[:, c, :].rearrange("p (h o) -> p h o", o=1), in_=cc3[:, :, totp:totp + 1], func=AF.Exp, scale=-LAM), reads=[('cc', c)], writes=['pdec'])
                ptasks = [gen_prep(c_) for c_ in range(4)]
                for t_ in ptasks:
                    next(t_)
                if pendQ is not None:
                    for _ in range(3):
                        next(pendQ, None)
                for t_ in ptasks:
                    next(t_)
                if pendQ is not None:
                    run_tasks([pendQ])
                    pendQ = None
                run_tasks(ptasks)
                for ch in range(NCH):
                    pz = ps[4 + ch % 2].bitcast(BF16)
                    pk = psk[4 + ch % 2]
                    pzv = pz[0:64, 0:512].rearrange("p (c n) -> p c n", c=4)
                    for c in range(4):
                        S.op('pe', lambda: PE.transpose(out=pzv[:, c, :], in_=vT(c)[:, tb * TBS + ch * 64: tb * TBS + (ch + 1) * 64], identity=ident),
                             reads=[('zs', 8 + c), 'ident'], writes=[pk], pe_acc=True)
                    S.op('act', lambda: ACT.copy(out=VZ[0:64, ch, :, :].rearrange("p h v -> p (h v)"), in_=pz[0:64, 0:512]), reads=[pk], writes=[('VZ', ch)])
                    S.op('act', lambda: ACT.copy(out=XV[0:64, ch, :, :].rearrange("p h v -> p (h v)"), in_=pz[0:64, 0:512]), reads=[pk], writes=[('XVv', ch)])
                    pzk = pz[:, 512:1024].rearrange("p (c n) -> p c n", c=4)
                    for c in range(4):
                        S.op('pe', lambda: PE.transpose(out=pzk[:, c, :], in_=KBh[:, c, ch, :, :].rearrange("p a t -> p (a t)"), identity=ident),
                             reads=['KBh', 'ident'], writes=[pk], pe_acc=True)
                    S.op('dve', lambda: V.tensor_copy(out=KBt[:, :, ch, :], in_=pzk), reads=[pk], writes=[('KBt', ch)])
                MNT = cmb[:, 11 + d, :]
                MN = cmb[:, 12 - d, :]

                def gen_D(ch, slot, par):
                    pA, pkA = ps[2 * par], psk[2 * par]
                    pB, pkB = ps[2 * par + 1], psk[2 * par + 1]
                    pA3 = pA.rearrange("p (j n) -> p j n", j=4)
                    pB3 = pB.rearrange("p (j n) -> p j n", j=4)
                    mzt = cmb[:, MZT[d], :]
                    for half in range(2):
                        pz3 = pA3 if half == 0 else pB3
                        pkz = pkA if half == 0 else pkB
                        for j in range(4):
                            h = half * 4 + j
                            c, hp = h // 2, h % 2
                            bk = BKt[:, c, ch, :, :].rearrange("p a t -> p (a t)")
                            S.op('pe', lambda: PE.matmul(pz3[:, j, :].rearrange("p (a t) -> p a t", a=2), lhsT=bk, rhs=ARz[:, c, ch, :, hp, :], start=True, stop=True),
                                 reads=['BKt', 'ARz'], writes=[pkz], pe_acc=True)
                        S.op('dve', lambda: V.tensor_tensor(out=ZTs[slot][half], in0=pz3, in1=bc(mzt.rearrange("p (o n) -> p o n", o=1), [128, 4, 128]), op=ALU.mult),
                             reads=[pkz, 'cmb'], writes=[('ZTs', slot, half)])
                    yield
                    for c in range(4):
                        bz = Bz[:, c, ch, :, :].rearrange("p a t -> p (a t)")
                        az = ARz[:, c, ch, 1, :, :].rearrange("p a t -> p (a t)")
                        S.op('pe', lambda: PE.matmul(pA3[:, c, :], lhsT=bz, rhs=az, start=True, stop=True), reads=['Bz', 'ARz'], writes=[pkA], pe_acc=True)
                        S.op('pe', lambda: PE.matmul(pB3[:, c, :], lhsT=az, rhs=bz, start=True, stop=True), reads=['Bz', 'ARz'], writes=[pkB], pe_acc=True)
                    S.op('dve', lambda: V.tensor_tensor(out=PTm[par][0], in0=pA3, in1=bc(MNT.rearrange("p (o n) -> p o n", o=1), [128, 4, 128]), op=ALU.mult),
                         reads=[pkA, 'cmb'], writes=[('PT', par, 0)])
                    S.op('dve', lambda: V.tensor_tensor(out=Pm[par][0], in0=pB3, in1=bc(MN.rearrange("p (o n) -> p o n", o=1), [128, 4, 128]), op=ALU.mult),
                         reads=[pkB, 'cmb'], writes=[('P', par, 0)])
                    S.op('pool', lambda: POOL.tensor_tensor(out=ATm[slot][0], in0=PTm[par][0], in1=ident4, op=ALU.add), reads=[('PT', par, 0), 'ident4'], writes=[('AT', slot, 0)])
                    S.op('pool', lambda: POOL.tensor_tensor(out=Am[par][0], in0=Pm[par][0], in1=ident4, op=ALU.add), reads=[('P', par, 0), 'ident4'], writes=[('A', par, 0)])
                    yield
                    cur = 0
                    for lev in range(1, 6):
                        nxt = 1 - cur
                        for j in range(4):
                            S.op('pe', lambda: PE.matmul(pA3[:, j, :], lhsT=Pm[par][cur][:, j, :], rhs=PTm[par][cur][:, j, :], start=True, stop=True),
                                 reads=[('P', par, cur), ('PT', par, cur)], writes=[pkA], pe_acc=True)
                            if lev < 5:
                                S.op('pe', lambda: PE.matmul(pB3[:, j, :], lhsT=PTm[par][cur][:, j, :], rhs=Pm[par][cur][:, j, :], start=True, stop=True),
                                     reads=[('P', par, cur), ('PT', par, cur)], writes=[pkB], pe_acc=True)
                        S.op('act', lambda: ACT.copy(out=PTm[par][nxt], in_=pA3), reads=[pkA], writes=[('PT', par, nxt)])
                        if lev < 5:
                            S.op('dve', lambda: V.tensor_copy(out=Pm[par][nxt], in_=pB3), reads=[pkB], writes=[('P', par, nxt)])
                        yield
                        for j in range(4):
                            S.op('pe', lambda: PE.matmul(pA3[:, j, :], lhsT=Am[par][cur][:, j, :], rhs=PTm[par][nxt][:, j, :], start=True, stop=True),
                                 reads=[('A', par, cur), ('PT', par, nxt)], writes=[pkA], pe_acc=True)
                            if lev < 5:
                                S.op('pe', lambda: PE.matmul(pB3[:, j, :], lhsT=PTm[par][nxt][:, j, :], rhs=Am[par][cur][:, j, :], start=True, stop=True),
                                     reads=[('A', par, cur), ('PT', par, nxt)], writes=[pkB], pe_acc=True)
                        S.op('dve', lambda: V.tensor_tensor(out=ATm[slot][nxt], in0=pA3, in1=ATm[slot][cur], op=ALU.add), reads=[pkA, ('AT', slot, cur)], writes=[('AT', slot, nxt)])
                        if lev < 5:
                            S.op('act', lambda: ACT.copy(out=Am[par][nxt], in_=pB3), reads=[pkB], writes=[('A', par, nxt)])
                            S.op('pool', lambda: POOL.tensor_tensor(out=Am[par][nxt], in0=Am[par][nxt], in1=Am[par][cur], op=ALU.add),
                                 reads=[('A', par, nxt), ('A', par, cur)], writes=[('A', par, nxt)])
                        yield
                        cur = nxt
                    assert cur == 1

                def gen_Q(ch, slot, pb, tb=tb, d=d):
                    nonlocal yev
                    fin = 1
                    gch = tb * NCH + ch
                    pW, pkW = ps[pb], psk[pb]
                    pW3 = pW[:, 0:256].rearrange("p (c v) -> p c v", c=4)
                    for h in range(8):
                        c, hp = h // 2, h % 2
                        S.op('pe', lambda: PE.matmul(pW3[hp * 64:(hp + 1) * 64, c, :], lhsT=ZTs[slot][h // 4][:, h % 4, 64:128], rhs=VZ[:, ch, h, :], start=True, stop=False),
                             reads=[('ZTs', slot, h // 4), ('VZ', ch)], writes=[pkW], pe_acc=True)
                        S.op('pe', lambda: PE.matmul(pW3[hp * 64:(hp + 1) * 64, c, :], lhsT=ARz[:, c, ch, 1, hp, :], rhs=Sb[d][:, c, :], start=False, stop=True),
                             reads=['ARz', ('Sb', d)], writes=[pkW], pe_acc=True)
                    S.op('act', lambda: ACT.copy(out=W1s, in_=pW3), reads=[pkW], writes=['W1s'])
                    yield
                    pX, pkX = ps[pb + 1], psk[pb + 1]
                    pX3 = pX.rearrange("p (h v) -> p h v", h=8)
                    for h in range(8):
                        c, hp = h // 2, h % 2
                        S.op('pe', lambda: PE.matmul(pX3[64:128, h, :], lhsT=ATm[slot][fin][:, c, hp * 64:(hp + 1) * 64], rhs=W1s[:, c, :], start=True, stop=True),
                             reads=[('AT', slot, fin), 'W1s'], writes=[pkX], pe_acc=True)
                    S.op('dve', lambda: V.tensor_copy(out=XV[64:128, ch, :, :], in_=pX3[64:128]), reads=[pkX], writes=[('XVu', ch)])
                    yield
                    pS, pkS = ps[pb + 3], psk[pb + 3]
                    pS3 = pS[:, 0:256].rearrange("p (c v) -> p c v", c=4)
                    for h in range(8):
                        c, hp = h // 2, h % 2
                        S.op('pe', lambda: PE.matmul(pS3[hp * 64:(hp + 1) * 64, c, :], lhsT=KBt[:, c, ch, hp * 64:(hp + 1) * 64], rhs=XV[:, ch, h, :], start=True, stop=True),
                             reads=[('KBt', ch), ('XVv', ch), ('XVu', ch)], writes=[pkS], pe_acc=True)
                    pY, pkY = ps[pb + 2], psk[pb + 2]
                    pY3 = pY.rearrange("p (h v) -> p h v", h=8)
                    for h in range(8):
                        c, hp = h // 2, h % 2
                        S.op('pe', lambda: PE.matmul(pY3[0:64, h, :], lhsT=ZTs[slot][h // 4][:, h % 4, 0:64], rhs=XV[:, ch, h, :], start=True, stop=False),
                             reads=[('ZTs', slot, h // 4), ('XVv', ch), ('XVu', ch)], writes=[pkY], pe_acc=True)
                        S.op('pe', lambda: PE.matmul(pY3[0:64, h, :], lhsT=ARz[:, c, ch, 0, hp, :], rhs=Sb[d][:, c, :], start=False, stop=True),
                             reads=['ARz', ('Sb', d)], writes=[pkY], pe_acc=True)
                    for c in range(4):
                        S.op('dve', lambda: V.scalar_tensor_tensor(out=S32[d][:, c, :], in0=S32[d][:, c, :], scalar=pdec[:, c, ch:ch + 1], in1=pS3[:, c, :], op0=ALU.mult, op1=ALU.add),
                             reads=[('S32', d), 'pdec', pkS], writes=[('S32', d)])
                    S.op('act', lambda: ACT.copy(out=Sb[d], in_=S32[d]), reads=[('S32', d)], writes=[('Sb', d)])
                    yb_ = ysb[yev % 2]
                    S.op('act', lambda: ACT.copy(out=yb_, in_=pY[0:64, :]), reads=[pkY], writes=[('ysb', yev % 2)])
                    S.dma('sp', y_d[d, gch * 64:(gch + 1) * 64, :], yb_, reads=[('ysb', yev % 2)], writes=[('yscr', d, gch // 2)])
                    yev += 1
                    yield

                chs = list(range(NCH)) if d == 0 else list(range(NCH - 1, -1, -1))
                Ds = [gen_D(chs[i_], i_, i_) for i_ in range(NCH)]
                fast, slow = Ds[0:2], Ds[2:4]
                alive = True
                while alive:
                    alive = False
                    for rep in range(2):
                        for t_ in fast:
                            if next(t_, 'done') != 'done':
                                alive = True
                    for t_ in slow:
                        next(t_, 'done')
                run_tasks([chain([gen_Q(chs[0], 0, 0), gen_Q(chs[1], 1, 0)])] + slow)
                pendQ = chain([gen_Q(chs[2], 2, 4), gen_Q(chs[3], 3, 4)])
        if pendQ is not None:
            run_tasks([pendQ])
            pendQ = None
        S.barrier()


    def phase_post(zsT, yaT, base):
        rT4 = zsT[:, 0:4, :]
        kT4 = zsT[:, 4:8, :]
        adT = zsT[:, 13, :]
        gdT = zsT[:, 14, :]
        AR.seek(base)
        gnwB = AR.alloc([128, 512], F32)
        gnbB = AR.alloc([128, 512], F32)
        P2 = lambda shape, dt: [AR.alloc(shape, dt) for _ in range(2)]
        Yf, Yb = P2([128, 512], F32), P2([128, 512], F32)
        ta0, ta1 = P2([128, 4, 128], F32), P2([128, 4, 128], F32)
        kf2 = P2([128, 4, 128], F32)
        prod2 = P2([128, 4, 128], BF16)
        rows2 = P2([128, 8], F32)
        bon2 = P2([128, 512], F32)
        y2 = P2([128, 512], F32)
        sq2 = P2([128, 512], F32)
        st2 = P2([128, 4, 8], F32)
        yab2 = P2([128, 512], BF16)
        S.dma('sp', gnwB, gnw_d.partition_broadcast(128), writes=['gnwB'])
        S.dma('sp', gnbB, gnb_d.partition_broadcast(128), writes=['gnbB'])
        def gen_tile(i):
            b = i % 2
            sl = slice(i * 128, (i + 1) * 128)
            ta = (ta0[b], ta1[b])
            kf, prod, rows, bon, y, sq, st, yab = kf2[b], prod2[b], rows2[b], bon2[b], y2[b], sq2[b], st2[b], yab2[b]
            bA, bB, bC, bD = 4 * b, 4 * b + 1, 4 * b + 2, 4 * b + 3
            S.dma('sp', Yf[b], y_d[0, sl, :], writes=[('Yf', b)])
            S.dma('sp', Yb[b], y_d[1, sl, :], writes=[('Yb', b)])
            for d in range(2):
                bk_ = bA if d == 0 else bB
                pz3 = ps[bk_].rearrange("p (c n) -> p c n", c=4)
                for c in range(4):
                    S.op('pe', lambda: PE.matmul(pz3[:, c, :], lhsT=a2c[:, d, c * 128:(c + 1) * 128], rhs=adT[:, sl], start=True, stop=True),
                         reads=['a2c'], writes=[psk[bk_]], pe_acc=True)
                for c in range(4):
                    S.op('act', lambda: ACT.activation(out=ta[d][:, c, :], in_=pz3[:, c, :], func=AF.Sigmoid, bias=col("a0", d * 4 + c), scale=1.0),
                         reads=[psk[bk_], 'pp'], writes=[('ta', b, d)])
            yield
            S.op('pool', lambda: POOL.tensor_tensor(out=ta[0], in0=ta[0], in1=ta[1], op=ALU.add), reads=[('ta', b, 0), ('ta', b, 1)], writes=[('ta', b, 0)])
            for c in range(4):
                S.op('pool', lambda: POOL.tensor_scalar(out=kf[:, c, :], in0=ta[0][:, c, :], scalar1=kar[:, c:c + 1], scalar2=c2r[:, c:c + 1], op0=ALU.mult, op1=ALU.add),
                     reads=[('ta', b, 0), 'kar'], writes=[('kf', b)])
            S.op('pool', lambda: POOL.tensor_tensor(out=kf, in0=kf, in1=kT4[:, :, sl], op=ALU.mult), reads=[('kf', b)], writes=[('kf', b)])
            S.op('pool', lambda: POOL.tensor_tensor(out=prod, in0=kf, in1=rT4[:, :, sl], op=ALU.mult), reads=[('kf', b)], writes=[('prod', b)])
            yield
            pr = ps[bB]
            for c in range(4):
                S.op('pe', lambda: PE.matmul(pr[:, c * 2:(c + 1) * 2], lhsT=prod[:, c, :], rhs=cmb[:, HSEL, 0:2], start=True, stop=True),
                     reads=[('prod', b), 'cmb'], writes=[psk[bB]], pe_acc=True)
            S.op('act', lambda: ACT.copy(out=rows, in_=pr[:, 0:8]), reads=[psk[bB]], writes=[('rows', b)])
            yield
            pv = ps[bD].bitcast(BF16)[:, 0:512]
            for c in range(4):
                S.op('pe', lambda: PE.transpose(out=pv[:, c * 128:(c + 1) * 128], in_=zsT[:, 8 + c, sl], identity=ident),
                     reads=['ident'], writes=[psk[bD]], pe_acc=True)
            S.op('dve', lambda: V.tensor_tensor(out=bon.rearrange("p (h v) -> p h v", h=8), in0=pv.rearrange("p (h v) -> p h v", h=8),
                                                in1=bc(rows.rearrange("p (h o) -> p h o", o=1), [128, 8, 64]), op=ALU.mult),
                 reads=[psk[bD], ('rows', b)], writes=[('bon', b)])
            yield
            pg = ps[bC]
            S.op('pe', lambda: PE.matmul(pg, lhsT=gdT[:, sl], rhs=g2, start=True, stop=True), reads=['g2'], writes=[psk[bC]], pe_acc=True)
            y3 = y.rearrange("p (h v) -> p h v", h=8)
            sq3 = sq.rearrange("p (h v) -> p h v", h=8)
            S.op('dve', lambda: V.tensor_tensor(out=y, in0=Yf[b], in1=Yb[b], op=ALU.add), reads=[('Yf', b), ('Yb', b)], writes=[('y', b)])
            yield
            S.op('dve', lambda: V.tensor_reduce(out=st[:, 0, :], in_=y3, axis=AX.X, op=ALU.add), reads=[('y', b)], writes=[('st0', b)])
            S.op('dve', lambda: V.tensor_scalar(out=st[:, 1, :], in0=st[:, 0, :], scalar1=-1.0 / 64, scalar2=None, op0=ALU.mult), reads=[('st0', b)], writes=[('st1', b)])
            S.op('dve', lambda: V.tensor_tensor(out=y3, in0=y3, in1=bc(st[:, 1, :].rearrange("p (h o) -> p h o", o=1), [128, 8, 64]), op=ALU.add),
                 reads=[('y', b), ('st1', b)], writes=[('y', b)])
            yield
            S.op('act', lambda: ACT.activation(out=sq, in_=y, func=AF.Square), reads=[('y', b)], writes=[('sq', b)])
            S.op('dve', lambda: V.tensor_reduce(out=st[:, 2, :], in_=sq3, axis=AX.X, op=ALU.add), reads=[('sq', b)], writes=[('st2', b)])
            yield
            S.op('act', lambda: ACT.activation(out=st[:, 3, :], in_=st[:, 2, :], func=AF.Sqrt, bias=epsc[:, 1:2], scale=1.0 / 64), reads=[('st2', b), 'epsc'], writes=[('st3', b)])
            S.op('dve', lambda: V.reciprocal(out=st[:, 3, :], in_=st[:, 3, :]), reads=[('st3', b)], writes=[('st3', b)])
            S.op('dve', lambda: V.tensor_tensor(out=y3, in0=y3, in1=bc(st[:, 3, :].rearrange("p (h o) -> p h o", o=1), [128, 8, 64]), op=ALU.mult),
                 reads=[('y', b), ('st3', b)], writes=[('y', b)])
            yield
            S.op('dve', lambda: V.tensor_tensor(out=y, in0=y, in1=gnwB, op=ALU.mult), reads=[('y', b), 'gnwB'], writes=[('y', b)])
            S.op('pool', lambda: POOL.tensor_tensor(out=bon, in0=bon, in1=gnbB, op=ALU.add), reads=[('bon', b), 'gnbB'], writes=[('bon', b)])
            S.op('dve', lambda: V.tensor_tensor(out=y, in0=y, in1=bon, op=ALU.add), reads=[('y', b), ('bon', b)], writes=[('y', b)])
            S.op('dve', lambda: V.tensor_tensor(out=yab, in0=y, in1=pg, op=ALU.mult), reads=[('y', b), psk[bC]], writes=[('yab', b)])
            yield
            pt = ps[bA].bitcast(BF16)[:, 0:512]
            for c in range(4):
                S.op('pe', lambda: PE.transpose(out=pt[:, c * 128:(c + 1) * 128], in_=yab[:, c * 128:(c + 1) * 128], identity=ident),
                     reads=[('yab', b), 'ident'], writes=[psk[bA]], pe_acc=True)
            S.op('act', lambda: ACT.copy(out=yaT[:, :, sl], in_=pt.rearrange("p (c n) -> p c n", c=4)), reads=[psk[bA]], writes=['yaT'])
            yield

        def run_tasks(tasks):
            tasks = list(tasks)
            while tasks:
                for t_ in list(tasks):
                    try:
                        next(t_)
                    except StopIteration:
                        tasks.remove(t_)

        for i in range(0, NT, 2):
            run_tasks([gen_tile(i), gen_tile(i + 1)])

    def phase_attn(s, uT, ybT, baseA, baseB):
        AR.seek(baseA)
        cosT = AR.alloc([128, T], F32)
        sinT = AR.alloc([128, T], F32)
        qT = AR.alloc([128, 4, T], BF16)
        kTt = AR.alloc([128, T], BF16)
        vp = AR.alloc([128, 2, NT, 128], BF16)
        AR.seek(baseB)
        wq = [AR.alloc([128, 8, 128], BF16) for _ in range(2)]
        qfL = [AR.alloc([128, 512], F32) for _ in range(2)]
        sqbL = [AR.alloc([128, 512], BF16) for _ in range(2)]
        rsL = [AR.alloc([128, 512], F32) for _ in range(2)]
        qnL = [AR.alloc([128, 512], F32) for _ in range(2)]
        qnbL = [AR.alloc([128, 512], BF16) for _ in range(2)]
        t1L = [AR.alloc([128, 512], F32) for _ in range(2)]
        t2L = [AR.alloc([128, 512], F32) for _ in range(2)]
        pTs = [AR.alloc([128, 512], BF16) for _ in range(6)]
        dn = AR.alloc([128, 512], F32)
        posi = AR.alloc([128, T], I32)
        ang = AR.alloc([128, T], F32)
        ki = AR.alloc([128, T], I32)
        kf = AR.alloc([128, T], F32)
        m1 = AR.alloc([128, T], F32)
        S.dma('sp', posi, pos_d[s].partition_broadcast(128), writes=['posi'])

        def table(dst, shift):
            S.op('dve', lambda: V.tensor_copy(out=ang, in_=posi), reads=['posi'], writes=['ang'])
            S.op('dve', lambda: V.tensor_scalar(out=ang, in0=ang, scalar1=col("invf"), scalar2=shift, op0=ALU.mult, op1=ALU.add), reads=['ang', 'pp'], writes=['ang'])
            S.op('dve', lambda: V.tensor_scalar(out=ki, in0=ang, scalar1=1.0 / TWO_PI, scalar2=None, op0=ALU.mult), reads=['ang'], writes=['ki'])
            S.op('pool', lambda: POOL.tensor_copy(out=kf, in_=ki), reads=['ki'], writes=['kf'])
            S.op('dve', lambda: V.scalar_tensor_tensor(out=ang, in0=kf, scalar=-C1, in1=ang, op0=ALU.mult, op1=ALU.add), reads=['kf', 'ang'], writes=['ang'])
            S.op('dve', lambda: V.scalar_tensor_tensor(out=ang, in0=kf, scalar=-C2, in1=ang, op0=ALU.mult, op1=ALU.add), reads=['kf', 'ang'], writes=['ang'])
            S.op('dve', lambda: V.tensor_scalar(out=m1, in0=ang, scalar1=float(np.pi), scalar2=-TWO_PI, op0=ALU.is_gt, op1=ALU.mult), reads=['ang'], writes=['m1'])
            S.op('pool', lambda: POOL.tensor_tensor(out=ang, in0=ang, in1=m1, op=ALU.add), reads=['ang', 'm1'], writes=['ang'])
            S.op('dve', lambda: V.tensor_scalar(out=m1, in0=ang, scalar1=float(-np.pi), scalar2=TWO_PI, op0=ALU.is_lt, op1=ALU.mult), reads=['ang'], writes=['m1'])
            S.op('pool', lambda: POOL.tensor_tensor(out=ang, in0=ang, in1=m1, op=ALU.add), reads=['ang', 'm1'], writes=['ang'])
            S.op('act', lambda: ACT.activation(out=dst, in_=ang, func=AF.Sin), reads=['ang'], writes=['tab'])

        table(sinT, 0.0)
        table(cosT, float(np.pi / 2))
        def c0_of(c):
            return 1920 + c * 128 if c < 4 else 2432

        def gen_qk(c, tb, L):
            b = c % 2
            gcol = col("qg") if c < 4 else col("kg")
            sl = slice(tb * 512, (tb + 1) * 512)
            qf_, sqb_, rs_, qn_, qnb_, t1_, t2_ = qfL[L], sqbL[L], rsL[L], qnL[L], qnbL[L], t1L[L], t2L[L]
            pz, pk = ps[L], psk[L]
            for k in range(8):
                S.op('pe', lambda: PE.matmul(pz, lhsT=wq[b][:, k, :], rhs=uT[:, k, sl], start=(k == 0), stop=(k == 7)),
                     reads=[('wq', b), ('uT', tb)], writes=[pk], pe_acc=True)
            S.op('act', lambda: ACT.copy(out=qf_, in_=pz), reads=[pk], writes=[('qf', L)])
            S.op('act', lambda: ACT.activation(out=sqb_, in_=qf_, func=AF.Square), reads=[('qf', L)], writes=[('sqb', L)])
            yield
            pr, pkr = ps[2 + L], psk[2 + L]
            S.op('pe', lambda: PE.matmul(pr, lhsT=cmb[:, BLK1, :], rhs=sqb_, start=True, stop=True), reads=[('sqb', L), 'cmb'], writes=[pkr], pe_acc=True)
            S.op('act', lambda: ACT.activation(out=rs_, in_=pr, func=AF.Sqrt, bias=epsc[:, 0:1], scale=1.0 / 64), reads=[pkr, 'epsc'], writes=[('rs', L)])
            yield
            S.op('dve', lambda: V.reciprocal(out=rs_, in_=rs_), reads=[('rs', L)], writes=[('rs', L)])
            S.op('dve', lambda: V.scalar_tensor_tensor(out=qn_, in0=qf_, scalar=gcol, in1=rs_, op0=ALU.mult, op1=ALU.mult), reads=[('qf', L), ('rs', L), 'pp'], writes=[('qn', L)])
            S.op('act', lambda: ACT.copy(out=qnb_, in_=qn_), reads=[('qn', L)], writes=[('qnb', L)])
            yield
            pro, pkro = ps[4 + L], psk[4 + L]
            S.op('pe', lambda: PE.matmul(pro, lhsT=cmb[:, ROT, :], rhs=qnb_, start=True, stop=True), reads=[('qnb', L), 'cmb'], writes=[pkro], pe_acc=True)
            S.op('pool', lambda: POOL.tensor_tensor(out=t1_, in0=qn_, in1=cosT[:, sl], op=ALU.mult), reads=[('qn', L), 'tab'], writes=[('t1', L)])
            yield
            S.op('dve', lambda: V.tensor_tensor(out=t2_, in0=pro, in1=sinT[:, sl], op=ALU.mult), reads=[pkro, 'tab'], writes=[('t2', L)])
            dst = qT[:, c, sl] if c < 4 else kTt[:, sl]
            S.op('dve', lambda: V.tensor_tensor(out=dst, in0=t1_, in1=t2_, op=ALU.add), reads=[('t1', L), ('t2', L)], writes=['qk'])
            yield

        def run_tasks(tasks):
            tasks = list(tasks)
            while tasks:
                for t_ in list(tasks):
                    try:
                        next(t_)
                    except StopIteration:
                        tasks.remove(t_)

        S.dma('pool', wq[0], win_d[:, c0_of(0):c0_of(0) + 128].rearrange("(k p) n -> p k n", p=128), writes=[('wq', 0)])
        for c in range(5):
            if c + 1 < 5:
                S.dma('pool', wq[(c + 1) % 2], win_d[:, c0_of(c + 1):c0_of(c + 1) + 128].rearrange("(k p) n -> p k n", p=128), writes=[('wq', (c + 1) % 2)])
            for tb in range(0, NB, 2):
                run_tasks([gen_qk(c, tb, 0), gen_qk(c, tb + 1, 1)])
        S.op('pool', lambda: POOL.memset(vp.rearrange("p a b c -> p (a b c)"), 0.0), writes=['vp'])
        S.dma('pool', wq[0], win_d[:, 2560:2688].rearrange("(k p) n -> p k n", p=128), writes=[('wq', 0)])
        for i in range(NT):
            pz, pk = ps[i % 2], psk[i % 2]
            for k in range(8):
                S.op('pe', lambda: PE.matmul(pz[:, 0:128], lhsT=uT[:, k, i * 128:(i + 1) * 128], rhs=wq[0][:, k, :], start=(k == 0), stop=(k == 7)),
                     reads=[('wq', 0), ('uT', i // 4)], writes=[pk], pe_acc=True)
            S.op('act', lambda: ACT.copy(out=vp[:, 0, i, 0:64], in_=pz[:, 0:64]), reads=[pk], writes=['vp'])
            S.op('dve', lambda: V.tensor_copy(out=vp[:, 1, i, 64:128], in_=pz[:, 64:128]), reads=[pk], writes=['vp'])
        for n in range(NT):
            qs = slice(n * 128, (n + 1) * 128)
            kbs = [kb for kb in (n - 1, n, n + 1) if 0 <= kb < NT]
            items = [(g, kb) for g in range(2) for kb in kbs]
            for idx, (g, kb) in enumerate(items):
                gp = slice(g * 64, (g + 1) * 64)
                pz, pk = ps[idx % 4], psk[idx % 4]
                S.op('pe', lambda: PE.matmul(pz.rearrange("p (j q) -> p j q", j=4), lhsT=kTt[gp, kb * 128:(kb + 1) * 128], rhs=qT[gp, :, qs], start=True, stop=True),
                     reads=['qk'], writes=[pk], pe_acc=True)
                pt_ = pTs[idx]
                S.op('act', lambda: ACT.activation(out=pt_, in_=pz, func=AF.Exp, scale=0.125), reads=[pk], writes=[('pT', idx)])
                if kb != n:
                    mk = cmb[:, MPREV if kb < n else MNEXT, :]
                    S.op('pool', lambda: POOL.tensor_tensor(out=pt_.rearrange("p (j q) -> p j q", j=4), in0=pt_.rearrange("p (j q) -> p j q", j=4),
                                                            in1=bc(mk.rearrange("p (o q) -> p o q", o=1), [128, 4, 128]), op=ALU.mult),
                         reads=[('pT', idx), 'cmb'], writes=[('pT', idx)])
            po, pko = ps[4 + n % 2], psk[4 + n % 2]
            pd_, pkd = ps[6 + n % 2], psk[6 + n % 2]
            for idx, (g, kb) in enumerate(items):
                S.op('pe', lambda: PE.matmul(po, lhsT=vp[:, g, kb, :], rhs=pTs[idx], start=(idx == 0), stop=(idx == len(items) - 1)),
                     reads=['vp', ('pT', idx)], writes=[pko], pe_acc=True)
            for idx, (g, kb) in enumerate(items):
                S.op('pe', lambda: PE.matmul(pd_, lhsT=cmb[:, VP[g], :], rhs=pTs[idx], start=(idx == 0), stop=(idx == len(items) - 1)),
                     reads=['cmb', ('pT', idx)], writes=[pkd], pe_acc=True)
            S.op('dve', lambda: V.tensor_tensor(out=dn.rearrange("p (j q) -> p j q", j=4), in0=pd_.rearrange("p (j q) -> p j q", j=4),
                                                in1=bc(esk.rearrange("p (j o) -> p j o", o=1), [128, 4, 128]), op=ALU.add), reads=[pkd, 'esk'], writes=['dn'])
            S.op('dve', lambda: V.reciprocal(out=dn, in_=dn), reads=['dn'], writes=['dn'])
            S.op('dve', lambda: V.tensor_tensor(out=ybT[:, :, qs], in0=po.rearrange("p (j q) -> p j q", j=4), in1=dn.rearrange("p (j q) -> p j q", j=4), op=ALU.mult),
                 reads=[pko, 'dn'], writes=['ybT'])

    def phase_merge(uT, yaT, ybT, mergedT, offs):
        AR.seek(offs[0])
        prw = AR.alloc([128, 4, D], BF16)
        AR.seek(offs[1])
        pat = AR.alloc([128, 4, D], BF16)
        wga = [AR.alloc([128, 8, 128], BF16) for _ in range(2)]
        wgb = [AR.alloc([128, 8, 128], BF16) for _ in range(2)]
        sgaP = [AR.alloc([128, 512], BF16) for _ in range(2)]
        sgbP = [AR.alloc([128, 512], BF16) for _ in range(2)]
        t1P = [AR.alloc([128, 512], F32) for _ in range(2)]
        t2P = [AR.alloc([128, 512], F32) for _ in range(2)]
        for hh in range(2):
            S.dma('pool', prw[:, hh * 2:(hh + 1) * 2, :], prw_d[hh * 256:(hh + 1) * 256, :].rearrange("(k p) n -> p k n", p=128), writes=['prw'])
            S.dma('pool', pat[:, hh * 2:(hh + 1) * 2, :], pat_d[hh * 256:(hh + 1) * 256, :].rearrange("(k p) n -> p k n", p=128), writes=['pat'])
        for oc in range(8):
            b = oc % 2
            S.dma('pool', wga[b], win_d[:, 2688 + oc * 128:2688 + (oc + 1) * 128].rearrange("(k p) n -> p k n", p=128), writes=[('wga', b)])
            S.dma('pool', wgb[b], win_d[:, 3712 + oc * 128:3712 + (oc + 1) * 128].rearrange("(k p) n -> p k n", p=128), writes=[('wgb', b)])
            for tb in range(NB):
                sl = slice(tb * 512, (tb + 1) * 512)
                L = tb % 2
                sga, sgb, t1, t2 = sgaP[L], sgbP[L], t1P[L], t2P[L]
                for k in range(8):
                    S.op('pe', lambda: PE.matmul(ps[0 + 4 * L], lhsT=wga[b][:, k, :], rhs=uT[:, k, sl], start=(k == 0), stop=(k == 7)),
                         reads=[('wga', b), ('uT', tb)], writes=[psk[0 + 4 * L]], pe_acc=True)
                S.op('act', lambda: ACT.activation(out=sga, in_=ps[0 + 4 * L], func=AF.Sigmoid), reads=[psk[0 + 4 * L]], writes=[('sga', L)])
                for k in range(8):
                    S.op('pe', lambda: PE.matmul(ps[1 + 4 * L], lhsT=wgb[b][:, k, :], rhs=uT[:, k, sl], start=(k == 0), stop=(k == 7)),
                         reads=[('wgb', b), ('uT', tb)], writes=[psk[1 + 4 * L]], pe_acc=True)
                S.op('act', lambda: ACT.activation(out=sgb, in_=ps[1 + 4 * L], func=AF.Sigmoid), reads=[psk[1 + 4 * L]], writes=[('sgb', L)])
                for k in range(4):
                    S.op('pe', lambda: PE.matmul(ps[2 + 4 * L], lhsT=prw[:, k, oc * 128:(oc + 1) * 128], rhs=yaT[:, k, sl], start=(k == 0), stop=(k == 3)),
                         reads=['prw', 'yaT'], writes=[psk[2 + 4 * L]], pe_acc=True)
                for k in range(4):
                    S.op('pe', lambda: PE.matmul(ps[3 + 4 * L], lhsT=pat[:, k, oc * 128:(oc + 1) * 128], rhs=ybT[:, k, sl], start=(k == 0), stop=(k == 3)),
                         reads=['pat', 'ybT'], writes=[psk[3 + 4 * L]], pe_acc=True)
                S.op('dve', lambda: V.tensor_tensor(out=t1, in0=ps[2 + 4 * L], in1=sga, op=ALU.mult), reads=[psk[2 + 4 * L], ('sga', L)], writes=[('t1', L)])
                S.op('dve', lambda: V.tensor_tensor(out=t2, in0=ps[3 + 4 * L], in1=sgb, op=ALU.mult), reads=[psk[3 + 4 * L], ('sgb', L)], writes=[('t2', L)])
                S.op('pool', lambda: POOL.tensor_tensor(out=mergedT[:, oc, sl], in0=t1, in1=t2, op=ALU.add), reads=[('t1', L), ('t2', L)], writes=[('mg', tb)])

    def phase_x1(s, mergedT, u2tm, base):
        AR.seek(base)
        wo = AR.alloc([128, 8, D], BF16)
        gt1B = AR.alloc([128, D], F32)
        sc2 = AR.alloc([128, D], F32)
        sh2 = AR.alloc([128, D], F32)
        xt = [AR.alloc([128, D], F32) for _ in range(2)]
        x1t = [AR.alloc([128, D], F32) for _ in range(2)]
        tmpP = [AR.alloc([128, D], F32) for _ in range(2)]
        junkP = [AR.alloc([128, D], BF16) for _ in range(2)]
        u2TP = [AR.alloc([128, 8, 128], BF16) for _ in range(2)]
        ssP = [AR.alloc([128, 8], F32) for _ in range(2)]
        exP = [AR.alloc([128, E], F32) for _ in range(2)]
        for hh in range(4):
            S.dma('pool', wo[:, hh * 2:(hh + 1) * 2, :], wout_d[hh * 256:(hh + 1) * 256, :].rearrange("(k p) n -> p k n", p=128), writes=['wo'])
        S.dma('sp', gt1B, mod_d[s, 2], writes=['gt1B'])
        S.dma('sp', sc2, mod_d[s, 4], writes=['sc2'])
        S.dma('sp', sh2, mod_d[s, 3], writes=['sh2'])
        lg = AR.alloc([128, NT, E], F32)
        mxs = AR.alloc([128, 3, NT], F32)

        def gen_x1(i):
            b = i % 2
            sl = slice(i * 128, (i + 1) * 128)
            S.dma('sp', xt[b], x_d[s, sl, :], writes=[('xt', b)])
            tmp, junk, u2T, ss = tmpP[b], junkP[b], u2TP[b], ssP[b]
            for cb in range(2):
                for k in range(8):
                    S.op('pe', lambda: PE.matmul(ps[cb + 6 * b], lhsT=mergedT[:, k, sl], rhs=wo[:, k, cb * 512:(cb + 1) * 512], start=(k == 0), stop=(k == 7)),
                         reads=[('mg', i // 4), 'wo'], writes=[psk[cb + 6 * b]], pe_acc=True)
                S.op('dve', lambda: V.tensor_tensor(out=tmp[:, cb * 512:(cb + 1) * 512], in0=ps[cb + 6 * b], in1=gt1B[:, cb * 512:(cb + 1) * 512], op=ALU.mult),
                     reads=[psk[cb + 6 * b], 'gt1B'], writes=[('tmp', b, cb)])
            yield
            S.op('pool', lambda: POOL.tensor_tensor(out=x1t[b], in0=tmp, in1=xt[b], op=ALU.add), reads=[('tmp', b, 0), ('tmp', b, 1), ('xt', b)], writes=[('x1t', b)])
            S.dma('sp', out_d[s, sl, :], x1t[b], reads=[('x1t', b)], writes=[('outd', i)])
            S.op('act', lambda: ACT.activation(out=junk, in_=x1t[b], func=AF.Square, accum_out=ss[:, 0:1]), reads=[('x1t', b)], writes=[('junk', b), ('ss0', b)])
            yield
            S.op('act', lambda: ACT.activation(out=ss[:, 1:2], in_=ss[:, 0:1], func=AF.Sqrt, bias=epsc[:, 0:1], scale=1.0 / D), reads=[('ss0', b), 'epsc'], writes=[('ss1', b)])
            S.op('dve', lambda: V.reciprocal(out=ss[:, 1:2], in_=ss[:, 1:2]), reads=[('ss1', b)], writes=[('ss1', b)])
            S.op('dve', lambda: V.scalar_tensor_tensor(out=tmp, in0=x1t[b], scalar=ss[:, 1:2], in1=sc2, op0=ALU.mult, op1=ALU.mult),
                 reads=[('x1t', b), ('ss1', b), 'sc2'], writes=[('tmp', b, 0), ('tmp', b, 1)])
            yield
            S.op('pool', lambda: POOL.tensor_tensor(out=u2tm[:, i, :], in0=tmp, in1=sh2, op=ALU.add), reads=[('tmp', b, 0), ('tmp', b, 1), 'sh2'], writes=[('u2', i)])
            pz = ps[2 + b].bitcast(BF16).rearrange("p (k t) -> p k t", k=8)
            pk = psk[2 + b]
            for k in range(8):
                S.op('pe', lambda: PE.transpose(out=pz[:, k, :], in_=u2tm[:, i, k * 128:(k + 1) * 128], identity=ident),
                     reads=[('u2', i), 'ident'], writes=[pk], pe_acc=True)
            yield
            S.op('act', lambda: ACT.copy(out=u2T, in_=pz), reads=[pk], writes=[('u2T', b)])
            pl, pkl = ps[4 + b], psk[4 + b]
            for k in range(8):
                S.op('pe', lambda: PE.matmul(pl[:, 0:E], lhsT=u2T[:, k, :], rhs=wr[:, k, :], start=(k == 0), stop=(k == 7)),
                     reads=[('u2T', b), 'wr'], writes=[pkl], pe_acc=True)
            yield
            S.op('act', lambda: ACT.copy(out=lg[:, i, :], in_=pl[:, 0:E]), reads=[pkl], writes=['lg'])
            yield

        def run_tasks(tasks):
            tasks = list(tasks)
            while tasks:
                for t_ in list(tasks):
                    try:
                        next(t_)
                    except StopIteration:
                        tasks.remove(t_)

        for i in range(0, NT, 2):
            run_tasks([gen_x1(i), gen_x1(i + 1)])
        S.op('dve', lambda: V.tensor_reduce(out=mxs[:, 0, :], in_=lg, axis=AX.X, op=ALU.max), reads=['lg'], writes=['mx0'])
        S.op('dve', lambda: V.tensor_tensor(out=lg, in0=lg, in1=bc(mxs[:, 0, :].rearrange("p (i o) -> p i o", o=1), [128, NT, E]), op=ALU.subtract),
             reads=['lg', 'mx0'], writes=['lg'])
        S.op('act', lambda: ACT.activation(out=lg, in_=lg, func=AF.Exp), reads=['lg'], writes=['lg'])
        S.op('dve', lambda: V.tensor_reduce(out=mxs[:, 1, :], in_=lg, axis=AX.X, op=ALU.add), reads=['lg'], writes=['mx1'])
        S.op('dve', lambda: V.reciprocal(out=mxs[:, 2, :], in_=mxs[:, 1, :]), reads=['mx1'], writes=['mx2'])
        S.op('dve', lambda: V.tensor_tensor(out=afftm, in0=lg, in1=bc(mxs[:, 2, :].rearrange("p (i o) -> p i o", o=1), [128, NT, E]), op=ALU.mult),
             reads=['lg', 'mx2'], writes=['afftm'])

    def phase_moe(s, u2tm, base):
        AR.seek(base)
        affT = AR.alloc([16, T], F32)
        work = AR.alloc([16, T], F32)
        maskT = AR.alloc([16, T], F32)
        slotT = AR.alloc([16, T], F32)
        mx8 = AR.alloc([16, 8], F32)
        for i in range(NT):
            pz = ps[i // 4]
            S.op('pe', lambda: PE.transpose(out=pz[0:16, (i % 4) * 128:(i % 4 + 1) * 128], in_=afftm[:, i, :], identity=identf),
                 reads=['afftm', 'identf'], writes=[psk[i // 4]], pe_acc=True)
        for q in range(4):
            S.op('act', lambda: ACT.copy(out=affT[:, q * 512:(q + 1) * 512], in_=ps[q][0:16, :]), reads=[psk[q]], writes=['affT'])
        S.op('dve', lambda: V.tensor_copy(out=work, in_=affT), reads=['affT'], writes=['work'])
        for it in range(CAP // 8):
            S.op('dve', lambda: V.max(out=mx8, in_=work), reads=['work'], writes=['mx8'])
            if it < CAP // 8 - 1:
                S.op('dve', lambda: V.match_replace(out=work, in_to_replace=mx8, in_values=work, imm_value=-1.0), reads=['work', 'mx8'], writes=['work'])
        S.op('dve', lambda: V.tensor_scalar(out=maskT, in0=affT, scalar1=mx8[:, 7:8], scalar2=None, op0=ALU.is_ge), reads=['affT', 'mx8'], writes=['maskT'])
        S.op('pool', lambda: POOL.memset(work, 1.0), reads=['work'], writes=['work'])
        S.op('dve', lambda: V.tensor_tensor_scan(out=slotT, data0=work, data1=maskT, initial=0.0, op0=ALU.mult, op1=ALU.add), reads=['work', 'maskT'], writes=['slotT'])
        S.op('dve', lambda: V.tensor_tensor(out=slotT, in0=slotT, in1=maskT, op=ALU.mult), reads=['slotT', 'maskT'], writes=['slotT'])
        S.op('dve', lambda: V.tensor_scalar(out=slotT, in0=slotT, scalar1=-1.0, scalar2=None, op0=ALU.add), reads=['slotT'], writes=['slotT'])
        pz = ps[4]
        for i in range(NT):
            S.op('pe', lambda: PE.transpose(out=pz[:, i * 16:(i + 1) * 16], in_=slotT[:, i * 128:(i + 1) * 128], identity=identf[0:16, 0:16]),
                 reads=['slotT', 'identf'], writes=[psk[4]], pe_acc=True)
        S.op('act', lambda: ACT.copy(out=slot_tm.rearrange("p i e -> p (i e)"), in_=pz[:, 0:256]), reads=[psk[4]], writes=['slot_tm'])
        S.op('dve', lambda: V.tensor_copy(out=affhl[:, :, :, 0], in_=afftm), reads=['afftm'], writes=['affhl'])
        S.op('dve', lambda: V.tensor_tensor(out=affhl[:, :, :, 1], in0=afftm, in1=affhl[:, :, :, 0], op=ALU.subtract), reads=['afftm', 'affhl'], writes=['affhl'])
        S.barrier()
        AR.seek(base)
        ye = AR.alloc([128, E, 2, D], BF16)
        Wg = AR.alloc([128, 8, D], BF16)
        Wu = AR.alloc([128, 8, D], BF16)
        Wd = AR.alloc([128, 8, D], BF16)
        wbase = AR.ptr
        Pe = AR.alloc([128, NT, CAP], BF16)
        xeT = AR.alloc([128, 8, CAP], BF16)
        hT = AR.alloc([128, 8, CAP], BF16)
        hs = AR.alloc([128, CAP], F32)
        affs = AR.alloc([128, 4], F32)
        gt2B = AR.alloc([128, D], F32)
        S.dma('sp', gt2B, mod_d[s, 5], writes=['gt2B'])
        for e in range(E):
            for (wt, wsrc, nm) in ((Wg, wg_d, 'Wg'), (Wu, wu_d, 'Wu'), (Wd, wd_d, 'Wd')):
                for hh in range(4):
                    S.dma('pool', wt[:, hh * 2:(hh + 1) * 2, :], wsrc[e, hh * 256:(hh + 1) * 256, :].rearrange("(k p) n -> p k n", p=128), writes=[(nm, hh)])
            for i in range(NT):
                S.op('dve', lambda: V.tensor_scalar(out=Pe[:, i, :], in0=iota_row, scalar1=slot_tm[:, i, e:e + 1], scalar2=None, op0=ALU.is_equal),
                     reads=['iota_row', 'slot_tm'], writes=[('Pe', i)])
            for fc in range(8):
                pz, pk = ps[fc // 2], psk[fc // 2]
                pzs = pz[:, (fc % 2) * 256:(fc % 2 + 1) * 256]
                for i in range(NT):
                    S.op('pe', lambda: PE.matmul(pzs, lhsT=u2tm[:, i, fc * 128:(fc + 1) * 128], rhs=Pe[:, i, :], start=(i == 0), stop=(i == NT - 1)),
                         reads=[('u2', i), ('Pe', i)], writes=[pk], pe_acc=True)
                S.op('act', lambda: ACT.copy(out=xeT[:, fc, :], in_=pzs), reads=[pk], writes=[('xeT', fc)])
            pa, pka = ps[4], psk[4]
            for half in range(2):
                for i in range(NT):
                    S.op('pe', lambda: PE.matmul(pa[:, half * 2:(half + 1) * 2], lhsT=Pe[:, i, half * 128:(half + 1) * 128], rhs=affhl[:, i, e, :], start=(i == 0), stop=(i == NT - 1)),
                         reads=[('Pe', i), 'affhl'], writes=[pka], pe_acc=True)
            S.op('dve', lambda: V.tensor_reduce(out=affs[:, 0:2], in_=pa[:, 0:4].rearrange("p (h t) -> p h t", t=2), axis=AX.X, op=ALU.add), reads=[pka], writes=['affs'])
            for fk in range(8):
                pg, pkg = ps[5], psk[5]
                pu, pku = ps[6], psk[6]
                for k in range(8):
                    S.op('pe', lambda: PE.matmul(pg[:, 0:CAP], lhsT=Wg[:, k, fk * 128:(fk + 1) * 128], rhs=xeT[:, k, :], start=(k == 0), stop=(k == 7)),
                         reads=[('Wg', k // 2), ('xeT', k)], writes=[pkg], pe_acc=True)
                for k in range(8):
                    S.op('pe', lambda: PE.matmul(pu[:, 0:CAP], lhsT=Wu[:, k, fk * 128:(fk + 1) * 128], rhs=xeT[:, k, :], start=(k == 0), stop=(k == 7)),
                         reads=[('Wu', k // 2), ('xeT', k)], writes=[pku], pe_acc=True)
                S.op('act', lambda: ACT.activation(out=hs, in_=pg[:, 0:CAP], func=AF.Silu), reads=[pkg], writes=['hs'])
                S.op('dve', lambda: V.tensor_tensor(out=hT[:, fk, :], in0=pu[:, 0:CAP], in1=hs, op=ALU.mult), reads=[pku, 'hs'], writes=[('hT', fk)])
            for half in range(2):
                for cb in range(2):
                    py, pky = ps[7] if (half * 2 + cb) % 2 else ps[4], psk[7] if (half * 2 + cb) % 2 else psk[4]
                    for fk in range(8):
                        S.op('pe', lambda: PE.matmul(py, lhsT=hT[:, fk, half * 128:(half + 1) * 128], rhs=Wd[:, fk, cb * 512:(cb + 1) * 512], start=(fk == 0), stop=(fk == 7)),
                             reads=[('hT', fk), ('Wd', fk // 2), 'affs'], writes=[pky], pe_acc=True)
                    S.op('dve', lambda: V.tensor_scalar(out=ye[:, e, half, cb * 512:(cb + 1) * 512], in0=py, scalar1=affs[:, half:half + 1], scalar2=None, op0=ALU.mult),
                         reads=[pky, 'affs'], writes=['ye'])
        S.barrier()
        AR.seek(wbase - 3 * 8 * D * 2)
        Pall = AR.alloc([128, E, CAP], BF16)
        PT = AR.alloc([128, 2 * E, 128], BF16)
        x1t = [AR.alloc([128, D], F32) for _ in range(2)]
        ot = [AR.alloc([128, D], F32) for _ in range(2)]
        for i in range(NT):
            b = i % 2
            sl = slice(i * 128, (i + 1) * 128)
            S.dma('sp', x1t[b], out_d[s, sl, :], reads=[('outd', i)], writes=[('x1t', b)])
            for e in range(E):
                S.op('dve', lambda: V.tensor_scalar(out=Pall[:, e, :], in0=iota_row, scalar1=slot_tm[:, i, e:e + 1], scalar2=None, op0=ALU.is_equal),
                     reads=['iota_row', 'slot_tm'], writes=[('Pall', e // 4)])
            for q in range(4):
                pz = ps[q].bitcast(BF16).rearrange("p (j t) -> p j t", j=8)
                for j in range(8):
                    idx = q * 8 + j
                    e, half = idx // 2, idx % 2
                    S.op('pe', lambda: PE.transpose(out=pz[:, j, :], in_=Pall[:, e, half * 128:(half + 1) * 128], identity=ident),
                         reads=[('Pall', e // 4), 'ident'], writes=[psk[q]], pe_acc=True)
                if q % 2 == 0:
                    S.op('act', lambda: ACT.copy(out=PT[:, q * 8:(q + 1) * 8, :], in_=pz), reads=[psk[q]], writes=[('PT', q)])
                else:
                    S.op('dve', lambda: V.tensor_copy(out=PT[:, q * 8:(q + 1) * 8, :], in_=pz), reads=[psk[q]], writes=[('PT', q)])
            for cb in range(2):
                po, pko = ps[4 + cb + 2 * (i % 2)], psk[4 + cb + 2 * (i % 2)]
                for idx in range(2 * E):
                    e, half = idx // 2, idx % 2
                    S.op('pe', lambda: PE.matmul(po, lhsT=PT[:, idx, :], rhs=ye[:, e, half, cb * 512:(cb + 1) * 512], start=(idx == 0), stop=(idx == 2 * E - 1)),
                         reads=[('PT', idx // 8), 'ye'], writes=[pko], pe_acc=True)
                S.op('dve', lambda: V.tensor_tensor(out=ot[b][:, cb * 512:(cb + 1) * 512], in0=po, in1=gt2B[:, cb * 512:(cb + 1) * 512], op=ALU.mult),
                     reads=[pko, 'gt2B'], writes=[('ot', b, cb)])
            S.op('dve', lambda: V.tensor_tensor(out=ot[b], in0=ot[b], in1=x1t[b], op=ALU.add), reads=[('ot', b, 0), ('ot', b, 1), ('x1t', b)], writes=[('ot', b, 0), ('ot', b, 1)])
            S.dma('sp', out_d[s, sl, :], ot[b], reads=[('ot', b, 0), ('ot', b, 1)], writes=[('outd', i)])

    def dbg_dump(src_ap, shape, key_reads=()):
        AR.seek(AR_TOP)
        t = AR.alloc(shape, F32)
        S.op('dve', lambda: V.tensor_copy(out=t, in_=src_ap), writes=['dbgt'])
        flat = t if len(shape) == 2 else t.rearrange("p a b -> p (a b)")
        S.dma('sp', dbg_d, flat, reads=['dbgt'])

    AR_TOP = 160 * 1024
    phase_adaln()
    for s in range(nseq):
        AR.seek(0)
        zsT = AR.alloc([128, 15, T], BF16)
        uT = AR.alloc([128, 8, T], BF16)
        base1 = AR.ptr
        phase_norm1(s, uT, base1)
        S.barrier()
        if dbg and dbg[0] == 'uT':
            dbg_dump(uT[:, :, 0:512], [128, 8, 512]); break
        for q in range(4):
            S.dma('sp', u_d[:, 2 * q:2 * q + 2, :], uT[:, 2 * q:2 * q + 2, :], reads=[('uT', 0), ('uT', 1), ('uT', 2), ('uT', 3)], writes=['uscr'])
        phase_rwkv_cols(uT, zsT, base1)
        S.barrier()
        if dbg and dbg[0] == 'zs':
            dbg_dump(zsT[:, :, 0:256], [128, 15, 256]); break
        AR.seek(61440)
        kkT = AR.alloc([128, 4, T], BF16)
        yaT = AR.alloc([128, 4, T], BF16)
        base3 = AR.ptr
        phase_scan(zsT, kkT, 77824)
        if dbg and dbg[0] == 'yscan':
            AR.seek(AR_TOP)
            t = AR.alloc([128, 2, 512], F32)
            S.dma('sp', t[:, 0, :], y_d[0, 0:128, :], writes=['dbgt'])
            S.dma('sp', t[:, 1, :], y_d[1, 0:128, :], writes=['dbgt'])
            S.dma('sp', dbg_d, t.rearrange("p a b -> p (a b)"), reads=['dbgt']); break
        phase_post(zsT, yaT, base3)
        S.barrier()
        if dbg and dbg[0] == 'yaT':
            dbg_dump(yaT[:, :, 0:512], [128, 4, 512]); break
        AR.seek(0)
        uT = AR.alloc([128, 8, T], BF16)
        AR.seek(94208)
        ybT = AR.alloc([128, 4, T], BF16)
        baseB = AR.ptr
        for q in range(4):
            S.dma('sp' if q % 2 == 0 else 'act', uT[:, 2 * q:2 * q + 2, :], u_d[:, 2 * q:2 * q + 2, :], writes=[('uT', 0), ('uT', 1), ('uT', 2), ('uT', 3)])
        phase_attn(s, uT, ybT, 32768, baseB)
        S.barrier()
        if dbg and dbg[0] == 'ybT':
            dbg_dump(ybT[:, :, 0:512], [128, 4, 512]); break
        AR.seek(32768)
        mergedT = AR.alloc([128, 8, T], BF16)
        phase_merge(uT, yaT, ybT, mergedT, (65536, baseB))
        S.barrier()
        if dbg and dbg[0] == 'merged':
            dbg_dump(mergedT[:, :, 0:512], [128, 8, 512]); break
        AR.seek(0)
        u2tm = AR.alloc([128, NT, D], BF16)
        phase_x1(s, mergedT, u2tm, 65536)
        S.barrier()
        if dbg and dbg[0] == 'aff':
            dbg_dump(afftm.rearrange("p i e -> p (i e)"), [128, 256]); break
        phase_moe(s, u2tm, 32768)
        S.barrier()

    S.finish('sp')
    print("ninstr", S.ninstr, "pe_incs", S.npe_inc, "arena hi", AR.hi)
    return nc


def _consts():
    cm = np.zeros((13, 128, 128), np.float32)
    p = np.arange(128)
    cm[0] = (p[:, None] // 64 == p[None, :] // 64).astype(np.float32)
    R = np.zeros((128, 128), np.float32)
    for blk in range(2):
        o = blk * 64
        for d_ in range(8):
            R[o + d_ + 8, o + d_] = -1.0
            R[o + d_, o + d_ + 8] = 1.0
    cm[1] = R
    cm[2] = (p[:, None] >= p[None, :]).astype(np.float32)
    cm[3] = (p[:, None] <= p[None, :]).astype(np.float32)
    s_ = (p % 64)[:, None]
    t_ = (p % 64)[None, :]
    a_col = (p[None, :] >= 64)
    fwd = np.where(a_col, s_ < t_, s_ <= t_)
    bwd = np.where(a_col, s_ > t_, s_ >= t_)
    cm[4] = fwd.astype(np.float32)
    cm[5] = bwd.astype(np.float32)
    cm[6] = cm[4].T
    cm[7] = cm[5].T
    cm[8][:, 0] = (p < 64)
    cm[8][:, 1] = (p >= 64)
    cm[9][:, 0:64] = 1.0
    cm[10][:, 64:128] = 1.0
    cm[11] = ((p % 64)[:, None] < (p % 64)[None, :]).astype(np.float32)
    cm[12] = ((p % 64)[:, None] > (p % 64)[None, :]).astype(np.float32)
    return np.ascontiguousarray(cm.transpose(1, 0, 2).reshape(128, 13 * 128))


def _prep_shared(inp):
    f = lambda a: np.ascontiguousarray(np.asarray(a, dtype=np.float32))
    L = 0
    w_in = f(inp["w_in"][L]).copy()
    qoff = 1920
    perm = []
    for c in range(4):
        perm += list(range(c * 64, (c + 1) * 64)) + list(range((4 + c) * 64, (5 + c) * 64))
    perm = np.array(perm)
    w_in[:, qoff:qoff + 512] = w_in[:, qoff:qoff + 512][:, perm]
    p_attn = f(inp["p_attn"][L])[perm, :]
    pp = np.zeros((128, NPP), np.float32)

    def put(name, arr):
        o, w = PP[name]
        pp[:, o:o + w] = arr

    chunked = lambda v: np.asarray(v, np.float32).reshape(-1, 128).T
    put("mp", chunked(inp["mu_prev"][L]))
    put("mn", chunked(inp["mu_next"][L]))
    put("w0", np.concatenate([chunked(inp["rwkv_w0"][L][0]), chunked(inp["rwkv_w0"][L][1])], 1))
    put("a0", np.concatenate([chunked(inp["rwkv_a0"][L][0]), chunked(inp["rwkv_a0"][L][1])], 1))
    put("kk", chunked(inp["rwkv_k_k"][L]))
    put("ka", chunked(inp["rwkv_k_a"][L]))
    put("rk", chunked(np.asarray(inp["rwkv_r_k"][L]).reshape(-1)))
    put("qg", np.tile(np.asarray(inp["q_norm_g"][L], np.float32), 2)[:, None])
    put("kg", np.tile(np.asarray(inp["k_norm_g"][L], np.float32), 2)[:, None])
    inv_freq = (500000.0 ** (-np.arange(0, 16, 2, dtype=np.float32) / 16)).astype(np.float32)
    invf = np.zeros(64, np.float32)
    invf[0:8] = inv_freq
    invf[8:16] = inv_freq
    put("invf", np.tile(invf, 2)[:, None])
    sink = np.asarray(inp["attn_sink"][L], np.float32)
    sk = np.zeros((128, 4), np.float32)
    for j in range(4):
        sk[0:64, j] = sink[j]
        sk[64:128, j] = sink[4 + j]
    put("sink", sk)
    w2cat = np.zeros((128, 2, 512), np.float32)
    a2cat = np.zeros((128, 2, 512), np.float32)
    for d_ in range(2):
        w2cat[d_ * 64:(d_ + 1) * 64, d_, :] = inp["rwkv_w2"][L][d_]
        a2cat[d_ * 64:(d_ + 1) * 64, d_, :] = inp["rwkv_a2"][L][d_]
    return {
        "w_ada": f(inp["w_ada"][L]), "b_ada": f(inp["b_ada"][L])[None, :] if np.asarray(inp["b_ada"][L]).ndim == 1 else f(inp["b_ada"][L]),
        "norm1_g": f(inp["norm1_g"][L]).reshape(1, D), "norm2_g": f(inp["norm2_g"][L]).reshape(1, D),
        "w_in": w_in, "pp": pp, "w2cat": w2cat.reshape(128, 1024), "a2cat": a2cat.reshape(128, 1024),
        "g2": f(inp["rwkv_g2"][L]), "gn_w": f(inp["rwkv_gn_w"][L]).reshape(1, 512), "gn_b": f(inp["rwkv_gn_b"][L]).reshape(1, 512),
        "p_rwkv": f(inp["p_rwkv"][L]), "p_attn": np.ascontiguousarray(p_attn), "w_out": f(inp["w_out"][L]),
        "w_router": f(inp["w_router"][L]), "w_gate": f(inp["w_gate"][L]), "w_up": f(inp["w_up"][L]), "w_down": f(inp["w_down"][L]),
        "cmats": _consts(),
    }


def _core_inputs(inp, shared, seqs):
    x = np.ascontiguousarray(np.asarray(inp["x"], np.float32)[seqs])
    c = np.asarray(inp["c"], np.float32)[seqs]
    cT = np.ascontiguousarray(c.reshape(len(seqs), 8, 128).transpose(0, 2, 1))
    pos = np.ascontiguousarray(np.asarray(inp["positions"]).astype(np.int32)[seqs][:, None, :])
    m = dict(shared)
    m.update({"x": x, "cT": cT, "pos": pos})
    return m


def kernel(**inputs):
    shared = _prep_shared(inputs)
    nc = build(NSEQ)
    in_maps = [_core_inputs(inputs, shared, list(range(i * NSEQ, (i + 1) * NSEQ))) for i in range(NCORES)]
    res = run_bass_kernel_spmd(nc, in_maps, core_ids=list(range(NCORES)))
    out = np.concatenate([np.asarray(r["out"]) for r in res.results], axis=0)
    return out.astype(np.float32)
```

```python
import numpy as np
import concourse.bass as bass
import concourse.mybir as mybir
from concourse.bass_utils import run_bass_kernel_spmd

F32 = mybir.dt.float32
BF16 = mybir.dt.bfloat16
I32 = mybir.dt.int32
ALU = mybir.AluOpType
AF = mybir.ActivationFunctionType
AX = mybir.AxisListType

T = 2048
D = 1024
NT = 16
NB = 4
NSEQ = 2
NCORES = 8
E = 16
CAP = 256
LAM = float(np.exp(-0.5))
NCH = 4
TBS = NCH * 64
NTB = T // TBS
TWO_PI = float(2 * np.pi)
C1 = 6.28125
C2 = TWO_PI - C1

PP = {}
_o = 0
for _n, _w in [("mp", 15), ("mn", 15), ("w0", 8), ("a0", 8), ("kk", 4), ("ka", 4), ("rk", 4), ("qg", 1), ("kg", 1),
               ("invf", 1), ("sink", 4)]:
    PP[_n] = (_o, _w)
    _o += _w
NPP = _o


class Ticket:
    __slots__ = ('ins', 'sem', 'val', 'parent')

    def __init__(self, ins):
        self.ins = ins
        self.sem = None
        self.val = None
        self.parent = None

    def root(self):
        t = self
        while t.parent is not None:
            t = t.parent
        return t


class Sync:
    SEM_MAX = 30000

    def __init__(self, nc):
        self.nc = nc
        self.E = {'pe': nc.tensor, 'act': nc.scalar, 'dve': nc.vector, 'pool': nc.gpsimd, 'sp': nc.sync}
        self.sem = {}
        self.cnt = {}
        self.nsem = 0
        for e in self.E:
            self._newsem(e)
        self.waited = {}
        self.lastw = {}
        self.reads = {}
        self.dma_sems = {}
        self.dma_rr = {}
        self.ninstr = 0
        self.pend = None
        self.pend_writes = None
        self.npe_inc = 0

    def _newsem(self, e):
        self.sem[e] = self.nc.alloc_semaphore(f"s_{e}_{self.nsem}")
        self.nsem += 1
        self.cnt[e] = 0

    def _flush_pe(self):
        t = self.pend
        if t is None:
            return
        if self.cnt['pe'] >= self.SEM_MAX:
            self._newsem('pe')
        self.cnt['pe'] += 1
        t.sem = self.sem['pe']
        t.val = self.cnt['pe']
        t.ins.then_inc(t.sem, 1)
        self.npe_inc += 1
        self.pend = None
        self.pend_writes = None

    def _wait(self, e, ev):
        if ev is None:
            return
        if isinstance(ev, Ticket):
            if e == 'pe':
                return
            t = ev.root()
            if t.val is None:
                assert t is self.pend
                self._flush_pe()
            sem, val = t.sem, t.val
        else:
            src, sem, val = ev
        k = (e, sem.name)
        if self.waited.get(k, 0) >= val:
            return
        self.waited[k] = val
        self.E[e].wait_ge(sem, val)

    def deps(self, e, reads, writes, pe_acc=False):
        for k in reads:
            self._wait(e, self.lastw.get(k))
        for k in writes:
            lw = self.lastw.get(k)
            if not (pe_acc and isinstance(lw, Ticket)):
                self._wait(e, lw)
            for ev in self.reads.get(k, {}).values():
                self._wait(e, ev)

    def commit(self, src, ev, reads, writes):
        for k in reads:
            self.reads.setdefault(k, {})[src] = ev
        for k in writes:
            self.lastw[k] = ev
            self.reads[k] = {}

    def op(self, e, fn, reads=(), writes=(), pe_acc=False):
        self.deps(e, reads, writes, pe_acc)
        if e == 'pe':
            ins = fn()
            t = Ticket(ins)
            if self.pend is not None:
                if self.pend_writes == tuple(writes):
                    self.pend.parent = t
                    self.pend = None
                else:
                    self._flush_pe()
            self.pend = t
            self.pend_writes = tuple(writes)
            self.commit('pe', t, reads, writes)
            self.ninstr += 1
            return t
        if self.cnt[e] >= self.SEM_MAX:
            self._newsem(e)
        ins = fn()
        self.cnt[e] += 1
        ev = (e, self.sem[e], self.cnt[e])
        ins.then_inc(self.sem[e], 1)
        self.commit(e, ev, reads, writes)
        self.ninstr += 1
        return ev

    def dma(self, e, out, in_, reads=(), writes=(), nslots=8, **kw):
        if e == 'pool':
            nslots = 2
        lst = self.dma_sems.setdefault(e, [])
        if len(lst) < nslots:
            lst.append([self.nc.alloc_semaphore(f"d_{e}_{len(lst)}"), 0])
        i = self.dma_rr.get(e, 0)
        self.dma_rr[e] = (i + 1) % nslots
        slot = lst[i % len(lst)]
        sem, uses = slot
        if uses > 0:
            self._wait(e, ('dma', sem, 16 * uses))
        self.deps(e, reads, writes)
        self.E[e].dma_start(out=out, in_=in_, **kw).then_inc(sem, 16)
        slot[1] = uses + 1
        ev = ('dma_%s_%d' % (e, i % len(lst)), sem, 16 * (uses + 1))
        self.commit(ev[0], ev, reads, writes)
        self.ninstr += 1
        return ev

    def barrier(self):
        self._flush_pe()
        evs = [(e, self.sem[e], self.cnt[e]) for e in self.E if self.cnt[e] > 0]
        for q, lst in self.dma_sems.items():
            for sem, uses in lst:
                if uses:
                    evs.append(('dma', sem, 16 * uses))
        for e in self.E:
            for ev in evs:
                if ev[0] != e:
                    self._wait(e, ev)
        self.lastw = {}
        self.reads = {}

    def finish(self, e='sp'):
        self._flush_pe()
        for q, lst in self.dma_sems.items():
            for sem, uses in lst:
                if uses:
                    self._wait(e, ('dma', sem, 16 * uses))


class Arena:
    def __init__(self, nc, name, nbytes):
        self.n4 = nbytes // 4
        self.t = nc.alloc_sbuf_tensor(name, [128, self.n4], F32).ap()
        self.ptr = 0
        self.hi = 0

    def seek(self, off):
        self.ptr = off

    def alloc(self, shape, dtype, parts=None):
        esz = 4 if dtype in (F32, I32) else 2
        n = int(np.prod(shape[1:]))
        nb = (n * esz + 31) // 32 * 32
        assert self.ptr % 4 == 0
        a = self.ptr // 4
        assert a + nb // 4 <= self.n4, f"arena overflow {self.ptr}+{nb} > {self.n4 * 4}"
        v = self.t[:, a:a + nb // 4]
        if dtype != F32:
            v = v.bitcast(dtype)
        v = v[0:shape[0], 0:n]
        if len(shape) > 2:
            names = " ".join(f"d{i}" for i in range(len(shape) - 1))
            kw = {f"d{i}": int(shape[i + 1]) for i in range(len(shape) - 1)}
            v = v.rearrange(f"p ({names}) -> p {names}", **kw)
        self.ptr += nb
        self.hi = max(self.hi, self.ptr)
        return v


def bc(ap, shape):
    return ap.to_broadcast(list(shape))


def build(nseq=NSEQ, dbg=None, stop_after=None):
    nc = bass.Bass("TRN2", target_bir_lowering=False)
    S = Sync(nc)
    V, ACT, POOL, PE = nc.vector, nc.scalar, nc.gpsimd, nc.tensor

    def din(name, shape, dt=F32):
        return nc.dram_tensor(name, list(shape), dt, kind="ExternalInput").ap()

    x_d = din("x", [nseq, T, D])
    cT_d = din("cT", [nseq, 128, 8])
    pos_d = din("pos", [nseq, 1, T], I32)
    wada_d = din("w_ada", [D, 6 * D])
    bada_d = din("b_ada", [1, 6 * D])
    n1g_d = din("norm1_g", [1, D])
    n2g_d = din("norm2_g", [1, D])
    win_d = din("w_in", [D, 4736])
    pp_d = din("pp", [128, NPP])
    w2c_d = din("w2cat", [128, 2 * 512])
    a2c_d = din("a2cat", [128, 2 * 512])
    g2_d = din("g2", [128, 512])
    gnw_d = din("gn_w", [1, 512])
    gnb_d = din("gn_b", [1, 512])
    prw_d = din("p_rwkv", [512, D])
    pat_d = din("p_attn", [512, D])
    wout_d = din("w_out", [D, D])
    wr_d = din("w_router", [D, E])
    wg_d = din("w_gate", [E, D, D])
    wu_d = din("w_up", [E, D, D])
    wd_d = din("w_down", [E, D, D])
    cm_d = din("cmats", [128, 13 * 128])
    out_d = nc.dram_tensor("out", [nseq, T, D], F32, kind="ExternalOutput").ap()
    mod_d = nc.dram_tensor("modscr", [nseq, 6, 128, D], F32, kind="Internal").ap()
    y_d = nc.dram_tensor("yscr", [2, T, 512], F32, kind="Internal").ap()
    u_d = nc.dram_tensor("uscr", [128, 8, T], BF16, kind="Internal").ap()
    dbg_d = None
    if dbg is not None:
        dbg_d = nc.dram_tensor("dbg", list(dbg[1]), F32, kind="ExternalOutput").ap()

    def sb(name, shape, dt=F32):
        return nc.alloc_sbuf_tensor('sb_' + name, list(shape), dt).ap()

    pp = sb("pp", [128, NPP])
    ident = sb("ident", [128, 128], BF16)
    identf = sb("identf", [128, 128])
    cmb = sb("cmb", [128, 13, 128], BF16)
    w2c = sb("w2c", [128, 2, 512], BF16)
    a2c = sb("a2c", [128, 2, 512], BF16)
    g2 = sb("g2", [128, 512], BF16)
    wr = sb("wr", [128, 8, E], BF16)
    epsc = sb("epsc", [128, 4])
    alpha = sb("alpha", [128, 15])
    oneminus_ka = sb("omka", [128, 4])
    two_omka = sb("omka2", [128, 4])
    negkkc = sb("negone", [128, 1])
    esk = sb("esk", [128, 4])
    rmask = sb("rmask", [128, TBS])
    iota_row = sb("iota_row", [128, CAP])
    ident4 = sb("ident4", [128, 4, 128], BF16)
    kar = sb("kar", [128, 4])
    c2r = sb("c2r", [128, 4])
    afftm = sb("afftm", [128, NT, E])
    slot_tm = sb("slot_tm", [128, NT, E])
    affhl = sb("affhl", [128, NT, E, 2], BF16)

    BLK1, ROT, MPREV, MNEXT = 0, 1, 2, 3
    MZT = (4, 5)
    MZ = (6, 7)
    HSEL = 8
    VP = (9, 10)

    ps = [nc.alloc_psum_tensor(f"ps{i}", [128, 512], F32).ap() for i in range(8)]
    psk = [f"ps{i}" for i in range(8)]

    AR = Arena(nc, "arena", 192 * 1024)

    def col(name, j=0, n=1):
        o, w = PP[name]
        return pp[:, o + j:o + j + n]

    S.dma('sp', pp, pp_d, writes=['pp'])
    S.dma('pool', cmb.rearrange("p a b -> p (a b)"), cm_d, writes=['cmb'])
    S.dma('pool', w2c.rearrange("p a b -> p (a b)"), w2c_d, writes=['w2c'])
    S.dma('pool', a2c.rearrange("p a b -> p (a b)"), a2c_d, writes=['a2c'])
    S.dma('pool', g2, g2_d, writes=['g2'])
    S.dma('pool', wr, wr_d.rearrange("(k p) e -> p k e", p=128), writes=['wr'])
    S.op('pool', lambda: POOL.memset(identf, 1.0), writes=['identf'])
    S.op('pool', lambda: POOL.affine_select(out=identf, in_=identf, pattern=[[1, 128]], compare_op=ALU.is_equal,
                                            fill=0.0, base=0, channel_multiplier=-1), reads=['identf'], writes=['identf'])
    S.op('dve', lambda: V.tensor_copy(out=ident, in_=identf), reads=['identf'], writes=['ident'])
    for j in range(4):
        S.op('dve', lambda: V.tensor_copy(out=ident4[:, j, :], in_=identf), reads=['identf'], writes=['ident4'])
    S.op('pool', lambda: POOL.memset(epsc[:, 0:1], 1e-6), writes=['epsc'])
    S.op('pool', lambda: POOL.memset(epsc[:, 1:2], 64e-5), reads=['epsc'], writes=['epsc'])
    S.op('pool', lambda: POOL.memset(epsc[:, 2:3], 1e-24), reads=['epsc'], writes=['epsc'])
    S.op('pool', lambda: POOL.memset(epsc[:, 3:4], 0.0), reads=['epsc'], writes=['epsc'])
    S.op('pool', lambda: POOL.memset(negkkc, -1.0), writes=['negone'])
    S.op('dve', lambda: V.tensor_tensor(out=alpha, in0=col("mp", 0, 15), in1=col("mn", 0, 15), op=ALU.add), reads=['pp'], writes=['alpha'])
    S.op('dve', lambda: V.tensor_scalar(out=alpha, in0=alpha, scalar1=-1.0, scalar2=1.0, op0=ALU.mult, op1=ALU.add), reads=['alpha'], writes=['alpha'])
    S.op('dve', lambda: V.tensor_scalar(out=oneminus_ka, in0=col("ka", 0, 4), scalar1=-1.0, scalar2=1.0, op0=ALU.mult, op1=ALU.add), reads=['pp'], writes=['omka'])
    S.op('dve', lambda: V.tensor_scalar(out=two_omka, in0=col("ka", 0, 4), scalar1=-2.0, scalar2=2.0, op0=ALU.mult, op1=ALU.add), reads=['pp'], writes=['omka2'])
    S.op('act', lambda: ACT.activation(out=esk, in_=col("sink", 0, 4), func=AF.Exp), reads=['pp'], writes=['esk'])
    S.op('dve', lambda: V.tensor_tensor(out=kar, in0=col("ka", 0, 4), in1=col("rk", 0, 4), op=ALU.mult), reads=['pp'], writes=['kar'])
    S.op('dve', lambda: V.tensor_tensor(out=c2r, in0=two_omka, in1=col("rk", 0, 4), op=ALU.mult), reads=['pp', 'omka2'], writes=['kar'])
    S.op('pool', lambda: POOL.memset(rmask, 1.0), writes=['rmask'])
    S.op('pool', lambda: POOL.memset(rmask.rearrange("p (c t) -> p c t", t=64)[:, :, 0:1], 0.0), reads=['rmask'], writes=['rmask'])
    S.op('pool', lambda: POOL.iota(iota_row, pattern=[[1, CAP]], base=0, channel_multiplier=0, allow_small_or_imprecise_dtypes=True), writes=['iota_row'])

    def debug_out(ap_sb, key, rows=None):
        S.dma('sp', dbg_d if rows is None else rows, ap_sb, reads=[key])

    def phase_adaln():
        AR.seek(0)
        csil = [AR.alloc([128, 8], F32) for _ in range(nseq)]
        crep = [AR.alloc([128, 9, 128], F32) for _ in range(nseq)]
        wblk = [AR.alloc([128, 9, 512], F32) for _ in range(3)]
        g1B = AR.alloc([128, D], F32)
        g2B = AR.alloc([128, D], F32)
        mt = [AR.alloc([128, 512], F32) for _ in range(4)]
        S.dma('sp', g1B, n1g_d.partition_broadcast(128), writes=['g1B'])
        S.dma('sp', g2B, n2g_d.partition_broadcast(128), writes=['g2B'])
        for b in range(3):
            S.op('pool', lambda: POOL.memset(wblk[b][:, 8, :], 0.0), writes=[('wblk', b)])
        for s in range(nseq):
            S.dma('sp', csil[s], cT_d[s], writes=[('csil', s)])
            S.op('act', lambda: ACT.activation(out=csil[s], in_=csil[s], func=AF.Silu), reads=[('csil', s)], writes=[('csil', s)])
            S.op('pool', lambda: POOL.memset(crep[s][:, 8, :], 0.0), writes=[('crep', s)])
            S.op('pool', lambda: POOL.memset(crep[s][0:1, 8, :], 1.0), reads=[('crep', s)], writes=[('crep', s)])
            S.op('dve', lambda: V.tensor_copy(out=crep[s][:, 0:8, :], in_=bc(csil[s].rearrange("p (k o) -> p k o", o=1), [128, 8, 128])),
                 reads=[('csil', s)], writes=[('crep', s)])
        ev = 0
        for jb in range(12):
            b = jb % 3
            piece = jb // 2
            c0 = jb * 512
            S.dma('sp', wblk[b][:, 0:4, :], wada_d[0:512, c0:c0 + 512].rearrange("(k p) n -> p k n", p=128), writes=[('wblk', b)])
            S.dma('act', wblk[b][:, 4:8, :], wada_d[512:1024, c0:c0 + 512].rearrange("(k p) n -> p k n", p=128), writes=[('wblk', b)])
            S.dma('sp', wblk[b][0:1, 8, :], bada_d[:, c0:c0 + 512], writes=[('wblk', b)])
            for s in range(nseq):
                pz, pkz = ps[ev % 4], psk[ev % 4]
                for k in range(9):
                    S.op('pe', lambda: PE.matmul(pz, lhsT=crep[s][:, k, :], rhs=wblk[b][:, k, :], start=(k == 0), stop=(k == 8)),
                         reads=[('crep', s), ('wblk', b)], writes=[pkz], pe_acc=True)
                m = mt[ev % 4]
                lc = (jb % 2) * 512
                if piece == 1:
                    S.op('dve', lambda: V.scalar_tensor_tensor(out=m, in0=pz, scalar=1.0, in1=g1B[:, lc:lc + 512], op0=ALU.add, op1=ALU.mult),
                         reads=[pkz, 'g1B'], writes=[('mt', ev % 4)])
                elif piece == 4:
                    S.op('dve', lambda: V.scalar_tensor_tensor(out=m, in0=pz, scalar=1.0, in1=g2B[:, lc:lc + 512], op0=ALU.add, op1=ALU.mult),
                         reads=[pkz, 'g2B'], writes=[('mt', ev % 4)])
                else:
                    S.op('act', lambda: ACT.copy(out=m, in_=pz), reads=[pkz], writes=[('mt', ev % 4)])
                S.dma('sp', mod_d[s, piece, :, lc:lc + 512], m, reads=[('mt', ev % 4)], writes=[('mod', s, piece)])
                ev += 1
        S.barrier()

    def phase_norm1(s, uT, base):
        AR.seek(base)
        scp = AR.alloc([128, D], F32)
        shp = AR.alloc([128, D], F32)
        xt = [AR.alloc([128, D], F32) for _ in range(2)]
        tmp2 = [AR.alloc([128, D], F32) for _ in range(2)]
        ub = [AR.alloc([128, D], BF16) for _ in range(2)]
        junk2 = [AR.alloc([128, D], BF16) for _ in range(2)]
        ss2 = [AR.alloc([128, 2], F32) for _ in range(2)]
        S.dma('sp', scp, mod_d[s, 1], reads=[('mod', s, 1)], writes=['scp'])
        S.dma('sp', shp, mod_d[s, 0], reads=[('mod', s, 0)], writes=['shp'])
        for i in range(NT):
            b = i % 2
            S.dma('sp', xt[b], x_d[s, i * 128:(i + 1) * 128, :], writes=[('xt', b)])
            tmp, junk, ss = tmp2[b], junk2[b], ss2[b]
            S.op('act', lambda: ACT.activation(out=junk, in_=xt[b], func=AF.Square, accum_out=ss[:, 0:1]), reads=[('xt', b)], writes=[('junk', b), ('ss', b)])
            S.op('act', lambda: ACT.activation(out=ss[:, 1:2], in_=ss[:, 0:1], func=AF.Sqrt, bias=epsc[:, 0:1], scale=1.0 / D), reads=[('ss', b), 'epsc'], writes=[('ss1', b)])
            S.op('dve', lambda: V.reciprocal(out=ss[:, 1:2], in_=ss[:, 1:2]), reads=[('ss1', b)], writes=[('ss1', b)])
            S.op('dve', lambda: V.scalar_tensor_tensor(out=tmp, in0=xt[b], scalar=ss[:, 1:2], in1=scp, op0=ALU.mult, op1=ALU.mult),
                 reads=[('xt', b), ('ss1', b), 'scp'], writes=[('tmp', b)])
            S.op('pool', lambda: POOL.tensor_tensor(out=ub[b], in0=tmp, in1=shp, op=ALU.add), reads=[('tmp', b), 'shp'], writes=[('ub', b)])
            pz = ps[i % 2].bitcast(BF16).rearrange("p (k t) -> p k t", k=8)
            for k in range(8):
                S.op('pe', lambda: PE.transpose(out=pz[:, k, :], in_=ub[b][:, k * 128:(k + 1) * 128], identity=ident),
                     reads=[('ub', b), 'ident'], writes=[psk[i % 2]], pe_acc=True)
            S.op('act', lambda: ACT.copy(out=uT[:, :, i * 128:(i + 1) * 128], in_=pz), reads=[psk[i % 2]], writes=[('uT', i // 4)])

    def phase_rwkv_cols(uT, zsT, base):
        AR.seek(base)
        wg = [AR.alloc([128, 8, 128], BF16) for _ in range(2)]
        ztmpP = [AR.alloc([128, T + 2], F32) for _ in range(2)]
        shtP = [AR.alloc([128, T], F32) for _ in range(2)]
        for q in range(2):
            S.op('pool', lambda: POOL.memset(ztmpP[q][:, 0:1], 0.0), writes=[('ztmp', q)])
            S.op('pool', lambda: POOL.memset(ztmpP[q][:, T + 1:T + 2], 0.0), reads=[('ztmp', q)], writes=[('ztmp', q)])
        for j in range(15):
            b = j % 2
            ztmp, sht = ztmpP[b], shtP[b]
            S.dma('pool', wg[b], win_d[:, j * 128:(j + 1) * 128].rearrange("(k p) n -> p k n", p=128), writes=[('wg', b)])
            for tb in range(NB):
                pz = ps[(j * NB + tb) % 4]
                pk = psk[(j * NB + tb) % 4]
                for k in range(8):
                    S.op('pe', lambda: PE.matmul(pz, lhsT=wg[b][:, k, :], rhs=uT[:, k, tb * 512:(tb + 1) * 512], start=(k == 0), stop=(k == 7)),
                         reads=[('wg', b), ('uT', tb)], writes=[pk], pe_acc=True)
                S.op('act', lambda: ACT.copy(out=ztmp[:, 1 + tb * 512:1 + (tb + 1) * 512], in_=pz), reads=[pk], writes=[('ztmp', b)])
            S.op('dve', lambda: V.tensor_scalar(out=sht, in0=ztmp[:, 1:T + 1], scalar1=alpha[:, j:j + 1], scalar2=None, op0=ALU.mult),
                 reads=[('ztmp', b), 'alpha'], writes=[('sht', b)])
            S.op('dve', lambda: V.scalar_tensor_tensor(out=sht, in0=ztmp[:, 0:T], scalar=col("mp", j), in1=sht, op0=ALU.mult, op1=ALU.add),
                 reads=[('ztmp', b), ('sht', b), 'pp'], writes=[('sht', b)])
            S.op('dve', lambda: V.scalar_tensor_tensor(out=zsT[:, j, :], in0=ztmp[:, 2:T + 2], scalar=col("mn", j), in1=sht, op0=ALU.mult, op1=ALU.add),
                 reads=[('ztmp', b), ('sht', b), 'pp'], writes=[('zs', j)])
            if j == 12:
                S.op('act', lambda: ACT.activation(out=zsT[:, j, :], in_=zsT[:, j, :], func=AF.Tanh), reads=[('zs', j)], writes=[('zs', j)])
            if j == 14:
                S.op('act', lambda: ACT.activation(out=zsT[:, j, :], in_=zsT[:, j, :], func=AF.Sigmoid), reads=[('zs', j)], writes=[('zs', j)])

    def phase_scan(zsT, kkT, base):
        rT = lambda c: zsT[:, c, :]
        kT = lambda c: zsT[:, 4 + c, :]
        vT = lambda c: zsT[:, 8 + c, :]
        wdT = zsT[:, 12, :]
        adT = zsT[:, 13, :]
        AR.seek(base)
        kraw = AR.alloc([128, 512], F32)
        ksq = AR.alloc([128, 512], BF16)
        krs = AR.alloc([128, 512], F32)
        for c in range(4):
            for tb in range(NB):
                sl = slice(tb * 512, (tb + 1) * 512)
                S.op('dve', lambda: V.tensor_scalar(out=kraw, in0=kT(c)[:, sl], scalar1=col("kk", c), scalar2=None, op0=ALU.mult), reads=[('zs', 4 + c), 'pp'], writes=['kraw'])
                S.op('act', lambda: ACT.activation(out=ksq, in_=kraw, func=AF.Square), reads=['kraw'], writes=['ksq'])
                pz, pk = ps[tb % 2], psk[tb % 2]
                S.op('pe', lambda: PE.matmul(pz, lhsT=cmb[:, BLK1, :], rhs=ksq, start=True, stop=True), reads=['ksq', 'cmb'], writes=[pk], pe_acc=True)
                S.op('act', lambda: ACT.activation(out=krs, in_=pz, func=AF.Sqrt, bias=epsc[:, 2:3], scale=1.0), reads=[pk, 'epsc'], writes=['krs'])
                S.op('dve', lambda: V.reciprocal(out=krs, in_=krs), reads=['krs'], writes=['krs'])
                S.op('dve', lambda: V.tensor_tensor(out=kkT[:, c, sl], in0=kraw, in1=krs, op=ALU.mult), reads=['kraw', 'krs'], writes=[('kk', c)])
        S.barrier()
        AR.seek(base)
        sg = AR.alloc([128, 4, TBS], F32)
        ad = AR.alloc([128, 4, TBS], F32)
        cc = AR.alloc([128, 4, TBS], F32)
        t1 = AR.alloc([128, 4, TBS], F32)
        ex = [[AR.alloc([128, TBS], F32) for _ in range(2)] for _ in range(4)]
        kd = AR.alloc([128, 4, TBS], F32)
        bb = AR.alloc([128, 4, TBS], F32)
        pdec = AR.alloc([128, 4, NCH], F32)
        ARz = AR.alloc([128, 4, NCH, 2, 2, 64], BF16)
        Bz = AR.alloc([128, 4, NCH, 2, 64], BF16)
        BKt = AR.alloc([128, 4, NCH, 2, 64], BF16)
        KBh = AR.alloc([128, 4, NCH, 2, 64], BF16)
        KBt = AR.alloc([128, 4, NCH, 128], BF16)
        VZ = AR.alloc([128, NCH, 8, 64], BF16)
        XV = AR.alloc([128, NCH, 8, 64], BF16)
        ZTs = [[AR.alloc([128, 4, 128], BF16) for _ in range(2)] for _ in range(NCH)]
        ATm = [[AR.alloc([128, 4, 128], BF16) for _ in range(2)] for _ in range(NCH)]
        PTm = [[AR.alloc([128, 4, 128], BF16) for _ in range(2)] for _ in range(NCH)]
        Pm = [[AR.alloc([128, 4, 128], BF16) for _ in range(2)] for _ in range(NCH)]
        Am = [[AR.alloc([128, 4, 128], BF16) for _ in range(2)] for _ in range(NCH)]
        W1s = AR.alloc([128, 4, 64], BF16)
        S32 = [AR.alloc([128, 4, 64], F32) for _ in range(2)]
        Sb = [AR.alloc([128, 4, 64], BF16) for _ in range(2)]
        ysb = [AR.alloc([64, 512], F32) for _ in range(2)]
        S.op('pool', lambda: POOL.memset(ARz.rearrange("p a b c d e -> p (a b c d e)"), 0.0), writes=['ARz'])
        S.op('pool', lambda: POOL.memset(Bz.rearrange("p a b c d -> p (a b c d)"), 0.0), writes=['Bz'])
        S.op('pool', lambda: POOL.memset(VZ.rearrange("p a b c -> p (a b c)"), 0.0), writes=['VZ'])

        def chain(gens):
            for g_ in gens:
                yield from g_

        def run_tasks(tasks):
            tasks = list(tasks)
            while tasks:
                for t_ in list(tasks):
                    try:
                        next(t_)
                    except StopIteration:
                        tasks.remove(t_)

        yev = 0
        pendQ = None
        for d in range(2):
            S.op('pool', lambda: POOL.memset(S32[d].rearrange("p a b -> p (a b)"), 0.0), writes=[('S32', d)])
            S.op('pool', lambda: POOL.memset(Sb[d].rearrange("p a b -> p (a b)"), 0.0), writes=[('Sb', d)])
            tbs = range(NTB) if d == 0 else range(NTB - 1, -1, -1)
            for tb in tbs:
                sl = slice(tb * TBS, (tb + 1) * TBS)
                def gen_prep(c):
                    pz, pk = ps[c % 2], psk[c % 2]
                    S.op('pe', lambda: PE.matmul(pz[:, 0:TBS], lhsT=w2c[:, d, c * 128:(c + 1) * 128], rhs=wdT[:, sl], start=True, stop=True),
                         reads=['w2c', ('zs', 12)], writes=[pk], pe_acc=True)
                    S.op('act', lambda: ACT.activation(out=sg[:, c, :], in_=pz[:, 0:TBS], func=AF.Sigmoid, bias=col("w0", d * 4 + c), scale=1.0),
                         reads=[pk, 'pp'], writes=[('sg', c)])
                    pz2, pk2 = ps[2 + c % 2], psk[2 + c % 2]
                    S.op('pe', lambda: PE.matmul(pz2[:, 0:TBS], lhsT=a2c[:, d, c * 128:(c + 1) * 128], rhs=adT[:, sl], start=True, stop=True),
                         reads=['a2c', ('zs', 13)], writes=[pk2], pe_acc=True)
                    S.op('act', lambda: ACT.activation(out=ad[:, c, :], in_=pz2[:, 0:TBS], func=AF.Sigmoid, bias=col("a0", d * 4 + c), scale=1.0),
                         reads=[pk2, 'pp'], writes=[('ad', c)])
                    yield
                    S.op('dve', lambda: V.tensor_tensor_scan(out=cc[:, c, :], data0=rmask, data1=sg[:, c, :], initial=0.0, op0=ALU.mult, op1=ALU.add),
                         reads=['rmask', ('sg', c)], writes=[('cc', c)])
                    cc3 = cc[:, c, :].rearrange("p (h t) -> p h t", t=64)
                    sg3 = sg[:, c, :].rearrange("p (h t) -> p h t", t=64)
                    t13 = t1[:, c, :].rearrange("p (h t) -> p h t", t=64)
                    if d == 1:
                        S.op('dve', lambda: V.tensor_tensor(out=t13, in0=bc(cc3[:, :, 63:64], [128, NCH, 64]), in1=cc3, op=ALU.subtract),
                             reads=[('cc', c)], writes=[('t1', c)])
                        S.op('dve', lambda: V.tensor_tensor(out=cc[:, c, :], in0=t1[:, c, :], in1=sg[:, c, :], op=ALU.add),
                             reads=[('t1', c), ('sg', c)], writes=[('cc', c)])
                    totp = 63 if d == 0 else 0
                    S.op('pool', lambda: POOL.tensor_scalar(out=kd[:, c, :], in0=ad[:, c, :], scalar1=col("ka", c), scalar2=oneminus_ka[:, c:c + 1], op0=ALU.mult, op1=ALU.add),
                         reads=[('ad', c), 'pp', 'omka'], writes=[('kd', c)])
                    S.op('pool', lambda: POOL.tensor_tensor(out=kd[:, c, :], in0=kd[:, c, :], in1=kT(c)[:, sl], op=ALU.mult),
                         reads=[('kd', c), ('zs', 4 + c)], writes=[('kd', c)])
                    S.op('pool', lambda: POOL.tensor_tensor(out=bb[:, c, :], in0=ad[:, c, :], in1=kkT[:, c, sl], op=ALU.mult),
                         reads=[('ad', c), ('kk', c)], writes=[('bb', c)])
                    yield
                    e = ex[c][0]
                    S.op('act', lambda: ACT.activation(out=e, in_=cc[:, c, :], func=AF.Exp, scale=-LAM), reads=[('cc', c)], writes=[('ex', c, 0)])
                    for hp in range(2):
                        pr = slice(hp * 64, (hp + 1) * 64)
                        S.op('dve', lambda: V.tensor_tensor(out=ARz[pr, c, :, 0, hp, :], in0=rT(c)[pr, sl].rearrange("p (h t) -> p h t", t=64),
                                                            in1=e[pr, :].rearrange("p (h t) -> p h t", t=64), op=ALU.mult),
                             reads=[('zs', c), ('ex', c, 0)], writes=['ARz'])
                    yield
                    e = ex[c][1]
                    S.op('act', lambda: ACT.activation(out=e, in_=cc[:, c, :], func=AF.Exp, scale=LAM), reads=[('cc', c)], writes=[('ex', c, 1)])
                    S.op('dve', lambda: V.tensor_tensor(out=BKt[:, c, :, 0, :], in0=kd[:, c, :].rearrange("p (h t) -> p h t", t=64),
                                                        in1=e.rearrange("p (h t) -> p h t", t=64), op=ALU.mult),
                         reads=[('kd', c), ('ex', c, 1)], writes=['BKt'])
                    S.op('dve', lambda: V.tensor_tensor(out=BKt[:, c, :, 1, :], in0=bb[:, c, :].rearrange("p (h t) -> p h t", t=64),
                                                        in1=e.rearrange("p (h t) -> p h t", t=64), op=ALU.mult),
                         reads=[('bb', c), ('ex', c, 1)], writes=['BKt'])
                    for hp in range(2):
                        pr = slice(hp * 64, (hp + 1) * 64)
                        S.op('act', lambda: ACT.copy(out=Bz[pr, c, :, hp, :], in_=BKt[pr, c, :, 1, :]), reads=['BKt'], writes=['Bz'])
                    yield
                    S.op('dve', lambda: V.tensor_tensor(out=t1[:, c, :], in0=cc[:, c, :], in1=sg[:, c, :], op=ALU.subtract),
                         reads=[('cc', c), ('sg', c)], writes=[('t1', c)])
                    e = ex[c][0]
                    S.op('act', lambda: ACT.activation(out=e, in_=t1[:, c, :], func=AF.Exp, scale=-LAM), reads=[('t1', c)], writes=[('ex', c, 0)])
                    for hp in range(2):
                        pr = slice(hp * 64, (hp + 1) * 64)
                        S.op('dve', lambda: V.scalar_tensor_tensor(out=ARz[pr, c, :, 1, hp, :], in0=kkT[pr, c, sl].rearrange("p (h t) -> p h t", t=64),
                                                                   scalar=-1.0, in1=e[pr, :].rearrange("p (h t) -> p h t", t=64), op0=ALU.mult, op1=ALU.mult),
                             reads=[('kk', c), ('ex', c, 0)], writes=['ARz'])
                    yield
                    S.op('dve', lambda: V.tensor_tensor(out=t13, in0=bc(cc3[:, :, totp:totp + 1], [128, NCH, 64]), in1=cc3, op=ALU.subtract),
                         reads=[('cc', c)], writes=[('t1', c)])
                    e = ex[c][1]
                    S.op('act', lambda: ACT.activation(out=e, in_=t1[:, c, :], func=AF.Exp, scale=-LAM), reads=[('t1', c)], writes=[('ex', c, 1)])
                    S.op('pool', lambda: POOL.tensor_tensor(out=KBh[:, c, :, 0, :], in0=kd[:, c, :].rearrange("p (h t) -> p h t", t=64),
                                                        in1=e.rearrange("p (h t) -> p h t", t=64), op=ALU.mult),
                         reads=[('kd', c), ('ex', c, 1)], writes=['KBh'])
                    S.op('pool', lambda: POOL.tensor_tensor(out=KBh[:, c, :, 1, :], in0=bb[:, c, :].rearrange("p (h t) -> p h t", t=64),
                                                        in1=e.rearrange("p (h t) -> p h t", t=64), op=ALU.mult),
                         reads=[('bb', c), ('ex', c, 1)], writes=['KBh'])
                    S.op('act', lambda: ACT.activation(out=pdec[:, c, :].rearrange("p (h o) -> p h o", o=1), in_=cc3[:, :, totp:totp + 1], func=AF.Exp, scale=-LAM), reads=[('cc', c)], writes=['pdec'])
                ptasks = [gen_prep(c_) for c_ in range(4)]
                for t_ in ptasks:
                    next(t_)
                if pendQ is not None:
                    for _ in range(3):
                        next(pendQ, None)
                for t_ in ptasks:
                    next(t_)
                if pendQ is not None:
                    run_tasks([pendQ])
                    pendQ = None
                run_tasks(ptasks)
                for ch in range(NCH):
                    pz = ps[4 + ch % 2].bitcast(BF16)
                    pk = psk[4 + ch % 2]
                    pzv = pz[0:64, 0:512].rearrange("p (c n) -> p c n", c=4)
                    for c in range(4):
                        S.op('pe', lambda: PE.transpose(out=pzv[:, c, :], in_=vT(c)[:, tb * TBS + ch * 64: tb * TBS + (ch + 1) * 64], identity=ident),
                             reads=[('zs', 8 + c), 'ident'], writes=[pk], pe_acc=True)
                    S.op('act', lambda: ACT.copy(out=VZ[0:64, ch, :, :].rearrange("p h v -> p (h v)"), in_=pz[0:64, 0:512]), reads=[pk], writes=[('VZ', ch)])
                    S.op('act', lambda: ACT.copy(out=XV[0:64, ch, :, :].rearrange("p h v -> p (h v)"), in_=pz[0:64, 0:512]), reads=[pk], writes=[('XVv', ch)])
                    pzk = pz[:, 512:1024].rearrange("p (c n) -> p c n", c=4)
                    for c in range(4):
                        S.op('pe', lambda: PE.transpose(out=pzk[:, c, :], in_=KBh[:, c, ch, :, :].rearrange("p a t -> p (a t)"), identity=ident),
                             reads=['KBh', 'ident'], writes=[pk], pe_acc=True)
                    S.op('dve', lambda: V.tensor_copy(out=KBt[:, :, ch, :], in_=pzk), reads=[pk], writes=[('KBt', ch)])
                MNT = cmb[:, 11 + d, :]
                MN = cmb[:, 12 - d, :]

                def gen_D(ch, slot, par):
                    pA, pkA = ps[2 * par], psk[2 * par]
                    pB, pkB = ps[2 * par + 1], psk[2 * par + 1]
                    pA3 = pA.rearrange("p (j n) -> p j n", j=4)
                    pB3 = pB.rearrange("p (j n) -> p j n", j=4)
                    mzt = cmb[:, MZT[d], :]
                    for half in range(2):
                        pz3 = pA3 if half == 0 else pB3
                        pkz = pkA if half == 0 else pkB
                        for j in range(4):
                            h = half * 4 + j
                            c, hp = h // 2, h % 2
                            bk = BKt[:, c, ch, :, :].rearrange("p a t -> p (a t)")
                            S.op('pe', lambda: PE.matmul(pz3[:, j, :].rearrange("p (a t) -> p a t", a=2), lhsT=bk, rhs=ARz[:, c, ch, :, hp, :], start=True, stop=True),
                                 reads=['BKt', 'ARz'], writes=[pkz], pe_acc=True)
                        S.op('dve', lambda: V.tensor_tensor(out=ZTs[slot][half], in0=pz3, in1=bc(mzt.rearrange("p (o n) -> p o n", o=1), [128, 4, 128]), op=ALU.mult),
                             reads=[pkz, 'cmb'], writes=[('ZTs', slot, half)])
                    yield
                    for c in range(4):
                        bz = Bz[:, c, ch, :, :].rearrange("p a t -> p (a t)")
                        az = ARz[:, c, ch, 1, :, :].rearrange("p a t -> p (a t)")
                        S.op('pe', lambda: PE.matmul(pA3[:, c, :], lhsT=bz, rhs=az, start=True, stop=True), reads=['Bz', 'ARz'], writes=[pkA], pe_acc=True)
                        S.op('pe', lambda: PE.matmul(pB3[:, c, :], lhsT=az, rhs=bz, start=True, stop=True), reads=['Bz', 'ARz'], writes=[pkB], pe_acc=True)
                    S.op('dve', lambda: V.tensor_tensor(out=PTm[par][0], in0=pA3, in1=bc(MNT.rearrange("p (o n) -> p o n", o=1), [128, 4, 128]), op=ALU.mult),
                         reads=[pkA, 'cmb'], writes=[('PT', par, 0)])
                    S.op('dve', lambda: V.tensor_tensor(out=Pm[par][0], in0=pB3, in1=bc(MN.rearrange("p (o n) -> p o n", o=1), [128, 4, 128]), op=ALU.mult),
                         reads=[pkB, 'cmb'], writes=[('P', par, 0)])
                    S.op('pool', lambda: POOL.tensor_tensor(out=ATm[slot][0], in0=PTm[par][0], in1=ident4, op=ALU.add), reads=[('PT', par, 0), 'ident4'], writes=[('AT', slot, 0)])
                    S.op('pool', lambda: POOL.tensor_tensor(out=Am[par][0], in0=Pm[par][0], in1=ident4, op=ALU.add), reads=[('P', par, 0), 'ident4'], writes=[('A', par, 0)])
                    yield
                    cur = 0
                    for lev in range(1, 6):
                        nxt = 1 - cur
                        for j in range(4):
                            S.op('pe', lambda: PE.matmul(pA3[:, j, :], lhsT=Pm[par][cur][:, j, :], rhs=PTm[par][cur][:, j, :], start=True, stop=True),
                                 reads=[('P', par, cur), ('PT', par, cur)], writes=[pkA], pe_acc=True)
                            if lev < 5:
                                S.op('pe', lambda: PE.matmul(pB3[:, j, :], lhsT=PTm[par][cur][:, j, :], rhs=Pm[par][cur][:, j, :], start=True, stop=True),
                                     reads=[('P', par, cur), ('PT', par, cur)], writes=[pkB], pe_acc=True)
                        S.op('act', lambda: ACT.copy(out=PTm[par][nxt], in_=pA3), reads=[pkA], writes=[('PT', par, nxt)])
                        if lev < 5:
                            S.op('dve', lambda: V.tensor_copy(out=Pm[par][nxt], in_=pB3), reads=[pkB], writes=[('P', par, nxt)])
                        yield
                        for j in range(4):
                            S.op('pe', lambda: PE.matmul(pA3[:, j, :], lhsT=Am[par][cur][:, j, :], rhs=PTm[par][nxt][:, j, :], start=True, stop=True),
                                 reads=[('A', par, cur), ('PT', par, nxt)], writes=[pkA], pe_acc=True)
                            if lev < 5:
                                S.op('pe', lambda: PE.matmul(pB3[:, j, :], lhsT=PTm[par][nxt][:, j, :], rhs=Am[par][cur][:, j, :], start=True, stop=True),
                                     reads=[('A', par, cur), ('PT', par, nxt)], writes=[pkB], pe_acc=True)
                        S.op('dve', lambda: V.tensor_tensor(out=ATm[slot][nxt], in0=pA3, in1=ATm[slot][cur], op=ALU.add), reads=[pkA, ('AT', slot, cur)], writes=[('AT', slot, nxt)])
                        if lev < 5:
                            S.op('act', lambda: ACT.copy(out=Am[par][nxt], in_=pB3), reads=[pkB], writes=[('A', par, nxt)])
                            S.op('pool', lambda: POOL.tensor_tensor(out=Am[par][nxt], in0=Am[par][nxt], in1=Am[par][cur], op=ALU.add),
                                 reads=[('A', par, nxt), ('A', par, cur)], writes=[('A', par, nxt)])
                        yield
                        cur = nxt
                    assert cur == 1

                def gen_Q(ch, slot, pb, tb=tb, d=d):
                    nonlocal yev
                    fin = 1
                    gch = tb * NCH + ch
                    pW, pkW = ps[pb], psk[pb]
                    pW3 = pW[:, 0:256].rearrange("p (c v) -> p c v", c=4)
                    for h in range(8):
                        c, hp = h // 2, h % 2
                        S.op('pe', lambda: PE.matmul(pW3[hp * 64:(hp + 1) * 64, c, :], lhsT=ZTs[slot][h // 4][:, h % 4, 64:128], rhs=VZ[:, ch, h, :], start=True, stop=False),
                             reads=[('ZTs', slot, h // 4), ('VZ', ch)], writes=[pkW], pe_acc=True)
                        S.op('pe', lambda: PE.matmul(pW3[hp * 64:(hp + 1) * 64, c, :], lhsT=ARz[:, c, ch, 1, hp, :], rhs=Sb[d][:, c, :], start=False, stop=True),
                             reads=['ARz', ('Sb', d)], writes=[pkW], pe_acc=True)
                    S.op('act', lambda: ACT.copy(out=W1s, in_=pW3), reads=[pkW], writes=['W1s'])
                    yield
                    pX, pkX = ps[pb + 1], psk[pb + 1]
                    pX3 = pX.rearrange("p (h v) -> p h v", h=8)
                    for h in range(8):
                        c, hp = h // 2, h % 2
                        S.op('pe', lambda: PE.matmul(pX3[64:128, h, :], lhsT=ATm[slot][fin][:, c, hp * 64:(hp + 1) * 64], rhs=W1s[:, c, :], start=True, stop=True),
                             reads=[('AT', slot, fin), 'W1s'], writes=[pkX], pe_acc=True)
                    S.op('dve', lambda: V.tensor_copy(out=XV[64:128, ch, :, :], in_=pX3[64:128]), reads=[pkX], writes=[('XVu', ch)])
                    yield
                    pS, pkS = ps[pb + 3], psk[pb + 3]
                    pS3 = pS[:, 0:256].rearrange("p (c v) -> p c v", c=4)
                    for h in range(8):
                        c, hp = h // 2, h % 2
                        S.op('pe', lambda: PE.matmul(pS3[hp * 64:(hp + 1) * 64, c, :], lhsT=KBt[:, c, ch, hp * 64:(hp + 1) * 64], rhs=XV[:, ch, h, :], start=True, stop=True),
                             reads=[('KBt', ch), ('XVv', ch), ('XVu', ch)], writes=[pkS], pe_acc=True)
                    pY, pkY = ps[pb + 2], psk[pb + 2]
                    pY3 = pY.rearrange("p (h v) -> p h v", h=8)
                    for h in range(8):
                        c, hp = h // 2, h % 2
                        S.op('pe', lambda: PE.matmul(pY3[0:64, h, :], lhsT=ZTs[slot][h // 4][:, h % 4, 0:64], rhs=XV[:, ch, h, :], start=True, stop=False),
                             reads=[('ZTs', slot, h // 4), ('XVv', ch), ('XVu', ch)], writes=[pkY], pe_acc=True)
                        S.op('pe', lambda: PE.matmul(pY3[0:64, h, :], lhsT=ARz[:, c, ch, 0, hp, :], rhs=Sb[d][:, c, :], start=False, stop=True),
                             reads=['ARz', ('Sb', d)], writes=[pkY], pe_acc=True)
                    for c in range(4):
                        S.op('dve', lambda: V.scalar_tensor_tensor(out=S32[d][:, c, :], in0=S32[d][:, c, :], scalar=pdec[:, c, ch:ch + 1], in1=pS3[:, c, :], op0=ALU.mult, op1=ALU.add),
                             reads=[('S32', d), 'pdec', pkS], writes=[('S32', d)])
                    S.op('act', lambda: ACT.copy(out=Sb[d], in_=S32[d]), reads=[('S32', d)], writes=[('Sb', d)])
                    yb_ = ysb[yev % 2]
                    S.op('act', lambda: ACT.copy(out=yb_, in_=pY[0:64, :]), reads=[pkY], writes=[('ysb', yev % 2)])
                    S.dma('sp', y_d[d, gch * 64:(gch + 1) * 64, :], yb_, reads=[('ysb', yev % 2)], writes=[('yscr', d, gch // 2)])
                    yev += 1
                    yield

                chs = list(range(NCH)) if d == 0 else list(range(NCH - 1, -1, -1))
                Ds = [gen_D(chs[i_], i_, i_) for i_ in range(NCH)]
                fast, slow = Ds[0:2], Ds[2:4]
                alive = True
                while alive:
                    alive = False
                    for rep in range(2):
                        for t_ in fast:
                            if next(t_, 'done') != 'done':
                                alive = True
                    for t_ in slow:
                        next(t_, 'done')
                run_tasks([chain([gen_Q(chs[0], 0, 0), gen_Q(chs[1], 1, 0)])] + slow)
                pendQ = chain([gen_Q(chs[2], 2, 4), gen_Q(chs[3], 3, 4)])
        if pendQ is not None:
            run_tasks([pendQ])
            pendQ = None
        S.barrier()


    def phase_post(zsT, yaT, base):
        rT4 = zsT[:, 0:4, :]
        kT4 = zsT[:, 4:8, :]
        adT = zsT[:, 13, :]
        gdT = zsT[:, 14, :]
        AR.seek(base)
        gnwB = AR.alloc([128, 512], F32)
        gnbB = AR.alloc([128, 512], F32)
        P2 = lambda shape, dt: [AR.alloc(shape, dt) for _ in range(2)]
        Yf, Yb = P2([128, 512], F32), P2([128, 512], F32)
        ta0, ta1 = P2([128, 4, 128], F32), P2([128, 4, 128], F32)
        kf2 = P2([128, 4, 128], F32)
        prod2 = P2([128, 4, 128], BF16)
        rows2 = P2([128, 8], F32)
        bon2 = P2([128, 512], F32)
        y2 = P2([128, 512], F32)
        sq2 = P2([128, 512], F32)
        st2 = P2([128, 4, 8], F32)
        yab2 = P2([128, 512], BF16)
        S.dma('sp', gnwB, gnw_d.partition_broadcast(128), writes=['gnwB'])
        S.dma('sp', gnbB, gnb_d.partition_broadcast(128), writes=['gnbB'])
        def gen_tile(i):
            b = i % 2
            sl = slice(i * 128, (i + 1) * 128)
            ta = (ta0[b], ta1[b])
            kf, prod, rows, bon, y, sq, st, yab = kf2[b], prod2[b], rows2[b], bon2[b], y2[b], sq2[b], st2[b], yab2[b]
            bA, bB, bC, bD = 4 * b, 4 * b + 1, 4 * b + 2, 4 * b + 3
            S.dma('sp', Yf[b], y_d[0, sl, :], writes=[('Yf', b)])
            S.dma('sp', Yb[b], y_d[1, sl, :], writes=[('Yb', b)])
            for d in range(2):
                bk_ = bA if d == 0 else bB
                pz3 = ps[bk_].rearrange("p (c n) -> p c n", c=4)
                for c in range(4):
                    S.op('pe', lambda: PE.matmul(pz3[:, c, :], lhsT=a2c[:, d, c * 128:(c + 1) * 128], rhs=adT[:, sl], start=True, stop=True),
                         reads=['a2c'], writes=[psk[bk_]], pe_acc=True)
                for c in range(4):
                    S.op('act', lambda: ACT.activation(out=ta[d][:, c, :], in_=pz3[:, c, :], func=AF.Sigmoid, bias=col("a0", d * 4 + c), scale=1.0),
                         reads=[psk[bk_], 'pp'], writes=[('ta', b, d)])
            yield
            S.op('pool', lambda: POOL.tensor_tensor(out=ta[0], in0=ta[0], in1=ta[1], op=ALU.add), reads=[('ta', b, 0), ('ta', b, 1)], writes=[('ta', b, 0)])
            for c in range(4):
                S.op('pool', lambda: POOL.tensor_scalar(out=kf[:, c, :], in0=ta[0][:, c, :], scalar1=kar[:, c:c + 1], scalar2=c2r[:, c:c + 1], op0=ALU.mult, op1=ALU.add),
                     reads=[('ta', b, 0), 'kar'], writes=[('kf', b)])
            S.op('pool', lambda: POOL.tensor_tensor(out=kf, in0=kf, in1=kT4[:, :, sl], op=ALU.mult), reads=[('kf', b)], writes=[('kf', b)])
            S.op('pool', lambda: POOL.tensor_tensor(out=prod, in0=kf, in1=rT4[:, :, sl], op=ALU.mult), reads=[('kf', b)], writes=[('prod', b)])
            yield
            pr = ps[bB]
            for c in range(4):
                S.op('pe', lambda: PE.matmul(pr[:, c * 2:(c + 1) * 2], lhsT=prod[:, c, :], rhs=cmb[:, HSEL, 0:2], start=True, stop=True),
                     reads=[('prod', b), 'cmb'], writes=[psk[bB]], pe_acc=True)
            S.op('act', lambda: ACT.copy(out=rows, in_=pr[:, 0:8]), reads=[psk[bB]], writes=[('rows', b)])
            yield
            pv = ps[bD].bitcast(BF16)[:, 0:512]
            for c in range(4):
                S.op('pe', lambda: PE.transpose(out=pv[:, c * 128:(c + 1) * 128], in_=zsT[:, 8 + c, sl], identity=ident),
                     reads=['ident'], writes=[psk[bD]], pe_acc=True)
            S.op('dve', lambda: V.tensor_tensor(out=bon.rearrange("p (h v) -> p h v", h=8), in0=pv.rearrange("p (h v) -> p h v", h=8),
                                                in1=bc(rows.rearrange("p (h o) -> p h o", o=1), [128, 8, 64]), op=ALU.mult),
                 reads=[psk[bD], ('rows', b)], writes=[('bon', b)])
            yield
            pg = ps[bC]
            S.op('pe', lambda: PE.matmul(pg, lhsT=gdT[:, sl], rhs=g2, start=True, stop=True), reads=['g2'], writes=[psk[bC]], pe_acc=True)
            y3 = y.rearrange("p (h v) -> p h v", h=8)
            sq3 = sq.rearrange("p (h v) -> p h v", h=8)
            S.op('dve', lambda: V.tensor_tensor(out=y, in0=Yf[b], in1=Yb[b], op=ALU.add), reads=[('Yf', b), ('Yb', b)], writes=[('y', b)])
            yield
            S.op('dve', lambda: V.tensor_reduce(out=st[:, 0, :], in_=y3, axis=AX.X, op=ALU.add), reads=[('y', b)], writes=[('st0', b)])
            S.op('dve', lambda: V.tensor_scalar(out=st[:, 1, :], in0=st[:, 0, :], scalar1=-1.0 / 64, scalar2=None, op0=ALU.mult), reads=[('st0', b)], writes=[('st1', b)])
            S.op('dve', lambda: V.tensor_tensor(out=y3, in0=y3, in1=bc(st[:, 1, :].rearrange("p (h o) -> p h o", o=1), [128, 8, 64]), op=ALU.add),
                 reads=[('y', b), ('st1', b)], writes=[('y', b)])
            yield
            S.op('act', lambda: ACT.activation(out=sq, in_=y, func=AF.Square), reads=[('y', b)], writes=[('sq', b)])
            S.op('dve', lambda: V.tensor_reduce(out=st[:, 2, :], in_=sq3, axis=AX.X, op=ALU.add), reads=[('sq', b)], writes=[('st2', b)])
            yield
            S.op('act', lambda: ACT.activation(out=st[:, 3, :], in_=st[:, 2, :], func=AF.Sqrt, bias=epsc[:, 1:2], scale=1.0 / 64), reads=[('st2', b), 'epsc'], writes=[('st3', b)])
            S.op('dve', lambda: V.reciprocal(out=st[:, 3, :], in_=st[:, 3, :]), reads=[('st3', b)], writes=[('st3', b)])
            S.op('dve', lambda: V.tensor_tensor(out=y3, in0=y3, in1=bc(st[:, 3, :].rearrange("p (h o) -> p h o", o=1), [128, 8, 64]), op=ALU.mult),
                 reads=[('y', b), ('st3', b)], writes=[('y', b)])
            yield
            S.op('dve', lambda: V.tensor_tensor(out=y, in0=y, in1=gnwB, op=ALU.mult), reads=[('y', b), 'gnwB'], writes=[('y', b)])
            S.op('pool', lambda: POOL.tensor_tensor(out=bon, in0=bon, in1=gnbB, op=ALU.add), reads=[('bon', b), 'gnbB'], writes=[('bon', b)])
            S.op('dve', lambda: V.tensor_tensor(out=y, in0=y, in1=bon, op=ALU.add), reads=[('y', b), ('bon', b)], writes=[('y', b)])
            S.op('dve', lambda: V.tensor_tensor(out=yab, in0=y, in1=pg, op=ALU.mult), reads=[('y', b), psk[bC]], writes=[('yab', b)])
            yield
            pt = ps[bA].bitcast(BF16)[:, 0:512]
            for c in range(4):
                S.op('pe', lambda: PE.transpose(out=pt[:, c * 128:(c + 1) * 128], in_=yab[:, c * 128:(c + 1) * 128], identity=ident),
                     reads=[('yab', b), 'ident'], writes=[psk[bA]], pe_acc=True)
            S.op('act', lambda: ACT.copy(out=yaT[:, :, sl], in_=pt.rearrange("p (c n) -> p c n", c=4)), reads=[psk[bA]], writes=['yaT'])
            yield

        def run_tasks(tasks):
            tasks = list(tasks)
            while tasks:
                for t_ in list(tasks):
                    try:
                        next(t_)
                    except StopIteration:
                        tasks.remove(t_)

        for i in range(0, NT, 2):
            run_tasks([gen_tile(i), gen_tile(i + 1)])

    def phase_attn(s, uT, ybT, baseA, baseB):
        AR.seek(baseA)
        cosT = AR.alloc([128, T], F32)
        sinT = AR.alloc([128, T], F32)
        qT = AR.alloc([128, 4, T], BF16)
        kTt = AR.alloc([128, T], BF16)
        vp = AR.alloc([128, 2, NT, 128], BF16)
        AR.seek(baseB)
        wq = [AR.alloc([128, 8, 128], BF16) for _ in range(2)]
        qfL = [AR.alloc([128, 512], F32) for _ in range(2)]
        sqbL = [AR.alloc([128, 512], BF16) for _ in range(2)]
        rsL = [AR.alloc([128, 512], F32) for _ in range(2)]
        qnL = [AR.alloc([128, 512], F32) for _ in range(2)]
        qnbL = [AR.alloc([128, 512], BF16) for _ in range(2)]
        t1L = [AR.alloc([128, 512], F32) for _ in range(2)]
        t2L = [AR.alloc([128, 512], F32) for _ in range(2)]
        pTs = [AR.alloc([128, 512], BF16) for _ in range(6)]
        dn = AR.alloc([128, 512], F32)
        posi = AR.alloc([128, T], I32)
        ang = AR.alloc([128, T], F32)
        ki = AR.alloc([128, T], I32)
        kf = AR.alloc([128, T], F32)
        m1 = AR.alloc([128, T], F32)
        S.dma('sp', posi, pos_d[s].partition_broadcast(128), writes=['posi'])

        def table(dst, shift):
            S.op('dve', lambda: V.tensor_copy(out=ang, in_=posi), reads=['posi'], writes=['ang'])
            S.op('dve', lambda: V.tensor_scalar(out=ang, in0=ang, scalar1=col("invf"), scalar2=shift, op0=ALU.mult, op1=ALU.add), reads=['ang', 'pp'], writes=['ang'])
            S.op('dve', lambda: V.tensor_scalar(out=ki, in0=ang, scalar1=1.0 / TWO_PI, scalar2=None, op0=ALU.mult), reads=['ang'], writes=['ki'])
            S.op('pool', lambda: POOL.tensor_copy(out=kf, in_=ki), reads=['ki'], writes=['kf'])
            S.op('dve', lambda: V.scalar_tensor_tensor(out=ang, in0=kf, scalar=-C1, in1=ang, op0=ALU.mult, op1=ALU.add), reads=['kf', 'ang'], writes=['ang'])
            S.op('dve', lambda: V.scalar_tensor_tensor(out=ang, in0=kf, scalar=-C2, in1=ang, op0=ALU.mult, op1=ALU.add), reads=['kf', 'ang'], writes=['ang'])
            S.op('dve', lambda: V.tensor_scalar(out=m1, in0=ang, scalar1=float(np.pi), scalar2=-TWO_PI, op0=ALU.is_gt, op1=ALU.mult), reads=['ang'], writes=['m1'])
            S.op('pool', lambda: POOL.tensor_tensor(out=ang, in0=ang, in1=m1, op=ALU.add), reads=['ang', 'm1'], writes=['ang'])
            S.op('dve', lambda: V.tensor_scalar(out=m1, in0=ang, scalar1=float(-np.pi), scalar2=TWO_PI, op0=ALU.is_lt, op1=ALU.mult), reads=['ang'], writes=['m1'])
            S.op('pool', lambda: POOL.tensor_tensor(out=ang, in0=ang, in1=m1, op=ALU.add), reads=['ang', 'm1'], writes=['ang'])
            S.op('act', lambda: ACT.activation(out=dst, in_=ang, func=AF.Sin), reads=['ang'], writes=['tab'])

        table(sinT, 0.0)
        table(cosT, float(np.pi / 2))
        def c0_of(c):
            return 1920 + c * 128 if c < 4 else 2432

        def gen_qk(c, tb, L):
            b = c % 2
            gcol = col("qg") if c < 4 else col("kg")
            sl = slice(tb * 512, (tb + 1) * 512)
            qf_, sqb_, rs_, qn_, qnb_, t1_, t2_ = qfL[L], sqbL[L], rsL[L], qnL[L], qnbL[L], t1L[L], t2L[L]
            pz, pk = ps[L], psk[L]
            for k in range(8):
                S.op('pe', lambda: PE.matmul(pz, lhsT=wq[b][:, k, :], rhs=uT[:, k, sl], start=(k == 0), stop=(k == 7)),
                     reads=[('wq', b), ('uT', tb)], writes=[pk], pe_acc=True)
            S.op('act', lambda: ACT.copy(out=qf_, in_=pz), reads=[pk], writes=[('qf', L)])
            S.op('act', lambda: ACT.activation(out=sqb_, in_=qf_, func=AF.Square), reads=[('qf', L)], writes=[('sqb', L)])
            yield
            pr, pkr = ps[2 + L], psk[2 + L]
            S.op('pe', lambda: PE.matmul(pr, lhsT=cmb[:, BLK1, :], rhs=sqb_, start=True, stop=True), reads=[('sqb', L), 'cmb'], writes=[pkr], pe_acc=True)
            S.op('act', lambda: ACT.activation(out=rs_, in_=pr, func=AF.Sqrt, bias=epsc[:, 0:1], scale=1.0 / 64), reads=[pkr, 'epsc'], writes=[('rs', L)])
            yield
            S.op('dve', lambda: V.reciprocal(out=rs_, in_=rs_), reads=[('rs', L)], writes=[('rs', L)])
            S.op('dve', lambda: V.scalar_tensor_tensor(out=qn_, in0=qf_, scalar=gcol, in1=rs_, op0=ALU.mult, op1=ALU.mult), reads=[('qf', L), ('rs', L), 'pp'], writes=[('qn', L)])
            S.op('act', lambda: ACT.copy(out=qnb_, in_=qn_), reads=[('qn', L)], writes=[('qnb', L)])
            yield
            pro, pkro = ps[4 + L], psk[4 + L]
            S.op('pe', lambda: PE.matmul(pro, lhsT=cmb[:, ROT, :], rhs=qnb_, start=True, stop=True), reads=[('qnb', L), 'cmb'], writes=[pkro], pe_acc=True)
            S.op('pool', lambda: POOL.tensor_tensor(out=t1_, in0=qn_, in1=cosT[:, sl], op=ALU.mult), reads=[('qn', L), 'tab'], writes=[('t1', L)])
            yield
            S.op('dve', lambda: V.tensor_tensor(out=t2_, in0=pro, in1=sinT[:, sl], op=ALU.mult), reads=[pkro, 'tab'], writes=[('t2', L)])
            dst = qT[:, c, sl] if c < 4 else kTt[:, sl]
            S.op('dve', lambda: V.tensor_tensor(out=dst, in0=t1_, in1=t2_, op=ALU.add), reads=[('t1', L), ('t2', L)], writes=['qk'])
            yield

        def run_tasks(tasks):
            tasks = list(tasks)
            while tasks:
                for t_ in list(tasks):
                    try:
                        next(t_)
                    except StopIteration:
                        tasks.remove(t_)

        S.dma('pool', wq[0], win_d[:, c0_of(0):c0_of(0) + 128].rearrange("(k p) n -> p k n", p=128), writes=[('wq', 0)])
        for c in range(5):
            if c + 1 < 5:
                S.dma('pool', wq[(c + 1) % 2], win_d[:, c0_of(c + 1):c0_of(c + 1) + 128].rearrange("(k p) n -> p k n", p=128), writes=[('wq', (c + 1) % 2)])
            for tb in range(0, NB, 2):
                run_tasks([gen_qk(c, tb, 0), gen_qk(c, tb + 1, 1)])
        S.op('pool', lambda: POOL.memset(vp.rearrange("p a b c -> p (a b c)"), 0.0), writes=['vp'])
        S.dma('pool', wq[0], win_d[:, 2560:2688].rearrange("(k p) n -> p k n", p=128), writes=[('wq', 0)])
        for i in range(NT):
            pz, pk = ps[i % 2], psk[i % 2]
            for k in range(8):
                S.op('pe', lambda: PE.matmul(pz[:, 0:128], lhsT=uT[:, k, i * 128:(i + 1) * 128], rhs=wq[0][:, k, :], start=(k == 0), stop=(k == 7)),
                     reads=[('wq', 0), ('uT', i // 4)], writes=[pk], pe_acc=True)
            S.op('act', lambda: ACT.copy(out=vp[:, 0, i, 0:64], in_=pz[:, 0:64]), reads=[pk], writes=['vp'])
            S.op('dve', lambda: V.tensor_copy(out=vp[:, 1, i, 64:128], in_=pz[:, 64:128]), reads=[pk], writes=['vp'])
        for n in range(NT):
            qs = slice(n * 128, (n + 1) * 128)
            kbs = [kb for kb in (n - 1, n, n + 1) if 0 <= kb < NT]
            items = [(g, kb) for g in range(2) for kb in kbs]
            for idx, (g, kb) in enumerate(items):
                gp = slice(g * 64, (g + 1) * 64)
                pz, pk = ps[idx % 4], psk[idx % 4]
                S.op('pe', lambda: PE.matmul(pz.rearrange("p (j q) -> p j q", j=4), lhsT=kTt[gp, kb * 128:(kb + 1) * 128], rhs=qT[gp, :, qs], start=True, stop=True),
                     reads=['qk'], writes=[pk], pe_acc=True)
                pt_ = pTs[idx]
                S.op('act', lambda: ACT.activation(out=pt_, in_=pz, func=AF.Exp, scale=0.125), reads=[pk], writes=[('pT', idx)])
                if kb != n:
                    mk = cmb[:, MPREV if kb < n else MNEXT, :]
                    S.op('pool', lambda: POOL.tensor_tensor(out=pt_.rearrange("p (j q) -> p j q", j=4), in0=pt_.rearrange("p (j q) -> p j q", j=4),
                                                            in1=bc(mk.rearrange("p (o q) -> p o q", o=1), [128, 4, 128]), op=ALU.mult),
                         reads=[('pT', idx), 'cmb'], writes=[('pT', idx)])
            po, pko = ps[4 + n % 2], psk[4 + n % 2]
            pd_, pkd = ps[6 + n % 2], psk[6 + n % 2]
            for idx, (g, kb) in enumerate(items):
                S.op('pe', lambda: PE.matmul(po, lhsT=vp[:, g, kb, :], rhs=pTs[idx], start=(idx == 0), stop=(idx == len(items) - 1)),
                     reads=['vp', ('pT', idx)], writes=[pko], pe_acc=True)
            for idx, (g, kb) in enumerate(items):
                S.op('pe', lambda: PE.matmul(pd_, lhsT=cmb[:, VP[g], :], rhs=pTs[idx], start=(idx == 0), stop=(idx == len(items) - 1)),
                     reads=['cmb', ('pT', idx)], writes=[pkd], pe_acc=True)
            S.op('dve', lambda: V.tensor_tensor(out=dn.rearrange("p (j q) -> p j q", j=4), in0=pd_.rearrange("p (j q) -> p j q", j=4),
                                                in1=bc(esk.rearrange("p (j o) -> p j o", o=1), [128, 4, 128]), op=ALU.add), reads=[pkd, 'esk'], writes=['dn'])
            S.op('dve', lambda: V.reciprocal(out=dn, in_=dn), reads=['dn'], writes=['dn'])
            S.op('dve', lambda: V.tensor_tensor(out=ybT[:, :, qs], in0=po.rearrange("p (j q) -> p j q", j=4), in1=dn.rearrange("p (j q) -> p j q", j=4), op=ALU.mult),
                 reads=[pko, 'dn'], writes=['ybT'])

    def phase_merge(uT, yaT, ybT, mergedT, offs):
        AR.seek(offs[0])
        prw = AR.alloc([128, 4, D], BF16)
        AR.seek(offs[1])
        pat = AR.alloc([128, 4, D], BF16)
        wga = [AR.alloc([128, 8, 128], BF16) for _ in range(2)]
        wgb = [AR.alloc([128, 8, 128], BF16) for _ in range(2)]
        sgaP = [AR.alloc([128, 512], BF16) for _ in range(2)]
        sgbP = [AR.alloc([128, 512], BF16) for _ in range(2)]
        t1P = [AR.alloc([128, 512], F32) for _ in range(2)]
        t2P = [AR.alloc([128, 512], F32) for _ in range(2)]
        for hh in range(2):
            S.dma('pool', prw[:, hh * 2:(hh + 1) * 2, :], prw_d[hh * 256:(hh + 1) * 256, :].rearrange("(k p) n -> p k n", p=128), writes=['prw'])
            S.dma('pool', pat[:, hh * 2:(hh + 1) * 2, :], pat_d[hh * 256:(hh + 1) * 256, :].rearrange("(k p) n -> p k n", p=128), writes=['pat'])
        for oc in range(8):
            b = oc % 2
            S.dma('pool', wga[b], win_d[:, 2688 + oc * 128:2688 + (oc + 1) * 128].rearrange("(k p) n -> p k n", p=128), writes=[('wga', b)])
            S.dma('pool', wgb[b], win_d[:, 3712 + oc * 128:3712 + (oc + 1) * 128].rearrange("(k p) n -> p k n", p=128), writes=[('wgb', b)])
            for tb in range(NB):
                sl = slice(tb * 512, (tb + 1) * 512)
                L = tb % 2
                sga, sgb, t1, t2 = sgaP[L], sgbP[L], t1P[L], t2P[L]
                for k in range(8):
                    S.op('pe', lambda: PE.matmul(ps[0 + 4 * L], lhsT=wga[b][:, k, :], rhs=uT[:, k, sl], start=(k == 0), stop=(k == 7)),
                         reads=[('wga', b), ('uT', tb)], writes=[psk[0 + 4 * L]], pe_acc=True)
                S.op('act', lambda: ACT.activation(out=sga, in_=ps[0 + 4 * L], func=AF.Sigmoid), reads=[psk[0 + 4 * L]], writes=[('sga', L)])
                for k in range(8):
                    S.op('pe', lambda: PE.matmul(ps[1 + 4 * L], lhsT=wgb[b][:, k, :], rhs=uT[:, k, sl], start=(k == 0), stop=(k == 7)),
                         reads=[('wgb', b), ('uT', tb)], writes=[psk[1 + 4 * L]], pe_acc=True)
                S.op('act', lambda: ACT.activation(out=sgb, in_=ps[1 + 4 * L], func=AF.Sigmoid), reads=[psk[1 + 4 * L]], writes=[('sgb', L)])
                for k in range(4):
                    S.op('pe', lambda: PE.matmul(ps[2 + 4 * L], lhsT=prw[:, k, oc * 128:(oc + 1) * 128], rhs=yaT[:, k, sl], start=(k == 0), stop=(k == 3)),
                         reads=['prw', 'yaT'], writes=[psk[2 + 4 * L]], pe_acc=True)
                for k in range(4):
                    S.op('pe', lambda: PE.matmul(ps[3 + 4 * L], lhsT=pat[:, k, oc * 128:(oc + 1) * 128], rhs=ybT[:, k, sl], start=(k == 0), stop=(k == 3)),
                         reads=['pat', 'ybT'], writes=[psk[3 + 4 * L]], pe_acc=True)
                S.op('dve', lambda: V.tensor_tensor(out=t1, in0=ps[2 + 4 * L], in1=sga, op=ALU.mult), reads=[psk[2 + 4 * L], ('sga', L)], writes=[('t1', L)])
                S.op('dve', lambda: V.tensor_tensor(out=t2, in0=ps[3 + 4 * L], in1=sgb, op=ALU.mult), reads=[psk[3 + 4 * L], ('sgb', L)], writes=[('t2', L)])
                S.op('pool', lambda: POOL.tensor_tensor(out=mergedT[:, oc, sl], in0=t1, in1=t2, op=ALU.add), reads=[('t1', L), ('t2', L)], writes=[('mg', tb)])

    def phase_x1(s, mergedT, u2tm, base):
        AR.seek(base)
        wo = AR.alloc([128, 8, D], BF16)
        gt1B = AR.alloc([128, D], F32)
        sc2 = AR.alloc([128, D], F32)
        sh2 = AR.alloc([128, D], F32)
        xt = [AR.alloc([128, D], F32) for _ in range(2)]
        x1t = [AR.alloc([128, D], F32) for _ in range(2)]
        tmpP = [AR.alloc([128, D], F32) for _ in range(2)]
        junkP = [AR.alloc([128, D], BF16) for _ in range(2)]
        u2TP = [AR.alloc([128, 8, 128], BF16) for _ in range(2)]
        ssP = [AR.alloc([128, 8], F32) for _ in range(2)]
        exP = [AR.alloc([128, E], F32) for _ in range(2)]
        for hh in range(4):
            S.dma('pool', wo[:, hh * 2:(hh + 1) * 2, :], wout_d[hh * 256:(hh + 1) * 256, :].rearrange("(k p) n -> p k n", p=128), writes=['wo'])
        S.dma('sp', gt1B, mod_d[s, 2], writes=['gt1B'])
        S.dma('sp', sc2, mod_d[s, 4], writes=['sc2'])
        S.dma('sp', sh2, mod_d[s, 3], writes=['sh2'])
        lg = AR.alloc([128, NT, E], F32)
        mxs = AR.alloc([128, 3, NT], F32)

        def gen_x1(i):
            b = i % 2
            sl = slice(i * 128, (i + 1) * 128)
            S.dma('sp', xt[b], x_d[s, sl, :], writes=[('xt', b)])
            tmp, junk, u2T, ss = tmpP[b], junkP[b], u2TP[b], ssP[b]
            for cb in range(2):
                for k in range(8):
                    S.op('pe', lambda: PE.matmul(ps[cb + 6 * b], lhsT=mergedT[:, k, sl], rhs=wo[:, k, cb * 512:(cb + 1) * 512], start=(k == 0), stop=(k == 7)),
                         reads=[('mg', i // 4), 'wo'], writes=[psk[cb + 6 * b]], pe_acc=True)
                S.op('dve', lambda: V.tensor_tensor(out=tmp[:, cb * 512:(cb + 1) * 512], in0=ps[cb + 6 * b], in1=gt1B[:, cb * 512:(cb + 1) * 512], op=ALU.mult),
                     reads=[psk[cb + 6 * b], 'gt1B'], writes=[('tmp', b, cb)])
            yield
            S.op('pool', lambda: POOL.tensor_tensor(out=x1t[b], in0=tmp, in1=xt[b], op=ALU.add), reads=[('tmp', b, 0), ('tmp', b, 1), ('xt', b)], writes=[('x1t', b)])
            S.dma('sp', out_d[s, sl, :], x1t[b], reads=[('x1t', b)], writes=[('outd', i)])
            S.op('act', lambda: ACT.activation(out=junk, in_=x1t[b], func=AF.Square, accum_out=ss[:, 0:1]), reads=[('x1t', b)], writes=[('junk', b), ('ss0', b)])
            yield
            S.op('act', lambda: ACT.activation(out=ss[:, 1:2], in_=ss[:, 0:1], func=AF.Sqrt, bias=epsc[:, 0:1], scale=1.0 / D), reads=[('ss0', b), 'epsc'], writes=[('ss1', b)])
            S.op('dve', lambda: V.reciprocal(out=ss[:, 1:2], in_=ss[:, 1:2]), reads=[('ss1', b)], writes=[('ss1', b)])
            S.op('dve', lambda: V.scalar_tensor_tensor(out=tmp, in0=x1t[b], scalar=ss[:, 1:2], in1=sc2, op0=ALU.mult, op1=ALU.mult),
                 reads=[('x1t', b), ('ss1', b), 'sc2'], writes=[('tmp', b, 0), ('tmp', b, 1)])
            yield
            S.op('pool', lambda: POOL.tensor_tensor(out=u2tm[:, i, :], in0=tmp, in1=sh2, op=ALU.add), reads=[('tmp', b, 0), ('tmp', b, 1), 'sh2'], writes=[('u2', i)])
            pz = ps[2 + b].bitcast(BF16).rearrange("p (k t) -> p k t", k=8)
            pk = psk[2 + b]
            for k in range(8):
                S.op('pe', lambda: PE.transpose(out=pz[:, k, :], in_=u2tm[:, i, k * 128:(k + 1) * 128], identity=ident),
                     reads=[('u2', i), 'ident'], writes=[pk], pe_acc=True)
            yield
            S.op('act', lambda: ACT.copy(out=u2T, in_=pz), reads=[pk], writes=[('u2T', b)])
            pl, pkl = ps[4 + b], psk[4 + b]
            for k in range(8):
                S.op('pe', lambda: PE.matmul(pl[:, 0:E], lhsT=u2T[:, k, :], rhs=wr[:, k, :], start=(k == 0), stop=(k == 7)),
                     reads=[('u2T', b), 'wr'], writes=[pkl], pe_acc=True)
            yield
            S.op('act', lambda: ACT.copy(out=lg[:, i, :], in_=pl[:, 0:E]), reads=[pkl], writes=['lg'])
            yield

        def run_tasks(tasks):
            tasks = list(tasks)
            while tasks:
                for t_ in list(tasks):
                    try:
                        next(t_)
                    except StopIteration:
                        tasks.remove(t_)

        for i in range(0, NT, 2):
            run_tasks([gen_x1(i), gen_x1(i + 1)])
        S.op('dve', lambda: V.tensor_reduce(out=mxs[:, 0, :], in_=lg, axis=AX.X, op=ALU.max), reads=['lg'], writes=['mx0'])
        S.op('dve', lambda: V.tensor_tensor(out=lg, in0=lg, in1=bc(mxs[:, 0, :].rearrange("p (i o) -> p i o", o=1), [128, NT, E]), op=ALU.subtract),
             reads=['lg', 'mx0'], writes=['lg'])
        S.op('act', lambda: ACT.activation(out=lg, in_=lg, func=AF.Exp), reads=['lg'], writes=['lg'])
        S.op('dve', lambda: V.tensor_reduce(out=mxs[:, 1, :], in_=lg, axis=AX.X, op=ALU.add), reads=['lg'], writes=['mx1'])
        S.op('dve', lambda: V.reciprocal(out=mxs[:, 2, :], in_=mxs[:, 1, :]), reads=['mx1'], writes=['mx2'])
        S.op('dve', lambda: V.tensor_tensor(out=afftm, in0=lg, in1=bc(mxs[:, 2, :].rearrange("p (i o) -> p i o", o=1), [128, NT, E]), op=ALU.mult),
             reads=['lg', 'mx2'], writes=['afftm'])

    def phase_moe(s, u2tm, base):
        AR.seek(base + E * 2 * D * 2)
        for wsrc in (wg_d, wu_d, wd_d):
            wt0 = AR.alloc([128, 8, D], BF16)
            for hh in range(4):
                S.dma('pool', wt0[:, hh * 2:(hh + 1) * 2, :], wsrc[0, hh * 256:(hh + 1) * 256, :].rearrange("(k p) n -> p k n", p=128), writes=[('Wpre', hh)])
        AR.seek(base)
        affT = AR.alloc([16, T], F32)
        work = AR.alloc([16, T], F32)
        maskT = AR.alloc([16, T], F32)
        slotT = AR.alloc([16, T], F32)
        mx8 = AR.alloc([16, 8], F32)
        for i in range(NT):
            pz = ps[i // 4]
            S.op('pe', lambda: PE.transpose(out=pz[0:16, (i % 4) * 128:(i % 4 + 1) * 128], in_=afftm[:, i, :], identity=identf),
                 reads=['afftm', 'identf'], writes=[psk[i // 4]], pe_acc=True)
        for q in range(4):
            S.op('act', lambda: ACT.copy(out=affT[:, q * 512:(q + 1) * 512], in_=ps[q][0:16, :]), reads=[psk[q]], writes=['affT'])
        S.op('dve', lambda: V.tensor_copy(out=work, in_=affT), reads=['affT'], writes=['work'])
        for it in range(CAP // 8):
            S.op('dve', lambda: V.max(out=mx8, in_=work), reads=['work'], writes=['mx8'])
            if it < CAP // 8 - 1:
                S.op('dve', lambda: V.match_replace(out=work, in_to_replace=mx8, in_values=work, imm_value=-1.0), reads=['work', 'mx8'], writes=['work'])
        S.op('dve', lambda: V.tensor_scalar(out=maskT, in0=affT, scalar1=mx8[:, 7:8], scalar2=None, op0=ALU.is_ge), reads=['affT', 'mx8'], writes=['maskT'])
        S.op('pool', lambda: POOL.memset(work, 1.0), reads=['work'], writes=['work'])
        S.op('dve', lambda: V.tensor_tensor_scan(out=slotT, data0=work, data1=maskT, initial=0.0, op0=ALU.mult, op1=ALU.add), reads=['work', 'maskT'], writes=['slotT'])
        S.op('dve', lambda: V.tensor_tensor(out=slotT, in0=slotT, in1=maskT, op=ALU.mult), reads=['slotT', 'maskT'], writes=['slotT'])
        S.op('dve', lambda: V.tensor_scalar(out=slotT, in0=slotT, scalar1=-1.0, scalar2=None, op0=ALU.add), reads=['slotT'], writes=['slotT'])
        pz = ps[4]
        for i in range(NT):
            S.op('pe', lambda: PE.transpose(out=pz[:, i * 16:(i + 1) * 16], in_=slotT[:, i * 128:(i + 1) * 128], identity=identf[0:16, 0:16]),
                 reads=['slotT', 'identf'], writes=[psk[4]], pe_acc=True)
        S.op('act', lambda: ACT.copy(out=slot_tm.rearrange("p i e -> p (i e)"), in_=pz[:, 0:256]), reads=[psk[4]], writes=['slot_tm'])
        S.op('dve', lambda: V.tensor_copy(out=affhl[:, :, :, 0], in_=afftm), reads=['afftm'], writes=['affhl'])
        S.op('dve', lambda: V.tensor_tensor(out=affhl[:, :, :, 1], in0=afftm, in1=affhl[:, :, :, 0], op=ALU.subtract), reads=['afftm', 'affhl'], writes=['affhl'])
        S.barrier()
        AR.seek(base)
        ye = AR.alloc([128, E, 2, D], BF16)
        Wg = AR.alloc([128, 8, D], BF16)
        Wu = AR.alloc([128, 8, D], BF16)
        Wd = AR.alloc([128, 8, D], BF16)
        wbase = AR.ptr
        Pe = AR.alloc([128, NT, CAP], BF16)
        xeT = AR.alloc([128, 8, CAP], BF16)
        hT = AR.alloc([128, 8, CAP], BF16)
        hs = AR.alloc([128, CAP], F32)
        affs = AR.alloc([128, 4], F32)
        gt2B = AR.alloc([128, D], F32)
        S.dma('sp', gt2B, mod_d[s, 5], writes=['gt2B'])
        for e in range(E):
            for (wt, wsrc, nm) in ((Wg, wg_d, 'Wg'), (Wu, wu_d, 'Wu'), (Wd, wd_d, 'Wd')):
                if e == 0:
                    continue
                for hh in range(4):
                    S.dma('pool', wt[:, hh * 2:(hh + 1) * 2, :], wsrc[e, hh * 256:(hh + 1) * 256, :].rearrange("(k p) n -> p k n", p=128), writes=[(nm, hh)])
            for i in range(NT):
                S.op('dve', lambda: V.tensor_scalar(out=Pe[:, i, :], in0=iota_row, scalar1=slot_tm[:, i, e:e + 1], scalar2=None, op0=ALU.is_equal),
                     reads=['iota_row', 'slot_tm'], writes=[('Pe', i)])
            for fc in range(8):
                pz, pk = ps[fc // 2], psk[fc // 2]
                pzs = pz[:, (fc % 2) * 256:(fc % 2 + 1) * 256]
                for i in range(NT):
                    S.op('pe', lambda: PE.matmul(pzs, lhsT=u2tm[:, i, fc * 128:(fc + 1) * 128], rhs=Pe[:, i, :], start=(i == 0), stop=(i == NT - 1)),
                         reads=[('u2', i), ('Pe', i)], writes=[pk], pe_acc=True)
                S.op('act', lambda: ACT.copy(out=xeT[:, fc, :], in_=pzs), reads=[pk], writes=[('xeT', fc)])
            pa, pka = ps[4], psk[4]
            for half in range(2):
                for i in range(NT):
                    S.op('pe', lambda: PE.matmul(pa[:, half * 2:(half + 1) * 2], lhsT=Pe[:, i, half * 128:(half + 1) * 128], rhs=affhl[:, i, e, :], start=(i == 0), stop=(i == NT - 1)),
                         reads=[('Pe', i), 'affhl'], writes=[pka], pe_acc=True)
            S.op('dve', lambda: V.tensor_reduce(out=affs[:, 0:2], in_=pa[:, 0:4].rearrange("p (h t) -> p h t", t=2), axis=AX.X, op=ALU.add), reads=[pka], writes=['affs'])
            for fk in range(8):
                pg, pkg = ps[5], psk[5]
                pu, pku = ps[6], psk[6]
                for k in range(8):
                    S.op('pe', lambda: PE.matmul(pg[:, 0:CAP], lhsT=Wg[:, k, fk * 128:(fk + 1) * 128], rhs=xeT[:, k, :], start=(k == 0), stop=(k == 7)),
                         reads=[('Wg', k // 2), ('xeT', k)], writes=[pkg], pe_acc=True)
                for k in range(8):
                    S.op('pe', lambda: PE.matmul(pu[:, 0:CAP], lhsT=Wu[:, k, fk * 128:(fk + 1) * 128], rhs=xeT[:, k, :], start=(k == 0), stop=(k == 7)),
                         reads=[('Wu', k // 2), ('xeT', k)], writes=[pku], pe_acc=True)
                S.op('act', lambda: ACT.activation(out=hs, in_=pg[:, 0:CAP], func=AF.Silu), reads=[pkg], writes=['hs'])
                S.op('dve', lambda: V.tensor_tensor(out=hT[:, fk, :], in0=pu[:, 0:CAP], in1=hs, op=ALU.mult), reads=[pku, 'hs'], writes=[('hT', fk)])
            for half in range(2):
                for cb in range(2):
                    py, pky = ps[7] if (half * 2 + cb) % 2 else ps[4], psk[7] if (half * 2 + cb) % 2 else psk[4]
                    for fk in range(8):
                        S.op('pe', lambda: PE.matmul(py, lhsT=hT[:, fk, half * 128:(half + 1) * 128], rhs=Wd[:, fk, cb * 512:(cb + 1) * 512], start=(fk == 0), stop=(fk == 7)),
                             reads=[('hT', fk), ('Wd', fk // 2), 'affs'], writes=[pky], pe_acc=True)
                    S.op('dve', lambda: V.tensor_scalar(out=ye[:, e, half, cb * 512:(cb + 1) * 512], in0=py, scalar1=affs[:, half:half + 1], scalar2=None, op0=ALU.mult),
                         reads=[pky, 'affs'], writes=['ye'])
        S.barrier()
        AR.seek(wbase - 3 * 8 * D * 2)
        Pall = AR.alloc([128, E, CAP], BF16)
        PT = AR.alloc([128, 2 * E, 128], BF16)
        x1t = [AR.alloc([128, D], F32) for _ in range(2)]
        ot = [AR.alloc([128, D], F32) for _ in range(2)]
        for i in range(NT):
            b = i % 2
            sl = slice(i * 128, (i + 1) * 128)
            S.dma('sp', x1t[b], out_d[s, sl, :], reads=[('outd', i)], writes=[('x1t', b)])
            for e in range(E):
                S.op('dve', lambda: V.tensor_scalar(out=Pall[:, e, :], in0=iota_row, scalar1=slot_tm[:, i, e:e + 1], scalar2=None, op0=ALU.is_equal),
                     reads=['iota_row', 'slot_tm'], writes=[('Pall', e // 4)])
            for q in range(4):
                pz = ps[q].bitcast(BF16).rearrange("p (j t) -> p j t", j=8)
                for j in range(8):
                    idx = q * 8 + j
                    e, half = idx // 2, idx % 2
                    S.op('pe', lambda: PE.transpose(out=pz[:, j, :], in_=Pall[:, e, half * 128:(half + 1) * 128], identity=ident),
                         reads=[('Pall', e // 4), 'ident'], writes=[psk[q]], pe_acc=True)
                if q % 2 == 0:
                    S.op('act', lambda: ACT.copy(out=PT[:, q * 8:(q + 1) * 8, :], in_=pz), reads=[psk[q]], writes=[('PT', q)])
                else:
                    S.op('dve', lambda: V.tensor_copy(out=PT[:, q * 8:(q + 1) * 8, :], in_=pz), reads=[psk[q]], writes=[('PT', q)])
            for cb in range(2):
                po, pko = ps[4 + cb + 2 * (i % 2)], psk[4 + cb + 2 * (i % 2)]
                for idx in range(2 * E):
                    e, half = idx // 2, idx % 2
                    S.op('pe', lambda: PE.matmul(po, lhsT=PT[:, idx, :], rhs=ye[:, e, half, cb * 512:(cb + 1) * 512], start=(idx == 0), stop=(idx == 2 * E - 1)),
                         reads=[('PT', idx // 8), 'ye'], writes=[pko], pe_acc=True)
                S.op('dve', lambda: V.tensor_tensor(out=ot[b][:, cb * 512:(cb + 1) * 512], in0=po, in1=gt2B[:, cb * 512:(cb + 1) * 512], op=ALU.mult),
                     reads=[pko, 'gt2B'], writes=[('ot', b, cb)])
            S.op('dve', lambda: V.tensor_tensor(out=ot[b], in0=ot[b], in1=x1t[b], op=ALU.add), reads=[('ot', b, 0), ('ot', b, 1), ('x1t', b)], writes=[('ot', b, 0), ('ot', b, 1)])
            S.dma('sp', out_d[s, sl, :], ot[b], reads=[('ot', b, 0), ('ot', b, 1)], writes=[('outd', i)])

    def dbg_dump(src_ap, shape, key_reads=()):
        AR.seek(AR_TOP)
        t = AR.alloc(shape, F32)
        S.op('dve', lambda: V.tensor_copy(out=t, in_=src_ap), writes=['dbgt'])
        flat = t if len(shape) == 2 else t.rearrange("p a b -> p (a b)")
        S.dma('sp', dbg_d, flat, reads=['dbgt'])

    AR_TOP = 160 * 1024
    phase_adaln()
    for s in range(nseq):
        AR.seek(0)
        zsT = AR.alloc([128, 15, T], BF16)
        uT = AR.alloc([128, 8, T], BF16)
        base1 = AR.ptr
        phase_norm1(s, uT, base1)
        S.barrier()
        if dbg and dbg[0] == 'uT':
            dbg_dump(uT[:, :, 0:512], [128, 8, 512]); break
        for q in range(4):
            S.dma('sp', u_d[:, 2 * q:2 * q + 2, :], uT[:, 2 * q:2 * q + 2, :], reads=[('uT', 0), ('uT', 1), ('uT', 2), ('uT', 3)], writes=['uscr'])
        phase_rwkv_cols(uT, zsT, base1)
        S.barrier()
        if dbg and dbg[0] == 'zs':
            dbg_dump(zsT[:, :, 0:256], [128, 15, 256]); break
        AR.seek(61440)
        kkT = AR.alloc([128, 4, T], BF16)
        yaT = AR.alloc([128, 4, T], BF16)
        base3 = AR.ptr
        phase_scan(zsT, kkT, 77824)
        if dbg and dbg[0] == 'yscan':
            AR.seek(AR_TOP)
            t = AR.alloc([128, 2, 512], F32)
            S.dma('sp', t[:, 0, :], y_d[0, 0:128, :], writes=['dbgt'])
            S.dma('sp', t[:, 1, :], y_d[1, 0:128, :], writes=['dbgt'])
            S.dma('sp', dbg_d, t.rearrange("p a b -> p (a b)"), reads=['dbgt']); break
        phase_post(zsT, yaT, base3)
        S.barrier()
        if dbg and dbg[0] == 'yaT':
            dbg_dump(yaT[:, :, 0:512], [128, 4, 512]); break
        AR.seek(0)
        uT = AR.alloc([128, 8, T], BF16)
        AR.seek(94208)
        ybT = AR.alloc([128, 4, T], BF16)
        baseB = AR.ptr
        for q in range(4):
            S.dma('sp' if q % 2 == 0 else 'act', uT[:, 2 * q:2 * q + 2, :], u_d[:, 2 * q:2 * q + 2, :], writes=[('uT', 0), ('uT', 1), ('uT', 2), ('uT', 3)])
        phase_attn(s, uT, ybT, 32768, baseB)
        S.barrier()
        if dbg and dbg[0] == 'ybT':
            dbg_dump(ybT[:, :, 0:512], [128, 4, 512]); break
        AR.seek(32768)
        mergedT = AR.alloc([128, 8, T], BF16)
        phase_merge(uT, yaT, ybT, mergedT, (65536, baseB))
        S.barrier()
        if dbg and dbg[0] == 'merged':
            dbg_dump(mergedT[:, :, 0:512], [128, 8, 512]); break
        AR.seek(0)
        u2tm = AR.alloc([128, NT, D], BF16)
        phase_x1(s, mergedT, u2tm, 65536)
        S.barrier()
        if dbg and dbg[0] == 'aff':
            dbg_dump(afftm.rearrange("p i e -> p (i e)"), [128, 256]); break
        phase_moe(s, u2tm, 32768)
        S.barrier()

    S.finish('sp')
    print("ninstr", S.ninstr, "pe_incs", S.npe_inc, "arena hi", AR.hi)
    return nc


def _consts():
    cm = np.zeros((13, 128, 128), np.float32)
    p = np.arange(128)
    cm[0] = (p[:, None] // 64 == p[None, :] // 64).astype(np.float32)
    R = np.zeros((128, 128), np.float32)
    for blk in range(2):
        o = blk * 64
        for d_ in range(8):
            R[o + d_ + 8, o + d_] = -1.0
            R[o + d_, o + d_ + 8] = 1.0
    cm[1] = R
    cm[2] = (p[:, None] >= p[None, :]).astype(np.float32)
    cm[3] = (p[:, None] <= p[None, :]).astype(np.float32)
    s_ = (p % 64)[:, None]
    t_ = (p % 64)[None, :]
    a_col = (p[None, :] >= 64)
    fwd = np.where(a_col, s_ < t_, s_ <= t_)
    bwd = np.where(a_col, s_ > t_, s_ >= t_)
    cm[4] = fwd.astype(np.float32)
    cm[5] = bwd.astype(np.float32)
    cm[6] = cm[4].T
    cm[7] = cm[5].T
    cm[8][:, 0] = (p < 64)
    cm[8][:, 1] = (p >= 64)
    cm[9][:, 0:64] = 1.0
    cm[10][:, 64:128] = 1.0
    cm[11] = ((p % 64)[:, None] < (p % 64)[None, :]).astype(np.float32)
    cm[12] = ((p % 64)[:, None] > (p % 64)[None, :]).astype(np.float32)
    return np.ascontiguousarray(cm.transpose(1, 0, 2).reshape(128, 13 * 128))


def _prep_shared(inp):
    f = lambda a: np.ascontiguousarray(np.asarray(a, dtype=np.float32))
    L = 0
    w_in = f(inp["w_in"][L]).copy()
    qoff = 1920
    perm = []
    for c in range(4):
        perm += list(range(c * 64, (c + 1) * 64)) + list(range((4 + c) * 64, (5 + c) * 64))
    perm = np.array(perm)
    w_in[:, qoff:qoff + 512] = w_in[:, qoff:qoff + 512][:, perm]
    p_attn = f(inp["p_attn"][L])[perm, :]
    pp = np.zeros((128, NPP), np.float32)

    def put(name, arr):
        o, w = PP[name]
        pp[:, o:o + w] = arr

    chunked = lambda v: np.asarray(v, np.float32).reshape(-1, 128).T
    put("mp", chunked(inp["mu_prev"][L]))
    put("mn", chunked(inp["mu_next"][L]))
    put("w0", np.concatenate([chunked(inp["rwkv_w0"][L][0]), chunked(inp["rwkv_w0"][L][1])], 1))
    put("a0", np.concatenate([chunked(inp["rwkv_a0"][L][0]), chunked(inp["rwkv_a0"][L][1])], 1))
    put("kk", chunked(inp["rwkv_k_k"][L]))
    put("ka", chunked(inp["rwkv_k_a"][L]))
    put("rk", chunked(np.asarray(inp["rwkv_r_k"][L]).reshape(-1)))
    put("qg", np.tile(np.asarray(inp["q_norm_g"][L], np.float32), 2)[:, None])
    put("kg", np.tile(np.asarray(inp["k_norm_g"][L], np.float32), 2)[:, None])
    inv_freq = (500000.0 ** (-np.arange(0, 16, 2, dtype=np.float32) / 16)).astype(np.float32)
    invf = np.zeros(64, np.float32)
    invf[0:8] = inv_freq
    invf[8:16] = inv_freq
    put("invf", np.tile(invf, 2)[:, None])
    sink = np.asarray(inp["attn_sink"][L], np.float32)
    sk = np.zeros((128, 4), np.float32)
    for j in range(4):
        sk[0:64, j] = sink[j]
        sk[64:128, j] = sink[4 + j]
    put("sink", sk)
    w2cat = np.zeros((128, 2, 512), np.float32)
    a2cat = np.zeros((128, 2, 512), np.float32)
    for d_ in range(2):
        w2cat[d_ * 64:(d_ + 1) * 64, d_, :] = inp["rwkv_w2"][L][d_]
        a2cat[d_ * 64:(d_ + 1) * 64, d_, :] = inp["rwkv_a2"][L][d_]
    return {
        "w_ada": f(inp["w_ada"][L]), "b_ada": f(inp["b_ada"][L])[None, :] if np.asarray(inp["b_ada"][L]).ndim == 1 else f(inp["b_ada"][L]),
        "norm1_g": f(inp["norm1_g"][L]).reshape(1, D), "norm2_g": f(inp["norm2_g"][L]).reshape(1, D),
        "w_in": w_in, "pp": pp, "w2cat": w2cat.reshape(128, 1024), "a2cat": a2cat.reshape(128, 1024),
        "g2": f(inp["rwkv_g2"][L]), "gn_w": f(inp["rwkv_gn_w"][L]).reshape(1, 512), "gn_b": f(inp["rwkv_gn_b"][L]).reshape(1, 512),
        "p_rwkv": f(inp["p_rwkv"][L]), "p_attn": np.ascontiguousarray(p_attn), "w_out": f(inp["w_out"][L]),
        "w_router": f(inp["w_router"][L]), "w_gate": f(inp["w_gate"][L]), "w_up": f(inp["w_up"][L]), "w_down": f(inp["w_down"][L]),
        "cmats": _consts(),
    }


def _core_inputs(inp, shared, seqs):
    x = np.ascontiguousarray(np.asarray(inp["x"], np.float32)[seqs])
    c = np.asarray(inp["c"], np.float32)[seqs]
    cT = np.ascontiguousarray(c.reshape(len(seqs), 8, 128).transpose(0, 2, 1))
    pos = np.ascontiguousarray(np.asarray(inp["positions"]).astype(np.int32)[seqs][:, None, :])
    m = dict(shared)
    m.update({"x": x, "cT": cT, "pos": pos})
    return m


def kernel(**inputs):
    shared = _prep_shared(inputs)
    nc = build(NSEQ)
    in_maps = [_core_inputs(inputs, shared, list(range(i * NSEQ, (i + 1) * NSEQ))) for i in range(NCORES)]
    res = run_bass_kernel_spmd(nc, in_maps, core_ids=list(range(NCORES)))
    out = np.concatenate([np.asarray(r["out"]) for r in res.results], axis=0)
    return out.astype(np.float32)
```

```python
import numpy as np
import concourse.bass as bass
import concourse.mybir as mybir
from concourse.bass_utils import run_bass_kernel_spmd

F32 = mybir.dt.float32
BF16 = mybir.dt.bfloat16
I32 = mybir.dt.int32
ALU = mybir.AluOpType
AF = mybir.ActivationFunctionType
AX = mybir.AxisListType

T = 2048
D = 1024
NT = 16
NB = 4
NSEQ = 2
NCORES = 8
E = 16
CAP = 256
LAM = float(np.exp(-0.5))
NCH = 4
TBS = NCH * 64
NTB = T // TBS
TWO_PI = float(2 * np.pi)
C1 = 6.28125
C2 = TWO_PI - C1

PP = {}
_o = 0
for _n, _w in [("mp", 15), ("mn", 15), ("w0", 8), ("a0", 8), ("kk", 4), ("ka", 4), ("rk", 4), ("qg", 1), ("kg", 1),
               ("invf", 1), ("sink", 4)]:
    PP[_n] = (_o, _w)
    _o += _w
NPP = _o


class Ticket:
    __slots__ = ('ins', 'sem', 'val', 'parent')

    def __init__(self, ins):
        self.ins = ins
        self.sem = None
        self.val = None
        self.parent = None

    def root(self):
        t = self
        while t.parent is not None:
            t = t.parent
        return t


class Sync:
    SEM_MAX = 30000

    def __init__(self, nc):
        self.nc = nc
        self.E = {'pe': nc.tensor, 'act': nc.scalar, 'dve': nc.vector, 'pool': nc.gpsimd, 'sp': nc.sync}
        self.sem = {}
        self.cnt = {}
        self.nsem = 0
        for e in self.E:
            self._newsem(e)
        self.waited = {}
        self.lastw = {}
        self.reads = {}
        self.dma_sems = {}
        self.dma_rr = {}
        self.ninstr = 0
        self.pend = None
        self.pend_writes = None
        self.npe_inc = 0

    def _newsem(self, e):
        self.sem[e] = self.nc.alloc_semaphore(f"s_{e}_{self.nsem}")
        self.nsem += 1
        self.cnt[e] = 0

    def _flush_pe(self):
        t = self.pend
        if t is None:
            return
        if self.cnt['pe'] >= self.SEM_MAX:
            self._newsem('pe')
        self.cnt['pe'] += 1
        t.sem = self.sem['pe']
        t.val = self.cnt['pe']
        t.ins.then_inc(t.sem, 1)
        self.npe_inc += 1
        self.pend = None
        self.pend_writes = None

    def _wait(self, e, ev):
        if ev is None:
            return
        if isinstance(ev, Ticket):
            if e == 'pe':
                return
            t = ev.root()
            if t.val is None:
                assert t is self.pend
                self._flush_pe()
            sem, val = t.sem, t.val
        else:
            src, sem, val = ev
        k = (e, sem.name)
        if self.waited.get(k, 0) >= val:
            return
        self.waited[k] = val
        self.E[e].wait_ge(sem, val)

    def deps(self, e, reads, writes, pe_acc=False):
        for k in reads:
            self._wait(e, self.lastw.get(k))
        for k in writes:
            lw = self.lastw.get(k)
            if not (pe_acc and isinstance(lw, Ticket)):
                self._wait(e, lw)
            for ev in self.reads.get(k, {}).values():
                self._wait(e, ev)

    def commit(self, src, ev, reads, writes):
        for k in reads:
            self.reads.setdefault(k, {})[src] = ev
        for k in writes:
            self.lastw[k] = ev
            self.reads[k] = {}

    def op(self, e, fn, reads=(), writes=(), pe_acc=False):
        self.deps(e, reads, writes, pe_acc)
        if e == 'pe':
            ins = fn()
            t = Ticket(ins)
            if self.pend is not None:
                if self.pend_writes == tuple(writes):
                    self.pend.parent = t
                    self.pend = None
                else:
                    self._flush_pe()
            self.pend = t
            self.pend_writes = tuple(writes)
            self.commit('pe', t, reads, writes)
            self.ninstr += 1
            return t
        if self.cnt[e] >= self.SEM_MAX:
            self._newsem(e)
        ins = fn()
        self.cnt[e] += 1
        ev = (e, self.sem[e], self.cnt[e])
        ins.then_inc(self.sem[e], 1)
        self.commit(e, ev, reads, writes)
        self.ninstr += 1
        return ev

    def dma(self, e, out, in_, reads=(), writes=(), nslots=8, **kw):
        if e == 'pool':
            nslots = 2
        lst = self.dma_sems.setdefault(e, [])
        if len(lst) < nslots:
            lst.append([self.nc.alloc_semaphore(f"d_{e}_{len(lst)}"), 0])
        i = self.dma_rr.get(e, 0)
        self.dma_rr[e] = (i + 1) % nslots
        slot = lst[i % len(lst)]
        sem, uses = slot
        if uses > 0:
            self._wait(e, ('dma', sem, 16 * uses))
        self.deps(e, reads, writes)
        self.E[e].dma_start(out=out, in_=in_, **kw).then_inc(sem, 16)
        slot[1] = uses + 1
        ev = ('dma_%s_%d' % (e, i % len(lst)), sem, 16 * (uses + 1))
        self.commit(ev[0], ev, reads, writes)
        self.ninstr += 1
        return ev

    def barrier(self):
        self._flush_pe()
        evs = [(e, self.sem[e], self.cnt[e]) for e in self.E if self.cnt[e] > 0]
        for q, lst in self.dma_sems.items():
            for sem, uses in lst:
                if uses:
                    evs.append(('dma', sem, 16 * uses))
        for e in self.E:
            for ev in evs:
                if ev[0] != e:
                    self._wait(e, ev)
        self.lastw = {}
        self.reads = {}

    def finish(self, e='sp'):
        self._flush_pe()
        for q, lst in self.dma_sems.items():
            for sem, uses in lst:
                if uses:
                    self._wait(e, ('dma', sem, 16 * uses))


class Arena:
    def __init__(self, nc, name, nbytes):
        self.n4 = nbytes // 4
        self.t = nc.alloc_sbuf_tensor(name, [128, self.n4], F32).ap()
        self.ptr = 0
        self.hi = 0

    def seek(self, off):
        self.ptr = off

    def alloc(self, shape, dtype, parts=None):
        esz = 4 if dtype in (F32, I32) else 2
        n = int(np.prod(shape[1:]))
        nb = (n * esz + 31) // 32 * 32
        assert self.ptr % 4 == 0
        a = self.ptr // 4
        assert a + nb // 4 <= self.n4, f"arena overflow {self.ptr}+{nb} > {self.n4 * 4}"
        v = self.t[:, a:a + nb // 4]
        if dtype != F32:
            v = v.bitcast(dtype)
        v = v[0:shape[0], 0:n]
        if len(shape) > 2:
            names = " ".join(f"d{i}" for i in range(len(shape) - 1))
            kw = {f"d{i}": int(shape[i + 1]) for i in range(len(shape) - 1)}
            v = v.rearrange(f"p ({names}) -> p {names}", **kw)
        self.ptr += nb
        self.hi = max(self.hi, self.ptr)
        return v


def bc(ap, shape):
    return ap.to_broadcast(list(shape))


def build(nseq=NSEQ, dbg=None, stop_after=None):
    nc = bass.Bass("TRN2", target_bir_lowering=False)
    S = Sync(nc)
    V, ACT, POOL, PE = nc.vector, nc.scalar, nc.gpsimd, nc.tensor

    def din(name, shape, dt=F32):
        return nc.dram_tensor(name, list(shape), dt, kind="ExternalInput").ap()

    x_d = din("x", [nseq, T, D])
    cT_d = din("cT", [nseq, 128, 8])
    pos_d = din("pos", [nseq, 1, T], I32)
    wada_d = din("w_ada", [D, 6 * D])
    bada_d = din("b_ada", [1, 6 * D])
    n1g_d = din("norm1_g", [1, D])
    n2g_d = din("norm2_g", [1, D])
    win_d = din("w_in", [D, 4736])
    pp_d = din("pp", [128, NPP])
    w2c_d = din("w2cat", [128, 2 * 512])
    a2c_d = din("a2cat", [128, 2 * 512])
    g2_d = din("g2", [128, 512])
    gnw_d = din("gn_w", [1, 512])
    gnb_d = din("gn_b", [1, 512])
    prw_d = din("p_rwkv", [512, D])
    pat_d = din("p_attn", [512, D])
    wout_d = din("w_out", [D, D])
    wr_d = din("w_router", [D, E])
    wg_d = din("w_gate", [E, D, D])
    wu_d = din("w_up", [E, D, D])
    wd_d = din("w_down", [E, D, D])
    cm_d = din("cmats", [128, 13 * 128])
    out_d = nc.dram_tensor("out", [nseq, T, D], F32, kind="ExternalOutput").ap()
    mod_d = nc.dram_tensor("modscr", [nseq, 6, 128, D], F32, kind="Internal").ap()
    y_d = nc.dram_tensor("yscr", [2, T, 512], F32, kind="Internal").ap()
    u_d = nc.dram_tensor("uscr", [128, 8, T], BF16, kind="Internal").ap()
    dbg_d = None
    if dbg is not None:
        dbg_d = nc.dram_tensor("dbg", list(dbg[1]), F32, kind="ExternalOutput").ap()

    def sb(name, shape, dt=F32):
        return nc.alloc_sbuf_tensor('sb_' + name, list(shape), dt).ap()

    pp = sb("pp", [128, NPP])
    ident = sb("ident", [128, 128], BF16)
    identf = sb("identf", [128, 128])
    cmb = sb("cmb", [128, 13, 128], BF16)
    w2c = sb("w2c", [128, 2, 512], BF16)
    a2c = sb("a2c", [128, 2, 512], BF16)
    g2 = sb("g2", [128, 512], BF16)
    wr = sb("wr", [128, 8, E], BF16)
    epsc = sb("epsc", [128, 4])
    alpha = sb("alpha", [128, 15])
    oneminus_ka = sb("omka", [128, 4])
    two_omka = sb("omka2", [128, 4])
    negkkc = sb("negone", [128, 1])
    esk = sb("esk", [128, 4])
    rmask = sb("rmask", [128, TBS])
    iota_row = sb("iota_row", [128, CAP])
    ident4 = sb("ident4", [128, 4, 128], BF16)
    kar = sb("kar", [128, 4])
    c2r = sb("c2r", [128, 4])
    afftm = sb("afftm", [128, NT, E])
    slot_tm = sb("slot_tm", [128, NT, E])
    affhl = sb("affhl", [128, NT, E, 2], BF16)

    BLK1, ROT, MPREV, MNEXT = 0, 1, 2, 3
    MZT = (4, 5)
    MZ = (6, 7)
    HSEL = 8
    VP = (9, 10)

    ps = [nc.alloc_psum_tensor(f"ps{i}", [128, 512], F32).ap() for i in range(8)]
    psk = [f"ps{i}" for i in range(8)]

    AR = Arena(nc, "arena", 192 * 1024)

    def col(name, j=0, n=1):
        o, w = PP[name]
        return pp[:, o + j:o + j + n]

    S.dma('sp', pp, pp_d, writes=['pp'])
    S.dma('pool', cmb.rearrange("p a b -> p (a b)"), cm_d, writes=['cmb'])
    S.dma('pool', w2c.rearrange("p a b -> p (a b)"), w2c_d, writes=['w2c'])
    S.dma('pool', a2c.rearrange("p a b -> p (a b)"), a2c_d, writes=['a2c'])
    S.dma('pool', g2, g2_d, writes=['g2'])
    S.dma('pool', wr, wr_d.rearrange("(k p) e -> p k e", p=128), writes=['wr'])
    S.op('pool', lambda: POOL.memset(identf, 1.0), writes=['identf'])
    S.op('pool', lambda: POOL.affine_select(out=identf, in_=identf, pattern=[[1, 128]], compare_op=ALU.is_equal,
                                            fill=0.0, base=0, channel_multiplier=-1), reads=['identf'], writes=['identf'])
    S.op('dve', lambda: V.tensor_copy(out=ident, in_=identf), reads=['identf'], writes=['ident'])
    for j in range(4):
        S.op('dve', lambda: V.tensor_copy(out=ident4[:, j, :], in_=identf), reads=['identf'], writes=['ident4'])
    S.op('pool', lambda: POOL.memset(epsc[:, 0:1], 1e-6), writes=['epsc'])
    S.op('pool', lambda: POOL.memset(epsc[:, 1:2], 64e-5), reads=['epsc'], writes=['epsc'])
    S.op('pool', lambda: POOL.memset(epsc[:, 2:3], 1e-24), reads=['epsc'], writes=['epsc'])
    S.op('pool', lambda: POOL.memset(epsc[:, 3:4], 0.0), reads=['epsc'], writes=['epsc'])
    S.op('pool', lambda: POOL.memset(negkkc, -1.0), writes=['negone'])
    S.op('dve', lambda: V.tensor_tensor(out=alpha, in0=col("mp", 0, 15), in1=col("mn", 0, 15), op=ALU.add), reads=['pp'], writes=['alpha'])
    S.op('dve', lambda: V.tensor_scalar(out=alpha, in0=alpha, scalar1=-1.0, scalar2=1.0, op0=ALU.mult, op1=ALU.add), reads=['alpha'], writes=['alpha'])
    S.op('dve', lambda: V.tensor_scalar(out=oneminus_ka, in0=col("ka", 0, 4), scalar1=-1.0, scalar2=1.0, op0=ALU.mult, op1=ALU.add), reads=['pp'], writes=['omka'])
    S.op('dve', lambda: V.tensor_scalar(out=two_omka, in0=col("ka", 0, 4), scalar1=-2.0, scalar2=2.0, op0=ALU.mult, op1=ALU.add), reads=['pp'], writes=['omka2'])
    S.op('act', lambda: ACT.activation(out=esk, in_=col("sink", 0, 4), func=AF.Exp), reads=['pp'], writes=['esk'])
    S.op('dve', lambda: V.tensor_tensor(out=kar, in0=col("ka", 0, 4), in1=col("rk", 0, 4), op=ALU.mult), reads=['pp'], writes=['kar'])
    S.op('dve', lambda: V.tensor_tensor(out=c2r, in0=two_omka, in1=col("rk", 0, 4), op=ALU.mult), reads=['pp', 'omka2'], writes=['kar'])
    S.op('pool', lambda: POOL.memset(rmask, 1.0), writes=['rmask'])
    S.op('pool', lambda: POOL.memset(rmask.rearrange("p (c t) -> p c t", t=64)[:, :, 0:1], 0.0), reads=['rmask'], writes=['rmask'])
    S.op('pool', lambda: POOL.iota(iota_row, pattern=[[1, CAP]], base=0, channel_multiplier=0, allow_small_or_imprecise_dtypes=True), writes=['iota_row'])

    def debug_out(ap_sb, key, rows=None):
        S.dma('sp', dbg_d if rows is None else rows, ap_sb, reads=[key])

    def phase_adaln():
        AR.seek(0)
        csil = [AR.alloc([128, 8], F32) for _ in range(nseq)]
        crep = [AR.alloc([128, 9, 128], F32) for _ in range(nseq)]
        wblk = [AR.alloc([128, 9, 512], F32) for _ in range(3)]
        g1B = AR.alloc([128, D], F32)
        g2B = AR.alloc([128, D], F32)
        mt = [AR.alloc([128, 512], F32) for _ in range(4)]
        S.dma('sp', g1B, n1g_d.partition_broadcast(128), writes=['g1B'])
        S.dma('sp', g2B, n2g_d.partition_broadcast(128), writes=['g2B'])
        for b in range(3):
            S.op('pool', lambda: POOL.memset(wblk[b][:, 8, :], 0.0), writes=[('wblk', b)])
        for s in range(nseq):
            S.dma('sp', csil[s], cT_d[s], writes=[('csil', s)])
            S.op('act', lambda: ACT.activation(out=csil[s], in_=csil[s], func=AF.Silu), reads=[('csil', s)], writes=[('csil', s)])
            S.op('pool', lambda: POOL.memset(crep[s][:, 8, :], 0.0), writes=[('crep', s)])
            S.op('pool', lambda: POOL.memset(crep[s][0:1, 8, :], 1.0), reads=[('crep', s)], writes=[('crep', s)])
            S.op('dve', lambda: V.tensor_copy(out=crep[s][:, 0:8, :], in_=bc(csil[s].rearrange("p (k o) -> p k o", o=1), [128, 8, 128])),
                 reads=[('csil', s)], writes=[('crep', s)])
        ev = 0
        for jb in range(12):
            b = jb % 3
            piece = jb // 2
            c0 = jb * 512
            S.dma('sp', wblk[b][:, 0:4, :], wada_d[0:512, c0:c0 + 512].rearrange("(k p) n -> p k n", p=128), writes=[('wblk', b)])
            S.dma('act', wblk[b][:, 4:8, :], wada_d[512:1024, c0:c0 + 512].rearrange("(k p) n -> p k n", p=128), writes=[('wblk', b)])
            S.dma('sp', wblk[b][0:1, 8, :], bada_d[:, c0:c0 + 512], writes=[('wblk', b)])
            for s in range(nseq):
                pz, pkz = ps[ev % 4], psk[ev % 4]
                for k in range(9):
                    S.op('pe', lambda: PE.matmul(pz, lhsT=crep[s][:, k, :], rhs=wblk[b][:, k, :], start=(k == 0), stop=(k == 8)),
                         reads=[('crep', s), ('wblk', b)], writes=[pkz], pe_acc=True)
                m = mt[ev % 4]
                lc = (jb % 2) * 512
                if piece == 1:
                    S.op('dve', lambda: V.scalar_tensor_tensor(out=m, in0=pz, scalar=1.0, in1=g1B[:, lc:lc + 512], op0=ALU.add, op1=ALU.mult),
                         reads=[pkz, 'g1B'], writes=[('mt', ev % 4)])
                elif piece == 4:
                    S.op('dve', lambda: V.scalar_tensor_tensor(out=m, in0=pz, scalar=1.0, in1=g2B[:, lc:lc + 512], op0=ALU.add, op1=ALU.mult),
                         reads=[pkz, 'g2B'], writes=[('mt', ev % 4)])
                else:
                    S.op('act', lambda: ACT.copy(out=m, in_=pz), reads=[pkz], writes=[('mt', ev % 4)])
                S.dma('sp', mod_d[s, piece, :, lc:lc + 512], m, reads=[('mt', ev % 4)], writes=[('mod', s, piece)])
                ev += 1
        S.barrier()

    def phase_norm1(s, uT, base):
        AR.seek(base)
        scp = AR.alloc([128, D], F32)
        shp = AR.alloc([128, D], F32)
        xt = [AR.alloc([128, D], F32) for _ in range(2)]
        tmp2 = [AR.alloc([128, D], F32) for _ in range(2)]
        ub = [AR.alloc([128, D], BF16) for _ in range(2)]
        junk2 = [AR.alloc([128, D], BF16) for _ in range(2)]
        ss2 = [AR.alloc([128, 2], F32) for _ in range(2)]
        S.dma('sp', scp, mod_d[s, 1], reads=[('mod', s, 1)], writes=['scp'])
        S.dma('sp', shp, mod_d[s, 0], reads=[('mod', s, 0)], writes=['shp'])
        for i in range(NT):
            b = i % 2
            S.dma('sp', xt[b], x_d[s, i * 128:(i + 1) * 128, :], writes=[('xt', b)])
            tmp, junk, ss = tmp2[b], junk2[b], ss2[b]
            S.op('act', lambda: ACT.activation(out=junk, in_=xt[b], func=AF.Square, accum_out=ss[:, 0:1]), reads=[('xt', b)], writes=[('junk', b), ('ss', b)])
            S.op('act', lambda: ACT.activation(out=ss[:, 1:2], in_=ss[:, 0:1], func=AF.Sqrt, bias=epsc[:, 0:1], scale=1.0 / D), reads=[('ss', b), 'epsc'], writes=[('ss1', b)])
            S.op('dve', lambda: V.reciprocal(out=ss[:, 1:2], in_=ss[:, 1:2]), reads=[('ss1', b)], writes=[('ss1', b)])
            S.op('dve', lambda: V.scalar_tensor_tensor(out=tmp, in0=xt[b], scalar=ss[:, 1:2], in1=scp, op0=ALU.mult, op1=ALU.mult),
                 reads=[('xt', b), ('ss1', b), 'scp'], writes=[('tmp', b)])
            S.op('pool', lambda: POOL.tensor_tensor(out=ub[b], in0=tmp, in1=shp, op=ALU.add), reads=[('tmp', b), 'shp'], writes=[('ub', b)])
            pz = ps[i % 2].bitcast(BF16).rearrange("p (k t) -> p k t", k=8)
            for k in range(8):
                S.op('pe', lambda: PE.transpose(out=pz[:, k, :], in_=ub[b][:, k * 128:(k + 1) * 128], identity=ident),
                     reads=[('ub', b), 'ident'], writes=[psk[i % 2]], pe_acc=True)
            S.op('act', lambda: ACT.copy(out=uT[:, :, i * 128:(i + 1) * 128], in_=pz), reads=[psk[i % 2]], writes=[('uT', i // 4)])

    def phase_rwkv_cols(uT, zsT, base):
        AR.seek(base)
        wg = [AR.alloc([128, 8, 128], BF16) for _ in range(2)]
        ztmpP = [AR.alloc([128, T + 2], F32) for _ in range(2)]
        shtP = [AR.alloc([128, T], F32) for _ in range(2)]
        for q in range(2):
            S.op('pool', lambda: POOL.memset(ztmpP[q][:, 0:1], 0.0), writes=[('ztmp', q)])
            S.op('pool', lambda: POOL.memset(ztmpP[q][:, T + 1:T + 2], 0.0), reads=[('ztmp', q)], writes=[('ztmp', q)])
        for j in range(15):
            b = j % 2
            ztmp, sht = ztmpP[b], shtP[b]
            S.dma('pool', wg[b], win_d[:, j * 128:(j + 1) * 128].rearrange("(k p) n -> p k n", p=128), writes=[('wg', b)])
            for tb in range(NB):
                pz = ps[(j * NB + tb) % 4]
                pk = psk[(j * NB + tb) % 4]
                for k in range(8):
                    S.op('pe', lambda: PE.matmul(pz, lhsT=wg[b][:, k, :], rhs=uT[:, k, tb * 512:(tb + 1) * 512], start=(k == 0), stop=(k == 7)),
                         reads=[('wg', b), ('uT', tb)], writes=[pk], pe_acc=True)
                S.op('act', lambda: ACT.copy(out=ztmp[:, 1 + tb * 512:1 + (tb + 1) * 512], in_=pz), reads=[pk], writes=[('ztmp', b)])
            S.op('dve', lambda: V.tensor_scalar(out=sht, in0=ztmp[:, 1:T + 1], scalar1=alpha[:, j:j + 1], scalar2=None, op0=ALU.mult),
                 reads=[('ztmp', b), 'alpha'], writes=[('sht', b)])
            S.op('dve', lambda: V.scalar_tensor_tensor(out=sht, in0=ztmp[:, 0:T], scalar=col("mp", j), in1=sht, op0=ALU.mult, op1=ALU.add),
                 reads=[('ztmp', b), ('sht', b), 'pp'], writes=[('sht', b)])
            S.op('dve', lambda: V.scalar_tensor_tensor(out=zsT[:, j, :], in0=ztmp[:, 2:T + 2], scalar=col("mn", j), in1=sht, op0=ALU.mult, op1=ALU.add),
                 reads=[('ztmp', b), ('sht', b), 'pp'], writes=[('zs', j)])
            if j == 12:
                S.op('act', lambda: ACT.activation(out=zsT[:, j, :], in_=zsT[:, j, :], func=AF.Tanh), reads=[('zs', j)], writes=[('zs', j)])
            if j == 14:
                S.op('act', lambda: ACT.activation(out=zsT[:, j, :], in_=zsT[:, j, :], func=AF.Sigmoid), reads=[('zs', j)], writes=[('zs', j)])

    def phase_scan(zsT, kkT, base):
        rT = lambda c: zsT[:, c, :]
        kT = lambda c: zsT[:, 4 + c, :]
        vT = lambda c: zsT[:, 8 + c, :]
        wdT = zsT[:, 12, :]
        adT = zsT[:, 13, :]
        AR.seek(base)
        kraw = AR.alloc([128, 512], F32)
        ksq = AR.alloc([128, 512], BF16)
        krs = AR.alloc([128, 512], F32)
        for c in range(4):
            for tb in range(NB):
                sl = slice(tb * 512, (tb + 1) * 512)
                S.op('dve', lambda: V.tensor_scalar(out=kraw, in0=kT(c)[:, sl], scalar1=col("kk", c), scalar2=None, op0=ALU.mult), reads=[('zs', 4 + c), 'pp'], writes=['kraw'])
                S.op('act', lambda: ACT.activation(out=ksq, in_=kraw, func=AF.Square), reads=['kraw'], writes=['ksq'])
                pz, pk = ps[tb % 2], psk[tb % 2]
                S.op('pe', lambda: PE.matmul(pz, lhsT=cmb[:, BLK1, :], rhs=ksq, start=True, stop=True), reads=['ksq', 'cmb'], writes=[pk], pe_acc=True)
                S.op('act', lambda: ACT.activation(out=krs, in_=pz, func=AF.Sqrt, bias=epsc[:, 2:3], scale=1.0), reads=[pk, 'epsc'], writes=['krs'])
                S.op('dve', lambda: V.reciprocal(out=krs, in_=krs), reads=['krs'], writes=['krs'])
                S.op('dve', lambda: V.tensor_tensor(out=kkT[:, c, sl], in0=kraw, in1=krs, op=ALU.mult), reads=['kraw', 'krs'], writes=[('kk', c)])
        S.barrier()
        AR.seek(base)
        sg = AR.alloc([128, 4, TBS], F32)
        ad = AR.alloc([128, 4, TBS], F32)
        cc = AR.alloc([128, 4, TBS], F32)
        t1 = AR.alloc([128, 4, TBS], F32)
        ex = [[AR.alloc([128, TBS], F32) for _ in range(2)] for _ in range(4)]
        kd = AR.alloc([128, 4, TBS], F32)
        bb = AR.alloc([128, 4, TBS], F32)
        pdec = AR.alloc([128, 4, NCH], F32)
        ARz = AR.alloc([128, 4, NCH, 2, 2, 64], BF16)
        Bz = AR.alloc([128, 4, NCH, 2, 64], BF16)
        BKt = AR.alloc([128, 4, NCH, 2, 64], BF16)
        KBh = AR.alloc([128, 4, NCH, 2, 64], BF16)
        KBt = AR.alloc([128, 4, NCH, 128], BF16)
        VZ = AR.alloc([128, NCH, 8, 64], BF16)
        XV = AR.alloc([128, NCH, 8, 64], BF16)
        ZTs = [[AR.alloc([128, 4, 128], BF16) for _ in range(2)] for _ in range(NCH)]
        ATm = [[AR.alloc([128, 4, 128], BF16) for _ in range(2)] for _ in range(NCH)]
        PTm = [[AR.alloc([128, 4, 128], BF16) for _ in range(2)] for _ in range(NCH)]
        Pm = [[AR.alloc([128, 4, 128], BF16) for _ in range(2)] for _ in range(NCH)]
        Am = [[AR.alloc([128, 4, 128], BF16) for _ in range(2)] for _ in range(NCH)]
        W1s = AR.alloc([128, 4, 64], BF16)
        S32 = [AR.alloc([128, 4, 64], F32) for _ in range(2)]
        Sb = [AR.alloc([128, 4, 64], BF16) for _ in range(2)]
        ysb = [AR.alloc([64, 512], F32) for _ in range(2)]
        S.op('pool', lambda: POOL.memset(ARz.rearrange("p a b c d e -> p (a b c d e)"), 0.0), writes=['ARz'])
        S.op('pool', lambda: POOL.memset(Bz.rearrange("p a b c d -> p (a b c d)"), 0.0), writes=['Bz'])
        S.op('pool', lambda: POOL.memset(VZ.rearrange("p a b c -> p (a b c)"), 0.0), writes=['VZ'])

        def chain(gens):
            for g_ in gens:
                yield from g_

        def run_tasks(tasks):
            tasks = list(tasks)
            while tasks:
                for t_ in list(tasks):
                    try:
                        next(t_)
                    except StopIteration:
                        tasks.remove(t_)

        yev = 0
        pendQ = None
        for d in range(2):
            S.op('pool', lambda: POOL.memset(S32[d].rearrange("p a b -> p (a b)"), 0.0), writes=[('S32', d)])
            S.op('pool', lambda: POOL.memset(Sb[d].rearrange("p a b -> p (a b)"), 0.0), writes=[('Sb', d)])
            tbs = range(NTB) if d == 0 else range(NTB - 1, -1, -1)
            for tb in tbs:
                sl = slice(tb * TBS, (tb + 1) * TBS)
                def gen_prep(c):
                    pz, pk = ps[c % 2], psk[c % 2]
                    S.op('pe', lambda: PE.matmul(pz[:, 0:TBS], lhsT=w2c[:, d, c * 128:(c + 1) * 128], rhs=wdT[:, sl], start=True, stop=True),
                         reads=['w2c', ('zs', 12)], writes=[pk], pe_acc=True)
                    S.op('act', lambda: ACT.activation(out=sg[:, c, :], in_=pz[:, 0:TBS], func=AF.Sigmoid, bias=col("w0", d * 4 + c), scale=1.0),
                         reads=[pk, 'pp'], writes=[('sg', c)])
                    pz2, pk2 = ps[2 + c % 2], psk[2 + c % 2]
                    S.op('pe', lambda: PE.matmul(pz2[:, 0:TBS], lhsT=a2c[:, d, c * 128:(c + 1) * 128], rhs=adT[:, sl], start=True, stop=True),
                         reads=['a2c', ('zs', 13)], writes=[pk2], pe_acc=True)
                    S.op('act', lambda: ACT.activation(out=ad[:, c, :], in_=pz2[:, 0:TBS], func=AF.Sigmoid, bias=col("a0", d * 4 + c), scale=1.0),
                         reads=[pk2, 'pp'], writes=[('ad', c)])
                    yield
                    S.op('dve', lambda: V.tensor_tensor_scan(out=cc[:, c, :], data0=rmask, data1=sg[:, c, :], initial=0.0, op0=ALU.mult, op1=ALU.add),
                         reads=['rmask', ('sg', c)], writes=[('cc', c)])
                    cc3 = cc[:, c, :].rearrange("p (h t) -> p h t", t=64)
                    sg3 = sg[:, c, :].rearrange("p (h t) -> p h t", t=64)
                    t13 = t1[:, c, :].rearrange("p (h t) -> p h t", t=64)
                    if d == 1:
                        S.op('dve', lambda: V.tensor_tensor(out=t13, in0=bc(cc3[:, :, 63:64], [128, NCH, 64]), in1=cc3, op=ALU.subtract),
                             reads=[('cc', c)], writes=[('t1', c)])
                        S.op('dve', lambda: V.tensor_tensor(out=cc[:, c, :], in0=t1[:, c, :], in1=sg[:, c, :], op=ALU.add),
                             reads=[('t1', c), ('sg', c)], writes=[('cc', c)])
                    totp = 63 if d == 0 else 0
                    S.op('pool', lambda: POOL.tensor_scalar(out=kd[:, c, :], in0=ad[:, c, :], scalar1=col("ka", c), scalar2=oneminus_ka[:, c:c + 1], op0=ALU.mult, op1=ALU.add),
                         reads=[('ad', c), 'pp', 'omka'], writes=[('kd', c)])
                    S.op('pool', lambda: POOL.tensor_tensor(out=kd[:, c, :], in0=kd[:, c, :], in1=kT(c)[:, sl], op=ALU.mult),
                         reads=[('kd', c), ('zs', 4 + c)], writes=[('kd', c)])
                    S.op('pool', lambda: POOL.tensor_tensor(out=bb[:, c, :], in0=ad[:, c, :], in1=kkT[:, c, sl], op=ALU.mult),
                         reads=[('ad', c), ('kk', c)], writes=[('bb', c)])
                    yield
                    e = ex[c][0]
                    S.op('act', lambda: ACT.activation(out=e, in_=cc[:, c, :], func=AF.Exp, scale=-LAM), reads=[('cc', c)], writes=[('ex', c, 0)])
                    for hp in range(2):
                        pr = slice(hp * 64, (hp + 1) * 64)
                        S.op('dve', lambda: V.tensor_tensor(out=ARz[pr, c, :, 0, hp, :], in0=rT(c)[pr, sl].rearrange("p (h t) -> p h t", t=64),
                                                            in1=e[pr, :].rearrange("p (h t) -> p h t", t=64), op=ALU.mult),
                             reads=[('zs', c), ('ex', c, 0)], writes=['ARz'])
                    yield
                    e = ex[c][1]
                    S.op('act', lambda: ACT.activation(out=e, in_=cc[:, c, :], func=AF.Exp, scale=LAM), reads=[('cc', c)], writes=[('ex', c, 1)])
                    S.op('dve', lambda: V.tensor_tensor(out=BKt[:, c, :, 0, :], in0=kd[:, c, :].rearrange("p (h t) -> p h t", t=64),
                                                        in1=e.rearrange("p (h t) -> p h t", t=64), op=ALU.mult),
                         reads=[('kd', c), ('ex', c, 1)], writes=['BKt'])
                    S.op('dve', lambda: V.tensor_tensor(out=BKt[:, c, :, 1, :], in0=bb[:, c, :].rearrange("p (h t) -> p h t", t=64),
                                                        in1=e.rearrange("p (h t) -> p h t", t=64), op=ALU.mult),
                         reads=[('bb', c), ('ex', c, 1)], writes=['BKt'])
                    for hp in range(2):
                        pr = slice(hp * 64, (hp + 1) * 64)
                        S.op('act', lambda: ACT.copy(out=Bz[pr, c, :, hp, :], in_=BKt[pr, c, :, 1, :]), reads=['BKt'], writes=['Bz'])
                    yield
                    S.op('dve', lambda: V.tensor_tensor(out=t1[:, c, :], in0=cc[:, c, :], in1=sg[:, c, :], op=ALU.subtract),
                         reads=[('cc', c), ('sg', c)], writes=[('t1', c)])
                    e = ex[c][0]
                    S.op('act', lambda: ACT.activation(out=e, in_=t1[:, c, :], func=AF.Exp, scale=-LAM), reads=[('t1', c)], writes=[('ex', c, 0)])
                    for hp in range(2):
                        pr = slice(hp * 64, (hp + 1) * 64)
                        S.op('dve', lambda: V.scalar_tensor_tensor(out=ARz[pr, c, :, 1, hp, :], in0=kkT[pr, c, sl].rearrange("p (h t) -> p h t", t=64),
                                                                   scalar=-1.0, in1=e[pr, :].rearrange("p (h t) -> p h t", t=64), op0=ALU.mult, op1=ALU.mult),
                             reads=[('kk', c), ('ex', c, 0)], writes=['ARz'])
                    yield
                    S.op('dve', lambda: V.tensor_tensor(out=t13, in0=bc(cc3[:, :, totp:totp + 1], [128, NCH, 64]), in1=cc3, op=ALU.subtract),
                         reads=[('cc', c)], writes=[('t1', c)])
                    e = ex[c][1]
                    S.op('act', lambda: ACT.activation(out=e, in_=t1[:, c, :], func=AF.Exp, scale=-LAM), reads=[('t1', c)], writes=[('ex', c, 1)])
                    S.op('pool', lambda: POOL.tensor_tensor(out=KBh[:, c, :, 0, :], in0=kd[:, c, :].rearrange("p (h t) -> p h t", t=64),
                                                        in1=e.rearrange("p (h t) -> p h t", t=64), op=ALU.mult),
                         reads=[('kd', c), ('ex', c, 1)], writes=['KBh'])
                    S.op('pool', lambda: POOL.tensor_tensor(out=KBh[:, c, :, 1, :], in0=bb[:, c, :].rearrange("p (h t) -> p h t", t=64),
                                                        in1=e.rearrange("p (h t) -> p h t", t=64), op=ALU.mult),
                         reads=[('bb', c), ('ex', c, 1)], writes=['KBh'])
                    S.op('act', lambda: ACT.activation(out=pdec[:, c, :].rearrange("p (h o) -> p h o", o=1), in_=cc3[:, :, totp:totp + 1], func=AF.Exp, scale=-LAM), reads=[('cc', c)], writes=['pdec'])
                ptasks = [gen_prep(c_) for c_ in range(4)]
                for t_ in ptasks:
                    next(t_)
                if pendQ is not None:
                    for _ in range(3):
                        next(pendQ, None)
                for t_ in ptasks:
                    next(t_)
                if pendQ is not None:
                    run_tasks([pendQ])
                    pendQ = None
                run_tasks(ptasks)
                for ch in range(NCH):
                    pz = ps[4 + ch % 2].bitcast(BF16)
                    pk = psk[4 + ch % 2]
                    pzv = pz[0:64, 0:512].rearrange("p (c n) -> p c n", c=4)
                    for c in range(4):
                        S.op('pe', lambda: PE.transpose(out=pzv[:, c, :], in_=vT(c)[:, tb * TBS + ch * 64: tb * TBS + (ch + 1) * 64], identity=ident),
                             reads=[('zs', 8 + c), 'ident'], writes=[pk], pe_acc=True)
                    S.op('act', lambda: ACT.copy(out=VZ[0:64, ch, :, :].rearrange("p h v -> p (h v)"), in_=pz[0:64, 0:512]), reads=[pk], writes=[('VZ', ch)])
                    S.op('act', lambda: ACT.copy(out=XV[0:64, ch, :, :].rearrange("p h v -> p (h v)"), in_=pz[0:64, 0:512]), reads=[pk], writes=[('XVv', ch)])
                    pzk = pz[:, 512:1024].rearrange("p (c n) -> p c n", c=4)
                    for c in range(4):
                        S.op('pe', lambda: PE.transpose(out=pzk[:, c, :], in_=KBh[:, c, ch, :, :].rearrange("p a t -> p (a t)"), identity=ident),
                             reads=['KBh', 'ident'], writes=[pk], pe_acc=True)
                    S.op('dve', lambda: V.tensor_copy(out=KBt[:, :, ch, :], in_=pzk), reads=[pk], writes=[('KBt', ch)])
                MNT = cmb[:, 11 + d, :]
                MN = cmb[:, 12 - d, :]

                def gen_D(ch, slot, par):
                    pA, pkA = ps[2 * par], psk[2 * par]
                    pB, pkB = ps[2 * par + 1], psk[2 * par + 1]
                    pA3 = pA.rearrange("p (j n) -> p j n", j=4)
                    pB3 = pB.rearrange("p (j n) -> p j n", j=4)
                    mzt = cmb[:, MZT[d], :]
                    for half in range(2):
                        pz3 = pA3 if half == 0 else pB3
                        pkz = pkA if half == 0 else pkB
                        for j in range(4):
                            h = half * 4 + j
                            c, hp = h // 2, h % 2
                            bk = BKt[:, c, ch, :, :].rearrange("p a t -> p (a t)")
                            S.op('pe', lambda: PE.matmul(pz3[:, j, :].rearrange("p (a t) -> p a t", a=2), lhsT=bk, rhs=ARz[:, c, ch, :, hp, :], start=True, stop=True),
                                 reads=['BKt', 'ARz'], writes=[pkz], pe_acc=True)
                        S.op('dve', lambda: V.tensor_tensor(out=ZTs[slot][half], in0=pz3, in1=bc(mzt.rearrange("p (o n) -> p o n", o=1), [128, 4, 128]), op=ALU.mult),
                             reads=[pkz, 'cmb'], writes=[('ZTs', slot, half)])
                    yield
                    for c in range(4):
                        bz = Bz[:, c, ch, :, :].rearrange("p a t -> p (a t)")
                        az = ARz[:, c, ch, 1, :, :].rearrange("p a t -> p (a t)")
                        S.op('pe', lambda: PE.matmul(pA3[:, c, :], lhsT=bz, rhs=az, start=True, stop=True), reads=['Bz', 'ARz'], writes=[pkA], pe_acc=True)
                        S.op('pe', lambda: PE.matmul(pB3[:, c, :], lhsT=az, rhs=bz, start=True, stop=True), reads=['Bz', 'ARz'], writes=[pkB], pe_acc=True)
                    S.op('dve', lambda: V.tensor_tensor(out=PTm[par][0], in0=pA3, in1=bc(MNT.rearrange("p (o n) -> p o n", o=1), [128, 4, 128]), op=ALU.mult),
                         reads=[pkA, 'cmb'], writes=[('PT', par, 0)])
                    S.op('dve', lambda: V.tensor_tensor(out=Pm[par][0], in0=pB3, in1=bc(MN.rearrange("p (o n) -> p o n", o=1), [128, 4, 128]), op=ALU.mult),
                         reads=[pkB, 'cmb'], writes=[('P', par, 0)])
                    S.op('pool', lambda: POOL.tensor_tensor(out=ATm[slot][0], in0=PTm[par][0], in1=ident4, op=ALU.add), reads=[('PT', par, 0), 'ident4'], writes=[('AT', slot, 0)])
                    S.op('pool', lambda: POOL.tensor_tensor(out=Am[par][0], in0=Pm[par][0], in1=ident4, op=ALU.add), reads=[('P', par, 0), 'ident4'], writes=[('A', par, 0)])
                    yield
                    cur = 0
                    for lev in range(1, 6):
                        nxt = 1 - cur
                        for j in range(4):
                            S.op('pe', lambda: PE.matmul(pA3[:, j, :], lhsT=Pm[par][cur][:, j, :], rhs=PTm[par][cur][:, j, :], start=True, stop=True),
                                 reads=[('P', par, cur), ('PT', par, cur)], writes=[pkA], pe_acc=True)
                            if lev < 5:
                                S.op('pe', lambda: PE.matmul(pB3[:, j, :], lhsT=PTm[par][cur][:, j, :], rhs=Pm[par][cur][:, j, :], start=True, stop=True),
                                     reads=[('P', par, cur), ('PT', par, cur)], writes=[pkB], pe_acc=True)
                        S.op('act', lambda: ACT.copy(out=PTm[par][nxt], in_=pA3), reads=[pkA], writes=[('PT', par, nxt)])
                        if lev < 5:
                            S.op('dve', lambda: V.tensor_copy(out=Pm[par][nxt], in_=pB3), reads=[pkB], writes=[('P', par, nxt)])
                        yield
                        for j in range(4):
                            S.op('pe', lambda: PE.matmul(pA3[:, j, :], lhsT=Am[par][cur][:, j, :], rhs=PTm[par][nxt][:, j, :], start=True, stop=True),
                                 reads=[('A', par, cur), ('PT', par, nxt)], writes=[pkA], pe_acc=True)
                            if lev < 5:
                                S.op('pe', lambda: PE.matmul(pB3[:, j, :], lhsT=PTm[par][nxt][:, j, :], rhs=Am[par][cur][:, j, :], start=True, stop=True),
                                     reads=[('A', par, cur), ('PT', par, nxt)], writes=[pkB], pe_acc=True)
                        S.op('dve', lambda: V.tensor_tensor(out=ATm[slot][nxt], in0=pA3, in1=ATm[slot][cur], op=ALU.add), reads=[pkA, ('AT', slot, cur)], writes=[('AT', slot, nxt)])
                        if lev < 5:
                            S.op('act', lambda: ACT.copy(out=Am[par][nxt], in_=pB3), reads=[pkB], writes=[('A', par, nxt)])
                            S.op('pool', lambda: POOL.tensor_tensor(out=Am[par][nxt], in0=Am[par][nxt], in1=Am[par][cur], op=ALU.add),
                                 reads=[('A', par, nxt), ('A', par, cur)], writes=[('A', par, nxt)])
                        yield
                        cur = nxt
                    assert cur == 1

                def gen_Q(ch, slot, pb, tb=tb, d=d):
                    nonlocal yev
                    fin = 1
                    gch = tb * NCH + ch
                    pW, pkW = ps[pb], psk[pb]
                    pW3 = pW[:, 0:256].rearrange("p (c v) -> p c v", c=4)
                    for h in range(8):
                        c, hp = h // 2, h % 2
                        S.op('pe', lambda: PE.matmul(pW3[hp * 64:(hp + 1) * 64, c, :], lhsT=ZTs[slot][h // 4][:, h % 4, 64:128], rhs=VZ[:, ch, h, :], start=True, stop=False),
                             reads=[('ZTs', slot, h // 4), ('VZ', ch)], writes=[pkW], pe_acc=True)
                        S.op('pe', lambda: PE.matmul(pW3[hp * 64:(hp + 1) * 64, c, :], lhsT=ARz[:, c, ch, 1, hp, :], rhs=Sb[d][:, c, :], start=False, stop=True),
                             reads=['ARz', ('Sb', d)], writes=[pkW], pe_acc=True)
                    S.op('act', lambda: ACT.copy(out=W1s, in_=pW3), reads=[pkW], writes=['W1s'])
                    yield
                    pX, pkX = ps[pb + 1], psk[pb + 1]
                    pX3 = pX.rearrange("p (h v) -> p h v", h=8)
                    for h in range(8):
                        c, hp = h // 2, h % 2
                        S.op('pe', lambda: PE.matmul(pX3[64:128, h, :], lhsT=ATm[slot][fin][:, c, hp * 64:(hp + 1) * 64], rhs=W1s[:, c, :], start=True, stop=True),
                             reads=[('AT', slot, fin), 'W1s'], writes=[pkX], pe_acc=True)
                    S.op('dve', lambda: V.tensor_copy(out=XV[64:128, ch, :, :], in_=pX3[64:128]), reads=[pkX], writes=[('XVu', ch)])
                    yield
                    pS, pkS = ps[pb + 3], psk[pb + 3]
                    pS3 = pS[:, 0:256].rearrange("p (c v) -> p c v", c=4)
                    for h in range(8):
                        c, hp = h // 2, h % 2
                        S.op('pe', lambda: PE.matmul(pS3[hp * 64:(hp + 1) * 64, c, :], lhsT=KBt[:, c, ch, hp * 64:(hp + 1) * 64], rhs=XV[:, ch, h, :], start=True, stop=True),
                             reads=[('KBt', ch), ('XVv', ch), ('XVu', ch)], writes=[pkS], pe_acc=True)
                    pY, pkY = ps[pb + 2], psk[pb + 2]
                    pY3 = pY.rearrange("p (h v) -> p h v", h=8)
                    for h in range(8):
                        c, hp = h // 2, h % 2
                        S.op('pe', lambda: PE.matmul(pY3[0:64, h, :], lhsT=ZTs[slot][h // 4][:, h % 4, 0:64], rhs=XV[:, ch, h, :], start=True, stop=False),
                             reads=[('ZTs', slot, h // 4), ('XVv', ch), ('XVu', ch)], writes=[pkY], pe_acc=True)
                        S.op('pe', lambda: PE.matmul(pY3[0:64, h, :], lhsT=ARz[:, c, ch, 0, hp, :], rhs=Sb[d][:, c, :], start=False, stop=True),
                             reads=['ARz', ('Sb', d)], writes=[pkY], pe_acc=True)
                    S.op('dve', lambda: V.tensor_tensor(out=S32[d], in0=S32[d], in1=bc(pdec[:, :, ch:ch + 1], [128, 4, 64]), op=ALU.mult),
                         reads=[('S32', d), 'pdec'], writes=[('S32', d)])
                    S.op('dve', lambda: V.tensor_tensor(out=S32[d], in0=S32[d], in1=pS3, op=ALU.add),
                         reads=[('S32', d), pkS], writes=[('S32', d)])
                    S.op('act', lambda: ACT.copy(out=Sb[d], in_=S32[d]), reads=[('S32', d)], writes=[('Sb', d)])
                    yb_ = ysb[yev % 2]
                    S.op('act', lambda: ACT.copy(out=yb_, in_=pY[0:64, :]), reads=[pkY], writes=[('ysb', yev % 2)])
                    S.dma('sp', y_d[d, gch * 64:(gch + 1) * 64, :], yb_, reads=[('ysb', yev % 2)], writes=[('yscr', d, gch // 2)])
                    yev += 1
                    yield

                chs = list(range(NCH)) if d == 0 else list(range(NCH - 1, -1, -1))
                Ds = [gen_D(chs[i_], i_, i_) for i_ in range(NCH)]
                fast, slow = Ds[0:2], Ds[2:4]
                alive = True
                while alive:
                    alive = False
                    for rep in range(2):
                        for t_ in fast:
                            if next(t_, 'done') != 'done':
                                alive = True
                    for t_ in slow:
                        next(t_, 'done')
                run_tasks([chain([gen_Q(chs[0], 0, 0), gen_Q(chs[1], 1, 0)])] + slow)
                pendQ = chain([gen_Q(chs[2], 2, 4), gen_Q(chs[3], 3, 4)])
        if pendQ is not None:
            run_tasks([pendQ])
            pendQ = None
        S.barrier()


    def phase_post(zsT, yaT, base):
        rT4 = zsT[:, 0:4, :]
        kT4 = zsT[:, 4:8, :]
        adT = zsT[:, 13, :]
        gdT = zsT[:, 14, :]
        AR.seek(base)
        gnwB = AR.alloc([128, 512], F32)
        gnbB = AR.alloc([128, 512], F32)
        P2 = lambda shape, dt: [AR.alloc(shape, dt) for _ in range(2)]
        Yf, Yb = P2([128, 512], F32), P2([128, 512], F32)
        ta0, ta1 = P2([128, 4, 128], F32), P2([128, 4, 128], F32)
        kf2 = P2([128, 4, 128], F32)
        prod2 = P2([128, 4, 128], BF16)
        rows2 = P2([128, 8], F32)
        bon2 = P2([128, 512], F32)
        y2 = P2([128, 512], F32)
        sq2 = P2([128, 512], F32)
        st2 = P2([128, 4, 8], F32)
        yab2 = P2([128, 512], BF16)
        S.dma('sp', gnwB, gnw_d.partition_broadcast(128), writes=['gnwB'])
        S.dma('sp', gnbB, gnb_d.partition_broadcast(128), writes=['gnbB'])
        def gen_tile(i):
            b = i % 2
            sl = slice(i * 128, (i + 1) * 128)
            ta = (ta0[b], ta1[b])
            kf, prod, rows, bon, y, sq, st, yab = kf2[b], prod2[b], rows2[b], bon2[b], y2[b], sq2[b], st2[b], yab2[b]
            bA, bB, bC, bD = 4 * b, 4 * b + 1, 4 * b + 2, 4 * b + 3
            S.dma('sp', Yf[b], y_d[0, sl, :], writes=[('Yf', b)])
            S.dma('sp', Yb[b], y_d[1, sl, :], writes=[('Yb', b)])
            for d in range(2):
                bk_ = bA if d == 0 else bB
                pz3 = ps[bk_].rearrange("p (c n) -> p c n", c=4)
                for c in range(4):
                    S.op('pe', lambda: PE.matmul(pz3[:, c, :], lhsT=a2c[:, d, c * 128:(c + 1) * 128], rhs=adT[:, sl], start=True, stop=True),
                         reads=['a2c'], writes=[psk[bk_]], pe_acc=True)
                for c in range(4):
                    S.op('act', lambda: ACT.activation(out=ta[d][:, c, :], in_=pz3[:, c, :], func=AF.Sigmoid, bias=col("a0", d * 4 + c), scale=1.0),
                         reads=[psk[bk_], 'pp'], writes=[('ta', b, d)])
            yield
            S.op('pool', lambda: POOL.tensor_tensor(out=ta[0], in0=ta[0], in1=ta[1], op=ALU.add), reads=[('ta', b, 0), ('ta', b, 1)], writes=[('ta', b, 0)])
            for c in range(4):
                S.op('pool', lambda: POOL.tensor_scalar(out=kf[:, c, :], in0=ta[0][:, c, :], scalar1=kar[:, c:c + 1], scalar2=c2r[:, c:c + 1], op0=ALU.mult, op1=ALU.add),
                     reads=[('ta', b, 0), 'kar'], writes=[('kf', b)])
            S.op('pool', lambda: POOL.tensor_tensor(out=kf, in0=kf, in1=kT4[:, :, sl], op=ALU.mult), reads=[('kf', b)], writes=[('kf', b)])
            S.op('pool', lambda: POOL.tensor_tensor(out=prod, in0=kf, in1=rT4[:, :, sl], op=ALU.mult), reads=[('kf', b)], writes=[('prod', b)])
            yield
            pr = ps[bB]
            for c in range(4):
                S.op('pe', lambda: PE.matmul(pr[:, c * 2:(c + 1) * 2], lhsT=prod[:, c, :], rhs=cmb[:, HSEL, 0:2], start=True, stop=True),
                     reads=[('prod', b), 'cmb'], writes=[psk[bB]], pe_acc=True)
            S.op('act', lambda: ACT.copy(out=rows, in_=pr[:, 0:8]), reads=[psk[bB]], writes=[('rows', b)])
            yield
            pv = ps[bD].bitcast(BF16)[:, 0:512]
            for c in range(4):
                S.op('pe', lambda: PE.transpose(out=pv[:, c * 128:(c + 1) * 128], in_=zsT[:, 8 + c, sl], identity=ident),
                     reads=['ident'], writes=[psk[bD]], pe_acc=True)
            S.op('dve', lambda: V.tensor_tensor(out=bon.rearrange("p (h v) -> p h v", h=8), in0=pv.rearrange("p (h v) -> p h v", h=8),
                                                in1=bc(rows.rearrange("p (h o) -> p h o", o=1), [128, 8, 64]), op=ALU.mult),
                 reads=[psk[bD], ('rows', b)], writes=[('bon', b)])
            yield
            pg = ps[bC]
            S.op('pe', lambda: PE.matmul(pg, lhsT=gdT[:, sl], rhs=g2, start=True, stop=True), reads=['g2'], writes=[psk[bC]], pe_acc=True)
            y3 = y.rearrange("p (h v) -> p h v", h=8)
            sq3 = sq.rearrange("p (h v) -> p h v", h=8)
            S.op('dve', lambda: V.tensor_tensor(out=y, in0=Yf[b], in1=Yb[b], op=ALU.add), reads=[('Yf', b), ('Yb', b)], writes=[('y', b)])
            yield
            S.op('dve', lambda: V.tensor_reduce(out=st[:, 0, :], in_=y3, axis=AX.X, op=ALU.add), reads=[('y', b)], writes=[('st0', b)])
            S.op('dve', lambda: V.tensor_scalar(out=st[:, 1, :], in0=st[:, 0, :], scalar1=-1.0 / 64, scalar2=None, op0=ALU.mult), reads=[('st0', b)], writes=[('st1', b)])
            S.op('dve', lambda: V.tensor_tensor(out=y3, in0=y3, in1=bc(st[:, 1, :].rearrange("p (h o) -> p h o", o=1), [128, 8, 64]), op=ALU.add),
                 reads=[('y', b), ('st1', b)], writes=[('y', b)])
            yield
            S.op('act', lambda: ACT.activation(out=sq, in_=y, func=AF.Square), reads=[('y', b)], writes=[('sq', b)])
            S.op('dve', lambda: V.tensor_reduce(out=st[:, 2, :], in_=sq3, axis=AX.X, op=ALU.add), reads=[('sq', b)], writes=[('st2', b)])
            yield
            S.op('act', lambda: ACT.activation(out=st[:, 3, :], in_=st[:, 2, :], func=AF.Sqrt, bias=epsc[:, 1:2], scale=1.0 / 64), reads=[('st2', b), 'epsc'], writes=[('st3', b)])
            S.op('dve', lambda: V.reciprocal(out=st[:, 3, :], in_=st[:, 3, :]), reads=[('st3', b)], writes=[('st3', b)])
            S.op('dve', lambda: V.tensor_tensor(out=y3, in0=y3, in1=bc(st[:, 3, :].rearrange("p (h o) -> p h o", o=1), [128, 8, 64]), op=ALU.mult),
                 reads=[('y', b), ('st3', b)], writes=[('y', b)])
            yield
            S.op('dve', lambda: V.tensor_tensor(out=y, in0=y, in1=gnwB, op=ALU.mult), reads=[('y', b), 'gnwB'], writes=[('y', b)])
            S.op('pool', lambda: POOL.tensor_tensor(out=bon, in0=bon, in1=gnbB, op=ALU.add), reads=[('bon', b), 'gnbB'], writes=[('bon', b)])
            S.op('dve', lambda: V.tensor_tensor(out=y, in0=y, in1=bon, op=ALU.add), reads=[('y', b), ('bon', b)], writes=[('y', b)])
            S.op('dve', lambda: V.tensor_tensor(out=yab, in0=y, in1=pg, op=ALU.mult), reads=[('y', b), psk[bC]], writes=[('yab', b)])
            yield
            pt = ps[bA].bitcast(BF16)[:, 0:512]
            for c in range(4):
                S.op('pe', lambda: PE.transpose(out=pt[:, c * 128:(c + 1) * 128], in_=yab[:, c * 128:(c + 1) * 128], identity=ident),
                     reads=[('yab', b), 'ident'], writes=[psk[bA]], pe_acc=True)
            S.op('act', lambda: ACT.copy(out=yaT[:, :, sl], in_=pt.rearrange("p (c n) -> p c n", c=4)), reads=[psk[bA]], writes=['yaT'])
            yield

        def run_tasks(tasks):
            tasks = list(tasks)
            while tasks:
                for t_ in list(tasks):
                    try:
                        next(t_)
                    except StopIteration:
                        tasks.remove(t_)

        for i in range(0, NT, 2):
            run_tasks([gen_tile(i), gen_tile(i + 1)])

    def phase_attn(s, uT, ybT, baseA, baseB):
        AR.seek(baseA)
        cosT = AR.alloc([128, T], F32)
        sinT = AR.alloc([128, T], F32)
        qT = AR.alloc([128, 4, T], BF16)
        kTt = AR.alloc([128, T], BF16)
        vp = AR.alloc([128, 2, NT, 128], BF16)
        AR.seek(baseB)
        wq = [AR.alloc([128, 8, 128], BF16) for _ in range(2)]
        qfL = [AR.alloc([128, 512], F32) for _ in range(2)]
        sqbL = [AR.alloc([128, 512], BF16) for _ in range(2)]
        rsL = [AR.alloc([128, 512], F32) for _ in range(2)]
        qnL = [AR.alloc([128, 512], F32) for _ in range(2)]
        qnbL = [AR.alloc([128, 512], BF16) for _ in range(2)]
        t1L = [AR.alloc([128, 512], F32) for _ in range(2)]
        t2L = [AR.alloc([128, 512], F32) for _ in range(2)]
        pTs = [AR.alloc([128, 512], BF16) for _ in range(6)]
        dn = AR.alloc([128, 512], F32)
        posi = AR.alloc([128, T], I32)
        ang = AR.alloc([128, T], F32)
        ki = AR.alloc([128, T], I32)
        kf = AR.alloc([128, T], F32)
        m1 = AR.alloc([128, T], F32)
        S.dma('sp', posi, pos_d[s].partition_broadcast(128), writes=['posi'])

        def table(dst, shift):
            S.op('dve', lambda: V.tensor_copy(out=ang, in_=posi), reads=['posi'], writes=['ang'])
            S.op('dve', lambda: V.tensor_scalar(out=ang, in0=ang, scalar1=col("invf"), scalar2=shift, op0=ALU.mult, op1=ALU.add), reads=['ang', 'pp'], writes=['ang'])
            S.op('dve', lambda: V.tensor_scalar(out=ki, in0=ang, scalar1=1.0 / TWO_PI, scalar2=None, op0=ALU.mult), reads=['ang'], writes=['ki'])
            S.op('pool', lambda: POOL.tensor_copy(out=kf, in_=ki), reads=['ki'], writes=['kf'])
            S.op('dve', lambda: V.scalar_tensor_tensor(out=ang, in0=kf, scalar=-C1, in1=ang, op0=ALU.mult, op1=ALU.add), reads=['kf', 'ang'], writes=['ang'])
            S.op('dve', lambda: V.scalar_tensor_tensor(out=ang, in0=kf, scalar=-C2, in1=ang, op0=ALU.mult, op1=ALU.add), reads=['kf', 'ang'], writes=['ang'])
            S.op('dve', lambda: V.tensor_scalar(out=m1, in0=ang, scalar1=float(np.pi), scalar2=-TWO_PI, op0=ALU.is_gt, op1=ALU.mult), reads=['ang'], writes=['m1'])
            S.op('pool', lambda: POOL.tensor_tensor(out=ang, in0=ang, in1=m1, op=ALU.add), reads=['ang', 'm1'], writes=['ang'])
            S.op('dve', lambda: V.tensor_scalar(out=m1, in0=ang, scalar1=float(-np.pi), scalar2=TWO_PI, op0=ALU.is_lt, op1=ALU.mult), reads=['ang'], writes=['m1'])
            S.op('pool', lambda: POOL.tensor_tensor(out=ang, in0=ang, in1=m1, op=ALU.add), reads=['ang', 'm1'], writes=['ang'])
            S.op('act', lambda: ACT.activation(out=dst, in_=ang, func=AF.Sin), reads=['ang'], writes=['tab'])

        table(sinT, 0.0)
        table(cosT, float(np.pi / 2))
        def c0_of(c):
            return 1920 + c * 128 if c < 4 else 2432

        def gen_qk(c, tb, L):
            b = c % 2
            gcol = col("qg") if c < 4 else col("kg")
            sl = slice(tb * 512, (tb + 1) * 512)
            qf_, sqb_, rs_, qn_, qnb_, t1_, t2_ = qfL[L], sqbL[L], rsL[L], qnL[L], qnbL[L], t1L[L], t2L[L]
            pz, pk = ps[L], psk[L]
            for k in range(8):
                S.op('pe', lambda: PE.matmul(pz, lhsT=wq[b][:, k, :], rhs=uT[:, k, sl], start=(k == 0), stop=(k == 7)),
                     reads=[('wq', b), ('uT', tb)], writes=[pk], pe_acc=True)
            S.op('act', lambda: ACT.copy(out=qf_, in_=pz), reads=[pk], writes=[('qf', L)])
            S.op('act', lambda: ACT.activation(out=sqb_, in_=qf_, func=AF.Square), reads=[('qf', L)], writes=[('sqb', L)])
            yield
            pr, pkr = ps[2 + L], psk[2 + L]
            S.op('pe', lambda: PE.matmul(pr, lhsT=cmb[:, BLK1, :], rhs=sqb_, start=True, stop=True), reads=[('sqb', L), 'cmb'], writes=[pkr], pe_acc=True)
            S.op('act', lambda: ACT.activation(out=rs_, in_=pr, func=AF.Sqrt, bias=epsc[:, 0:1], scale=1.0 / 64), reads=[pkr, 'epsc'], writes=[('rs', L)])
            yield
            S.op('dve', lambda: V.reciprocal(out=rs_, in_=rs_), reads=[('rs', L)], writes=[('rs', L)])
            S.op('dve', lambda: V.scalar_tensor_tensor(out=qn_, in0=qf_, scalar=gcol, in1=rs_, op0=ALU.mult, op1=ALU.mult), reads=[('qf', L), ('rs', L), 'pp'], writes=[('qn', L)])
            S.op('act', lambda: ACT.copy(out=qnb_, in_=qn_), reads=[('qn', L)], writes=[('qnb', L)])
            yield
            pro, pkro = ps[4 + L], psk[4 + L]
            S.op('pe', lambda: PE.matmul(pro, lhsT=cmb[:, ROT, :], rhs=qnb_, start=True, stop=True), reads=[('qnb', L), 'cmb'], writes=[pkro], pe_acc=True)
            S.op('pool', lambda: POOL.tensor_tensor(out=t1_, in0=qn_, in1=cosT[:, sl], op=ALU.mult), reads=[('qn', L), 'tab'], writes=[('t1', L)])
            yield
            S.op('dve', lambda: V.tensor_tensor(out=t2_, in0=pro, in1=sinT[:, sl], op=ALU.mult), reads=[pkro, 'tab'], writes=[('t2', L)])
            dst = qT[:, c, sl] if c < 4 else kTt[:, sl]
            S.op('dve', lambda: V.tensor_tensor(out=dst, in0=t1_, in1=t2_, op=ALU.add), reads=[('t1', L), ('t2', L)], writes=['qk'])
            yield

        def run_tasks(tasks):
            tasks = list(tasks)
            while tasks:
                for t_ in list(tasks):
                    try:
                        next(t_)
                    except StopIteration:
                        tasks.remove(t_)

        S.dma('pool', wq[0], win_d[:, c0_of(0):c0_of(0) + 128].rearrange("(k p) n -> p k n", p=128), writes=[('wq', 0)])
        for c in range(5):
            if c + 1 < 5:
                S.dma('pool', wq[(c + 1) % 2], win_d[:, c0_of(c + 1):c0_of(c + 1) + 128].rearrange("(k p) n -> p k n", p=128), writes=[('wq', (c + 1) % 2)])
            for tb in range(0, NB, 2):
                run_tasks([gen_qk(c, tb, 0), gen_qk(c, tb + 1, 1)])
        S.op('pool', lambda: POOL.memset(vp.rearrange("p a b c -> p (a b c)"), 0.0), writes=['vp'])
        S.dma('pool', wq[0], win_d[:, 2560:2688].rearrange("(k p) n -> p k n", p=128), writes=[('wq', 0)])
        for i in range(NT):
            pz, pk = ps[i % 2], psk[i % 2]
            for k in range(8):
                S.op('pe', lambda: PE.matmul(pz[:, 0:128], lhsT=uT[:, k, i * 128:(i + 1) * 128], rhs=wq[0][:, k, :], start=(k == 0), stop=(k == 7)),
                     reads=[('wq', 0), ('uT', i // 4)], writes=[pk], pe_acc=True)
            S.op('act', lambda: ACT.copy(out=vp[:, 0, i, 0:64], in_=pz[:, 0:64]), reads=[pk], writes=['vp'])
            S.op('dve', lambda: V.tensor_copy(out=vp[:, 1, i, 64:128], in_=pz[:, 64:128]), reads=[pk], writes=['vp'])
        for n in range(NT):
            qs = slice(n * 128, (n + 1) * 128)
            kbs = [kb for kb in (n - 1, n, n + 1) if 0 <= kb < NT]
            items = [(g, kb) for g in range(2) for kb in kbs]
            for idx, (g, kb) in enumerate(items):
                gp = slice(g * 64, (g + 1) * 64)
                pz, pk = ps[idx % 4], psk[idx % 4]
                S.op('pe', lambda: PE.matmul(pz.rearrange("p (j q) -> p j q", j=4), lhsT=kTt[gp, kb * 128:(kb + 1) * 128], rhs=qT[gp, :, qs], start=True, stop=True),
                     reads=['qk'], writes=[pk], pe_acc=True)
                pt_ = pTs[idx]
                S.op('act', lambda: ACT.activation(out=pt_, in_=pz, func=AF.Exp, scale=0.125), reads=[pk], writes=[('pT', idx)])
                if kb != n:
                    mk = cmb[:, MPREV if kb < n else MNEXT, :]
                    S.op('pool', lambda: POOL.tensor_tensor(out=pt_.rearrange("p (j q) -> p j q", j=4), in0=pt_.rearrange("p (j q) -> p j q", j=4),
                                                            in1=bc(mk.rearrange("p (o q) -> p o q", o=1), [128, 4, 128]), op=ALU.mult),
                         reads=[('pT', idx), 'cmb'], writes=[('pT', idx)])
            po, pko = ps[4 + n % 2], psk[4 + n % 2]
            pd_, pkd = ps[6 + n % 2], psk[6 + n % 2]
            for idx, (g, kb) in enumerate(items):
                S.op('pe', lambda: PE.matmul(po, lhsT=vp[:, g, kb, :], rhs=pTs[idx], start=(idx == 0), stop=(idx == len(items) - 1)),
                     reads=['vp', ('pT', idx)], writes=[pko], pe_acc=True)
            for idx, (g, kb) in enumerate(items):
                S.op('pe', lambda: PE.matmul(pd_, lhsT=cmb[:, VP[g], :], rhs=pTs[idx], start=(idx == 0), stop=(idx == len(items) - 1)),
                     reads=['cmb', ('pT', idx)], writes=[pkd], pe_acc=True)
            S.op('dve', lambda: V.tensor_tensor(out=dn.rearrange("p (j q) -> p j q", j=4), in0=pd_.rearrange("p (j q) -> p j q", j=4),
                                                in1=bc(esk.rearrange("p (j o) -> p j o", o=1), [128, 4, 128]), op=ALU.add), reads=[pkd, 'esk'], writes=['dn'])
            S.op('dve', lambda: V.reciprocal(out=dn, in_=dn), reads=['dn'], writes=['dn'])
            S.op('dve', lambda: V.tensor_tensor(out=ybT[:, :, qs], in0=po.rearrange("p (j q) -> p j q", j=4), in1=dn.rearrange("p (j q) -> p j q", j=4), op=ALU.mult),
                 reads=[pko, 'dn'], writes=['ybT'])

    def phase_merge(uT, yaT, ybT, mergedT, offs):
        AR.seek(offs[0])
        prw = AR.alloc([128, 4, D], BF16)
        AR.seek(offs[1])
        pat = AR.alloc([128, 4, D], BF16)
        wga = [AR.alloc([128, 8, 128], BF16) for _ in range(2)]
        wgb = [AR.alloc([128, 8, 128], BF16) for _ in range(2)]
        sgaP = [AR.alloc([128, 512], BF16) for _ in range(2)]
        sgbP = [AR.alloc([128, 512], BF16) for _ in range(2)]
        t1P = [AR.alloc([128, 512], F32) for _ in range(2)]
        t2P = [AR.alloc([128, 512], F32) for _ in range(2)]
        for hh in range(2):
            S.dma('pool', prw[:, hh * 2:(hh + 1) * 2, :], prw_d[hh * 256:(hh + 1) * 256, :].rearrange("(k p) n -> p k n", p=128), writes=['prw'])
            S.dma('pool', pat[:, hh * 2:(hh + 1) * 2, :], pat_d[hh * 256:(hh + 1) * 256, :].rearrange("(k p) n -> p k n", p=128), writes=['pat'])
        for oc in range(8):
            b = oc % 2
            S.dma('pool', wga[b], win_d[:, 2688 + oc * 128:2688 + (oc + 1) * 128].rearrange("(k p) n -> p k n", p=128), writes=[('wga', b)])
            S.dma('pool', wgb[b], win_d[:, 3712 + oc * 128:3712 + (oc + 1) * 128].rearrange("(k p) n -> p k n", p=128), writes=[('wgb', b)])
            for tb in range(NB):
                sl = slice(tb * 512, (tb + 1) * 512)
                L = tb % 2
                sga, sgb, t1, t2 = sgaP[L], sgbP[L], t1P[L], t2P[L]
                for k in range(8):
                    S.op('pe', lambda: PE.matmul(ps[0 + 4 * L], lhsT=wga[b][:, k, :], rhs=uT[:, k, sl], start=(k == 0), stop=(k == 7)),
                         reads=[('wga', b), ('uT', tb)], writes=[psk[0 + 4 * L]], pe_acc=True)
                S.op('act', lambda: ACT.activation(out=sga, in_=ps[0 + 4 * L], func=AF.Sigmoid), reads=[psk[0 + 4 * L]], writes=[('sga', L)])
                for k in range(8):
                    S.op('pe', lambda: PE.matmul(ps[1 + 4 * L], lhsT=wgb[b][:, k, :], rhs=uT[:, k, sl], start=(k == 0), stop=(k == 7)),
                         reads=[('wgb', b), ('uT', tb)], writes=[psk[1 + 4 * L]], pe_acc=True)
                S.op('act', lambda: ACT.activation(out=sgb, in_=ps[1 + 4 * L], func=AF.Sigmoid), reads=[psk[1 + 4 * L]], writes=[('sgb', L)])
                for k in range(4):
                    S.op('pe', lambda: PE.matmul(ps[2 + 4 * L], lhsT=prw[:, k, oc * 128:(oc + 1) * 128], rhs=yaT[:, k, sl], start=(k == 0), stop=(k == 3)),
                         reads=['prw', 'yaT'], writes=[psk[2 + 4 * L]], pe_acc=True)
                for k in range(4):
                    S.op('pe', lambda: PE.matmul(ps[3 + 4 * L], lhsT=pat[:, k, oc * 128:(oc + 1) * 128], rhs=ybT[:, k, sl], start=(k == 0), stop=(k == 3)),
                         reads=['pat', 'ybT'], writes=[psk[3 + 4 * L]], pe_acc=True)
                S.op('dve', lambda: V.tensor_tensor(out=t1, in0=ps[2 + 4 * L], in1=sga, op=ALU.mult), reads=[psk[2 + 4 * L], ('sga', L)], writes=[('t1', L)])
                S.op('dve', lambda: V.tensor_tensor(out=t2, in0=ps[3 + 4 * L], in1=sgb, op=ALU.mult), reads=[psk[3 + 4 * L], ('sgb', L)], writes=[('t2', L)])
                S.op('pool', lambda: POOL.tensor_tensor(out=mergedT[:, oc, sl], in0=t1, in1=t2, op=ALU.add), reads=[('t1', L), ('t2', L)], writes=[('mg', tb)])

    def phase_x1(s, mergedT, u2tm, base):
        AR.seek(base)
        wo = AR.alloc([128, 8, D], BF16)
        gt1B = AR.alloc([128, D], F32)
        sc2 = AR.alloc([128, D], F32)
        sh2 = AR.alloc([128, D], F32)
        xt = [AR.alloc([128, D], F32) for _ in range(2)]
        x1t = [AR.alloc([128, D], F32) for _ in range(2)]
        tmpP = [AR.alloc([128, D], F32) for _ in range(2)]
        junkP = [AR.alloc([128, D], BF16) for _ in range(2)]
        u2TP = [AR.alloc([128, 8, 128], BF16) for _ in range(2)]
        ssP = [AR.alloc([128, 8], F32) for _ in range(2)]
        exP = [AR.alloc([128, E], F32) for _ in range(2)]
        for hh in range(4):
            S.dma('pool', wo[:, hh * 2:(hh + 1) * 2, :], wout_d[hh * 256:(hh + 1) * 256, :].rearrange("(k p) n -> p k n", p=128), writes=['wo'])
        S.dma('sp', gt1B, mod_d[s, 2], writes=['gt1B'])
        S.dma('sp', sc2, mod_d[s, 4], writes=['sc2'])
        S.dma('sp', sh2, mod_d[s, 3], writes=['sh2'])
        lg = AR.alloc([128, NT, E], F32)
        mxs = AR.alloc([128, 3, NT], F32)

        def gen_x1(i):
            b = i % 2
            sl = slice(i * 128, (i + 1) * 128)
            S.dma('sp', xt[b], x_d[s, sl, :], writes=[('xt', b)])
            tmp, junk, u2T, ss = tmpP[b], junkP[b], u2TP[b], ssP[b]
            for cb in range(2):
                for k in range(8):
                    S.op('pe', lambda: PE.matmul(ps[cb + 6 * b], lhsT=mergedT[:, k, sl], rhs=wo[:, k, cb * 512:(cb + 1) * 512], start=(k == 0), stop=(k == 7)),
                         reads=[('mg', i // 4), 'wo'], writes=[psk[cb + 6 * b]], pe_acc=True)
                S.op('dve', lambda: V.tensor_tensor(out=tmp[:, cb * 512:(cb + 1) * 512], in0=ps[cb + 6 * b], in1=gt1B[:, cb * 512:(cb + 1) * 512], op=ALU.mult),
                     reads=[psk[cb + 6 * b], 'gt1B'], writes=[('tmp', b, cb)])
            yield
            S.op('pool', lambda: POOL.tensor_tensor(out=x1t[b], in0=tmp, in1=xt[b], op=ALU.add), reads=[('tmp', b, 0), ('tmp', b, 1), ('xt', b)], writes=[('x1t', b)])
            S.dma('sp', out_d[s, sl, :], x1t[b], reads=[('x1t', b)], writes=[('outd', i)])
            S.op('act', lambda: ACT.activation(out=junk, in_=x1t[b], func=AF.Square, accum_out=ss[:, 0:1]), reads=[('x1t', b)], writes=[('junk', b), ('ss0', b)])
            yield
            S.op('act', lambda: ACT.activation(out=ss[:, 1:2], in_=ss[:, 0:1], func=AF.Sqrt, bias=epsc[:, 0:1], scale=1.0 / D), reads=[('ss0', b), 'epsc'], writes=[('ss1', b)])
            S.op('dve', lambda: V.reciprocal(out=ss[:, 1:2], in_=ss[:, 1:2]), reads=[('ss1', b)], writes=[('ss1', b)])
            S.op('dve', lambda: V.scalar_tensor_tensor(out=tmp, in0=x1t[b], scalar=ss[:, 1:2], in1=sc2, op0=ALU.mult, op1=ALU.mult),
                 reads=[('x1t', b), ('ss1', b), 'sc2'], writes=[('tmp', b, 0), ('tmp', b, 1)])
            yield
            S.op('pool', lambda: POOL.tensor_tensor(out=u2tm[:, i, :], in0=tmp, in1=sh2, op=ALU.add), reads=[('tmp', b, 0), ('tmp', b, 1), 'sh2'], writes=[('u2', i)])
            pz = ps[2 + b].bitcast(BF16).rearrange("p (k t) -> p k t", k=8)
            pk = psk[2 + b]
            for k in range(8):
                S.op('pe', lambda: PE.transpose(out=pz[:, k, :], in_=u2tm[:, i, k * 128:(k + 1) * 128], identity=ident),
                     reads=[('u2', i), 'ident'], writes=[pk], pe_acc=True)
            yield
            S.op('act', lambda: ACT.copy(out=u2T, in_=pz), reads=[pk], writes=[('u2T', b)])
            pl, pkl = ps[4 + b], psk[4 + b]
            for k in range(8):
                S.op('pe', lambda: PE.matmul(pl[:, 0:E], lhsT=u2T[:, k, :], rhs=wr[:, k, :], start=(k == 0), stop=(k == 7)),
                     reads=[('u2T', b), 'wr'], writes=[pkl], pe_acc=True)
            yield
            S.op('act', lambda: ACT.copy(out=lg[:, i, :], in_=pl[:, 0:E]), reads=[pkl], writes=['lg'])
            yield

        def run_tasks(tasks):
            tasks = list(tasks)
            while tasks:
                for t_ in list(tasks):
                    try:
                        next(t_)
                    except StopIteration:
                        tasks.remove(t_)

        for i in range(0, NT, 2):
            run_tasks([gen_x1(i), gen_x1(i + 1)])
        S.op('dve', lambda: V.tensor_reduce(out=mxs[:, 0, :], in_=lg, axis=AX.X, op=ALU.max), reads=['lg'], writes=['mx0'])
        S.op('dve', lambda: V.tensor_tensor(out=lg, in0=lg, in1=bc(mxs[:, 0, :].rearrange("p (i o) -> p i o", o=1), [128, NT, E]), op=ALU.subtract),
             reads=['lg', 'mx0'], writes=['lg'])
        S.op('act', lambda: ACT.activation(out=lg, in_=lg, func=AF.Exp), reads=['lg'], writes=['lg'])
        S.op('dve', lambda: V.tensor_reduce(out=mxs[:, 1, :], in_=lg, axis=AX.X, op=ALU.add), reads=['lg'], writes=['mx1'])
        S.op('dve', lambda: V.reciprocal(out=mxs[:, 2, :], in_=mxs[:, 1, :]), reads=['mx1'], writes=['mx2'])
        S.op('dve', lambda: V.tensor_tensor(out=afftm, in0=lg, in1=bc(mxs[:, 2, :].rearrange("p (i o) -> p i o", o=1), [128, NT, E]), op=ALU.mult),
             reads=['lg', 'mx2'], writes=['afftm'])

    def phase_moe(s, u2tm, base):
        AR.seek(base + E * 2 * D * 2)
        for wsrc in (wg_d, wu_d, wd_d):
            wt0 = AR.alloc([128, 8, D], BF16)
            for hh in range(4):
                S.dma('pool', wt0[:, hh * 2:(hh + 1) * 2, :], wsrc[0, hh * 256:(hh + 1) * 256, :].rearrange("(k p) n -> p k n", p=128), writes=[('Wpre', hh)])
        AR.seek(base)
        affT = AR.alloc([16, T], F32)
        work = AR.alloc([16, T], F32)
        maskT = AR.alloc([16, T], F32)
        slotT = AR.alloc([16, T], F32)
        mx8 = AR.alloc([16, 8], F32)
        for i in range(NT):
            pz = ps[i // 4]
            S.op('pe', lambda: PE.transpose(out=pz[0:16, (i % 4) * 128:(i % 4 + 1) * 128], in_=afftm[:, i, :], identity=identf),
                 reads=['afftm', 'identf'], writes=[psk[i // 4]], pe_acc=True)
        for q in range(4):
            S.op('act', lambda: ACT.copy(out=affT[:, q * 512:(q + 1) * 512], in_=ps[q][0:16, :]), reads=[psk[q]], writes=['affT'])
        S.op('dve', lambda: V.tensor_copy(out=work, in_=affT), reads=['affT'], writes=['work'])
        for it in range(CAP // 8):
            S.op('dve', lambda: V.max(out=mx8, in_=work), reads=['work'], writes=['mx8'])
            if it < CAP // 8 - 1:
                S.op('dve', lambda: V.match_replace(out=work, in_to_replace=mx8, in_values=work, imm_value=-1.0), reads=['work', 'mx8'], writes=['work'])
        S.op('dve', lambda: V.tensor_scalar(out=maskT, in0=affT, scalar1=mx8[:, 7:8], scalar2=None, op0=ALU.is_ge), reads=['affT', 'mx8'], writes=['maskT'])
        S.op('pool', lambda: POOL.memset(work, 1.0), reads=['work'], writes=['work'])
        S.op('dve', lambda: V.tensor_tensor_scan(out=slotT, data0=work, data1=maskT, initial=0.0, op0=ALU.mult, op1=ALU.add), reads=['work', 'maskT'], writes=['slotT'])
        S.op('dve', lambda: V.tensor_tensor(out=slotT, in0=slotT, in1=maskT, op=ALU.mult), reads=['slotT', 'maskT'], writes=['slotT'])
        S.op('dve', lambda: V.tensor_scalar(out=slotT, in0=slotT, scalar1=-1.0, scalar2=None, op0=ALU.add), reads=['slotT'], writes=['slotT'])
        pz = ps[4]
        for i in range(NT):
            S.op('pe', lambda: PE.transpose(out=pz[:, i * 16:(i + 1) * 16], in_=slotT[:, i * 128:(i + 1) * 128], identity=identf[0:16, 0:16]),
                 reads=['slotT', 'identf'], writes=[psk[4]], pe_acc=True)
        S.op('act', lambda: ACT.copy(out=slot_tm.rearrange("p i e -> p (i e)"), in_=pz[:, 0:256]), reads=[psk[4]], writes=['slot_tm'])
        S.op('dve', lambda: V.tensor_copy(out=affhl[:, :, :, 0], in_=afftm), reads=['afftm'], writes=['affhl'])
        S.op('dve', lambda: V.tensor_tensor(out=affhl[:, :, :, 1], in0=afftm, in1=affhl[:, :, :, 0], op=ALU.subtract), reads=['afftm', 'affhl'], writes=['affhl'])
        S.barrier()
        AR.seek(base)
        ye = AR.alloc([128, E, 2, D], BF16)
        Wg = AR.alloc([128, 8, D], BF16)
        Wu = AR.alloc([128, 8, D], BF16)
        Wd = AR.alloc([128, 8, D], BF16)
        wbase = AR.ptr
        Pe = AR.alloc([128, NT, CAP], BF16)
        xeT = AR.alloc([128, 8, CAP], BF16)
        hT = AR.alloc([128, 8, CAP], BF16)
        hs = AR.alloc([128, CAP], F32)
        affs = AR.alloc([128, 4], F32)
        gt2B = AR.alloc([128, D], F32)
        S.dma('sp', gt2B, mod_d[s, 5], writes=['gt2B'])
        for e in range(E):
            for (wt, wsrc, nm) in ((Wg, wg_d, 'Wg'), (Wu, wu_d, 'Wu'), (Wd, wd_d, 'Wd')):
                if e == 0:
                    continue
                for hh in range(4):
                    S.dma('pool', wt[:, hh * 2:(hh + 1) * 2, :], wsrc[e, hh * 256:(hh + 1) * 256, :].rearrange("(k p) n -> p k n", p=128), writes=[(nm, hh)])
            for i in range(NT):
                S.op('dve', lambda: V.tensor_scalar(out=Pe[:, i, :], in0=iota_row, scalar1=slot_tm[:, i, e:e + 1], scalar2=None, op0=ALU.is_equal),
                     reads=['iota_row', 'slot_tm'], writes=[('Pe', i)])
            for fc in range(8):
                pz, pk = ps[fc // 2], psk[fc // 2]
                pzs = pz[:, (fc % 2) * 256:(fc % 2 + 1) * 256]
                for i in range(NT):
                    S.op('pe', lambda: PE.matmul(pzs, lhsT=u2tm[:, i, fc * 128:(fc + 1) * 128], rhs=Pe[:, i, :], start=(i == 0), stop=(i == NT - 1)),
                         reads=[('u2', i), ('Pe', i)], writes=[pk], pe_acc=True)
                S.op('act', lambda: ACT.copy(out=xeT[:, fc, :], in_=pzs), reads=[pk], writes=[('xeT', fc)])
            pa, pka = ps[4], psk[4]
            for half in range(2):
                for i in range(NT):
                    S.op('pe', lambda: PE.matmul(pa[:, half * 2:(half + 1) * 2], lhsT=Pe[:, i, half * 128:(half + 1) * 128], rhs=affhl[:, i, e, :], start=(i == 0), stop=(i == NT - 1)),
                         reads=[('Pe', i), 'affhl'], writes=[pka], pe_acc=True)
            S.op('dve', lambda: V.tensor_reduce(out=affs[:, 0:2], in_=pa[:, 0:4].rearrange("p (h t) -> p h t", t=2), axis=AX.X, op=ALU.add), reads=[pka], writes=['affs'])
            for fk in range(8):
                pg, pkg = ps[5], psk[5]
                pu, pku = ps[6], psk[6]
                for k in range(8):
                    S.op('pe', lambda: PE.matmul(pg[:, 0:CAP], lhsT=Wg[:, k, fk * 128:(fk + 1) * 128], rhs=xeT[:, k, :], start=(k == 0), stop=(k == 7)),
                         reads=[('Wg', k // 2), ('xeT', k)], writes=[pkg], pe_acc=True)
                for k in range(8):
                    S.op('pe', lambda: PE.matmul(pu[:, 0:CAP], lhsT=Wu[:, k, fk * 128:(fk + 1) * 128], rhs=xeT[:, k, :], start=(k == 0), stop=(k == 7)),
                         reads=[('Wu', k // 2), ('xeT', k)], writes=[pku], pe_acc=True)
                S.op('act', lambda: ACT.activation(out=hs, in_=pg[:, 0:CAP], func=AF.Silu), reads=[pkg], writes=['hs'])
                S.op('dve', lambda: V.tensor_tensor(out=hT[:, fk, :], in0=pu[:, 0:CAP], in1=hs, op=ALU.mult), reads=[pku, 'hs'], writes=[('hT', fk)])
            for half in range(2):
                for cb in range(2):
                    py, pky = ps[7] if (half * 2 + cb) % 2 else ps[4], psk[7] if (half * 2 + cb) % 2 else psk[4]
                    for fk in range(8):
                        S.op('pe', lambda: PE.matmul(py, lhsT=hT[:, fk, half * 128:(half + 1) * 128], rhs=Wd[:, fk, cb * 512:(cb + 1) * 512], start=(fk == 0), stop=(fk == 7)),
                             reads=[('hT', fk), ('Wd', fk // 2), 'affs'], writes=[pky], pe_acc=True)
                    S.op('dve', lambda: V.tensor_scalar(out=ye[:, e, half, cb * 512:(cb + 1) * 512], in0=py, scalar1=affs[:, half:half + 1], scalar2=None, op0=ALU.mult),
                         reads=[pky, 'affs'], writes=['ye'])
        S.barrier()
        AR.seek(wbase - 3 * 8 * D * 2)
        Pall = AR.alloc([128, E, CAP], BF16)
        PT = AR.alloc([128, 2 * E, 128], BF16)
        x1t = [AR.alloc([128, D], F32) for _ in range(2)]
        ot = [AR.alloc([128, D], F32) for _ in range(2)]
        for i in range(NT):
            b = i % 2
            sl = slice(i * 128, (i + 1) * 128)
            S.dma('sp', x1t[b], out_d[s, sl, :], reads=[('outd', i)], writes=[('x1t', b)])
            for e in range(E):
                S.op('dve', lambda: V.tensor_scalar(out=Pall[:, e, :], in0=iota_row, scalar1=slot_tm[:, i, e:e + 1], scalar2=None, op0=ALU.is_equal),
                     reads=['iota_row', 'slot_tm'], writes=[('Pall', e // 4)])
            for q in range(4):
                pz = ps[q].bitcast(BF16).rearrange("p (j t) -> p j t", j=8)
                for j in range(8):
                    idx = q * 8 + j
                    e, half = idx // 2, idx % 2
                    S.op('pe', lambda: PE.transpose(out=pz[:, j, :], in_=Pall[:, e, half * 128:(half + 1) * 128], identity=ident),
                         reads=[('Pall', e // 4), 'ident'], writes=[psk[q]], pe_acc=True)
                if q % 2 == 0:
                    S.op('act', lambda: ACT.copy(out=PT[:, q * 8:(q + 1) * 8, :], in_=pz), reads=[psk[q]], writes=[('PT', q)])
                else:
                    S.op('dve', lambda: V.tensor_copy(out=PT[:, q * 8:(q + 1) * 8, :], in_=pz), reads=[psk[q]], writes=[('PT', q)])
            for cb in range(2):
                po, pko = ps[4 + cb + 2 * (i % 2)], psk[4 + cb + 2 * (i % 2)]
                for idx in range(2 * E):
                    e, half = idx // 2, idx % 2
                    S.op('pe', lambda: PE.matmul(po, lhsT=PT[:, idx, :], rhs=ye[:, e, half, cb * 512:(cb + 1) * 512], start=(idx == 0), stop=(idx == 2 * E - 1)),
                         reads=[('PT', idx // 8), 'ye'], writes=[pko], pe_acc=True)
                S.op('dve', lambda: V.tensor_tensor(out=ot[b][:, cb * 512:(cb + 1) * 512], in0=po, in1=gt2B[:, cb * 512:(cb + 1) * 512], op=ALU.mult),
                     reads=[pko, 'gt2B'], writes=[('ot', b, cb)])
            S.op('dve', lambda: V.tensor_tensor(out=ot[b], in0=ot[b], in1=x1t[b], op=ALU.add), reads=[('ot', b, 0), ('ot', b, 1), ('x1t', b)], writes=[('ot', b, 0), ('ot', b, 1)])
            S.dma('sp', out_d[s, sl, :], ot[b], reads=[('ot', b, 0), ('ot', b, 1)], writes=[('outd', i)])

    def dbg_dump(src_ap, shape, key_reads=()):
        AR.seek(AR_TOP)
        t = AR.alloc(shape, F32)
        S.op('dve', lambda: V.tensor_copy(out=t, in_=src_ap), writes=['dbgt'])
        flat = t if len(shape) == 2 else t.rearrange("p a b -> p (a b)")
        S.dma('sp', dbg_d, flat, reads=['dbgt'])

    AR_TOP = 160 * 1024
    phase_adaln()
    for s in range(nseq):
        AR.seek(0)
        zsT = AR.alloc([128, 15, T], BF16)
        uT = AR.alloc([128, 8, T], BF16)
        base1 = AR.ptr
        phase_norm1(s, uT, base1)
        S.barrier()
        if dbg and dbg[0] == 'uT':
            dbg_dump(uT[:, :, 0:512], [128, 8, 512]); break
        for q in range(4):
            S.dma('sp', u_d[:, 2 * q:2 * q + 2, :], uT[:, 2 * q:2 * q + 2, :], reads=[('uT', 0), ('uT', 1), ('uT', 2), ('uT', 3)], writes=['uscr'])
        phase_rwkv_cols(uT, zsT, base1)
        S.barrier()
        if dbg and dbg[0] == 'zs':
            dbg_dump(zsT[:, :, 0:256], [128, 15, 256]); break
        AR.seek(61440)
        kkT = AR.alloc([128, 4, T], BF16)
        yaT = AR.alloc([128, 4, T], BF16)
        base3 = AR.ptr
        phase_scan(zsT, kkT, 77824)
        if dbg and dbg[0] == 'yscan':
            AR.seek(AR_TOP)
            t = AR.alloc([128, 2, 512], F32)
            S.dma('sp', t[:, 0, :], y_d[0, 0:128, :], writes=['dbgt'])
            S.dma('sp', t[:, 1, :], y_d[1, 0:128, :], writes=['dbgt'])
            S.dma('sp', dbg_d, t.rearrange("p a b -> p (a b)"), reads=['dbgt']); break
        phase_post(zsT, yaT, base3)
        S.barrier()
        if dbg and dbg[0] == 'yaT':
            dbg_dump(yaT[:, :, 0:512], [128, 4, 512]); break
        AR.seek(0)
        uT = AR.alloc([128, 8, T], BF16)
        AR.seek(94208)
        ybT = AR.alloc([128, 4, T], BF16)
        baseB = AR.ptr
        for q in range(4):
            S.dma('sp' if q % 2 == 0 else 'act', uT[:, 2 * q:2 * q + 2, :], u_d[:, 2 * q:2 * q + 2, :], writes=[('uT', 0), ('uT', 1), ('uT', 2), ('uT', 3)])
        phase_attn(s, uT, ybT, 32768, baseB)
        S.barrier()
        if dbg and dbg[0] == 'ybT':
            dbg_dump(ybT[:, :, 0:512], [128, 4, 512]); break
        AR.seek(32768)
        mergedT = AR.alloc([128, 8, T], BF16)
        phase_merge(uT, yaT, ybT, mergedT, (65536, baseB))
        S.barrier()
        if dbg and dbg[0] == 'merged':
            dbg_dump(mergedT[:, :, 0:512], [128, 8, 512]); break
        AR.seek(0)
        u2tm = AR.alloc([128, NT, D], BF16)
        phase_x1(s, mergedT, u2tm, 65536)
        S.barrier()
        if dbg and dbg[0] == 'aff':
            dbg_dump(afftm.rearrange("p i e -> p (i e)"), [128, 256]); break
        phase_moe(s, u2tm, 32768)
        S.barrier()

    S.finish('sp')
    print("ninstr", S.ninstr, "pe_incs", S.npe_inc, "arena hi", AR.hi)
    return nc


def _consts():
    cm = np.zeros((13, 128, 128), np.float32)
    p = np.arange(128)
    cm[0] = (p[:, None] // 64 == p[None, :] // 64).astype(np.float32)
    R = np.zeros((128, 128), np.float32)
    for blk in range(2):
        o = blk * 64
        for d_ in range(8):
            R[o + d_ + 8, o + d_] = -1.0
            R[o + d_, o + d_ + 8] = 1.0
    cm[1] = R
    cm[2] = (p[:, None] >= p[None, :]).astype(np.float32)
    cm[3] = (p[:, None] <= p[None, :]).astype(np.float32)
    s_ = (p % 64)[:, None]
    t_ = (p % 64)[None, :]
    a_col = (p[None, :] >= 64)
    fwd = np.where(a_col, s_ < t_, s_ <= t_)
    bwd = np.where(a_col, s_ > t_, s_ >= t_)
    cm[4] = fwd.astype(np.float32)
    cm[5] = bwd.astype(np.float32)
    cm[6] = cm[4].T
    cm[7] = cm[5].T
    cm[8][:, 0] = (p < 64)
    cm[8][:, 1] = (p >= 64)
    cm[9][:, 0:64] = 1.0
    cm[10][:, 64:128] = 1.0
    cm[11] = ((p % 64)[:, None] < (p % 64)[None, :]).astype(np.float32)
    cm[12] = ((p % 64)[:, None] > (p % 64)[None, :]).astype(np.float32)
    return np.ascontiguousarray(cm.transpose(1, 0, 2).reshape(128, 13 * 128))


def _prep_shared(inp):
    f = lambda a: np.ascontiguousarray(np.asarray(a, dtype=np.float32))
    L = 0
    w_in = f(inp["w_in"][L]).copy()
    qoff = 1920
    perm = []
    for c in range(4):
        perm += list(range(c * 64, (c + 1) * 64)) + list(range((4 + c) * 64, (5 + c) * 64))
    perm = np.array(perm)
    w_in[:, qoff:qoff + 512] = w_in[:, qoff:qoff + 512][:, perm]
    p_attn = f(inp["p_attn"][L])[perm, :]
    pp = np.zeros((128, NPP), np.float32)

    def put(name, arr):
        o, w = PP[name]
        pp[:, o:o + w] = arr

    chunked = lambda v: np.asarray(v, np.float32).reshape(-1, 128).T
    put("mp", chunked(inp["mu_prev"][L]))
    put("mn", chunked(inp["mu_next"][L]))
    put("w0", np.concatenate([chunked(inp["rwkv_w0"][L][0]), chunked(inp["rwkv_w0"][L][1])], 1))
    put("a0", np.concatenate([chunked(inp["rwkv_a0"][L][0]), chunked(inp["rwkv_a0"][L][1])], 1))
    put("kk", chunked(inp["rwkv_k_k"][L]))
    put("ka", chunked(inp["rwkv_k_a"][L]))
    put("rk", chunked(np.asarray(inp["rwkv_r_k"][L]).reshape(-1)))
    put("qg", np.tile(np.asarray(inp["q_norm_g"][L], np.float32), 2)[:, None])
    put("kg", np.tile(np.asarray(inp["k_norm_g"][L], np.float32), 2)[:, None])
    inv_freq = (500000.0 ** (-np.arange(0, 16, 2, dtype=np.float32) / 16)).astype(np.float32)
    invf = np.zeros(64, np.float32)
    invf[0:8] = inv_freq
    invf[8:16] = inv_freq
    put("invf", np.tile(invf, 2)[:, None])
    sink = np.asarray(inp["attn_sink"][L], np.float32)
    sk = np.zeros((128, 4), np.float32)
    for j in range(4):
        sk[0:64, j] = sink[j]
        sk[64:128, j] = sink[4 + j]
    put("sink", sk)
    w2cat = np.zeros((128, 2, 512), np.float32)
    a2cat = np.zeros((128, 2, 512), np.float32)
    for d_ in range(2):
        w2cat[d_ * 64:(d_ + 1) * 64, d_, :] = inp["rwkv_w2"][L][d_]
        a2cat[d_ * 64:(d_ + 1) * 64, d_, :] = inp["rwkv_a2"][L][d_]
    return {
        "w_ada": f(inp["w_ada"][L]), "b_ada": f(inp["b_ada"][L])[None, :] if np.asarray(inp["b_ada"][L]).ndim == 1 else f(inp["b_ada"][L]),
        "norm1_g": f(inp["norm1_g"][L]).reshape(1, D), "norm2_g": f(inp["norm2_g"][L]).reshape(1, D),
        "w_in": w_in, "pp": pp, "w2cat": w2cat.reshape(128, 1024), "a2cat": a2cat.reshape(128, 1024),
        "g2": f(inp["rwkv_g2"][L]), "gn_w": f(inp["rwkv_gn_w"][L]).reshape(1, 512), "gn_b": f(inp["rwkv_gn_b"][L]).reshape(1, 512),
        "p_rwkv": f(inp["p_rwkv"][L]), "p_attn": np.ascontiguousarray(p_attn), "w_out": f(inp["w_out"][L]),
        "w_router": f(inp["w_router"][L]), "w_gate": f(inp["w_gate"][L]), "w_up": f(inp["w_up"][L]), "w_down": f(inp["w_down"][L]),
        "cmats": _consts(),
    }


def _core_inputs(inp, shared, seqs):
    x = np.ascontiguousarray(np.asarray(inp["x"], np.float32)[seqs])
    c = np.asarray(inp["c"], np.float32)[seqs]
    cT = np.ascontiguousarray(c.reshape(len(seqs), 8, 128).transpose(0, 2, 1))
    pos = np.ascontiguousarray(np.asarray(inp["positions"]).astype(np.int32)[seqs][:, None, :])
    m = dict(shared)
    m.update({"x": x, "cT": cT, "pos": pos})
    return m


def kernel(**inputs):
    shared = _prep_shared(inputs)
    nc = build(NSEQ)
    in_maps = [_core_inputs(inputs, shared, list(range(i * NSEQ, (i + 1) * NSEQ))) for i in range(NCORES)]
    res = run_bass_kernel_spmd(nc, in_maps, core_ids=list(range(NCORES)))
    out = np.concatenate([np.asarray(r["out"]) for r in res.results], axis=0)
    return out.astype(np.float32)
```

```python
import numpy as np
import concourse.bass as bass
import concourse.mybir as mybir
from concourse.bass_utils import run_bass_kernel_spmd

F32 = mybir.dt.float32
BF16 = mybir.dt.bfloat16
I32 = mybir.dt.int32
ALU = mybir.AluOpType
AF = mybir.ActivationFunctionType
AX = mybir.AxisListType

T = 2048
D = 1024
NT = 16
NB = 4
NSEQ = 2
NCORES = 8
E = 16
CAP = 256
LAM = float(np.exp(-0.5))
NCH = 4
TBS = NCH * 64
NTB = T // TBS
TWO_PI = float(2 * np.pi)
C1 = 6.28125
C2 = TWO_PI - C1

PP = {}
_o = 0
for _n, _w in [("mp", 15), ("mn", 15), ("w0", 8), ("a0", 8), ("kk", 4), ("ka", 4), ("rk", 4), ("qg", 1), ("kg", 1),
               ("invf", 1), ("sink", 4)]:
    PP[_n] = (_o, _w)
    _o += _w
NPP = _o


class Ticket:
    __slots__ = ('ins', 'sem', 'val', 'parent')

    def __init__(self, ins):
        self.ins = ins
        self.sem = None
        self.val = None
        self.parent = None

    def root(self):
        t = self
        while t.parent is not None:
            t = t.parent
        return t


class Sync:
    SEM_MAX = 30000

    def __init__(self, nc):
        self.nc = nc
        self.E = {'pe': nc.tensor, 'act': nc.scalar, 'dve': nc.vector, 'pool': nc.gpsimd, 'sp': nc.sync}
        self.sem = {}
        self.cnt = {}
        self.nsem = 0
        for e in self.E:
            self._newsem(e)
        self.waited = {}
        self.lastw = {}
        self.reads = {}
        self.dma_sems = {}
        self.dma_rr = {}
        self.ninstr = 0
        self.pend = None
        self.pend_writes = None
        self.npe_inc = 0

    def _newsem(self, e):
        self.sem[e] = self.nc.alloc_semaphore(f"s_{e}_{self.nsem}")
        self.nsem += 1
        self.cnt[e] = 0

    def _flush_pe(self):
        t = self.pend
        if t is None:
            return
        if self.cnt['pe'] >= self.SEM_MAX:
            self._newsem('pe')
        self.cnt['pe'] += 1
        t.sem = self.sem['pe']
        t.val = self.cnt['pe']
        t.ins.then_inc(t.sem, 1)
        self.npe_inc += 1
        self.pend = None
        self.pend_writes = None

    def _wait(self, e, ev):
        if ev is None:
            return
        if isinstance(ev, Ticket):
            if e == 'pe':
                return
            t = ev.root()
            if t.val is None:
                assert t is self.pend
                self._flush_pe()
            sem, val = t.sem, t.val
        else:
            src, sem, val = ev
        k = (e, sem.name)
        if self.waited.get(k, 0) >= val:
            return
        self.waited[k] = val
        self.E[e].wait_ge(sem, val)

    def deps(self, e, reads, writes, pe_acc=False):
        for k in reads:
            self._wait(e, self.lastw.get(k))
        for k in writes:
            lw = self.lastw.get(k)
            if not (pe_acc and isinstance(lw, Ticket)):
                self._wait(e, lw)
            for ev in self.reads.get(k, {}).values():
                self._wait(e, ev)

    def commit(self, src, ev, reads, writes):
        for k in reads:
            self.reads.setdefault(k, {})[src] = ev
        for k in writes:
            self.lastw[k] = ev
            self.reads[k] = {}

    def op(self, e, fn, reads=(), writes=(), pe_acc=False):
        self.deps(e, reads, writes, pe_acc)
        if e == 'pe':
            ins = fn()
            t = Ticket(ins)
            if self.pend is not None:
                if self.pend_writes == tuple(writes):
                    self.pend.parent = t
                    self.pend = None
                else:
                    self._flush_pe()
            self.pend = t
            self.pend_writes = tuple(writes)
            self.commit('pe', t, reads, writes)
            self.ninstr += 1
            return t
        if self.cnt[e] >= self.SEM_MAX:
            self._newsem(e)
        ins = fn()
        self.cnt[e] += 1
        ev = (e, self.sem[e], self.cnt[e])
        ins.then_inc(self.sem[e], 1)
        self.commit(e, ev, reads, writes)
        self.ninstr += 1
        return ev

    def dma(self, e, out, in_, reads=(), writes=(), nslots=8, **kw):
        if e == 'pool':
            nslots = 2
        lst = self.dma_sems.setdefault(e, [])
        if len(lst) < nslots:
            lst.append([self.nc.alloc_semaphore(f"d_{e}_{len(lst)}"), 0])
        i = self.dma_rr.get(e, 0)
        self.dma_rr[e] = (i + 1) % nslots
        slot = lst[i % len(lst)]
        sem, uses = slot
        if uses > 0:
            self._wait(e, ('dma', sem, 16 * uses))
        self.deps(e, reads, writes)
        self.E[e].dma_start(out=out, in_=in_, **kw).then_inc(sem, 16)
        slot[1] = uses + 1
        ev = ('dma_%s_%d' % (e, i % len(lst)), sem, 16 * (uses + 1))
        self.commit(ev[0], ev, reads, writes)
        self.ninstr += 1
        return ev

    def barrier(self):
        self._flush_pe()
        evs = [(e, self.sem[e], self.cnt[e]) for e in self.E if self.cnt[e] > 0]
        for q, lst in self.dma_sems.items():
            for sem, uses in lst:
                if uses:
                    evs.append(('dma', sem, 16 * uses))
        for e in self.E:
            for ev in evs:
                if ev[0] != e:
                    self._wait(e, ev)
        self.lastw = {}
        self.reads = {}

    def finish(self, e='sp'):
        self._flush_pe()
        for q, lst in self.dma_sems.items():
            for sem, uses in lst:
                if uses:
                    self._wait(e, ('dma', sem, 16 * uses))


class Arena:
    def __init__(self, nc, name, nbytes):
        self.n4 = nbytes // 4
        self.t = nc.alloc_sbuf_tensor(name, [128, self.n4], F32).ap()
        self.ptr = 0
        self.hi = 0

    def seek(self, off):
        self.ptr = off

    def alloc(self, shape, dtype, parts=None):
        esz = 4 if dtype in (F32, I32) else 2
        n = int(np.prod(shape[1:]))
        nb = (n * esz + 31) // 32 * 32
        assert self.ptr % 4 == 0
        a = self.ptr // 4
        assert a + nb // 4 <= self.n4, f"arena overflow {self.ptr}+{nb} > {self.n4 * 4}"
        v = self.t[:, a:a + nb // 4]
        if dtype != F32:
            v = v.bitcast(dtype)
        v = v[0:shape[0], 0:n]
        if len(shape) > 2:
            names = " ".join(f"d{i}" for i in range(len(shape) - 1))
            kw = {f"d{i}": int(shape[i + 1]) for i in range(len(shape) - 1)}
            v = v.rearrange(f"p ({names}) -> p {names}", **kw)
        self.ptr += nb
        self.hi = max(self.hi, self.ptr)
        return v


def bc(ap, shape):
    return ap.to_broadcast(list(shape))


def build(nseq=NSEQ, dbg=None, stop_after=None):
    nc = bass.Bass("TRN2", target_bir_lowering=False)
    S = Sync(nc)
    V, ACT, POOL, PE = nc.vector, nc.scalar, nc.gpsimd, nc.tensor

    def din(name, shape, dt=F32):
        return nc.dram_tensor(name, list(shape), dt, kind="ExternalInput").ap()

    x_d = din("x", [nseq, T, D])
    cT_d = din("cT", [nseq, 128, 8])
    pos_d = din("pos", [nseq, 1, T], I32)
    wada_d = din("w_ada", [D, 6 * D])
    bada_d = din("b_ada", [1, 6 * D])
    n1g_d = din("norm1_g", [1, D])
    n2g_d = din("norm2_g", [1, D])
    win_d = din("w_in", [D, 4736])
    pp_d = din("pp", [128, NPP])
    w2c_d = din("w2cat", [128, 2 * 512])
    a2c_d = din("a2cat", [128, 2 * 512])
    g2_d = din("g2", [128, 512])
    gnw_d = din("gn_w", [1, 512])
    gnb_d = din("gn_b", [1, 512])
    prw_d = din("p_rwkv", [512, D])
    pat_d = din("p_attn", [512, D])
    wout_d = din("w_out", [D, D])
    wr_d = din("w_router", [D, E])
    wg_d = din("w_gate", [E, D, D])
    wu_d = din("w_up", [E, D, D])
    wd_d = din("w_down", [E, D, D])
    cm_d = din("cmats", [128, 13 * 128])
    out_d = nc.dram_tensor("out", [nseq, T, D], F32, kind="ExternalOutput").ap()
    mod_d = nc.dram_tensor("modscr", [nseq, 6, 128, D], F32, kind="Internal").ap()
    y_d = nc.dram_tensor("yscr", [2, T, 512], F32, kind="Internal").ap()
    u_d = nc.dram_tensor("uscr", [128, 8, T], BF16, kind="Internal").ap()
    dbg_d = None
    if dbg is not None:
        dbg_d = nc.dram_tensor("dbg", list(dbg[1]), F32, kind="ExternalOutput").ap()

    def sb(name, shape, dt=F32):
        return nc.alloc_sbuf_tensor('sb_' + name, list(shape), dt).ap()

    pp = sb("pp", [128, NPP])
    ident = sb("ident", [128, 128], BF16)
    identf = sb("identf", [128, 128])
    cmb = sb("cmb", [128, 13, 128], BF16)
    w2c = sb("w2c", [128, 2, 512], BF16)
    a2c = sb("a2c", [128, 2, 512], BF16)
    g2 = sb("g2", [128, 512], BF16)
    wr = sb("wr", [128, 8, E], BF16)
    epsc = sb("epsc", [128, 4])
    alpha = sb("alpha", [128, 15])
    oneminus_ka = sb("omka", [128, 4])
    two_omka = sb("omka2", [128, 4])
    negkkc = sb("negone", [128, 1])
    esk = sb("esk", [128, 4])
    rmask = sb("rmask", [128, TBS])
    iota_row = sb("iota_row", [128, CAP])
    ident4 = sb("ident4", [128, 4, 128], BF16)
    kar = sb("kar", [128, 4])
    c2r = sb("c2r", [128, 4])
    afftm = sb("afftm", [128, NT, E])
    slot_tm = sb("slot_tm", [128, NT, E])
    affhl = sb("affhl", [128, NT, E, 2], BF16)

    BLK1, ROT, MPREV, MNEXT = 0, 1, 2, 3
    MZT = (4, 5)
    MZ = (6, 7)
    HSEL = 8
    VP = (9, 10)

    ps = [nc.alloc_psum_tensor(f"ps{i}", [128, 512], F32).ap() for i in range(8)]
    psk = [f"ps{i}" for i in range(8)]

    AR = Arena(nc, "arena", 192 * 1024)

    def col(name, j=0, n=1):
        o, w = PP[name]
        return pp[:, o + j:o + j + n]

    S.dma('sp', pp, pp_d, writes=['pp'])
    S.dma('pool', cmb.rearrange("p a b -> p (a b)"), cm_d, writes=['cmb'])
    S.dma('pool', w2c.rearrange("p a b -> p (a b)"), w2c_d, writes=['w2c'])
    S.dma('pool', a2c.rearrange("p a b -> p (a b)"), a2c_d, writes=['a2c'])
    S.dma('pool', g2, g2_d, writes=['g2'])
    S.dma('pool', wr, wr_d.rearrange("(k p) e -> p k e", p=128), writes=['wr'])
    S.op('pool', lambda: POOL.memset(identf, 1.0), writes=['identf'])
    S.op('pool', lambda: POOL.affine_select(out=identf, in_=identf, pattern=[[1, 128]], compare_op=ALU.is_equal,
                                            fill=0.0, base=0, channel_multiplier=-1), reads=['identf'], writes=['identf'])
    S.op('dve', lambda: V.tensor_copy(out=ident, in_=identf), reads=['identf'], writes=['ident'])
    for j in range(4):
        S.op('dve', lambda: V.tensor_copy(out=ident4[:, j, :], in_=identf), reads=['identf'], writes=['ident4'])
    S.op('pool', lambda: POOL.memset(epsc[:, 0:1], 1e-6), writes=['epsc'])
    S.op('pool', lambda: POOL.memset(epsc[:, 1:2], 64e-5), reads=['epsc'], writes=['epsc'])
    S.op('pool', lambda: POOL.memset(epsc[:, 2:3], 1e-24), reads=['epsc'], writes=['epsc'])
    S.op('pool', lambda: POOL.memset(epsc[:, 3:4], 0.0), reads=['epsc'], writes=['epsc'])
    S.op('pool', lambda: POOL.memset(negkkc, -1.0), writes=['negone'])
    S.op('dve', lambda: V.tensor_tensor(out=alpha, in0=col("mp", 0, 15), in1=col("mn", 0, 15), op=ALU.add), reads=['pp'], writes=['alpha'])
    S.op('dve', lambda: V.tensor_scalar(out=alpha, in0=alpha, scalar1=-1.0, scalar2=1.0, op0=ALU.mult, op1=ALU.add), reads=['alpha'], writes=['alpha'])
    S.op('dve', lambda: V.tensor_scalar(out=oneminus_ka, in0=col("ka", 0, 4), scalar1=-1.0, scalar2=1.0, op0=ALU.mult, op1=ALU.add), reads=['pp'], writes=['omka'])
    S.op('dve', lambda: V.tensor_scalar(out=two_omka, in0=col("ka", 0, 4), scalar1=-2.0, scalar2=2.0, op0=ALU.mult, op1=ALU.add), reads=['pp'], writes=['omka2'])
    S.op('act', lambda: ACT.activation(out=esk, in_=col("sink", 0, 4), func=AF.Exp), reads=['pp'], writes=['esk'])
    S.op('dve', lambda: V.tensor_tensor(out=kar, in0=col("ka", 0, 4), in1=col("rk", 0, 4), op=ALU.mult), reads=['pp'], writes=['kar'])
    S.op('dve', lambda: V.tensor_tensor(out=c2r, in0=two_omka, in1=col("rk", 0, 4), op=ALU.mult), reads=['pp', 'omka2'], writes=['kar'])
    S.op('pool', lambda: POOL.memset(rmask, 1.0), writes=['rmask'])
    S.op('pool', lambda: POOL.memset(rmask.rearrange("p (c t) -> p c t", t=64)[:, :, 0:1], 0.0), reads=['rmask'], writes=['rmask'])
    S.op('pool', lambda: POOL.iota(iota_row, pattern=[[1, CAP]], base=0, channel_multiplier=0, allow_small_or_imprecise_dtypes=True), writes=['iota_row'])

    def debug_out(ap_sb, key, rows=None):
        S.dma('sp', dbg_d if rows is None else rows, ap_sb, reads=[key])

    def phase_adaln():
        AR.seek(0)
        csil = [AR.alloc([128, 8], F32) for _ in range(nseq)]
        crep = [AR.alloc([128, 9, 128], F32) for _ in range(nseq)]
        wblk = [AR.alloc([128, 9, 512], F32) for _ in range(3)]
        g1B = AR.alloc([128, D], F32)
        g2B = AR.alloc([128, D], F32)
        mt = [AR.alloc([128, 512], F32) for _ in range(4)]
        S.dma('sp', g1B, n1g_d.partition_broadcast(128), writes=['g1B'])
        S.dma('sp', g2B, n2g_d.partition_broadcast(128), writes=['g2B'])
        for b in range(3):
            S.op('pool', lambda: POOL.memset(wblk[b][:, 8, :], 0.0), writes=[('wblk', b)])
        for s in range(nseq):
            S.dma('sp', csil[s], cT_d[s], writes=[('csil', s)])
            S.op('act', lambda: ACT.activation(out=csil[s], in_=csil[s], func=AF.Silu), reads=[('csil', s)], writes=[('csil', s)])
            S.op('pool', lambda: POOL.memset(crep[s][:, 8, :], 0.0), writes=[('crep', s)])
            S.op('pool', lambda: POOL.memset(crep[s][0:1, 8, :], 1.0), reads=[('crep', s)], writes=[('crep', s)])
            S.op('dve', lambda: V.tensor_copy(out=crep[s][:, 0:8, :], in_=bc(csil[s].rearrange("p (k o) -> p k o", o=1), [128, 8, 128])),
                 reads=[('csil', s)], writes=[('crep', s)])
        ev = 0
        for jb in range(12):
            b = jb % 3
            piece = jb // 2
            c0 = jb * 512
            S.dma('sp', wblk[b][:, 0:4, :], wada_d[0:512, c0:c0 + 512].rearrange("(k p) n -> p k n", p=128), writes=[('wblk', b)])
            S.dma('act', wblk[b][:, 4:8, :], wada_d[512:1024, c0:c0 + 512].rearrange("(k p) n -> p k n", p=128), writes=[('wblk', b)])
            S.dma('sp', wblk[b][0:1, 8, :], bada_d[:, c0:c0 + 512], writes=[('wblk', b)])
            for s in range(nseq):
                pz, pkz = ps[ev % 4], psk[ev % 4]
                for k in range(9):
                    S.op('pe', lambda: PE.matmul(pz, lhsT=crep[s][:, k, :], rhs=wblk[b][:, k, :], start=(k == 0), stop=(k == 8)),
                         reads=[('crep', s), ('wblk', b)], writes=[pkz], pe_acc=True)
                m = mt[ev % 4]
                lc = (jb % 2) * 512
                if piece == 1:
                    S.op('dve', lambda: V.scalar_tensor_tensor(out=m, in0=pz, scalar=1.0, in1=g1B[:, lc:lc + 512], op0=ALU.add, op1=ALU.mult),
                         reads=[pkz, 'g1B'], writes=[('mt', ev % 4)])
                elif piece == 4:
                    S.op('dve', lambda: V.scalar_tensor_tensor(out=m, in0=pz, scalar=1.0, in1=g2B[:, lc:lc + 512], op0=ALU.add, op1=ALU.mult),
                         reads=[pkz, 'g2B'], writes=[('mt', ev % 4)])
                else:
                    S.op('act', lambda: ACT.copy(out=m, in_=pz), reads=[pkz], writes=[('mt', ev % 4)])
                S.dma('sp', mod_d[s, piece, :, lc:lc + 512], m, reads=[('mt', ev % 4)], writes=[('mod', s, piece)])
                ev += 1
        S.barrier()

    def phase_norm1(s, uT, base):
        AR.seek(base)
        scp = AR.alloc([128, D], F32)
        shp = AR.alloc([128, D], F32)
        xt = [AR.alloc([128, D], F32) for _ in range(2)]
        tmp2 = [AR.alloc([128, D], F32) for _ in range(2)]
        ub = [AR.alloc([128, D], BF16) for _ in range(2)]
        junk2 = [AR.alloc([128, D], BF16) for _ in range(2)]
        ss2 = [AR.alloc([128, 2], F32) for _ in range(2)]
        S.dma('sp', scp, mod_d[s, 1], reads=[('mod', s, 1)], writes=['scp'])
        S.dma('sp', shp, mod_d[s, 0], reads=[('mod', s, 0)], writes=['shp'])
        for i in range(NT):
            b = i % 2
            S.dma('sp', xt[b], x_d[s, i * 128:(i + 1) * 128, :], writes=[('xt', b)])
            tmp, junk, ss = tmp2[b], junk2[b], ss2[b]
            S.op('act', lambda: ACT.activation(out=junk, in_=xt[b], func=AF.Square, accum_out=ss[:, 0:1]), reads=[('xt', b)], writes=[('junk', b), ('ss', b)])
            S.op('act', lambda: ACT.activation(out=ss[:, 1:2], in_=ss[:, 0:1], func=AF.Sqrt, bias=epsc[:, 0:1], scale=1.0 / D), reads=[('ss', b), 'epsc'], writes=[('ss1', b)])
            S.op('dve', lambda: V.reciprocal(out=ss[:, 1:2], in_=ss[:, 1:2]), reads=[('ss1', b)], writes=[('ss1', b)])
            S.op('dve', lambda: V.scalar_tensor_tensor(out=tmp, in0=xt[b], scalar=ss[:, 1:2], in1=scp, op0=ALU.mult, op1=ALU.mult),
                 reads=[('xt', b), ('ss1', b), 'scp'], writes=[('tmp', b)])
            S.op('pool', lambda: POOL.tensor_tensor(out=ub[b], in0=tmp, in1=shp, op=ALU.add), reads=[('tmp', b), 'shp'], writes=[('ub', b)])
            pz = ps[i % 2].bitcast(BF16).rearrange("p (k t) -> p k t", k=8)
            for k in range(8):
                S.op('pe', lambda: PE.transpose(out=pz[:, k, :], in_=ub[b][:, k * 128:(k + 1) * 128], identity=ident),
                     reads=[('ub', b), 'ident'], writes=[psk[i % 2]], pe_acc=True)
            S.op('act', lambda: ACT.copy(out=uT[:, :, i * 128:(i + 1) * 128], in_=pz), reads=[psk[i % 2]], writes=[('uT', i // 4)])

    def phase_rwkv_cols(uT, zsT, base):
        AR.seek(base)
        wg = [AR.alloc([128, 8, 128], BF16) for _ in range(2)]
        ztmpP = [AR.alloc([128, T + 2], F32) for _ in range(2)]
        shtP = [AR.alloc([128, T], F32) for _ in range(2)]
        for q in range(2):
            S.op('pool', lambda: POOL.memset(ztmpP[q][:, 0:1], 0.0), writes=[('ztmp', q)])
            S.op('pool', lambda: POOL.memset(ztmpP[q][:, T + 1:T + 2], 0.0), reads=[('ztmp', q)], writes=[('ztmp', q)])
        for j in range(15):
            b = j % 2
            ztmp, sht = ztmpP[b], shtP[b]
            S.dma('pool', wg[b], win_d[:, j * 128:(j + 1) * 128].rearrange("(k p) n -> p k n", p=128), writes=[('wg', b)])
            for tb in range(NB):
                pz = ps[(j * NB + tb) % 4]
                pk = psk[(j * NB + tb) % 4]
                for k in range(8):
                    S.op('pe', lambda: PE.matmul(pz, lhsT=wg[b][:, k, :], rhs=uT[:, k, tb * 512:(tb + 1) * 512], start=(k == 0), stop=(k == 7)),
                         reads=[('wg', b), ('uT', tb)], writes=[pk], pe_acc=True)
                S.op('act', lambda: ACT.copy(out=ztmp[:, 1 + tb * 512:1 + (tb + 1) * 512], in_=pz), reads=[pk], writes=[('ztmp', b)])
            S.op('dve', lambda: V.tensor_scalar(out=sht, in0=ztmp[:, 1:T + 1], scalar1=alpha[:, j:j + 1], scalar2=None, op0=ALU.mult),
                 reads=[('ztmp', b), 'alpha'], writes=[('sht', b)])
            S.op('dve', lambda: V.scalar_tensor_tensor(out=sht, in0=ztmp[:, 0:T], scalar=col("mp", j), in1=sht, op0=ALU.mult, op1=ALU.add),
                 reads=[('ztmp', b), ('sht', b), 'pp'], writes=[('sht', b)])
            S.op('dve', lambda: V.scalar_tensor_tensor(out=zsT[:, j, :], in0=ztmp[:, 2:T + 2], scalar=col("mn", j), in1=sht, op0=ALU.mult, op1=ALU.add),
                 reads=[('ztmp', b), ('sht', b), 'pp'], writes=[('zs', j)])
            if j == 12:
                S.op('act', lambda: ACT.activation(out=zsT[:, j, :], in_=zsT[:, j, :], func=AF.Tanh), reads=[('zs', j)], writes=[('zs', j)])
            if j == 14:
                S.op('act', lambda: ACT.activation(out=zsT[:, j, :], in_=zsT[:, j, :], func=AF.Sigmoid), reads=[('zs', j)], writes=[('zs', j)])

    def phase_scan(zsT, kkT, base):
        rT = lambda c: zsT[:, c, :]
        kT = lambda c: zsT[:, 4 + c, :]
        vT = lambda c: zsT[:, 8 + c, :]
        wdT = zsT[:, 12, :]
        adT = zsT[:, 13, :]
        AR.seek(base)
        kraw = AR.alloc([128, 512], F32)
        ksq = AR.alloc([128, 512], BF16)
        krs = AR.alloc([128, 512], F32)
        for c in range(4):
            for tb in range(NB):
                sl = slice(tb * 512, (tb + 1) * 512)
                S.op('dve', lambda: V.tensor_scalar(out=kraw, in0=kT(c)[:, sl], scalar1=col("kk", c), scalar2=None, op0=ALU.mult), reads=[('zs', 4 + c), 'pp'], writes=['kraw'])
                S.op('act', lambda: ACT.activation(out=ksq, in_=kraw, func=AF.Square), reads=['kraw'], writes=['ksq'])
                pz, pk = ps[tb % 2], psk[tb % 2]
                S.op('pe', lambda: PE.matmul(pz, lhsT=cmb[:, BLK1, :], rhs=ksq, start=True, stop=True), reads=['ksq', 'cmb'], writes=[pk], pe_acc=True)
                S.op('act', lambda: ACT.activation(out=krs, in_=pz, func=AF.Sqrt, bias=epsc[:, 2:3], scale=1.0), reads=[pk, 'epsc'], writes=['krs'])
                S.op('dve', lambda: V.reciprocal(out=krs, in_=krs), reads=['krs'], writes=['krs'])
                S.op('dve', lambda: V.tensor_tensor(out=kkT[:, c, sl], in0=kraw, in1=krs, op=ALU.mult), reads=['kraw', 'krs'], writes=[('kk', c)])
        S.barrier()
        AR.seek(base)
        sg = AR.alloc([128, 4, TBS], F32)
        ad = AR.alloc([128, 4, TBS], F32)
        cc = AR.alloc([128, 4, TBS], F32)
        t1 = AR.alloc([128, 4, TBS], F32)
        ex = [[AR.alloc([128, TBS], F32) for _ in range(2)] for _ in range(4)]
        kd = AR.alloc([128, 4, TBS], F32)
        bb = AR.alloc([128, 4, TBS], F32)
        pdec = AR.alloc([128, 4, NCH], F32)
        ARz = AR.alloc([128, 4, NCH, 2, 2, 64], BF16)
        Bz = AR.alloc([128, 4, NCH, 2, 64], BF16)
        BKt = AR.alloc([128, 4, NCH, 2, 64], BF16)
        KBh = AR.alloc([128, 4, NCH, 2, 64], BF16)
        KBt = AR.alloc([128, 4, NCH, 128], BF16)
        VZ = AR.alloc([128, NCH, 8, 64], BF16)
        XV = AR.alloc([128, NCH, 8, 64], BF16)
        ZTs = [[AR.alloc([128, 4, 128], BF16) for _ in range(2)] for _ in range(NCH)]
        ATm = [[AR.alloc([128, 4, 128], BF16) for _ in range(2)] for _ in range(NCH)]
        PTm = [[AR.alloc([128, 4, 128], BF16) for _ in range(2)] for _ in range(NCH)]
        Pm = [[AR.alloc([128, 4, 128], BF16) for _ in range(2)] for _ in range(NCH)]
        Am = [[AR.alloc([128, 4, 128], BF16) for _ in range(2)] for _ in range(NCH)]
        W1s = AR.alloc([128, 4, 64], BF16)
        S32 = [AR.alloc([128, 4, 64], F32) for _ in range(2)]
        Sb = [AR.alloc([128, 4, 64], BF16) for _ in range(2)]
        ysb = [AR.alloc([64, 512], F32) for _ in range(2)]
        S.op('pool', lambda: POOL.memset(ARz.rearrange("p a b c d e -> p (a b c d e)"), 0.0), writes=['ARz'])
        S.op('pool', lambda: POOL.memset(Bz.rearrange("p a b c d -> p (a b c d)"), 0.0), writes=['Bz'])
        S.op('pool', lambda: POOL.memset(VZ.rearrange("p a b c -> p (a b c)"), 0.0), writes=['VZ'])

        def chain(gens):
            for g_ in gens:
                yield from g_

        def run_tasks(tasks):
            tasks = list(tasks)
            while tasks:
                for t_ in list(tasks):
                    try:
                        next(t_)
                    except StopIteration:
                        tasks.remove(t_)

        yev = 0
        pendQ = None
        for d in range(2):
            S.op('pool', lambda: POOL.memset(S32[d].rearrange("p a b -> p (a b)"), 0.0), writes=[('S32', d)])
            S.op('pool', lambda: POOL.memset(Sb[d].rearrange("p a b -> p (a b)"), 0.0), writes=[('Sb', d)])
            tbs = range(NTB) if d == 0 else range(NTB - 1, -1, -1)
            for tb in tbs:
                sl = slice(tb * TBS, (tb + 1) * TBS)
                def gen_prep(c):
                    pz, pk = ps[c % 2], psk[c % 2]
                    S.op('pe', lambda: PE.matmul(pz[:, 0:TBS], lhsT=w2c[:, d, c * 128:(c + 1) * 128], rhs=wdT[:, sl], start=True, stop=True),
                         reads=['w2c', ('zs', 12)], writes=[pk], pe_acc=True)
                    S.op('act', lambda: ACT.activation(out=sg[:, c, :], in_=pz[:, 0:TBS], func=AF.Sigmoid, bias=col("w0", d * 4 + c), scale=1.0),
                         reads=[pk, 'pp'], writes=[('sg', c)])
                    pz2, pk2 = ps[2 + c % 2], psk[2 + c % 2]
                    S.op('pe', lambda: PE.matmul(pz2[:, 0:TBS], lhsT=a2c[:, d, c * 128:(c + 1) * 128], rhs=adT[:, sl], start=True, stop=True),
                         reads=['a2c', ('zs', 13)], writes=[pk2], pe_acc=True)
                    S.op('act', lambda: ACT.activation(out=ad[:, c, :], in_=pz2[:, 0:TBS], func=AF.Sigmoid, bias=col("a0", d * 4 + c), scale=1.0),
                         reads=[pk2, 'pp'], writes=[('ad', c)])
                    yield
                    S.op('dve', lambda: V.tensor_tensor_scan(out=cc[:, c, :], data0=rmask, data1=sg[:, c, :], initial=0.0, op0=ALU.mult, op1=ALU.add),
                         reads=['rmask', ('sg', c)], writes=[('cc', c)])
                    cc3 = cc[:, c, :].rearrange("p (h t) -> p h t", t=64)
                    sg3 = sg[:, c, :].rearrange("p (h t) -> p h t", t=64)
                    t13 = t1[:, c, :].rearrange("p (h t) -> p h t", t=64)
                    if d == 1:
                        S.op('dve', lambda: V.tensor_tensor(out=t13, in0=bc(cc3[:, :, 63:64], [128, NCH, 64]), in1=cc3, op=ALU.subtract),
                             reads=[('cc', c)], writes=[('t1', c)])
                        S.op('dve', lambda: V.tensor_tensor(out=cc[:, c, :], in0=t1[:, c, :], in1=sg[:, c, :], op=ALU.add),
                             reads=[('t1', c), ('sg', c)], writes=[('cc', c)])
                    totp = 63 if d == 0 else 0
                    S.op('pool', lambda: POOL.tensor_scalar(out=kd[:, c, :], in0=ad[:, c, :], scalar1=col("ka", c), scalar2=oneminus_ka[:, c:c + 1], op0=ALU.mult, op1=ALU.add),
                         reads=[('ad', c), 'pp', 'omka'], writes=[('kd', c)])
                    S.op('pool', lambda: POOL.tensor_tensor(out=kd[:, c, :], in0=kd[:, c, :], in1=kT(c)[:, sl], op=ALU.mult),
                         reads=[('kd', c), ('zs', 4 + c)], writes=[('kd', c)])
                    S.op('pool', lambda: POOL.tensor_tensor(out=bb[:, c, :], in0=ad[:, c, :], in1=kkT[:, c, sl], op=ALU.mult),
                         reads=[('ad', c), ('kk', c)], writes=[('bb', c)])
                    yield
                    e = ex[c][0]
                    S.op('act', lambda: ACT.activation(out=e, in_=cc[:, c, :], func=AF.Exp, scale=-LAM), reads=[('cc', c)], writes=[('ex', c, 0)])
                    for hp in range(2):
                        pr = slice(hp * 64, (hp + 1) * 64)
                        S.op('dve', lambda: V.tensor_tensor(out=ARz[pr, c, :, 0, hp, :], in0=rT(c)[pr, sl].rearrange("p (h t) -> p h t", t=64),
                                                            in1=e[pr, :].rearrange("p (h t) -> p h t", t=64), op=ALU.mult),
                             reads=[('zs', c), ('ex', c, 0)], writes=['ARz'])
                    yield
                    e = ex[c][1]
                    S.op('act', lambda: ACT.activation(out=e, in_=cc[:, c, :], func=AF.Exp, scale=LAM), reads=[('cc', c)], writes=[('ex', c, 1)])
                    S.op('dve', lambda: V.tensor_tensor(out=BKt[:, c, :, 0, :], in0=kd[:, c, :].rearrange("p (h t) -> p h t", t=64),
                                                        in1=e.rearrange("p (h t) -> p h t", t=64), op=ALU.mult),
                         reads=[('kd', c), ('ex', c, 1)], writes=['BKt'])
                    S.op('dve', lambda: V.tensor_tensor(out=BKt[:, c, :, 1, :], in0=bb[:, c, :].rearrange("p (h t) -> p h t", t=64),
                                                        in1=e.rearrange("p (h t) -> p h t", t=64), op=ALU.mult),
                         reads=[('bb', c), ('ex', c, 1)], writes=['BKt'])
                    for hp in range(2):
                        pr = slice(hp * 64, (hp + 1) * 64)
                        S.op('act', lambda: ACT.copy(out=Bz[pr, c, :, hp, :], in_=BKt[pr, c, :, 1, :]), reads=['BKt'], writes=['Bz'])
                    yield
                    S.op('dve', lambda: V.tensor_tensor(out=t1[:, c, :], in0=cc[:, c, :], in1=sg[:, c, :], op=ALU.subtract),
                         reads=[('cc', c), ('sg', c)], writes=[('t1', c)])
                    e = ex[c][0]
                    S.op('act', lambda: ACT.activation(out=e, in_=t1[:, c, :], func=AF.Exp, scale=-LAM), reads=[('t1', c)], writes=[('ex', c, 0)])
                    for hp in range(2):
                        pr = slice(hp * 64, (hp + 1) * 64)
                        S.op('dve', lambda: V.scalar_tensor_tensor(out=ARz[pr, c, :, 1, hp, :], in0=kkT[pr, c, sl].rearrange("p (h t) -> p h t", t=64),
                                                                   scalar=-1.0, in1=e[pr, :].rearrange("p (h t) -> p h t", t=64), op0=ALU.mult, op1=ALU.mult),
                             reads=[('kk', c), ('ex', c, 0)], writes=['ARz'])
                    yield
                    S.op('dve', lambda: V.tensor_tensor(out=t13, in0=bc(cc3[:, :, totp:totp + 1], [128, NCH, 64]), in1=cc3, op=ALU.subtract),
                         reads=[('cc', c)], writes=[('t1', c)])
                    e = ex[c][1]
                    S.op('act', lambda: ACT.activation(out=e, in_=t1[:, c, :], func=AF.Exp, scale=-LAM), reads=[('t1', c)], writes=[('ex', c, 1)])
                    S.op('pool', lambda: POOL.tensor_tensor(out=KBh[:, c, :, 0, :], in0=kd[:, c, :].rearrange("p (h t) -> p h t", t=64),
                                                        in1=e.rearrange("p (h t) -> p h t", t=64), op=ALU.mult),
                         reads=[('kd', c), ('ex', c, 1)], writes=['KBh'])
                    S.op('pool', lambda: POOL.tensor_tensor(out=KBh[:, c, :, 1, :], in0=bb[:, c, :].rearrange("p (h t) -> p h t", t=64),
                                                        in1=e.rearrange("p (h t) -> p h t", t=64), op=ALU.mult),
                         reads=[('bb', c), ('ex', c, 1)], writes=['KBh'])
                    S.op('act', lambda: ACT.activation(out=pdec[:, c, :].rearrange("p (h o) -> p h o", o=1), in_=cc3[:, :, totp:totp + 1], func=AF.Exp, scale=-LAM), reads=[('cc', c)], writes=['pdec'])
                ptasks = [gen_prep(c_) for c_ in range(4)]
                for t_ in ptasks:
                    next(t_)
                if pendQ is not None:
                    for _ in range(3):
                        next(pendQ, None)
                for t_ in ptasks:
                    next(t_)
                if pendQ is not None:
                    run_tasks([pendQ])
                    pendQ = None
                run_tasks(ptasks)
                for ch in range(NCH):
                    pz = ps[4 + ch % 2].bitcast(BF16)
                    pk = psk[4 + ch % 2]
                    pzv = pz[0:64, 0:512].rearrange("p (c n) -> p c n", c=4)
                    for c in range(4):
                        S.op('pe', lambda: PE.transpose(out=pzv[:, c, :], in_=vT(c)[:, tb * TBS + ch * 64: tb * TBS + (ch + 1) * 64], identity=ident),
                             reads=[('zs', 8 + c), 'ident'], writes=[pk], pe_acc=True)
                    S.op('act', lambda: ACT.copy(out=VZ[0:64, ch, :, :].rearrange("p h v -> p (h v)"), in_=pz[0:64, 0:512]), reads=[pk], writes=[('VZ', ch)])
                    S.op('act', lambda: ACT.copy(out=XV[0:64, ch, :, :].rearrange("p h v -> p (h v)"), in_=pz[0:64, 0:512]), reads=[pk], writes=[('XVv', ch)])
                    pzk = pz[:, 512:1024].rearrange("p (c n) -> p c n", c=4)
                    for c in range(4):
                        S.op('pe', lambda: PE.transpose(out=pzk[:, c, :], in_=KBh[:, c, ch, :, :].rearrange("p a t -> p (a t)"), identity=ident),
                             reads=['KBh', 'ident'], writes=[pk], pe_acc=True)
                    S.op('dve', lambda: V.tensor_copy(out=KBt[:, :, ch, :], in_=pzk), reads=[pk], writes=[('KBt', ch)])
                MNT = cmb[:, 11 + d, :]
                MN = cmb[:, 12 - d, :]

                def gen_D(ch, slot, par):
                    pA, pkA = ps[2 * par], psk[2 * par]
                    pB, pkB = ps[2 * par + 1], psk[2 * par + 1]
                    pA3 = pA.rearrange("p (j n) -> p j n", j=4)
                    pB3 = pB.rearrange("p (j n) -> p j n", j=4)
                    mzt = cmb[:, MZT[d], :]
                    for half in range(2):
                        pz3 = pA3 if half == 0 else pB3
                        pkz = pkA if half == 0 else pkB
                        for j in range(4):
                            h = half * 4 + j
                            c, hp = h // 2, h % 2
                            bk = BKt[:, c, ch, :, :].rearrange("p a t -> p (a t)")
                            S.op('pe', lambda: PE.matmul(pz3[:, j, :].rearrange("p (a t) -> p a t", a=2), lhsT=bk, rhs=ARz[:, c, ch, :, hp, :], start=True, stop=True),
                                 reads=['BKt', 'ARz'], writes=[pkz], pe_acc=True)
                        S.op('dve', lambda: V.tensor_tensor(out=ZTs[slot][half], in0=pz3, in1=bc(mzt.rearrange("p (o n) -> p o n", o=1), [128, 4, 128]), op=ALU.mult),
                             reads=[pkz, 'cmb'], writes=[('ZTs', slot, half)])
                    yield
                    for c in range(4):
                        bz = Bz[:, c, ch, :, :].rearrange("p a t -> p (a t)")
                        az = ARz[:, c, ch, 1, :, :].rearrange("p a t -> p (a t)")
                        S.op('pe', lambda: PE.matmul(pA3[:, c, :], lhsT=bz, rhs=az, start=True, stop=True), reads=['Bz', 'ARz'], writes=[pkA], pe_acc=True)
                        S.op('pe', lambda: PE.matmul(pB3[:, c, :], lhsT=az, rhs=bz, start=True, stop=True), reads=['Bz', 'ARz'], writes=[pkB], pe_acc=True)
                    S.op('dve', lambda: V.tensor_tensor(out=PTm[par][0], in0=pA3, in1=bc(MNT.rearrange("p (o n) -> p o n", o=1), [128, 4, 128]), op=ALU.mult),
                         reads=[pkA, 'cmb'], writes=[('PT', par, 0)])
                    S.op('dve', lambda: V.tensor_tensor(out=Pm[par][0], in0=pB3, in1=bc(MN.rearrange("p (o n) -> p o n", o=1), [128, 4, 128]), op=ALU.mult),
                         reads=[pkB, 'cmb'], writes=[('P', par, 0)])
                    S.op('pool', lambda: POOL.tensor_tensor(out=ATm[slot][0], in0=PTm[par][0], in1=ident4, op=ALU.add), reads=[('PT', par, 0), 'ident4'], writes=[('AT', slot, 0)])
                    S.op('pool', lambda: POOL.tensor_tensor(out=Am[par][0], in0=Pm[par][0], in1=ident4, op=ALU.add), reads=[('P', par, 0), 'ident4'], writes=[('A', par, 0)])
                    yield
                    cur = 0
                    for lev in range(1, 6):
                        nxt = 1 - cur
                        for j in range(4):
                            S.op('pe', lambda: PE.matmul(pA3[:, j, :], lhsT=Pm[par][cur][:, j, :], rhs=PTm[par][cur][:, j, :], start=True, stop=True),
                                 reads=[('P', par, cur), ('PT', par, cur)], writes=[pkA], pe_acc=True)
                            if lev < 5:
                                S.op('pe', lambda: PE.matmul(pB3[:, j, :], lhsT=PTm[par][cur][:, j, :], rhs=Pm[par][cur][:, j, :], start=True, stop=True),
                                     reads=[('P', par, cur), ('PT', par, cur)], writes=[pkB], pe_acc=True)
                        S.op('act', lambda: ACT.copy(out=PTm[par][nxt], in_=pA3), reads=[pkA], writes=[('PT', par, nxt)])
                        if lev < 5:
                            S.op('dve', lambda: V.tensor_copy(out=Pm[par][nxt], in_=pB3), reads=[pkB], writes=[('P', par, nxt)])
                        yield
                        for j in range(4):
                            S.op('pe', lambda: PE.matmul(pA3[:, j, :], lhsT=Am[par][cur][:, j, :], rhs=PTm[par][nxt][:, j, :], start=True, stop=True),
                                 reads=[('A', par, cur), ('PT', par, nxt)], writes=[pkA], pe_acc=True)
                            if lev < 5:
                                S.op('pe', lambda: PE.matmul(pB3[:, j, :], lhsT=PTm[par][nxt][:, j, :], rhs=Am[par][cur][:, j, :], start=True, stop=True),
                                     reads=[('A', par, cur), ('PT', par, nxt)], writes=[pkB], pe_acc=True)
                        S.op('dve', lambda: V.tensor_tensor(out=ATm[slot][nxt], in0=pA3, in1=ATm[slot][cur], op=ALU.add), reads=[pkA, ('AT', slot, cur)], writes=[('AT', slot, nxt)])
                        if lev < 5:
                            S.op('act', lambda: ACT.copy(out=Am[par][nxt], in_=pB3), reads=[pkB], writes=[('A', par, nxt)])
                            S.op('pool', lambda: POOL.tensor_tensor(out=Am[par][nxt], in0=Am[par][nxt], in1=Am[par][cur], op=ALU.add),
                                 reads=[('A', par, nxt), ('A', par, cur)], writes=[('A', par, nxt)])
                        yield
                        cur = nxt
                    assert cur == 1

                def gen_Q(ch, slot, pb, tb=tb, d=d):
                    nonlocal yev
                    fin = 1
                    gch = tb * NCH + ch
                    pW, pkW = ps[pb], psk[pb]
                    pW3 = pW[:, 0:256].rearrange("p (c v) -> p c v", c=4)
                    for h in range(8):
                        c, hp = h // 2, h % 2
                        S.op('pe', lambda: PE.matmul(pW3[hp * 64:(hp + 1) * 64, c, :], lhsT=ZTs[slot][h // 4][:, h % 4, 64:128], rhs=VZ[:, ch, h, :], start=True, stop=False),
                             reads=[('ZTs', slot, h // 4), ('VZ', ch)], writes=[pkW], pe_acc=True)
                        S.op('pe', lambda: PE.matmul(pW3[hp * 64:(hp + 1) * 64, c, :], lhsT=ARz[:, c, ch, 1, hp, :], rhs=Sb[d][:, c, :], start=False, stop=True),
                             reads=['ARz', ('Sb', d)], writes=[pkW], pe_acc=True)
                    S.op('act', lambda: ACT.copy(out=W1s, in_=pW3), reads=[pkW], writes=['W1s'])
                    yield
                    pX, pkX = ps[pb + 1], psk[pb + 1]
                    pX3 = pX.rearrange("p (h v) -> p h v", h=8)
                    for h in range(8):
                        c, hp = h // 2, h % 2
                        S.op('pe', lambda: PE.matmul(pX3[64:128, h, :], lhsT=ATm[slot][fin][:, c, hp * 64:(hp + 1) * 64], rhs=W1s[:, c, :], start=True, stop=True),
                             reads=[('AT', slot, fin), 'W1s'], writes=[pkX], pe_acc=True)
                    S.op('dve', lambda: V.tensor_copy(out=XV[64:128, ch, :, :], in_=pX3[64:128]), reads=[pkX], writes=[('XVu', ch)])
                    yield
                    pS, pkS = ps[pb + 3], psk[pb + 3]
                    pS3 = pS[:, 0:256].rearrange("p (c v) -> p c v", c=4)
                    for h in range(8):
                        c, hp = h // 2, h % 2
                        S.op('pe', lambda: PE.matmul(pS3[hp * 64:(hp + 1) * 64, c, :], lhsT=KBt[:, c, ch, hp * 64:(hp + 1) * 64], rhs=XV[:, ch, h, :], start=True, stop=True),
                             reads=[('KBt', ch), ('XVv', ch), ('XVu', ch)], writes=[pkS], pe_acc=True)
                    pY, pkY = ps[pb + 2], psk[pb + 2]
                    pY3 = pY.rearrange("p (h v) -> p h v", h=8)
                    for h in range(8):
                        c, hp = h // 2, h % 2
                        S.op('pe', lambda: PE.matmul(pY3[0:64, h, :], lhsT=ZTs[slot][h // 4][:, h % 4, 0:64], rhs=XV[:, ch, h, :], start=True, stop=False),
                             reads=[('ZTs', slot, h // 4), ('XVv', ch), ('XVu', ch)], writes=[pkY], pe_acc=True)
                        S.op('pe', lambda: PE.matmul(pY3[0:64, h, :], lhsT=ARz[:, c, ch, 0, hp, :], rhs=Sb[d][:, c, :], start=False, stop=True),
                             reads=['ARz', ('Sb', d)], writes=[pkY], pe_acc=True)
                    S.op('dve', lambda: V.tensor_tensor(out=S32[d], in0=S32[d], in1=bc(pdec[:, :, ch:ch + 1], [128, 4, 64]), op=ALU.mult),
                         reads=[('S32', d), 'pdec'], writes=[('S32', d)])
                    S.op('dve', lambda: V.tensor_tensor(out=Sb[d], in0=S32[d], in1=pS3, op=ALU.add),
                         reads=[('S32', d), pkS], writes=[('Sb', d)])
                    S.op('dve', lambda: V.tensor_tensor(out=S32[d], in0=S32[d], in1=pS3, op=ALU.add),
                         reads=[('S32', d), pkS], writes=[('S32', d)])
                    yb_ = ysb[yev % 2]
                    S.op('act', lambda: ACT.copy(out=yb_, in_=pY[0:64, :]), reads=[pkY], writes=[('ysb', yev % 2)])
                    S.dma('sp', y_d[d, gch * 64:(gch + 1) * 64, :], yb_, reads=[('ysb', yev % 2)], writes=[('yscr', d, gch // 2)])
                    yev += 1
                    yield

                chs = list(range(NCH)) if d == 0 else list(range(NCH - 1, -1, -1))
                Ds = [gen_D(chs[i_], i_, i_) for i_ in range(NCH)]
                fast, slow = Ds[0:2], Ds[2:4]
                alive = True
                while alive:
                    alive = False
                    for rep in range(2):
                        for t_ in fast:
                            if next(t_, 'done') != 'done':
                                alive = True
                    for t_ in slow:
                        next(t_, 'done')
                run_tasks([chain([gen_Q(chs[0], 0, 0), gen_Q(chs[1], 1, 0)])] + slow)
                pendQ = chain([gen_Q(chs[2], 2, 4), gen_Q(chs[3], 3, 4)])
        if pendQ is not None:
            run_tasks([pendQ])
            pendQ = None
        S.barrier()


    def phase_post(zsT, yaT, base):
        rT4 = zsT[:, 0:4, :]
        kT4 = zsT[:, 4:8, :]
        adT = zsT[:, 13, :]
        gdT = zsT[:, 14, :]
        AR.seek(base)
        gnwB = AR.alloc([128, 512], F32)
        gnbB = AR.alloc([128, 512], F32)
        P2 = lambda shape, dt: [AR.alloc(shape, dt) for _ in range(2)]
        Yf, Yb = P2([128, 512], F32), P2([128, 512], F32)
        ta0, ta1 = P2([128, 4, 128], F32), P2([128, 4, 128], F32)
        kf2 = P2([128, 4, 128], F32)
        prod2 = P2([128, 4, 128], BF16)
        rows2 = P2([128, 8], F32)
        bon2 = P2([128, 512], F32)
        y2 = P2([128, 512], F32)
        sq2 = P2([128, 512], F32)
        st2 = P2([128, 4, 8], F32)
        yab2 = P2([128, 512], BF16)
        S.dma('sp', gnwB, gnw_d.partition_broadcast(128), writes=['gnwB'])
        S.dma('sp', gnbB, gnb_d.partition_broadcast(128), writes=['gnbB'])
        def gen_tile(i):
            b = i % 2
            sl = slice(i * 128, (i + 1) * 128)
            ta = (ta0[b], ta1[b])
            kf, prod, rows, bon, y, sq, st, yab = kf2[b], prod2[b], rows2[b], bon2[b], y2[b], sq2[b], st2[b], yab2[b]
            bA, bB, bC, bD = 4 * b, 4 * b + 1, 4 * b + 2, 4 * b + 3
            S.dma('sp', Yf[b], y_d[0, sl, :], writes=[('Yf', b)])
            S.dma('sp', Yb[b], y_d[1, sl, :], writes=[('Yb', b)])
            for d in range(2):
                bk_ = bA if d == 0 else bB
                pz3 = ps[bk_].rearrange("p (c n) -> p c n", c=4)
                for c in range(4):
                    S.op('pe', lambda: PE.matmul(pz3[:, c, :], lhsT=a2c[:, d, c * 128:(c + 1) * 128], rhs=adT[:, sl], start=True, stop=True),
                         reads=['a2c'], writes=[psk[bk_]], pe_acc=True)
                for c in range(4):
                    S.op('act', lambda: ACT.activation(out=ta[d][:, c, :], in_=pz3[:, c, :], func=AF.Sigmoid, bias=col("a0", d * 4 + c), scale=1.0),
                         reads=[psk[bk_], 'pp'], writes=[('ta', b, d)])
            yield
            S.op('pool', lambda: POOL.tensor_tensor(out=ta[0], in0=ta[0], in1=ta[1], op=ALU.add), reads=[('ta', b, 0), ('ta', b, 1)], writes=[('ta', b, 0)])
            for c in range(4):
                S.op('pool', lambda: POOL.tensor_scalar(out=kf[:, c, :], in0=ta[0][:, c, :], scalar1=kar[:, c:c + 1], scalar2=c2r[:, c:c + 1], op0=ALU.mult, op1=ALU.add),
                     reads=[('ta', b, 0), 'kar'], writes=[('kf', b)])
            S.op('pool', lambda: POOL.tensor_tensor(out=kf, in0=kf, in1=kT4[:, :, sl], op=ALU.mult), reads=[('kf', b)], writes=[('kf', b)])
            S.op('pool', lambda: POOL.tensor_tensor(out=prod, in0=kf, in1=rT4[:, :, sl], op=ALU.mult), reads=[('kf', b)], writes=[('prod', b)])
            yield
            pr = ps[bB]
            for c in range(4):
                S.op('pe', lambda: PE.matmul(pr[:, c * 2:(c + 1) * 2], lhsT=prod[:, c, :], rhs=cmb[:, HSEL, 0:2], start=True, stop=True),
                     reads=[('prod', b), 'cmb'], writes=[psk[bB]], pe_acc=True)
            S.op('act', lambda: ACT.copy(out=rows, in_=pr[:, 0:8]), reads=[psk[bB]], writes=[('rows', b)])
            yield
            pv = ps[bD].bitcast(BF16)[:, 0:512]
            for c in range(4):
                S.op('pe', lambda: PE.transpose(out=pv[:, c * 128:(c + 1) * 128], in_=zsT[:, 8 + c, sl], identity=ident),
                     reads=['ident'], writes=[psk[bD]], pe_acc=True)
            S.op('dve', lambda: V.tensor_tensor(out=bon.rearrange("p (h v) -> p h v", h=8), in0=pv.rearrange("p (h v) -> p h v", h=8),
                                                in1=bc(rows.rearrange("p (h o) -> p h o", o=1), [128, 8, 64]), op=ALU.mult),
                 reads=[psk[bD], ('rows', b)], writes=[('bon', b)])
            yield
            pg = ps[bC]
            S.op('pe', lambda: PE.matmul(pg, lhsT=gdT[:, sl], rhs=g2, start=True, stop=True), reads=['g2'], writes=[psk[bC]], pe_acc=True)
            y3 = y.rearrange("p (h v) -> p h v", h=8)
            sq3 = sq.rearrange("p (h v) -> p h v", h=8)
            S.op('dve', lambda: V.tensor_tensor(out=y, in0=Yf[b], in1=Yb[b], op=ALU.add), reads=[('Yf', b), ('Yb', b)], writes=[('y', b)])
            yield
            S.op('dve', lambda: V.tensor_reduce(out=st[:, 0, :], in_=y3, axis=AX.X, op=ALU.add), reads=[('y', b)], writes=[('st0', b)])
            S.op('dve', lambda: V.tensor_scalar(out=st[:, 1, :], in0=st[:, 0, :], scalar1=-1.0 / 64, scalar2=None, op0=ALU.mult), reads=[('st0', b)], writes=[('st1', b)])
            S.op('dve', lambda: V.tensor_tensor(out=y3, in0=y3, in1=bc(st[:, 1, :].rearrange("p (h o) -> p h o", o=1), [128, 8, 64]), op=ALU.add),
                 reads=[('y', b), ('st1', b)], writes=[('y', b)])
            yield
            S.op('act', lambda: ACT.activation(out=sq, in_=y, func=AF.Square), reads=[('y', b)], writes=[('sq', b)])
            S.op('dve', lambda: V.tensor_reduce(out=st[:, 2, :], in_=sq3, axis=AX.X, op=ALU.add), reads=[('sq', b)], writes=[('st2', b)])
            yield
            S.op('act', lambda: ACT.activation(out=st[:, 3, :], in_=st[:, 2, :], func=AF.Sqrt, bias=epsc[:, 1:2], scale=1.0 / 64), reads=[('st2', b), 'epsc'], writes=[('st3', b)])
            S.op('dve', lambda: V.reciprocal(out=st[:, 3, :], in_=st[:, 3, :]), reads=[('st3', b)], writes=[('st3', b)])
            S.op('dve', lambda: V.tensor_tensor(out=y3, in0=y3, in1=bc(st[:, 3, :].rearrange("p (h o) -> p h o", o=1), [128, 8, 64]), op=ALU.mult),
                 reads=[('y', b), ('st3', b)], writes=[('y', b)])
            yield
            S.op('dve', lambda: V.tensor_tensor(out=y, in0=y, in1=gnwB, op=ALU.mult), reads=[('y', b), 'gnwB'], writes=[('y', b)])
            S.op('pool', lambda: POOL.tensor_tensor(out=bon, in0=bon, in1=gnbB, op=ALU.add), reads=[('bon', b), 'gnbB'], writes=[('bon', b)])
            S.op('dve', lambda: V.tensor_tensor(out=y, in0=y, in1=bon, op=ALU.add), reads=[('y', b), ('bon', b)], writes=[('y', b)])
            S.op('dve', lambda: V.tensor_tensor(out=yab, in0=y, in1=pg, op=ALU.mult), reads=[('y', b), psk[bC]], writes=[('yab', b)])
            yield
            pt = ps[bA].bitcast(BF16)[:, 0:512]
            for c in range(4):
                S.op('pe', lambda: PE.transpose(out=pt[:, c * 128:(c + 1) * 128], in_=yab[:, c * 128:(c + 1) * 128], identity=ident),
                     reads=[('yab', b), 'ident'], writes=[psk[bA]], pe_acc=True)
            S.op('act', lambda: ACT.copy(out=yaT[:, :, sl], in_=pt.rearrange("p (c n) -> p c n", c=4)), reads=[psk[bA]], writes=['yaT'])
            yield

        def run_tasks(tasks):
            tasks = list(tasks)
            while tasks:
                for t_ in list(tasks):
                    try:
                        next(t_)
                    except StopIteration:
                        tasks.remove(t_)

        for i in range(0, NT, 2):
            run_tasks([gen_tile(i), gen_tile(i + 1)])

    def phase_attn(s, uT, ybT, baseA, baseB):
        AR.seek(baseA)
        cosT = AR.alloc([128, T], F32)
        sinT = AR.alloc([128, T], F32)
        qT = AR.alloc([128, 4, T], BF16)
        kTt = AR.alloc([128, T], BF16)
        vp = AR.alloc([128, 2, NT, 128], BF16)
        AR.seek(baseB)
        wq = [AR.alloc([128, 8, 128], BF16) for _ in range(2)]
        qfL = [AR.alloc([128, 512], F32) for _ in range(2)]
        sqbL = [AR.alloc([128, 512], BF16) for _ in range(2)]
        rsL = [AR.alloc([128, 512], F32) for _ in range(2)]
        qnL = [AR.alloc([128, 512], F32) for _ in range(2)]
        qnbL = [AR.alloc([128, 512], BF16) for _ in range(2)]
        t1L = [AR.alloc([128, 512], F32) for _ in range(2)]
        t2L = [AR.alloc([128, 512], F32) for _ in range(2)]
        pTs = [AR.alloc([128, 512], BF16) for _ in range(6)]
        dn = AR.alloc([128, 512], F32)
        posi = AR.alloc([128, T], I32)
        ang = AR.alloc([128, T], F32)
        ki = AR.alloc([128, T], I32)
        kf = AR.alloc([128, T], F32)
        m1 = AR.alloc([128, T], F32)
        S.dma('sp', posi, pos_d[s].partition_broadcast(128), writes=['posi'])

        def table(dst, shift):
            S.op('dve', lambda: V.tensor_copy(out=ang, in_=posi), reads=['posi'], writes=['ang'])
            S.op('dve', lambda: V.tensor_scalar(out=ang, in0=ang, scalar1=col("invf"), scalar2=shift, op0=ALU.mult, op1=ALU.add), reads=['ang', 'pp'], writes=['ang'])
            S.op('dve', lambda: V.tensor_scalar(out=ki, in0=ang, scalar1=1.0 / TWO_PI, scalar2=None, op0=ALU.mult), reads=['ang'], writes=['ki'])
            S.op('pool', lambda: POOL.tensor_copy(out=kf, in_=ki), reads=['ki'], writes=['kf'])
            S.op('dve', lambda: V.scalar_tensor_tensor(out=ang, in0=kf, scalar=-C1, in1=ang, op0=ALU.mult, op1=ALU.add), reads=['kf', 'ang'], writes=['ang'])
            S.op('dve', lambda: V.scalar_tensor_tensor(out=ang, in0=kf, scalar=-C2, in1=ang, op0=ALU.mult, op1=ALU.add), reads=['kf', 'ang'], writes=['ang'])
            S.op('dve', lambda: V.tensor_scalar(out=m1, in0=ang, scalar1=float(np.pi), scalar2=-TWO_PI, op0=ALU.is_gt, op1=ALU.mult), reads=['ang'], writes=['m1'])
            S.op('pool', lambda: POOL.tensor_tensor(out=ang, in0=ang, in1=m1, op=ALU.add), reads=['ang', 'm1'], writes=['ang'])
            S.op('dve', lambda: V.tensor_scalar(out=m1, in0=ang, scalar1=float(-np.pi), scalar2=TWO_PI, op0=ALU.is_lt, op1=ALU.mult), reads=['ang'], writes=['m1'])
            S.op('pool', lambda: POOL.tensor_tensor(out=ang, in0=ang, in1=m1, op=ALU.add), reads=['ang', 'm1'], writes=['ang'])
            S.op('act', lambda: ACT.activation(out=dst, in_=ang, func=AF.Sin), reads=['ang'], writes=['tab'])

        table(sinT, 0.0)
        table(cosT, float(np.pi / 2))
        def c0_of(c):
            return 1920 + c * 128 if c < 4 else 2432

        def gen_qk(c, tb, L):
            b = c % 2
            gcol = col("qg") if c < 4 else col("kg")
            sl = slice(tb * 512, (tb + 1) * 512)
            qf_, sqb_, rs_, qn_, qnb_, t1_, t2_ = qfL[L], sqbL[L], rsL[L], qnL[L], qnbL[L], t1L[L], t2L[L]
            pz, pk = ps[L], psk[L]
            for k in range(8):
                S.op('pe', lambda: PE.matmul(pz, lhsT=wq[b][:, k, :], rhs=uT[:, k, sl], start=(k == 0), stop=(k == 7)),
                     reads=[('wq', b), ('uT', tb)], writes=[pk], pe_acc=True)
            S.op('act', lambda: ACT.copy(out=qf_, in_=pz), reads=[pk], writes=[('qf', L)])
            S.op('act', lambda: ACT.activation(out=sqb_, in_=qf_, func=AF.Square), reads=[('qf', L)], writes=[('sqb', L)])
            yield
            pr, pkr = ps[2 + L], psk[2 + L]
            S.op('pe', lambda: PE.matmul(pr, lhsT=cmb[:, BLK1, :], rhs=sqb_, start=True, stop=True), reads=[('sqb', L), 'cmb'], writes=[pkr], pe_acc=True)
            S.op('act', lambda: ACT.activation(out=rs_, in_=pr, func=AF.Sqrt, bias=epsc[:, 0:1], scale=1.0 / 64), reads=[pkr, 'epsc'], writes=[('rs', L)])
            yield
            S.op('dve', lambda: V.reciprocal(out=rs_, in_=rs_), reads=[('rs', L)], writes=[('rs', L)])
            S.op('dve', lambda: V.scalar_tensor_tensor(out=qn_, in0=qf_, scalar=gcol, in1=rs_, op0=ALU.mult, op1=ALU.mult), reads=[('qf', L), ('rs', L), 'pp'], writes=[('qn', L)])
            S.op('act', lambda: ACT.copy(out=qnb_, in_=qn_), reads=[('qn', L)], writes=[('qnb', L)])
            yield
            pro, pkro = ps[4 + L], psk[4 + L]
            S.op('pe', lambda: PE.matmul(pro, lhsT=cmb[:, ROT, :], rhs=qnb_, start=True, stop=True), reads=[('qnb', L), 'cmb'], writes=[pkro], pe_acc=True)
            S.op('pool', lambda: POOL.tensor_tensor(out=t1_, in0=qn_, in1=cosT[:, sl], op=ALU.mult), reads=[('qn', L), 'tab'], writes=[('t1', L)])
            yield
            S.op('dve', lambda: V.tensor_tensor(out=t2_, in0=pro, in1=sinT[:, sl], op=ALU.mult), reads=[pkro, 'tab'], writes=[('t2', L)])
            dst = qT[:, c, sl] if c < 4 else kTt[:, sl]
            S.op('dve', lambda: V.tensor_tensor(out=dst, in0=t1_, in1=t2_, op=ALU.add), reads=[('t1', L), ('t2', L)], writes=['qk'])
            yield

        def run_tasks(tasks):
            tasks = list(tasks)
            while tasks:
                for t_ in list(tasks):
                    try:
                        next(t_)
                    except StopIteration:
                        tasks.remove(t_)

        S.dma('pool', wq[0], win_d[:, c0_of(0):c0_of(0) + 128].rearrange("(k p) n -> p k n", p=128), writes=[('wq', 0)])
        for c in range(5):
            if c + 1 < 5:
                S.dma('pool', wq[(c + 1) % 2], win_d[:, c0_of(c + 1):c0_of(c + 1) + 128].rearrange("(k p) n -> p k n", p=128), writes=[('wq', (c + 1) % 2)])
            for tb in range(0, NB, 2):
                run_tasks([gen_qk(c, tb, 0), gen_qk(c, tb + 1, 1)])
        S.op('pool', lambda: POOL.memset(vp.rearrange("p a b c -> p (a b c)"), 0.0), writes=['vp'])
        S.dma('pool', wq[0], win_d[:, 2560:2688].rearrange("(k p) n -> p k n", p=128), writes=[('wq', 0)])
        for i in range(NT):
            pz, pk = ps[i % 2], psk[i % 2]
            for k in range(8):
                S.op('pe', lambda: PE.matmul(pz[:, 0:128], lhsT=uT[:, k, i * 128:(i + 1) * 128], rhs=wq[0][:, k, :], start=(k == 0), stop=(k == 7)),
                     reads=[('wq', 0), ('uT', i // 4)], writes=[pk], pe_acc=True)
            S.op('act', lambda: ACT.copy(out=vp[:, 0, i, 0:64], in_=pz[:, 0:64]), reads=[pk], writes=['vp'])
            S.op('dve', lambda: V.tensor_copy(out=vp[:, 1, i, 64:128], in_=pz[:, 64:128]), reads=[pk], writes=['vp'])
        for n in range(NT):
            qs = slice(n * 128, (n + 1) * 128)
            kbs = [kb for kb in (n - 1, n, n + 1) if 0 <= kb < NT]
            items = [(g, kb) for g in range(2) for kb in kbs]
            for idx, (g, kb) in enumerate(items):
                gp = slice(g * 64, (g + 1) * 64)
                pz, pk = ps[idx % 4], psk[idx % 4]
                S.op('pe', lambda: PE.matmul(pz.rearrange("p (j q) -> p j q", j=4), lhsT=kTt[gp, kb * 128:(kb + 1) * 128], rhs=qT[gp, :, qs], start=True, stop=True),
                     reads=['qk'], writes=[pk], pe_acc=True)
                pt_ = pTs[idx]
                S.op('act', lambda: ACT.activation(out=pt_, in_=pz, func=AF.Exp, scale=0.125), reads=[pk], writes=[('pT', idx)])
                if kb != n:
                    mk = cmb[:, MPREV if kb < n else MNEXT, :]
                    S.op('pool', lambda: POOL.tensor_tensor(out=pt_.rearrange("p (j q) -> p j q", j=4), in0=pt_.rearrange("p (j q) -> p j q", j=4),
                                                            in1=bc(mk.rearrange("p (o q) -> p o q", o=1), [128, 4, 128]), op=ALU.mult),
                         reads=[('pT', idx), 'cmb'], writes=[('pT', idx)])
            po, pko = ps[4 + n % 2], psk[4 + n % 2]
            pd_, pkd = ps[6 + n % 2], psk[6 + n % 2]
            for idx, (g, kb) in enumerate(items):
                S.op('pe', lambda: PE.matmul(po, lhsT=vp[:, g, kb, :], rhs=pTs[idx], start=(idx == 0), stop=(idx == len(items) - 1)),
                     reads=['vp', ('pT', idx)], writes=[pko], pe_acc=True)
            for idx, (g, kb) in enumerate(items):
                S.op('pe', lambda: PE.matmul(pd_, lhsT=cmb[:, VP[g], :], rhs=pTs[idx], start=(idx == 0), stop=(idx == len(items) - 1)),
                     reads=['cmb', ('pT', idx)], writes=[pkd], pe_acc=True)
            S.op('dve', lambda: V.tensor_tensor(out=dn.rearrange("p (j q) -> p j q", j=4), in0=pd_.rearrange("p (j q) -> p j q", j=4),
                                                in1=bc(esk.rearrange("p (j o) -> p j o", o=1), [128, 4, 128]), op=ALU.add), reads=[pkd, 'esk'], writes=['dn'])
            S.op('dve', lambda: V.reciprocal(out=dn, in_=dn), reads=['dn'], writes=['dn'])
            S.op('dve', lambda: V.tensor_tensor(out=ybT[:, :, qs], in0=po.rearrange("p (j q) -> p j q", j=4), in1=dn.rearrange("p (j q) -> p j q", j=4), op=ALU.mult),
                 reads=[pko, 'dn'], writes=['ybT'])

    def phase_merge(uT, yaT, ybT, mergedT, offs):
        AR.seek(offs[0])
        prw = AR.alloc([128, 4, D], BF16)
        AR.seek(offs[1])
        pat = AR.alloc([128, 4, D], BF16)
        wga = [AR.alloc([128, 8, 128], BF16) for _ in range(2)]
        wgb = [AR.alloc([128, 8, 128], BF16) for _ in range(2)]
        sgaP = [AR.alloc([128, 512], BF16) for _ in range(2)]
        sgbP = [AR.alloc([128, 512], BF16) for _ in range(2)]
        t1P = [AR.alloc([128, 512], F32) for _ in range(2)]
        t2P = [AR.alloc([128, 512], F32) for _ in range(2)]
        for hh in range(2):
            S.dma('pool', prw[:, hh * 2:(hh + 1) * 2, :], prw_d[hh * 256:(hh + 1) * 256, :].rearrange("(k p) n -> p k n", p=128), writes=['prw'])
            S.dma('pool', pat[:, hh * 2:(hh + 1) * 2, :], pat_d[hh * 256:(hh + 1) * 256, :].rearrange("(k p) n -> p k n", p=128), writes=['pat'])
        for oc in range(8):
            b = oc % 2
            S.dma('pool', wga[b], win_d[:, 2688 + oc * 128:2688 + (oc + 1) * 128].rearrange("(k p) n -> p k n", p=128), writes=[('wga', b)])
            S.dma('pool', wgb[b], win_d[:, 3712 + oc * 128:3712 + (oc + 1) * 128].rearrange("(k p) n -> p k n", p=128), writes=[('wgb', b)])
            for tb in range(NB):
                sl = slice(tb * 512, (tb + 1) * 512)
                L = tb % 2
                sga, sgb, t1, t2 = sgaP[L], sgbP[L], t1P[L], t2P[L]
                for k in range(8):
                    S.op('pe', lambda: PE.matmul(ps[0 + 4 * L], lhsT=wga[b][:, k, :], rhs=uT[:, k, sl], start=(k == 0), stop=(k == 7)),
                         reads=[('wga', b), ('uT', tb)], writes=[psk[0 + 4 * L]], pe_acc=True)
                S.op('act', lambda: ACT.activation(out=sga, in_=ps[0 + 4 * L], func=AF.Sigmoid), reads=[psk[0 + 4 * L]], writes=[('sga', L)])
                for k in range(8):
                    S.op('pe', lambda: PE.matmul(ps[1 + 4 * L], lhsT=wgb[b][:, k, :], rhs=uT[:, k, sl], start=(k == 0), stop=(k == 7)),
                         reads=[('wgb', b), ('uT', tb)], writes=[psk[1 + 4 * L]], pe_acc=True)
                S.op('act', lambda: ACT.activation(out=sgb, in_=ps[1 + 4 * L], func=AF.Sigmoid), reads=[psk[1 + 4 * L]], writes=[('sgb', L)])
                for k in range(4):
                    S.op('pe', lambda: PE.matmul(ps[2 + 4 * L], lhsT=prw[:, k, oc * 128:(oc + 1) * 128], rhs=yaT[:, k, sl], start=(k == 0), stop=(k == 3)),
                         reads=['prw', 'yaT'], writes=[psk[2 + 4 * L]], pe_acc=True)
                for k in range(4):
                    S.op('pe', lambda: PE.matmul(ps[3 + 4 * L], lhsT=pat[:, k, oc * 128:(oc + 1) * 128], rhs=ybT[:, k, sl], start=(k == 0), stop=(k == 3)),
                         reads=['pat', 'ybT'], writes=[psk[3 + 4 * L]], pe_acc=True)
                S.op('dve', lambda: V.tensor_tensor(out=t1, in0=ps[2 + 4 * L], in1=sga, op=ALU.mult), reads=[psk[2 + 4 * L], ('sga', L)], writes=[('t1', L)])
                S.op('dve', lambda: V.tensor_tensor(out=t2, in0=ps[3 + 4 * L], in1=sgb, op=ALU.mult), reads=[psk[3 + 4 * L], ('sgb', L)], writes=[('t2', L)])
                S.op('pool', lambda: POOL.tensor_tensor(out=mergedT[:, oc, sl], in0=t1, in1=t2, op=ALU.add), reads=[('t1', L), ('t2', L)], writes=[('mg', tb)])

    def phase_x1(s, mergedT, u2tm, base):
        AR.seek(base)
        wo = AR.alloc([128, 8, D], BF16)
        gt1B = AR.alloc([128, D], F32)
        sc2 = AR.alloc([128, D], F32)
        sh2 = AR.alloc([128, D], F32)
        xt = [AR.alloc([128, D], F32) for _ in range(2)]
        x1t = [AR.alloc([128, D], F32) for _ in range(2)]
        tmpP = [AR.alloc([128, D], F32) for _ in range(2)]
        junkP = [AR.alloc([128, D], BF16) for _ in range(2)]
        u2TP = [AR.alloc([128, 8, 128], BF16) for _ in range(2)]
        ssP = [AR.alloc([128, 8], F32) for _ in range(2)]
        exP = [AR.alloc([128, E], F32) for _ in range(2)]
        for hh in range(4):
            S.dma('pool', wo[:, hh * 2:(hh + 1) * 2, :], wout_d[hh * 256:(hh + 1) * 256, :].rearrange("(k p) n -> p k n", p=128), writes=['wo'])
        S.dma('sp', gt1B, mod_d[s, 2], writes=['gt1B'])
        S.dma('sp', sc2, mod_d[s, 4], writes=['sc2'])
        S.dma('sp', sh2, mod_d[s, 3], writes=['sh2'])
        lg = AR.alloc([128, NT, E], F32)
        mxs = AR.alloc([128, 3, NT], F32)

        def gen_x1(i):
            b = i % 2
            sl = slice(i * 128, (i + 1) * 128)
            S.dma('sp', xt[b], x_d[s, sl, :], writes=[('xt', b)])
            tmp, junk, u2T, ss = tmpP[b], junkP[b], u2TP[b], ssP[b]
            for cb in range(2):
                for k in range(8):
                    S.op('pe', lambda: PE.matmul(ps[cb + 6 * b], lhsT=mergedT[:, k, sl], rhs=wo[:, k, cb * 512:(cb + 1) * 512], start=(k == 0), stop=(k == 7)),
                         reads=[('mg', i // 4), 'wo'], writes=[psk[cb + 6 * b]], pe_acc=True)
                S.op('dve', lambda: V.tensor_tensor(out=tmp[:, cb * 512:(cb + 1) * 512], in0=ps[cb + 6 * b], in1=gt1B[:, cb * 512:(cb + 1) * 512], op=ALU.mult),
                     reads=[psk[cb + 6 * b], 'gt1B'], writes=[('tmp', b, cb)])
            yield
            S.op('pool', lambda: POOL.tensor_tensor(out=x1t[b], in0=tmp, in1=xt[b], op=ALU.add), reads=[('tmp', b, 0), ('tmp', b, 1), ('xt', b)], writes=[('x1t', b)])
            S.dma('sp', out_d[s, sl, :], x1t[b], reads=[('x1t', b)], writes=[('outd', i)])
            S.op('act', lambda: ACT.activation(out=junk, in_=x1t[b], func=AF.Square, accum_out=ss[:, 0:1]), reads=[('x1t', b)], writes=[('junk', b), ('ss0', b)])
            yield
            S.op('act', lambda: ACT.activation(out=ss[:, 1:2], in_=ss[:, 0:1], func=AF.Sqrt, bias=epsc[:, 0:1], scale=1.0 / D), reads=[('ss0', b), 'epsc'], writes=[('ss1', b)])
            S.op('dve', lambda: V.reciprocal(out=ss[:, 1:2], in_=ss[:, 1:2]), reads=[('ss1', b)], writes=[('ss1', b)])
            S.op('dve', lambda: V.scalar_tensor_tensor(out=tmp, in0=x1t[b], scalar=ss[:, 1:2], in1=sc2, op0=ALU.mult, op1=ALU.mult),
                 reads=[('x1t', b), ('ss1', b), 'sc2'], writes=[('tmp', b, 0), ('tmp', b, 1)])
            yield
            S.op('pool', lambda: POOL.tensor_tensor(out=u2tm[:, i, :], in0=tmp, in1=sh2, op=ALU.add), reads=[('tmp', b, 0), ('tmp', b, 1), 'sh2'], writes=[('u2', i)])
            pz = ps[2 + b].bitcast(BF16).rearrange("p (k t) -> p k t", k=8)
            pk = psk[2 + b]
            for k in range(8):
                S.op('pe', lambda: PE.transpose(out=pz[:, k, :], in_=u2tm[:, i, k * 128:(k + 1) * 128], identity=ident),
                     reads=[('u2', i), 'ident'], writes=[pk], pe_acc=True)
            yield
            S.op('act', lambda: ACT.copy(out=u2T, in_=pz), reads=[pk], writes=[('u2T', b)])
            pl, pkl = ps[4 + b], psk[4 + b]
            for k in range(8):
                S.op('pe', lambda: PE.matmul(pl[:, 0:E], lhsT=u2T[:, k, :], rhs=wr[:, k, :], start=(k == 0), stop=(k == 7)),
                     reads=[('u2T', b), 'wr'], writes=[pkl], pe_acc=True)
            yield
            S.op('act', lambda: ACT.copy(out=lg[:, i, :], in_=pl[:, 0:E]), reads=[pkl], writes=['lg'])
            yield

        def run_tasks(tasks):
            tasks = list(tasks)
            while tasks:
                for t_ in list(tasks):
                    try:
                        next(t_)
                    except StopIteration:
                        tasks.remove(t_)

        for i in range(0, NT, 2):
            run_tasks([gen_x1(i), gen_x1(i + 1)])
        S.op('dve', lambda: V.tensor_reduce(out=mxs[:, 0, :], in_=lg, axis=AX.X, op=ALU.max), reads=['lg'], writes=['mx0'])
        S.op('dve', lambda: V.tensor_tensor(out=lg, in0=lg, in1=bc(mxs[:, 0, :].rearrange("p (i o) -> p i o", o=1), [128, NT, E]), op=ALU.subtract),
             reads=['lg', 'mx0'], writes=['lg'])
        S.op('act', lambda: ACT.activation(out=lg, in_=lg, func=AF.Exp), reads=['lg'], writes=['lg'])
        S.op('dve', lambda: V.tensor_reduce(out=mxs[:, 1, :], in_=lg, axis=AX.X, op=ALU.add), reads=['lg'], writes=['mx1'])
        S.op('dve', lambda: V.reciprocal(out=mxs[:, 2, :], in_=mxs[:, 1, :]), reads=['mx1'], writes=['mx2'])
        S.op('dve', lambda: V.tensor_tensor(out=afftm, in0=lg, in1=bc(mxs[:, 2, :].rearrange("p (i o) -> p i o", o=1), [128, NT, E]), op=ALU.mult),
             reads=['lg', 'mx2'], writes=['afftm'])

    def phase_moe(s, u2tm, base):
        AR.seek(base + E * 2 * D * 2)
        for wsrc in (wg_d, wu_d, wd_d):
            wt0 = AR.alloc([128, 8, D], BF16)
            for hh in range(4):
                S.dma('pool', wt0[:, hh * 2:(hh + 1) * 2, :], wsrc[0, hh * 256:(hh + 1) * 256, :].rearrange("(k p) n -> p k n", p=128), writes=[('Wpre', hh)])
        AR.seek(base)
        affT = AR.alloc([16, T], F32)
        work = AR.alloc([16, T], F32)
        maskT = AR.alloc([16, T], F32)
        slotT = AR.alloc([16, T], F32)
        mx8 = AR.alloc([16, 8], F32)
        for i in range(NT):
            pz = ps[i // 4]
            S.op('pe', lambda: PE.transpose(out=pz[0:16, (i % 4) * 128:(i % 4 + 1) * 128], in_=afftm[:, i, :], identity=identf),
                 reads=['afftm', 'identf'], writes=[psk[i // 4]], pe_acc=True)
        for q in range(4):
            S.op('act', lambda: ACT.copy(out=affT[:, q * 512:(q + 1) * 512], in_=ps[q][0:16, :]), reads=[psk[q]], writes=['affT'])
        S.op('dve', lambda: V.tensor_copy(out=work, in_=affT), reads=['affT'], writes=['work'])
        for it in range(CAP // 8):
            S.op('dve', lambda: V.max(out=mx8, in_=work), reads=['work'], writes=['mx8'])
            if it < CAP // 8 - 1:
                S.op('dve', lambda: V.match_replace(out=work, in_to_replace=mx8, in_values=work, imm_value=-1.0), reads=['work', 'mx8'], writes=['work'])
        S.op('dve', lambda: V.tensor_scalar(out=maskT, in0=affT, scalar1=mx8[:, 7:8], scalar2=None, op0=ALU.is_ge), reads=['affT', 'mx8'], writes=['maskT'])
        S.op('pool', lambda: POOL.memset(work, 1.0), reads=['work'], writes=['work'])
        S.op('dve', lambda: V.tensor_tensor_scan(out=slotT, data0=work, data1=maskT, initial=0.0, op0=ALU.mult, op1=ALU.add), reads=['work', 'maskT'], writes=['slotT'])
        S.op('dve', lambda: V.tensor_tensor(out=slotT, in0=slotT, in1=maskT, op=ALU.mult), reads=['slotT', 'maskT'], writes=['slotT'])
        S.op('dve', lambda: V.tensor_scalar(out=slotT, in0=slotT, scalar1=-1.0, scalar2=None, op0=ALU.add), reads=['slotT'], writes=['slotT'])
        pz = ps[4]
        for i in range(NT):
            S.op('pe', lambda: PE.transpose(out=pz[:, i * 16:(i + 1) * 16], in_=slotT[:, i * 128:(i + 1) * 128], identity=identf[0:16, 0:16]),
                 reads=['slotT', 'identf'], writes=[psk[4]], pe_acc=True)
        S.op('act', lambda: ACT.copy(out=slot_tm.rearrange("p i e -> p (i e)"), in_=pz[:, 0:256]), reads=[psk[4]], writes=['slot_tm'])
        S.op('dve', lambda: V.tensor_copy(out=affhl[:, :, :, 0], in_=afftm), reads=['afftm'], writes=['affhl'])
        S.op('dve', lambda: V.tensor_tensor(out=affhl[:, :, :, 1], in0=afftm, in1=affhl[:, :, :, 0], op=ALU.subtract), reads=['afftm', 'affhl'], writes=['affhl'])
        S.barrier()
        AR.seek(base)
        ye = AR.alloc([128, E, 2, D], BF16)
        Wg = AR.alloc([128, 8, D], BF16)
        Wu = AR.alloc([128, 8, D], BF16)
        Wd = AR.alloc([128, 8, D], BF16)
        wbase = AR.ptr
        Pe = AR.alloc([128, NT, CAP], BF16)
        xeT = AR.alloc([128, 8, CAP], BF16)
        hT = AR.alloc([128, 8, CAP], BF16)
        hs = AR.alloc([128, CAP], F32)
        affs = AR.alloc([128, 4], F32)
        gt2B = AR.alloc([128, D], F32)
        S.dma('sp', gt2B, mod_d[s, 5], writes=['gt2B'])
        for e in range(E):
            for (wt, wsrc, nm) in ((Wg, wg_d, 'Wg'), (Wu, wu_d, 'Wu'), (Wd, wd_d, 'Wd')):
                if e == 0:
                    continue
                for hh in range(4):
                    S.dma('pool', wt[:, hh * 2:(hh + 1) * 2, :], wsrc[e, hh * 256:(hh + 1) * 256, :].rearrange("(k p) n -> p k n", p=128), writes=[(nm, hh)])
            for i in range(NT):
                S.op('dve', lambda: V.tensor_scalar(out=Pe[:, i, :], in0=iota_row, scalar1=slot_tm[:, i, e:e + 1], scalar2=None, op0=ALU.is_equal),
                     reads=['iota_row', 'slot_tm'], writes=[('Pe', i)])
            for fc in range(8):
                pz, pk = ps[fc // 2], psk[fc // 2]
                pzs = pz[:, (fc % 2) * 256:(fc % 2 + 1) * 256]
                for i in range(NT):
                    S.op('pe', lambda: PE.matmul(pzs, lhsT=u2tm[:, i, fc * 128:(fc + 1) * 128], rhs=Pe[:, i, :], start=(i == 0), stop=(i == NT - 1)),
                         reads=[('u2', i), ('Pe', i)], writes=[pk], pe_acc=True)
                S.op('act', lambda: ACT.copy(out=xeT[:, fc, :], in_=pzs), reads=[pk], writes=[('xeT', fc)])
            pa, pka = ps[4], psk[4]
            for half in range(2):
                for i in range(NT):
                    S.op('pe', lambda: PE.matmul(pa[:, half * 2:(half + 1) * 2], lhsT=Pe[:, i, half * 128:(half + 1) * 128], rhs=affhl[:, i, e, :], start=(i == 0), stop=(i == NT - 1)),
                         reads=[('Pe', i), 'affhl'], writes=[pka], pe_acc=True)
            S.op('dve', lambda: V.tensor_reduce(out=affs[:, 0:2], in_=pa[:, 0:4].rearrange("p (h t) -> p h t", t=2), axis=AX.X, op=ALU.add), reads=[pka], writes=['affs'])
            for fk in range(8):
                pg, pkg = ps[5], psk[5]
                pu, pku = ps[6], psk[6]
                for k in range(8):
                    S.op('pe', lambda: PE.matmul(pg[:, 0:CAP], lhsT=Wg[:, k, fk * 128:(fk + 1) * 128], rhs=xeT[:, k, :], start=(k == 0), stop=(k == 7)),
                         reads=[('Wg', k // 2), ('xeT', k)], writes=[pkg], pe_acc=True)
                for k in range(8):
                    S.op('pe', lambda: PE.matmul(pu[:, 0:CAP], lhsT=Wu[:, k, fk * 128:(fk + 1) * 128], rhs=xeT[:, k, :], start=(k == 0), stop=(k == 7)),
                         reads=[('Wu', k // 2), ('xeT', k)], writes=[pku], pe_acc=True)
                S.op('act', lambda: ACT.activation(out=hs, in_=pg[:, 0:CAP], func=AF.Silu), reads=[pkg], writes=['hs'])
                S.op('dve', lambda: V.tensor_tensor(out=hT[:, fk, :], in0=pu[:, 0:CAP], in1=hs, op=ALU.mult), reads=[pku, 'hs'], writes=[('hT', fk)])
            for half in range(2):
                for cb in range(2):
                    py, pky = ps[7] if (half * 2 + cb) % 2 else ps[4], psk[7] if (half * 2 + cb) % 2 else psk[4]
                    for fk in range(8):
                        S.op('pe', lambda: PE.matmul(py, lhsT=hT[:, fk, half * 128:(half + 1) * 128], rhs=Wd[:, fk, cb * 512:(cb + 1) * 512], start=(fk == 0), stop=(fk == 7)),
                             reads=[('hT', fk), ('Wd', fk // 2), 'affs'], writes=[pky], pe_acc=True)
                    S.op('dve', lambda: V.tensor_scalar(out=ye[:, e, half, cb * 512:(cb + 1) * 512], in0=py, scalar1=affs[:, half:half + 1], scalar2=None, op0=ALU.mult),
                         reads=[pky, 'affs'], writes=['ye'])
        S.barrier()
        AR.seek(wbase - 3 * 8 * D * 2)
        Pall = AR.alloc([128, E, CAP], BF16)
        PT = AR.alloc([128, 2 * E, 128], BF16)
        x1t = [AR.alloc([128, D], F32) for _ in range(2)]
        ot = [AR.alloc([128, D], F32) for _ in range(2)]
        for i in range(NT):
            b = i % 2
            sl = slice(i * 128, (i + 1) * 128)
            S.dma('sp', x1t[b], out_d[s, sl, :], reads=[('outd', i)], writes=[('x1t', b)])
            for e in range(E):
                S.op('dve', lambda: V.tensor_scalar(out=Pall[:, e, :], in0=iota_row, scalar1=slot_tm[:, i, e:e + 1], scalar2=None, op0=ALU.is_equal),
                     reads=['iota_row', 'slot_tm'], writes=[('Pall', e // 4)])
            for q in range(4):
                pz = ps[q].bitcast(BF16).rearrange("p (j t) -> p j t", j=8)
                for j in range(8):
                    idx = q * 8 + j
                    e, half = idx // 2, idx % 2
                    S.op('pe', lambda: PE.transpose(out=pz[:, j, :], in_=Pall[:, e, half * 128:(half + 1) * 128], identity=ident),
                         reads=[('Pall', e // 4), 'ident'], writes=[psk[q]], pe_acc=True)
                if q % 2 == 0:
                    S.op('act', lambda: ACT.copy(out=PT[:, q * 8:(q + 1) * 8, :], in_=pz), reads=[psk[q]], writes=[('PT', q)])
                else:
                    S.op('dve', lambda: V.tensor_copy(out=PT[:, q * 8:(q + 1) * 8, :], in_=pz), reads=[psk[q]], writes=[('PT', q)])
            for cb in range(2):
                po, pko = ps[4 + cb + 2 * (i % 2)], psk[4 + cb + 2 * (i % 2)]
                for idx in range(2 * E):
                    e, half = idx // 2, idx % 2
                    S.op('pe', lambda: PE.matmul(po, lhsT=PT[:, idx, :], rhs=ye[:, e, half, cb * 512:(cb + 1) * 512], start=(idx == 0), stop=(idx == 2 * E - 1)),
                         reads=[('PT', idx // 8), 'ye'], writes=[pko], pe_acc=True)
                S.op('dve', lambda: V.tensor_tensor(out=ot[b][:, cb * 512:(cb + 1) * 512], in0=po, in1=gt2B[:, cb * 512:(cb + 1) * 512], op=ALU.mult),
                     reads=[pko, 'gt2B'], writes=[('ot', b, cb)])
            S.op('dve', lambda: V.tensor_tensor(out=ot[b], in0=ot[b], in1=x1t[b], op=ALU.add), reads=[('ot', b, 0), ('ot', b, 1), ('x1t', b)], writes=[('ot', b, 0), ('ot', b, 1)])
            S.dma('sp', out_d[s, sl, :], ot[b], reads=[('ot', b, 0), ('ot', b, 1)], writes=[('outd', i)])

    def dbg_dump(src_ap, shape, key_reads=()):
        AR.seek(AR_TOP)
        t = AR.alloc(shape, F32)
        S.op('dve', lambda: V.tensor_copy(out=t, in_=src_ap), writes=['dbgt'])
        flat = t if len(shape) == 2 else t.rearrange("p a b -> p (a b)")
        S.dma('sp', dbg_d, flat, reads=['dbgt'])

    AR_TOP = 160 * 1024
    phase_adaln()
    for s in range(nseq):
        AR.seek(0)
        zsT = AR.alloc([128, 15, T], BF16)
        uT = AR.alloc([128, 8, T], BF16)
        base1 = AR.ptr
        phase_norm1(s, uT, base1)
        S.barrier()
        if dbg and dbg[0] == 'uT':
            dbg_dump(uT[:, :, 0:512], [128, 8, 512]); break
        for q in range(4):
            S.dma('sp', u_d[:, 2 * q:2 * q + 2, :], uT[:, 2 * q:2 * q + 2, :], reads=[('uT', 0), ('uT', 1), ('uT', 2), ('uT', 3)], writes=['uscr'])
        phase_rwkv_cols(uT, zsT, base1)
        S.barrier()
        if dbg and dbg[0] == 'zs':
            dbg_dump(zsT[:, :, 0:256], [128, 15, 256]); break
        AR.seek(61440)
        kkT = AR.alloc([128, 4, T], BF16)
        yaT = AR.alloc([128, 4, T], BF16)
        base3 = AR.ptr
        phase_scan(zsT, kkT, 77824)
        if dbg and dbg[0] == 'yscan':
            AR.seek(AR_TOP)
            t = AR.alloc([128, 2, 512], F32)
            S.dma('sp', t[:, 0, :], y_d[0, 0:128, :], writes=['dbgt'])
            S.dma('sp', t[:, 1, :], y_d[1, 0:128, :], writes=['dbgt'])
            S.dma('sp', dbg_d, t.rearrange("p a b -> p (a b)"), reads=['dbgt']); break
        phase_post(zsT, yaT, base3)
        S.barrier()
        if dbg and dbg[0] == 'yaT':
            dbg_dump(yaT[:, :, 0:512], [128, 4, 512]); break
        AR.seek(0)
        uT = AR.alloc([128, 8, T], BF16)
        AR.seek(94208)
        ybT = AR.alloc([128, 4, T], BF16)
        baseB = AR.ptr
        for q in range(4):
            S.dma('sp' if q % 2 == 0 else 'act', uT[:, 2 * q:2 * q + 2, :], u_d[:, 2 * q:2 * q + 2, :], writes=[('uT', 0), ('uT', 1), ('uT', 2), ('uT', 3)])
        phase_attn(s, uT, ybT, 32768, baseB)
        S.barrier()
        if dbg and dbg[0] == 'ybT':
            dbg_dump(ybT[:, :, 0:512], [128, 4, 512]); break
        AR.seek(32768)
        mergedT = AR.alloc([128, 8, T], BF16)
        phase_merge(uT, yaT, ybT, mergedT, (65536, baseB))
        S.barrier()
        if dbg and dbg[0] == 'merged':
            dbg_dump(mergedT[:, :, 0:512], [128, 8, 512]); break
        AR.seek(0)
        u2tm = AR.alloc([128, NT, D], BF16)
        phase_x1(s, mergedT, u2tm, 65536)
        S.barrier()
        if dbg and dbg[0] == 'aff':
            dbg_dump(afftm.rearrange("p i e -> p (i e)"), [128, 256]); break
        phase_moe(s, u2tm, 32768)
        S.barrier()

    S.finish('sp')
    print("ninstr", S.ninstr, "pe_incs", S.npe_inc, "arena hi", AR.hi)
    return nc


def _consts():
    cm = np.zeros((13, 128, 128), np.float32)
    p = np.arange(128)
    cm[0] = (p[:, None] // 64 == p[None, :] // 64).astype(np.float32)
    R = np.zeros((128, 128), np.float32)
    for blk in range(2):
        o = blk * 64
        for d_ in range(8):
            R[o + d_ + 8, o + d_] = -1.0
            R[o + d_, o + d_ + 8] = 1.0
    cm[1] = R
    cm[2] = (p[:, None] >= p[None, :]).astype(np.float32)
    cm[3] = (p[:, None] <= p[None, :]).astype(np.float32)
    s_ = (p % 64)[:, None]
    t_ = (p % 64)[None, :]
    a_col = (p[None, :] >= 64)
    fwd = np.where(a_col, s_ < t_, s_ <= t_)
    bwd = np.where(a_col, s_ > t_, s_ >= t_)
    cm[4] = fwd.astype(np.float32)
    cm[5] = bwd.astype(np.float32)
    cm[6] = cm[4].T
    cm[7] = cm[5].T
    cm[8][:, 0] = (p < 64)
    cm[8][:, 1] = (p >= 64)
    cm[9][:, 0:64] = 1.0
    cm[10][:, 64:128] = 1.0
    cm[11] = ((p % 64)[:, None] < (p % 64)[None, :]).astype(np.float32)
    cm[12] = ((p % 64)[:, None] > (p % 64)[None, :]).astype(np.float32)
    return np.ascontiguousarray(cm.transpose(1, 0, 2).reshape(128, 13 * 128))


def _prep_shared(inp):
    f = lambda a: np.ascontiguousarray(np.asarray(a, dtype=np.float32))
    L = 0
    w_in = f(inp["w_in"][L]).copy()
    qoff = 1920
    perm = []
    for c in range(4):
        perm += list(range(c * 64, (c + 1) * 64)) + list(range((4 + c) * 64, (5 + c) * 64))
    perm = np.array(perm)
    w_in[:, qoff:qoff + 512] = w_in[:, qoff:qoff + 512][:, perm]
    p_attn = f(inp["p_attn"][L])[perm, :]
    pp = np.zeros((128, NPP), np.float32)

    def put(name, arr):
        o, w = PP[name]
        pp[:, o:o + w] = arr

    chunked = lambda v: np.asarray(v, np.float32).reshape(-1, 128).T
    put("mp", chunked(inp["mu_prev"][L]))
    put("mn", chunked(inp["mu_next"][L]))
    put("w0", np.concatenate([chunked(inp["rwkv_w0"][L][0]), chunked(inp["rwkv_w0"][L][1])], 1))
    put("a0", np.concatenate([chunked(inp["rwkv_a0"][L][0]), chunked(inp["rwkv_a0"][L][1])], 1))
    put("kk", chunked(inp["rwkv_k_k"][L]))
    put("ka", chunked(inp["rwkv_k_a"][L]))
    put("rk", chunked(np.asarray(inp["rwkv_r_k"][L]).reshape(-1)))
    put("qg", np.tile(np.asarray(inp["q_norm_g"][L], np.float32), 2)[:, None])
    put("kg", np.tile(np.asarray(inp["k_norm_g"][L], np.float32), 2)[:, None])
    inv_freq = (500000.0 ** (-np.arange(0, 16, 2, dtype=np.float32) / 16)).astype(np.float32)
    invf = np.zeros(64, np.float32)
    invf[0:8] = inv_freq
    invf[8:16] = inv_freq
    put("invf", np.tile(invf, 2)[:, None])
    sink = np.asarray(inp["attn_sink"][L], np.float32)
    sk = np.zeros((128, 4), np.float32)
    for j in range(4):
        sk[0:64, j] = sink[j]
        sk[64:128, j] = sink[4 + j]
    put("sink", sk)
    w2cat = np.zeros((128, 2, 512), np.float32)
    a2cat = np.zeros((128, 2, 512), np.float32)
    for d_ in range(2):
        w2cat[d_ * 64:(d_ + 1) * 64, d_, :] = inp["rwkv_w2"][L][d_]
        a2cat[d_ * 64:(d_ + 1) * 64, d_, :] = inp["rwkv_a2"][L][d_]
    return {
        "w_ada": f(inp["w_ada"][L]), "b_ada": f(inp["b_ada"][L])[None, :] if np.asarray(inp["b_ada"][L]).ndim == 1 else f(inp["b_ada"][L]),
        "norm1_g": f(inp["norm1_g"][L]).reshape(1, D), "norm2_g": f(inp["norm2_g"][L]).reshape(1, D),
        "w_in": w_in, "pp": pp, "w2cat": w2cat.reshape(128, 1024), "a2cat": a2cat.reshape(128, 1024),
        "g2": f(inp["rwkv_g2"][L]), "gn_w": f(inp["rwkv_gn_w"][L]).reshape(1, 512), "gn_b": f(inp["rwkv_gn_b"][L]).reshape(1, 512),
        "p_rwkv": f(inp["p_rwkv"][L]), "p_attn": np.ascontiguousarray(p_attn), "w_out": f(inp["w_out"][L]),
        "w_router": f(inp["w_router"][L]), "w_gate": f(inp["w_gate"][L]), "w_up": f(inp["w_up"][L]), "w_down": f(inp["w_down"][L]),
        "cmats": _consts(),
    }


def _core_inputs(inp, shared, seqs):
    x = np.ascontiguousarray(np.asarray(inp["x"], np.float32)[seqs])
    c = np.asarray(inp["c"], np.float32)[seqs]
    cT = np.ascontiguousarray(c.reshape(len(seqs), 8, 128).transpose(0, 2, 1))
    pos = np.ascontiguousarray(np.asarray(inp["positions"]).astype(np.int32)[seqs][:, None, :])
    m = dict(shared)
    m.update({"x": x, "cT": cT, "pos": pos})
    return m


def kernel(**inputs):
    shared = _prep_shared(inputs)
    nc = build(NSEQ)
    in_maps = [_core_inputs(inputs, shared, list(range(i * NSEQ, (i + 1) * NSEQ))) for i in range(NCORES)]
    res = run_bass_kernel_spmd(nc, in_maps, core_ids=list(range(NCORES)))
    out = np.concatenate([np.asarray(r["out"]) for r in res.results], axis=0)
    return out.astype(np.float32)
```
